# Optimizing a Trainium2 kernel written in Bass

```python
import jax, jax.numpy as jnp
from jax import lax
import numpy as np

D_MODEL = 1024
BATCH = 2
SEQ = 8192
DEPTH = 1

GRID_W = 64
CTX_LEN = 256
N_HEADS = 8
N_KV_HEADS = 2
HEAD_DIM = 64
GQA_GROUP = N_HEADS // N_KV_HEADS
ATT_WIDTH = N_HEADS * HEAD_DIM
KV_WIDTH = N_KV_HEADS * HEAD_DIM
CONV_WIDTH = D_MODEL - ATT_WIDTH
CONV_GROUPS = 8
CONV_K = 3
WINDOW = 128
BLOCK = 128
ROPE_THETA = 10000.0
N_EXPERTS = 16
EC_CAPACITY_FACTOR = 2
D_EXPERT = 1024
EPS = 1e-6
IN_WIDTH = ATT_WIDTH + 2 * KV_WIDTH + 3 * CONV_WIDTH
SPLIT_POINTS = [ATT_WIDTH, ATT_WIDTH + KV_WIDTH, ATT_WIDTH + 2 * KV_WIDTH,
                ATT_WIDTH + 2 * KV_WIDTH + CONV_WIDTH, ATT_WIDTH + 2 * KV_WIDTH + 2 * CONV_WIDTH]

kernel_name = "hybrid_dit_swa_shortconv_ecmoe"


def rmsnorm(x, g):
    xf = x.astype(jnp.float32)
    r = xf * lax.rsqrt(jnp.mean(xf * xf, axis=-1, keepdims=True) + EPS)
    return (r * g.astype(jnp.float32)).astype(x.dtype)


def modulate(h, shift, scale):
    return h * (1 + scale) + shift


def axial_rope_tables(seq_len):
    rows = seq_len // GRID_W
    row = jnp.repeat(jnp.arange(rows), GRID_W, total_repeat_length=rows * GRID_W)
    col = jnp.tile(jnp.arange(GRID_W), rows)
    n_freq = HEAD_DIM // 4
    inv = ROPE_THETA ** (-jnp.arange(n_freq, dtype=jnp.float32) / n_freq)
    ang = jnp.concatenate([row.astype(jnp.float32)[:, None] * inv,
                           col.astype(jnp.float32)[:, None] * inv], axis=-1)
    return jnp.cos(ang), jnp.sin(ang)


def apply_rope(x, cos, sin):
    xf = x.astype(jnp.float32)
    half = HEAD_DIM // 2
    x1, x2 = xf[..., :half], xf[..., half:]
    cs, sn = cos[None, :, None, :], sin[None, :, None, :]
    return jnp.concatenate([x1 * cs - x2 * sn, x1 * sn + x2 * cs], axis=-1).astype(x.dtype)


def windowed_attention(q, k, v, k_ctx, v_ctx, sink):
    bsz, seq, _, hd = q.shape
    nb = seq // BLOCK
    scale = hd ** -0.5
    qb = q.reshape(bsz, nb, BLOCK, N_KV_HEADS, GQA_GROUP, hd)
    pad = ((0, 0), (BLOCK, BLOCK), (0, 0), (0, 0))
    kp = jnp.pad(k, pad).reshape(bsz, nb + 2, BLOCK, N_KV_HEADS, hd)
    vp = jnp.pad(v, pad).reshape(bsz, nb + 2, BLOCK, N_KV_HEADS, hd)
    kb = jnp.concatenate([kp[:, :-2], kp[:, 1:-1], kp[:, 2:]], axis=2)
    vb = jnp.concatenate([vp[:, :-2], vp[:, 1:-1], vp[:, 2:]], axis=2)
    s_loc = jnp.einsum('bnqkgd,bnjkd->bnkgqj', qb, kb).astype(jnp.float32) * scale
    s_ctx = jnp.einsum('bnqkgd,bjkd->bnkgqj', qb, k_ctx).astype(jnp.float32) * scale
    i = jnp.arange(BLOCK)[None, :, None]
    j = jnp.arange(3 * BLOCK)[None, None, :]
    kpos = jnp.arange(nb)[:, None, None] * BLOCK + j - BLOCK
    valid = (jnp.abs(j - BLOCK - i) <= WINDOW) & (kpos >= 0) & (kpos < seq)
    s_loc = jnp.where(valid[None, :, None, None], s_loc, -jnp.inf)
    sk = jnp.broadcast_to(sink.astype(jnp.float32).reshape(N_KV_HEADS, GQA_GROUP)[None, None, :, :, None, None],
                          s_loc.shape[:-1] + (1,))
    p = jax.nn.softmax(jnp.concatenate([sk, s_ctx, s_loc], axis=-1), axis=-1)
    n_ctx = k_ctx.shape[1]
    p_ctx = p[..., 1:1 + n_ctx].astype(v.dtype)
    p_loc = p[..., 1 + n_ctx:].astype(v.dtype)
    out = (jnp.einsum('bnkgqj,bjkd->bnqkgd', p_ctx, v_ctx)
           + jnp.einsum('bnkgqj,bnjkd->bnqkgd', p_loc, vb))
    return out.reshape(bsz, seq, N_HEADS * hd)


def context_attention(q, k, v, sink):
    bsz, n, _, hd = q.shape
    qg = q.reshape(bsz, n, N_KV_HEADS, GQA_GROUP, hd)
    s = jnp.einsum('bqkgd,bjkd->bkgqj', qg, k).astype(jnp.float32) * hd ** -0.5
    sk = jnp.broadcast_to(sink.astype(jnp.float32).reshape(N_KV_HEADS, GQA_GROUP)[None, :, :, None, None],
                          s.shape[:-1] + (1,))
    p = jax.nn.softmax(jnp.concatenate([sk, s], axis=-1), axis=-1)[..., 1:].astype(v.dtype)
    return jnp.einsum('bkgqj,bjkd->bqkgd', p, v).reshape(bsz, n, N_HEADS * hd)


def short_conv_mixer(bg, cg, hv, conv_w):
    u = cg * hv
    up = jnp.pad(u, ((0, 0), (1, 1), (0, 0)))
    y = conv_w[0] * up[:, :-2] + conv_w[1] * up[:, 1:-1] + conv_w[2] * up[:, 2:]
    return bg * y


def expert_choice_ffn(h, w_router, w_gate, w_up, w_down):
    bsz, n, d = h.shape
    cap = EC_CAPACITY_FACTOR * n // N_EXPERTS
    aff = jax.nn.softmax((h @ w_router).astype(jnp.float32), axis=-1)
    g, idx = lax.top_k(jnp.swapaxes(aff, 1, 2), cap)
    xs = jax.vmap(lambda hb, ib: hb[ib])(h, idx)
    a = jnp.einsum('becd,edf->becf', xs, w_gate)
    u = jnp.einsum('becd,edf->becf', xs, w_up)
    y = jnp.einsum('becf,efd->becd', jax.nn.silu(a) * u, w_down) * g[..., None].astype(h.dtype)
    flat = (idx + (jnp.arange(bsz) * n)[:, None, None]).reshape(-1)
    out = jax.ops.segment_sum(y.reshape(-1, d), flat, num_segments=bsz * n)
    return out.reshape(bsz, n, d)


def setup_inputs(seed: int = 0) -> dict:
    key = jax.random.key(seed)
    ks = jax.random.split(key, 17)
    D = D_MODEL
    nrm = jax.random.normal
    f32 = jnp.float32
    return {
        "x": nrm(ks[0], (BATCH, SEQ, D), f32),
        "c": nrm(ks[1], (BATCH, D), f32),
        "ctx": nrm(ks[2], (BATCH, CTX_LEN, D), f32),
        "c_ctx": nrm(ks[3], (D,), f32),
        "w_ada": nrm(ks[4], (DEPTH, D, 6 * D), f32) * (0.5 * D ** -0.5),
        "b_ada": nrm(ks[5], (DEPTH, 6 * D), f32) * 0.01,
        "g_mix": 1.0 + 0.1 * nrm(ks[6], (DEPTH, D), f32),
        "w_in": nrm(ks[7], (DEPTH, D, IN_WIDTH), f32) * D ** -0.5,
        "conv_w": nrm(ks[8], (DEPTH, CONV_K, CONV_WIDTH), f32) * CONV_K ** -0.5,
        "sink": nrm(ks[9], (DEPTH, N_HEADS), f32),
        "w_out": nrm(ks[10], (DEPTH, D, D), f32) * D ** -0.5,
        "g_ffn": 1.0 + 0.1 * nrm(ks[11], (DEPTH, D), f32),
        "w_router": nrm(ks[12], (DEPTH, D, N_EXPERTS), f32) * D ** -0.5,
        "w_gate": nrm(ks[13], (DEPTH, N_EXPERTS, D, D_EXPERT), f32) * D ** -0.5,
        "w_up": nrm(ks[14], (DEPTH, N_EXPERTS, D, D_EXPERT), f32) * D ** -0.5,
        "w_down": nrm(ks[15], (DEPTH, N_EXPERTS, D_EXPERT, D), f32) * D_EXPERT ** -0.5,
        "g_final": 1.0 + 0.1 * nrm(ks[16], (D,), f32),
    }


def reference(x, c, ctx, c_ctx, w_ada, b_ada, g_mix, w_in, conv_w, sink, w_out, g_ffn,
              w_router, w_gate, w_up, w_down, g_final):
    bsz, seq, _ = x.shape
    cos, sin = axial_rope_tables(seq)
    for l in range(DEPTH):
        last = l == DEPTH - 1
        mod = jax.nn.silu(c) @ w_ada[l] + b_ada[l]
        sh1, sc1, gt1, sh2, sc2, gt2 = [m[:, None, :] for m in jnp.split(mod, 6, axis=-1)]
        mod_c = jax.nn.silu(c_ctx) @ w_ada[l] + b_ada[l]
        csh1, csc1, cgt1, csh2, csc2, cgt2 = jnp.split(mod_c, 6, axis=-1)

        hc = modulate(rmsnorm(ctx, g_mix[l]), csh1, csc1)
        n_ctx = ctx.shape[1]
        if last:
            kv_c = hc @ w_in[l][:, ATT_WIDTH:ATT_WIDTH + 2 * KV_WIDTH]
            k_c, v_c = jnp.split(kv_c, 2, axis=-1)
            k_c = k_c.reshape(bsz, n_ctx, N_KV_HEADS, HEAD_DIM)
            v_c = v_c.reshape(bsz, n_ctx, N_KV_HEADS, HEAD_DIM)
        else:
            q_c, k_c, v_c, bg_c, cg_c, hv_c = jnp.split(hc @ w_in[l], SPLIT_POINTS, axis=-1)
            q_c = q_c.reshape(bsz, n_ctx, N_HEADS, HEAD_DIM)
            k_c = k_c.reshape(bsz, n_ctx, N_KV_HEADS, HEAD_DIM)
            v_c = v_c.reshape(bsz, n_ctx, N_KV_HEADS, HEAD_DIM)
            att_c = context_attention(q_c, k_c, v_c, sink[l])
            conv_c = short_conv_mixer(bg_c, cg_c, hv_c, conv_w[l])
            ctx_new = ctx + cgt1 * (jnp.concatenate([att_c, conv_c], axis=-1) @ w_out[l])
            hc2 = modulate(rmsnorm(ctx_new, g_ffn[l]), csh2, csc2)
            ctx_new = ctx_new + cgt2 * expert_choice_ffn(hc2, w_router[l], w_gate[l], w_up[l], w_down[l])

        h = modulate(rmsnorm(x, g_mix[l]), sh1, sc1)
        q, k, v, bg, cg, hv = jnp.split(h @ w_in[l], SPLIT_POINTS, axis=-1)
        q = apply_rope(q.reshape(bsz, seq, N_HEADS, HEAD_DIM), cos, sin)
        k = apply_rope(k.reshape(bsz, seq, N_KV_HEADS, HEAD_DIM), cos, sin)
        v = v.reshape(bsz, seq, N_KV_HEADS, HEAD_DIM)
        att = windowed_attention(q, k, v, k_c, v_c, sink[l])
        conv = short_conv_mixer(bg, cg, hv, conv_w[l])
        x = x + gt1 * (jnp.concatenate([att, conv], axis=-1) @ w_out[l])

        h2 = modulate(rmsnorm(x, g_ffn[l]), sh2, sc2)
        x = x + gt2 * expert_choice_ffn(h2, w_router[l], w_gate[l], w_up[l], w_down[l])

        if not last:
            ctx = ctx_new
    return rmsnorm(x, g_final)
```

```python
import os
import numpy as np
import concourse.bass as bass
import concourse.mybir as mybir
from concourse.bass_utils import run_bass_kernel_spmd

F32 = mybir.dt.float32
BF16 = mybir.dt.bfloat16
I32 = mybir.dt.int32
ALU = mybir.AluOpType
AF = mybir.ActivationFunctionType
AX = mybir.AxisListType

COMPUTE = ("tensor", "vector", "scalar", "gpsimd")
QUEUES = ("sync",)
NCORES = 8
GROUPS = [[0, 1, 2, 3], [4, 5, 6, 7]]
W = 2304
NT = 16
NIT = 7


class Prog:
    def __init__(self, nc):
        self.nc = nc
        self.streams = {e: [] for e in COMPUTE + QUEUES}
        self.cnt = {e: 0 for e in COMPUTE}
        self.dma_cnt = {}
        self.waited = {}
        self.res = {}
        self.sem_handles = {}
        self.final_events = []
        self.sb_off = 16512
        self.sb_top = 229344

    def sb(self, name, shape, dtype, off=None):
        esz = {F32: 4, BF16: 2, I32: 4}[dtype]
        n = 1
        for s in shape[1:]:
            n *= s
        nbytes = (n * esz + 63) // 64 * 64
        if off is None:
            off = self.sb_off
            self.sb_off += nbytes
        assert off >= 16512 and off + nbytes <= self.sb_top, (name, off, nbytes)
        return self.nc.alloc_sbuf_tensor_at(name, list(shape), dtype, offset=off)

    def _deps(self, reads, writes):
        need = []
        for r in reads:
            st = self.res.get(r)
            if st and st["w"] is not None:
                need.append(st["w"])
        for w in writes:
            st = self.res.get(w)
            if st:
                if st["w"] is not None:
                    need.append(st["w"])
                need.extend(st["r"])
        return need

    def _commit(self, ev, reads, writes):
        for r in reads:
            st = self.res.setdefault(r, {"w": None, "r": []})
            st["r"].append(ev)
        for w in writes:
            self.res[w] = {"w": ev, "r": []}

    def _waits(self, eng, need):
        best = {}
        for (k, v) in need:
            if k == "tensor" and eng == "tensor":
                continue
            if v > best.get(k, 0):
                best[k] = v
        out = []
        for k, v in best.items():
            if self.waited.get((eng, k), 0) >= v:
                continue
            self.waited[(eng, k)] = v
            out.append((k, v))
        return out

    def op(self, eng, fn, reads=(), writes=()):
        need = self._deps(reads, writes)
        waits = self._waits(eng, need)
        self.cnt[eng] += 1
        ev = (eng, self.cnt[eng])
        self.streams[eng].append((waits, fn, (eng, 1)))
        self._commit(ev, reads, writes)
        return ev

    def dma(self, q, sem, fn, reads=(), writes=(), inc=16):
        need = self._deps(reads, writes)
        waits = self._waits(q, need)
        self.dma_cnt[sem] = self.dma_cnt.get(sem, 0) + inc
        ev = (sem, self.dma_cnt[sem])
        self.streams[q].append((waits, fn, (sem, inc)))
        self._commit(ev, reads, writes)
        return ev

    def finish(self, eng, events):
        self.final_events.append((eng, events))

    def check_deadlock(self):
        sem = {}
        pos = {e: 0 for e in self.streams}
        progressed = True
        while progressed:
            progressed = False
            for e, st in self.streams.items():
                while pos[e] < len(st):
                    waits, fn, inc = st[pos[e]]
                    if all(sem.get(k, 0) >= v for (k, v) in waits):
                        sem[inc[0]] = sem.get(inc[0], 0) + inc[1]
                        pos[e] += 1
                        progressed = True
                    else:
                        break
        stuck = {e: (pos[e], len(st), st[pos[e]][0]) for e, st in self.streams.items() if pos[e] < len(st)}
        assert not stuck, ("DEADLOCK", stuck, {k: sem.get(k) for e in stuck for (k, v) in stuck[e][2]})

    def emit(self):
        self.check_deadlock()
        nc = self.nc
        names = set(COMPUTE)
        for e in self.streams:
            for (waits, fn, inc) in self.streams[e]:
                names.add(inc[0])
                for (k, v) in waits:
                    names.add(k)
        for n in sorted(names):
            self.sem_handles[n] = nc.alloc_semaphore("s_" + n)
        H = self.sem_handles
        fin = {}
        for eng, evs in self.final_events:
            fin.setdefault(eng, []).extend(evs)
        with nc.Block() as block:
            def make(ename):
                def body(e):
                    for (waits, fn, inc) in self.streams[ename]:
                        for (k, v) in waits:
                            e.wait_ge(H[k], v)
                        fn(e).then_inc(H[inc[0]], inc[1])
                    best = {}
                    for (k, v) in fin.get(ename, []):
                        best[k] = max(best.get(k, 0), v)
                    for k, v in best.items():
                        e.wait_ge(H[k], v)
                return body
            for ename in self.streams:
                if not self.streams[ename] and ename not in fin:
                    continue
                getattr(block, ename)(make(ename))


def build_nc(stage=99, dbg=()):
    nc = bass.Bass("TRN2", target_bir_lowering=False)
    P = Prog(nc)
    dbg_outs = {}

    def din(name, shape, dt=F32):
        return nc.dram_tensor(name, list(shape), dt, kind="ExternalInput").ap()

    x = din("x", [W, 1024])
    ctx = din("ctx", [256, 1024])
    cc = din("cc", [128, 16])
    meta = din("meta", [128, 80])
    w_ada = din("w_ada", [1024, 6144])
    b_ada = din("b_ada", [6144])
    g_mix = din("g_mix", [1024])
    g_ffn = din("g_ffn", [1024])
    g_final = din("g_final", [1024])
    w_in = din("w_in", [1024, 2304])
    conv_w = din("conv_w", [3, 512])
    sink = din("sink", [8])
    w_out = din("w_out", [1024, 1024])
    w_router = din("w_router", [1024, 16])
    w_gate = din("w_gate", [16, 1024, 1024])
    w_up = din("w_up", [16, 1024, 1024])
    w_down = din("w_down", [16, 1024, 1024])
    out = nc.dram_tensor("out", [2048, 1024], F32, kind="ExternalOutput").ap()

    x1d = nc.dram_tensor("x1d", [2048, 1024], F32).ap()
    h2loc = nc.dram_tensor("h2loc", [2048, 1024], BF16).ap()
    h2all = nc.dram_tensor("h2all", [8192, 1024], BF16).ap()
    affloc = nc.dram_tensor("affloc", [16, 2048], F32).ap()
    affall = nc.dram_tensor("affall", [64, 2048], F32).ap()
    tabd = nc.dram_tensor("tabd", [640, 128], F32).ap()
    accd = nc.dram_tensor("accd", [2048, 1024], F32).ap()

    def dbg_out(name, shape, dt=F32):
        t = nc.dram_tensor("dbg_" + name, list(shape), dt, kind="ExternalOutput").ap()
        dbg_outs[name] = t
        return t

    def ACT(out_, in_, func, r, w, **kw):
        return P.op("scalar", lambda e: e.activation(out=out_, in_=in_, func=func, **kw), r, w)

    def TT(eng, out_, in0, in1, op, r, w):
        return P.op(eng, lambda e: e.tensor_tensor(out=out_, in0=in0, in1=in1, op=op), r, w)

    def TS(eng, out_, in0, s1, s2, op0, op1, r, w):
        if op1 is None:
            return P.op(eng, lambda e: e.tensor_scalar(out=out_, in0=in0, scalar1=s1, scalar2=None, op0=op0), r, w)
        return P.op(eng, lambda e: e.tensor_scalar(out=out_, in0=in0, scalar1=s1, scalar2=s2, op0=op0, op1=op1), r, w)

    def STT(eng, out_, in0, scalar, in1, op0, op1, r, w):
        return P.op(eng, lambda e: e.scalar_tensor_tensor(out=out_, in0=in0, scalar=scalar, in1=in1, op0=op0, op1=op1), r, w)

    def RED(eng, out_, in_, op, r, w):
        return P.op(eng, lambda e: e.tensor_reduce(out=out_, in_=in_, axis=AX.X, op=op), r, w)

    def CP(eng, out_, in_, r, w):
        return P.op(eng, lambda e: e.tensor_copy(out=out_, in_=in_), r, w)

    def MSET(eng, out_, val, w):
        return P.op(eng, lambda e: e.memset(out_, val), (), w)

    def MM(out_, lhsT, rhs, start, stop, r, w):
        return P.op("tensor", lambda e: e.matmul(out_, lhsT, rhs, start=start, stop=stop), r, w)

    def TR(out_, in_, ident, r, w):
        return P.op("tensor", lambda e: e.transpose(out_, in_, ident), r, w)

    def DMA(q, sem, out_, in_, r, w):
        return P.dma(q, sem, lambda e: e.dma_start(out=out_, in_=in_), r, w)

    PSF = [nc.alloc_psum_tensor(f"psf{i}", [128, 512], F32) for i in range(6)]
    PSB = [nc.alloc_psum_tensor(f"psb{i}", [128, 1024], BF16) for i in range(2)]
    psf_rr = {"v": 0, "a": 0}

    def psf(cons):
        i = psf_rr[cons] % 3 + (0 if cons == "v" else 3)
        psf_rr[cons] += 1
        return PSF[i], f"psf{i}"

    psb_rr = [0]

    def psb():
        i = psb_rr[0] % 2
        psb_rr[0] += 1
        return PSB[i], f"psb{i}"

    ident_f = P.sb("ident_f", [128, 128], F32)
    ident_b = P.sb("ident_b", [128, 128], BF16)
    iot = P.sb("iot", [128, 128], F32)
    ones_b = P.sb("ones_b", [128, 128], BF16)
    U_b = P.sb("U_b", [128, 128], BF16)
    UI_b = P.sb("UI_b", [128, 128], BF16)
    mask3 = P.sb("mask3", [128, 3, 384], BF16)
    metat = P.sb("metat", [128, 80], F32)
    esink = P.sb("esink", [128, 8], F32)
    rows = {}
    for nm in ("S1", "G1", "GT1", "S2", "G2", "GT2", "cS1", "cG1"):
        rows[nm] = P.sb("row_" + nm, [128, 1024], F32)
    REG0 = P.sb_off

    P.op("gpsimd", lambda e: e.iota(iot[:], pattern=[[1, 128]], base=0, channel_multiplier=-1,
                                    allow_small_or_imprecise_dtypes=True), (), ["iot"])
    TS("vector", ident_f[:], iot[:], 0.0, None, ALU.is_equal, None, ["iot"], ["ident_f"])
    CP("vector", ident_b[:], ident_f[:], ["ident_f"], ["ident_b"])
    TS("vector", U_b[:], iot[:], 0.0, None, ALU.is_ge, None, ["iot"], ["U_b"])
    MSET("vector", ones_b[:], 1.0, ["ones_b"])
    DMA("sync", "d_meta", metat[:], meta, [], ["metat"])
    DMA("sync", "d_sink", esink[:], sink.partition_broadcast(128), [], ["esink"])
    ACT(esink[:], esink[:], AF.Exp, ["esink"], ["esink"])
    for v in range(3):
        TS("vector", mask3[:, v, 0:128], iot[:], 0.0, None, ALU.is_le, None, ["iot"], [("mask3", v)])
        MSET("vector", mask3[:, v, 128:256], 1.0, [("mask3", v, 1)])
        TS("vector", mask3[:, v, 256:384], iot[:], 0.0, None, ALU.is_ge, None, ["iot"], [("mask3", v, 2)])
    TS("vector", mask3[:, 1, 0:128], mask3[:, 1, 0:128], metat[:, 0:1], None, ALU.mult, None,
       ["metat", ("mask3", 1)], [("mask3", 1)])
    TS("vector", mask3[:, 2, 256:384], mask3[:, 2, 256:384], metat[:, 1:2], None, ALU.mult, None,
       ["metat", ("mask3", 2, 2)], [("mask3", 2, 2)])

    if "const" in dbg:
        d1 = dbg_out("ident", [128, 128])
        d2 = dbg_out("mask3", [128, 3 * 384], BF16)
        d3 = dbg_out("esink", [128, 8])
        e1 = DMA("sync", "d_dbg", d1, ident_f[:], ["ident_f"], ["dbg1"])
        e2 = DMA("sync", "d_dbg", d2, mask3[:].rearrange("p a b -> p (a b)"),
                 [("mask3", v) for v in range(3)] + [("mask3", v, 1) for v in range(3)] + [("mask3", v, 2) for v in range(3)], ["dbg2"])
        e3 = DMA("sync", "d_dbg", d3, esink[:], ["esink"], ["dbg3"])
        P.finish("sync", [e1, e2, e3])
    if stage <= 0:
        P.emit()
        return nc, dbg_outs

    o = REG0
    WIN = P.sb("WIN", [128, 8, 2944], BF16, off=o)
    mixT = P.sb("mixT", [128, 8, 2048], BF16, off=o)
    o += 47104
    COS = P.sb("COS", [128, W], F32, off=o); o += W * 4
    SINS = P.sb("SINS", [128, W], F32, off=o); o += W * 4
    xt = [P.sb(f"xt{i}", [128, 1024], F32, off=o + i * 4096) for i in range(2)]; o += 8192
    tf = P.sb("tf", [128, 1024], F32, off=o); o += 4096
    hb = [P.sb(f"hb{i}", [128, 1024], BF16, off=o + i * 2048) for i in range(2)]; o += 4096
    hT = [P.sb(f"hT{i}", [128, 8, 512], BF16, off=o + i * 8192) for i in range(2)]
    wo = P.sb("wo", [128, 8, 1024], BF16, off=o)
    o += 16384
    qT_off = o
    qT = P.sb("qT", [128, 4, W], BF16, off=o); o += 4 * W * 2
    kT_off = o
    kT = P.sb("kT", [128, W], BF16, off=o); o += W * 2
    Vt = P.sb("Vt", [128, 18, 2, 65], BF16, off=o); o += 4736
    kcT = P.sb("kcT", [128, 256], BF16, off=o); o += 512
    Vc = P.sb("Vc", [128, 2, 2, 65], BF16, off=o); o += 576
    bgT_off = o
    bgT = P.sb("bgT", [128, 4, 2048], BF16, off=o)
    stg = P.sb("stg", [128, 8, 640], F32, off=o)
    o += 20480
    uT_off = o
    uT = P.sb("uT", [128, 4, W], BF16, off=o); o += 4 * W * 2
    rt1_off = o
    rt1 = P.sb("rt1", [128, 512], F32, off=o); o += 2048
    rt2 = P.sb("rt2", [128, 512], F32, off=o); o += 2048
    cgs = P.sb("cgs", [128, 512], F32, off=o); o += 2048
    small = P.sb("small", [128, 64], F32, off=o); o += 256
    cw = P.sb("cw", [128, 4, 3], F32, off=o); o += 64
    assert o <= P.sb_top, o
    A_END = o

    wa = [P.sb("wa0", [128, 8, 1024], BF16, off=qT_off), P.sb("wa1", [128, 8, 1024], BF16, off=uT_off)]
    o = kT_off
    lb = P.sb("lb", [128, 8, 2, 128], BF16, off=o); o += 4096
    brow = P.sb("brow", [128, 1024], F32, off=o); o += 4096
    cct = P.sb("cct", [128, 8, 2], F32, off=o); o += 64
    scl = P.sb("scl", [128, 8, 2], F32, off=o); o += 64
    assert o <= bgT_off
    gmrow = P.sb("gmrow", [128, 1024], F32, off=rt1_off)

    DMA("sync", "d_cc", cct[:], cc.rearrange("p (k v) -> p k v", v=2), [], ["cct"])
    ACT(scl[:], cct[:], AF.Silu, ["cct"], ["scl"])
    for v in range(2):
        CP("vector", lb[:, :, v, :], scl[:, :, v:v + 1].to_broadcast([128, 8, 128]), ["scl"], [("lb", v)])
    if stage <= 0.3:
        d1 = dbg_out("lb", [128, 8 * 2 * 128], BF16)
        e1 = DMA("sync", "d_dbg", d1, lb[:].rearrange("p a b c -> p (a b c)"), [("lb", 0), ("lb", 1)], ["dbg1"])
        P.finish("sync", [e1])
        P.emit()
        return nc, dbg_outs
    w_ada_v = w_ada.rearrange("(k p) n -> p k n", p=128)
    grp = [(0, [("S1", 0), ("cS1", 1)]), (1, [("G1", 0), ("cG1", 1)]), (2, [("GT1", 0)]),
           (3, [("S2", 0)]), (4, [("G2", 0)]), (5, [("GT2", 0)])]
    for gi, (g, uses) in enumerate(grp):
        wb = wa[gi % 2]
        wn = f"wa{gi % 2}"
        P.dma("gpsimd", "d_" + wn, (lambda wb=wb, g=g: (lambda e: e.dma_start(out=wb[:], in_=w_ada_v[:, :, g * 1024:(g + 1) * 1024])))(),
              [], [wn])
        DMA("sync", "d_brow", brow[:], b_ada[g * 1024:(g + 1) * 1024].partition_broadcast(128), [], ["brow"])
        if stage <= 0.5:
            d1 = dbg_out("wa", [128, 8 * 1024], BF16)
            d2 = dbg_out("brow", [128, 1024])
            e1 = DMA("sync", "d_dbg", d1, wb[:].rearrange("p a b -> p (a b)"), [wn], ["dbg1"])
            e2 = DMA("sync", "d_dbg", d2, brow[:], ["brow"], ["dbg2"])
            P.finish("sync", [e1, e2])
            P.emit()
            return nc, dbg_outs
        for (nm, v) in uses:
            for n in range(2):
                ps, psn = psf("v")
                for k in range(8):
                    MM(ps[:], lb[:, k, v, :], wb[:, k, n * 512:(n + 1) * 512], k == 0, k == 7,
                       [("lb", v), wn], [psn])
                TT("vector", rows[nm][:, n * 512:(n + 1) * 512], ps[:], brow[:, n * 512:(n + 1) * 512], ALU.add,
                   [psn, "brow"], [("row", nm, n)])
                if stage <= 0.7:
                    d1 = dbg_out("r0", [128, 512])
                    e1 = DMA("sync", "d_dbg", d1, rows[nm][:, 0:512], [("row", nm, n)], ["dbg1"])
                    P.finish("sync", [e1])
                    P.emit()
                    return nc, dbg_outs
    for (gsrc, names) in (((g_mix, ("G1", "cG1")), (g_ffn, ("G2",))) if stage > 0.8 else ()):
        DMA("sync", "d_gmrow", gmrow[:], gsrc.partition_broadcast(128), [], ["gmrow"])
        for nm in names:
            TS("vector", rows[nm][:], rows[nm][:], 1.0, None, ALU.add, None,
               [("row", nm, 0), ("row", nm, 1)], [("row", nm, 0), ("row", nm, 1)])
            TT("vector", rows[nm][:], rows[nm][:], gmrow[:], ALU.mult,
               [("row", nm, 0), ("row", nm, 1), "gmrow"], [("row", nm, 0), ("row", nm, 1)])

    def rowdeps(nm):
        return [("row", nm, 0), ("row", nm, 1)]

    if "rows" in dbg:
        d = dbg_out("rows", [8, 128, 1024])
        for i, nm in enumerate(("S1", "G1", "GT1", "S2", "G2", "GT2", "cS1", "cG1")):
            ev = DMA("sync", "d_dbg", d[i], rows[nm][:], rowdeps(nm), ["dbg"])
        P.finish("sync", [ev])
    if stage <= 1:
        P.emit()
        return nc, dbg_outs


    def sc(i):
        return small[:, i:i + 1]
    pid, dd, i32_, isC, ff, inv, invC, invR, sgn, tmpc = [sc(i) for i in range(10)]
    P.op("gpsimd", lambda e: e.iota(small[:, 0:1], pattern=[[0, 1]], base=0, channel_multiplier=1,
                                    allow_small_or_imprecise_dtypes=True), (), ["small"])
    TS("vector", tmpc, pid, 64.0, -64.0, ALU.is_ge, ALU.mult, ["small"], ["small"])
    TT("vector", dd, pid, tmpc, ALU.add, ["small"], ["small"])
    TS("vector", sgn, dd, 32.0, None, ALU.is_ge, None, ["small"], ["small"])
    TS("vector", tmpc, sgn, -32.0, None, ALU.mult, None, ["small"], ["small"])
    TT("vector", i32_, dd, tmpc, ALU.add, ["small"], ["small"])
    TS("vector", isC, i32_, 16.0, None, ALU.is_ge, None, ["small"], ["small"])
    TS("vector", tmpc, isC, -16.0, None, ALU.mult, None, ["small"], ["small"])
    TT("vector", ff, i32_, tmpc, ALU.add, ["small"], ["small"])
    ACT(inv, ff, AF.Exp, ["small"], ["small"], scale=-float(np.log(10000.0) / 16.0))
    TT("vector", invC, inv, isC, ALU.mult, ["small"], ["small"])
    TT("vector", invR, inv, invC, ALU.subtract, ["small"], ["small"])
    TS("vector", sgn, sgn, 2.0, -1.0, ALU.mult, ALU.add, ["small"], ["small"])
    rrA = P.sb("rrA", [128, W], F32, off=qT_off)
    rrI = P.sb("rrI", [128, W], I32, off=qT_off + W * 4)
    ang = P.sb("ang", [128, W], F32, off=uT_off)
    P.op("gpsimd", lambda e: e.iota(COS[:], pattern=[[1, 36], [0, 64]], base=0, channel_multiplier=0,
                                    allow_small_or_imprecise_dtypes=True), (), ["COS"])
    P.op("gpsimd", lambda e: e.iota(SINS[:], pattern=[[0, 36], [1, 64]], base=0, channel_multiplier=0,
                                    allow_small_or_imprecise_dtypes=True), (), ["SINS"])
    HW_ = W // 2
    TWO_PI = float(2 * np.pi)
    for hh in range(2):
        sl = slice(hh * HW_, (hh + 1) * HW_)
        TS("vector", COS[:, sl], COS[:, sl], metat[:, 2:3], None, ALU.add, None, ["COS", "metat"], ["COS"])
        TS("vector", COS[:, sl], COS[:, sl], invR, None, ALU.mult, None, ["COS", "small"], ["COS"])
        TS("vector", SINS[:, sl], SINS[:, sl], invC, None, ALU.mult, None, ["SINS", "small"], ["SINS"])
    TT("vector", ang[:], COS[:], SINS[:], ALU.add, ["COS", "SINS"], ["ang"])

    def range_reduce_sin(dst, dstn, offset):
        TS("vector", rrA[:], ang[:], 1.0 / TWO_PI, offset / TWO_PI + 8.5, ALU.mult, ALU.add, ["ang"], ["rrA"])
        CP("vector", rrI[:], rrA[:], ["rrA"], ["rrI"])
        CP("vector", rrA[:], rrI[:], ["rrI"], ["rrA"])
        TS("vector", rrA[:], rrA[:], -TWO_PI, 8 * TWO_PI + offset, ALU.mult, ALU.add, ["rrA"], ["rrA"])
        TT("vector", dst[:], ang[:], rrA[:], ALU.add, ["ang", "rrA"], [dstn])
        TS("vector", rrA[:], dst[:], float(np.pi), -TWO_PI, ALU.is_gt, ALU.mult, [dstn], ["rrA"])
        TT("vector", dst[:], dst[:], rrA[:], ALU.add, [dstn, "rrA"], [dstn])
        TS("vector", rrA[:], dst[:], -float(np.pi), TWO_PI, ALU.is_lt, ALU.mult, [dstn], ["rrA"])
        TT("vector", dst[:], dst[:], rrA[:], ALU.add, [dstn, "rrA"], [dstn])
        ACT(dst[:], dst[:], AF.Sin, [dstn], [dstn])

    range_reduce_sin(SINS, "SINS", 0.0)
    range_reduce_sin(COS, "COS", float(np.pi / 2))
    for hh in range(2):
        sl = slice(hh * HW_, (hh + 1) * HW_)
        TS("vector", SINS[:, sl], SINS[:, sl], sgn, None, ALU.mult, None, ["SINS", "small"], ["SINS"])

    w_in_v = w_in.rearrange("(k p) n -> p k n", p=128)
    DMA("sync", "d_stg", stg[:], w_in_v[:, :, 0:640], [], ["stg"])
    qd = WIN[:, :, 0:512].rearrange("p k (c h d) -> p k c h d", c=4, h=2, d=64)
    qs = stg[:, :, 0:512].rearrange("p k (h c d) -> p k c h d", h=2, c=4, d=64)
    for h in range(2):
        ACT(qd[:, :, :, h, :], qs[:, :, :, h, :], AF.Copy, ["stg"], [("WIN", "q", h)])
    qd2 = WIN[:, :, 512:1024].rearrange("p k (c h s d) -> p k c h s d", c=4, h=2, s=2, d=32)
    qs2 = stg[:, :, 0:512].rearrange("p k (h c s d) -> p k c h s d", h=2, c=4, s=2, d=32)
    for h in range(2):
        for s in range(2):
            ACT(qd2[:, :, :, h, s, :], qs2[:, :, :, h, 1 - s, :], AF.Copy, ["stg"], [("WIN", "qsw", h, s)])
    ACT(WIN[:, :, 1024:1152], stg[:, :, 512:640], AF.Copy, ["stg"], [("WIN", "k")])
    kd2 = WIN[:, :, 1152:1280].rearrange("p k (h s d) -> p k h s d", h=2, s=2, d=32)
    ks2 = stg[:, :, 512:640].rearrange("p k (h s d) -> p k h s d", h=2, s=2, d=32)
    for s in range(2):
        ACT(kd2[:, :, :, s, :], ks2[:, :, :, 1 - s, :], AF.Copy, ["stg"], [("WIN", "ksw", s)])
    WINQ = [("WIN", "q", 0), ("WIN", "q", 1)]
    WINQS = [("WIN", "qsw", h, s) for h in range(2) for s in range(2)]
    WINK = [("WIN", "k")]
    WINKS = [("WIN", "ksw", 0), ("WIN", "ksw", 1)]
    for (nm, d0, s0, n) in (("v", 1280, 640, 128), ("bg", 1408, 768, 512), ("cg", 1920, 1280, 512), ("hv", 2432, 1792, 512)):
        P.dma("gpsimd", "d_win_" + nm, (lambda d0=d0, s0=s0, n=n: (lambda e: e.dma_start(out=WIN[:, :, d0:d0 + n], in_=w_in_v[:, :, s0:s0 + n])))(),
              [], [("WIN", nm)])
    for kk in range(3):
        for c4 in range(4):
            P.dma("sync", "d_cw", (lambda kk=kk, c4=c4: (lambda e: e.dma_start(
                out=cw[:, c4, kk:kk + 1], in_=conv_w[kk, c4 * 128:(c4 + 1) * 128].rearrange("(p o) -> p o", o=1))))(),
                [], [("cw", kk, c4)])
    MSET("vector", Vt[:, :, :, 64:65], 1.0, [("Vt", "ones")])
    MSET("vector", Vc[:, :, :, 64:65], 1.0, [("Vc", "ones")])

    xt_rr = [0]

    def norm_mod(src_rows, Gn, Sn, hbuf, hname, extra_r=()):
        i = xt_rr[0] % 2
        xt_rr[0] += 1
        xtile, xn = xt[i], f"xt{i}"
        DMA("sync", "d_" + xn, xtile[:], src_rows, list(extra_r), [xn])
        norm_mod_sb(xtile, xn, Gn, Sn, hbuf, hname)
        return xtile, xn

    def norm_mod_sb(xtile, xn, Gn, Sn, hbuf, hname):
        ss = small[:, 16:17]
        rstd = small[:, 17:18]
        ACT(tf[:], xtile[:], AF.Square, [xn], ["tf"])
        RED("vector", ss, tf[:], ALU.add, ["tf"], ["ss"])
        TS("vector", rstd, ss, 1.0 / 1024.0, 1e-6, ALU.mult, ALU.add, ["ss"], ["rstd"])
        ACT(rstd, rstd, AF.Ln, ["rstd"], ["rstd"])
        ACT(rstd, rstd, AF.Exp, ["rstd"], ["rstd"], scale=-0.5)
        ACT(tf[:], xtile[:], AF.Copy, [xn, "rstd"], ["tf"], scale=rstd)
        TT("vector", tf[:], tf[:], rows[Gn][:], ALU.mult, ["tf"] + rowdeps(Gn), ["tf"])
        TT("gpsimd", hbuf[:], tf[:], rows[Sn][:], ALU.add, ["tf"] + rowdeps(Sn), [hname])

    def transpose_to(hbuf, hname, dst, dst_name):
        pb, pbn = psb()
        pbv = pb[:].rearrange("p (k t) -> p k t", k=8)
        for k in range(8):
            TR(pbv[:, k, :], hbuf[:, k * 128:(k + 1) * 128], ident_b[:], [hname, "ident_b"], [(pbn, k)])
        ACT(dst, pbv, AF.Copy, [(pbn, k) for k in range(8)], [dst_name])

    hcT = hT[0]
    for t in range(2):
        norm_mod(ctx[t * 128:(t + 1) * 128, :], "cG1", "cS1", hb[t % 2], f"hb{t % 2}")
        transpose_to(hb[t % 2], f"hb{t % 2}", hcT[:, :, t * 128:(t + 1) * 128], ("hT0", t))
    ps, psn = psf("a")
    for k in range(8):
        MM(ps[:, 0:256], WIN[:, k, 1024:1152], hcT[:, k, 0:256], k == 0, k == 7,
           WINK + [("hT0", 0), ("hT0", 1)], [psn])
    ACT(kcT[:], ps[:, 0:256], AF.Copy, [psn], ["kcT"])
    for t in range(2):
        ps, psn = psf("a")
        for k in range(8):
            MM(ps[:, 0:128], hcT[:, k, t * 128:(t + 1) * 128], WIN[:, k, 1280:1408], k == 0, k == 7,
               [("WIN", "v"), ("hT0", t)], [psn])
        ACT(Vc[:, t, :, 0:64], ps[:, 0:128].rearrange("p (h d) -> p h d", h=2), AF.Copy, [psn], [("Vc", t)])

    if "ctx" in dbg:
        d1 = dbg_out("kcT", [128, 256], BF16)
        d2 = dbg_out("Vc", [128, 2 * 2 * 65], BF16)
        e1 = DMA("sync", "d_dbg", d1, kcT[:], ["kcT"], ["dbg1"])
        e2 = DMA("sync", "d_dbg", d2, Vc[:].rearrange("p a b c -> p (a b c)"), [("Vc", 0), ("Vc", 1), ("Vc", "ones")], ["dbg2"])
        P.finish("sync", [e1, e2])
    if stage <= 2:
        P.emit()
        return nc, dbg_outs

    chunks = [(0, 128, False)] + [(128 + 512 * i, 512, True) for i in range(4)] + [(2176, 128, False)]
    NCONV = int(os.environ.get("MK_NCONV", "4"))
    for ci, (w0, n, central) in enumerate(chunks):
        if (stage <= 2.5 and ci >= 1) or (stage <= 2.7 and ci >= 2):
            break
        hTc, hTn = hT[ci % 2], f"hT{ci % 2}"
        ntile = n // 128
        for t in range(ntile):
            j = (ci * 4 + t) % 2
            norm_mod(x[w0 + t * 128:w0 + (t + 1) * 128, :], "G1", "S1", hb[j], f"hb{j}")
            transpose_to(hb[j], f"hb{j}", hTc[:, :, t * 128:(t + 1) * 128], (hTn, t))
        hdeps = [(hTn, t) for t in range(ntile)]

        def proj(col0, wdeps, cons):
            ps, psn = psf(cons)
            for k in range(8):
                MM(ps[:, 0:n], WIN[:, k, col0:col0 + 128], hTc[:, k, 0:n], k == 0, k == 7, wdeps + hdeps, [psn])
            return ps, psn

        def rope_out(col0, colsw, wd, wsd, dst, dstn):
            pa, pan = proj(col0, wd, "v")
            pb_, pbn_ = proj(colsw, wsd, "v")
            TT("vector", rt1[:, 0:n], pa[:, 0:n], COS[:, w0:w0 + n], ALU.mult, [pan, "COS"], ["rt1"])
            TT("vector", rt2[:, 0:n], pb_[:, 0:n], SINS[:, w0:w0 + n], ALU.mult, [pbn_, "SINS"], ["rt2"])
            TT("gpsimd", dst, rt1[:, 0:n], rt2[:, 0:n], ALU.add, ["rt1", "rt2"], [dstn])

        if central:
            for c in range(4):
                rope_out(c * 128, 512 + c * 128, WINQ, WINQS, qT[:, c, w0:w0 + n], ("qT", c, ci))
        PARTS = os.environ.get("MK_PARTS", "rvc")
        if "r" in PARTS:
            rope_out(1024, 1152, WINK, WINKS, kT[:, w0:w0 + n], ("kT", ci))
        for t in (range(ntile) if "v" in PARTS else ()):
            ps, psn = psf("a")
            for k in range(8):
                MM(ps[:, 0:128], hTc[:, k, t * 128:(t + 1) * 128], WIN[:, k, 1280:1408], k == 0, k == 7,
                   [("WIN", "v"), (hTn, t)], [psn])
            wt = w0 // 128 + t
            ACT(Vt[:, wt, :, 0:64], ps[:, 0:128].rearrange("p (h d) -> p h d", h=2), AF.Copy, [psn], [("Vt", wt)])
        for c in (range(NCONV) if "c" in PARTS else ()):
            if central:
                ps, psn = proj(1408 + c * 128, [("WIN", "bg")], "a")
                ACT(bgT[:, c, w0 - 128:w0 - 128 + n], ps[:, 0:n], AF.Copy, [psn], [("bgT", c, ci)])
            pc, pcn = proj(1920 + c * 128, [("WIN", "cg")], "a")
            ph, phn = proj(2432 + c * 128, [("WIN", "hv")], "v")
            ACT(cgs[:, 0:n], pc[:, 0:n], AF.Copy, [pcn], ["cgs"])
            TT("vector", uT[:, c, w0:w0 + n], ph[:, 0:n], cgs[:, 0:n], ALU.mult, [phn, "cgs"], [("uT", c, ci)])

    if "proj" in dbg:
        d1 = dbg_out("qT", [128, 4 * W], BF16)
        d2 = dbg_out("kT", [128, W], BF16)
        d3 = dbg_out("Vt", [128, 18 * 130], BF16)
        d4 = dbg_out("uT", [128, 4 * W], BF16)
        d5 = dbg_out("bgT", [128, 4 * 2048], BF16)
        allq = [("qT", c, ci) for c in range(4) for ci in range(1, 5)]
        allk = [("kT", ci) for ci in range(6)]
        allv = [("Vt", t) for t in range(18)] + [("Vt", "ones")]
        allu = [("uT", c, ci) for c in range(4) for ci in range(6)]
        allb = [("bgT", c, ci) for c in range(4) for ci in range(1, 5)]
        evs = [DMA("sync", "d_dbg", d1, qT[:].rearrange("p a b -> p (a b)"), allq, ["dbg1"]),
               DMA("sync", "d_dbg", d2, kT[:], allk, ["dbg2"]),
               DMA("sync", "d_dbg", d3, Vt[:].rearrange("p a b c -> p (a b c)"), allv, ["dbg3"]),
               DMA("sync", "d_dbg", d4, uT[:].rearrange("p a b -> p (a b)"), allu, ["dbg4"]),
               DMA("sync", "d_dbg", d5, bgT[:].rearrange("p a b -> p (a b)"), allb, ["dbg5"])]
        P.finish("sync", evs)
    if stage <= 3:
        P.emit()
        return nc, dbg_outs

    ALLWIN = WINQ + WINQS + WINK + WINKS + [("WIN", nm) for nm in ("v", "bg", "cg", "hv")]
    o2 = REG0 + 32768
    PL = [P.sb(f"PL{i}", [128, 384], BF16, off=o2 + i * 768) for i in range(2)]; o2 += 1536
    PC = [P.sb(f"PC{i}", [128, 256], BF16, off=o2 + i * 512) for i in range(2)]; o2 += 1024
    att_tm = P.sb("att_tm", [128, 512], BF16, off=o2); o2 += 1024
    rec = P.sb("rec", [128, 8], F32, off=o2); o2 += 64
    cvt = [P.sb(f"cvt{i}", [128, 512], F32, off=o2 + i * 2048) for i in range(2)]; o2 += 4096
    assert o2 <= REG0 + 47104

    def kchunk(wb):
        return 0 if wb == 0 else (5 if wb == 17 else 1 + (wb - 1) // 4)

    VONES = [("Vt", "ones")]
    for i in range(1, 17):
        ci_q = 1 + (i - 1) // 4
        mv = 1 if i == 1 else (2 if i == 16 else 0)
        pvs = [psf("v"), psf("v")]
        for hn in range(8):
            half, c = hn // 4, hn % 4
            r0 = half * 64
            j = hn % 2
            sl, sln = psf("a")
            sc_, scn = psf("a")
            qsl = qT[r0:r0 + 64, c, i * 128:(i + 1) * 128]
            for kb in range(3):
                wb = i - 1 + kb
                MM(sl[:, kb * 128:(kb + 1) * 128], kT[r0:r0 + 64, wb * 128:(wb + 1) * 128], qsl, True, True,
                   [("qT", c, ci_q), ("kT", kchunk(wb))], [sln])
            for cb in range(2):
                MM(sc_[:, cb * 128:(cb + 1) * 128], kcT[r0:r0 + 64, cb * 128:(cb + 1) * 128], qsl, True, True,
                   [("qT", c, ci_q), "kcT"], [scn])
            ACT(PL[j][:], sl[:, 0:384], AF.Exp, [sln], [f"PL{j}"] + ALLWIN, scale=0.125)
            ACT(PC[j][:], sc_[:, 0:256], AF.Exp, [scn], [f"PC{j}"] + ALLWIN, scale=0.125)
            TT("vector", PL[j][:], PL[j][:], mask3[:, mv, :], ALU.mult,
               [f"PL{j}", ("mask3", mv), ("mask3", mv, 1), ("mask3", mv, 2)], [f"PL{j}"])
            pv, pvn = pvs[half]
            pvr = pv[:, c * 65:(c + 1) * 65]
            for kb in range(3):
                wb = i - 1 + kb
                MM(pvr, PL[j][:, kb * 128:(kb + 1) * 128], Vt[:, wb, half, :], kb == 0, False,
                   [f"PL{j}", ("Vt", wb)] + VONES, [pvn])
            for cb in range(2):
                MM(pvr, PC[j][:, cb * 128:(cb + 1) * 128], Vc[:, cb, half, :], False, cb == 1,
                   [f"PC{j}", ("Vc", cb), ("Vc", "ones")], [pvn])
        for b in range(2):
            pv, pvn = pvs[b]
            pvv = pv[:, 0:260].rearrange("p (h e) -> p h e", h=4)
            TT("vector", rec[:, b * 4:(b + 1) * 4].unsqueeze(2), pvv[:, :, 64:65], esink[:, b * 4:(b + 1) * 4].unsqueeze(2),
               ALU.add, [pvn, "esink"], [("rec", b)] + ALLWIN)
            P.op("vector", (lambda b=b: (lambda e: e.reciprocal(rec[:, b * 4:(b + 1) * 4], rec[:, b * 4:(b + 1) * 4])))(),
                 [("rec", b)], [("rec", b)])
            TT("vector", att_tm[:, b * 256:(b + 1) * 256].rearrange("p (h d) -> p h d", h=4), pvv[:, :, 0:64],
               rec[:, b * 4:(b + 1) * 4].unsqueeze(2).to_broadcast([128, 4, 64]), ALU.mult,
               [pvn, ("rec", b)], [("att_tm", b)] + ALLWIN)
        pb, pbn = psb()
        pbv = pb[:, 0:512].rearrange("p (k t) -> p k t", k=4)
        for cc in range(4):
            TR(pbv[:, cc, :], att_tm[:, cc * 128:(cc + 1) * 128], ident_b[:], [("att_tm", cc // 2), "ident_b"], [(pbn, cc)])
        ACT(mixT[:, 0:4, (i - 1) * 128:i * 128], pbv, AF.Copy, [(pbn, cc) for cc in range(4)],
            [("mixT", "att", i - 1)] + ALLWIN)

    TS("gpsimd", uT[:, :, 127:128], uT[:, :, 127:128], metat[:, 0:1], None, ALU.mult, None,
       [("uT", c, 0) for c in range(4)] + ["metat"], [("uT", c, 0) for c in range(4)])
    TS("gpsimd", uT[:, :, 2176:2177], uT[:, :, 2176:2177], metat[:, 1:2], None, ALU.mult, None,
       [("uT", c, 5) for c in range(4)] + ["metat"], [("uT", c, 5) for c in range(4)])
    for tcn in range(4):
        w0 = 128 + tcn * 512
        for c in range(4):
            ud = [("uT", c, ci) for ci in (tcn, tcn + 1, tcn + 2)]
            cwd = [("cw", kk, c4) for kk in range(3) for c4 in range(4)]
            TS("gpsimd", cvt[0][:], uT[:, c, w0 - 1:w0 + 511], cw[:, c, 0:1], None, ALU.mult, None, ud + cwd, ["cvt0"] + ALLWIN)
            TS("gpsimd", cvt[1][:], uT[:, c, w0:w0 + 512], cw[:, c, 1:2], None, ALU.mult, None, ud + cwd, ["cvt1"] + ALLWIN)
            TT("gpsimd", cvt[0][:], cvt[0][:], cvt[1][:], ALU.add, ["cvt0", "cvt1"], ["cvt0"])
            TS("gpsimd", cvt[1][:], uT[:, c, w0 + 1:w0 + 513], cw[:, c, 2:3], None, ALU.mult, None, ud + cwd, ["cvt1"])
            TT("gpsimd", cvt[0][:], cvt[0][:], cvt[1][:], ALU.add, ["cvt0", "cvt1"], ["cvt0"])
            TT("gpsimd", mixT[:, 4 + c, tcn * 512:(tcn + 1) * 512], cvt[0][:], bgT[:, c, tcn * 512:(tcn + 1) * 512], ALU.mult,
               ["cvt0", ("bgT", c, tcn + 1)], [("mixT", "conv", c, tcn)] + ALLWIN)

    if stage <= 4:
        P.emit()
        return nc, dbg_outs

    HTALL = [(f"hT{a}", t) for a in range(2) for t in range(4)]
    P.dma("gpsimd", "d_wo", lambda e: e.dma_start(out=wo[:], in_=w_out.rearrange("(k p) n -> p k n", p=128)), [], ["wo"] + HTALL)
    QALL = [("qT", c, ci) for c in range(4) for ci in range(1, 5)]
    o3 = qT_off
    o3 += 4096
    h2Tall = P.sb("h2Tall", [128, 8, 2048], BF16, off=bgT_off)
    CONVDEAD = [("uT", c, ci) for c in range(4) for ci in range(6)] + [("bgT", c, ci) for c in range(4) for ci in range(1, 5)]
    rows_off = REG0 - 8 * 4096
    affTM = P.sb("affTM", [128, 16, 16], F32, off=rows_off)
    gm = P.sb("gm", [128, 16, 16], F32, off=rows_off + 1024)
    thr = P.sb("thr", [128, 16], F32, off=rows_off + 2048)
    affT = P.sb("affT", [16, 2048], F32, off=o3); o3 += 8192
    wr = P.sb("wr", [128, 8, 16], BF16, off=o3); o3 += 256
    sm = P.sb("sm", [128, 64], F32, off=o3); o3 += 256
    assert o3 <= qT_off + 4 * W * 2
    P.dma("gpsimd", "d_wr", lambda e: e.dma_start(out=wr[:], in_=w_router.rearrange("(k p) e -> p k e", p=128)), [], ["wr"] + QALL)
    for tile in range(16):
        tcn = tile // 4
        mdeps = [("mixT", "att", tile)] + [("mixT", "conv", c, tcn) for c in range(4)]
        i = xt_rr[0] % 2
        xt_rr[0] += 1
        xtile, xn = xt[i], f"xt{i}"
        DMA("sync", "d_" + xn, xtile[:], x[128 + tile * 128:256 + tile * 128, :], [], [xn])
        for n in range(2):
            ps, psn = psf("v")
            for k in range(8):
                MM(ps[:], mixT[:, k, tile * 128:(tile + 1) * 128], wo[:, k, n * 512:(n + 1) * 512], k == 0, k == 7,
                   mdeps + ["wo"], [psn])
            TT("vector", tf[:, n * 512:(n + 1) * 512], ps[:], rows["GT1"][:, n * 512:(n + 1) * 512], ALU.mult,
               [psn] + rowdeps("GT1"), ["tf"])
        TT("gpsimd", xtile[:], xtile[:], tf[:], ALU.add, [xn, "tf"], [xn])
        DMA("sync", "d_x1d_" + xn, x1d[tile * 128:(tile + 1) * 128, :], xtile[:], [xn], [("x1d", tile)])
        j = tile % 2
        norm_mod_sb(xtile, xn, "G2", "S2", hb[j], f"hb{j}")
        DMA("sync", f"d_h2loc{j}", h2loc[tile * 128:(tile + 1) * 128, :], hb[j][:], [f"hb{j}"], [("h2loc", tile)])
        h2v = h2Tall[:, :, tile * 128:(tile + 1) * 128]
        pb, pbn = psb()
        pbv = pb[:].rearrange("p (k t) -> p k t", k=8)
        for k in range(8):
            TR(pbv[:, k, :], hb[j][:, k * 128:(k + 1) * 128], ident_b[:], [f"hb{j}", "ident_b"], [(pbn, k)])
        ACT(h2v, pbv, AF.Copy, [(pbn, k) for k in range(8)], [("h2T", tile)] + CONVDEAD)
        ps, psn = psf("v")
        for k in range(8):
            MM(ps[:, 0:16], h2Tall[:, k, tile * 128:(tile + 1) * 128], wr[:, k, :], k == 0, k == 7, [("h2T", tile), "wr"], [psn])
        mx, nmx, ssum, ex = sm[:, 0:1], sm[:, 1:2], sm[:, 2:3], sm[:, 16:32]
        af = affTM[:, tile, :]
        RED("vector", mx, ps[:, 0:16], ALU.max, [psn], ["sm_mx"])
        TS("vector", nmx, mx, -1.0, None, ALU.mult, None, ["sm_mx"], ["sm_nmx"])
        ACT(ex, ps[:, 0:16], AF.Exp, [psn, "sm_nmx"], ["sm_ex"], bias=nmx)
        RED("vector", ssum, ex, ALU.add, ["sm_ex"], ["sm_sum"])
        P.op("vector", lambda e: e.reciprocal(sm[:, 2:3], sm[:, 2:3]), ["sm_sum"], ["sm_sum"])
        TS("vector", af, ex, ssum, None, ALU.mult, None, ["sm_ex", "sm_sum"], [("affTM", tile)])
        pt, ptn = psf("a")
        TR(pt[0:16, 0:128], af, ident_f[:], [("affTM", tile), "ident_f"], [ptn])
        ACT(affT[:, tile * 128:(tile + 1) * 128], pt[0:16, 0:128], AF.Copy, [ptn], [("affT", tile)] + QALL)
    DMA("sync", "d_affloc", affloc, affT[:], [("affT", t) for t in range(16)], ["affloc"])

    if "a3" in dbg:
        d1 = dbg_out("x1", [2048, 1024])
        d2 = dbg_out("aff", [16, 2048])
        d3 = dbg_out("h2", [2048, 1024], BF16)
        e1 = DMA("sync", "d_dbg1", d1, x1d, [("x1d", t) for t in range(16)], ["dbg1"])
        e2 = DMA("sync", "d_dbg2", d2, affloc, ["affloc"], ["dbg2"])
        e3 = DMA("sync", "d_dbg3", d3, h2loc, [("h2loc", t) for t in range(16)], ["dbg3"])
        P.finish("sync", [e1, e2, e3])
    if stage <= 5:
        P.emit()
        return nc, dbg_outs

    FENCE = [k for k in P.res.keys() if not (isinstance(k, str) and (k.startswith("psf") or k in ("ident_b", "ident_f", "ones_b", "metat")))
             and not (isinstance(k, tuple) and k[0] in ("row", "h2T", "affTM", "x1d", "h2loc"))]
    NTB, NITB = 8, 8
    P.dma("gpsimd", "d_ag_aff", lambda e: e.collective_compute("AllGather", ALU.bypass, replica_groups=GROUPS,
                                                               ins=[affloc.opt()], outs=[affall.opt()]),
          ["affloc"], ["affall"], inc=1)
    ob = REG0 + 159936 - 0
    ob = bgT_off + 32768
    AFt = P.sb("AFt", [128, 16, 64], F32, off=ob); ob += 4096
    FR = P.sb("FR", [128, 16, NTB], F32, off=ob); ob += 512
    Tt = P.sb("Tt", [128, 16, NTB], F32, off=ob); ob += 512
    tmpa = P.sb("tmpa", [128, 16, NTB], F32, off=ob); ob += 512
    get = P.sb("get", [128, 16, NTB], F32, off=ob); ob += 512
    cntb = P.sb("cntb", [128, 16 * NTB], BF16, off=ob); ob += 256
    lo = P.sb("lo", [128, 16], F32, off=ob); ob += 64
    hi = P.sb("hi", [128, 16], F32, off=ob); ob += 64
    wdt = P.sb("wdt", [128, 16], F32, off=ob); ob += 64
    red = P.sb("red", [128, 16], F32, off=ob); ob += 64
    idxt = P.sb("idxt", [128, 16], I32, off=ob); ob += 64
    assert ob <= rt1_off + 6144
    cmpb = P.sb("cmpb", [128, 16, NTB, 64], BF16, off=REG0 + 49152)
    for r in range(4):
        DMA("sync", "d_AFt", AFt[32 * r:32 * (r + 1), :, :],
            affall[r * 16:(r + 1) * 16, :].rearrange("e (p j) -> p e j", p=32, j=64), ["affall"], [("AFt", r)] + FENCE)
    AFD = [("AFt", r) for r in range(4)]
    P.op("gpsimd", lambda e: e.iota(FR[:], pattern=[[0, 16], [1, NTB]], base=1, channel_multiplier=0,
                                    allow_small_or_imprecise_dtypes=True), (), ["FR"] + FENCE)
    P.op("gpsimd", lambda e: e.iota(idxt[:], pattern=[[128, 16]], base=0, channel_multiplier=1), (), ["idxt"] + FENCE)
    TS("vector", FR[:], FR[:], 1.0 / (NTB + 1), None, ALU.mult, None, ["FR"], ["FR"])
    MSET("vector", lo[:], 0.0, ["lo"] + FENCE)
    MSET("vector", hi[:], 1.0, ["hi"])
    for it in range(NITB):
        TT("vector", wdt[:], hi[:], lo[:], ALU.subtract, ["hi", "lo"], ["wdt"])
        TT("vector", Tt[:], FR[:], wdt[:].unsqueeze(2).to_broadcast([128, 16, NTB]), ALU.mult, ["FR", "wdt"], ["Tt"])
        TT("vector", Tt[:], Tt[:], lo[:].unsqueeze(2).to_broadcast([128, 16, NTB]), ALU.add, ["Tt", "lo"], ["Tt"])
        TT("vector", cmpb[:], AFt[:].unsqueeze(2).to_broadcast([128, 16, NTB, 64]),
           Tt[:].unsqueeze(3).to_broadcast([128, 16, NTB, 64]), ALU.is_ge, AFD + ["Tt"], ["cmpb"] + FENCE)
        RED("vector", tmpa[:], cmpb[:], ALU.add, ["cmpb"], ["tmpa"])
        CP("vector", cntb[:], tmpa[:].rearrange("p e k -> p (e k)"), ["tmpa"], ["cntb"])
        ps, psn = psf("v")
        MM(ps[:, 0:16 * NTB], ones_b[:], cntb[:], True, True, ["cntb", "ones_b"], [psn])
        TS("vector", get[:].rearrange("p e k -> p (e k)"), ps[:, 0:16 * NTB], 1024.0, None, ALU.is_ge, None, [psn], ["get"])
        TT("vector", tmpa[:], Tt[:], get[:], ALU.mult, ["Tt", "get"], ["tmpa"])
        RED("vector", red[:], tmpa[:], ALU.max, ["tmpa"], ["red"])
        TT("vector", lo[:], lo[:], red[:], ALU.max, ["lo", "red"], ["lo"])
        TS("vector", tmpa[:], get[:], 2.0, None, ALU.mult, None, ["get"], ["tmpa"])
        TT("vector", tmpa[:], tmpa[:], Tt[:], ALU.add, ["tmpa", "Tt"], ["tmpa"])
        RED("vector", red[:], tmpa[:], ALU.min, ["tmpa"], ["red"])
        TT("vector", hi[:], hi[:], red[:], ALU.min, ["hi", "red"], ["hi"])
    CP("vector", thr[:], lo[:], ["lo"], ["thr"])
    AFFTM = [("affTM", t) for t in range(16)]
    TT("vector", gm[:], affTM[:], thr[:].unsqueeze(1).to_broadcast([128, 16, 16]), ALU.is_ge, AFFTM + ["thr"], ["gm"])
    TT("vector", gm[:], gm[:], affTM[:], ALU.mult, ["gm"] + AFFTM, ["gm"])

    if "thr" in dbg:
        d1 = dbg_out("thr", [128, 16])
        d2 = dbg_out("gm", [128, 256])
        e1 = DMA("sync", "d_dbg1", d1, thr[:], ["thr"], ["dbg1"])
        e2 = DMA("sync", "d_dbg2", d2, gm[:].rearrange("p a b -> p (a b)"), ["gm"], ["dbg2"])
        P.finish("sync", [e1, e2])
    if stage <= 6:
        P.emit()
        return nc, dbg_outs

    wslot = [P.sb(f"wslot{i}", [128, 8, 1024], BF16, off=REG0 + i * 16384) for i in range(4)]
    wdt_ = P.sb("wd_", [128, 8, 1024], BF16, off=REG0 + 81920)
    hid = [P.sb(f"hid{i}", [128, 8, 512], BF16, off=qT_off + i * 8192) for i in range(2)]
    yg = [P.sb(f"yg{i}", [128, 1024], F32, off=kT_off + i * 4096) for i in range(2)]
    sgs = P.sb("sgs", [128, 512], F32, off=rt1_off + 2048 + 2048)
    zt = yg[0]
    MSET("vector", zt[:], 0.0, ["yg0"] + FENCE)
    for t in range(16):
        DMA("sync", "d_acc0", accd[t * 128:(t + 1) * 128, :], zt[:], ["yg0"], [("accd", t)])
    H2ALL = [("h2T", t) for t in range(16)]
    wgv = w_gate.rearrange("e (k p) n -> e p k n", p=128)
    wuv = w_up.rearrange("e (k p) n -> e p k n", p=128)
    wdv = w_down.rearrange("e (k p) n -> e p k n", p=128)
    yrr = [0]
    for ex_ in range(16):
        sg_, su_ = (ex_ % 2) * 2, (ex_ % 2) * 2 + 1
        wg_t, wu_t = wslot[sg_], wslot[su_]
        extra = ["cmpb"] if su_ == 3 else []
        P.dma("gpsimd", f"d_ws{sg_}", (lambda wg_t=wg_t, ex_=ex_: (lambda e: e.dma_start(out=wg_t[:], in_=wgv[ex_])))(), [], [f"ws{sg_}"] + (FENCE if ex_ < 2 else []))
        P.dma("gpsimd", f"d_ws{su_}", (lambda wu_t=wu_t, ex_=ex_: (lambda e: e.dma_start(out=wu_t[:], in_=wuv[ex_])))(), [], [f"ws{su_}"] + extra + (FENCE if ex_ < 2 else []))
        P.dma("gpsimd", "d_wd", (lambda ex_=ex_: (lambda e: e.dma_start(out=wdt_[:], in_=wdv[ex_])))(), [], ["wd_"] + (FENCE if ex_ < 1 else []))
        for tch in range(4):
            hd = hid[(ex_ * 4 + tch) % 2]
            hdn = f"hid{(ex_ * 4 + tch) % 2}"
            hdeps = [("h2T", tch * 4 + t) for t in range(4)]
            for fo in range(8):
                pa, pan = psf("a")
                for k in range(8):
                    MM(pa[:], wg_t[:, k, fo * 128:(fo + 1) * 128], h2Tall[:, k, tch * 512:(tch + 1) * 512], k == 0, k == 7,
                       [f"ws{sg_}"] + hdeps, [pan])
                pu, pun = psf("v")
                for k in range(8):
                    MM(pu[:], wu_t[:, k, fo * 128:(fo + 1) * 128], h2Tall[:, k, tch * 512:(tch + 1) * 512], k == 0, k == 7,
                       [f"ws{su_}"] + hdeps, [pun])
                ACT(sgs[:], pa[:], AF.Silu, [pan], ["sgs"] + (FENCE if ex_ == 0 and tch == 0 and fo == 0 else []))
                TT("vector", hd[:, fo, :], pu[:], sgs[:], ALU.mult, [pun, "sgs"], [(hdn, fo)] + (FENCE if ex_ == 0 and tch < 2 else []))
            for t in range(4):
                tile = tch * 4 + t
                yi = yrr[0] % 2
                yrr[0] += 1
                for dn in range(2):
                    py, pyn = psf("v")
                    for k in range(8):
                        MM(py[:], hd[:, k, t * 128:(t + 1) * 128], wdt_[:, k, dn * 512:(dn + 1) * 512], k == 0, k == 7,
                           [(hdn, k), "wd_"], [pyn])
                    TS("vector", yg[yi][:, dn * 512:(dn + 1) * 512], py[:], gm[:, tile, ex_:ex_ + 1], None, ALU.mult, None,
                       [pyn, "gm"], [f"yg{yi}"])
                P.dma("gpsimd", f"d_sc{yi}", (lambda yi=yi, tile=tile: (lambda e: e.indirect_dma_start(
                    out=accd, out_offset=bass.IndirectOffsetOnAxis(ap=idxt[:, tile:tile + 1], axis=0), in_=yg[yi][:], in_offset=None,
                    compute_op=ALU.add, oob_is_err=True)))(), [f"yg{yi}", "idxt", ("accd", tile)], [("accd", tile)])

    if stage <= 7:
        P.emit()
        return nc, dbg_outs

    gfrow = P.sb("gfrow", [128, 1024], F32, off=rows_off + 4096)
    DMA("sync", "d_gfrow", gfrow[:], g_final.partition_broadcast(128), [], ["gfrow"] + FENCE)
    evs = []
    for tile in range(16):
        i = xt_rr[0] % 2
        xt_rr[0] += 1
        xtile, xn = xt[i], f"xt{i}"
        DMA("sync", "d_" + xn, xtile[:], x1d[tile * 128:(tile + 1) * 128, :], [("x1d", tile)], [xn])
        DMA("sync", "d_tfc", tf[:], accd[tile * 128:(tile + 1) * 128, :], [("accd", tile)], ["tf"])
        TT("vector", tf[:], tf[:], rows["GT2"][:], ALU.mult, ["tf"] + rowdeps("GT2"), ["tf"])
        TT("gpsimd", xtile[:], xtile[:], tf[:], ALU.add, [xn, "tf"], [xn])
        ss = small[:, 16:17]
        rstd = small[:, 17:18]
        ACT(tf[:], xtile[:], AF.Square, [xn], ["tf"])
        RED("vector", ss, tf[:], ALU.add, ["tf"], ["ss"])
        TS("vector", rstd, ss, 1.0 / 1024.0, 1e-6, ALU.mult, ALU.add, ["ss"], ["rstd"])
        ACT(rstd, rstd, AF.Ln, ["rstd"], ["rstd"])
        ACT(rstd, rstd, AF.Exp, ["rstd"], ["rstd"], scale=-0.5)
        ACT(tf[:], xtile[:], AF.Copy, [xn, "rstd"], ["tf"], scale=rstd)
        TT("vector", xtile[:], tf[:], gfrow[:], ALU.mult, ["tf", "gfrow"], [xn])
        evs.append(DMA("sync", "d_out_" + xn, out[tile * 128:(tile + 1) * 128, :], xtile[:], [xn], [("out", tile)]))
    P.finish("sync", evs)
    P.emit()
    return nc, dbg_outs


def make_in_maps(inp):
    x = np.ascontiguousarray(inp["x"], dtype=np.float32)
    maps = []
    for c in range(NCORES):
        b, q = c // 4, c % 4
        t0 = q * 2048
        xw = np.zeros((W, 1024), np.float32)
        lo, hi = t0 - 128, t0 + 2048 + 128
        slo, shi = max(lo, 0), min(hi, 8192)
        xw[slo - lo:shi - lo] = x[b, slo:shi]
        ccv = np.stack([inp["c"][b].reshape(8, 128).T, inp["c_ctx"].reshape(8, 128).T], axis=-1)
        meta = np.zeros((128, 80), np.float32)
        meta[:, 0] = 1.0 if q > 0 else 0.0
        meta[:, 1] = 1.0 if q < 3 else 0.0
        meta[:, 2] = float(q * 32 - 2)
        meta[:, 3] = float(q * 2048)
        for k in range(4):
            meta[:, 8 + (4 * q + k) * 4 + k] = 1.0
        maps.append({
            "x": xw, "ctx": np.ascontiguousarray(inp["ctx"][b]), "cc": np.ascontiguousarray(ccv.reshape(128, 16)),
            "meta": meta, "w_ada": inp["w_ada"][0], "b_ada": inp["b_ada"][0], "g_mix": inp["g_mix"][0],
            "g_ffn": inp["g_ffn"][0], "g_final": inp["g_final"], "w_in": inp["w_in"][0], "conv_w": inp["conv_w"][0],
            "sink": inp["sink"][0], "w_out": inp["w_out"][0], "w_router": inp["w_router"][0],
            "w_gate": inp["w_gate"][0], "w_up": inp["w_up"][0], "w_down": inp["w_down"][0],
        })
    return maps


def kernel(**inputs):
    inp = {k: np.asarray(v) for k, v in inputs.items()}
    nc, _ = build_nc()
    res = run_bass_kernel_spmd(nc, make_in_maps(inp), core_ids=list(range(NCORES)))
    outp = np.zeros((2, 8192, 1024), np.float32)
    for c in range(NCORES):
        b, q = c // 4, c % 4
        outp[b, q * 2048:(q + 1) * 2048] = res.results[c]["out"]
    return outp
```

```python
import os
import numpy as np
import concourse.bass as bass
import concourse.mybir as mybir
from concourse.bass_utils import run_bass_kernel_spmd

F32 = mybir.dt.float32
BF16 = mybir.dt.bfloat16
I32 = mybir.dt.int32
ALU = mybir.AluOpType
AF = mybir.ActivationFunctionType
AX = mybir.AxisListType

COMPUTE = ("tensor", "vector", "scalar", "gpsimd")
QUEUES = ("sync",)
NCORES = 8
GROUPS = [[0, 1, 2, 3], [4, 5, 6, 7]]
W = 2304
NT = 16
NIT = 7


class Prog:
    def __init__(self, nc):
        self.nc = nc
        self.streams = {e: [] for e in COMPUTE + QUEUES}
        self.cnt = {e: 0 for e in COMPUTE}
        self.dma_cnt = {}
        self.waited = {}
        self.res = {}
        self.sem_handles = {}
        self.final_events = []
        self.sb_off = 16512
        self.sb_top = 229344

    def sb(self, name, shape, dtype, off=None):
        esz = {F32: 4, BF16: 2, I32: 4}[dtype]
        n = 1
        for s in shape[1:]:
            n *= s
        nbytes = (n * esz + 63) // 64 * 64
        if off is None:
            off = self.sb_off
            self.sb_off += nbytes
        assert off >= 16512 and off + nbytes <= self.sb_top, (name, off, nbytes)
        return self.nc.alloc_sbuf_tensor_at(name, list(shape), dtype, offset=off)

    def _deps(self, reads, writes):
        need = []
        for r in reads:
            st = self.res.get(r)
            if st and st["w"] is not None:
                need.append(st["w"])
        for w in writes:
            st = self.res.get(w)
            if st:
                if st["w"] is not None:
                    need.append(st["w"])
                need.extend(st["r"])
        return need

    def _commit(self, ev, reads, writes):
        for r in reads:
            st = self.res.setdefault(r, {"w": None, "r": []})
            st["r"].append(ev)
        for w in writes:
            self.res[w] = {"w": ev, "r": []}

    def _waits(self, eng, need):
        best = {}
        for (k, v) in need:
            if k == "tensor" and eng == "tensor":
                continue
            if v > best.get(k, 0):
                best[k] = v
        out = []
        for k, v in best.items():
            if self.waited.get((eng, k), 0) >= v:
                continue
            self.waited[(eng, k)] = v
            out.append((k, v))
        return out

    def op(self, eng, fn, reads=(), writes=()):
        need = self._deps(reads, writes)
        waits = self._waits(eng, need)
        self.cnt[eng] += 1
        ev = (eng, self.cnt[eng])
        self.streams[eng].append((waits, fn, (eng, 1)))
        self._commit(ev, reads, writes)
        return ev

    def dma(self, q, sem, fn, reads=(), writes=(), inc=16):
        need = self._deps(reads, writes)
        waits = self._waits(q, need)
        self.dma_cnt[sem] = self.dma_cnt.get(sem, 0) + inc
        ev = (sem, self.dma_cnt[sem])
        self.streams[q].append((waits, fn, (sem, inc)))
        self._commit(ev, reads, writes)
        return ev

    def finish(self, eng, events):
        self.final_events.append((eng, events))

    def check_deadlock(self):
        sem = {}
        pos = {e: 0 for e in self.streams}
        progressed = True
        while progressed:
            progressed = False
            for e, st in self.streams.items():
                while pos[e] < len(st):
                    waits, fn, inc = st[pos[e]]
                    if all(sem.get(k, 0) >= v for (k, v) in waits):
                        sem[inc[0]] = sem.get(inc[0], 0) + inc[1]
                        pos[e] += 1
                        progressed = True
                    else:
                        break
        stuck = {e: (pos[e], len(st), st[pos[e]][0]) for e, st in self.streams.items() if pos[e] < len(st)}
        assert not stuck, ("DEADLOCK", stuck, {k: sem.get(k) for e in stuck for (k, v) in stuck[e][2]})

    def emit(self):
        self.check_deadlock()
        nc = self.nc
        names = set(COMPUTE)
        for e in self.streams:
            for (waits, fn, inc) in self.streams[e]:
                names.add(inc[0])
                for (k, v) in waits:
                    names.add(k)
        for n in sorted(names):
            self.sem_handles[n] = nc.alloc_semaphore("s_" + n)
        H = self.sem_handles
        fin = {}
        for eng, evs in self.final_events:
            fin.setdefault(eng, []).extend(evs)
        with nc.Block() as block:
            def make(ename):
                def body(e):
                    for (waits, fn, inc) in self.streams[ename]:
                        for (k, v) in waits:
                            e.wait_ge(H[k], v)
                        fn(e).then_inc(H[inc[0]], inc[1])
                    best = {}
                    for (k, v) in fin.get(ename, []):
                        best[k] = max(best.get(k, 0), v)
                    for k, v in best.items():
                        e.wait_ge(H[k], v)
                return body
            for ename in self.streams:
                if not self.streams[ename] and ename not in fin:
                    continue
                getattr(block, ename)(make(ename))


def build_nc(stage=99, dbg=()):
    nc = bass.Bass("TRN2", target_bir_lowering=False)
    P = Prog(nc)
    dbg_outs = {}

    def din(name, shape, dt=F32):
        return nc.dram_tensor(name, list(shape), dt, kind="ExternalInput").ap()

    x = din("x", [W, 1024])
    ctx = din("ctx", [256, 1024])
    cc = din("cc", [128, 16])
    meta = din("meta", [128, 80])
    w_ada = din("w_ada", [1024, 6144])
    b_ada = din("b_ada", [6144])
    g_mix = din("g_mix", [1024])
    g_ffn = din("g_ffn", [1024])
    g_final = din("g_final", [1024])
    w_in = din("w_in", [1024, 2304])
    conv_w = din("conv_w", [3, 512])
    sink = din("sink", [8])
    w_out = din("w_out", [1024, 1024])
    w_router = din("w_router", [1024, 16])
    w_gate = din("w_gate", [4, 1024, 1024])
    w_up = din("w_up", [4, 1024, 1024])
    w_down = din("w_down", [4, 1024, 1024])
    out = nc.dram_tensor("out", [2048, 1024], F32, kind="ExternalOutput").ap()

    x1d = nc.dram_tensor("x1d", [2048, 1024], F32).ap()
    h2loc = nc.dram_tensor("h2loc", [2048, 1024], BF16).ap()
    h2all = nc.dram_tensor("h2all", [8192, 1024], BF16).ap()
    affloc = nc.dram_tensor("affloc", [16, 2048], F32).ap()
    affall = nc.dram_tensor("affall", [64, 2048], F32).ap()
    tabd = nc.dram_tensor("tabd", [2048, 128], F32).ap()
    Zd = nc.dram_tensor("Zd", [8192, 1024], BF16).ap()
    Zall = nc.dram_tensor("Zall", [32768, 1024], BF16).ap()

    def dbg_out(name, shape, dt=F32):
        t = nc.dram_tensor("dbg_" + name, list(shape), dt, kind="ExternalOutput").ap()
        dbg_outs[name] = t
        return t

    def ACT(out_, in_, func, r, w, **kw):
        return P.op("scalar", lambda e: e.activation(out=out_, in_=in_, func=func, **kw), r, w)

    def TT(eng, out_, in0, in1, op, r, w):
        return P.op(eng, lambda e: e.tensor_tensor(out=out_, in0=in0, in1=in1, op=op), r, w)

    def TS(eng, out_, in0, s1, s2, op0, op1, r, w):
        if op1 is None:
            return P.op(eng, lambda e: e.tensor_scalar(out=out_, in0=in0, scalar1=s1, scalar2=None, op0=op0), r, w)
        return P.op(eng, lambda e: e.tensor_scalar(out=out_, in0=in0, scalar1=s1, scalar2=s2, op0=op0, op1=op1), r, w)

    def STT(eng, out_, in0, scalar, in1, op0, op1, r, w):
        return P.op(eng, lambda e: e.scalar_tensor_tensor(out=out_, in0=in0, scalar=scalar, in1=in1, op0=op0, op1=op1), r, w)

    def RED(eng, out_, in_, op, r, w):
        return P.op(eng, lambda e: e.tensor_reduce(out=out_, in_=in_, axis=AX.X, op=op), r, w)

    def CP(eng, out_, in_, r, w):
        return P.op(eng, lambda e: e.tensor_copy(out=out_, in_=in_), r, w)

    def MSET(eng, out_, val, w):
        return P.op(eng, lambda e: e.memset(out_, val), (), w)

    def MM(out_, lhsT, rhs, start, stop, r, w):
        return P.op("tensor", lambda e: e.matmul(out_, lhsT, rhs, start=start, stop=stop), r, w)

    def TR(out_, in_, ident, r, w):
        return P.op("tensor", lambda e: e.transpose(out_, in_, ident), r, w)

    def DMA(q, sem, out_, in_, r, w):
        return P.dma(q, sem, lambda e: e.dma_start(out=out_, in_=in_), r, w)

    PSF = [nc.alloc_psum_tensor(f"psf{i}", [128, 512], F32) for i in range(6)]
    PSB = [nc.alloc_psum_tensor(f"psb{i}", [128, 1024], BF16) for i in range(2)]
    psf_rr = {"v": 0, "a": 0}

    def psf(cons):
        i = psf_rr[cons] % 3 + (0 if cons == "v" else 3)
        psf_rr[cons] += 1
        return PSF[i], f"psf{i}"

    psb_rr = [0]

    def psb():
        i = psb_rr[0] % 2
        psb_rr[0] += 1
        return PSB[i], f"psb{i}"

    ident_f = P.sb("ident_f", [128, 128], F32)
    ident_b = P.sb("ident_b", [128, 128], BF16)
    iot = P.sb("iot", [128, 128], F32)
    ones_b = P.sb("ones_b", [128, 128], BF16)
    U_b = P.sb("U_b", [128, 128], BF16)
    UI_b = P.sb("UI_b", [128, 128], BF16)
    mask3 = P.sb("mask3", [128, 3, 384], BF16)
    metat = P.sb("metat", [128, 80], F32)
    esink = P.sb("esink", [128, 8], F32)
    rows = {}
    for nm in ("S1", "G1", "GT1", "S2", "G2", "GT2", "cS1", "cG1"):
        rows[nm] = P.sb("row_" + nm, [128, 1024], F32)
    REG0 = P.sb_off

    P.op("gpsimd", lambda e: e.iota(iot[:], pattern=[[1, 128]], base=0, channel_multiplier=-1,
                                    allow_small_or_imprecise_dtypes=True), (), ["iot"])
    TS("vector", ident_f[:], iot[:], 0.0, None, ALU.is_equal, None, ["iot"], ["ident_f"])
    CP("vector", ident_b[:], ident_f[:], ["ident_f"], ["ident_b"])
    TS("vector", U_b[:], iot[:], 0.0, None, ALU.is_ge, None, ["iot"], ["U_b"])
    MSET("vector", ones_b[:], 1.0, ["ones_b"])
    DMA("sync", "d_meta", metat[:], meta, [], ["metat"])
    DMA("sync", "d_sink", esink[:], sink.partition_broadcast(128), [], ["esink"])
    ACT(esink[:], esink[:], AF.Exp, ["esink"], ["esink"])
    for v in range(3):
        TS("vector", mask3[:, v, 0:128], iot[:], 0.0, None, ALU.is_le, None, ["iot"], [("mask3", v)])
        MSET("vector", mask3[:, v, 128:256], 1.0, [("mask3", v, 1)])
        TS("vector", mask3[:, v, 256:384], iot[:], 0.0, None, ALU.is_ge, None, ["iot"], [("mask3", v, 2)])
    TS("vector", mask3[:, 1, 0:128], mask3[:, 1, 0:128], metat[:, 0:1], None, ALU.mult, None,
       ["metat", ("mask3", 1)], [("mask3", 1)])
    TS("vector", mask3[:, 2, 256:384], mask3[:, 2, 256:384], metat[:, 1:2], None, ALU.mult, None,
       ["metat", ("mask3", 2, 2)], [("mask3", 2, 2)])

    if "const" in dbg:
        d1 = dbg_out("ident", [128, 128])
        d2 = dbg_out("mask3", [128, 3 * 384], BF16)
        d3 = dbg_out("esink", [128, 8])
        e1 = DMA("sync", "d_dbg", d1, ident_f[:], ["ident_f"], ["dbg1"])
        e2 = DMA("sync", "d_dbg", d2, mask3[:].rearrange("p a b -> p (a b)"),
                 [("mask3", v) for v in range(3)] + [("mask3", v, 1) for v in range(3)] + [("mask3", v, 2) for v in range(3)], ["dbg2"])
        e3 = DMA("sync", "d_dbg", d3, esink[:], ["esink"], ["dbg3"])
        P.finish("sync", [e1, e2, e3])
    if stage <= 0:
        P.emit()
        return nc, dbg_outs

    o = REG0
    WIN = P.sb("WIN", [128, 8, 2944], BF16, off=o)
    mixT = P.sb("mixT", [128, 8, 2048], BF16, off=o)
    o += 47104
    COS = P.sb("COS", [128, W], F32, off=o); o += W * 4
    SINS = P.sb("SINS", [128, W], F32, off=o); o += W * 4
    xt = [P.sb(f"xt{i}", [128, 1024], F32, off=o + i * 4096) for i in range(2)]; o += 8192
    tf = P.sb("tf", [128, 1024], F32, off=o); o += 4096
    hb = [P.sb(f"hb{i}", [128, 1024], BF16, off=o + i * 2048) for i in range(2)]; o += 4096
    hT = [P.sb(f"hT{i}", [128, 8, 512], BF16, off=o + i * 8192) for i in range(2)]
    wo = P.sb("wo", [128, 8, 1024], BF16, off=o)
    o += 16384
    qT_off = o
    qT = P.sb("qT", [128, 4, W], BF16, off=o); o += 4 * W * 2
    kT_off = o
    kT = P.sb("kT", [128, W], BF16, off=o); o += W * 2
    Vt = P.sb("Vt", [128, 18, 2, 65], BF16, off=o); o += 4736
    kcT = P.sb("kcT", [128, 256], BF16, off=o); o += 512
    Vc = P.sb("Vc", [128, 2, 2, 65], BF16, off=o); o += 576
    bgT_off = o
    bgT = P.sb("bgT", [128, 4, 2048], BF16, off=o)
    stg = P.sb("stg", [128, 8, 640], F32, off=o)
    o += 20480
    uT_off = o
    uT = P.sb("uT", [128, 4, W], BF16, off=o); o += 4 * W * 2
    rt1_off = o
    rt1 = P.sb("rt1", [128, 512], F32, off=o); o += 2048
    rt2 = P.sb("rt2", [128, 512], F32, off=o); o += 2048
    cgs = P.sb("cgs", [128, 512], F32, off=o); o += 2048
    small = P.sb("small", [128, 64], F32, off=o); o += 256
    cw = P.sb("cw", [128, 4, 3], F32, off=o); o += 64
    assert o <= P.sb_top, o
    A_END = o

    wa = [P.sb("wa0", [128, 8, 1024], BF16, off=qT_off), P.sb("wa1", [128, 8, 1024], BF16, off=uT_off)]
    o = kT_off
    lb = P.sb("lb", [128, 8, 2, 128], BF16, off=o); o += 4096
    brow = P.sb("brow", [128, 1024], F32, off=o); o += 4096
    cct = P.sb("cct", [128, 8, 2], F32, off=o); o += 64
    scl = P.sb("scl", [128, 8, 2], F32, off=o); o += 64
    assert o <= bgT_off
    gmrow = P.sb("gmrow", [128, 1024], F32, off=rt1_off)

    DMA("sync", "d_cc", cct[:], cc.rearrange("p (k v) -> p k v", v=2), [], ["cct"])
    ACT(scl[:], cct[:], AF.Silu, ["cct"], ["scl"])
    for v in range(2):
        CP("vector", lb[:, :, v, :], scl[:, :, v:v + 1].to_broadcast([128, 8, 128]), ["scl"], [("lb", v)])
    if stage <= 0.3:
        d1 = dbg_out("lb", [128, 8 * 2 * 128], BF16)
        e1 = DMA("sync", "d_dbg", d1, lb[:].rearrange("p a b c -> p (a b c)"), [("lb", 0), ("lb", 1)], ["dbg1"])
        P.finish("sync", [e1])
        P.emit()
        return nc, dbg_outs
    w_ada_v = w_ada.rearrange("(k p) n -> p k n", p=128)
    grp = [(0, [("S1", 0), ("cS1", 1)]), (1, [("G1", 0), ("cG1", 1)]), (2, [("GT1", 0)]),
           (3, [("S2", 0)]), (4, [("G2", 0)]), (5, [("GT2", 0)])]
    for gi, (g, uses) in enumerate(grp):
        wb = wa[gi % 2]
        wn = f"wa{gi % 2}"
        P.dma("gpsimd", "d_" + wn, (lambda wb=wb, g=g: (lambda e: e.dma_start(out=wb[:], in_=w_ada_v[:, :, g * 1024:(g + 1) * 1024])))(),
              [], [wn])
        DMA("sync", "d_brow", brow[:], b_ada[g * 1024:(g + 1) * 1024].partition_broadcast(128), [], ["brow"])
        if stage <= 0.5:
            d1 = dbg_out("wa", [128, 8 * 1024], BF16)
            d2 = dbg_out("brow", [128, 1024])
            e1 = DMA("sync", "d_dbg", d1, wb[:].rearrange("p a b -> p (a b)"), [wn], ["dbg1"])
            e2 = DMA("sync", "d_dbg", d2, brow[:], ["brow"], ["dbg2"])
            P.finish("sync", [e1, e2])
            P.emit()
            return nc, dbg_outs
        for (nm, v) in uses:
            for n in range(2):
                ps, psn = psf("v")
                for k in range(8):
                    MM(ps[:], lb[:, k, v, :], wb[:, k, n * 512:(n + 1) * 512], k == 0, k == 7,
                       [("lb", v), wn], [psn])
                TT("vector", rows[nm][:, n * 512:(n + 1) * 512], ps[:], brow[:, n * 512:(n + 1) * 512], ALU.add,
                   [psn, "brow"], [("row", nm, n)])
                if stage <= 0.7:
                    d1 = dbg_out("r0", [128, 512])
                    e1 = DMA("sync", "d_dbg", d1, rows[nm][:, 0:512], [("row", nm, n)], ["dbg1"])
                    P.finish("sync", [e1])
                    P.emit()
                    return nc, dbg_outs
    for (gsrc, names) in (((g_mix, ("G1", "cG1")), (g_ffn, ("G2",))) if stage > 0.8 else ()):
        DMA("sync", "d_gmrow", gmrow[:], gsrc.partition_broadcast(128), [], ["gmrow"])
        for nm in names:
            TS("vector", rows[nm][:], rows[nm][:], 1.0, None, ALU.add, None,
               [("row", nm, 0), ("row", nm, 1)], [("row", nm, 0), ("row", nm, 1)])
            TT("vector", rows[nm][:], rows[nm][:], gmrow[:], ALU.mult,
               [("row", nm, 0), ("row", nm, 1), "gmrow"], [("row", nm, 0), ("row", nm, 1)])

    def rowdeps(nm):
        return [("row", nm, 0), ("row", nm, 1)]

    if "rows" in dbg:
        d = dbg_out("rows", [8, 128, 1024])
        for i, nm in enumerate(("S1", "G1", "GT1", "S2", "G2", "GT2", "cS1", "cG1")):
            ev = DMA("sync", "d_dbg", d[i], rows[nm][:], rowdeps(nm), ["dbg"])
        P.finish("sync", [ev])
    if stage <= 1:
        P.emit()
        return nc, dbg_outs


    def sc(i):
        return small[:, i:i + 1]
    pid, dd, i32_, isC, ff, inv, invC, invR, sgn, tmpc = [sc(i) for i in range(10)]
    P.op("gpsimd", lambda e: e.iota(small[:, 0:1], pattern=[[0, 1]], base=0, channel_multiplier=1,
                                    allow_small_or_imprecise_dtypes=True), (), ["small"])
    TS("vector", tmpc, pid, 64.0, -64.0, ALU.is_ge, ALU.mult, ["small"], ["small"])
    TT("vector", dd, pid, tmpc, ALU.add, ["small"], ["small"])
    TS("vector", sgn, dd, 32.0, None, ALU.is_ge, None, ["small"], ["small"])
    TS("vector", tmpc, sgn, -32.0, None, ALU.mult, None, ["small"], ["small"])
    TT("vector", i32_, dd, tmpc, ALU.add, ["small"], ["small"])
    TS("vector", isC, i32_, 16.0, None, ALU.is_ge, None, ["small"], ["small"])
    TS("vector", tmpc, isC, -16.0, None, ALU.mult, None, ["small"], ["small"])
    TT("vector", ff, i32_, tmpc, ALU.add, ["small"], ["small"])
    ACT(inv, ff, AF.Exp, ["small"], ["small"], scale=-float(np.log(10000.0) / 16.0))
    TT("vector", invC, inv, isC, ALU.mult, ["small"], ["small"])
    TT("vector", invR, inv, invC, ALU.subtract, ["small"], ["small"])
    TS("vector", sgn, sgn, 2.0, -1.0, ALU.mult, ALU.add, ["small"], ["small"])
    rrA = P.sb("rrA", [128, W], F32, off=qT_off)
    rrI = P.sb("rrI", [128, W], I32, off=qT_off + W * 4)
    ang = P.sb("ang", [128, W], F32, off=uT_off)
    P.op("gpsimd", lambda e: e.iota(COS[:], pattern=[[1, 36], [0, 64]], base=0, channel_multiplier=0,
                                    allow_small_or_imprecise_dtypes=True), (), ["COS"])
    P.op("gpsimd", lambda e: e.iota(SINS[:], pattern=[[0, 36], [1, 64]], base=0, channel_multiplier=0,
                                    allow_small_or_imprecise_dtypes=True), (), ["SINS"])
    HW_ = W // 2
    TWO_PI = float(2 * np.pi)
    for hh in range(2):
        sl = slice(hh * HW_, (hh + 1) * HW_)
        TS("vector", COS[:, sl], COS[:, sl], metat[:, 2:3], None, ALU.add, None, ["COS", "metat"], ["COS"])
        TS("vector", COS[:, sl], COS[:, sl], invR, None, ALU.mult, None, ["COS", "small"], ["COS"])
        TS("vector", SINS[:, sl], SINS[:, sl], invC, None, ALU.mult, None, ["SINS", "small"], ["SINS"])
    TT("vector", ang[:], COS[:], SINS[:], ALU.add, ["COS", "SINS"], ["ang"])

    def range_reduce_sin(dst, dstn, offset):
        TS("vector", rrA[:], ang[:], 1.0 / TWO_PI, offset / TWO_PI + 8.5, ALU.mult, ALU.add, ["ang"], ["rrA"])
        CP("vector", rrI[:], rrA[:], ["rrA"], ["rrI"])
        CP("vector", rrA[:], rrI[:], ["rrI"], ["rrA"])
        TS("vector", rrA[:], rrA[:], -TWO_PI, 8 * TWO_PI + offset, ALU.mult, ALU.add, ["rrA"], ["rrA"])
        TT("vector", dst[:], ang[:], rrA[:], ALU.add, ["ang", "rrA"], [dstn])
        TS("vector", rrA[:], dst[:], float(np.pi), -TWO_PI, ALU.is_gt, ALU.mult, [dstn], ["rrA"])
        TT("vector", dst[:], dst[:], rrA[:], ALU.add, [dstn, "rrA"], [dstn])
        TS("vector", rrA[:], dst[:], -float(np.pi), TWO_PI, ALU.is_lt, ALU.mult, [dstn], ["rrA"])
        TT("vector", dst[:], dst[:], rrA[:], ALU.add, [dstn, "rrA"], [dstn])
        ACT(dst[:], dst[:], AF.Sin, [dstn], [dstn])

    range_reduce_sin(SINS, "SINS", 0.0)
    range_reduce_sin(COS, "COS", float(np.pi / 2))
    for hh in range(2):
        sl = slice(hh * HW_, (hh + 1) * HW_)
        TS("vector", SINS[:, sl], SINS[:, sl], sgn, None, ALU.mult, None, ["SINS", "small"], ["SINS"])

    w_in_v = w_in.rearrange("(k p) n -> p k n", p=128)
    DMA("sync", "d_stg", stg[:], w_in_v[:, :, 0:640], [], ["stg"])
    qd = WIN[:, :, 0:512].rearrange("p k (c h d) -> p k c h d", c=4, h=2, d=64)
    qs = stg[:, :, 0:512].rearrange("p k (h c d) -> p k c h d", h=2, c=4, d=64)
    for h in range(2):
        ACT(qd[:, :, :, h, :], qs[:, :, :, h, :], AF.Copy, ["stg"], [("WIN", "q", h)])
    qd2 = WIN[:, :, 512:1024].rearrange("p k (c h s d) -> p k c h s d", c=4, h=2, s=2, d=32)
    qs2 = stg[:, :, 0:512].rearrange("p k (h c s d) -> p k c h s d", h=2, c=4, s=2, d=32)
    for h in range(2):
        for s in range(2):
            ACT(qd2[:, :, :, h, s, :], qs2[:, :, :, h, 1 - s, :], AF.Copy, ["stg"], [("WIN", "qsw", h, s)])
    ACT(WIN[:, :, 1024:1152], stg[:, :, 512:640], AF.Copy, ["stg"], [("WIN", "k")])
    kd2 = WIN[:, :, 1152:1280].rearrange("p k (h s d) -> p k h s d", h=2, s=2, d=32)
    ks2 = stg[:, :, 512:640].rearrange("p k (h s d) -> p k h s d", h=2, s=2, d=32)
    for s in range(2):
        ACT(kd2[:, :, :, s, :], ks2[:, :, :, 1 - s, :], AF.Copy, ["stg"], [("WIN", "ksw", s)])
    WINQ = [("WIN", "q", 0), ("WIN", "q", 1)]
    WINQS = [("WIN", "qsw", h, s) for h in range(2) for s in range(2)]
    WINK = [("WIN", "k")]
    WINKS = [("WIN", "ksw", 0), ("WIN", "ksw", 1)]
    for (nm, d0, s0, n) in (("v", 1280, 640, 128), ("bg", 1408, 768, 512), ("cg", 1920, 1280, 512), ("hv", 2432, 1792, 512)):
        P.dma("gpsimd", "d_win_" + nm, (lambda d0=d0, s0=s0, n=n: (lambda e: e.dma_start(out=WIN[:, :, d0:d0 + n], in_=w_in_v[:, :, s0:s0 + n])))(),
              [], [("WIN", nm)])
    for kk in range(3):
        for c4 in range(4):
            P.dma("sync", "d_cw", (lambda kk=kk, c4=c4: (lambda e: e.dma_start(
                out=cw[:, c4, kk:kk + 1], in_=conv_w[kk, c4 * 128:(c4 + 1) * 128].rearrange("(p o) -> p o", o=1))))(),
                [], [("cw", kk, c4)])
    MSET("vector", Vt[:, :, :, 64:65], 1.0, [("Vt", "ones")])
    MSET("vector", Vc[:, :, :, 64:65], 1.0, [("Vc", "ones")])

    xt_rr = [0]

    def norm_mod(src_rows, Gn, Sn, hbuf, hname, extra_r=()):
        i = xt_rr[0] % 2
        xt_rr[0] += 1
        xtile, xn = xt[i], f"xt{i}"
        DMA("sync", "d_" + xn, xtile[:], src_rows, list(extra_r), [xn])
        norm_mod_sb(xtile, xn, Gn, Sn, hbuf, hname)
        return xtile, xn

    def norm_mod_sb(xtile, xn, Gn, Sn, hbuf, hname):
        ss = small[:, 16:17]
        rstd = small[:, 17:18]
        ACT(tf[:], xtile[:], AF.Square, [xn], ["tf"])
        RED("vector", ss, tf[:], ALU.add, ["tf"], ["ss"])
        TS("vector", rstd, ss, 1.0 / 1024.0, 1e-6, ALU.mult, ALU.add, ["ss"], ["rstd"])
        ACT(rstd, rstd, AF.Ln, ["rstd"], ["rstd"])
        ACT(rstd, rstd, AF.Exp, ["rstd"], ["rstd"], scale=-0.5)
        ACT(tf[:], xtile[:], AF.Copy, [xn, "rstd"], ["tf"], scale=rstd)
        TT("vector", tf[:], tf[:], rows[Gn][:], ALU.mult, ["tf"] + rowdeps(Gn), ["tf"])
        TT("gpsimd", hbuf[:], tf[:], rows[Sn][:], ALU.add, ["tf"] + rowdeps(Sn), [hname])

    def transpose_to(hbuf, hname, dst, dst_name):
        pb, pbn = psb()
        pbv = pb[:].rearrange("p (k t) -> p k t", k=8)
        for k in range(8):
            TR(pbv[:, k, :], hbuf[:, k * 128:(k + 1) * 128], ident_b[:], [hname, "ident_b"], [(pbn, k)])
        ACT(dst, pbv, AF.Copy, [(pbn, k) for k in range(8)], [dst_name])

    hcT = hT[0]
    for t in range(2):
        norm_mod(ctx[t * 128:(t + 1) * 128, :], "cG1", "cS1", hb[t % 2], f"hb{t % 2}")
        transpose_to(hb[t % 2], f"hb{t % 2}", hcT[:, :, t * 128:(t + 1) * 128], ("hT0", t))
    ps, psn = psf("a")
    for k in range(8):
        MM(ps[:, 0:256], WIN[:, k, 1024:1152], hcT[:, k, 0:256], k == 0, k == 7,
           WINK + [("hT0", 0), ("hT0", 1)], [psn])
    ACT(kcT[:], ps[:, 0:256], AF.Copy, [psn], ["kcT"])
    for t in range(2):
        ps, psn = psf("a")
        for k in range(8):
            MM(ps[:, 0:128], hcT[:, k, t * 128:(t + 1) * 128], WIN[:, k, 1280:1408], k == 0, k == 7,
               [("WIN", "v"), ("hT0", t)], [psn])
        ACT(Vc[:, t, :, 0:64], ps[:, 0:128].rearrange("p (h d) -> p h d", h=2), AF.Copy, [psn], [("Vc", t)])

    if "ctx" in dbg:
        d1 = dbg_out("kcT", [128, 256], BF16)
        d2 = dbg_out("Vc", [128, 2 * 2 * 65], BF16)
        e1 = DMA("sync", "d_dbg", d1, kcT[:], ["kcT"], ["dbg1"])
        e2 = DMA("sync", "d_dbg", d2, Vc[:].rearrange("p a b c -> p (a b c)"), [("Vc", 0), ("Vc", 1), ("Vc", "ones")], ["dbg2"])
        P.finish("sync", [e1, e2])
    if stage <= 2:
        P.emit()
        return nc, dbg_outs

    chunks = [(0, 128, False)] + [(128 + 512 * i, 512, True) for i in range(4)] + [(2176, 128, False)]
    NCONV = int(os.environ.get("MK_NCONV", "4"))
    for ci, (w0, n, central) in enumerate(chunks):
        if (stage <= 2.5 and ci >= 1) or (stage <= 2.7 and ci >= 2):
            break
        hTc, hTn = hT[ci % 2], f"hT{ci % 2}"
        ntile = n // 128
        for t in range(ntile):
            j = (ci * 4 + t) % 2
            norm_mod(x[w0 + t * 128:w0 + (t + 1) * 128, :], "G1", "S1", hb[j], f"hb{j}")
            transpose_to(hb[j], f"hb{j}", hTc[:, :, t * 128:(t + 1) * 128], (hTn, t))
        hdeps = [(hTn, t) for t in range(ntile)]

        def proj(col0, wdeps, cons):
            ps, psn = psf(cons)
            for k in range(8):
                MM(ps[:, 0:n], WIN[:, k, col0:col0 + 128], hTc[:, k, 0:n], k == 0, k == 7, wdeps + hdeps, [psn])
            return ps, psn

        def rope_out(col0, colsw, wd, wsd, dst, dstn):
            pa, pan = proj(col0, wd, "v")
            pb_, pbn_ = proj(colsw, wsd, "v")
            TT("vector", rt1[:, 0:n], pa[:, 0:n], COS[:, w0:w0 + n], ALU.mult, [pan, "COS"], ["rt1"])
            TT("vector", rt2[:, 0:n], pb_[:, 0:n], SINS[:, w0:w0 + n], ALU.mult, [pbn_, "SINS"], ["rt2"])
            TT("gpsimd", dst, rt1[:, 0:n], rt2[:, 0:n], ALU.add, ["rt1", "rt2"], [dstn])

        if central:
            for c in range(4):
                rope_out(c * 128, 512 + c * 128, WINQ, WINQS, qT[:, c, w0:w0 + n], ("qT", c, ci))
        PARTS = os.environ.get("MK_PARTS", "rvc")
        if "r" in PARTS:
            rope_out(1024, 1152, WINK, WINKS, kT[:, w0:w0 + n], ("kT", ci))
        for t in (range(ntile) if "v" in PARTS else ()):
            ps, psn = psf("a")
            for k in range(8):
                MM(ps[:, 0:128], hTc[:, k, t * 128:(t + 1) * 128], WIN[:, k, 1280:1408], k == 0, k == 7,
                   [("WIN", "v"), (hTn, t)], [psn])
            wt = w0 // 128 + t
            ACT(Vt[:, wt, :, 0:64], ps[:, 0:128].rearrange("p (h d) -> p h d", h=2), AF.Copy, [psn], [("Vt", wt)])
        for c in (range(NCONV) if "c" in PARTS else ()):
            if central:
                ps, psn = proj(1408 + c * 128, [("WIN", "bg")], "a")
                ACT(bgT[:, c, w0 - 128:w0 - 128 + n], ps[:, 0:n], AF.Copy, [psn], [("bgT", c, ci)])
            pc, pcn = proj(1920 + c * 128, [("WIN", "cg")], "a")
            ph, phn = proj(2432 + c * 128, [("WIN", "hv")], "v")
            ACT(cgs[:, 0:n], pc[:, 0:n], AF.Copy, [pcn], ["cgs"])
            TT("vector", uT[:, c, w0:w0 + n], ph[:, 0:n], cgs[:, 0:n], ALU.mult, [phn, "cgs"], [("uT", c, ci)])

    if "proj" in dbg:
        d1 = dbg_out("qT", [128, 4 * W], BF16)
        d2 = dbg_out("kT", [128, W], BF16)
        d3 = dbg_out("Vt", [128, 18 * 130], BF16)
        d4 = dbg_out("uT", [128, 4 * W], BF16)
        d5 = dbg_out("bgT", [128, 4 * 2048], BF16)
        allq = [("qT", c, ci) for c in range(4) for ci in range(1, 5)]
        allk = [("kT", ci) for ci in range(6)]
        allv = [("Vt", t) for t in range(18)] + [("Vt", "ones")]
        allu = [("uT", c, ci) for c in range(4) for ci in range(6)]
        allb = [("bgT", c, ci) for c in range(4) for ci in range(1, 5)]
        evs = [DMA("sync", "d_dbg", d1, qT[:].rearrange("p a b -> p (a b)"), allq, ["dbg1"]),
               DMA("sync", "d_dbg", d2, kT[:], allk, ["dbg2"]),
               DMA("sync", "d_dbg", d3, Vt[:].rearrange("p a b c -> p (a b c)"), allv, ["dbg3"]),
               DMA("sync", "d_dbg", d4, uT[:].rearrange("p a b -> p (a b)"), allu, ["dbg4"]),
               DMA("sync", "d_dbg", d5, bgT[:].rearrange("p a b -> p (a b)"), allb, ["dbg5"])]
        P.finish("sync", evs)
    if stage <= 3:
        P.emit()
        return nc, dbg_outs

    ALLWIN = WINQ + WINQS + WINK + WINKS + [("WIN", nm) for nm in ("v", "bg", "cg", "hv")]
    o2 = REG0 + 32768
    PL = [P.sb(f"PL{i}", [128, 384], BF16, off=o2 + i * 768) for i in range(2)]; o2 += 1536
    PC = [P.sb(f"PC{i}", [128, 256], BF16, off=o2 + i * 512) for i in range(2)]; o2 += 1024
    att_tm = P.sb("att_tm", [128, 512], BF16, off=o2); o2 += 1024
    rec = P.sb("rec", [128, 8], F32, off=o2); o2 += 64
    cvt = [P.sb(f"cvt{i}", [128, 512], F32, off=o2 + i * 2048) for i in range(2)]; o2 += 4096
    assert o2 <= REG0 + 47104

    def kchunk(wb):
        return 0 if wb == 0 else (5 if wb == 17 else 1 + (wb - 1) // 4)

    VONES = [("Vt", "ones")]
    for i in range(1, 17):
        ci_q = 1 + (i - 1) // 4
        mv = 1 if i == 1 else (2 if i == 16 else 0)
        pvs = [psf("v"), psf("v")]
        for hn in range(8):
            half, c = hn // 4, hn % 4
            r0 = half * 64
            j = hn % 2
            sl, sln = psf("a")
            sc_, scn = psf("a")
            qsl = qT[r0:r0 + 64, c, i * 128:(i + 1) * 128]
            for kb in range(3):
                wb = i - 1 + kb
                MM(sl[:, kb * 128:(kb + 1) * 128], kT[r0:r0 + 64, wb * 128:(wb + 1) * 128], qsl, True, True,
                   [("qT", c, ci_q), ("kT", kchunk(wb))], [sln])
            for cb in range(2):
                MM(sc_[:, cb * 128:(cb + 1) * 128], kcT[r0:r0 + 64, cb * 128:(cb + 1) * 128], qsl, True, True,
                   [("qT", c, ci_q), "kcT"], [scn])
            ACT(PL[j][:], sl[:, 0:384], AF.Exp, [sln], [f"PL{j}"] + ALLWIN, scale=0.125)
            ACT(PC[j][:], sc_[:, 0:256], AF.Exp, [scn], [f"PC{j}"] + ALLWIN, scale=0.125)
            TT("vector", PL[j][:], PL[j][:], mask3[:, mv, :], ALU.mult,
               [f"PL{j}", ("mask3", mv), ("mask3", mv, 1), ("mask3", mv, 2)], [f"PL{j}"])
            pv, pvn = pvs[half]
            pvr = pv[:, c * 65:(c + 1) * 65]
            for kb in range(3):
                wb = i - 1 + kb
                MM(pvr, PL[j][:, kb * 128:(kb + 1) * 128], Vt[:, wb, half, :], kb == 0, False,
                   [f"PL{j}", ("Vt", wb)] + VONES, [pvn])
            for cb in range(2):
                MM(pvr, PC[j][:, cb * 128:(cb + 1) * 128], Vc[:, cb, half, :], False, cb == 1,
                   [f"PC{j}", ("Vc", cb), ("Vc", "ones")], [pvn])
        for b in range(2):
            pv, pvn = pvs[b]
            pvv = pv[:, 0:260].rearrange("p (h e) -> p h e", h=4)
            TT("vector", rec[:, b * 4:(b + 1) * 4].unsqueeze(2), pvv[:, :, 64:65], esink[:, b * 4:(b + 1) * 4].unsqueeze(2),
               ALU.add, [pvn, "esink"], [("rec", b)] + ALLWIN)
            P.op("vector", (lambda b=b: (lambda e: e.reciprocal(rec[:, b * 4:(b + 1) * 4], rec[:, b * 4:(b + 1) * 4])))(),
                 [("rec", b)], [("rec", b)])
            TT("vector", att_tm[:, b * 256:(b + 1) * 256].rearrange("p (h d) -> p h d", h=4), pvv[:, :, 0:64],
               rec[:, b * 4:(b + 1) * 4].unsqueeze(2).to_broadcast([128, 4, 64]), ALU.mult,
               [pvn, ("rec", b)], [("att_tm", b)] + ALLWIN)
        pb, pbn = psb()
        pbv = pb[:, 0:512].rearrange("p (k t) -> p k t", k=4)
        for cc in range(4):
            TR(pbv[:, cc, :], att_tm[:, cc * 128:(cc + 1) * 128], ident_b[:], [("att_tm", cc // 2), "ident_b"], [(pbn, cc)])
        ACT(mixT[:, 0:4, (i - 1) * 128:i * 128], pbv, AF.Copy, [(pbn, cc) for cc in range(4)],
            [("mixT", "att", i - 1)] + ALLWIN)

    TS("gpsimd", uT[:, :, 127:128], uT[:, :, 127:128], metat[:, 0:1], None, ALU.mult, None,
       [("uT", c, 0) for c in range(4)] + ["metat"], [("uT", c, 0) for c in range(4)])
    TS("gpsimd", uT[:, :, 2176:2177], uT[:, :, 2176:2177], metat[:, 1:2], None, ALU.mult, None,
       [("uT", c, 5) for c in range(4)] + ["metat"], [("uT", c, 5) for c in range(4)])
    for tcn in range(4):
        w0 = 128 + tcn * 512
        for c in range(4):
            ud = [("uT", c, ci) for ci in (tcn, tcn + 1, tcn + 2)]
            cwd = [("cw", kk, c4) for kk in range(3) for c4 in range(4)]
            TS("gpsimd", cvt[0][:], uT[:, c, w0 - 1:w0 + 511], cw[:, c, 0:1], None, ALU.mult, None, ud + cwd, ["cvt0"] + ALLWIN)
            TS("gpsimd", cvt[1][:], uT[:, c, w0:w0 + 512], cw[:, c, 1:2], None, ALU.mult, None, ud + cwd, ["cvt1"] + ALLWIN)
            TT("gpsimd", cvt[0][:], cvt[0][:], cvt[1][:], ALU.add, ["cvt0", "cvt1"], ["cvt0"])
            TS("gpsimd", cvt[1][:], uT[:, c, w0 + 1:w0 + 513], cw[:, c, 2:3], None, ALU.mult, None, ud + cwd, ["cvt1"])
            TT("gpsimd", cvt[0][:], cvt[0][:], cvt[1][:], ALU.add, ["cvt0", "cvt1"], ["cvt0"])
            TT("gpsimd", mixT[:, 4 + c, tcn * 512:(tcn + 1) * 512], cvt[0][:], bgT[:, c, tcn * 512:(tcn + 1) * 512], ALU.mult,
               ["cvt0", ("bgT", c, tcn + 1)], [("mixT", "conv", c, tcn)] + ALLWIN)

    if stage <= 4:
        P.emit()
        return nc, dbg_outs

    HTALL = [(f"hT{a}", t) for a in range(2) for t in range(4)]
    P.dma("gpsimd", "d_wo", lambda e: e.dma_start(out=wo[:], in_=w_out.rearrange("(k p) n -> p k n", p=128)), [], ["wo"] + HTALL)
    QALL = [("qT", c, ci) for c in range(4) for ci in range(1, 5)]
    o3 = qT_off
    o3 += 4096
    h2Tall = P.sb("h2Tall", [128, 8, 2048], BF16, off=bgT_off)
    CONVDEAD = [("uT", c, ci) for c in range(4) for ci in range(6)] + [("bgT", c, ci) for c in range(4) for ci in range(1, 5)]
    rows_off = REG0 - 8 * 4096
    affTM = P.sb("affTM", [128, 16, 16], F32, off=rows_off)
    gm = P.sb("gm", [128, 16, 16], F32, off=rows_off + 1024)
    thr = P.sb("thr", [128, 16], F32, off=rows_off + 2048)
    affT = P.sb("affT", [16, 2048], F32, off=o3); o3 += 8192
    wr = P.sb("wr", [128, 8, 16], BF16, off=o3); o3 += 256
    sm = P.sb("sm", [128, 64], F32, off=o3); o3 += 256
    assert o3 <= qT_off + 4 * W * 2
    P.dma("gpsimd", "d_wr", lambda e: e.dma_start(out=wr[:], in_=w_router.rearrange("(k p) e -> p k e", p=128)), [], ["wr"] + QALL)
    for tile in range(16):
        tcn = tile // 4
        mdeps = [("mixT", "att", tile)] + [("mixT", "conv", c, tcn) for c in range(4)]
        i = xt_rr[0] % 2
        xt_rr[0] += 1
        xtile, xn = xt[i], f"xt{i}"
        DMA("sync", "d_" + xn, xtile[:], x[128 + tile * 128:256 + tile * 128, :], [], [xn])
        for n in range(2):
            ps, psn = psf("v")
            for k in range(8):
                MM(ps[:], mixT[:, k, tile * 128:(tile + 1) * 128], wo[:, k, n * 512:(n + 1) * 512], k == 0, k == 7,
                   mdeps + ["wo"], [psn])
            TT("vector", tf[:, n * 512:(n + 1) * 512], ps[:], rows["GT1"][:, n * 512:(n + 1) * 512], ALU.mult,
               [psn] + rowdeps("GT1"), ["tf"])
        TT("gpsimd", xtile[:], xtile[:], tf[:], ALU.add, [xn, "tf"], [xn])
        DMA("sync", "d_x1d_" + xn, x1d[tile * 128:(tile + 1) * 128, :], xtile[:], [xn], [("x1d", tile)])
        j = tile % 2
        norm_mod_sb(xtile, xn, "G2", "S2", hb[j], f"hb{j}")
        DMA("sync", f"d_h2loc{j}", h2loc[tile * 128:(tile + 1) * 128, :], hb[j][:], [f"hb{j}"], [("h2loc", tile)])
        h2v = h2Tall[:, :, tile * 128:(tile + 1) * 128]
        pb, pbn = psb()
        pbv = pb[:].rearrange("p (k t) -> p k t", k=8)
        for k in range(8):
            TR(pbv[:, k, :], hb[j][:, k * 128:(k + 1) * 128], ident_b[:], [f"hb{j}", "ident_b"], [(pbn, k)])
        ACT(h2v, pbv, AF.Copy, [(pbn, k) for k in range(8)], [("h2T", tile)] + CONVDEAD)
        ps, psn = psf("v")
        for k in range(8):
            MM(ps[:, 0:16], h2Tall[:, k, tile * 128:(tile + 1) * 128], wr[:, k, :], k == 0, k == 7, [("h2T", tile), "wr"], [psn])
        mx, nmx, ssum, ex = sm[:, 0:1], sm[:, 1:2], sm[:, 2:3], sm[:, 16:32]
        af = affTM[:, tile, :]
        RED("vector", mx, ps[:, 0:16], ALU.max, [psn], ["sm_mx"])
        TS("vector", nmx, mx, -1.0, None, ALU.mult, None, ["sm_mx"], ["sm_nmx"])
        ACT(ex, ps[:, 0:16], AF.Exp, [psn, "sm_nmx"], ["sm_ex"], bias=nmx)
        RED("vector", ssum, ex, ALU.add, ["sm_ex"], ["sm_sum"])
        P.op("vector", lambda e: e.reciprocal(sm[:, 2:3], sm[:, 2:3]), ["sm_sum"], ["sm_sum"])
        TS("vector", af, ex, ssum, None, ALU.mult, None, ["sm_ex", "sm_sum"], [("affTM", tile)])
        pt, ptn = psf("a")
        TR(pt[0:16, 0:128], af, ident_f[:], [("affTM", tile), "ident_f"], [ptn])
        ACT(affT[:, tile * 128:(tile + 1) * 128], pt[0:16, 0:128], AF.Copy, [ptn], [("affT", tile)] + QALL)
    DMA("sync", "d_affloc", affloc, affT[:], [("affT", t) for t in range(16)], ["affloc"])

    if "a3" in dbg:
        d1 = dbg_out("x1", [2048, 1024])
        d2 = dbg_out("aff", [16, 2048])
        d3 = dbg_out("h2", [2048, 1024], BF16)
        e1 = DMA("sync", "d_dbg1", d1, x1d, [("x1d", t) for t in range(16)], ["dbg1"])
        e2 = DMA("sync", "d_dbg2", d2, affloc, ["affloc"], ["dbg2"])
        e3 = DMA("sync", "d_dbg3", d3, h2loc, [("h2loc", t) for t in range(16)], ["dbg3"])
        P.finish("sync", [e1, e2, e3])
    if stage <= 5:
        P.emit()
        return nc, dbg_outs

    FENCE = [k for k in P.res.keys() if not (isinstance(k, str) and (k.startswith("psf") or k in ("ident_b", "ident_f", "ones_b", "metat")))
             and not (isinstance(k, tuple) and k[0] in ("row", "h2T", "affTM", "x1d", "h2loc"))]
    NTB, NITB = 8, 8
    P.dma("gpsimd", "d_ag_aff", lambda e: e.collective_compute("AllGather", ALU.bypass, replica_groups=GROUPS,
                                                               ins=[affloc.opt()], outs=[affall.opt()]),
          ["affloc"], ["affall", "agchain"], inc=1)
    ob = REG0 + 159936 - 0
    ob = bgT_off + 32768
    AFt = P.sb("AFt", [128, 16, 64], F32, off=ob); ob += 4096
    FR = P.sb("FR", [128, 16, NTB], F32, off=ob); ob += 512
    Tt = P.sb("Tt", [128, 16, NTB], F32, off=ob); ob += 512
    tmpa = P.sb("tmpa", [128, 16, NTB], F32, off=ob); ob += 512
    get = P.sb("get", [128, 16, NTB], F32, off=ob); ob += 512
    cntb = P.sb("cntb", [128, 16 * NTB], BF16, off=ob); ob += 256
    lo = P.sb("lo", [128, 16], F32, off=ob); ob += 64
    hi = P.sb("hi", [128, 16], F32, off=ob); ob += 64
    wdt = P.sb("wdt", [128, 16], F32, off=ob); ob += 64
    red = P.sb("red", [128, 16], F32, off=ob); ob += 64
    idxt = P.sb("idxt", [128, 16], I32, off=ob); ob += 64
    assert ob <= rt1_off + 6144
    cmpb = P.sb("cmpb", [128, 16, NTB, 64], BF16, off=REG0 + 49152)
    for r in range(4):
        DMA("sync", "d_AFt", AFt[32 * r:32 * (r + 1), :, :],
            affall[r * 16:(r + 1) * 16, :].rearrange("e (p j) -> p e j", p=32, j=64), ["affall"], [("AFt", r)] + FENCE)
    AFD = [("AFt", r) for r in range(4)]
    P.op("gpsimd", lambda e: e.iota(FR[:], pattern=[[0, 16], [1, NTB]], base=1, channel_multiplier=0,
                                    allow_small_or_imprecise_dtypes=True), (), ["FR"] + FENCE)
    P.op("gpsimd", lambda e: e.iota(idxt[:], pattern=[[128, 16]], base=0, channel_multiplier=1), (), ["idxt"] + FENCE)
    TS("vector", FR[:], FR[:], 1.0 / (NTB + 1), None, ALU.mult, None, ["FR"], ["FR"])
    MSET("vector", lo[:], 0.0, ["lo"] + FENCE)
    MSET("vector", hi[:], 1.0, ["hi"])
    for it in range(NITB):
        TT("vector", wdt[:], hi[:], lo[:], ALU.subtract, ["hi", "lo"], ["wdt"])
        TT("vector", Tt[:], FR[:], wdt[:].unsqueeze(2).to_broadcast([128, 16, NTB]), ALU.mult, ["FR", "wdt"], ["Tt"])
        TT("vector", Tt[:], Tt[:], lo[:].unsqueeze(2).to_broadcast([128, 16, NTB]), ALU.add, ["Tt", "lo"], ["Tt"])
        TT("vector", cmpb[:], AFt[:].unsqueeze(2).to_broadcast([128, 16, NTB, 64]),
           Tt[:].unsqueeze(3).to_broadcast([128, 16, NTB, 64]), ALU.is_ge, AFD + ["Tt"], ["cmpb"] + FENCE)
        RED("vector", tmpa[:], cmpb[:], ALU.add, ["cmpb"], ["tmpa"])
        CP("vector", cntb[:], tmpa[:].rearrange("p e k -> p (e k)"), ["tmpa"], ["cntb"])
        ps, psn = psf("v")
        MM(ps[:, 0:16 * NTB], ones_b[:], cntb[:], True, True, ["cntb", "ones_b"], [psn])
        TS("vector", get[:].rearrange("p e k -> p (e k)"), ps[:, 0:16 * NTB], 1024.0, None, ALU.is_ge, None, [psn], ["get"])
        TT("vector", tmpa[:], Tt[:], get[:], ALU.mult, ["Tt", "get"], ["tmpa"])
        RED("vector", red[:], tmpa[:], ALU.max, ["tmpa"], ["red"])
        TT("vector", lo[:], lo[:], red[:], ALU.max, ["lo", "red"], ["lo"])
        TS("vector", tmpa[:], get[:], 2.0, None, ALU.mult, None, ["get"], ["tmpa"])
        TT("vector", tmpa[:], tmpa[:], Tt[:], ALU.add, ["tmpa", "Tt"], ["tmpa"])
        RED("vector", red[:], tmpa[:], ALU.min, ["tmpa"], ["red"])
        TT("vector", hi[:], hi[:], red[:], ALU.min, ["hi", "red"], ["hi"])
    CP("vector", thr[:], lo[:], ["lo"], ["thr"])
    AFFTM = [("affTM", t) for t in range(16)]
    TT("vector", gm[:], affTM[:], thr[:].unsqueeze(1).to_broadcast([128, 16, 16]), ALU.is_ge, AFFTM + ["thr"], ["gm"])
    TT("vector", gm[:], gm[:], affTM[:], ALU.mult, ["gm"] + AFFTM, ["gm"])

    if "thr" in dbg:
        d1 = dbg_out("thr", [128, 16])
        d2 = dbg_out("gm", [128, 256])
        e1 = DMA("sync", "d_dbg1", d1, thr[:], ["thr"], ["dbg1"])
        e2 = DMA("sync", "d_dbg2", d2, gm[:].rearrange("p a b -> p (a b)"), ["gm"], ["dbg2"])
        P.finish("sync", [e1, e2])
    if stage <= 6:
        P.emit()
        return nc, dbg_outs

    for j4 in range(4):
        P.dma("gpsimd", "d_ag_h2", (lambda j4=j4: (lambda e: e.collective_compute(
            "AllGather", ALU.bypass, replica_groups=GROUPS,
            ins=[h2loc[j4 * 512:(j4 + 1) * 512, :].opt()], outs=[h2all[j4 * 2048:(j4 + 1) * 2048, :].opt()])))(),
            [("h2loc", t) for t in range(j4 * 4, j4 * 4 + 4)] + ["agchain"], [("h2all", j4), "agchain"], inc=1)
    H2ALLD = [("h2all", j4) for j4 in range(4)]
    TAB = P.sb("TAB", [128, 16, 128], F32, off=qT_off)
    ob2 = kT_off
    ones64 = P.sb("ones64", [128, 64], F32, off=ob2); ob2 += 256
    n4 = P.sb("n4", [128, 4], F32, off=ob2); ob2 += 64
    t16 = P.sb("t16", [128, 16], F32, off=ob2); ob2 += 64
    rhsU = P.sb("rhsU", [128, 4, 128], BF16, off=ob2); ob2 += 1024
    rhsI = P.sb("rhsI", [128, 4, 128], BF16, off=ob2); ob2 += 1024
    offs_sb = P.sb("offs_sb", [128, 4, 128], F32, off=ob2); ob2 += 2048
    nrow_sb = P.sb("nrow_sb", [128, 4, 128], F32, off=ob2); ob2 += 2048
    sval = P.sb("sval", [128, 8], F32, off=ob2); ob2 += 64
    koffs = P.sb("koffs", [128, 4, 8], F32, off=ob2); ob2 += 128
    pS = P.sb("pS", [128, 4, 8], F32, off=ob2); ob2 += 128
    oex = P.sb("oex", [128, 4, 8], F32, off=ob2); ob2 += 128
    rS = P.sb("rS", [128, 4, 8], F32, off=ob2); ob2 += 128
    jS = P.sb("jS", [128, 4, 8], F32, off=ob2); ob2 += 128
    gS = P.sb("gS", [128, 4, 8], F32, off=ob2); ob2 += 128
    tSf = P.sb("tSf", [128, 4, 8], F32, off=ob2); ob2 += 128
    RIDX = P.sb("RIDX", [128, 4, 8], I32, off=ob2); ob2 += 128
    TIDX = P.sb("TIDX", [128, 4, 8], I32, off=ob2); ob2 += 128
    ZIDXf = P.sb("ZIDXf", [128, 4, 16], F32, off=ob2); ob2 += 256
    ZIDX = P.sb("ZIDX", [128, 4, 16], I32, off=ob2); ob2 += 256
    yz = [P.sb(f"yz{i}", [128, 1024], BF16, off=rows_off + 3 * 4096 + i * 2048) for i in range(2)]
    assert ob2 <= bgT_off, ob2
    cmpP = P.sb("cmpP", [128, 4, 8, 128], F32, off=REG0 + 32768)
    Gt = P.sb("Gt", [128, 4, 8, 128], F32, off=REG0 + 49152)

    MSET("vector", ones64[:], 1.0, ["ones64"] + FENCE)
    TT("vector", TAB[:, :, 64:128], AFt[:], thr[:].unsqueeze(2).to_broadcast([128, 16, 64]), ALU.is_ge, AFD + ["thr"], ["TABm"] + FENCE)
    for e16 in range(16):
        P.op("vector", (lambda e16=e16: (lambda e: e.tensor_tensor_scan(out=TAB[:, e16, 0:64], data0=ones64[:], data1=TAB[:, e16, 64:128],
                                                                          initial=0.0, op0=ALU.mult, op1=ALU.add)))(),
             ["TABm", "ones64"], [("TABc", e16)])
    TABC = [("TABc", e16) for e16 in range(16)]
    TT("vector", TAB[:, :, 64:128], TAB[:, :, 64:128], AFt[:], ALU.mult, ["TABm"] + TABC + AFD, ["TABm"])
    DMA("sync", "d_tabd", tabd.rearrange("(e p) c -> p e c", p=128), TAB[:], ["TABm"] + TABC, ["tabd"])
    selv = metat[:, 8:72].rearrange("p (e k) -> p e k", k=4)
    for k in range(4):
        TT("vector", t16[:], TAB[:, :, 63], selv[:, :, k], ALU.mult, TABC + ["metat"], ["t16"])
        RED("vector", n4[:, k:k + 1], t16[:], ALU.add, ["t16"], [("n4", k)])
    N4 = [("n4", k) for k in range(4)]
    TT("vector", rhsU[:], n4[:].unsqueeze(2).to_broadcast([128, 4, 128]), U_b[:].unsqueeze(1).to_broadcast([128, 4, 128]), ALU.mult,
       N4 + ["U_b"], ["rhsU"])
    TT("vector", rhsI[:], n4[:].unsqueeze(2).to_broadcast([128, 4, 128]), ident_b[:].unsqueeze(1).to_broadcast([128, 4, 128]), ALU.mult,
       N4 + ["ident_b"], ["rhsI"])
    ps, psn = psf("v")
    MM(ps[:], ones_b[:], rhsU[:].rearrange("p k q -> p (k q)"), True, True, ["rhsU", "ones_b"], [psn])
    CP("vector", offs_sb[:].rearrange("p k q -> p (k q)"), ps[:], [psn], ["offs_sb"])
    ps, psn = psf("v")
    MM(ps[:], ones_b[:], rhsI[:].rearrange("p k q -> p (k q)"), True, True, ["rhsI", "ones_b"], [psn])
    CP("vector", nrow_sb[:].rearrange("p k q -> p (k q)"), ps[:], [psn], ["nrow_sb"])
    P.op("gpsimd", lambda e: e.iota(sval[:], pattern=[[128, 8]], base=0, channel_multiplier=1,
                                    allow_small_or_imprecise_dtypes=True), (), ["sval"])
    P.op("gpsimd", lambda e: e.iota(koffs[:], pattern=[[128, 4], [0, 8]], base=0, channel_multiplier=0,
                                    allow_small_or_imprecise_dtypes=True), (), ["koffs"])
    P.op("gpsimd", lambda e: e.iota(ZIDXf[:], pattern=[[512, 4], [2048, 4], [128, 4]], base=0, channel_multiplier=1,
                                    allow_small_or_imprecise_dtypes=True), (), ["ZIDXf"])
    TS("vector", koffs[:], koffs[:], metat[:, 4:5], None, ALU.add, None, ["koffs", "metat"], ["koffs"])
    TS("vector", ZIDXf[:], ZIDXf[:], metat[:, 5:6], None, ALU.add, None, ["ZIDXf", "metat"], ["ZIDXf"])
    CP("vector", ZIDX[:], ZIDXf[:], ["ZIDXf"], ["ZIDX"])
    svb = sval[:].unsqueeze(1).to_broadcast([128, 4, 8])
    TT("vector", cmpP[:], offs_sb[:].unsqueeze(2).to_broadcast([128, 4, 8, 128]),
       svb.unsqueeze(3).to_broadcast([128, 4, 8, 128]), ALU.is_le, ["offs_sb", "sval"], ["cmpP"] + FENCE)
    RED("vector", pS[:], cmpP[:], ALU.add, ["cmpP"], ["pS"])
    TT("vector", cmpP[:], cmpP[:], nrow_sb[:].unsqueeze(2).to_broadcast([128, 4, 8, 128]), ALU.mult, ["cmpP", "nrow_sb"], ["cmpP"])
    RED("vector", oex[:], cmpP[:], ALU.add, ["cmpP"], ["oex"])
    TT("vector", rS[:], svb, oex[:], ALU.subtract, ["sval", "oex"], ["rS"])
    TT("vector", tSf[:], pS[:], koffs[:], ALU.add, ["pS", "koffs"], ["tSf"])
    CP("vector", RIDX[:], tSf[:], ["tSf"], ["RIDX"])
    for k in range(4):
        for c in range(8):
            P.dma("gpsimd", "d_G", (lambda k=k, c=c: (lambda e: e.indirect_dma_start(
                out=Gt[:, k, c, :], out_offset=None, in_=tabd, in_offset=bass.IndirectOffsetOnAxis(ap=RIDX[:, k, c:c + 1], axis=0))))(),
                ["tabd", "RIDX"], [("Gt", k, c), "cmpb"] if (k == 0 and c == 0) else [("Gt", k, c)])
    GALL = [("Gt", k, c) for k in range(4) for c in range(8)]
    cmpG = cmpP[:, :, :, 0:64]
    TT("vector", cmpG, Gt[:, :, :, 0:64], rS[:].unsqueeze(3).to_broadcast([128, 4, 8, 64]), ALU.is_le, GALL + ["rS"], ["cmpP"])
    RED("vector", jS[:], cmpG, ALU.add, ["cmpP"], ["jS"])
    TS("vector", oex[:], rS[:], 1.0, None, ALU.add, None, ["rS"], ["oex"])
    TT("vector", cmpG, Gt[:, :, :, 0:64], oex[:].unsqueeze(3).to_broadcast([128, 4, 8, 64]), ALU.is_equal, GALL + ["oex"], ["cmpP"])
    TT("vector", cmpG, cmpG, Gt[:, :, :, 64:128], ALU.mult, ["cmpP"] + GALL, ["cmpP"])
    RED("vector", gS[:], cmpG, ALU.add, ["cmpP"], ["gS"])
    TS("vector", tSf[:], pS[:], 64.0, None, ALU.mult, None, ["pS"], ["tSf"])
    TT("vector", tSf[:], tSf[:], jS[:], ALU.add, ["tSf", "jS"], ["tSf"])
    CP("vector", TIDX[:], tSf[:], ["tSf"], ["TIDX"])
    ra = P.sb("ra", [128, 4, 8], F32, off=ob2); rb = P.sb("rb", [128, 4, 8], F32, off=ob2 + 128)
    rj = P.sb("rj", [128, 4, 8], F32, off=ob2 + 256); GIDX = P.sb("GIDX", [128, 4, 8], I32, off=ob2 + 384)
    assert ob2 + 512 <= bgT_off
    TS("vector", ra[:], tSf[:], 2048.0, None, ALU.is_ge, None, ["tSf"], ["ra"])
    for thv in (4096.0, 6144.0):
        TS("vector", rb[:], tSf[:], thv, None, ALU.is_ge, None, ["tSf"], ["rb"])
        TT("vector", ra[:], ra[:], rb[:], ALU.add, ["ra", "rb"], ["ra"])
    TS("vector", rb[:], ra[:], -2048.0, None, ALU.mult, None, ["ra"], ["rb"])
    TT("vector", rb[:], rb[:], tSf[:], ALU.add, ["rb", "tSf"], ["rb"])
    TS("vector", rj[:], rb[:], 512.0, None, ALU.is_ge, None, ["rb"], ["rj"])
    for thv in (1024.0, 1536.0):
        TS("vector", oex[:], rb[:], thv, None, ALU.is_ge, None, ["rb"], ["oex"])
        TT("vector", rj[:], rj[:], oex[:], ALU.add, ["rj", "oex"], ["rj"])
    TT("vector", rj[:], rj[:], ra[:], ALU.subtract, ["rj", "ra"], ["rj"])
    TS("vector", rj[:], rj[:], 1536.0, None, ALU.mult, None, ["rj"], ["rj"])
    TT("vector", rj[:], rj[:], tSf[:], ALU.add, ["rj", "tSf"], ["rj"])
    CP("vector", GIDX[:], rj[:], ["rj"], ["GIDX"])

    if "idx" in dbg:
        d1 = dbg_out("tidx", [128, 32])
        d2 = dbg_out("gS", [128, 32])
        e1 = DMA("sync", "d_dbg1", d1, tSf[:].rearrange("p a b -> p (a b)"), ["tSf", "TIDX"], ["dbg1"])
        e2 = DMA("sync", "d_dbg2", d2, gS[:].rearrange("p a b -> p (a b)"), ["gS"], ["dbg2"])
        P.finish("sync", [e1, e2])
    if stage <= 6.5:
        P.emit()
        return nc, dbg_outs

    wslot = [P.sb(f"wslot{i}", [128, 8, 1024], BF16, off=REG0 + i * 16384) for i in range(4)]
    wdt_ = P.sb("wd_", [128, 8, 1024], BF16, off=REG0 + 81920)
    hid = [P.sb(f"hid{i}", [128, 8, 512], BF16, off=qT_off + i * 8192) for i in range(2)]
    XS = P.sb("XS", [128, 8, 1024], BF16, off=bgT_off)
    xsT = P.sb("xsT", [128, 8, 1024], BF16, off=bgT_off + 16384)
    sgs = P.sb("sgs", [128, 512], F32, off=rt1_off + 4096)
    MSET("vector", XS[:], 0.0, ["XS"] + FENCE)
    ZD0 = []
    for t in range(8):
        ZD0.append(("Zd0", t))
        DMA("sync", "d_z0", Zd[t * 1024:(t + 1) * 1024, :].rearrange("(p c) d -> p c d", c=8), XS[:], ["XS"], [("Zd0", t)])
    wgv = w_gate.rearrange("e (k p) n -> e p k n", p=128)
    wuv = w_up.rearrange("e (k p) n -> e p k n", p=128)
    wdv = w_down.rearrange("e (k p) n -> e p k n", p=128)
    yrr = [0]
    for k4 in range(4):
        sg_, su_ = (k4 % 2) * 2, (k4 % 2) * 2 + 1
        wg_t, wu_t = wslot[sg_], wslot[su_]
        ex2 = (["cmpP"] if sg_ == 2 else [])
        ex3 = (["cmpb"] + GALL if su_ == 3 else [])
        P.dma("gpsimd", f"d_ws{sg_}", (lambda wg_t=wg_t, k4=k4: (lambda e: e.dma_start(out=wg_t[:], in_=wgv[k4])))(), [], [f"ws{sg_}"] + ex2 + (FENCE if k4 < 2 else []))
        P.dma("gpsimd", f"d_ws{su_}", (lambda wu_t=wu_t, k4=k4: (lambda e: e.dma_start(out=wu_t[:], in_=wuv[k4])))(), [], [f"ws{su_}"] + ex3 + (FENCE if k4 < 2 else []))
        P.dma("gpsimd", "d_wd", (lambda k4=k4: (lambda e: e.dma_start(out=wdt_[:], in_=wdv[k4])))(), [], ["wd_"] + (FENCE if k4 < 1 else []))
        for c in range(8):
            P.dma("gpsimd", f"d_XS{c}", (lambda k4=k4, c=c: (lambda e: e.indirect_dma_start(
                out=XS[:, c, :], out_offset=None, in_=h2all, in_offset=bass.IndirectOffsetOnAxis(ap=GIDX[:, k4, c:c + 1], axis=0))))(),
                H2ALLD + ["GIDX"], [("XS", c)] + (["XS"] if c == 0 else []))
        for c in range(8):
            pb, pbn = psb()
            pbv = pb[:].rearrange("p (k t) -> p k t", k=8)
            for kc in range(8):
                TR(pbv[:, kc, :], XS[:, c, kc * 128:(kc + 1) * 128], ident_b[:], [("XS", c), "XS", "ident_b"], [(pbn, kc)])
            ACT(xsT[:, :, c * 128:(c + 1) * 128], pbv, AF.Copy, [(pbn, kc) for kc in range(8)], [("xsT", c)] + (FENCE if k4 == 0 else []))
        for sch in range(2):
            hd = hid[(k4 * 2 + sch) % 2]
            hdn = f"hid{(k4 * 2 + sch) % 2}"
            xdeps = [("xsT", sch * 4 + t) for t in range(4)]
            for fo in range(8):
                pa, pan = psf("a")
                for kc in range(8):
                    MM(pa[:], wg_t[:, kc, fo * 128:(fo + 1) * 128], xsT[:, kc, sch * 512:(sch + 1) * 512], kc == 0, kc == 7,
                       [f"ws{sg_}"] + xdeps, [pan])
                pu, pun = psf("v")
                for kc in range(8):
                    MM(pu[:], wu_t[:, kc, fo * 128:(fo + 1) * 128], xsT[:, kc, sch * 512:(sch + 1) * 512], kc == 0, kc == 7,
                       [f"ws{su_}"] + xdeps, [pun])
                ACT(sgs[:], pa[:], AF.Silu, [pan], ["sgs"] + (FENCE if k4 == 0 and sch == 0 and fo == 0 else []))
                TT("vector", hd[:, fo, :], pu[:], sgs[:], ALU.mult, [pun, "sgs"], [(hdn, fo)] + (FENCE + ["TABm"] + TABC if k4 == 0 else []))
            for t in range(4):
                c = sch * 4 + t
                yi = yrr[0] % 2
                yrr[0] += 1
                for dn in range(2):
                    py, pyn = psf("v")
                    for kc in range(8):
                        MM(py[:], hd[:, kc, t * 128:(t + 1) * 128], wdt_[:, kc, dn * 512:(dn + 1) * 512], kc == 0, kc == 7,
                           [(hdn, kc), "wd_"], [pyn])
                    TS("vector", yz[yi][:, dn * 512:(dn + 1) * 512], py[:], gS[:, k4, c:c + 1], None, ALU.mult, None,
                       [pyn, "gS"], [f"yz{yi}"])
                P.dma("gpsimd", f"d_sz{yi}", (lambda yi=yi, k4=k4, c=c: (lambda e: e.indirect_dma_start(
                    out=Zd, out_offset=bass.IndirectOffsetOnAxis(ap=TIDX[:, k4, c:c + 1], axis=0), in_=yz[yi][:], in_offset=None,
                    compute_op=ALU.add, oob_is_err=True)))(), [f"yz{yi}", "TIDX", "Zd"] + ZD0, ["Zd"])

    for j16 in range(16):
        P.dma("gpsimd", "d_ag_z", (lambda j16=j16: (lambda e: e.collective_compute(
            "AllGather", ALU.bypass, replica_groups=GROUPS,
            ins=[Zd[j16 * 512:(j16 + 1) * 512, :].opt()], outs=[Zall[j16 * 2048:(j16 + 1) * 2048, :].opt()])))(),
            ["Zd", "agchain"], [("Zall", j16), "agchain"], inc=1)
    ZALLD = [("Zall", j16) for j16 in range(16)]
    if stage <= 7:
        P.emit()
        return nc, dbg_outs

    gfrow = P.sb("gfrow", [128, 1024], F32, off=rows_off + 4096)
    DMA("sync", "d_gfrow", gfrow[:], g_final.partition_broadcast(128), [], ["gfrow"] + FENCE)
    z4 = [P.sb(f"z4_{i}", [128, 4, 1024], BF16, off=bgT_off + i * 8192) for i in range(2)]
    evs = []
    for tile in range(16):
        i = xt_rr[0] % 2
        xt_rr[0] += 1
        xtile, xn = xt[i], f"xt{i}"
        zi = tile % 2
        DMA("sync", "d_" + xn, xtile[:], x1d[tile * 128:(tile + 1) * 128, :], [("x1d", tile)], [xn])
        for r in range(4):
            P.dma("gpsimd", f"d_z4_{zi}_{r}", (lambda zi=zi, r=r, tile=tile: (lambda e: e.indirect_dma_start(
                out=z4[zi][:, r, :], out_offset=None, in_=Zall, in_offset=bass.IndirectOffsetOnAxis(ap=ZIDX[:, r, tile:tile + 1], axis=0))))(),
                ZALLD + ["ZIDX"], [(f"z4_{zi}", r)] + ([("XS", c) for c in range(8)] + ["XS"] if tile < 2 else []))
        zd = [(f"z4_{zi}", r) for r in range(4)]
        TT("vector", tf[:], z4[zi][:, 0, :], z4[zi][:, 1, :], ALU.add, zd, ["tf"])
        TT("vector", tf[:], tf[:], z4[zi][:, 2, :], ALU.add, zd + ["tf"], ["tf"])
        TT("vector", tf[:], tf[:], z4[zi][:, 3, :], ALU.add, zd + ["tf"], ["tf"])
        TT("vector", tf[:], tf[:], rows["GT2"][:], ALU.mult, ["tf"] + rowdeps("GT2"), ["tf"])
        TT("gpsimd", xtile[:], xtile[:], tf[:], ALU.add, [xn, "tf"], [xn])
        ss = small[:, 16:17]
        rstd = small[:, 17:18]
        ACT(tf[:], xtile[:], AF.Square, [xn], ["tf"])
        RED("vector", ss, tf[:], ALU.add, ["tf"], ["ss"])
        TS("vector", rstd, ss, 1.0 / 1024.0, 1e-6, ALU.mult, ALU.add, ["ss"], ["rstd"])
        ACT(rstd, rstd, AF.Ln, ["rstd"], ["rstd"])
        ACT(rstd, rstd, AF.Exp, ["rstd"], ["rstd"], scale=-0.5)
        ACT(tf[:], xtile[:], AF.Copy, [xn, "rstd"], ["tf"], scale=rstd)
        TT("vector", xtile[:], tf[:], gfrow[:], ALU.mult, ["tf", "gfrow"], [xn])
        evs.append(DMA("sync", "d_out_" + xn, out[tile * 128:(tile + 1) * 128, :], xtile[:], [xn], [("out", tile)]))
    P.finish("sync", evs)
    P.emit()
    return nc, dbg_outs


def make_in_maps(inp):
    x = np.ascontiguousarray(inp["x"], dtype=np.float32)
    maps = []
    for c in range(NCORES):
        b, q = c // 4, c % 4
        t0 = q * 2048
        xw = np.zeros((W, 1024), np.float32)
        lo, hi = t0 - 128, t0 + 2048 + 128
        slo, shi = max(lo, 0), min(hi, 8192)
        xw[slo - lo:shi - lo] = x[b, slo:shi]
        ccv = np.stack([inp["c"][b].reshape(8, 128).T, inp["c_ctx"].reshape(8, 128).T], axis=-1)
        meta = np.zeros((128, 80), np.float32)
        meta[:, 0] = 1.0 if q > 0 else 0.0
        meta[:, 1] = 1.0 if q < 3 else 0.0
        meta[:, 2] = float(q * 32 - 2)
        meta[:, 3] = float(q * 2048)
        meta[:, 4] = float(4 * q * 128)
        meta[:, 5] = float(q * 8192)
        for k in range(4):
            meta[:, 8 + (4 * q + k) * 4 + k] = 1.0
        maps.append({
            "x": xw, "ctx": np.ascontiguousarray(inp["ctx"][b]), "cc": np.ascontiguousarray(ccv.reshape(128, 16)),
            "meta": meta, "w_ada": inp["w_ada"][0], "b_ada": inp["b_ada"][0], "g_mix": inp["g_mix"][0],
            "g_ffn": inp["g_ffn"][0], "g_final": inp["g_final"], "w_in": inp["w_in"][0], "conv_w": inp["conv_w"][0],
            "sink": inp["sink"][0], "w_out": inp["w_out"][0], "w_router": inp["w_router"][0],
            "w_gate": np.ascontiguousarray(inp["w_gate"][0, 4 * q:4 * q + 4]),
            "w_up": np.ascontiguousarray(inp["w_up"][0, 4 * q:4 * q + 4]),
            "w_down": np.ascontiguousarray(inp["w_down"][0, 4 * q:4 * q + 4]),
        })
    return maps


def kernel(**inputs):
    inp = {k: np.asarray(v) for k, v in inputs.items()}
    nc, _ = build_nc()
    res = run_bass_kernel_spmd(nc, make_in_maps(inp), core_ids=list(range(NCORES)))
    outp = np.zeros((2, 8192, 1024), np.float32)
    for c in range(NCORES):
        b, q = c // 4, c % 4
        outp[b, q * 2048:(q + 1) * 2048] = res.results[c]["out"]
    return outp
```

```python
import os
import numpy as np
import concourse.bass as bass
import concourse.mybir as mybir
from concourse.bass_utils import run_bass_kernel_spmd

F32 = mybir.dt.float32
BF16 = mybir.dt.bfloat16
I32 = mybir.dt.int32
ALU = mybir.AluOpType
AF = mybir.ActivationFunctionType
AX = mybir.AxisListType

COMPUTE = ("tensor", "vector", "scalar", "gpsimd")
QUEUES = ("sync",)
NCORES = 8
GROUPS = [[0, 1, 2, 3], [4, 5, 6, 7]]
W = 2304
NT = 16
NIT = 7


class Prog:
    def __init__(self, nc):
        self.nc = nc
        self.streams = {e: [] for e in COMPUTE + QUEUES}
        self.cnt = {e: 0 for e in COMPUTE}
        self.dma_cnt = {}
        self.waited = {}
        self.res = {}
        self.sem_handles = {}
        self.final_events = []
        self.sb_off = 16512
        self.sb_top = 229344

    def sb(self, name, shape, dtype, off=None):
        esz = {F32: 4, BF16: 2, I32: 4}[dtype]
        n = 1
        for s in shape[1:]:
            n *= s
        nbytes = (n * esz + 63) // 64 * 64
        if off is None:
            off = self.sb_off
            self.sb_off += nbytes
        assert off >= 16512 and off + nbytes <= self.sb_top, (name, off, nbytes)
        return self.nc.alloc_sbuf_tensor_at(name, list(shape), dtype, offset=off)

    def _deps(self, reads, writes):
        need = []
        for r in reads:
            st = self.res.get(r)
            if st and st["w"] is not None:
                need.append(st["w"])
        for w in writes:
            st = self.res.get(w)
            if st:
                if st["w"] is not None:
                    need.append(st["w"])
                need.extend(st["r"])
        return need

    def _commit(self, ev, reads, writes):
        for r in reads:
            st = self.res.setdefault(r, {"w": None, "r": []})
            st["r"].append(ev)
        for w in writes:
            self.res[w] = {"w": ev, "r": []}

    def _waits(self, eng, need):
        best = {}
        for (k, v) in need:
            if k == "tensor" and eng == "tensor":
                continue
            if v > best.get(k, 0):
                best[k] = v
        out = []
        for k, v in best.items():
            if self.waited.get((eng, k), 0) >= v:
                continue
            self.waited[(eng, k)] = v
            out.append((k, v))
        return out

    def op(self, eng, fn, reads=(), writes=()):
        need = self._deps(reads, writes)
        waits = self._waits(eng, need)
        self.cnt[eng] += 1
        ev = (eng, self.cnt[eng])
        self.streams[eng].append((waits, fn, (eng, 1)))
        self._commit(ev, reads, writes)
        return ev

    def dma(self, q, sem, fn, reads=(), writes=(), inc=16):
        need = self._deps(reads, writes)
        waits = self._waits(q, need)
        self.dma_cnt[sem] = self.dma_cnt.get(sem, 0) + inc
        ev = (sem, self.dma_cnt[sem])
        self.streams[q].append((waits, fn, (sem, inc)))
        self._commit(ev, reads, writes)
        return ev

    def finish(self, eng, events):
        self.final_events.append((eng, events))

    def check_deadlock(self):
        sem = {}
        pos = {e: 0 for e in self.streams}
        progressed = True
        while progressed:
            progressed = False
            for e, st in self.streams.items():
                while pos[e] < len(st):
                    waits, fn, inc = st[pos[e]]
                    if all(sem.get(k, 0) >= v for (k, v) in waits):
                        sem[inc[0]] = sem.get(inc[0], 0) + inc[1]
                        pos[e] += 1
                        progressed = True
                    else:
                        break
        stuck = {e: (pos[e], len(st), st[pos[e]][0]) for e, st in self.streams.items() if pos[e] < len(st)}
        assert not stuck, ("DEADLOCK", stuck, {k: sem.get(k) for e in stuck for (k, v) in stuck[e][2]})

    def emit(self):
        self.check_deadlock()
        nc = self.nc
        names = set(COMPUTE)
        for e in self.streams:
            for (waits, fn, inc) in self.streams[e]:
                names.add(inc[0])
                for (k, v) in waits:
                    names.add(k)
        for n in sorted(names):
            self.sem_handles[n] = nc.alloc_semaphore("s_" + n)
        H = self.sem_handles
        fin = {}
        for eng, evs in self.final_events:
            fin.setdefault(eng, []).extend(evs)
        with nc.Block() as block:
            def make(ename):
                def body(e):
                    for (waits, fn, inc) in self.streams[ename]:
                        for (k, v) in waits:
                            e.wait_ge(H[k], v)
                        fn(e).then_inc(H[inc[0]], inc[1])
                    best = {}
                    for (k, v) in fin.get(ename, []):
                        best[k] = max(best.get(k, 0), v)
                    for k, v in best.items():
                        e.wait_ge(H[k], v)
                return body
            for ename in self.streams:
                if not self.streams[ename] and ename not in fin:
                    continue
                getattr(block, ename)(make(ename))


def build_nc(stage=99, dbg=()):
    nc = bass.Bass("TRN2", target_bir_lowering=False)
    P = Prog(nc)
    dbg_outs = {}

    def din(name, shape, dt=F32):
        return nc.dram_tensor(name, list(shape), dt, kind="ExternalInput").ap()

    x = din("x", [W, 1024])
    ctx = din("ctx", [256, 1024])
    cc = din("cc", [128, 16])
    meta = din("meta", [128, 80])
    w_ada = din("w_ada", [1024, 6144])
    b_ada = din("b_ada", [6144])
    g_mix = din("g_mix", [1024])
    g_ffn = din("g_ffn", [1024])
    g_final = din("g_final", [1024])
    w_in = din("w_in", [1024, 2304])
    conv_w = din("conv_w", [3, 512])
    sink = din("sink", [8])
    w_out = din("w_out", [1024, 1024])
    w_router = din("w_router", [1024, 16])
    w_gate = din("w_gate", [4, 1024, 1024])
    w_up = din("w_up", [4, 1024, 1024])
    w_down = din("w_down", [4, 1024, 1024])
    out = nc.dram_tensor("out", [2048, 1024], F32, kind="ExternalOutput").ap()

    x1d = nc.dram_tensor("x1d", [2048, 1024], F32).ap()
    h2loc = nc.dram_tensor("h2loc", [2048, 1024], BF16).ap()
    h2all = nc.dram_tensor("h2all", [8192, 1024], BF16).ap()
    affloc = nc.dram_tensor("affloc", [16, 2048], F32).ap()
    affall = nc.dram_tensor("affall", [64, 2048], F32).ap()
    tabd = nc.dram_tensor("tabd", [2048, 128], F32).ap()
    Zd = nc.dram_tensor("Zd", [8192, 1024], BF16).ap()
    Zall = nc.dram_tensor("Zall", [32768, 1024], BF16).ap()

    def dbg_out(name, shape, dt=F32):
        t = nc.dram_tensor("dbg_" + name, list(shape), dt, kind="ExternalOutput").ap()
        dbg_outs[name] = t
        return t

    def ACT(out_, in_, func, r, w, **kw):
        return P.op("scalar", lambda e: e.activation(out=out_, in_=in_, func=func, **kw), r, w)

    def TT(eng, out_, in0, in1, op, r, w):
        return P.op(eng, lambda e: e.tensor_tensor(out=out_, in0=in0, in1=in1, op=op), r, w)

    def TS(eng, out_, in0, s1, s2, op0, op1, r, w):
        if op1 is None:
            return P.op(eng, lambda e: e.tensor_scalar(out=out_, in0=in0, scalar1=s1, scalar2=None, op0=op0), r, w)
        return P.op(eng, lambda e: e.tensor_scalar(out=out_, in0=in0, scalar1=s1, scalar2=s2, op0=op0, op1=op1), r, w)

    def STT(eng, out_, in0, scalar, in1, op0, op1, r, w):
        return P.op(eng, lambda e: e.scalar_tensor_tensor(out=out_, in0=in0, scalar=scalar, in1=in1, op0=op0, op1=op1), r, w)

    def RED(eng, out_, in_, op, r, w):
        return P.op(eng, lambda e: e.tensor_reduce(out=out_, in_=in_, axis=AX.X, op=op), r, w)

    def CP(eng, out_, in_, r, w):
        return P.op(eng, lambda e: e.tensor_copy(out=out_, in_=in_), r, w)

    def MSET(eng, out_, val, w):
        return P.op(eng, lambda e: e.memset(out_, val), (), w)

    def MM(out_, lhsT, rhs, start, stop, r, w):
        return P.op("tensor", lambda e: e.matmul(out_, lhsT, rhs, start=start, stop=stop), r, w)

    def TR(out_, in_, ident, r, w):
        return P.op("tensor", lambda e: e.transpose(out_, in_, ident), r, w)

    def DMA(q, sem, out_, in_, r, w):
        return P.dma(q, sem, lambda e: e.dma_start(out=out_, in_=in_), r, w)

    PSF = [nc.alloc_psum_tensor(f"psf{i}", [128, 512], F32) for i in range(6)]
    PSB = [nc.alloc_psum_tensor(f"psb{i}", [128, 1024], BF16) for i in range(2)]
    psf_rr = {"v": 0, "a": 0}

    def psf(cons):
        i = psf_rr[cons] % 3 + (0 if cons == "v" else 3)
        psf_rr[cons] += 1
        return PSF[i], f"psf{i}"

    psb_rr = [0]

    def psb():
        i = psb_rr[0] % 2
        psb_rr[0] += 1
        return PSB[i], f"psb{i}"

    ident_f = P.sb("ident_f", [128, 128], F32)
    ident_b = P.sb("ident_b", [128, 128], BF16)
    iot = P.sb("iot", [128, 128], F32)
    ones_b = P.sb("ones_b", [128, 128], BF16)
    U_b = P.sb("U_b", [128, 128], BF16)
    UI_b = P.sb("UI_b", [128, 128], BF16)
    mask3 = P.sb("mask3", [128, 3, 384], BF16)
    metat = P.sb("metat", [128, 80], F32)
    esink = P.sb("esink", [128, 8], F32)
    rows = {}
    for nm in ("S1", "G1", "GT1", "S2", "G2", "GT2", "cS1", "cG1"):
        rows[nm] = P.sb("row_" + nm, [128, 1024], F32)
    REG0 = P.sb_off

    P.op("gpsimd", lambda e: e.iota(iot[:], pattern=[[1, 128]], base=0, channel_multiplier=-1,
                                    allow_small_or_imprecise_dtypes=True), (), ["iot"])
    TS("vector", ident_f[:], iot[:], 0.0, None, ALU.is_equal, None, ["iot"], ["ident_f"])
    CP("vector", ident_b[:], ident_f[:], ["ident_f"], ["ident_b"])
    TS("vector", U_b[:], iot[:], 0.0, None, ALU.is_ge, None, ["iot"], ["U_b"])
    MSET("vector", ones_b[:], 1.0, ["ones_b"])
    DMA("sync", "d_meta", metat[:], meta, [], ["metat"])
    DMA("sync", "d_sink", esink[:], sink.partition_broadcast(128), [], ["esink"])
    ACT(esink[:], esink[:], AF.Exp, ["esink"], ["esink"])
    for v in range(3):
        TS("vector", mask3[:, v, 0:128], iot[:], 0.0, None, ALU.is_le, None, ["iot"], [("mask3", v)])
        MSET("vector", mask3[:, v, 128:256], 1.0, [("mask3", v, 1)])
        TS("vector", mask3[:, v, 256:384], iot[:], 0.0, None, ALU.is_ge, None, ["iot"], [("mask3", v, 2)])
    TS("vector", mask3[:, 1, 0:128], mask3[:, 1, 0:128], metat[:, 0:1], None, ALU.mult, None,
       ["metat", ("mask3", 1)], [("mask3", 1)])
    TS("vector", mask3[:, 2, 256:384], mask3[:, 2, 256:384], metat[:, 1:2], None, ALU.mult, None,
       ["metat", ("mask3", 2, 2)], [("mask3", 2, 2)])

    if "const" in dbg:
        d1 = dbg_out("ident", [128, 128])
        d2 = dbg_out("mask3", [128, 3 * 384], BF16)
        d3 = dbg_out("esink", [128, 8])
        e1 = DMA("sync", "d_dbg", d1, ident_f[:], ["ident_f"], ["dbg1"])
        e2 = DMA("sync", "d_dbg", d2, mask3[:].rearrange("p a b -> p (a b)"),
                 [("mask3", v) for v in range(3)] + [("mask3", v, 1) for v in range(3)] + [("mask3", v, 2) for v in range(3)], ["dbg2"])
        e3 = DMA("sync", "d_dbg", d3, esink[:], ["esink"], ["dbg3"])
        P.finish("sync", [e1, e2, e3])
    if stage <= 0:
        P.emit()
        return nc, dbg_outs

    o = REG0
    WIN = P.sb("WIN", [128, 8, 2944], BF16, off=o)
    mixT = P.sb("mixT", [128, 8, 2048], BF16, off=o)
    o += 47104
    COS = P.sb("COS", [128, W], F32, off=o); o += W * 4
    SINS = P.sb("SINS", [128, W], F32, off=o); o += W * 4
    xt = [P.sb(f"xt{i}", [128, 1024], F32, off=o + i * 4096) for i in range(2)]; o += 8192
    tf = P.sb("tf", [128, 1024], F32, off=o); o += 4096
    hb = [P.sb(f"hb{i}", [128, 1024], BF16, off=o + i * 2048) for i in range(2)]; o += 4096
    hT = [P.sb(f"hT{i}", [128, 8, 512], BF16, off=o + i * 8192) for i in range(2)]
    wo = P.sb("wo", [128, 8, 1024], BF16, off=o)
    o += 16384
    qT_off = o
    qT = P.sb("qT", [128, 4, W], BF16, off=o); o += 4 * W * 2
    kT_off = o
    kT = P.sb("kT", [128, W], BF16, off=o); o += W * 2
    Vt = P.sb("Vt", [128, 18, 2, 65], BF16, off=o); o += 4736
    kcT = P.sb("kcT", [128, 256], BF16, off=o); o += 512
    Vc = P.sb("Vc", [128, 2, 2, 65], BF16, off=o); o += 576
    bgT_off = o
    bgT = P.sb("bgT", [128, 4, 2048], BF16, off=o)
    stg = P.sb("stg", [128, 8, 640], F32, off=o)
    o += 20480
    uT_off = o
    uT = P.sb("uT", [128, 4, W], BF16, off=o); o += 4 * W * 2
    rt1_off = o
    rt1 = P.sb("rt1", [128, 512], F32, off=o); o += 2048
    rt2 = P.sb("rt2", [128, 512], F32, off=o); o += 2048
    cgs = P.sb("cgs", [128, 512], F32, off=o); o += 2048
    small = P.sb("small", [128, 64], F32, off=o); o += 256
    cw = P.sb("cw", [128, 4, 3], F32, off=o); o += 64
    assert o <= P.sb_top, o
    A_END = o

    wa = [P.sb("wa0", [128, 8, 1024], BF16, off=qT_off), P.sb("wa1", [128, 8, 1024], BF16, off=uT_off)]
    o = kT_off
    lb = P.sb("lb", [128, 8, 2, 128], BF16, off=o); o += 4096
    brow = P.sb("brow", [128, 1024], F32, off=o); o += 4096
    cct = P.sb("cct", [128, 8, 2], F32, off=o); o += 64
    scl = P.sb("scl", [128, 8, 2], F32, off=o); o += 64
    assert o <= bgT_off
    gmrow = P.sb("gmrow", [128, 1024], F32, off=rt1_off)

    DMA("sync", "d_cc", cct[:], cc.rearrange("p (k v) -> p k v", v=2), [], ["cct"])
    ACT(scl[:], cct[:], AF.Silu, ["cct"], ["scl"])
    for v in range(2):
        CP("vector", lb[:, :, v, :], scl[:, :, v:v + 1].to_broadcast([128, 8, 128]), ["scl"], [("lb", v)])
    if stage <= 0.3:
        d1 = dbg_out("lb", [128, 8 * 2 * 128], BF16)
        e1 = DMA("sync", "d_dbg", d1, lb[:].rearrange("p a b c -> p (a b c)"), [("lb", 0), ("lb", 1)], ["dbg1"])
        P.finish("sync", [e1])
        P.emit()
        return nc, dbg_outs
    w_ada_v = w_ada.rearrange("(k p) n -> p k n", p=128)
    grp = [(0, [("S1", 0), ("cS1", 1)]), (1, [("G1", 0), ("cG1", 1)]), (2, [("GT1", 0)]),
           (3, [("S2", 0)]), (4, [("G2", 0)]), (5, [("GT2", 0)])]
    for gi, (g, uses) in enumerate(grp):
        wb = wa[gi % 2]
        wn = f"wa{gi % 2}"
        P.dma("gpsimd", "d_" + wn, (lambda wb=wb, g=g: (lambda e: e.dma_start(out=wb[:], in_=w_ada_v[:, :, g * 1024:(g + 1) * 1024])))(),
              [], [wn])
        DMA("sync", "d_brow", brow[:], b_ada[g * 1024:(g + 1) * 1024].partition_broadcast(128), [], ["brow"])
        if stage <= 0.5:
            d1 = dbg_out("wa", [128, 8 * 1024], BF16)
            d2 = dbg_out("brow", [128, 1024])
            e1 = DMA("sync", "d_dbg", d1, wb[:].rearrange("p a b -> p (a b)"), [wn], ["dbg1"])
            e2 = DMA("sync", "d_dbg", d2, brow[:], ["brow"], ["dbg2"])
            P.finish("sync", [e1, e2])
            P.emit()
            return nc, dbg_outs
        for (nm, v) in uses:
            for n in range(2):
                ps, psn = psf("v")
                for k in range(8):
                    MM(ps[:], lb[:, k, v, :], wb[:, k, n * 512:(n + 1) * 512], k == 0, k == 7,
                       [("lb", v), wn], [psn])
                TT("vector", rows[nm][:, n * 512:(n + 1) * 512], ps[:], brow[:, n * 512:(n + 1) * 512], ALU.add,
                   [psn, "brow"], [("row", nm, n)])
                if stage <= 0.7:
                    d1 = dbg_out("r0", [128, 512])
                    e1 = DMA("sync", "d_dbg", d1, rows[nm][:, 0:512], [("row", nm, n)], ["dbg1"])
                    P.finish("sync", [e1])
                    P.emit()
                    return nc, dbg_outs
    for (gsrc, names) in (((g_mix, ("G1", "cG1")), (g_ffn, ("G2",))) if stage > 0.8 else ()):
        DMA("sync", "d_gmrow", gmrow[:], gsrc.partition_broadcast(128), [], ["gmrow"])
        for nm in names:
            TS("vector", rows[nm][:], rows[nm][:], 1.0, None, ALU.add, None,
               [("row", nm, 0), ("row", nm, 1)], [("row", nm, 0), ("row", nm, 1)])
            TT("vector", rows[nm][:], rows[nm][:], gmrow[:], ALU.mult,
               [("row", nm, 0), ("row", nm, 1), "gmrow"], [("row", nm, 0), ("row", nm, 1)])

    def rowdeps(nm):
        return [("row", nm, 0), ("row", nm, 1)]

    if "rows" in dbg:
        d = dbg_out("rows", [8, 128, 1024])
        for i, nm in enumerate(("S1", "G1", "GT1", "S2", "G2", "GT2", "cS1", "cG1")):
            ev = DMA("sync", "d_dbg", d[i], rows[nm][:], rowdeps(nm), ["dbg"])
        P.finish("sync", [ev])
    if stage <= 1:
        P.emit()
        return nc, dbg_outs


    def sc(i):
        return small[:, i:i + 1]
    pid, dd, i32_, isC, ff, inv, invC, invR, sgn, tmpc = [sc(i) for i in range(10)]
    P.op("gpsimd", lambda e: e.iota(small[:, 0:1], pattern=[[0, 1]], base=0, channel_multiplier=1,
                                    allow_small_or_imprecise_dtypes=True), (), ["small"])
    TS("vector", tmpc, pid, 64.0, -64.0, ALU.is_ge, ALU.mult, ["small"], ["small"])
    TT("vector", dd, pid, tmpc, ALU.add, ["small"], ["small"])
    TS("vector", sgn, dd, 32.0, None, ALU.is_ge, None, ["small"], ["small"])
    TS("vector", tmpc, sgn, -32.0, None, ALU.mult, None, ["small"], ["small"])
    TT("vector", i32_, dd, tmpc, ALU.add, ["small"], ["small"])
    TS("vector", isC, i32_, 16.0, None, ALU.is_ge, None, ["small"], ["small"])
    TS("vector", tmpc, isC, -16.0, None, ALU.mult, None, ["small"], ["small"])
    TT("vector", ff, i32_, tmpc, ALU.add, ["small"], ["small"])
    ACT(inv, ff, AF.Exp, ["small"], ["small"], scale=-float(np.log(10000.0) / 16.0))
    TT("vector", invC, inv, isC, ALU.mult, ["small"], ["small"])
    TT("vector", invR, inv, invC, ALU.subtract, ["small"], ["small"])
    TS("vector", sgn, sgn, 2.0, -1.0, ALU.mult, ALU.add, ["small"], ["small"])
    rrA = P.sb("rrA", [128, W], F32, off=qT_off)
    rrI = P.sb("rrI", [128, W], I32, off=qT_off + W * 4)
    ang = P.sb("ang", [128, W], F32, off=uT_off)
    P.op("gpsimd", lambda e: e.iota(COS[:], pattern=[[1, 36], [0, 64]], base=0, channel_multiplier=0,
                                    allow_small_or_imprecise_dtypes=True), (), ["COS"])
    P.op("gpsimd", lambda e: e.iota(SINS[:], pattern=[[0, 36], [1, 64]], base=0, channel_multiplier=0,
                                    allow_small_or_imprecise_dtypes=True), (), ["SINS"])
    HW_ = W // 2
    TWO_PI = float(2 * np.pi)
    for hh in range(2):
        sl = slice(hh * HW_, (hh + 1) * HW_)
        TS("vector", COS[:, sl], COS[:, sl], metat[:, 2:3], None, ALU.add, None, ["COS", "metat"], ["COS"])
        TS("vector", COS[:, sl], COS[:, sl], invR, None, ALU.mult, None, ["COS", "small"], ["COS"])
        TS("vector", SINS[:, sl], SINS[:, sl], invC, None, ALU.mult, None, ["SINS", "small"], ["SINS"])
    TT("vector", ang[:], COS[:], SINS[:], ALU.add, ["COS", "SINS"], ["ang"])

    def range_reduce_sin(dst, dstn, offset):
        TS("vector", rrA[:], ang[:], 1.0 / TWO_PI, offset / TWO_PI + 8.5, ALU.mult, ALU.add, ["ang"], ["rrA"])
        CP("vector", rrI[:], rrA[:], ["rrA"], ["rrI"])
        CP("vector", rrA[:], rrI[:], ["rrI"], ["rrA"])
        TS("vector", rrA[:], rrA[:], -TWO_PI, 8 * TWO_PI + offset, ALU.mult, ALU.add, ["rrA"], ["rrA"])
        TT("vector", dst[:], ang[:], rrA[:], ALU.add, ["ang", "rrA"], [dstn])
        TS("vector", rrA[:], dst[:], float(np.pi), -TWO_PI, ALU.is_gt, ALU.mult, [dstn], ["rrA"])
        TT("vector", dst[:], dst[:], rrA[:], ALU.add, [dstn, "rrA"], [dstn])
        TS("vector", rrA[:], dst[:], -float(np.pi), TWO_PI, ALU.is_lt, ALU.mult, [dstn], ["rrA"])
        TT("vector", dst[:], dst[:], rrA[:], ALU.add, [dstn, "rrA"], [dstn])
        ACT(dst[:], dst[:], AF.Sin, [dstn], [dstn])

    range_reduce_sin(SINS, "SINS", 0.0)
    range_reduce_sin(COS, "COS", float(np.pi / 2))
    for hh in range(2):
        sl = slice(hh * HW_, (hh + 1) * HW_)
        TS("vector", SINS[:, sl], SINS[:, sl], sgn, None, ALU.mult, None, ["SINS", "small"], ["SINS"])

    w_in_v = w_in.rearrange("(k p) n -> p k n", p=128)
    DMA("sync", "d_stg", stg[:], w_in_v[:, :, 0:640], [], ["stg"])
    qd = WIN[:, :, 0:512].rearrange("p k (c h d) -> p k c h d", c=4, h=2, d=64)
    qs = stg[:, :, 0:512].rearrange("p k (h c d) -> p k c h d", h=2, c=4, d=64)
    for h in range(2):
        ACT(qd[:, :, :, h, :], qs[:, :, :, h, :], AF.Copy, ["stg"], [("WIN", "q", h)])
    qd2 = WIN[:, :, 512:1024].rearrange("p k (c h s d) -> p k c h s d", c=4, h=2, s=2, d=32)
    qs2 = stg[:, :, 0:512].rearrange("p k (h c s d) -> p k c h s d", h=2, c=4, s=2, d=32)
    for h in range(2):
        for s in range(2):
            ACT(qd2[:, :, :, h, s, :], qs2[:, :, :, h, 1 - s, :], AF.Copy, ["stg"], [("WIN", "qsw", h, s)])
    ACT(WIN[:, :, 1024:1152], stg[:, :, 512:640], AF.Copy, ["stg"], [("WIN", "k")])
    kd2 = WIN[:, :, 1152:1280].rearrange("p k (h s d) -> p k h s d", h=2, s=2, d=32)
    ks2 = stg[:, :, 512:640].rearrange("p k (h s d) -> p k h s d", h=2, s=2, d=32)
    for s in range(2):
        ACT(kd2[:, :, :, s, :], ks2[:, :, :, 1 - s, :], AF.Copy, ["stg"], [("WIN", "ksw", s)])
    WINQ = [("WIN", "q", 0), ("WIN", "q", 1)]
    WINQS = [("WIN", "qsw", h, s) for h in range(2) for s in range(2)]
    WINK = [("WIN", "k")]
    WINKS = [("WIN", "ksw", 0), ("WIN", "ksw", 1)]
    for (nm, d0, s0, n) in (("v", 1280, 640, 128), ("bg", 1408, 768, 512), ("cg", 1920, 1280, 512), ("hv", 2432, 1792, 512)):
        P.dma("gpsimd", "d_win_" + nm, (lambda d0=d0, s0=s0, n=n: (lambda e: e.dma_start(out=WIN[:, :, d0:d0 + n], in_=w_in_v[:, :, s0:s0 + n])))(),
              [], [("WIN", nm)])
    for kk in range(3):
        for c4 in range(4):
            P.dma("sync", "d_cw", (lambda kk=kk, c4=c4: (lambda e: e.dma_start(
                out=cw[:, c4, kk:kk + 1], in_=conv_w[kk, c4 * 128:(c4 + 1) * 128].rearrange("(p o) -> p o", o=1))))(),
                [], [("cw", kk, c4)])
    MSET("vector", Vt[:, :, :, 64:65], 1.0, [("Vt", "ones")])
    MSET("vector", Vc[:, :, :, 64:65], 1.0, [("Vc", "ones")])

    xt_rr = [0]

    def norm_mod(src_rows, Gn, Sn, hbuf, hname, extra_r=()):
        i = xt_rr[0] % 2
        xt_rr[0] += 1
        xtile, xn = xt[i], f"xt{i}"
        DMA("sync", "d_" + xn, xtile[:], src_rows, list(extra_r), [xn])
        norm_mod_sb(xtile, xn, Gn, Sn, hbuf, hname)
        return xtile, xn

    tf2 = P.sb("tf2", [128, 1024], F32, off=bgT_off + 16384)
    nm_rr = [0]

    def norm_mod_sb(xtile, xn, Gn, Sn, hbuf, hname):
        pi = nm_rr[0] % 2
        nm_rr[0] += 1
        tfx, tfn = (tf, "tf") if pi == 0 else (tf2, "tf2")
        ss = small[:, 16 + 2 * pi:17 + 2 * pi]
        rstd = small[:, 17 + 2 * pi:18 + 2 * pi]
        ssn, rsn = f"ss{pi}", f"rstd{pi}"
        ACT(tfx[:], xtile[:], AF.Square, [xn], [tfn])
        RED("vector", ss, tfx[:], ALU.add, [tfn], [ssn])
        TS("vector", rstd, ss, 1.0 / 1024.0, 1e-6, ALU.mult, ALU.add, [ssn], [rsn])
        ACT(rstd, rstd, AF.Ln, [rsn], [rsn])
        ACT(rstd, rstd, AF.Exp, [rsn], [rsn], scale=-0.5)
        ACT(tfx[:], xtile[:], AF.Copy, [xn, rsn], [tfn], scale=rstd)
        TT("vector", tfx[:], tfx[:], rows[Gn][:], ALU.mult, [tfn] + rowdeps(Gn), [tfn])
        TT("vector", hbuf[:], tfx[:], rows[Sn][:], ALU.add, [tfn] + rowdeps(Sn), [hname])

    def transpose_to(hbuf, hname, dst, dst_name):
        pb, pbn = psb()
        pbv = pb[:].rearrange("p (k t) -> p k t", k=8)
        for k in range(8):
            TR(pbv[:, k, :], hbuf[:, k * 128:(k + 1) * 128], ident_b[:], [hname, "ident_b"], [(pbn, k)])
        ACT(dst, pbv, AF.Copy, [(pbn, k) for k in range(8)], [dst_name])

    hcT = hT[0]
    for t in range(2):
        norm_mod(ctx[t * 128:(t + 1) * 128, :], "cG1", "cS1", hb[t % 2], f"hb{t % 2}")
        transpose_to(hb[t % 2], f"hb{t % 2}", hcT[:, :, t * 128:(t + 1) * 128], ("hT0", t))
    ps, psn = psf("a")
    for k in range(8):
        MM(ps[:, 0:256], WIN[:, k, 1024:1152], hcT[:, k, 0:256], k == 0, k == 7,
           WINK + [("hT0", 0), ("hT0", 1)], [psn])
    ACT(kcT[:], ps[:, 0:256], AF.Copy, [psn], ["kcT"])
    for t in range(2):
        ps, psn = psf("a")
        for k in range(8):
            MM(ps[:, 0:128], hcT[:, k, t * 128:(t + 1) * 128], WIN[:, k, 1280:1408], k == 0, k == 7,
               [("WIN", "v"), ("hT0", t)], [psn])
        ACT(Vc[:, t, :, 0:64], ps[:, 0:128].rearrange("p (h d) -> p h d", h=2), AF.Copy, [psn], [("Vc", t)])

    if "ctx" in dbg:
        d1 = dbg_out("kcT", [128, 256], BF16)
        d2 = dbg_out("Vc", [128, 2 * 2 * 65], BF16)
        e1 = DMA("sync", "d_dbg", d1, kcT[:], ["kcT"], ["dbg1"])
        e2 = DMA("sync", "d_dbg", d2, Vc[:].rearrange("p a b c -> p (a b c)"), [("Vc", 0), ("Vc", 1), ("Vc", "ones")], ["dbg2"])
        P.finish("sync", [e1, e2])
    if stage <= 2:
        P.emit()
        return nc, dbg_outs

    chunks = [(0, 128, False)] + [(128 + 512 * i, 512, True) for i in range(4)] + [(2176, 128, False)]
    NCONV = int(os.environ.get("MK_NCONV", "4"))
    for ci, (w0, n, central) in enumerate(chunks):
        if (stage <= 2.5 and ci >= 1) or (stage <= 2.7 and ci >= 2):
            break
        hTc, hTn = hT[ci % 2], f"hT{ci % 2}"
        ntile = n // 128
        for t in range(ntile):
            j = (ci * 4 + t) % 2
            norm_mod(x[w0 + t * 128:w0 + (t + 1) * 128, :], "G1", "S1", hb[j], f"hb{j}")
            transpose_to(hb[j], f"hb{j}", hTc[:, :, t * 128:(t + 1) * 128], (hTn, t))
        hdeps = [(hTn, t) for t in range(ntile)]

        def proj(col0, wdeps, cons):
            ps, psn = psf(cons)
            for k in range(8):
                MM(ps[:, 0:n], WIN[:, k, col0:col0 + 128], hTc[:, k, 0:n], k == 0, k == 7, wdeps + hdeps, [psn])
            return ps, psn

        def rope_out(col0, colsw, wd, wsd, dst, dstn):
            pa, pan = proj(col0, wd, "v")
            pb_, pbn_ = proj(colsw, wsd, "v")
            TT("vector", rt1[:, 0:n], pa[:, 0:n], COS[:, w0:w0 + n], ALU.mult, [pan, "COS"], ["rt1"])
            TT("vector", rt2[:, 0:n], pb_[:, 0:n], SINS[:, w0:w0 + n], ALU.mult, [pbn_, "SINS"], ["rt2"])
            TT("vector", dst, rt1[:, 0:n], rt2[:, 0:n], ALU.add, ["rt1", "rt2"], [dstn])

        if central:
            for c in range(4):
                rope_out(c * 128, 512 + c * 128, WINQ, WINQS, qT[:, c, w0:w0 + n], ("qT", c, ci))
        PARTS = os.environ.get("MK_PARTS", "rvc")
        if "r" in PARTS:
            rope_out(1024, 1152, WINK, WINKS, kT[:, w0:w0 + n], ("kT", ci))
        for t in (range(ntile) if "v" in PARTS else ()):
            ps, psn = psf("a")
            for k in range(8):
                MM(ps[:, 0:128], hTc[:, k, t * 128:(t + 1) * 128], WIN[:, k, 1280:1408], k == 0, k == 7,
                   [("WIN", "v"), (hTn, t)], [psn])
            wt = w0 // 128 + t
            ACT(Vt[:, wt, :, 0:64], ps[:, 0:128].rearrange("p (h d) -> p h d", h=2), AF.Copy, [psn], [("Vt", wt)])
        for c in (range(NCONV) if "c" in PARTS else ()):
            if central:
                ps, psn = proj(1408 + c * 128, [("WIN", "bg")], "a")
                ACT(bgT[:, c, w0 - 128:w0 - 128 + n], ps[:, 0:n], AF.Copy, [psn], [("bgT", c, ci)])
            pc, pcn = proj(1920 + c * 128, [("WIN", "cg")], "a")
            ph, phn = proj(2432 + c * 128, [("WIN", "hv")], "v")
            ACT(cgs[:, 0:n], pc[:, 0:n], AF.Copy, [pcn], ["cgs"])
            TT("vector", uT[:, c, w0:w0 + n], ph[:, 0:n], cgs[:, 0:n], ALU.mult, [phn, "cgs"], [("uT", c, ci)])

    if "proj" in dbg:
        d1 = dbg_out("qT", [128, 4 * W], BF16)
        d2 = dbg_out("kT", [128, W], BF16)
        d3 = dbg_out("Vt", [128, 18 * 130], BF16)
        d4 = dbg_out("uT", [128, 4 * W], BF16)
        d5 = dbg_out("bgT", [128, 4 * 2048], BF16)
        allq = [("qT", c, ci) for c in range(4) for ci in range(1, 5)]
        allk = [("kT", ci) for ci in range(6)]
        allv = [("Vt", t) for t in range(18)] + [("Vt", "ones")]
        allu = [("uT", c, ci) for c in range(4) for ci in range(6)]
        allb = [("bgT", c, ci) for c in range(4) for ci in range(1, 5)]
        evs = [DMA("sync", "d_dbg", d1, qT[:].rearrange("p a b -> p (a b)"), allq, ["dbg1"]),
               DMA("sync", "d_dbg", d2, kT[:], allk, ["dbg2"]),
               DMA("sync", "d_dbg", d3, Vt[:].rearrange("p a b c -> p (a b c)"), allv, ["dbg3"]),
               DMA("sync", "d_dbg", d4, uT[:].rearrange("p a b -> p (a b)"), allu, ["dbg4"]),
               DMA("sync", "d_dbg", d5, bgT[:].rearrange("p a b -> p (a b)"), allb, ["dbg5"])]
        P.finish("sync", evs)
    if stage <= 3:
        P.emit()
        return nc, dbg_outs

    ALLWIN = WINQ + WINQS + WINK + WINKS + [("WIN", nm) for nm in ("v", "bg", "cg", "hv")]
    o2 = REG0 + 32768
    PL = [P.sb(f"PL{i}", [128, 384], BF16, off=o2 + i * 768) for i in range(2)]; o2 += 1536
    PC = [P.sb(f"PC{i}", [128, 256], BF16, off=o2 + i * 512) for i in range(2)]; o2 += 1024
    att_tm = P.sb("att_tm", [128, 512], BF16, off=o2); o2 += 1024
    rec = P.sb("rec", [128, 8], F32, off=o2); o2 += 64
    cvt = [P.sb(f"cvt{i}", [128, 512], F32, off=o2 + i * 2048) for i in range(2)]; o2 += 4096
    assert o2 <= REG0 + 47104

    def kchunk(wb):
        return 0 if wb == 0 else (5 if wb == 17 else 1 + (wb - 1) // 4)

    VONES = [("Vt", "ones")]
    for i in range(1, 17):
        ci_q = 1 + (i - 1) // 4
        mv = 1 if i == 1 else (2 if i == 16 else 0)
        pvs = [psf("v"), psf("v")]
        for hn in range(8):
            half, c = hn // 4, hn % 4
            r0 = half * 64
            j = hn % 2
            sl, sln = psf("a")
            sc_, scn = psf("a")
            qsl = qT[r0:r0 + 64, c, i * 128:(i + 1) * 128]
            for kb in range(3):
                wb = i - 1 + kb
                MM(sl[:, kb * 128:(kb + 1) * 128], kT[r0:r0 + 64, wb * 128:(wb + 1) * 128], qsl, True, True,
                   [("qT", c, ci_q), ("kT", kchunk(wb))], [sln])
            for cb in range(2):
                MM(sc_[:, cb * 128:(cb + 1) * 128], kcT[r0:r0 + 64, cb * 128:(cb + 1) * 128], qsl, True, True,
                   [("qT", c, ci_q), "kcT"], [scn])
            ACT(PL[j][:], sl[:, 0:384], AF.Exp, [sln], [f"PL{j}"] + ALLWIN, scale=0.125)
            ACT(PC[j][:], sc_[:, 0:256], AF.Exp, [scn], [f"PC{j}"] + ALLWIN, scale=0.125)
            TT("vector", PL[j][:], PL[j][:], mask3[:, mv, :], ALU.mult,
               [f"PL{j}", ("mask3", mv), ("mask3", mv, 1), ("mask3", mv, 2)], [f"PL{j}"])
            pv, pvn = pvs[half]
            pvr = pv[:, c * 65:(c + 1) * 65]
            for kb in range(3):
                wb = i - 1 + kb
                MM(pvr, PL[j][:, kb * 128:(kb + 1) * 128], Vt[:, wb, half, :], kb == 0, False,
                   [f"PL{j}", ("Vt", wb)] + VONES, [pvn])
            for cb in range(2):
                MM(pvr, PC[j][:, cb * 128:(cb + 1) * 128], Vc[:, cb, half, :], False, cb == 1,
                   [f"PC{j}", ("Vc", cb), ("Vc", "ones")], [pvn])
        for b in range(2):
            pv, pvn = pvs[b]
            pvv = pv[:, 0:260].rearrange("p (h e) -> p h e", h=4)
            TT("vector", rec[:, b * 4:(b + 1) * 4].unsqueeze(2), pvv[:, :, 64:65], esink[:, b * 4:(b + 1) * 4].unsqueeze(2),
               ALU.add, [pvn, "esink"], [("rec", b)] + ALLWIN)
            P.op("vector", (lambda b=b: (lambda e: e.reciprocal(rec[:, b * 4:(b + 1) * 4], rec[:, b * 4:(b + 1) * 4])))(),
                 [("rec", b)], [("rec", b)])
            TT("vector", att_tm[:, b * 256:(b + 1) * 256].rearrange("p (h d) -> p h d", h=4), pvv[:, :, 0:64],
               rec[:, b * 4:(b + 1) * 4].unsqueeze(2).to_broadcast([128, 4, 64]), ALU.mult,
               [pvn, ("rec", b)], [("att_tm", b)] + ALLWIN)
        pb, pbn = psb()
        pbv = pb[:, 0:512].rearrange("p (k t) -> p k t", k=4)
        for cc in range(4):
            TR(pbv[:, cc, :], att_tm[:, cc * 128:(cc + 1) * 128], ident_b[:], [("att_tm", cc // 2), "ident_b"], [(pbn, cc)])
        ACT(mixT[:, 0:4, (i - 1) * 128:i * 128], pbv, AF.Copy, [(pbn, cc) for cc in range(4)],
            [("mixT", "att", i - 1)] + ALLWIN)

    TS("gpsimd", uT[:, :, 127:128], uT[:, :, 127:128], metat[:, 0:1], None, ALU.mult, None,
       [("uT", c, 0) for c in range(4)] + ["metat"], [("uT", c, 0) for c in range(4)])
    TS("gpsimd", uT[:, :, 2176:2177], uT[:, :, 2176:2177], metat[:, 1:2], None, ALU.mult, None,
       [("uT", c, 5) for c in range(4)] + ["metat"], [("uT", c, 5) for c in range(4)])
    for tcn in range(4):
        w0 = 128 + tcn * 512
        for c in range(4):
            ud = [("uT", c, ci) for ci in (tcn, tcn + 1, tcn + 2)]
            cwd = [("cw", kk, c4) for kk in range(3) for c4 in range(4)]
            TS("gpsimd", cvt[0][:], uT[:, c, w0 - 1:w0 + 511], cw[:, c, 0:1], None, ALU.mult, None, ud + cwd, ["cvt0"] + ALLWIN)
            TS("gpsimd", cvt[1][:], uT[:, c, w0:w0 + 512], cw[:, c, 1:2], None, ALU.mult, None, ud + cwd, ["cvt1"] + ALLWIN)
            TT("gpsimd", cvt[0][:], cvt[0][:], cvt[1][:], ALU.add, ["cvt0", "cvt1"], ["cvt0"])
            TS("gpsimd", cvt[1][:], uT[:, c, w0 + 1:w0 + 513], cw[:, c, 2:3], None, ALU.mult, None, ud + cwd, ["cvt1"])
            TT("gpsimd", cvt[0][:], cvt[0][:], cvt[1][:], ALU.add, ["cvt0", "cvt1"], ["cvt0"])
            TT("gpsimd", mixT[:, 4 + c, tcn * 512:(tcn + 1) * 512], cvt[0][:], bgT[:, c, tcn * 512:(tcn + 1) * 512], ALU.mult,
               ["cvt0", ("bgT", c, tcn + 1)], [("mixT", "conv", c, tcn)] + ALLWIN)

    if stage <= 4:
        P.emit()
        return nc, dbg_outs

    HTALL = [(f"hT{a}", t) for a in range(2) for t in range(4)]
    P.dma("gpsimd", "d_wo", lambda e: e.dma_start(out=wo[:], in_=w_out.rearrange("(k p) n -> p k n", p=128)), [], ["wo"] + HTALL)
    QALL = [("qT", c, ci) for c in range(4) for ci in range(1, 5)]
    o3 = qT_off
    o3 += 4096
    h2Tall = P.sb("h2Tall", [128, 8, 2048], BF16, off=bgT_off)
    CONVDEAD = [("uT", c, ci) for c in range(4) for ci in range(6)] + [("bgT", c, ci) for c in range(4) for ci in range(1, 5)]
    rows_off = REG0 - 8 * 4096
    affTM = P.sb("affTM", [128, 16, 16], F32, off=rows_off)
    gm = P.sb("gm", [128, 16, 16], F32, off=rows_off + 1024)
    thr = P.sb("thr", [128, 16], F32, off=rows_off + 2048)
    affT = P.sb("affT", [16, 2048], F32, off=o3); o3 += 8192
    wr = P.sb("wr", [128, 8, 16], BF16, off=o3); o3 += 256
    sm = P.sb("sm", [128, 64], F32, off=o3); o3 += 256
    assert o3 <= qT_off + 4 * W * 2
    P.dma("gpsimd", "d_wr", lambda e: e.dma_start(out=wr[:], in_=w_router.rearrange("(k p) e -> p k e", p=128)), [], ["wr"] + QALL)
    for tile in range(16):
        tcn = tile // 4
        mdeps = [("mixT", "att", tile)] + [("mixT", "conv", c, tcn) for c in range(4)]
        i = xt_rr[0] % 2
        xt_rr[0] += 1
        xtile, xn = xt[i], f"xt{i}"
        DMA("sync", "d_" + xn, xtile[:], x[128 + tile * 128:256 + tile * 128, :], [], [xn])
        for n in range(2):
            ps, psn = psf("v")
            for k in range(8):
                MM(ps[:], mixT[:, k, tile * 128:(tile + 1) * 128], wo[:, k, n * 512:(n + 1) * 512], k == 0, k == 7,
                   mdeps + ["wo"], [psn])
            TT("vector", tf[:, n * 512:(n + 1) * 512], ps[:], rows["GT1"][:, n * 512:(n + 1) * 512], ALU.mult,
               [psn] + rowdeps("GT1"), ["tf"])
        TT("vector", xtile[:], xtile[:], tf[:], ALU.add, [xn, "tf"], [xn])
        DMA("sync", "d_x1d_" + xn, x1d[tile * 128:(tile + 1) * 128, :], xtile[:], [xn], [("x1d", tile)])
        j = tile % 2
        norm_mod_sb(xtile, xn, "G2", "S2", hb[j], f"hb{j}")
        DMA("sync", f"d_h2loc{j}", h2loc[tile * 128:(tile + 1) * 128, :], hb[j][:], [f"hb{j}"], [("h2loc", tile)])
        h2v = h2Tall[:, :, tile * 128:(tile + 1) * 128]
        pb, pbn = psb()
        pbv = pb[:].rearrange("p (k t) -> p k t", k=8)
        for k in range(8):
            TR(pbv[:, k, :], hb[j][:, k * 128:(k + 1) * 128], ident_b[:], [f"hb{j}", "ident_b"], [(pbn, k)])
        ACT(h2v, pbv, AF.Copy, [(pbn, k) for k in range(8)], [("h2T", tile)] + CONVDEAD)
        ps, psn = psf("v")
        for k in range(8):
            MM(ps[:, 0:16], h2Tall[:, k, tile * 128:(tile + 1) * 128], wr[:, k, :], k == 0, k == 7, [("h2T", tile), "wr"], [psn])
        mx, nmx, ssum, ex = sm[:, 0:1], sm[:, 1:2], sm[:, 2:3], sm[:, 16:32]
        af = affTM[:, tile, :]
        RED("vector", mx, ps[:, 0:16], ALU.max, [psn], ["sm_mx"])
        TS("vector", nmx, mx, -1.0, None, ALU.mult, None, ["sm_mx"], ["sm_nmx"])
        ACT(ex, ps[:, 0:16], AF.Exp, [psn, "sm_nmx"], ["sm_ex"], bias=nmx)
        RED("vector", ssum, ex, ALU.add, ["sm_ex"], ["sm_sum"])
        P.op("vector", lambda e: e.reciprocal(sm[:, 2:3], sm[:, 2:3]), ["sm_sum"], ["sm_sum"])
        TS("vector", af, ex, ssum, None, ALU.mult, None, ["sm_ex", "sm_sum"], [("affTM", tile)])
        pt, ptn = psf("a")
        TR(pt[0:16, 0:128], af, ident_f[:], [("affTM", tile), "ident_f"], [ptn])
        ACT(affT[:, tile * 128:(tile + 1) * 128], pt[0:16, 0:128], AF.Copy, [ptn], [("affT", tile)] + QALL)
    DMA("sync", "d_affloc", affloc, affT[:], [("affT", t) for t in range(16)], ["affloc"])

    if "a3" in dbg:
        d1 = dbg_out("x1", [2048, 1024])
        d2 = dbg_out("aff", [16, 2048])
        d3 = dbg_out("h2", [2048, 1024], BF16)
        e1 = DMA("sync", "d_dbg1", d1, x1d, [("x1d", t) for t in range(16)], ["dbg1"])
        e2 = DMA("sync", "d_dbg2", d2, affloc, ["affloc"], ["dbg2"])
        e3 = DMA("sync", "d_dbg3", d3, h2loc, [("h2loc", t) for t in range(16)], ["dbg3"])
        P.finish("sync", [e1, e2, e3])
    if stage <= 5:
        P.emit()
        return nc, dbg_outs

    FENCE = [k for k in P.res.keys() if not (isinstance(k, str) and (k.startswith("psf") or k in ("ident_b", "ident_f", "ones_b", "metat")))
             and not (isinstance(k, tuple) and k[0] in ("row", "h2T", "affTM", "x1d", "h2loc"))]
    NTB, NITB = 8, 8
    P.dma("gpsimd", "d_ag_aff", lambda e: e.collective_compute("AllGather", ALU.bypass, replica_groups=GROUPS,
                                                               ins=[affloc.opt()], outs=[affall.opt()]),
          ["affloc"], ["affall", "agchain"], inc=1)
    ob = REG0 + 159936 - 0
    ob = bgT_off + 32768
    AFt = P.sb("AFt", [128, 16, 64], F32, off=ob); ob += 4096
    FR = P.sb("FR", [128, 16, NTB], F32, off=ob); ob += 512
    Tt = P.sb("Tt", [128, 16, NTB], F32, off=ob); ob += 512
    tmpa = P.sb("tmpa", [128, 16, NTB], F32, off=ob); ob += 512
    get = P.sb("get", [128, 16, NTB], F32, off=ob); ob += 512
    cntb = P.sb("cntb", [128, 16 * NTB], BF16, off=ob); ob += 256
    lo = P.sb("lo", [128, 16], F32, off=ob); ob += 64
    hi = P.sb("hi", [128, 16], F32, off=ob); ob += 64
    wdt = P.sb("wdt", [128, 16], F32, off=ob); ob += 64
    red = P.sb("red", [128, 16], F32, off=ob); ob += 64
    idxt = P.sb("idxt", [128, 16], I32, off=ob); ob += 64
    assert ob <= rt1_off + 6144
    cmpb = P.sb("cmpb", [128, 16, NTB, 64], BF16, off=REG0 + 49152)
    for r in range(4):
        DMA("sync", "d_AFt", AFt[32 * r:32 * (r + 1), :, :],
            affall[r * 16:(r + 1) * 16, :].rearrange("e (p j) -> p e j", p=32, j=64), ["affall"], [("AFt", r)] + FENCE)
    AFD = [("AFt", r) for r in range(4)]
    P.op("gpsimd", lambda e: e.iota(FR[:], pattern=[[0, 16], [1, NTB]], base=1, channel_multiplier=0,
                                    allow_small_or_imprecise_dtypes=True), (), ["FR"] + FENCE)
    P.op("gpsimd", lambda e: e.iota(idxt[:], pattern=[[128, 16]], base=0, channel_multiplier=1), (), ["idxt"] + FENCE)
    TS("vector", FR[:], FR[:], 1.0 / (NTB + 1), None, ALU.mult, None, ["FR"], ["FR"])
    MSET("vector", lo[:], 0.0, ["lo"] + FENCE)
    MSET("vector", hi[:], 1.0, ["hi"])
    for it in range(NITB):
        TT("vector", wdt[:], hi[:], lo[:], ALU.subtract, ["hi", "lo"], ["wdt"])
        TT("vector", Tt[:], FR[:], wdt[:].unsqueeze(2).to_broadcast([128, 16, NTB]), ALU.mult, ["FR", "wdt"], ["Tt"])
        TT("vector", Tt[:], Tt[:], lo[:].unsqueeze(2).to_broadcast([128, 16, NTB]), ALU.add, ["Tt", "lo"], ["Tt"])
        TT("vector", cmpb[:], AFt[:].unsqueeze(2).to_broadcast([128, 16, NTB, 64]),
           Tt[:].unsqueeze(3).to_broadcast([128, 16, NTB, 64]), ALU.is_ge, AFD + ["Tt"], ["cmpb"] + FENCE)
        RED("vector", tmpa[:], cmpb[:], ALU.add, ["cmpb"], ["tmpa"])
        CP("vector", cntb[:], tmpa[:].rearrange("p e k -> p (e k)"), ["tmpa"], ["cntb"])
        ps, psn = psf("v")
        MM(ps[:, 0:16 * NTB], ones_b[:], cntb[:], True, True, ["cntb", "ones_b"], [psn])
        TS("vector", get[:].rearrange("p e k -> p (e k)"), ps[:, 0:16 * NTB], 1024.0, None, ALU.is_ge, None, [psn], ["get"])
        TT("vector", tmpa[:], Tt[:], get[:], ALU.mult, ["Tt", "get"], ["tmpa"])
        RED("vector", red[:], tmpa[:], ALU.max, ["tmpa"], ["red"])
        TT("vector", lo[:], lo[:], red[:], ALU.max, ["lo", "red"], ["lo"])
        TS("vector", tmpa[:], get[:], 2.0, None, ALU.mult, None, ["get"], ["tmpa"])
        TT("vector", tmpa[:], tmpa[:], Tt[:], ALU.add, ["tmpa", "Tt"], ["tmpa"])
        RED("vector", red[:], tmpa[:], ALU.min, ["tmpa"], ["red"])
        TT("vector", hi[:], hi[:], red[:], ALU.min, ["hi", "red"], ["hi"])
    CP("vector", thr[:], lo[:], ["lo"], ["thr"])
    AFFTM = [("affTM", t) for t in range(16)]
    TT("vector", gm[:], affTM[:], thr[:].unsqueeze(1).to_broadcast([128, 16, 16]), ALU.is_ge, AFFTM + ["thr"], ["gm"])
    TT("vector", gm[:], gm[:], affTM[:], ALU.mult, ["gm"] + AFFTM, ["gm"])

    if "thr" in dbg:
        d1 = dbg_out("thr", [128, 16])
        d2 = dbg_out("gm", [128, 256])
        e1 = DMA("sync", "d_dbg1", d1, thr[:], ["thr"], ["dbg1"])
        e2 = DMA("sync", "d_dbg2", d2, gm[:].rearrange("p a b -> p (a b)"), ["gm"], ["dbg2"])
        P.finish("sync", [e1, e2])
    if stage <= 6:
        P.emit()
        return nc, dbg_outs

    for j4 in range(4):
        P.dma("gpsimd", "d_ag_h2", (lambda j4=j4: (lambda e: e.collective_compute(
            "AllGather", ALU.bypass, replica_groups=GROUPS,
            ins=[h2loc[j4 * 512:(j4 + 1) * 512, :].opt()], outs=[h2all[j4 * 2048:(j4 + 1) * 2048, :].opt()])))(),
            [("h2loc", t) for t in range(j4 * 4, j4 * 4 + 4)] + ["agchain"], [("h2all", j4), "agchain"], inc=1)
    H2ALLD = [("h2all", j4) for j4 in range(4)]
    TAB = P.sb("TAB", [128, 16, 128], F32, off=qT_off)
    ob2 = kT_off
    ones64 = P.sb("ones64", [128, 64], F32, off=ob2); ob2 += 256
    n4 = P.sb("n4", [128, 4], F32, off=ob2); ob2 += 64
    t16 = P.sb("t16", [128, 16], F32, off=ob2); ob2 += 64
    rhsU = P.sb("rhsU", [128, 4, 128], BF16, off=ob2); ob2 += 1024
    rhsI = P.sb("rhsI", [128, 4, 128], BF16, off=ob2); ob2 += 1024
    offs_sb = P.sb("offs_sb", [128, 4, 128], F32, off=ob2); ob2 += 2048
    nrow_sb = P.sb("nrow_sb", [128, 4, 128], F32, off=ob2); ob2 += 2048
    sval = P.sb("sval", [128, 8], F32, off=ob2); ob2 += 64
    koffs = P.sb("koffs", [128, 4, 8], F32, off=ob2); ob2 += 128
    pS = P.sb("pS", [128, 4, 8], F32, off=ob2); ob2 += 128
    oex = P.sb("oex", [128, 4, 8], F32, off=ob2); ob2 += 128
    rS = P.sb("rS", [128, 4, 8], F32, off=ob2); ob2 += 128
    jS = P.sb("jS", [128, 4, 8], F32, off=ob2); ob2 += 128
    gS = P.sb("gS", [128, 4, 8], F32, off=ob2); ob2 += 128
    tSf = P.sb("tSf", [128, 4, 8], F32, off=ob2); ob2 += 128
    RIDX = P.sb("RIDX", [128, 4, 8], I32, off=ob2); ob2 += 128
    TIDX = P.sb("TIDX", [128, 4, 8], I32, off=ob2); ob2 += 128
    ZIDXf = P.sb("ZIDXf", [128, 4, 16], F32, off=ob2); ob2 += 256
    ZIDX = P.sb("ZIDX", [128, 4, 16], I32, off=ob2); ob2 += 256
    yz = [P.sb(f"yz{i}", [128, 1024], BF16, off=rows_off + 3 * 4096 + i * 2048) for i in range(2)]
    assert ob2 <= bgT_off, ob2
    cmpP = P.sb("cmpP", [128, 4, 8, 128], F32, off=REG0 + 32768)
    Gt = P.sb("Gt", [128, 4, 8, 128], F32, off=REG0 + 49152)

    MSET("vector", ones64[:], 1.0, ["ones64"] + FENCE)
    TT("vector", TAB[:, :, 64:128], AFt[:], thr[:].unsqueeze(2).to_broadcast([128, 16, 64]), ALU.is_ge, AFD + ["thr"], ["TABm"] + FENCE)
    for e16 in range(16):
        P.op("vector", (lambda e16=e16: (lambda e: e.tensor_tensor_scan(out=TAB[:, e16, 0:64], data0=ones64[:], data1=TAB[:, e16, 64:128],
                                                                          initial=0.0, op0=ALU.mult, op1=ALU.add)))(),
             ["TABm", "ones64"], [("TABc", e16)])
    TABC = [("TABc", e16) for e16 in range(16)]
    TT("vector", TAB[:, :, 64:128], TAB[:, :, 64:128], AFt[:], ALU.mult, ["TABm"] + TABC + AFD, ["TABm"])
    DMA("sync", "d_tabd", tabd.rearrange("(e p) c -> p e c", p=128), TAB[:], ["TABm"] + TABC, ["tabd"])
    selv = metat[:, 8:72].rearrange("p (e k) -> p e k", k=4)
    for k in range(4):
        TT("vector", t16[:], TAB[:, :, 63], selv[:, :, k], ALU.mult, TABC + ["metat"], ["t16"])
        RED("vector", n4[:, k:k + 1], t16[:], ALU.add, ["t16"], [("n4", k)])
    N4 = [("n4", k) for k in range(4)]
    TT("vector", rhsU[:], n4[:].unsqueeze(2).to_broadcast([128, 4, 128]), U_b[:].unsqueeze(1).to_broadcast([128, 4, 128]), ALU.mult,
       N4 + ["U_b"], ["rhsU"])
    TT("vector", rhsI[:], n4[:].unsqueeze(2).to_broadcast([128, 4, 128]), ident_b[:].unsqueeze(1).to_broadcast([128, 4, 128]), ALU.mult,
       N4 + ["ident_b"], ["rhsI"])
    ps, psn = psf("v")
    MM(ps[:], ones_b[:], rhsU[:].rearrange("p k q -> p (k q)"), True, True, ["rhsU", "ones_b"], [psn])
    CP("vector", offs_sb[:].rearrange("p k q -> p (k q)"), ps[:], [psn], ["offs_sb"])
    ps, psn = psf("v")
    MM(ps[:], ones_b[:], rhsI[:].rearrange("p k q -> p (k q)"), True, True, ["rhsI", "ones_b"], [psn])
    CP("vector", nrow_sb[:].rearrange("p k q -> p (k q)"), ps[:], [psn], ["nrow_sb"])
    P.op("gpsimd", lambda e: e.iota(sval[:], pattern=[[128, 8]], base=0, channel_multiplier=1,
                                    allow_small_or_imprecise_dtypes=True), (), ["sval"])
    P.op("gpsimd", lambda e: e.iota(koffs[:], pattern=[[128, 4], [0, 8]], base=0, channel_multiplier=0,
                                    allow_small_or_imprecise_dtypes=True), (), ["koffs"])
    P.op("gpsimd", lambda e: e.iota(ZIDXf[:], pattern=[[512, 4], [2048, 4], [128, 4]], base=0, channel_multiplier=1,
                                    allow_small_or_imprecise_dtypes=True), (), ["ZIDXf"])
    TS("vector", koffs[:], koffs[:], metat[:, 4:5], None, ALU.add, None, ["koffs", "metat"], ["koffs"])
    TS("vector", ZIDXf[:], ZIDXf[:], metat[:, 5:6], None, ALU.add, None, ["ZIDXf", "metat"], ["ZIDXf"])
    CP("vector", ZIDX[:], ZIDXf[:], ["ZIDXf"], ["ZIDX"])
    svb = sval[:].unsqueeze(1).to_broadcast([128, 4, 8])
    TT("vector", cmpP[:], offs_sb[:].unsqueeze(2).to_broadcast([128, 4, 8, 128]),
       svb.unsqueeze(3).to_broadcast([128, 4, 8, 128]), ALU.is_le, ["offs_sb", "sval"], ["cmpP"] + FENCE)
    RED("vector", pS[:], cmpP[:], ALU.add, ["cmpP"], ["pS"])
    TT("vector", cmpP[:], cmpP[:], nrow_sb[:].unsqueeze(2).to_broadcast([128, 4, 8, 128]), ALU.mult, ["cmpP", "nrow_sb"], ["cmpP"])
    RED("vector", oex[:], cmpP[:], ALU.add, ["cmpP"], ["oex"])
    TT("vector", rS[:], svb, oex[:], ALU.subtract, ["sval", "oex"], ["rS"])
    TT("vector", tSf[:], pS[:], koffs[:], ALU.add, ["pS", "koffs"], ["tSf"])
    CP("vector", RIDX[:], tSf[:], ["tSf"], ["RIDX"])
    for k in range(4):
        for c in range(8):
            P.dma("gpsimd", "d_G", (lambda k=k, c=c: (lambda e: e.indirect_dma_start(
                out=Gt[:, k, c, :], out_offset=None, in_=tabd, in_offset=bass.IndirectOffsetOnAxis(ap=RIDX[:, k, c:c + 1], axis=0))))(),
                ["tabd", "RIDX"], [("Gt", k, c), "cmpb"] if (k == 0 and c == 0) else [("Gt", k, c)])
    GALL = [("Gt", k, c) for k in range(4) for c in range(8)]
    cmpG = cmpP[:, :, :, 0:64]
    TT("vector", cmpG, Gt[:, :, :, 0:64], rS[:].unsqueeze(3).to_broadcast([128, 4, 8, 64]), ALU.is_le, GALL + ["rS"], ["cmpP"])
    RED("vector", jS[:], cmpG, ALU.add, ["cmpP"], ["jS"])
    TS("vector", oex[:], rS[:], 1.0, None, ALU.add, None, ["rS"], ["oex"])
    TT("vector", cmpG, Gt[:, :, :, 0:64], oex[:].unsqueeze(3).to_broadcast([128, 4, 8, 64]), ALU.is_equal, GALL + ["oex"], ["cmpP"])
    TT("vector", cmpG, cmpG, Gt[:, :, :, 64:128], ALU.mult, ["cmpP"] + GALL, ["cmpP"])
    RED("vector", gS[:], cmpG, ALU.add, ["cmpP"], ["gS"])
    TS("vector", tSf[:], pS[:], 64.0, None, ALU.mult, None, ["pS"], ["tSf"])
    TT("vector", tSf[:], tSf[:], jS[:], ALU.add, ["tSf", "jS"], ["tSf"])
    CP("vector", TIDX[:], tSf[:], ["tSf"], ["TIDX"])
    ra = P.sb("ra", [128, 4, 8], F32, off=ob2); rb = P.sb("rb", [128, 4, 8], F32, off=ob2 + 128)
    rj = P.sb("rj", [128, 4, 8], F32, off=ob2 + 256); GIDX = P.sb("GIDX", [128, 4, 8], I32, off=ob2 + 384)
    assert ob2 + 512 <= bgT_off
    TS("vector", ra[:], tSf[:], 2048.0, None, ALU.is_ge, None, ["tSf"], ["ra"])
    for thv in (4096.0, 6144.0):
        TS("vector", rb[:], tSf[:], thv, None, ALU.is_ge, None, ["tSf"], ["rb"])
        TT("vector", ra[:], ra[:], rb[:], ALU.add, ["ra", "rb"], ["ra"])
    TS("vector", rb[:], ra[:], -2048.0, None, ALU.mult, None, ["ra"], ["rb"])
    TT("vector", rb[:], rb[:], tSf[:], ALU.add, ["rb", "tSf"], ["rb"])
    TS("vector", rj[:], rb[:], 512.0, None, ALU.is_ge, None, ["rb"], ["rj"])
    for thv in (1024.0, 1536.0):
        TS("vector", oex[:], rb[:], thv, None, ALU.is_ge, None, ["rb"], ["oex"])
        TT("vector", rj[:], rj[:], oex[:], ALU.add, ["rj", "oex"], ["rj"])
    TT("vector", rj[:], rj[:], ra[:], ALU.subtract, ["rj", "ra"], ["rj"])
    TS("vector", rj[:], rj[:], 1536.0, None, ALU.mult, None, ["rj"], ["rj"])
    TT("vector", rj[:], rj[:], tSf[:], ALU.add, ["rj", "tSf"], ["rj"])
    CP("vector", GIDX[:], rj[:], ["rj"], ["GIDX"])

    if "idx" in dbg:
        d1 = dbg_out("tidx", [128, 32])
        d2 = dbg_out("gS", [128, 32])
        e1 = DMA("sync", "d_dbg1", d1, tSf[:].rearrange("p a b -> p (a b)"), ["tSf", "TIDX"], ["dbg1"])
        e2 = DMA("sync", "d_dbg2", d2, gS[:].rearrange("p a b -> p (a b)"), ["gS"], ["dbg2"])
        P.finish("sync", [e1, e2])
    if stage <= 6.5:
        P.emit()
        return nc, dbg_outs

    wslot = [P.sb(f"wslot{i}", [128, 8, 1024], BF16, off=REG0 + i * 16384) for i in range(4)]
    wdt_ = P.sb("wd_", [128, 8, 1024], BF16, off=REG0 + 81920)
    hid = [P.sb(f"hid{i}", [128, 8, 512], BF16, off=qT_off + i * 8192) for i in range(2)]
    XS = P.sb("XS", [128, 8, 1024], BF16, off=bgT_off)
    xsT = P.sb("xsT", [128, 8, 1024], BF16, off=bgT_off + 16384)
    sgs = P.sb("sgs", [128, 512], F32, off=rt1_off + 4096)
    MSET("vector", XS[:], 0.0, ["XS"] + FENCE)
    ZD0 = []
    for t in range(8):
        ZD0.append(("Zd0", t))
        DMA("sync", "d_z0", Zd[t * 1024:(t + 1) * 1024, :].rearrange("(p c) d -> p c d", c=8), XS[:], ["XS"], [("Zd0", t)])
    wgv = w_gate.rearrange("e (k p) n -> e p k n", p=128)
    wuv = w_up.rearrange("e (k p) n -> e p k n", p=128)
    wdv = w_down.rearrange("e (k p) n -> e p k n", p=128)
    yrr = [0]
    for k4 in range(4):
        sg_, su_ = (k4 % 2) * 2, (k4 % 2) * 2 + 1
        wg_t, wu_t = wslot[sg_], wslot[su_]
        ex2 = (["cmpP"] if sg_ == 2 else [])
        ex3 = (["cmpb"] + GALL if su_ == 3 else [])
        P.dma("gpsimd", f"d_ws{sg_}", (lambda wg_t=wg_t, k4=k4: (lambda e: e.dma_start(out=wg_t[:], in_=wgv[k4])))(), [], [f"ws{sg_}"] + ex2 + (FENCE if k4 < 2 else []))
        P.dma("gpsimd", f"d_ws{su_}", (lambda wu_t=wu_t, k4=k4: (lambda e: e.dma_start(out=wu_t[:], in_=wuv[k4])))(), [], [f"ws{su_}"] + ex3 + (FENCE if k4 < 2 else []))
        P.dma("gpsimd", "d_wd", (lambda k4=k4: (lambda e: e.dma_start(out=wdt_[:], in_=wdv[k4])))(), [], ["wd_"] + (FENCE if k4 < 1 else []))
        for c in range(8):
            P.dma("gpsimd", f"d_XS{c}", (lambda k4=k4, c=c: (lambda e: e.indirect_dma_start(
                out=XS[:, c, :], out_offset=None, in_=h2all, in_offset=bass.IndirectOffsetOnAxis(ap=GIDX[:, k4, c:c + 1], axis=0))))(),
                H2ALLD + ["GIDX"], [("XS", c)] + (["XS"] if c == 0 else []))
        for c in range(8):
            pb, pbn = psb()
            pbv = pb[:].rearrange("p (k t) -> p k t", k=8)
            for kc in range(8):
                TR(pbv[:, kc, :], XS[:, c, kc * 128:(kc + 1) * 128], ident_b[:], [("XS", c), "XS", "ident_b"], [(pbn, kc)])
            ACT(xsT[:, :, c * 128:(c + 1) * 128], pbv, AF.Copy, [(pbn, kc) for kc in range(8)], [("xsT", c)] + (FENCE if k4 == 0 else []))
        for sch in range(2):
            hd = hid[(k4 * 2 + sch) % 2]
            hdn = f"hid{(k4 * 2 + sch) % 2}"
            xdeps = [("xsT", sch * 4 + t) for t in range(4)]
            for fo in range(8):
                pa, pan = psf("a")
                for kc in range(8):
                    MM(pa[:], wg_t[:, kc, fo * 128:(fo + 1) * 128], xsT[:, kc, sch * 512:(sch + 1) * 512], kc == 0, kc == 7,
                       [f"ws{sg_}"] + xdeps, [pan])
                pu, pun = psf("v")
                for kc in range(8):
                    MM(pu[:], wu_t[:, kc, fo * 128:(fo + 1) * 128], xsT[:, kc, sch * 512:(sch + 1) * 512], kc == 0, kc == 7,
                       [f"ws{su_}"] + xdeps, [pun])
                ACT(sgs[:], pa[:], AF.Silu, [pan], ["sgs"] + (FENCE if k4 == 0 and sch == 0 and fo == 0 else []))
                TT("vector", hd[:, fo, :], pu[:], sgs[:], ALU.mult, [pun, "sgs"], [(hdn, fo)] + (FENCE + ["TABm"] + TABC if k4 == 0 else []))
            for t in range(4):
                c = sch * 4 + t
                yi = yrr[0] % 2
                yrr[0] += 1
                for dn in range(2):
                    py, pyn = psf("v")
                    for kc in range(8):
                        MM(py[:], hd[:, kc, t * 128:(t + 1) * 128], wdt_[:, kc, dn * 512:(dn + 1) * 512], kc == 0, kc == 7,
                           [(hdn, kc), "wd_"], [pyn])
                    TS("vector", yz[yi][:, dn * 512:(dn + 1) * 512], py[:], gS[:, k4, c:c + 1], None, ALU.mult, None,
                       [pyn, "gS"], [f"yz{yi}"])
                P.dma("gpsimd", f"d_sz{yi}", (lambda yi=yi, k4=k4, c=c: (lambda e: e.indirect_dma_start(
                    out=Zd, out_offset=bass.IndirectOffsetOnAxis(ap=TIDX[:, k4, c:c + 1], axis=0), in_=yz[yi][:], in_offset=None,
                    compute_op=ALU.add, oob_is_err=True)))(), [f"yz{yi}", "TIDX", "Zd"] + ZD0, ["Zd"])

    for j16 in range(16):
        P.dma("gpsimd", "d_ag_z", (lambda j16=j16: (lambda e: e.collective_compute(
            "AllGather", ALU.bypass, replica_groups=GROUPS,
            ins=[Zd[j16 * 512:(j16 + 1) * 512, :].opt()], outs=[Zall[j16 * 2048:(j16 + 1) * 2048, :].opt()])))(),
            ["Zd", "agchain"], [("Zall", j16), "agchain"], inc=1)
    ZALLD = [("Zall", j16) for j16 in range(16)]
    if stage <= 7:
        P.emit()
        return nc, dbg_outs

    gfrow = P.sb("gfrow", [128, 1024], F32, off=rows_off + 4096)
    DMA("sync", "d_gfrow", gfrow[:], g_final.partition_broadcast(128), [], ["gfrow"] + FENCE)
    z4 = [P.sb(f"z4_{i}", [128, 4, 1024], BF16, off=bgT_off + i * 8192) for i in range(2)]
    evs = []
    for tile in range(16):
        i = xt_rr[0] % 2
        xt_rr[0] += 1
        xtile, xn = xt[i], f"xt{i}"
        zi = tile % 2
        DMA("sync", "d_" + xn, xtile[:], x1d[tile * 128:(tile + 1) * 128, :], [("x1d", tile)], [xn])
        for r in range(4):
            P.dma("gpsimd", f"d_z4_{zi}_{r}", (lambda zi=zi, r=r, tile=tile: (lambda e: e.indirect_dma_start(
                out=z4[zi][:, r, :], out_offset=None, in_=Zall, in_offset=bass.IndirectOffsetOnAxis(ap=ZIDX[:, r, tile:tile + 1], axis=0))))(),
                ZALLD + ["ZIDX"], [(f"z4_{zi}", r)] + ([("XS", c) for c in range(8)] + ["XS"] if tile < 2 else []))
        zd = [(f"z4_{zi}", r) for r in range(4)]
        TT("vector", tf[:], z4[zi][:, 0, :], z4[zi][:, 1, :], ALU.add, zd, ["tf"])
        TT("vector", tf[:], tf[:], z4[zi][:, 2, :], ALU.add, zd + ["tf"], ["tf"])
        TT("vector", tf[:], tf[:], z4[zi][:, 3, :], ALU.add, zd + ["tf"], ["tf"])
        TT("vector", tf[:], tf[:], rows["GT2"][:], ALU.mult, ["tf"] + rowdeps("GT2"), ["tf"])
        TT("vector", xtile[:], xtile[:], tf[:], ALU.add, [xn, "tf"], [xn])
        ss = small[:, 16:17]
        rstd = small[:, 17:18]
        ACT(tf[:], xtile[:], AF.Square, [xn], ["tf"])
        RED("vector", ss, tf[:], ALU.add, ["tf"], ["ss"])
        TS("vector", rstd, ss, 1.0 / 1024.0, 1e-6, ALU.mult, ALU.add, ["ss"], ["rstd"])
        ACT(rstd, rstd, AF.Ln, ["rstd"], ["rstd"])
        ACT(rstd, rstd, AF.Exp, ["rstd"], ["rstd"], scale=-0.5)
        ACT(tf[:], xtile[:], AF.Copy, [xn, "rstd"], ["tf"], scale=rstd)
        TT("vector", xtile[:], tf[:], gfrow[:], ALU.mult, ["tf", "gfrow"], [xn])
        evs.append(DMA("sync", "d_out_" + xn, out[tile * 128:(tile + 1) * 128, :], xtile[:], [xn], [("out", tile)]))
    P.finish("sync", evs)
    P.emit()
    return nc, dbg_outs


def make_in_maps(inp):
    x = np.ascontiguousarray(inp["x"], dtype=np.float32)
    maps = []
    for c in range(NCORES):
        b, q = c // 4, c % 4
        t0 = q * 2048
        xw = np.zeros((W, 1024), np.float32)
        lo, hi = t0 - 128, t0 + 2048 + 128
        slo, shi = max(lo, 0), min(hi, 8192)
        xw[slo - lo:shi - lo] = x[b, slo:shi]
        ccv = np.stack([inp["c"][b].reshape(8, 128).T, inp["c_ctx"].reshape(8, 128).T], axis=-1)
        meta = np.zeros((128, 80), np.float32)
        meta[:, 0] = 1.0 if q > 0 else 0.0
        meta[:, 1] = 1.0 if q < 3 else 0.0
        meta[:, 2] = float(q * 32 - 2)
        meta[:, 3] = float(q * 2048)
        meta[:, 4] = float(4 * q * 128)
        meta[:, 5] = float(q * 8192)
        for k in range(4):
            meta[:, 8 + (4 * q + k) * 4 + k] = 1.0
        maps.append({
            "x": xw, "ctx": np.ascontiguousarray(inp["ctx"][b]), "cc": np.ascontiguousarray(ccv.reshape(128, 16)),
            "meta": meta, "w_ada": inp["w_ada"][0], "b_ada": inp["b_ada"][0], "g_mix": inp["g_mix"][0],
            "g_ffn": inp["g_ffn"][0], "g_final": inp["g_final"], "w_in": inp["w_in"][0], "conv_w": inp["conv_w"][0],
            "sink": inp["sink"][0], "w_out": inp["w_out"][0], "w_router": inp["w_router"][0],
            "w_gate": np.ascontiguousarray(inp["w_gate"][0, 4 * q:4 * q + 4]),
            "w_up": np.ascontiguousarray(inp["w_up"][0, 4 * q:4 * q + 4]),
            "w_down": np.ascontiguousarray(inp["w_down"][0, 4 * q:4 * q + 4]),
        })
    return maps


def kernel(**inputs):
    inp = {k: np.asarray(v) for k, v in inputs.items()}
    nc, _ = build_nc()
    res = run_bass_kernel_spmd(nc, make_in_maps(inp), core_ids=list(range(NCORES)))
    outp = np.zeros((2, 8192, 1024), np.float32)
    for c in range(NCORES):
        b, q = c // 4, c % 4
        outp[b, q * 2048:(q + 1) * 2048] = res.results[c]["out"]
    return outp
```

```python
import os
import numpy as np
import concourse.bass as bass
import concourse.mybir as mybir
from concourse.bass_utils import run_bass_kernel_spmd

F32 = mybir.dt.float32
BF16 = mybir.dt.bfloat16
I32 = mybir.dt.int32
ALU = mybir.AluOpType
AF = mybir.ActivationFunctionType
AX = mybir.AxisListType

COMPUTE = ("tensor", "vector", "scalar", "gpsimd")
QUEUES = ("sync",)
NCORES = 8
GROUPS = [[0, 1, 2, 3], [4, 5, 6, 7]]
W = 2304
NT = 16
NIT = 7


class Prog:
    def __init__(self, nc):
        self.nc = nc
        self.streams = {e: [] for e in COMPUTE + QUEUES}
        self.cnt = {e: 0 for e in COMPUTE}
        self.dma_cnt = {}
        self.waited = {}
        self.res = {}
        self.sem_handles = {}
        self.final_events = []
        self.sb_off = 16512
        self.sb_top = 229344

    def sb(self, name, shape, dtype, off=None):
        esz = {F32: 4, BF16: 2, I32: 4}[dtype]
        n = 1
        for s in shape[1:]:
            n *= s
        nbytes = (n * esz + 63) // 64 * 64
        if off is None:
            off = self.sb_off
            self.sb_off += nbytes
        assert off >= 16512 and off + nbytes <= self.sb_top, (name, off, nbytes)
        return self.nc.alloc_sbuf_tensor_at(name, list(shape), dtype, offset=off)

    def _deps(self, reads, writes):
        need = []
        for r in reads:
            st = self.res.get(r)
            if st and st["w"] is not None:
                need.append(st["w"])
        for w in writes:
            st = self.res.get(w)
            if st:
                if st["w"] is not None:
                    need.append(st["w"])
                need.extend(st["r"])
        return need

    def _commit(self, ev, reads, writes):
        for r in reads:
            st = self.res.setdefault(r, {"w": None, "r": []})
            st["r"].append(ev)
        for w in writes:
            self.res[w] = {"w": ev, "r": []}

    def _waits(self, eng, need):
        best = {}
        for (k, v) in need:
            if k == "tensor" and eng == "tensor":
                continue
            if v > best.get(k, 0):
                best[k] = v
        out = []
        for k, v in best.items():
            if self.waited.get((eng, k), 0) >= v:
                continue
            self.waited[(eng, k)] = v
            out.append((k, v))
        return out

    def op(self, eng, fn, reads=(), writes=()):
        need = self._deps(reads, writes)
        waits = self._waits(eng, need)
        self.cnt[eng] += 1
        ev = (eng, self.cnt[eng])
        self.streams[eng].append((waits, fn, (eng, 1)))
        self._commit(ev, reads, writes)
        return ev

    def dma(self, q, sem, fn, reads=(), writes=(), inc=16):
        need = self._deps(reads, writes)
        waits = self._waits(q, need)
        self.dma_cnt[sem] = self.dma_cnt.get(sem, 0) + inc
        ev = (sem, self.dma_cnt[sem])
        self.streams[q].append((waits, fn, (sem, inc)))
        self._commit(ev, reads, writes)
        return ev

    def finish(self, eng, events):
        self.final_events.append((eng, events))

    def check_deadlock(self):
        sem = {}
        pos = {e: 0 for e in self.streams}
        progressed = True
        while progressed:
            progressed = False
            for e, st in self.streams.items():
                while pos[e] < len(st):
                    waits, fn, inc = st[pos[e]]
                    if all(sem.get(k, 0) >= v for (k, v) in waits):
                        sem[inc[0]] = sem.get(inc[0], 0) + inc[1]
                        pos[e] += 1
                        progressed = True
                    else:
                        break
        stuck = {e: (pos[e], len(st), st[pos[e]][0]) for e, st in self.streams.items() if pos[e] < len(st)}
        assert not stuck, ("DEADLOCK", stuck, {k: sem.get(k) for e in stuck for (k, v) in stuck[e][2]})

    def emit(self):
        self.check_deadlock()
        nc = self.nc
        names = set(COMPUTE)
        for e in self.streams:
            for (waits, fn, inc) in self.streams[e]:
                names.add(inc[0])
                for (k, v) in waits:
                    names.add(k)
        for n in sorted(names):
            self.sem_handles[n] = nc.alloc_semaphore("s_" + n)
        H = self.sem_handles
        fin = {}
        for eng, evs in self.final_events:
            fin.setdefault(eng, []).extend(evs)
        with nc.Block() as block:
            def make(ename):
                def body(e):
                    for (waits, fn, inc) in self.streams[ename]:
                        for (k, v) in waits:
                            e.wait_ge(H[k], v)
                        fn(e).then_inc(H[inc[0]], inc[1])
                    best = {}
                    for (k, v) in fin.get(ename, []):
                        best[k] = max(best.get(k, 0), v)
                    for k, v in best.items():
                        e.wait_ge(H[k], v)
                return body
            for ename in self.streams:
                if not self.streams[ename] and ename not in fin:
                    continue
                getattr(block, ename)(make(ename))


def build_nc(stage=99, dbg=()):
    nc = bass.Bass("TRN2", target_bir_lowering=False)
    P = Prog(nc)
    dbg_outs = {}

    def din(name, shape, dt=F32):
        return nc.dram_tensor(name, list(shape), dt, kind="ExternalInput").ap()

    x = din("x", [W, 1024])
    ctx = din("ctx", [256, 1024])
    cc = din("cc", [128, 16])
    meta = din("meta", [128, 80])
    w_ada = din("w_ada", [1024, 6144])
    b_ada = din("b_ada", [6144])
    g_mix = din("g_mix", [1024])
    g_ffn = din("g_ffn", [1024])
    g_final = din("g_final", [1024])
    w_in = din("w_in", [1024, 2304])
    conv_w = din("conv_w", [3, 512])
    sink = din("sink", [8])
    w_out = din("w_out", [1024, 1024])
    w_router = din("w_router", [1024, 16])
    w_gate = din("w_gate", [4, 1024, 1024])
    w_up = din("w_up", [4, 1024, 1024])
    w_down = din("w_down", [4, 1024, 1024])
    out = nc.dram_tensor("out", [2048, 1024], F32, kind="ExternalOutput").ap()

    x1d = nc.dram_tensor("x1d", [2048, 1024], F32).ap()
    h2loc = nc.dram_tensor("h2loc", [2048, 1024], BF16).ap()
    h2all = nc.dram_tensor("h2all", [8192, 1024], BF16).ap()
    affloc = nc.dram_tensor("affloc", [16, 2048], F32).ap()
    affall = nc.dram_tensor("affall", [64, 2048], F32).ap()
    tabd = nc.dram_tensor("tabd", [2048, 128], F32).ap()
    Zd = nc.dram_tensor("Zd", [8192, 1024], BF16).ap()
    Zall = nc.dram_tensor("Zall", [32768, 1024], BF16).ap()

    def dbg_out(name, shape, dt=F32):
        t = nc.dram_tensor("dbg_" + name, list(shape), dt, kind="ExternalOutput").ap()
        dbg_outs[name] = t
        return t

    def ACT(out_, in_, func, r, w, **kw):
        return P.op("scalar", lambda e: e.activation(out=out_, in_=in_, func=func, **kw), r, w)

    def TT(eng, out_, in0, in1, op, r, w):
        return P.op(eng, lambda e: e.tensor_tensor(out=out_, in0=in0, in1=in1, op=op), r, w)

    def TS(eng, out_, in0, s1, s2, op0, op1, r, w):
        if op1 is None:
            return P.op(eng, lambda e: e.tensor_scalar(out=out_, in0=in0, scalar1=s1, scalar2=None, op0=op0), r, w)
        return P.op(eng, lambda e: e.tensor_scalar(out=out_, in0=in0, scalar1=s1, scalar2=s2, op0=op0, op1=op1), r, w)

    def STT(eng, out_, in0, scalar, in1, op0, op1, r, w):
        return P.op(eng, lambda e: e.scalar_tensor_tensor(out=out_, in0=in0, scalar=scalar, in1=in1, op0=op0, op1=op1), r, w)

    def RED(eng, out_, in_, op, r, w):
        return P.op(eng, lambda e: e.tensor_reduce(out=out_, in_=in_, axis=AX.X, op=op), r, w)

    def CP(eng, out_, in_, r, w):
        return P.op(eng, lambda e: e.tensor_copy(out=out_, in_=in_), r, w)

    def MSET(eng, out_, val, w):
        return P.op(eng, lambda e: e.memset(out_, val), (), w)

    def MM(out_, lhsT, rhs, start, stop, r, w):
        return P.op("tensor", lambda e: e.matmul(out_, lhsT, rhs, start=start, stop=stop), r, w)

    def TR(out_, in_, ident, r, w):
        return P.op("tensor", lambda e: e.transpose(out_, in_, ident), r, w)

    def DMA(q, sem, out_, in_, r, w):
        return P.dma(q, sem, lambda e: e.dma_start(out=out_, in_=in_), r, w)

    PSF = [nc.alloc_psum_tensor(f"psf{i}", [128, 512], F32) for i in range(6)]
    PSB = [nc.alloc_psum_tensor(f"psb{i}", [128, 1024], BF16) for i in range(2)]
    psf_rr = {"v": 0, "a": 0}

    def psf(cons):
        i = psf_rr[cons] % 3 + (0 if cons == "v" else 3)
        psf_rr[cons] += 1
        return PSF[i], f"psf{i}"

    psb_rr = [0]

    def psb():
        i = psb_rr[0] % 2
        psb_rr[0] += 1
        return PSB[i], f"psb{i}"

    ident_f = P.sb("ident_f", [128, 128], F32)
    ident_b = P.sb("ident_b", [128, 128], BF16)
    iot = P.sb("iot", [128, 128], F32)
    ones_b = P.sb("ones_b", [128, 128], BF16)
    U_b = P.sb("U_b", [128, 128], BF16)
    UI_b = P.sb("UI_b", [128, 128], BF16)
    mask3 = P.sb("mask3", [128, 3, 384], BF16)
    metat = P.sb("metat", [128, 80], F32)
    esink = P.sb("esink", [128, 8], F32)
    rows = {}
    for nm in ("S1", "G1", "GT1", "S2", "G2", "GT2", "cS1", "cG1"):
        rows[nm] = P.sb("row_" + nm, [128, 1024], F32)
    REG0 = P.sb_off

    P.op("gpsimd", lambda e: e.iota(iot[:], pattern=[[1, 128]], base=0, channel_multiplier=-1,
                                    allow_small_or_imprecise_dtypes=True), (), ["iot"])
    TS("vector", ident_f[:], iot[:], 0.0, None, ALU.is_equal, None, ["iot"], ["ident_f"])
    CP("vector", ident_b[:], ident_f[:], ["ident_f"], ["ident_b"])
    TS("vector", U_b[:], iot[:], 0.0, None, ALU.is_ge, None, ["iot"], ["U_b"])
    MSET("vector", ones_b[:], 1.0, ["ones_b"])
    DMA("sync", "d_meta", metat[:], meta, [], ["metat"])
    DMA("sync", "d_sink", esink[:], sink.partition_broadcast(128), [], ["esink"])
    ACT(esink[:], esink[:], AF.Exp, ["esink"], ["esink"])
    for v in range(3):
        TS("vector", mask3[:, v, 0:128], iot[:], 0.0, None, ALU.is_le, None, ["iot"], [("mask3", v)])
        MSET("vector", mask3[:, v, 128:256], 1.0, [("mask3", v, 1)])
        TS("vector", mask3[:, v, 256:384], iot[:], 0.0, None, ALU.is_ge, None, ["iot"], [("mask3", v, 2)])
    TS("vector", mask3[:, 1, 0:128], mask3[:, 1, 0:128], metat[:, 0:1], None, ALU.mult, None,
       ["metat", ("mask3", 1)], [("mask3", 1)])
    TS("vector", mask3[:, 2, 256:384], mask3[:, 2, 256:384], metat[:, 1:2], None, ALU.mult, None,
       ["metat", ("mask3", 2, 2)], [("mask3", 2, 2)])

    if "const" in dbg:
        d1 = dbg_out("ident", [128, 128])
        d2 = dbg_out("mask3", [128, 3 * 384], BF16)
        d3 = dbg_out("esink", [128, 8])
        e1 = DMA("sync", "d_dbg", d1, ident_f[:], ["ident_f"], ["dbg1"])
        e2 = DMA("sync", "d_dbg", d2, mask3[:].rearrange("p a b -> p (a b)"),
                 [("mask3", v) for v in range(3)] + [("mask3", v, 1) for v in range(3)] + [("mask3", v, 2) for v in range(3)], ["dbg2"])
        e3 = DMA("sync", "d_dbg", d3, esink[:], ["esink"], ["dbg3"])
        P.finish("sync", [e1, e2, e3])
    if stage <= 0:
        P.emit()
        return nc, dbg_outs

    o = REG0
    WIN = P.sb("WIN", [128, 8, 2944], BF16, off=o)
    mixT = P.sb("mixT", [128, 8, 2048], BF16, off=o)
    o += 47104
    COS = P.sb("COS", [128, W], F32, off=o); o += W * 4
    SINS = P.sb("SINS", [128, W], F32, off=o); o += W * 4
    xt = [P.sb(f"xt{i}", [128, 1024], F32, off=o + i * 4096) for i in range(2)]; o += 8192
    tf = P.sb("tf", [128, 1024], F32, off=o); o += 4096
    hb = [P.sb(f"hb{i}", [128, 1024], BF16, off=o + i * 2048) for i in range(2)]; o += 4096
    hT = [P.sb(f"hT{i}", [128, 8, 512], BF16, off=o + i * 8192) for i in range(2)]
    wo = P.sb("wo", [128, 8, 1024], BF16, off=o)
    o += 16384
    qT_off = o
    qT = P.sb("qT", [128, 4, W], BF16, off=o); o += 4 * W * 2
    kT_off = o
    kT = P.sb("kT", [128, W], BF16, off=o); o += W * 2
    Vt = P.sb("Vt", [128, 18, 2, 65], BF16, off=o); o += 4736
    kcT = P.sb("kcT", [128, 256], BF16, off=o); o += 512
    Vc = P.sb("Vc", [128, 2, 2, 65], BF16, off=o); o += 576
    bgT_off = o
    bgT = P.sb("bgT", [128, 4, 2048], BF16, off=o)
    stg = P.sb("stg", [128, 8, 640], F32, off=o)
    o += 20480
    uT_off = o
    uT = P.sb("uT", [128, 4, W], BF16, off=o); o += 4 * W * 2
    rt1_off = o
    rt1 = P.sb("rt1", [128, 512], F32, off=o); o += 2048
    rt2 = P.sb("rt2", [128, 512], F32, off=o); o += 2048
    cgs = P.sb("cgs", [128, 512], F32, off=o); o += 2048
    small = P.sb("small", [128, 64], F32, off=o); o += 256
    cw = P.sb("cw", [128, 4, 3], F32, off=o); o += 64
    assert o <= P.sb_top, o
    A_END = o

    wa = [P.sb("wa0", [128, 8, 1024], BF16, off=qT_off), P.sb("wa1", [128, 8, 1024], BF16, off=uT_off)]
    o = kT_off
    lb = P.sb("lb", [128, 8, 2, 128], BF16, off=o); o += 4096
    brow = P.sb("brow", [128, 1024], F32, off=o); o += 4096
    cct = P.sb("cct", [128, 8, 2], F32, off=o); o += 64
    scl = P.sb("scl", [128, 8, 2], F32, off=o); o += 64
    assert o <= bgT_off
    gmrow = P.sb("gmrow", [128, 1024], F32, off=rt1_off)

    DMA("sync", "d_cc", cct[:], cc.rearrange("p (k v) -> p k v", v=2), [], ["cct"])
    ACT(scl[:], cct[:], AF.Silu, ["cct"], ["scl"])
    for v in range(2):
        CP("vector", lb[:, :, v, :], scl[:, :, v:v + 1].to_broadcast([128, 8, 128]), ["scl"], [("lb", v)])
    if stage <= 0.3:
        d1 = dbg_out("lb", [128, 8 * 2 * 128], BF16)
        e1 = DMA("sync", "d_dbg", d1, lb[:].rearrange("p a b c -> p (a b c)"), [("lb", 0), ("lb", 1)], ["dbg1"])
        P.finish("sync", [e1])
        P.emit()
        return nc, dbg_outs
    w_ada_v = w_ada.rearrange("(k p) n -> p k n", p=128)
    grp = [(0, [("S1", 0), ("cS1", 1)]), (1, [("G1", 0), ("cG1", 1)]), (2, [("GT1", 0)]),
           (3, [("S2", 0)]), (4, [("G2", 0)]), (5, [("GT2", 0)])]
    for gi, (g, uses) in enumerate(grp):
        wb = wa[gi % 2]
        wn = f"wa{gi % 2}"
        P.dma("gpsimd", "d_" + wn, (lambda wb=wb, g=g: (lambda e: e.dma_start(out=wb[:], in_=w_ada_v[:, :, g * 1024:(g + 1) * 1024])))(),
              [], [wn])
        DMA("sync", "d_brow", brow[:], b_ada[g * 1024:(g + 1) * 1024].partition_broadcast(128), [], ["brow"])
        if stage <= 0.5:
            d1 = dbg_out("wa", [128, 8 * 1024], BF16)
            d2 = dbg_out("brow", [128, 1024])
            e1 = DMA("sync", "d_dbg", d1, wb[:].rearrange("p a b -> p (a b)"), [wn], ["dbg1"])
            e2 = DMA("sync", "d_dbg", d2, brow[:], ["brow"], ["dbg2"])
            P.finish("sync", [e1, e2])
            P.emit()
            return nc, dbg_outs
        for (nm, v) in uses:
            for n in range(2):
                ps, psn = psf("v")
                for k in range(8):
                    MM(ps[:], lb[:, k, v, :], wb[:, k, n * 512:(n + 1) * 512], k == 0, k == 7,
                       [("lb", v), wn], [psn])
                TT("vector", rows[nm][:, n * 512:(n + 1) * 512], ps[:], brow[:, n * 512:(n + 1) * 512], ALU.add,
                   [psn, "brow"], [("row", nm, n)])
                if stage <= 0.7:
                    d1 = dbg_out("r0", [128, 512])
                    e1 = DMA("sync", "d_dbg", d1, rows[nm][:, 0:512], [("row", nm, n)], ["dbg1"])
                    P.finish("sync", [e1])
                    P.emit()
                    return nc, dbg_outs
    for (gsrc, names) in (((g_mix, ("G1", "cG1")), (g_ffn, ("G2",))) if stage > 0.8 else ()):
        DMA("sync", "d_gmrow", gmrow[:], gsrc.partition_broadcast(128), [], ["gmrow"])
        for nm in names:
            TS("vector", rows[nm][:], rows[nm][:], 1.0, None, ALU.add, None,
               [("row", nm, 0), ("row", nm, 1)], [("row", nm, 0), ("row", nm, 1)])
            TT("vector", rows[nm][:], rows[nm][:], gmrow[:], ALU.mult,
               [("row", nm, 0), ("row", nm, 1), "gmrow"], [("row", nm, 0), ("row", nm, 1)])

    def rowdeps(nm):
        return [("row", nm, 0), ("row", nm, 1)]

    if "rows" in dbg:
        d = dbg_out("rows", [8, 128, 1024])
        for i, nm in enumerate(("S1", "G1", "GT1", "S2", "G2", "GT2", "cS1", "cG1")):
            ev = DMA("sync", "d_dbg", d[i], rows[nm][:], rowdeps(nm), ["dbg"])
        P.finish("sync", [ev])
    if stage <= 1:
        P.emit()
        return nc, dbg_outs


    def sc(i):
        return small[:, i:i + 1]
    pid, dd, i32_, isC, ff, inv, invC, invR, sgn, tmpc = [sc(i) for i in range(10)]
    P.op("gpsimd", lambda e: e.iota(small[:, 0:1], pattern=[[0, 1]], base=0, channel_multiplier=1,
                                    allow_small_or_imprecise_dtypes=True), (), ["small"])
    TS("vector", tmpc, pid, 64.0, -64.0, ALU.is_ge, ALU.mult, ["small"], ["small"])
    TT("vector", dd, pid, tmpc, ALU.add, ["small"], ["small"])
    TS("vector", sgn, dd, 32.0, None, ALU.is_ge, None, ["small"], ["small"])
    TS("vector", tmpc, sgn, -32.0, None, ALU.mult, None, ["small"], ["small"])
    TT("vector", i32_, dd, tmpc, ALU.add, ["small"], ["small"])
    TS("vector", isC, i32_, 16.0, None, ALU.is_ge, None, ["small"], ["small"])
    TS("vector", tmpc, isC, -16.0, None, ALU.mult, None, ["small"], ["small"])
    TT("vector", ff, i32_, tmpc, ALU.add, ["small"], ["small"])
    ACT(inv, ff, AF.Exp, ["small"], ["small"], scale=-float(np.log(10000.0) / 16.0))
    TT("vector", invC, inv, isC, ALU.mult, ["small"], ["small"])
    TT("vector", invR, inv, invC, ALU.subtract, ["small"], ["small"])
    TS("vector", sgn, sgn, 2.0, -1.0, ALU.mult, ALU.add, ["small"], ["small"])
    rrA = P.sb("rrA", [128, W], F32, off=qT_off)
    rrI = P.sb("rrI", [128, W], I32, off=qT_off + W * 4)
    ang = P.sb("ang", [128, W], F32, off=uT_off)
    P.op("gpsimd", lambda e: e.iota(COS[:], pattern=[[1, 36], [0, 64]], base=0, channel_multiplier=0,
                                    allow_small_or_imprecise_dtypes=True), (), ["COS"])
    P.op("gpsimd", lambda e: e.iota(SINS[:], pattern=[[0, 36], [1, 64]], base=0, channel_multiplier=0,
                                    allow_small_or_imprecise_dtypes=True), (), ["SINS"])
    HW_ = W // 2
    TWO_PI = float(2 * np.pi)
    for hh in range(2):
        sl = slice(hh * HW_, (hh + 1) * HW_)
        TS("vector", COS[:, sl], COS[:, sl], metat[:, 2:3], None, ALU.add, None, ["COS", "metat"], ["COS"])
        TS("vector", COS[:, sl], COS[:, sl], invR, None, ALU.mult, None, ["COS", "small"], ["COS"])
        TS("vector", SINS[:, sl], SINS[:, sl], invC, None, ALU.mult, None, ["SINS", "small"], ["SINS"])
    TT("vector", ang[:], COS[:], SINS[:], ALU.add, ["COS", "SINS"], ["ang"])

    def range_reduce_sin(dst, dstn, offset):
        TS("vector", rrA[:], ang[:], 1.0 / TWO_PI, offset / TWO_PI + 8.5, ALU.mult, ALU.add, ["ang"], ["rrA"])
        CP("vector", rrI[:], rrA[:], ["rrA"], ["rrI"])
        CP("vector", rrA[:], rrI[:], ["rrI"], ["rrA"])
        TS("vector", rrA[:], rrA[:], -TWO_PI, 8 * TWO_PI + offset, ALU.mult, ALU.add, ["rrA"], ["rrA"])
        TT("vector", dst[:], ang[:], rrA[:], ALU.add, ["ang", "rrA"], [dstn])
        TS("vector", rrA[:], dst[:], float(np.pi), -TWO_PI, ALU.is_gt, ALU.mult, [dstn], ["rrA"])
        TT("vector", dst[:], dst[:], rrA[:], ALU.add, [dstn, "rrA"], [dstn])
        TS("vector", rrA[:], dst[:], -float(np.pi), TWO_PI, ALU.is_lt, ALU.mult, [dstn], ["rrA"])
        TT("vector", dst[:], dst[:], rrA[:], ALU.add, [dstn, "rrA"], [dstn])
        ACT(dst[:], dst[:], AF.Sin, [dstn], [dstn])

    range_reduce_sin(SINS, "SINS", 0.0)
    range_reduce_sin(COS, "COS", float(np.pi / 2))
    for hh in range(2):
        sl = slice(hh * HW_, (hh + 1) * HW_)
        TS("vector", SINS[:, sl], SINS[:, sl], sgn, None, ALU.mult, None, ["SINS", "small"], ["SINS"])

    w_in_v = w_in.rearrange("(k p) n -> p k n", p=128)
    DMA("sync", "d_stg", stg[:], w_in_v[:, :, 0:640], [], ["stg"])
    qd = WIN[:, :, 0:512].rearrange("p k (c h d) -> p k c h d", c=4, h=2, d=64)
    qs = stg[:, :, 0:512].rearrange("p k (h c d) -> p k c h d", h=2, c=4, d=64)
    for h in range(2):
        ACT(qd[:, :, :, h, :], qs[:, :, :, h, :], AF.Copy, ["stg"], [("WIN", "q", h)])
    qd2 = WIN[:, :, 512:1024].rearrange("p k (c h s d) -> p k c h s d", c=4, h=2, s=2, d=32)
    qs2 = stg[:, :, 0:512].rearrange("p k (h c s d) -> p k c h s d", h=2, c=4, s=2, d=32)
    for h in range(2):
        for s in range(2):
            ACT(qd2[:, :, :, h, s, :], qs2[:, :, :, h, 1 - s, :], AF.Copy, ["stg"], [("WIN", "qsw", h, s)])
    ACT(WIN[:, :, 1024:1152], stg[:, :, 512:640], AF.Copy, ["stg"], [("WIN", "k")])
    kd2 = WIN[:, :, 1152:1280].rearrange("p k (h s d) -> p k h s d", h=2, s=2, d=32)
    ks2 = stg[:, :, 512:640].rearrange("p k (h s d) -> p k h s d", h=2, s=2, d=32)
    for s in range(2):
        ACT(kd2[:, :, :, s, :], ks2[:, :, :, 1 - s, :], AF.Copy, ["stg"], [("WIN", "ksw", s)])
    WINQ = [("WIN", "q", 0), ("WIN", "q", 1)]
    WINQS = [("WIN", "qsw", h, s) for h in range(2) for s in range(2)]
    WINK = [("WIN", "k")]
    WINKS = [("WIN", "ksw", 0), ("WIN", "ksw", 1)]
    for (nm, d0, s0, n) in (("v", 1280, 640, 128), ("bg", 1408, 768, 512), ("cg", 1920, 1280, 512), ("hv", 2432, 1792, 512)):
        P.dma("gpsimd", "d_win_" + nm, (lambda d0=d0, s0=s0, n=n: (lambda e: e.dma_start(out=WIN[:, :, d0:d0 + n], in_=w_in_v[:, :, s0:s0 + n])))(),
              [], [("WIN", nm)])
    for kk in range(3):
        for c4 in range(4):
            P.dma("sync", "d_cw", (lambda kk=kk, c4=c4: (lambda e: e.dma_start(
                out=cw[:, c4, kk:kk + 1], in_=conv_w[kk, c4 * 128:(c4 + 1) * 128].rearrange("(p o) -> p o", o=1))))(),
                [], [("cw", kk, c4)])
    MSET("vector", Vt[:, :, :, 64:65], 1.0, [("Vt", "ones")])
    MSET("vector", Vc[:, :, :, 64:65], 1.0, [("Vc", "ones")])

    xt_rr = [0]

    def norm_mod(src_rows, Gn, Sn, hbuf, hname, extra_r=()):
        i = xt_rr[0] % 2
        xt_rr[0] += 1
        xtile, xn = xt[i], f"xt{i}"
        DMA("sync", "d_" + xn, xtile[:], src_rows, list(extra_r), [xn])
        norm_mod_sb(xtile, xn, Gn, Sn, hbuf, hname)
        return xtile, xn

    tf2 = P.sb("tf2", [128, 1024], F32, off=bgT_off + 16384)
    nm_rr = [0]

    def norm_mod_sb(xtile, xn, Gn, Sn, hbuf, hname):
        pi = nm_rr[0] % 2
        nm_rr[0] += 1
        tfx, tfn = (tf, "tf") if pi == 0 else (tf2, "tf2")
        ss = small[:, 16 + 2 * pi:17 + 2 * pi]
        rstd = small[:, 17 + 2 * pi:18 + 2 * pi]
        ssn, rsn = f"ss{pi}", f"rstd{pi}"
        ACT(tfx[:], xtile[:], AF.Square, [xn], [tfn])
        RED("vector", ss, tfx[:], ALU.add, [tfn], [ssn])
        TS("vector", rstd, ss, 1.0 / 1024.0, 1e-6, ALU.mult, ALU.add, [ssn], [rsn])
        ACT(rstd, rstd, AF.Ln, [rsn], [rsn])
        ACT(rstd, rstd, AF.Exp, [rsn], [rsn], scale=-0.5)
        ACT(tfx[:], xtile[:], AF.Copy, [xn, rsn], [tfn], scale=rstd)
        TT("vector", tfx[:], tfx[:], rows[Gn][:], ALU.mult, [tfn] + rowdeps(Gn), [tfn])
        TT("vector", hbuf[:], tfx[:], rows[Sn][:], ALU.add, [tfn] + rowdeps(Sn), [hname])

    def transpose_to(hbuf, hname, dst, dst_name):
        pb, pbn = psb()
        pbv = pb[:].rearrange("p (k t) -> p k t", k=8)
        for k in range(8):
            TR(pbv[:, k, :], hbuf[:, k * 128:(k + 1) * 128], ident_b[:], [hname, "ident_b"], [(pbn, k)])
        ACT(dst, pbv, AF.Copy, [(pbn, k) for k in range(8)], [dst_name])

    hcT = hT[0]
    for t in range(2):
        norm_mod(ctx[t * 128:(t + 1) * 128, :], "cG1", "cS1", hb[t % 2], f"hb{t % 2}")
        transpose_to(hb[t % 2], f"hb{t % 2}", hcT[:, :, t * 128:(t + 1) * 128], ("hT0", t))
    ps, psn = psf("a")
    for k in range(8):
        MM(ps[:, 0:256], WIN[:, k, 1024:1152], hcT[:, k, 0:256], k == 0, k == 7,
           WINK + [("hT0", 0), ("hT0", 1)], [psn])
    ACT(kcT[:], ps[:, 0:256], AF.Copy, [psn], ["kcT"])
    for t in range(2):
        ps, psn = psf("a")
        for k in range(8):
            MM(ps[:, 0:128], hcT[:, k, t * 128:(t + 1) * 128], WIN[:, k, 1280:1408], k == 0, k == 7,
               [("WIN", "v"), ("hT0", t)], [psn])
        ACT(Vc[:, t, :, 0:64], ps[:, 0:128].rearrange("p (h d) -> p h d", h=2), AF.Copy, [psn], [("Vc", t)])

    if "ctx" in dbg:
        d1 = dbg_out("kcT", [128, 256], BF16)
        d2 = dbg_out("Vc", [128, 2 * 2 * 65], BF16)
        e1 = DMA("sync", "d_dbg", d1, kcT[:], ["kcT"], ["dbg1"])
        e2 = DMA("sync", "d_dbg", d2, Vc[:].rearrange("p a b c -> p (a b c)"), [("Vc", 0), ("Vc", 1), ("Vc", "ones")], ["dbg2"])
        P.finish("sync", [e1, e2])
    if stage <= 2:
        P.emit()
        return nc, dbg_outs

    chunks = [(0, 128, False)] + [(128 + 512 * i, 512, True) for i in range(4)] + [(2176, 128, False)]
    NCONV = int(os.environ.get("MK_NCONV", "4"))
    for ci, (w0, n, central) in enumerate(chunks):
        if (stage <= 2.5 and ci >= 1) or (stage <= 2.7 and ci >= 2):
            break
        hTc, hTn = hT[ci % 2], f"hT{ci % 2}"
        ntile = n // 128
        for t in range(ntile):
            j = (ci * 4 + t) % 2
            norm_mod(x[w0 + t * 128:w0 + (t + 1) * 128, :], "G1", "S1", hb[j], f"hb{j}")
            transpose_to(hb[j], f"hb{j}", hTc[:, :, t * 128:(t + 1) * 128], (hTn, t))
        hdeps = [(hTn, t) for t in range(ntile)]

        def proj(col0, wdeps, cons):
            ps, psn = psf(cons)
            for k in range(8):
                MM(ps[:, 0:n], WIN[:, k, col0:col0 + 128], hTc[:, k, 0:n], k == 0, k == 7, wdeps + hdeps, [psn])
            return ps, psn

        def rope_out(col0, colsw, wd, wsd, dst, dstn):
            pa, pan = proj(col0, wd, "v")
            pb_, pbn_ = proj(colsw, wsd, "v")
            TT("vector", rt1[:, 0:n], pa[:, 0:n], COS[:, w0:w0 + n], ALU.mult, [pan, "COS"], ["rt1"])
            TT("vector", rt2[:, 0:n], pb_[:, 0:n], SINS[:, w0:w0 + n], ALU.mult, [pbn_, "SINS"], ["rt2"])
            TT("vector", dst, rt1[:, 0:n], rt2[:, 0:n], ALU.add, ["rt1", "rt2"], [dstn])

        if central:
            for c in range(4):
                rope_out(c * 128, 512 + c * 128, WINQ, WINQS, qT[:, c, w0:w0 + n], ("qT", c, ci))
        PARTS = os.environ.get("MK_PARTS", "rvc")
        if "r" in PARTS:
            rope_out(1024, 1152, WINK, WINKS, kT[:, w0:w0 + n], ("kT", ci))
        for t in (range(ntile) if "v" in PARTS else ()):
            ps, psn = psf("a")
            for k in range(8):
                MM(ps[:, 0:128], hTc[:, k, t * 128:(t + 1) * 128], WIN[:, k, 1280:1408], k == 0, k == 7,
                   [("WIN", "v"), (hTn, t)], [psn])
            wt = w0 // 128 + t
            ACT(Vt[:, wt, :, 0:64], ps[:, 0:128].rearrange("p (h d) -> p h d", h=2), AF.Copy, [psn], [("Vt", wt)])
        for c in (range(NCONV) if "c" in PARTS else ()):
            if central:
                ps, psn = proj(1408 + c * 128, [("WIN", "bg")], "a")
                ACT(bgT[:, c, w0 - 128:w0 - 128 + n], ps[:, 0:n], AF.Copy, [psn], [("bgT", c, ci)])
            pc, pcn = proj(1920 + c * 128, [("WIN", "cg")], "a")
            ph, phn = proj(2432 + c * 128, [("WIN", "hv")], "v")
            ACT(cgs[:, 0:n], pc[:, 0:n], AF.Copy, [pcn], ["cgs"])
            TT("vector", uT[:, c, w0:w0 + n], ph[:, 0:n], cgs[:, 0:n], ALU.mult, [phn, "cgs"], [("uT", c, ci)])

    if "proj" in dbg:
        d1 = dbg_out("qT", [128, 4 * W], BF16)
        d2 = dbg_out("kT", [128, W], BF16)
        d3 = dbg_out("Vt", [128, 18 * 130], BF16)
        d4 = dbg_out("uT", [128, 4 * W], BF16)
        d5 = dbg_out("bgT", [128, 4 * 2048], BF16)
        allq = [("qT", c, ci) for c in range(4) for ci in range(1, 5)]
        allk = [("kT", ci) for ci in range(6)]
        allv = [("Vt", t) for t in range(18)] + [("Vt", "ones")]
        allu = [("uT", c, ci) for c in range(4) for ci in range(6)]
        allb = [("bgT", c, ci) for c in range(4) for ci in range(1, 5)]
        evs = [DMA("sync", "d_dbg", d1, qT[:].rearrange("p a b -> p (a b)"), allq, ["dbg1"]),
               DMA("sync", "d_dbg", d2, kT[:], allk, ["dbg2"]),
               DMA("sync", "d_dbg", d3, Vt[:].rearrange("p a b c -> p (a b c)"), allv, ["dbg3"]),
               DMA("sync", "d_dbg", d4, uT[:].rearrange("p a b -> p (a b)"), allu, ["dbg4"]),
               DMA("sync", "d_dbg", d5, bgT[:].rearrange("p a b -> p (a b)"), allb, ["dbg5"])]
        P.finish("sync", evs)
    if stage <= 3:
        P.emit()
        return nc, dbg_outs

    ALLWIN = WINQ + WINQS + WINK + WINKS + [("WIN", nm) for nm in ("v", "bg", "cg", "hv")]
    o2 = REG0 + 32768
    PL = [P.sb(f"PL{i}", [128, 384], BF16, off=o2 + i * 768) for i in range(2)]; o2 += 1536
    PC = [P.sb(f"PC{i}", [128, 256], BF16, off=o2 + i * 512) for i in range(2)]; o2 += 1024
    att_tm = P.sb("att_tm", [128, 512], BF16, off=o2); o2 += 1024
    rec = P.sb("rec", [128, 8], F32, off=o2); o2 += 64
    cvt = [P.sb(f"cvt{i}", [128, 512], F32, off=o2 + i * 2048) for i in range(2)]; o2 += 4096
    assert o2 <= REG0 + 47104

    def kchunk(wb):
        return 0 if wb == 0 else (5 if wb == 17 else 1 + (wb - 1) // 4)

    VONES = [("Vt", "ones")]
    TS("vector", uT[:, :, 127:128], uT[:, :, 127:128], metat[:, 0:1], None, ALU.mult, None,
       [("uT", c, 0) for c in range(4)] + ["metat"], [("uT", c, 0) for c in range(4)])
    TS("vector", uT[:, :, 2176:2177], uT[:, :, 2176:2177], metat[:, 1:2], None, ALU.mult, None,
       [("uT", c, 5) for c in range(4)] + ["metat"], [("uT", c, 5) for c in range(4)])

    def conv_unit(tcn, c):
        w0 = 128 + tcn * 512
        ud = [("uT", c, ci) for ci in (tcn, tcn + 1, tcn + 2)]
        cwd = [("cw", kk, c4) for kk in range(3) for c4 in range(4)]
        TS("vector", cvt[0][:], uT[:, c, w0 - 1:w0 + 511], cw[:, c, 0:1], None, ALU.mult, None, ud + cwd, ["cvt0"] + ALLWIN)
        TS("vector", cvt[1][:], uT[:, c, w0:w0 + 512], cw[:, c, 1:2], None, ALU.mult, None, ud + cwd, ["cvt1"] + ALLWIN)
        TT("vector", cvt[0][:], cvt[0][:], cvt[1][:], ALU.add, ["cvt0", "cvt1"], ["cvt0"])
        TS("vector", cvt[1][:], uT[:, c, w0 + 1:w0 + 513], cw[:, c, 2:3], None, ALU.mult, None, ud + cwd, ["cvt1"])
        TT("vector", cvt[0][:], cvt[0][:], cvt[1][:], ALU.add, ["cvt0", "cvt1"], ["cvt0"])
        TT("vector", mixT[:, 4 + c, tcn * 512:(tcn + 1) * 512], cvt[0][:], bgT[:, c, tcn * 512:(tcn + 1) * 512], ALU.mult,
           ["cvt0", ("bgT", c, tcn + 1)], [("mixT", "conv", c, tcn)] + ALLWIN)

    for i in range(1, 17):
        ci_q = 1 + (i - 1) // 4
        mv = 1 if i == 1 else (2 if i == 16 else 0)
        pvs = [psf("v"), psf("v")]
        for hn in range(8):
            half, c = hn // 4, hn % 4
            r0 = half * 64
            j = hn % 2
            sl, sln = psf("a")
            sc_, scn = psf("a")
            qsl = qT[r0:r0 + 64, c, i * 128:(i + 1) * 128]
            for kb in range(3):
                wb = i - 1 + kb
                MM(sl[:, kb * 128:(kb + 1) * 128], kT[r0:r0 + 64, wb * 128:(wb + 1) * 128], qsl, True, True,
                   [("qT", c, ci_q), ("kT", kchunk(wb))], [sln])
            for cb in range(2):
                MM(sc_[:, cb * 128:(cb + 1) * 128], kcT[r0:r0 + 64, cb * 128:(cb + 1) * 128], qsl, True, True,
                   [("qT", c, ci_q), "kcT"], [scn])
            ACT(PL[j][:], sl[:, 0:384], AF.Exp, [sln], [f"PL{j}"] + ALLWIN, scale=0.125)
            ACT(PC[j][:], sc_[:, 0:256], AF.Exp, [scn], [f"PC{j}"] + ALLWIN, scale=0.125)
            TT("vector", PL[j][:], PL[j][:], mask3[:, mv, :], ALU.mult,
               [f"PL{j}", ("mask3", mv), ("mask3", mv, 1), ("mask3", mv, 2)], [f"PL{j}"])
            pv, pvn = pvs[half]
            pvr = pv[:, c * 65:(c + 1) * 65]
            for kb in range(3):
                wb = i - 1 + kb
                MM(pvr, PL[j][:, kb * 128:(kb + 1) * 128], Vt[:, wb, half, :], kb == 0, False,
                   [f"PL{j}", ("Vt", wb)] + VONES, [pvn])
            for cb in range(2):
                MM(pvr, PC[j][:, cb * 128:(cb + 1) * 128], Vc[:, cb, half, :], False, cb == 1,
                   [f"PC{j}", ("Vc", cb), ("Vc", "ones")], [pvn])
        for b in range(2):
            pv, pvn = pvs[b]
            pvv = pv[:, 0:260].rearrange("p (h e) -> p h e", h=4)
            TT("vector", rec[:, b * 4:(b + 1) * 4].unsqueeze(2), pvv[:, :, 64:65], esink[:, b * 4:(b + 1) * 4].unsqueeze(2),
               ALU.add, [pvn, "esink"], [("rec", b)] + ALLWIN)
            P.op("vector", (lambda b=b: (lambda e: e.reciprocal(rec[:, b * 4:(b + 1) * 4], rec[:, b * 4:(b + 1) * 4])))(),
                 [("rec", b)], [("rec", b)])
            TT("vector", att_tm[:, b * 256:(b + 1) * 256].rearrange("p (h d) -> p h d", h=4), pvv[:, :, 0:64],
               rec[:, b * 4:(b + 1) * 4].unsqueeze(2).to_broadcast([128, 4, 64]), ALU.mult,
               [pvn, ("rec", b)], [("att_tm", b)] + ALLWIN)
        pb, pbn = psb()
        pbv = pb[:, 0:512].rearrange("p (k t) -> p k t", k=4)
        for cc in range(4):
            TR(pbv[:, cc, :], att_tm[:, cc * 128:(cc + 1) * 128], ident_b[:], [("att_tm", cc // 2), "ident_b"], [(pbn, cc)])
        ACT(mixT[:, 0:4, (i - 1) * 128:i * 128], pbv, AF.Copy, [(pbn, cc) for cc in range(4)],
            [("mixT", "att", i - 1)] + ALLWIN)
        conv_unit((i - 1) // 4, (i - 1) % 4)

    if stage <= 4:
        P.emit()
        return nc, dbg_outs

    HTALL = [(f"hT{a}", t) for a in range(2) for t in range(4)]
    P.dma("gpsimd", "d_wo", lambda e: e.dma_start(out=wo[:], in_=w_out.rearrange("(k p) n -> p k n", p=128)), [], ["wo"] + HTALL)
    QALL = [("qT", c, ci) for c in range(4) for ci in range(1, 5)]
    o3 = qT_off
    o3 += 4096
    h2Tall = P.sb("h2Tall", [128, 8, 2048], BF16, off=bgT_off)
    CONVDEAD = [("uT", c, ci) for c in range(4) for ci in range(6)] + [("bgT", c, ci) for c in range(4) for ci in range(1, 5)]
    rows_off = REG0 - 8 * 4096
    affTM = P.sb("affTM", [128, 16, 16], F32, off=rows_off)
    gm = P.sb("gm", [128, 16, 16], F32, off=rows_off + 1024)
    thr = P.sb("thr", [128, 16], F32, off=rows_off + 2048)
    affT = P.sb("affT", [16, 2048], F32, off=o3); o3 += 8192
    wr = P.sb("wr", [128, 8, 16], BF16, off=o3); o3 += 256
    sm = P.sb("sm", [128, 64], F32, off=o3); o3 += 256
    assert o3 <= qT_off + 4 * W * 2
    P.dma("gpsimd", "d_wr", lambda e: e.dma_start(out=wr[:], in_=w_router.rearrange("(k p) e -> p k e", p=128)), [], ["wr"] + QALL)
    for tile in range(16):
        tcn = tile // 4
        mdeps = [("mixT", "att", tile)] + [("mixT", "conv", c, tcn) for c in range(4)]
        i = xt_rr[0] % 2
        xt_rr[0] += 1
        xtile, xn = xt[i], f"xt{i}"
        DMA("sync", "d_" + xn, xtile[:], x[128 + tile * 128:256 + tile * 128, :], [], [xn])
        for n in range(2):
            ps, psn = psf("v")
            for k in range(8):
                MM(ps[:], mixT[:, k, tile * 128:(tile + 1) * 128], wo[:, k, n * 512:(n + 1) * 512], k == 0, k == 7,
                   mdeps + ["wo"], [psn])
            TT("vector", tf[:, n * 512:(n + 1) * 512], ps[:], rows["GT1"][:, n * 512:(n + 1) * 512], ALU.mult,
               [psn] + rowdeps("GT1"), ["tf"])
        TT("vector", xtile[:], xtile[:], tf[:], ALU.add, [xn, "tf"], [xn])
        DMA("sync", "d_x1d_" + xn, x1d[tile * 128:(tile + 1) * 128, :], xtile[:], [xn], [("x1d", tile)])
        j = tile % 2
        norm_mod_sb(xtile, xn, "G2", "S2", hb[j], f"hb{j}")
        DMA("sync", f"d_h2loc{j}", h2loc[tile * 128:(tile + 1) * 128, :], hb[j][:], [f"hb{j}"], [("h2loc", tile)])
        h2v = h2Tall[:, :, tile * 128:(tile + 1) * 128]
        pb, pbn = psb()
        pbv = pb[:].rearrange("p (k t) -> p k t", k=8)
        for k in range(8):
            TR(pbv[:, k, :], hb[j][:, k * 128:(k + 1) * 128], ident_b[:], [f"hb{j}", "ident_b"], [(pbn, k)])
        ACT(h2v, pbv, AF.Copy, [(pbn, k) for k in range(8)], [("h2T", tile)] + CONVDEAD)
        ps, psn = psf("v")
        for k in range(8):
            MM(ps[:, 0:16], h2Tall[:, k, tile * 128:(tile + 1) * 128], wr[:, k, :], k == 0, k == 7, [("h2T", tile), "wr"], [psn])
        mx, nmx, ssum, ex = sm[:, 0:1], sm[:, 1:2], sm[:, 2:3], sm[:, 16:32]
        af = affTM[:, tile, :]
        RED("vector", mx, ps[:, 0:16], ALU.max, [psn], ["sm_mx"])
        TS("vector", nmx, mx, -1.0, None, ALU.mult, None, ["sm_mx"], ["sm_nmx"])
        ACT(ex, ps[:, 0:16], AF.Exp, [psn, "sm_nmx"], ["sm_ex"], bias=nmx)
        RED("vector", ssum, ex, ALU.add, ["sm_ex"], ["sm_sum"])
        P.op("vector", lambda e: e.reciprocal(sm[:, 2:3], sm[:, 2:3]), ["sm_sum"], ["sm_sum"])
        TS("vector", af, ex, ssum, None, ALU.mult, None, ["sm_ex", "sm_sum"], [("affTM", tile)])
        pt, ptn = psf("a")
        TR(pt[0:16, 0:128], af, ident_f[:], [("affTM", tile), "ident_f"], [ptn])
        ACT(affT[:, tile * 128:(tile + 1) * 128], pt[0:16, 0:128], AF.Copy, [ptn], [("affT", tile)] + QALL)
    DMA("sync", "d_affloc", affloc, affT[:], [("affT", t) for t in range(16)], ["affloc"])

    if "a3" in dbg:
        d1 = dbg_out("x1", [2048, 1024])
        d2 = dbg_out("aff", [16, 2048])
        d3 = dbg_out("h2", [2048, 1024], BF16)
        e1 = DMA("sync", "d_dbg1", d1, x1d, [("x1d", t) for t in range(16)], ["dbg1"])
        e2 = DMA("sync", "d_dbg2", d2, affloc, ["affloc"], ["dbg2"])
        e3 = DMA("sync", "d_dbg3", d3, h2loc, [("h2loc", t) for t in range(16)], ["dbg3"])
        P.finish("sync", [e1, e2, e3])
    if stage <= 5:
        P.emit()
        return nc, dbg_outs

    FENCE = [k for k in P.res.keys() if not (isinstance(k, str) and (k.startswith("psf") or k in ("ident_b", "ident_f", "ones_b", "metat")))
             and not (isinstance(k, tuple) and k[0] in ("row", "h2T", "affTM", "x1d", "h2loc"))]
    NTB, NITB = 8, 8
    P.dma("gpsimd", "d_ag_aff", lambda e: e.collective_compute("AllGather", ALU.bypass, replica_groups=GROUPS,
                                                               ins=[affloc.opt()], outs=[affall.opt()]),
          ["affloc"], ["affall", "agchain"], inc=1)
    ob = REG0 + 159936 - 0
    ob = bgT_off + 32768
    AFt = P.sb("AFt", [128, 16, 64], F32, off=ob); ob += 4096
    FR = P.sb("FR", [128, 16, NTB], F32, off=ob); ob += 512
    Tt = P.sb("Tt", [128, 16, NTB], F32, off=ob); ob += 512
    tmpa = P.sb("tmpa", [128, 16, NTB], F32, off=ob); ob += 512
    get = P.sb("get", [128, 16, NTB], F32, off=ob); ob += 512
    cntb = P.sb("cntb", [128, 16 * NTB], BF16, off=ob); ob += 256
    lo = P.sb("lo", [128, 16], F32, off=ob); ob += 64
    hi = P.sb("hi", [128, 16], F32, off=ob); ob += 64
    wdt = P.sb("wdt", [128, 16], F32, off=ob); ob += 64
    red = P.sb("red", [128, 16], F32, off=ob); ob += 64
    idxt = P.sb("idxt", [128, 16], I32, off=ob); ob += 64
    assert ob <= rt1_off + 6144
    cmpb = P.sb("cmpb", [128, 16, NTB, 64], BF16, off=REG0 + 49152)
    for r in range(4):
        DMA("sync", "d_AFt", AFt[32 * r:32 * (r + 1), :, :],
            affall[r * 16:(r + 1) * 16, :].rearrange("e (p j) -> p e j", p=32, j=64), ["affall"], [("AFt", r)] + FENCE)
    AFD = [("AFt", r) for r in range(4)]
    P.op("gpsimd", lambda e: e.iota(FR[:], pattern=[[0, 16], [1, NTB]], base=1, channel_multiplier=0,
                                    allow_small_or_imprecise_dtypes=True), (), ["FR"] + FENCE)
    P.op("gpsimd", lambda e: e.iota(idxt[:], pattern=[[128, 16]], base=0, channel_multiplier=1), (), ["idxt"] + FENCE)
    TS("vector", FR[:], FR[:], 1.0 / (NTB + 1), None, ALU.mult, None, ["FR"], ["FR"])
    MSET("vector", lo[:], 0.0, ["lo"] + FENCE)
    MSET("vector", hi[:], 1.0, ["hi"])
    for it in range(NITB):
        TT("vector", wdt[:], hi[:], lo[:], ALU.subtract, ["hi", "lo"], ["wdt"])
        TT("vector", Tt[:], FR[:], wdt[:].unsqueeze(2).to_broadcast([128, 16, NTB]), ALU.mult, ["FR", "wdt"], ["Tt"])
        TT("vector", Tt[:], Tt[:], lo[:].unsqueeze(2).to_broadcast([128, 16, NTB]), ALU.add, ["Tt", "lo"], ["Tt"])
        TT("vector", cmpb[:], AFt[:].unsqueeze(2).to_broadcast([128, 16, NTB, 64]),
           Tt[:].unsqueeze(3).to_broadcast([128, 16, NTB, 64]), ALU.is_ge, AFD + ["Tt"], ["cmpb"] + FENCE)
        RED("vector", tmpa[:], cmpb[:], ALU.add, ["cmpb"], ["tmpa"])
        CP("vector", cntb[:], tmpa[:].rearrange("p e k -> p (e k)"), ["tmpa"], ["cntb"])
        ps, psn = psf("v")
        MM(ps[:, 0:16 * NTB], ones_b[:], cntb[:], True, True, ["cntb", "ones_b"], [psn])
        TS("vector", get[:].rearrange("p e k -> p (e k)"), ps[:, 0:16 * NTB], 1024.0, None, ALU.is_ge, None, [psn], ["get"])
        TT("vector", tmpa[:], Tt[:], get[:], ALU.mult, ["Tt", "get"], ["tmpa"])
        RED("vector", red[:], tmpa[:], ALU.max, ["tmpa"], ["red"])
        TT("vector", lo[:], lo[:], red[:], ALU.max, ["lo", "red"], ["lo"])
        TS("vector", tmpa[:], get[:], 2.0, None, ALU.mult, None, ["get"], ["tmpa"])
        TT("vector", tmpa[:], tmpa[:], Tt[:], ALU.add, ["tmpa", "Tt"], ["tmpa"])
        RED("vector", red[:], tmpa[:], ALU.min, ["tmpa"], ["red"])
        TT("vector", hi[:], hi[:], red[:], ALU.min, ["hi", "red"], ["hi"])
    CP("vector", thr[:], lo[:], ["lo"], ["thr"])
    AFFTM = [("affTM", t) for t in range(16)]
    TT("vector", gm[:], affTM[:], thr[:].unsqueeze(1).to_broadcast([128, 16, 16]), ALU.is_ge, AFFTM + ["thr"], ["gm"])
    TT("vector", gm[:], gm[:], affTM[:], ALU.mult, ["gm"] + AFFTM, ["gm"])

    if "thr" in dbg:
        d1 = dbg_out("thr", [128, 16])
        d2 = dbg_out("gm", [128, 256])
        e1 = DMA("sync", "d_dbg1", d1, thr[:], ["thr"], ["dbg1"])
        e2 = DMA("sync", "d_dbg2", d2, gm[:].rearrange("p a b -> p (a b)"), ["gm"], ["dbg2"])
        P.finish("sync", [e1, e2])
    if stage <= 6:
        P.emit()
        return nc, dbg_outs

    for j4 in range(4):
        P.dma("gpsimd", "d_ag_h2", (lambda j4=j4: (lambda e: e.collective_compute(
            "AllGather", ALU.bypass, replica_groups=GROUPS,
            ins=[h2loc[j4 * 512:(j4 + 1) * 512, :].opt()], outs=[h2all[j4 * 2048:(j4 + 1) * 2048, :].opt()])))(),
            [("h2loc", t) for t in range(j4 * 4, j4 * 4 + 4)] + ["agchain"], [("h2all", j4), "agchain"], inc=1)
    H2ALLD = [("h2all", j4) for j4 in range(4)]
    TAB = P.sb("TAB", [128, 16, 128], F32, off=qT_off)
    ob2 = kT_off
    ones64 = P.sb("ones64", [128, 64], F32, off=ob2); ob2 += 256
    n4 = P.sb("n4", [128, 4], F32, off=ob2); ob2 += 64
    t16 = P.sb("t16", [128, 16], F32, off=ob2); ob2 += 64
    rhsU = P.sb("rhsU", [128, 4, 128], BF16, off=ob2); ob2 += 1024
    rhsI = P.sb("rhsI", [128, 4, 128], BF16, off=ob2); ob2 += 1024
    offs_sb = P.sb("offs_sb", [128, 4, 128], F32, off=ob2); ob2 += 2048
    nrow_sb = P.sb("nrow_sb", [128, 4, 128], F32, off=ob2); ob2 += 2048
    sval = P.sb("sval", [128, 8], F32, off=ob2); ob2 += 64
    koffs = P.sb("koffs", [128, 4, 8], F32, off=ob2); ob2 += 128
    pS = P.sb("pS", [128, 4, 8], F32, off=ob2); ob2 += 128
    oex = P.sb("oex", [128, 4, 8], F32, off=ob2); ob2 += 128
    rS = P.sb("rS", [128, 4, 8], F32, off=ob2); ob2 += 128
    jS = P.sb("jS", [128, 4, 8], F32, off=ob2); ob2 += 128
    gS = P.sb("gS", [128, 4, 8], F32, off=ob2); ob2 += 128
    tSf = P.sb("tSf", [128, 4, 8], F32, off=ob2); ob2 += 128
    RIDX = P.sb("RIDX", [128, 4, 8], I32, off=ob2); ob2 += 128
    TIDX = P.sb("TIDX", [128, 4, 8], I32, off=ob2); ob2 += 128
    ZIDXf = P.sb("ZIDXf", [128, 4, 16], F32, off=ob2); ob2 += 256
    ZIDX = P.sb("ZIDX", [128, 4, 16], I32, off=ob2); ob2 += 256
    yz = [P.sb(f"yz{i}", [128, 1024], BF16, off=rows_off + 3 * 4096 + i * 2048) for i in range(2)]
    assert ob2 <= bgT_off, ob2
    cmpP = P.sb("cmpP", [128, 4, 8, 128], F32, off=REG0 + 32768)
    Gt = P.sb("Gt", [128, 4, 8, 128], F32, off=REG0 + 49152)

    MSET("vector", ones64[:], 1.0, ["ones64"] + FENCE)
    TT("vector", TAB[:, :, 64:128], AFt[:], thr[:].unsqueeze(2).to_broadcast([128, 16, 64]), ALU.is_ge, AFD + ["thr"], ["TABm"] + FENCE)
    for e16 in range(16):
        P.op("vector", (lambda e16=e16: (lambda e: e.tensor_tensor_scan(out=TAB[:, e16, 0:64], data0=ones64[:], data1=TAB[:, e16, 64:128],
                                                                          initial=0.0, op0=ALU.mult, op1=ALU.add)))(),
             ["TABm", "ones64"], [("TABc", e16)])
    TABC = [("TABc", e16) for e16 in range(16)]
    TT("vector", TAB[:, :, 64:128], TAB[:, :, 64:128], AFt[:], ALU.mult, ["TABm"] + TABC + AFD, ["TABm"])
    DMA("sync", "d_tabd", tabd.rearrange("(e p) c -> p e c", p=128), TAB[:], ["TABm"] + TABC, ["tabd"])
    selv = metat[:, 8:72].rearrange("p (e k) -> p e k", k=4)
    for k in range(4):
        TT("vector", t16[:], TAB[:, :, 63], selv[:, :, k], ALU.mult, TABC + ["metat"], ["t16"])
        RED("vector", n4[:, k:k + 1], t16[:], ALU.add, ["t16"], [("n4", k)])
    N4 = [("n4", k) for k in range(4)]
    TT("vector", rhsU[:], n4[:].unsqueeze(2).to_broadcast([128, 4, 128]), U_b[:].unsqueeze(1).to_broadcast([128, 4, 128]), ALU.mult,
       N4 + ["U_b"], ["rhsU"])
    TT("vector", rhsI[:], n4[:].unsqueeze(2).to_broadcast([128, 4, 128]), ident_b[:].unsqueeze(1).to_broadcast([128, 4, 128]), ALU.mult,
       N4 + ["ident_b"], ["rhsI"])
    ps, psn = psf("v")
    MM(ps[:], ones_b[:], rhsU[:].rearrange("p k q -> p (k q)"), True, True, ["rhsU", "ones_b"], [psn])
    CP("vector", offs_sb[:].rearrange("p k q -> p (k q)"), ps[:], [psn], ["offs_sb"])
    ps, psn = psf("v")
    MM(ps[:], ones_b[:], rhsI[:].rearrange("p k q -> p (k q)"), True, True, ["rhsI", "ones_b"], [psn])
    CP("vector", nrow_sb[:].rearrange("p k q -> p (k q)"), ps[:], [psn], ["nrow_sb"])
    P.op("gpsimd", lambda e: e.iota(sval[:], pattern=[[128, 8]], base=0, channel_multiplier=1,
                                    allow_small_or_imprecise_dtypes=True), (), ["sval"])
    P.op("gpsimd", lambda e: e.iota(koffs[:], pattern=[[128, 4], [0, 8]], base=0, channel_multiplier=0,
                                    allow_small_or_imprecise_dtypes=True), (), ["koffs"])
    P.op("gpsimd", lambda e: e.iota(ZIDXf[:], pattern=[[512, 4], [2048, 4], [128, 4]], base=0, channel_multiplier=1,
                                    allow_small_or_imprecise_dtypes=True), (), ["ZIDXf"])
    TS("vector", koffs[:], koffs[:], metat[:, 4:5], None, ALU.add, None, ["koffs", "metat"], ["koffs"])
    TS("vector", ZIDXf[:], ZIDXf[:], metat[:, 5:6], None, ALU.add, None, ["ZIDXf", "metat"], ["ZIDXf"])
    CP("vector", ZIDX[:], ZIDXf[:], ["ZIDXf"], ["ZIDX"])
    svb = sval[:].unsqueeze(1).to_broadcast([128, 4, 8])
    TT("vector", cmpP[:], offs_sb[:].unsqueeze(2).to_broadcast([128, 4, 8, 128]),
       svb.unsqueeze(3).to_broadcast([128, 4, 8, 128]), ALU.is_le, ["offs_sb", "sval"], ["cmpP"] + FENCE)
    RED("vector", pS[:], cmpP[:], ALU.add, ["cmpP"], ["pS"])
    TT("vector", cmpP[:], cmpP[:], nrow_sb[:].unsqueeze(2).to_broadcast([128, 4, 8, 128]), ALU.mult, ["cmpP", "nrow_sb"], ["cmpP"])
    RED("vector", oex[:], cmpP[:], ALU.add, ["cmpP"], ["oex"])
    TT("vector", rS[:], svb, oex[:], ALU.subtract, ["sval", "oex"], ["rS"])
    TT("vector", tSf[:], pS[:], koffs[:], ALU.add, ["pS", "koffs"], ["tSf"])
    CP("vector", RIDX[:], tSf[:], ["tSf"], ["RIDX"])
    for k in range(4):
        for c in range(8):
            P.dma("gpsimd", "d_G", (lambda k=k, c=c: (lambda e: e.indirect_dma_start(
                out=Gt[:, k, c, :], out_offset=None, in_=tabd, in_offset=bass.IndirectOffsetOnAxis(ap=RIDX[:, k, c:c + 1], axis=0))))(),
                ["tabd", "RIDX"], [("Gt", k, c), "cmpb"] if (k == 0 and c == 0) else [("Gt", k, c)])
    GALL = [("Gt", k, c) for k in range(4) for c in range(8)]
    cmpG = cmpP[:, :, :, 0:64]
    TT("vector", cmpG, Gt[:, :, :, 0:64], rS[:].unsqueeze(3).to_broadcast([128, 4, 8, 64]), ALU.is_le, GALL + ["rS"], ["cmpP"])
    RED("vector", jS[:], cmpG, ALU.add, ["cmpP"], ["jS"])
    TS("vector", oex[:], rS[:], 1.0, None, ALU.add, None, ["rS"], ["oex"])
    TT("vector", cmpG, Gt[:, :, :, 0:64], oex[:].unsqueeze(3).to_broadcast([128, 4, 8, 64]), ALU.is_equal, GALL + ["oex"], ["cmpP"])
    TT("vector", cmpG, cmpG, Gt[:, :, :, 64:128], ALU.mult, ["cmpP"] + GALL, ["cmpP"])
    RED("vector", gS[:], cmpG, ALU.add, ["cmpP"], ["gS"])
    TS("vector", tSf[:], pS[:], 64.0, None, ALU.mult, None, ["pS"], ["tSf"])
    TT("vector", tSf[:], tSf[:], jS[:], ALU.add, ["tSf", "jS"], ["tSf"])
    CP("vector", TIDX[:], tSf[:], ["tSf"], ["TIDX"])
    ra = P.sb("ra", [128, 4, 8], F32, off=ob2); rb = P.sb("rb", [128, 4, 8], F32, off=ob2 + 128)
    rj = P.sb("rj", [128, 4, 8], F32, off=ob2 + 256); GIDX = P.sb("GIDX", [128, 4, 8], I32, off=ob2 + 384)
    assert ob2 + 512 <= bgT_off
    TS("vector", ra[:], tSf[:], 2048.0, None, ALU.is_ge, None, ["tSf"], ["ra"])
    for thv in (4096.0, 6144.0):
        TS("vector", rb[:], tSf[:], thv, None, ALU.is_ge, None, ["tSf"], ["rb"])
        TT("vector", ra[:], ra[:], rb[:], ALU.add, ["ra", "rb"], ["ra"])
    TS("vector", rb[:], ra[:], -2048.0, None, ALU.mult, None, ["ra"], ["rb"])
    TT("vector", rb[:], rb[:], tSf[:], ALU.add, ["rb", "tSf"], ["rb"])
    TS("vector", rj[:], rb[:], 512.0, None, ALU.is_ge, None, ["rb"], ["rj"])
    for thv in (1024.0, 1536.0):
        TS("vector", oex[:], rb[:], thv, None, ALU.is_ge, None, ["rb"], ["oex"])
        TT("vector", rj[:], rj[:], oex[:], ALU.add, ["rj", "oex"], ["rj"])
    TT("vector", rj[:], rj[:], ra[:], ALU.subtract, ["rj", "ra"], ["rj"])
    TS("vector", rj[:], rj[:], 1536.0, None, ALU.mult, None, ["rj"], ["rj"])
    TT("vector", rj[:], rj[:], tSf[:], ALU.add, ["rj", "tSf"], ["rj"])
    CP("vector", GIDX[:], rj[:], ["rj"], ["GIDX"])

    if "idx" in dbg:
        d1 = dbg_out("tidx", [128, 32])
        d2 = dbg_out("gS", [128, 32])
        e1 = DMA("sync", "d_dbg1", d1, tSf[:].rearrange("p a b -> p (a b)"), ["tSf", "TIDX"], ["dbg1"])
        e2 = DMA("sync", "d_dbg2", d2, gS[:].rearrange("p a b -> p (a b)"), ["gS"], ["dbg2"])
        P.finish("sync", [e1, e2])
    if stage <= 6.5:
        P.emit()
        return nc, dbg_outs

    wslot = [P.sb(f"wslot{i}", [128, 8, 1024], BF16, off=REG0 + i * 16384) for i in range(4)]
    wdt_ = P.sb("wd_", [128, 8, 1024], BF16, off=REG0 + 81920)
    hid = [P.sb(f"hid{i}", [128, 8, 512], BF16, off=qT_off + i * 8192) for i in range(2)]
    XS = P.sb("XS", [128, 8, 1024], BF16, off=bgT_off)
    xsT = P.sb("xsT", [128, 8, 1024], BF16, off=bgT_off + 16384)
    sgs = P.sb("sgs", [128, 512], F32, off=rt1_off + 4096)
    MSET("vector", XS[:], 0.0, ["XS"] + FENCE)
    ZD0 = []
    for t in range(8):
        ZD0.append(("Zd0", t))
        DMA("sync", "d_z0", Zd[t * 1024:(t + 1) * 1024, :].rearrange("(p c) d -> p c d", c=8), XS[:], ["XS"], [("Zd0", t)])
    wgv = w_gate.rearrange("e (k p) n -> e p k n", p=128)
    wuv = w_up.rearrange("e (k p) n -> e p k n", p=128)
    wdv = w_down.rearrange("e (k p) n -> e p k n", p=128)
    yrr = [0]
    for k4 in range(4):
        sg_, su_ = (k4 % 2) * 2, (k4 % 2) * 2 + 1
        wg_t, wu_t = wslot[sg_], wslot[su_]
        ex2 = (["cmpP"] if sg_ == 2 else [])
        ex3 = (["cmpb"] + GALL if su_ == 3 else [])
        P.dma("gpsimd", f"d_ws{sg_}", (lambda wg_t=wg_t, k4=k4: (lambda e: e.dma_start(out=wg_t[:], in_=wgv[k4])))(), [], [f"ws{sg_}"] + ex2 + (FENCE if k4 < 2 else []))
        P.dma("gpsimd", f"d_ws{su_}", (lambda wu_t=wu_t, k4=k4: (lambda e: e.dma_start(out=wu_t[:], in_=wuv[k4])))(), [], [f"ws{su_}"] + ex3 + (FENCE if k4 < 2 else []))
        P.dma("gpsimd", "d_wd", (lambda k4=k4: (lambda e: e.dma_start(out=wdt_[:], in_=wdv[k4])))(), [], ["wd_"] + (FENCE if k4 < 1 else []))
        for c in range(8):
            P.dma("gpsimd", f"d_XS{c}", (lambda k4=k4, c=c: (lambda e: e.indirect_dma_start(
                out=XS[:, c, :], out_offset=None, in_=h2all, in_offset=bass.IndirectOffsetOnAxis(ap=GIDX[:, k4, c:c + 1], axis=0))))(),
                H2ALLD + ["GIDX"], [("XS", c)] + (["XS"] if c == 0 else []))
        for c in range(8):
            pb, pbn = psb()
            pbv = pb[:].rearrange("p (k t) -> p k t", k=8)
            for kc in range(8):
                TR(pbv[:, kc, :], XS[:, c, kc * 128:(kc + 1) * 128], ident_b[:], [("XS", c), "XS", "ident_b"], [(pbn, kc)])
            ACT(xsT[:, :, c * 128:(c + 1) * 128], pbv, AF.Copy, [(pbn, kc) for kc in range(8)], [("xsT", c)] + (FENCE if k4 == 0 else []))
        for sch in range(2):
            hd = hid[(k4 * 2 + sch) % 2]
            hdn = f"hid{(k4 * 2 + sch) % 2}"
            xdeps = [("xsT", sch * 4 + t) for t in range(4)]
            for fo in range(8):
                pa, pan = psf("a")
                for kc in range(8):
                    MM(pa[:], wg_t[:, kc, fo * 128:(fo + 1) * 128], xsT[:, kc, sch * 512:(sch + 1) * 512], kc == 0, kc == 7,
                       [f"ws{sg_}"] + xdeps, [pan])
                pu, pun = psf("v")
                for kc in range(8):
                    MM(pu[:], wu_t[:, kc, fo * 128:(fo + 1) * 128], xsT[:, kc, sch * 512:(sch + 1) * 512], kc == 0, kc == 7,
                       [f"ws{su_}"] + xdeps, [pun])
                ACT(sgs[:], pa[:], AF.Silu, [pan], ["sgs"] + (FENCE if k4 == 0 and sch == 0 and fo == 0 else []))
                TT("vector", hd[:, fo, :], pu[:], sgs[:], ALU.mult, [pun, "sgs"], [(hdn, fo)] + (FENCE + ["TABm"] + TABC if k4 == 0 else []))
            for t in range(4):
                c = sch * 4 + t
                yi = yrr[0] % 2
                yrr[0] += 1
                for dn in range(2):
                    py, pyn = psf("v")
                    for kc in range(8):
                        MM(py[:], hd[:, kc, t * 128:(t + 1) * 128], wdt_[:, kc, dn * 512:(dn + 1) * 512], kc == 0, kc == 7,
                           [(hdn, kc), "wd_"], [pyn])
                    TS("vector", yz[yi][:, dn * 512:(dn + 1) * 512], py[:], gS[:, k4, c:c + 1], None, ALU.mult, None,
                       [pyn, "gS"], [f"yz{yi}"])
                P.dma("gpsimd", f"d_sz{yi}", (lambda yi=yi, k4=k4, c=c: (lambda e: e.indirect_dma_start(
                    out=Zd, out_offset=bass.IndirectOffsetOnAxis(ap=TIDX[:, k4, c:c + 1], axis=0), in_=yz[yi][:], in_offset=None,
                    compute_op=ALU.add, oob_is_err=True)))(), [f"yz{yi}", "TIDX", "Zd"] + ZD0, ["Zd"])

    for j16 in range(16):
        P.dma("gpsimd", "d_ag_z", (lambda j16=j16: (lambda e: e.collective_compute(
            "AllGather", ALU.bypass, replica_groups=GROUPS,
            ins=[Zd[j16 * 512:(j16 + 1) * 512, :].opt()], outs=[Zall[j16 * 2048:(j16 + 1) * 2048, :].opt()])))(),
            ["Zd", "agchain"], [("Zall", j16), "agchain"], inc=1)
    ZALLD = [("Zall", j16) for j16 in range(16)]
    if stage <= 7:
        P.emit()
        return nc, dbg_outs

    gfrow = P.sb("gfrow", [128, 1024], F32, off=rows_off + 4096)
    DMA("sync", "d_gfrow", gfrow[:], g_final.partition_broadcast(128), [], ["gfrow"] + FENCE)
    z4 = [P.sb(f"z4_{i}", [128, 4, 1024], BF16, off=bgT_off + i * 8192) for i in range(2)]
    evs = []
    for tile in range(16):
        i = xt_rr[0] % 2
        xt_rr[0] += 1
        xtile, xn = xt[i], f"xt{i}"
        zi = tile % 2
        DMA("sync", "d_" + xn, xtile[:], x1d[tile * 128:(tile + 1) * 128, :], [("x1d", tile)], [xn])
        for r in range(4):
            P.dma("gpsimd", f"d_z4_{zi}_{r}", (lambda zi=zi, r=r, tile=tile: (lambda e: e.indirect_dma_start(
                out=z4[zi][:, r, :], out_offset=None, in_=Zall, in_offset=bass.IndirectOffsetOnAxis(ap=ZIDX[:, r, tile:tile + 1], axis=0))))(),
                ZALLD + ["ZIDX"], [(f"z4_{zi}", r)] + ([("XS", c) for c in range(8)] + ["XS"] if tile < 2 else []))
        zd = [(f"z4_{zi}", r) for r in range(4)]
        TT("vector", tf[:], z4[zi][:, 0, :], z4[zi][:, 1, :], ALU.add, zd, ["tf"])
        TT("vector", tf[:], tf[:], z4[zi][:, 2, :], ALU.add, zd + ["tf"], ["tf"])
        TT("vector", tf[:], tf[:], z4[zi][:, 3, :], ALU.add, zd + ["tf"], ["tf"])
        TT("vector", tf[:], tf[:], rows["GT2"][:], ALU.mult, ["tf"] + rowdeps("GT2"), ["tf"])
        TT("vector", xtile[:], xtile[:], tf[:], ALU.add, [xn, "tf"], [xn])
        ss = small[:, 16:17]
        rstd = small[:, 17:18]
        ACT(tf[:], xtile[:], AF.Square, [xn], ["tf"])
        RED("vector", ss, tf[:], ALU.add, ["tf"], ["ss"])
        TS("vector", rstd, ss, 1.0 / 1024.0, 1e-6, ALU.mult, ALU.add, ["ss"], ["rstd"])
        ACT(rstd, rstd, AF.Ln, ["rstd"], ["rstd"])
        ACT(rstd, rstd, AF.Exp, ["rstd"], ["rstd"], scale=-0.5)
        ACT(tf[:], xtile[:], AF.Copy, [xn, "rstd"], ["tf"], scale=rstd)
        TT("vector", xtile[:], tf[:], gfrow[:], ALU.mult, ["tf", "gfrow"], [xn])
        evs.append(DMA("sync", "d_out_" + xn, out[tile * 128:(tile + 1) * 128, :], xtile[:], [xn], [("out", tile)]))
    P.finish("sync", evs)
    P.emit()
    return nc, dbg_outs


def make_in_maps(inp):
    x = np.ascontiguousarray(inp["x"], dtype=np.float32)
    maps = []
    for c in range(NCORES):
        b, q = c // 4, c % 4
        t0 = q * 2048
        xw = np.zeros((W, 1024), np.float32)
        lo, hi = t0 - 128, t0 + 2048 + 128
        slo, shi = max(lo, 0), min(hi, 8192)
        xw[slo - lo:shi - lo] = x[b, slo:shi]
        ccv = np.stack([inp["c"][b].reshape(8, 128).T, inp["c_ctx"].reshape(8, 128).T], axis=-1)
        meta = np.zeros((128, 80), np.float32)
        meta[:, 0] = 1.0 if q > 0 else 0.0
        meta[:, 1] = 1.0 if q < 3 else 0.0
        meta[:, 2] = float(q * 32 - 2)
        meta[:, 3] = float(q * 2048)
        meta[:, 4] = float(4 * q * 128)
        meta[:, 5] = float(q * 8192)
        for k in range(4):
            meta[:, 8 + (4 * q + k) * 4 + k] = 1.0
        maps.append({
            "x": xw, "ctx": np.ascontiguousarray(inp["ctx"][b]), "cc": np.ascontiguousarray(ccv.reshape(128, 16)),
            "meta": meta, "w_ada": inp["w_ada"][0], "b_ada": inp["b_ada"][0], "g_mix": inp["g_mix"][0],
            "g_ffn": inp["g_ffn"][0], "g_final": inp["g_final"], "w_in": inp["w_in"][0], "conv_w": inp["conv_w"][0],
            "sink": inp["sink"][0], "w_out": inp["w_out"][0], "w_router": inp["w_router"][0],
            "w_gate": np.ascontiguousarray(inp["w_gate"][0, 4 * q:4 * q + 4]),
            "w_up": np.ascontiguousarray(inp["w_up"][0, 4 * q:4 * q + 4]),
            "w_down": np.ascontiguousarray(inp["w_down"][0, 4 * q:4 * q + 4]),
        })
    return maps


def kernel(**inputs):
    inp = {k: np.asarray(v) for k, v in inputs.items()}
    nc, _ = build_nc()
    res = run_bass_kernel_spmd(nc, make_in_maps(inp), core_ids=list(range(NCORES)))
    outp = np.zeros((2, 8192, 1024), np.float32)
    for c in range(NCORES):
        b, q = c // 4, c % 4
        outp[b, q * 2048:(q + 1) * 2048] = res.results[c]["out"]
    return outp
```

```python
import os
import numpy as np
import concourse.bass as bass
import concourse.mybir as mybir
from concourse.bass_utils import run_bass_kernel_spmd

F32 = mybir.dt.float32
BF16 = mybir.dt.bfloat16
I32 = mybir.dt.int32
ALU = mybir.AluOpType
AF = mybir.ActivationFunctionType
AX = mybir.AxisListType

COMPUTE = ("tensor", "vector", "scalar", "gpsimd")
QUEUES = ("sync",)
NCORES = 8
GROUPS = [[0, 1, 2, 3], [4, 5, 6, 7]]
W = 2304
NT = 16
NIT = 7


class Prog:
    def __init__(self, nc):
        self.nc = nc
        self.streams = {e: [] for e in COMPUTE + QUEUES}
        self.cnt = {e: 0 for e in COMPUTE}
        self.dma_cnt = {}
        self.waited = {}
        self.res = {}
        self.sem_handles = {}
        self.final_events = []
        self.sb_off = 16512
        self.sb_top = 229344

    def sb(self, name, shape, dtype, off=None):
        esz = {F32: 4, BF16: 2, I32: 4}[dtype]
        n = 1
        for s in shape[1:]:
            n *= s
        nbytes = (n * esz + 63) // 64 * 64
        if off is None:
            off = self.sb_off
            self.sb_off += nbytes
        assert off >= 16512 and off + nbytes <= self.sb_top, (name, off, nbytes)
        return self.nc.alloc_sbuf_tensor_at(name, list(shape), dtype, offset=off)

    def _deps(self, reads, writes):
        need = []
        for r in reads:
            st = self.res.get(r)
            if st and st["w"] is not None:
                need.append(st["w"])
        for w in writes:
            st = self.res.get(w)
            if st:
                if st["w"] is not None:
                    need.append(st["w"])
                need.extend(st["r"])
        return need

    def _commit(self, ev, reads, writes):
        for r in reads:
            st = self.res.setdefault(r, {"w": None, "r": []})
            st["r"].append(ev)
        for w in writes:
            self.res[w] = {"w": ev, "r": []}

    def _waits(self, eng, need):
        best = {}
        for (k, v) in need:
            if k == "tensor" and eng == "tensor":
                continue
            if v > best.get(k, 0):
                best[k] = v
        out = []
        for k, v in best.items():
            if self.waited.get((eng, k), 0) >= v:
                continue
            self.waited[(eng, k)] = v
            out.append((k, v))
        return out

    def op(self, eng, fn, reads=(), writes=()):
        need = self._deps(reads, writes)
        waits = self._waits(eng, need)
        self.cnt[eng] += 1
        ev = (eng, self.cnt[eng])
        self.streams[eng].append((waits, fn, (eng, 1)))
        self._commit(ev, reads, writes)
        return ev

    def dma(self, q, sem, fn, reads=(), writes=(), inc=16):
        need = self._deps(reads, writes)
        waits = self._waits(q, need)
        self.dma_cnt[sem] = self.dma_cnt.get(sem, 0) + inc
        ev = (sem, self.dma_cnt[sem])
        self.streams[q].append((waits, fn, (sem, inc)))
        self._commit(ev, reads, writes)
        return ev

    def finish(self, eng, events):
        self.final_events.append((eng, events))

    def check_deadlock(self):
        sem = {}
        pos = {e: 0 for e in self.streams}
        progressed = True
        while progressed:
            progressed = False
            for e, st in self.streams.items():
                while pos[e] < len(st):
                    waits, fn, inc = st[pos[e]]
                    if all(sem.get(k, 0) >= v for (k, v) in waits):
                        sem[inc[0]] = sem.get(inc[0], 0) + inc[1]
                        pos[e] += 1
                        progressed = True
                    else:
                        break
        stuck = {e: (pos[e], len(st), st[pos[e]][0]) for e, st in self.streams.items() if pos[e] < len(st)}
        assert not stuck, ("DEADLOCK", stuck, {k: sem.get(k) for e in stuck for (k, v) in stuck[e][2]})

    def emit(self):
        self.check_deadlock()
        nc = self.nc
        names = set(COMPUTE)
        for e in self.streams:
            for (waits, fn, inc) in self.streams[e]:
                names.add(inc[0])
                for (k, v) in waits:
                    names.add(k)
        for n in sorted(names):
            self.sem_handles[n] = nc.alloc_semaphore("s_" + n)
        H = self.sem_handles
        fin = {}
        for eng, evs in self.final_events:
            fin.setdefault(eng, []).extend(evs)
        with nc.Block() as block:
            def make(ename):
                def body(e):
                    for (waits, fn, inc) in self.streams[ename]:
                        for (k, v) in waits:
                            e.wait_ge(H[k], v)
                        fn(e).then_inc(H[inc[0]], inc[1])
                    best = {}
                    for (k, v) in fin.get(ename, []):
                        best[k] = max(best.get(k, 0), v)
                    for k, v in best.items():
                        e.wait_ge(H[k], v)
                return body
            for ename in self.streams:
                if not self.streams[ename] and ename not in fin:
                    continue
                getattr(block, ename)(make(ename))


def build_nc(stage=99, dbg=()):
    nc = bass.Bass("TRN2", target_bir_lowering=False)
    P = Prog(nc)
    dbg_outs = {}

    def din(name, shape, dt=F32):
        return nc.dram_tensor(name, list(shape), dt, kind="ExternalInput").ap()

    x = din("x", [W, 1024])
    ctx = din("ctx", [256, 1024])
    cc = din("cc", [128, 16])
    meta = din("meta", [128, 80])
    w_ada = din("w_ada", [1024, 6144])
    b_ada = din("b_ada", [6144])
    g_mix = din("g_mix", [1024])
    g_ffn = din("g_ffn", [1024])
    g_final = din("g_final", [1024])
    w_in = din("w_in", [1024, 2304])
    conv_w = din("conv_w", [3, 512])
    sink = din("sink", [8])
    w_out = din("w_out", [1024, 1024])
    w_router = din("w_router", [1024, 16])
    w_gate = din("w_gate", [4, 1024, 1024])
    w_up = din("w_up", [4, 1024, 1024])
    w_down = din("w_down", [4, 1024, 1024])
    out = nc.dram_tensor("out", [2048, 1024], F32, kind="ExternalOutput").ap()

    x1d = nc.dram_tensor("x1d", [2048, 1024], F32).ap()
    h2loc = nc.dram_tensor("h2loc", [2048, 1024], BF16).ap()
    h2all = nc.dram_tensor("h2all", [8192, 1024], BF16).ap()
    affloc = nc.dram_tensor("affloc", [16, 2048], F32).ap()
    affall = nc.dram_tensor("affall", [64, 2048], F32).ap()
    tabd = nc.dram_tensor("tabd", [2048, 128], F32).ap()
    Zd = nc.dram_tensor("Zd", [8192, 1024], BF16).ap()
    Zall = nc.dram_tensor("Zall", [32768, 1024], BF16).ap()

    def dbg_out(name, shape, dt=F32):
        t = nc.dram_tensor("dbg_" + name, list(shape), dt, kind="ExternalOutput").ap()
        dbg_outs[name] = t
        return t

    def ACT(out_, in_, func, r, w, **kw):
        return P.op("scalar", lambda e: e.activation(out=out_, in_=in_, func=func, **kw), r, w)

    def TT(eng, out_, in0, in1, op, r, w):
        return P.op(eng, lambda e: e.tensor_tensor(out=out_, in0=in0, in1=in1, op=op), r, w)

    def TS(eng, out_, in0, s1, s2, op0, op1, r, w):
        if op1 is None:
            return P.op(eng, lambda e: e.tensor_scalar(out=out_, in0=in0, scalar1=s1, scalar2=None, op0=op0), r, w)
        return P.op(eng, lambda e: e.tensor_scalar(out=out_, in0=in0, scalar1=s1, scalar2=s2, op0=op0, op1=op1), r, w)

    def STT(eng, out_, in0, scalar, in1, op0, op1, r, w):
        return P.op(eng, lambda e: e.scalar_tensor_tensor(out=out_, in0=in0, scalar=scalar, in1=in1, op0=op0, op1=op1), r, w)

    def RED(eng, out_, in_, op, r, w):
        return P.op(eng, lambda e: e.tensor_reduce(out=out_, in_=in_, axis=AX.X, op=op), r, w)

    def CP(eng, out_, in_, r, w):
        return P.op(eng, lambda e: e.tensor_copy(out=out_, in_=in_), r, w)

    def MSET(eng, out_, val, w):
        return P.op(eng, lambda e: e.memset(out_, val), (), w)

    def MM(out_, lhsT, rhs, start, stop, r, w):
        return P.op("tensor", lambda e: e.matmul(out_, lhsT, rhs, start=start, stop=stop), r, w)

    def TR(out_, in_, ident, r, w):
        return P.op("tensor", lambda e: e.transpose(out_, in_, ident), r, w)

    def DMA(q, sem, out_, in_, r, w):
        return P.dma(q, sem, lambda e: e.dma_start(out=out_, in_=in_), r, w)

    PSF = [nc.alloc_psum_tensor(f"psf{i}", [128, 512], F32) for i in range(6)]
    PSB = [nc.alloc_psum_tensor(f"psb{i}", [128, 1024], BF16) for i in range(2)]
    psf_rr = {"v": 0, "a": 0}

    def psf(cons):
        i = psf_rr[cons] % 3 + (0 if cons == "v" else 3)
        psf_rr[cons] += 1
        return PSF[i], f"psf{i}"

    psb_rr = [0]

    def psb():
        i = psb_rr[0] % 2
        psb_rr[0] += 1
        return PSB[i], f"psb{i}"

    ident_f = P.sb("ident_f", [128, 128], F32)
    ident_b = P.sb("ident_b", [128, 128], BF16)
    iot = P.sb("iot", [128, 128], F32)
    ones_b = P.sb("ones_b", [128, 128], BF16)
    U_b = P.sb("U_b", [128, 128], BF16)
    UI_b = P.sb("UI_b", [128, 128], BF16)
    mask3 = P.sb("mask3", [128, 3, 384], BF16)
    metat = P.sb("metat", [128, 80], F32)
    esink = P.sb("esink", [128, 8], F32)
    rows = {}
    for nm in ("S1", "G1", "GT1", "S2", "G2", "GT2", "cS1", "cG1"):
        rows[nm] = P.sb("row_" + nm, [128, 1024], F32)
    REG0 = P.sb_off

    P.op("gpsimd", lambda e: e.iota(iot[:], pattern=[[1, 128]], base=0, channel_multiplier=-1,
                                    allow_small_or_imprecise_dtypes=True), (), ["iot"])
    TS("vector", ident_f[:], iot[:], 0.0, None, ALU.is_equal, None, ["iot"], ["ident_f"])
    CP("vector", ident_b[:], ident_f[:], ["ident_f"], ["ident_b"])
    TS("vector", U_b[:], iot[:], 0.0, None, ALU.is_ge, None, ["iot"], ["U_b"])
    MSET("vector", ones_b[:], 1.0, ["ones_b"])
    DMA("sync", "d_meta", metat[:], meta, [], ["metat"])
    DMA("sync", "d_sink", esink[:], sink.partition_broadcast(128), [], ["esink"])
    ACT(esink[:], esink[:], AF.Exp, ["esink"], ["esink"])
    for v in range(3):
        TS("vector", mask3[:, v, 0:128], iot[:], 0.0, None, ALU.is_le, None, ["iot"], [("mask3", v)])
        MSET("vector", mask3[:, v, 128:256], 1.0, [("mask3", v, 1)])
        TS("vector", mask3[:, v, 256:384], iot[:], 0.0, None, ALU.is_ge, None, ["iot"], [("mask3", v, 2)])
    TS("vector", mask3[:, 1, 0:128], mask3[:, 1, 0:128], metat[:, 0:1], None, ALU.mult, None,
       ["metat", ("mask3", 1)], [("mask3", 1)])
    TS("vector", mask3[:, 2, 256:384], mask3[:, 2, 256:384], metat[:, 1:2], None, ALU.mult, None,
       ["metat", ("mask3", 2, 2)], [("mask3", 2, 2)])

    if "const" in dbg:
        d1 = dbg_out("ident", [128, 128])
        d2 = dbg_out("mask3", [128, 3 * 384], BF16)
        d3 = dbg_out("esink", [128, 8])
        e1 = DMA("sync", "d_dbg", d1, ident_f[:], ["ident_f"], ["dbg1"])
        e2 = DMA("sync", "d_dbg", d2, mask3[:].rearrange("p a b -> p (a b)"),
                 [("mask3", v) for v in range(3)] + [("mask3", v, 1) for v in range(3)] + [("mask3", v, 2) for v in range(3)], ["dbg2"])
        e3 = DMA("sync", "d_dbg", d3, esink[:], ["esink"], ["dbg3"])
        P.finish("sync", [e1, e2, e3])
    if stage <= 0:
        P.emit()
        return nc, dbg_outs

    o = REG0
    WIN = P.sb("WIN", [128, 8, 2944], BF16, off=o)
    mixT = P.sb("mixT", [128, 8, 2048], BF16, off=o)
    o += 47104
    COS = P.sb("COS", [128, W], F32, off=o); o += W * 4
    SINS = P.sb("SINS", [128, W], F32, off=o); o += W * 4
    xt = [P.sb(f"xt{i}", [128, 1024], F32, off=o + i * 4096) for i in range(2)]; o += 8192
    tf = P.sb("tf", [128, 1024], F32, off=o); o += 4096
    hb = [P.sb(f"hb{i}", [128, 1024], BF16, off=o + i * 2048) for i in range(2)]; o += 4096
    hT = [P.sb(f"hT{i}", [128, 8, 512], BF16, off=o + i * 8192) for i in range(2)]
    wo = P.sb("wo", [128, 8, 1024], BF16, off=o)
    o += 16384
    qT_off = o
    qT = P.sb("qT", [128, 4, W], BF16, off=o); o += 4 * W * 2
    kT_off = o
    kT = P.sb("kT", [128, W], BF16, off=o); o += W * 2
    Vt = P.sb("Vt", [128, 18, 2, 65], BF16, off=o); o += 4736
    kcT = P.sb("kcT", [128, 256], BF16, off=o); o += 512
    Vc = P.sb("Vc", [128, 2, 2, 65], BF16, off=o); o += 576
    bgT_off = o
    bgT = P.sb("bgT", [128, 4, 2048], BF16, off=o)
    stg = P.sb("stg", [128, 8, 640], F32, off=o)
    o += 20480
    uT_off = o
    uT = P.sb("uT", [128, 4, W], BF16, off=o); o += 4 * W * 2
    rt1_off = o
    rt1 = P.sb("rt1", [128, 512], F32, off=o); o += 2048
    rt2 = P.sb("rt2", [128, 512], F32, off=o); o += 2048
    cgs = P.sb("cgs", [128, 512], F32, off=o); o += 2048
    small = P.sb("small", [128, 64], F32, off=o); o += 256
    cw = P.sb("cw", [128, 4, 3], F32, off=o); o += 64
    assert o <= P.sb_top, o
    A_END = o

    wa = [P.sb("wa0", [128, 8, 1024], BF16, off=qT_off), P.sb("wa1", [128, 8, 1024], BF16, off=uT_off)]
    o = kT_off
    lb = P.sb("lb", [128, 8, 2, 128], BF16, off=o); o += 4096
    brow = P.sb("brow", [128, 1024], F32, off=o); o += 4096
    cct = P.sb("cct", [128, 8, 2], F32, off=o); o += 64
    scl = P.sb("scl", [128, 8, 2], F32, off=o); o += 64
    assert o <= bgT_off
    gmrow = P.sb("gmrow", [128, 1024], F32, off=rt1_off)

    DMA("sync", "d_cc", cct[:], cc.rearrange("p (k v) -> p k v", v=2), [], ["cct"])
    ACT(scl[:], cct[:], AF.Silu, ["cct"], ["scl"])
    for v in range(2):
        CP("vector", lb[:, :, v, :], scl[:, :, v:v + 1].to_broadcast([128, 8, 128]), ["scl"], [("lb", v)])
    if stage <= 0.3:
        d1 = dbg_out("lb", [128, 8 * 2 * 128], BF16)
        e1 = DMA("sync", "d_dbg", d1, lb[:].rearrange("p a b c -> p (a b c)"), [("lb", 0), ("lb", 1)], ["dbg1"])
        P.finish("sync", [e1])
        P.emit()
        return nc, dbg_outs
    w_ada_v = w_ada.rearrange("(k p) n -> p k n", p=128)
    grp = [(0, [("S1", 0), ("cS1", 1)]), (1, [("G1", 0), ("cG1", 1)]), (2, [("GT1", 0)]),
           (3, [("S2", 0)]), (4, [("G2", 0)]), (5, [("GT2", 0)])]
    for gi, (g, uses) in enumerate(grp):
        wb = wa[gi % 2]
        wn = f"wa{gi % 2}"
        P.dma("gpsimd", "d_" + wn, (lambda wb=wb, g=g: (lambda e: e.dma_start(out=wb[:], in_=w_ada_v[:, :, g * 1024:(g + 1) * 1024])))(),
              [], [wn])
        DMA("sync", "d_brow", brow[:], b_ada[g * 1024:(g + 1) * 1024].partition_broadcast(128), [], ["brow"])
        if stage <= 0.5:
            d1 = dbg_out("wa", [128, 8 * 1024], BF16)
            d2 = dbg_out("brow", [128, 1024])
            e1 = DMA("sync", "d_dbg", d1, wb[:].rearrange("p a b -> p (a b)"), [wn], ["dbg1"])
            e2 = DMA("sync", "d_dbg", d2, brow[:], ["brow"], ["dbg2"])
            P.finish("sync", [e1, e2])
            P.emit()
            return nc, dbg_outs
        for (nm, v) in uses:
            for n in range(2):
                ps, psn = psf("v")
                for k in range(8):
                    MM(ps[:], lb[:, k, v, :], wb[:, k, n * 512:(n + 1) * 512], k == 0, k == 7,
                       [("lb", v), wn], [psn])
                TT("vector", rows[nm][:, n * 512:(n + 1) * 512], ps[:], brow[:, n * 512:(n + 1) * 512], ALU.add,
                   [psn, "brow"], [("row", nm, n)])
                if stage <= 0.7:
                    d1 = dbg_out("r0", [128, 512])
                    e1 = DMA("sync", "d_dbg", d1, rows[nm][:, 0:512], [("row", nm, n)], ["dbg1"])
                    P.finish("sync", [e1])
                    P.emit()
                    return nc, dbg_outs
    for (gsrc, names) in (((g_mix, ("G1", "cG1")), (g_ffn, ("G2",))) if stage > 0.8 else ()):
        DMA("sync", "d_gmrow", gmrow[:], gsrc.partition_broadcast(128), [], ["gmrow"])
        for nm in names:
            TS("vector", rows[nm][:], rows[nm][:], 1.0, None, ALU.add, None,
               [("row", nm, 0), ("row", nm, 1)], [("row", nm, 0), ("row", nm, 1)])
            TT("vector", rows[nm][:], rows[nm][:], gmrow[:], ALU.mult,
               [("row", nm, 0), ("row", nm, 1), "gmrow"], [("row", nm, 0), ("row", nm, 1)])

    def rowdeps(nm):
        return [("row", nm, 0), ("row", nm, 1)]

    if "rows" in dbg:
        d = dbg_out("rows", [8, 128, 1024])
        for i, nm in enumerate(("S1", "G1", "GT1", "S2", "G2", "GT2", "cS1", "cG1")):
            ev = DMA("sync", "d_dbg", d[i], rows[nm][:], rowdeps(nm), ["dbg"])
        P.finish("sync", [ev])
    if stage <= 1:
        P.emit()
        return nc, dbg_outs


    def sc(i):
        return small[:, i:i + 1]
    pid, dd, i32_, isC, ff, inv, invC, invR, sgn, tmpc = [sc(i) for i in range(10)]
    P.op("gpsimd", lambda e: e.iota(small[:, 0:1], pattern=[[0, 1]], base=0, channel_multiplier=1,
                                    allow_small_or_imprecise_dtypes=True), (), ["small"])
    TS("vector", tmpc, pid, 64.0, -64.0, ALU.is_ge, ALU.mult, ["small"], ["small"])
    TT("vector", dd, pid, tmpc, ALU.add, ["small"], ["small"])
    TS("vector", sgn, dd, 32.0, None, ALU.is_ge, None, ["small"], ["small"])
    TS("vector", tmpc, sgn, -32.0, None, ALU.mult, None, ["small"], ["small"])
    TT("vector", i32_, dd, tmpc, ALU.add, ["small"], ["small"])
    TS("vector", isC, i32_, 16.0, None, ALU.is_ge, None, ["small"], ["small"])
    TS("vector", tmpc, isC, -16.0, None, ALU.mult, None, ["small"], ["small"])
    TT("vector", ff, i32_, tmpc, ALU.add, ["small"], ["small"])
    ACT(inv, ff, AF.Exp, ["small"], ["small"], scale=-float(np.log(10000.0) / 16.0))
    TT("vector", invC, inv, isC, ALU.mult, ["small"], ["small"])
    TT("vector", invR, inv, invC, ALU.subtract, ["small"], ["small"])
    TS("vector", sgn, sgn, 2.0, -1.0, ALU.mult, ALU.add, ["small"], ["small"])
    rrA = P.sb("rrA", [128, W], F32, off=qT_off)
    rrI = P.sb("rrI", [128, W], I32, off=qT_off + W * 4)
    ang = P.sb("ang", [128, W], F32, off=uT_off)
    P.op("gpsimd", lambda e: e.iota(COS[:], pattern=[[1, 36], [0, 64]], base=0, channel_multiplier=0,
                                    allow_small_or_imprecise_dtypes=True), (), ["COS"])
    P.op("gpsimd", lambda e: e.iota(SINS[:], pattern=[[0, 36], [1, 64]], base=0, channel_multiplier=0,
                                    allow_small_or_imprecise_dtypes=True), (), ["SINS"])
    HW_ = W // 2
    TWO_PI = float(2 * np.pi)
    for hh in range(2):
        sl = slice(hh * HW_, (hh + 1) * HW_)
        TS("vector", COS[:, sl], COS[:, sl], metat[:, 2:3], None, ALU.add, None, ["COS", "metat"], ["COS"])
        TS("vector", COS[:, sl], COS[:, sl], invR, None, ALU.mult, None, ["COS", "small"], ["COS"])
        TS("vector", SINS[:, sl], SINS[:, sl], invC, None, ALU.mult, None, ["SINS", "small"], ["SINS"])
    TT("vector", ang[:], COS[:], SINS[:], ALU.add, ["COS", "SINS"], ["ang"])

    def range_reduce_sin(dst, dstn, offset):
        TS("vector", rrA[:], ang[:], 1.0 / TWO_PI, offset / TWO_PI + 8.5, ALU.mult, ALU.add, ["ang"], ["rrA"])
        CP("vector", rrI[:], rrA[:], ["rrA"], ["rrI"])
        CP("vector", rrA[:], rrI[:], ["rrI"], ["rrA"])
        TS("vector", rrA[:], rrA[:], -TWO_PI, 8 * TWO_PI + offset, ALU.mult, ALU.add, ["rrA"], ["rrA"])
        TT("vector", dst[:], ang[:], rrA[:], ALU.add, ["ang", "rrA"], [dstn])
        TS("vector", rrA[:], dst[:], float(np.pi), -TWO_PI, ALU.is_gt, ALU.mult, [dstn], ["rrA"])
        TT("vector", dst[:], dst[:], rrA[:], ALU.add, [dstn, "rrA"], [dstn])
        TS("vector", rrA[:], dst[:], -float(np.pi), TWO_PI, ALU.is_lt, ALU.mult, [dstn], ["rrA"])
        TT("vector", dst[:], dst[:], rrA[:], ALU.add, [dstn, "rrA"], [dstn])
        ACT(dst[:], dst[:], AF.Sin, [dstn], [dstn])

    range_reduce_sin(SINS, "SINS", 0.0)
    range_reduce_sin(COS, "COS", float(np.pi / 2))
    for hh in range(2):
        sl = slice(hh * HW_, (hh + 1) * HW_)
        TS("vector", SINS[:, sl], SINS[:, sl], sgn, None, ALU.mult, None, ["SINS", "small"], ["SINS"])

    w_in_v = w_in.rearrange("(k p) n -> p k n", p=128)
    DMA("sync", "d_stg", stg[:], w_in_v[:, :, 0:640], [], ["stg"])
    qd = WIN[:, :, 0:512].rearrange("p k (c h d) -> p k c h d", c=4, h=2, d=64)
    qs = stg[:, :, 0:512].rearrange("p k (h c d) -> p k c h d", h=2, c=4, d=64)
    for h in range(2):
        ACT(qd[:, :, :, h, :], qs[:, :, :, h, :], AF.Copy, ["stg"], [("WIN", "q", h)])
    qd2 = WIN[:, :, 512:1024].rearrange("p k (c h s d) -> p k c h s d", c=4, h=2, s=2, d=32)
    qs2 = stg[:, :, 0:512].rearrange("p k (h c s d) -> p k c h s d", h=2, c=4, s=2, d=32)
    for h in range(2):
        for s in range(2):
            ACT(qd2[:, :, :, h, s, :], qs2[:, :, :, h, 1 - s, :], AF.Copy, ["stg"], [("WIN", "qsw", h, s)])
    ACT(WIN[:, :, 1024:1152], stg[:, :, 512:640], AF.Copy, ["stg"], [("WIN", "k")])
    kd2 = WIN[:, :, 1152:1280].rearrange("p k (h s d) -> p k h s d", h=2, s=2, d=32)
    ks2 = stg[:, :, 512:640].rearrange("p k (h s d) -> p k h s d", h=2, s=2, d=32)
    for s in range(2):
        ACT(kd2[:, :, :, s, :], ks2[:, :, :, 1 - s, :], AF.Copy, ["stg"], [("WIN", "ksw", s)])
    WINQ = [("WIN", "q", 0), ("WIN", "q", 1)]
    WINQS = [("WIN", "qsw", h, s) for h in range(2) for s in range(2)]
    WINK = [("WIN", "k")]
    WINKS = [("WIN", "ksw", 0), ("WIN", "ksw", 1)]
    for (nm, d0, s0, n) in (("v", 1280, 640, 128), ("bg", 1408, 768, 512), ("cg", 1920, 1280, 512), ("hv", 2432, 1792, 512)):
        P.dma("gpsimd", "d_win_" + nm, (lambda d0=d0, s0=s0, n=n: (lambda e: e.dma_start(out=WIN[:, :, d0:d0 + n], in_=w_in_v[:, :, s0:s0 + n])))(),
              [], [("WIN", nm)])
    for kk in range(3):
        for c4 in range(4):
            P.dma("sync", "d_cw", (lambda kk=kk, c4=c4: (lambda e: e.dma_start(
                out=cw[:, c4, kk:kk + 1], in_=conv_w[kk, c4 * 128:(c4 + 1) * 128].rearrange("(p o) -> p o", o=1))))(),
                [], [("cw", kk, c4)])
    MSET("vector", Vt[:, :, :, 64:65], 1.0, [("Vt", "ones")])
    MSET("vector", Vc[:, :, :, 64:65], 1.0, [("Vc", "ones")])

    xt_rr = [0]

    def norm_mod(src_rows, Gn, Sn, hbuf, hname, extra_r=()):
        i = xt_rr[0] % 2
        xt_rr[0] += 1
        xtile, xn = xt[i], f"xt{i}"
        DMA("sync", "d_" + xn, xtile[:], src_rows, list(extra_r), [xn])
        norm_mod_sb(xtile, xn, Gn, Sn, hbuf, hname)
        return xtile, xn

    tf2 = P.sb("tf2", [128, 1024], F32, off=bgT_off + 16384)
    nm_rr = [0]

    def norm_mod_sb(xtile, xn, Gn, Sn, hbuf, hname):
        pi = nm_rr[0] % 2
        nm_rr[0] += 1
        tfx, tfn = (tf, "tf") if pi == 0 else (tf2, "tf2")
        ss = small[:, 16 + 2 * pi:17 + 2 * pi]
        rstd = small[:, 17 + 2 * pi:18 + 2 * pi]
        ssn, rsn = f"ss{pi}", f"rstd{pi}"
        ACT(tfx[:], xtile[:], AF.Square, [xn], [tfn])
        RED("vector", ss, tfx[:], ALU.add, [tfn], [ssn])
        TS("vector", rstd, ss, 1.0 / 1024.0, 1e-6, ALU.mult, ALU.add, [ssn], [rsn])
        ACT(rstd, rstd, AF.Ln, [rsn], [rsn])
        ACT(rstd, rstd, AF.Exp, [rsn], [rsn], scale=-0.5)
        ACT(tfx[:], xtile[:], AF.Copy, [xn, rsn], [tfn], scale=rstd)
        TT("vector", tfx[:], tfx[:], rows[Gn][:], ALU.mult, [tfn] + rowdeps(Gn), [tfn])
        TT("vector", hbuf[:], tfx[:], rows[Sn][:], ALU.add, [tfn] + rowdeps(Sn), [hname])

    def transpose_to(hbuf, hname, dst, dst_name):
        pb, pbn = psb()
        pbv = pb[:].rearrange("p (k t) -> p k t", k=8)
        for k in range(8):
            TR(pbv[:, k, :], hbuf[:, k * 128:(k + 1) * 128], ident_b[:], [hname, "ident_b"], [(pbn, k)])
        ACT(dst, pbv, AF.Copy, [(pbn, k) for k in range(8)], [dst_name])

    hcT = hT[0]
    for t in range(2):
        norm_mod(ctx[t * 128:(t + 1) * 128, :], "cG1", "cS1", hb[t % 2], f"hb{t % 2}")
        transpose_to(hb[t % 2], f"hb{t % 2}", hcT[:, :, t * 128:(t + 1) * 128], ("hT0", t))
    ps, psn = psf("a")
    for k in range(8):
        MM(ps[:, 0:256], WIN[:, k, 1024:1152], hcT[:, k, 0:256], k == 0, k == 7,
           WINK + [("hT0", 0), ("hT0", 1)], [psn])
    ACT(kcT[:], ps[:, 0:256], AF.Copy, [psn], ["kcT"])
    for t in range(2):
        ps, psn = psf("a")
        for k in range(8):
            MM(ps[:, 0:128], hcT[:, k, t * 128:(t + 1) * 128], WIN[:, k, 1280:1408], k == 0, k == 7,
               [("WIN", "v"), ("hT0", t)], [psn])
        ACT(Vc[:, t, :, 0:64], ps[:, 0:128].rearrange("p (h d) -> p h d", h=2), AF.Copy, [psn], [("Vc", t)])

    if "ctx" in dbg:
        d1 = dbg_out("kcT", [128, 256], BF16)
        d2 = dbg_out("Vc", [128, 2 * 2 * 65], BF16)
        e1 = DMA("sync", "d_dbg", d1, kcT[:], ["kcT"], ["dbg1"])
        e2 = DMA("sync", "d_dbg", d2, Vc[:].rearrange("p a b c -> p (a b c)"), [("Vc", 0), ("Vc", 1), ("Vc", "ones")], ["dbg2"])
        P.finish("sync", [e1, e2])
    if stage <= 2:
        P.emit()
        return nc, dbg_outs

    chunks = [(0, 128, False)] + [(128 + 512 * i, 512, True) for i in range(4)] + [(2176, 128, False)]
    NCONV = int(os.environ.get("MK_NCONV", "4"))
    for ci, (w0, n, central) in enumerate(chunks):
        if (stage <= 2.5 and ci >= 1) or (stage <= 2.7 and ci >= 2):
            break
        hTc, hTn = hT[ci % 2], f"hT{ci % 2}"
        ntile = n // 128
        for t in range(ntile):
            j = (ci * 4 + t) % 2
            norm_mod(x[w0 + t * 128:w0 + (t + 1) * 128, :], "G1", "S1", hb[j], f"hb{j}")
            transpose_to(hb[j], f"hb{j}", hTc[:, :, t * 128:(t + 1) * 128], (hTn, t))
        hdeps = [(hTn, t) for t in range(ntile)]

        def proj(col0, wdeps, cons):
            ps, psn = psf(cons)
            for k in range(8):
                MM(ps[:, 0:n], WIN[:, k, col0:col0 + 128], hTc[:, k, 0:n], k == 0, k == 7, wdeps + hdeps, [psn])
            return ps, psn

        def rope_out(col0, colsw, wd, wsd, dst, dstn):
            pa, pan = proj(col0, wd, "v")
            pb_, pbn_ = proj(colsw, wsd, "v")
            TT("vector", rt1[:, 0:n], pa[:, 0:n], COS[:, w0:w0 + n], ALU.mult, [pan, "COS"], ["rt1"])
            TT("vector", rt2[:, 0:n], pb_[:, 0:n], SINS[:, w0:w0 + n], ALU.mult, [pbn_, "SINS"], ["rt2"])
            TT("vector", dst, rt1[:, 0:n], rt2[:, 0:n], ALU.add, ["rt1", "rt2"], [dstn])

        if central:
            for c in range(4):
                rope_out(c * 128, 512 + c * 128, WINQ, WINQS, qT[:, c, w0:w0 + n], ("qT", c, ci))
        PARTS = os.environ.get("MK_PARTS", "rvc")
        if "r" in PARTS:
            rope_out(1024, 1152, WINK, WINKS, kT[:, w0:w0 + n], ("kT", ci))
        for t in (range(ntile) if "v" in PARTS else ()):
            ps, psn = psf("a")
            for k in range(8):
                MM(ps[:, 0:128], hTc[:, k, t * 128:(t + 1) * 128], WIN[:, k, 1280:1408], k == 0, k == 7,
                   [("WIN", "v"), (hTn, t)], [psn])
            wt = w0 // 128 + t
            ACT(Vt[:, wt, :, 0:64], ps[:, 0:128].rearrange("p (h d) -> p h d", h=2), AF.Copy, [psn], [("Vt", wt)])
        for c in (range(NCONV) if "c" in PARTS else ()):
            if central:
                ps, psn = proj(1408 + c * 128, [("WIN", "bg")], "a")
                ACT(bgT[:, c, w0 - 128:w0 - 128 + n], ps[:, 0:n], AF.Copy, [psn], [("bgT", c, ci)])
            pc, pcn = proj(1920 + c * 128, [("WIN", "cg")], "a")
            ph, phn = proj(2432 + c * 128, [("WIN", "hv")], "v")
            ACT(cgs[:, 0:n], pc[:, 0:n], AF.Copy, [pcn], ["cgs"])
            TT("vector", uT[:, c, w0:w0 + n], ph[:, 0:n], cgs[:, 0:n], ALU.mult, [phn, "cgs"], [("uT", c, ci)])

    if "proj" in dbg:
        d1 = dbg_out("qT", [128, 4 * W], BF16)
        d2 = dbg_out("kT", [128, W], BF16)
        d3 = dbg_out("Vt", [128, 18 * 130], BF16)
        d4 = dbg_out("uT", [128, 4 * W], BF16)
        d5 = dbg_out("bgT", [128, 4 * 2048], BF16)
        allq = [("qT", c, ci) for c in range(4) for ci in range(1, 5)]
        allk = [("kT", ci) for ci in range(6)]
        allv = [("Vt", t) for t in range(18)] + [("Vt", "ones")]
        allu = [("uT", c, ci) for c in range(4) for ci in range(6)]
        allb = [("bgT", c, ci) for c in range(4) for ci in range(1, 5)]
        evs = [DMA("sync", "d_dbg", d1, qT[:].rearrange("p a b -> p (a b)"), allq, ["dbg1"]),
               DMA("sync", "d_dbg", d2, kT[:], allk, ["dbg2"]),
               DMA("sync", "d_dbg", d3, Vt[:].rearrange("p a b c -> p (a b c)"), allv, ["dbg3"]),
               DMA("sync", "d_dbg", d4, uT[:].rearrange("p a b -> p (a b)"), allu, ["dbg4"]),
               DMA("sync", "d_dbg", d5, bgT[:].rearrange("p a b -> p (a b)"), allb, ["dbg5"])]
        P.finish("sync", evs)
    if stage <= 3:
        P.emit()
        return nc, dbg_outs

    ALLWIN = WINQ + WINQS + WINK + WINKS + [("WIN", nm) for nm in ("v", "bg", "cg", "hv")]
    o2 = REG0 + 32768
    PL = [P.sb(f"PL{i}", [128, 384], BF16, off=o2 + i * 768) for i in range(2)]; o2 += 1536
    PC = [P.sb(f"PC{i}", [128, 256], BF16, off=o2 + i * 512) for i in range(2)]; o2 += 1024
    att_tm = P.sb("att_tm", [128, 512], BF16, off=o2); o2 += 1024
    rec = P.sb("rec", [128, 8], F32, off=o2); o2 += 64
    cvt = [P.sb(f"cvt{i}", [128, 512], F32, off=o2 + i * 2048) for i in range(2)]; o2 += 4096
    assert o2 <= REG0 + 47104

    def kchunk(wb):
        return 0 if wb == 0 else (5 if wb == 17 else 1 + (wb - 1) // 4)

    VONES = [("Vt", "ones")]
    TS("vector", uT[:, :, 127:128], uT[:, :, 127:128], metat[:, 0:1], None, ALU.mult, None,
       [("uT", c, 0) for c in range(4)] + ["metat"], [("uT", c, 0) for c in range(4)])
    TS("vector", uT[:, :, 2176:2177], uT[:, :, 2176:2177], metat[:, 1:2], None, ALU.mult, None,
       [("uT", c, 5) for c in range(4)] + ["metat"], [("uT", c, 5) for c in range(4)])

    def conv_unit(tcn, c):
        w0 = 128 + tcn * 512
        ud = [("uT", c, ci) for ci in (tcn, tcn + 1, tcn + 2)]
        cwd = [("cw", kk, c4) for kk in range(3) for c4 in range(4)]
        TS("vector", cvt[0][:], uT[:, c, w0 - 1:w0 + 511], cw[:, c, 0:1], None, ALU.mult, None, ud + cwd, ["cvt0"] + ALLWIN)
        TS("vector", cvt[1][:], uT[:, c, w0:w0 + 512], cw[:, c, 1:2], None, ALU.mult, None, ud + cwd, ["cvt1"] + ALLWIN)
        TT("vector", cvt[0][:], cvt[0][:], cvt[1][:], ALU.add, ["cvt0", "cvt1"], ["cvt0"])
        TS("vector", cvt[1][:], uT[:, c, w0 + 1:w0 + 513], cw[:, c, 2:3], None, ALU.mult, None, ud + cwd, ["cvt1"])
        TT("vector", cvt[0][:], cvt[0][:], cvt[1][:], ALU.add, ["cvt0", "cvt1"], ["cvt0"])
        TT("vector", mixT[:, 4 + c, tcn * 512:(tcn + 1) * 512], cvt[0][:], bgT[:, c, tcn * 512:(tcn + 1) * 512], ALU.mult,
           ["cvt0", ("bgT", c, tcn + 1)], [("mixT", "conv", c, tcn)] + ALLWIN)

    for i in range(1, 17):
        ci_q = 1 + (i - 1) // 4
        mv = 1 if i == 1 else (2 if i == 16 else 0)
        pvs = [psf("v"), psf("v")]
        for hn in range(8):
            half, c = hn // 4, hn % 4
            r0 = half * 64
            j = hn % 2
            sl, sln = psf("a")
            sc_, scn = psf("a")
            qsl = qT[r0:r0 + 64, c, i * 128:(i + 1) * 128]
            for kb in range(3):
                wb = i - 1 + kb
                MM(sl[:, kb * 128:(kb + 1) * 128], kT[r0:r0 + 64, wb * 128:(wb + 1) * 128], qsl, True, True,
                   [("qT", c, ci_q), ("kT", kchunk(wb))], [sln])
            for cb in range(2):
                MM(sc_[:, cb * 128:(cb + 1) * 128], kcT[r0:r0 + 64, cb * 128:(cb + 1) * 128], qsl, True, True,
                   [("qT", c, ci_q), "kcT"], [scn])
            ACT(PL[j][:], sl[:, 0:384], AF.Exp, [sln], [f"PL{j}"] + ALLWIN, scale=0.125)
            ACT(PC[j][:], sc_[:, 0:256], AF.Exp, [scn], [f"PC{j}"] + ALLWIN, scale=0.125)
            TT("vector", PL[j][:], PL[j][:], mask3[:, mv, :], ALU.mult,
               [f"PL{j}", ("mask3", mv), ("mask3", mv, 1), ("mask3", mv, 2)], [f"PL{j}"])
            pv, pvn = pvs[half]
            pvr = pv[:, c * 65:(c + 1) * 65]
            for kb in range(3):
                wb = i - 1 + kb
                MM(pvr, PL[j][:, kb * 128:(kb + 1) * 128], Vt[:, wb, half, :], kb == 0, False,
                   [f"PL{j}", ("Vt", wb)] + VONES, [pvn])
            for cb in range(2):
                MM(pvr, PC[j][:, cb * 128:(cb + 1) * 128], Vc[:, cb, half, :], False, cb == 1,
                   [f"PC{j}", ("Vc", cb), ("Vc", "ones")], [pvn])
        for b in range(2):
            pv, pvn = pvs[b]
            pvv = pv[:, 0:260].rearrange("p (h e) -> p h e", h=4)
            TT("vector", rec[:, b * 4:(b + 1) * 4].unsqueeze(2), pvv[:, :, 64:65], esink[:, b * 4:(b + 1) * 4].unsqueeze(2),
               ALU.add, [pvn, "esink"], [("rec", b)] + ALLWIN)
            P.op("vector", (lambda b=b: (lambda e: e.reciprocal(rec[:, b * 4:(b + 1) * 4], rec[:, b * 4:(b + 1) * 4])))(),
                 [("rec", b)], [("rec", b)])
            TT("vector", att_tm[:, b * 256:(b + 1) * 256].rearrange("p (h d) -> p h d", h=4), pvv[:, :, 0:64],
               rec[:, b * 4:(b + 1) * 4].unsqueeze(2).to_broadcast([128, 4, 64]), ALU.mult,
               [pvn, ("rec", b)], [("att_tm", b)] + ALLWIN)
        pb, pbn = psb()
        pbv = pb[:, 0:512].rearrange("p (k t) -> p k t", k=4)
        for cc in range(4):
            TR(pbv[:, cc, :], att_tm[:, cc * 128:(cc + 1) * 128], ident_b[:], [("att_tm", cc // 2), "ident_b"], [(pbn, cc)])
        ACT(mixT[:, 0:4, (i - 1) * 128:i * 128], pbv, AF.Copy, [(pbn, cc) for cc in range(4)],
            [("mixT", "att", i - 1)] + ALLWIN)
        conv_unit((i - 1) // 4, (i - 1) % 4)

    if stage <= 4:
        P.emit()
        return nc, dbg_outs

    HTALL = [(f"hT{a}", t) for a in range(2) for t in range(4)]
    P.dma("gpsimd", "d_wo", lambda e: e.dma_start(out=wo[:], in_=w_out.rearrange("(k p) n -> p k n", p=128)), [], ["wo"] + HTALL)
    QALL = [("qT", c, ci) for c in range(4) for ci in range(1, 5)]
    o3 = qT_off
    h2T = [P.sb(f"h2T{i}", [128, 8, 128], BF16, off=o3 + i * 2048) for i in range(2)]; o3 += 4096
    CONVDEAD = [("uT", c, ci) for c in range(4) for ci in range(6)] + [("bgT", c, ci) for c in range(4) for ci in range(1, 5)]
    rows_off = REG0 - 8 * 4096
    affTM = P.sb("affTM", [128, 16, 16], F32, off=rows_off)
    gm = P.sb("gm", [128, 16, 16], F32, off=rows_off + 1024)
    thr = P.sb("thr", [128, 16], F32, off=rows_off + 2048)
    affT = P.sb("affT", [16, 2048], F32, off=o3); o3 += 8192
    wr = P.sb("wr", [128, 8, 16], BF16, off=o3); o3 += 256
    sm = P.sb("sm", [128, 64], F32, off=o3); o3 += 256
    assert o3 <= qT_off + 4 * W * 2
    P.dma("gpsimd", "d_wr", lambda e: e.dma_start(out=wr[:], in_=w_router.rearrange("(k p) e -> p k e", p=128)), [], ["wr"] + QALL)
    def a3_A(tile):
        tcn = tile // 4
        mdeps = [("mixT", "att", tile)] + [("mixT", "conv", c, tcn) for c in range(4)]
        i = xt_rr[0] % 2
        xt_rr[0] += 1
        xtile, xn = xt[i], f"xt{i}"
        DMA("sync", "d_" + xn, xtile[:], x[128 + tile * 128:256 + tile * 128, :], [], [xn])
        for n in range(2):
            ps, psn = psf("v")
            for k in range(8):
                MM(ps[:], mixT[:, k, tile * 128:(tile + 1) * 128], wo[:, k, n * 512:(n + 1) * 512], k == 0, k == 7,
                   mdeps + ["wo"], [psn])
            TT("vector", tf[:, n * 512:(n + 1) * 512], ps[:], rows["GT1"][:, n * 512:(n + 1) * 512], ALU.mult,
               [psn] + rowdeps("GT1"), ["tf"])
        TT("vector", xtile[:], xtile[:], tf[:], ALU.add, [xn, "tf"], [xn])
        DMA("sync", "d_x1d_" + xn, x1d[tile * 128:(tile + 1) * 128, :], xtile[:], [xn], [("x1d", tile)])
        j = tile % 2
        norm_mod_sb(xtile, xn, "G2", "S2", hb[j], f"hb{j}")
        DMA("sync", f"d_h2loc{j}", h2loc[tile * 128:(tile + 1) * 128, :], hb[j][:], [f"hb{j}"], [("h2loc", tile)])

    def a3_B(tile):
        j = tile % 2
        h2v = h2T[j][:]
        pb, pbn = psb()
        pbv = pb[:].rearrange("p (k t) -> p k t", k=8)
        for k in range(8):
            TR(pbv[:, k, :], hb[j][:, k * 128:(k + 1) * 128], ident_b[:], [f"hb{j}", "ident_b"], [(pbn, k)])
        ACT(h2v, pbv, AF.Copy, [(pbn, k) for k in range(8)], [f"h2T{j}"] + QALL)
        ps, psn = psf("v")
        for k in range(8):
            MM(ps[:, 0:16], h2T[j][:, k, :], wr[:, k, :], k == 0, k == 7, [f"h2T{j}", "wr"], [psn])
        mx, nmx, ssum, ex = sm[:, 0:1], sm[:, 1:2], sm[:, 2:3], sm[:, 16:32]
        af = affTM[:, tile, :]
        RED("vector", mx, ps[:, 0:16], ALU.max, [psn], ["sm_mx"])
        TS("vector", nmx, mx, -1.0, None, ALU.mult, None, ["sm_mx"], ["sm_nmx"])
        ACT(ex, ps[:, 0:16], AF.Exp, [psn, "sm_nmx"], ["sm_ex"], bias=nmx)
        RED("vector", ssum, ex, ALU.add, ["sm_ex"], ["sm_sum"])
        P.op("vector", lambda e: e.reciprocal(sm[:, 2:3], sm[:, 2:3]), ["sm_sum"], ["sm_sum"])
        TS("vector", af, ex, ssum, None, ALU.mult, None, ["sm_ex", "sm_sum"], [("affTM", tile)])
        pt, ptn = psf("a")
        TR(pt[0:16, 0:128], af, ident_f[:], [("affTM", tile), "ident_f"], [ptn])
        ACT(affT[:, tile * 128:(tile + 1) * 128], pt[0:16, 0:128], AF.Copy, [ptn], [("affT", tile)] + QALL)

    a3_A(0)
    for tile in range(16):
        if tile + 1 < 16:
            a3_A(tile + 1)
        a3_B(tile)
    DMA("sync", "d_affloc", affloc, affT[:], [("affT", t) for t in range(16)], ["affloc"])

    if "a3" in dbg:
        d1 = dbg_out("x1", [2048, 1024])
        d2 = dbg_out("aff", [16, 2048])
        d3 = dbg_out("h2", [2048, 1024], BF16)
        e1 = DMA("sync", "d_dbg1", d1, x1d, [("x1d", t) for t in range(16)], ["dbg1"])
        e2 = DMA("sync", "d_dbg2", d2, affloc, ["affloc"], ["dbg2"])
        e3 = DMA("sync", "d_dbg3", d3, h2loc, [("h2loc", t) for t in range(16)], ["dbg3"])
        P.finish("sync", [e1, e2, e3])
    if stage <= 5:
        P.emit()
        return nc, dbg_outs

    FENCE = [k for k in P.res.keys() if not (isinstance(k, str) and (k.startswith("psf") or k in ("ident_b", "ident_f", "ones_b", "metat")))
             and not (isinstance(k, tuple) and k[0] in ("row", "h2T", "affTM", "x1d", "h2loc"))]
    NTB, NITB = 8, 8
    P.dma("gpsimd", "d_ag_aff", lambda e: e.collective_compute("AllGather", ALU.bypass, replica_groups=GROUPS,
                                                               ins=[affloc.opt()], outs=[affall.opt()]),
          ["affloc"], ["affall", "agchain"], inc=1)
    ob = REG0 + 159936 - 0
    ob = bgT_off + 32768
    AFt = P.sb("AFt", [128, 16, 64], F32, off=ob); ob += 4096
    FR = P.sb("FR", [128, 16, NTB], F32, off=ob); ob += 512
    Tt = P.sb("Tt", [128, 16, NTB], F32, off=ob); ob += 512
    tmpa = P.sb("tmpa", [128, 16, NTB], F32, off=ob); ob += 512
    get = P.sb("get", [128, 16, NTB], F32, off=ob); ob += 512
    cntb = P.sb("cntb", [128, 16 * NTB], BF16, off=ob); ob += 256
    lo = P.sb("lo", [128, 16], F32, off=ob); ob += 64
    hi = P.sb("hi", [128, 16], F32, off=ob); ob += 64
    wdt = P.sb("wdt", [128, 16], F32, off=ob); ob += 64
    red = P.sb("red", [128, 16], F32, off=ob); ob += 64
    idxt = P.sb("idxt", [128, 16], I32, off=ob); ob += 64
    assert ob <= rt1_off + 6144
    cmpb = P.sb("cmpb", [128, 16, NTB, 64], BF16, off=REG0 + 49152)
    for r in range(4):
        DMA("sync", "d_AFt", AFt[32 * r:32 * (r + 1), :, :],
            affall[r * 16:(r + 1) * 16, :].rearrange("e (p j) -> p e j", p=32, j=64), ["affall"], [("AFt", r)] + FENCE)
    AFD = [("AFt", r) for r in range(4)]
    P.op("gpsimd", lambda e: e.iota(FR[:], pattern=[[0, 16], [1, NTB]], base=1, channel_multiplier=0,
                                    allow_small_or_imprecise_dtypes=True), (), ["FR"] + FENCE)
    P.op("gpsimd", lambda e: e.iota(idxt[:], pattern=[[128, 16]], base=0, channel_multiplier=1), (), ["idxt"] + FENCE)
    TS("vector", FR[:], FR[:], 1.0 / (NTB + 1), None, ALU.mult, None, ["FR"], ["FR"])
    MSET("vector", lo[:], 0.0, ["lo"] + FENCE)
    MSET("vector", hi[:], 1.0, ["hi"])
    for it in range(NITB):
        TT("vector", wdt[:], hi[:], lo[:], ALU.subtract, ["hi", "lo"], ["wdt"])
        TT("vector", Tt[:], FR[:], wdt[:].unsqueeze(2).to_broadcast([128, 16, NTB]), ALU.mult, ["FR", "wdt"], ["Tt"])
        TT("vector", Tt[:], Tt[:], lo[:].unsqueeze(2).to_broadcast([128, 16, NTB]), ALU.add, ["Tt", "lo"], ["Tt"])
        TT("vector", cmpb[:], AFt[:].unsqueeze(2).to_broadcast([128, 16, NTB, 64]),
           Tt[:].unsqueeze(3).to_broadcast([128, 16, NTB, 64]), ALU.is_ge, AFD + ["Tt"], ["cmpb"] + FENCE)
        RED("vector", tmpa[:], cmpb[:], ALU.add, ["cmpb"], ["tmpa"])
        CP("vector", cntb[:], tmpa[:].rearrange("p e k -> p (e k)"), ["tmpa"], ["cntb"])
        ps, psn = psf("v")
        MM(ps[:, 0:16 * NTB], ones_b[:], cntb[:], True, True, ["cntb", "ones_b"], [psn])
        TS("vector", get[:].rearrange("p e k -> p (e k)"), ps[:, 0:16 * NTB], 1024.0, None, ALU.is_ge, None, [psn], ["get"])
        TT("vector", tmpa[:], Tt[:], get[:], ALU.mult, ["Tt", "get"], ["tmpa"])
        RED("vector", red[:], tmpa[:], ALU.max, ["tmpa"], ["red"])
        TT("vector", lo[:], lo[:], red[:], ALU.max, ["lo", "red"], ["lo"])
        TS("vector", tmpa[:], get[:], 2.0, None, ALU.mult, None, ["get"], ["tmpa"])
        TT("vector", tmpa[:], tmpa[:], Tt[:], ALU.add, ["tmpa", "Tt"], ["tmpa"])
        RED("vector", red[:], tmpa[:], ALU.min, ["tmpa"], ["red"])
        TT("vector", hi[:], hi[:], red[:], ALU.min, ["hi", "red"], ["hi"])
    CP("vector", thr[:], lo[:], ["lo"], ["thr"])
    AFFTM = [("affTM", t) for t in range(16)]
    TT("vector", gm[:], affTM[:], thr[:].unsqueeze(1).to_broadcast([128, 16, 16]), ALU.is_ge, AFFTM + ["thr"], ["gm"])
    TT("vector", gm[:], gm[:], affTM[:], ALU.mult, ["gm"] + AFFTM, ["gm"])

    if "thr" in dbg:
        d1 = dbg_out("thr", [128, 16])
        d2 = dbg_out("gm", [128, 256])
        e1 = DMA("sync", "d_dbg1", d1, thr[:], ["thr"], ["dbg1"])
        e2 = DMA("sync", "d_dbg2", d2, gm[:].rearrange("p a b -> p (a b)"), ["gm"], ["dbg2"])
        P.finish("sync", [e1, e2])
    if stage <= 6:
        P.emit()
        return nc, dbg_outs

    for j4 in range(4):
        P.dma("gpsimd", "d_ag_h2", (lambda j4=j4: (lambda e: e.collective_compute(
            "AllGather", ALU.bypass, replica_groups=GROUPS,
            ins=[h2loc[j4 * 512:(j4 + 1) * 512, :].opt()], outs=[h2all[j4 * 2048:(j4 + 1) * 2048, :].opt()])))(),
            [("h2loc", t) for t in range(j4 * 4, j4 * 4 + 4)] + ["agchain"], [("h2all", j4), "agchain"], inc=1)
    H2ALLD = [("h2all", j4) for j4 in range(4)]
    TAB = P.sb("TAB", [128, 16, 128], F32, off=qT_off)
    ob2 = kT_off
    ones64 = P.sb("ones64", [128, 64], F32, off=ob2); ob2 += 256
    n4 = P.sb("n4", [128, 4], F32, off=ob2); ob2 += 64
    t16 = P.sb("t16", [128, 16], F32, off=ob2); ob2 += 64
    rhsU = P.sb("rhsU", [128, 4, 128], BF16, off=ob2); ob2 += 1024
    rhsI = P.sb("rhsI", [128, 4, 128], BF16, off=ob2); ob2 += 1024
    offs_sb = P.sb("offs_sb", [128, 4, 128], F32, off=ob2); ob2 += 2048
    nrow_sb = P.sb("nrow_sb", [128, 4, 128], F32, off=ob2); ob2 += 2048
    sval = P.sb("sval", [128, 8], F32, off=ob2); ob2 += 64
    koffs = P.sb("koffs", [128, 4, 8], F32, off=ob2); ob2 += 128
    pS = P.sb("pS", [128, 4, 8], F32, off=ob2); ob2 += 128
    oex = P.sb("oex", [128, 4, 8], F32, off=ob2); ob2 += 128
    rS = P.sb("rS", [128, 4, 8], F32, off=ob2); ob2 += 128
    jS = P.sb("jS", [128, 4, 8], F32, off=ob2); ob2 += 128
    gS = P.sb("gS", [128, 4, 8], F32, off=ob2); ob2 += 128
    tSf = P.sb("tSf", [128, 4, 8], F32, off=ob2); ob2 += 128
    RIDX = P.sb("RIDX", [128, 4, 8], I32, off=ob2); ob2 += 128
    TIDX = P.sb("TIDX", [128, 4, 8], I32, off=ob2); ob2 += 128
    ZIDXf = P.sb("ZIDXf", [128, 4, 16], F32, off=ob2); ob2 += 256
    ZIDX = P.sb("ZIDX", [128, 4, 16], I32, off=ob2); ob2 += 256
    yz = [P.sb(f"yz{i}", [128, 1024], BF16, off=rows_off + 3 * 4096 + i * 2048) for i in range(2)]
    assert ob2 <= bgT_off, ob2
    cmpP = P.sb("cmpP", [128, 4, 8, 128], F32, off=REG0 + 32768)
    Gt = P.sb("Gt", [128, 4, 8, 128], F32, off=REG0 + 49152)

    MSET("vector", ones64[:], 1.0, ["ones64"] + FENCE)
    TT("vector", TAB[:, :, 64:128], AFt[:], thr[:].unsqueeze(2).to_broadcast([128, 16, 64]), ALU.is_ge, AFD + ["thr"], ["TABm"] + FENCE)
    for e16 in range(16):
        P.op("vector", (lambda e16=e16: (lambda e: e.tensor_tensor_scan(out=TAB[:, e16, 0:64], data0=ones64[:], data1=TAB[:, e16, 64:128],
                                                                          initial=0.0, op0=ALU.mult, op1=ALU.add)))(),
             ["TABm", "ones64"], [("TABc", e16)])
    TABC = [("TABc", e16) for e16 in range(16)]
    TT("vector", TAB[:, :, 64:128], TAB[:, :, 64:128], AFt[:], ALU.mult, ["TABm"] + TABC + AFD, ["TABm"])
    DMA("sync", "d_tabd", tabd.rearrange("(e p) c -> p e c", p=128), TAB[:], ["TABm"] + TABC, ["tabd"])
    selv = metat[:, 8:72].rearrange("p (e k) -> p e k", k=4)
    for k in range(4):
        TT("vector", t16[:], TAB[:, :, 63], selv[:, :, k], ALU.mult, TABC + ["metat"], ["t16"])
        RED("vector", n4[:, k:k + 1], t16[:], ALU.add, ["t16"], [("n4", k)])
    N4 = [("n4", k) for k in range(4)]
    TT("vector", rhsU[:], n4[:].unsqueeze(2).to_broadcast([128, 4, 128]), U_b[:].unsqueeze(1).to_broadcast([128, 4, 128]), ALU.mult,
       N4 + ["U_b"], ["rhsU"])
    TT("vector", rhsI[:], n4[:].unsqueeze(2).to_broadcast([128, 4, 128]), ident_b[:].unsqueeze(1).to_broadcast([128, 4, 128]), ALU.mult,
       N4 + ["ident_b"], ["rhsI"])
    ps, psn = psf("v")
    MM(ps[:], ones_b[:], rhsU[:].rearrange("p k q -> p (k q)"), True, True, ["rhsU", "ones_b"], [psn])
    CP("vector", offs_sb[:].rearrange("p k q -> p (k q)"), ps[:], [psn], ["offs_sb"])
    ps, psn = psf("v")
    MM(ps[:], ones_b[:], rhsI[:].rearrange("p k q -> p (k q)"), True, True, ["rhsI", "ones_b"], [psn])
    CP("vector", nrow_sb[:].rearrange("p k q -> p (k q)"), ps[:], [psn], ["nrow_sb"])
    P.op("gpsimd", lambda e: e.iota(sval[:], pattern=[[128, 8]], base=0, channel_multiplier=1,
                                    allow_small_or_imprecise_dtypes=True), (), ["sval"])
    P.op("gpsimd", lambda e: e.iota(koffs[:], pattern=[[128, 4], [0, 8]], base=0, channel_multiplier=0,
                                    allow_small_or_imprecise_dtypes=True), (), ["koffs"])
    P.op("gpsimd", lambda e: e.iota(ZIDXf[:], pattern=[[512, 4], [2048, 4], [128, 4]], base=0, channel_multiplier=1,
                                    allow_small_or_imprecise_dtypes=True), (), ["ZIDXf"])
    TS("vector", koffs[:], koffs[:], metat[:, 4:5], None, ALU.add, None, ["koffs", "metat"], ["koffs"])
    TS("vector", ZIDXf[:], ZIDXf[:], metat[:, 5:6], None, ALU.add, None, ["ZIDXf", "metat"], ["ZIDXf"])
    CP("vector", ZIDX[:], ZIDXf[:], ["ZIDXf"], ["ZIDX"])
    svb = sval[:].unsqueeze(1).to_broadcast([128, 4, 8])
    TT("vector", cmpP[:], offs_sb[:].unsqueeze(2).to_broadcast([128, 4, 8, 128]),
       svb.unsqueeze(3).to_broadcast([128, 4, 8, 128]), ALU.is_le, ["offs_sb", "sval"], ["cmpP"] + FENCE)
    RED("vector", pS[:], cmpP[:], ALU.add, ["cmpP"], ["pS"])
    TT("vector", cmpP[:], cmpP[:], nrow_sb[:].unsqueeze(2).to_broadcast([128, 4, 8, 128]), ALU.mult, ["cmpP", "nrow_sb"], ["cmpP"])
    RED("vector", oex[:], cmpP[:], ALU.add, ["cmpP"], ["oex"])
    TT("vector", rS[:], svb, oex[:], ALU.subtract, ["sval", "oex"], ["rS"])
    TT("vector", tSf[:], pS[:], koffs[:], ALU.add, ["pS", "koffs"], ["tSf"])
    CP("vector", RIDX[:], tSf[:], ["tSf"], ["RIDX"])
    for k in range(4):
        for c in range(8):
            P.dma("gpsimd", "d_G", (lambda k=k, c=c: (lambda e: e.indirect_dma_start(
                out=Gt[:, k, c, :], out_offset=None, in_=tabd, in_offset=bass.IndirectOffsetOnAxis(ap=RIDX[:, k, c:c + 1], axis=0))))(),
                ["tabd", "RIDX"], [("Gt", k, c), "cmpb"] if (k == 0 and c == 0) else [("Gt", k, c)])
    GALL = [("Gt", k, c) for k in range(4) for c in range(8)]
    cmpG = cmpP[:, :, :, 0:64]
    TT("vector", cmpG, Gt[:, :, :, 0:64], rS[:].unsqueeze(3).to_broadcast([128, 4, 8, 64]), ALU.is_le, GALL + ["rS"], ["cmpP"])
    RED("vector", jS[:], cmpG, ALU.add, ["cmpP"], ["jS"])
    TS("vector", oex[:], rS[:], 1.0, None, ALU.add, None, ["rS"], ["oex"])
    TT("vector", cmpG, Gt[:, :, :, 0:64], oex[:].unsqueeze(3).to_broadcast([128, 4, 8, 64]), ALU.is_equal, GALL + ["oex"], ["cmpP"])
    TT("vector", cmpG, cmpG, Gt[:, :, :, 64:128], ALU.mult, ["cmpP"] + GALL, ["cmpP"])
    RED("vector", gS[:], cmpG, ALU.add, ["cmpP"], ["gS"])
    TS("vector", tSf[:], pS[:], 64.0, None, ALU.mult, None, ["pS"], ["tSf"])
    TT("vector", tSf[:], tSf[:], jS[:], ALU.add, ["tSf", "jS"], ["tSf"])
    CP("vector", TIDX[:], tSf[:], ["tSf"], ["TIDX"])
    ra = P.sb("ra", [128, 4, 8], F32, off=ob2); rb = P.sb("rb", [128, 4, 8], F32, off=ob2 + 128)
    rj = P.sb("rj", [128, 4, 8], F32, off=ob2 + 256); GIDX = P.sb("GIDX", [128, 4, 8], I32, off=ob2 + 384)
    assert ob2 + 512 <= bgT_off
    TS("vector", ra[:], tSf[:], 2048.0, None, ALU.is_ge, None, ["tSf"], ["ra"])
    for thv in (4096.0, 6144.0):
        TS("vector", rb[:], tSf[:], thv, None, ALU.is_ge, None, ["tSf"], ["rb"])
        TT("vector", ra[:], ra[:], rb[:], ALU.add, ["ra", "rb"], ["ra"])
    TS("vector", rb[:], ra[:], -2048.0, None, ALU.mult, None, ["ra"], ["rb"])
    TT("vector", rb[:], rb[:], tSf[:], ALU.add, ["rb", "tSf"], ["rb"])
    TS("vector", rj[:], rb[:], 512.0, None, ALU.is_ge, None, ["rb"], ["rj"])
    for thv in (1024.0, 1536.0):
        TS("vector", oex[:], rb[:], thv, None, ALU.is_ge, None, ["rb"], ["oex"])
        TT("vector", rj[:], rj[:], oex[:], ALU.add, ["rj", "oex"], ["rj"])
    TT("vector", rj[:], rj[:], ra[:], ALU.subtract, ["rj", "ra"], ["rj"])
    TS("vector", rj[:], rj[:], 1536.0, None, ALU.mult, None, ["rj"], ["rj"])
    TT("vector", rj[:], rj[:], tSf[:], ALU.add, ["rj", "tSf"], ["rj"])
    CP("vector", GIDX[:], rj[:], ["rj"], ["GIDX"])

    if "idx" in dbg:
        d1 = dbg_out("tidx", [128, 32])
        d2 = dbg_out("gS", [128, 32])
        e1 = DMA("sync", "d_dbg1", d1, tSf[:].rearrange("p a b -> p (a b)"), ["tSf", "TIDX"], ["dbg1"])
        e2 = DMA("sync", "d_dbg2", d2, gS[:].rearrange("p a b -> p (a b)"), ["gS"], ["dbg2"])
        P.finish("sync", [e1, e2])
    if stage <= 6.5:
        P.emit()
        return nc, dbg_outs

    wslot = [P.sb(f"wslot{i}", [128, 8, 1024], BF16, off=REG0 + i * 16384) for i in range(4)]
    wdt_ = P.sb("wd_", [128, 8, 1024], BF16, off=REG0 + 81920)
    hid = [P.sb(f"hid{i}", [128, 8, 512], BF16, off=qT_off + i * 8192) for i in range(2)]
    XS = P.sb("XS", [128, 8, 1024], BF16, off=bgT_off)
    xsT = P.sb("xsT", [128, 8, 1024], BF16, off=bgT_off + 16384)
    sgs = P.sb("sgs", [128, 512], F32, off=rt1_off + 4096)
    MSET("vector", XS[:], 0.0, ["XS"] + FENCE)
    ZD0 = []
    for t in range(8):
        ZD0.append(("Zd0", t))
        DMA("sync", "d_z0", Zd[t * 1024:(t + 1) * 1024, :].rearrange("(p c) d -> p c d", c=8), XS[:], ["XS"], [("Zd0", t)])
    wgv = w_gate.rearrange("e (k p) n -> e p k n", p=128)
    wuv = w_up.rearrange("e (k p) n -> e p k n", p=128)
    wdv = w_down.rearrange("e (k p) n -> e p k n", p=128)
    yrr = [0]
    def issue_loads(k4):
        sg_, su_ = (k4 % 2) * 2, (k4 % 2) * 2 + 1
        wg_t, wu_t = wslot[sg_], wslot[su_]
        ex2 = (["cmpP"] if sg_ == 2 else [])
        ex3 = (["cmpb"] + GALL if su_ == 3 else [])
        P.dma("gpsimd", f"d_ws{sg_}", (lambda wg_t=wg_t, k4=k4: (lambda e: e.dma_start(out=wg_t[:], in_=wgv[k4])))(), [], [f"ws{sg_}"] + ex2 + (FENCE if k4 < 2 else []))
        P.dma("gpsimd", f"d_ws{su_}", (lambda wu_t=wu_t, k4=k4: (lambda e: e.dma_start(out=wu_t[:], in_=wuv[k4])))(), [], [f"ws{su_}"] + ex3 + (FENCE if k4 < 2 else []))
        for c in range(8):
            P.dma("gpsimd", f"d_XS{c}", (lambda k4=k4, c=c: (lambda e: e.indirect_dma_start(
                out=XS[:, c, :], out_offset=None, in_=h2all, in_offset=bass.IndirectOffsetOnAxis(ap=GIDX[:, k4, c:c + 1], axis=0))))(),
                H2ALLD + ["GIDX"], [("XS", c)] + (["XS"] if c == 0 else []))

    def issue_wd(k4):
        P.dma("gpsimd", "d_wd", (lambda k4=k4: (lambda e: e.dma_start(out=wdt_[:], in_=wdv[k4])))(), [], ["wd_"] + (FENCE if k4 < 1 else []))

    issue_loads(0)
    issue_wd(0)
    for k4 in range(4):
        sg_, su_ = (k4 % 2) * 2, (k4 % 2) * 2 + 1
        wg_t, wu_t = wslot[sg_], wslot[su_]
        for c in range(8):
            pb, pbn = psb()
            pbv = pb[:].rearrange("p (k t) -> p k t", k=8)
            for kc in range(8):
                TR(pbv[:, kc, :], XS[:, c, kc * 128:(kc + 1) * 128], ident_b[:], [("XS", c), "XS", "ident_b"], [(pbn, kc)])
            ACT(xsT[:, :, c * 128:(c + 1) * 128], pbv, AF.Copy, [(pbn, kc) for kc in range(8)], [("xsT", c)] + (FENCE if k4 == 0 else []))
        if k4 < 3:
            issue_loads(k4 + 1)
        for sch in range(2):
            hd = hid[(k4 * 2 + sch) % 2]
            hdn = f"hid{(k4 * 2 + sch) % 2}"
            xdeps = [("xsT", sch * 4 + t) for t in range(4)]
            for fo in range(8):
                pa, pan = psf("a")
                for kc in range(8):
                    MM(pa[:], wg_t[:, kc, fo * 128:(fo + 1) * 128], xsT[:, kc, sch * 512:(sch + 1) * 512], kc == 0, kc == 7,
                       [f"ws{sg_}"] + xdeps, [pan])
                pu, pun = psf("v")
                for kc in range(8):
                    MM(pu[:], wu_t[:, kc, fo * 128:(fo + 1) * 128], xsT[:, kc, sch * 512:(sch + 1) * 512], kc == 0, kc == 7,
                       [f"ws{su_}"] + xdeps, [pun])
                ACT(sgs[:], pa[:], AF.Silu, [pan], ["sgs"] + (FENCE if k4 == 0 and sch == 0 and fo == 0 else []))
                TT("vector", hd[:, fo, :], pu[:], sgs[:], ALU.mult, [pun, "sgs"], [(hdn, fo)] + (FENCE + ["TABm"] + TABC if k4 == 0 else []))
            for t in range(4):
                c = sch * 4 + t
                yi = yrr[0] % 2
                yrr[0] += 1
                for dn in range(2):
                    py, pyn = psf("v")
                    for kc in range(8):
                        MM(py[:], hd[:, kc, t * 128:(t + 1) * 128], wdt_[:, kc, dn * 512:(dn + 1) * 512], kc == 0, kc == 7,
                           [(hdn, kc), "wd_"], [pyn])
                    TS("vector", yz[yi][:, dn * 512:(dn + 1) * 512], py[:], gS[:, k4, c:c + 1], None, ALU.mult, None,
                       [pyn, "gS"], [f"yz{yi}"])
                P.dma("gpsimd", f"d_sz{yi}", (lambda yi=yi, k4=k4, c=c: (lambda e: e.indirect_dma_start(
                    out=Zd, out_offset=bass.IndirectOffsetOnAxis(ap=TIDX[:, k4, c:c + 1], axis=0), in_=yz[yi][:], in_offset=None,
                    compute_op=ALU.add, oob_is_err=True)))(), [f"yz{yi}", "TIDX", "Zd"] + ZD0, ["Zd"])
        if k4 < 3:
            issue_wd(k4 + 1)

    for j16 in range(16):
        P.dma("gpsimd", "d_ag_z", (lambda j16=j16: (lambda e: e.collective_compute(
            "AllGather", ALU.bypass, replica_groups=GROUPS,
            ins=[Zd[j16 * 512:(j16 + 1) * 512, :].opt()], outs=[Zall[j16 * 2048:(j16 + 1) * 2048, :].opt()])))(),
            ["Zd", "agchain"], [("Zall", j16), "agchain"], inc=1)
    ZALLD = [("Zall", j16) for j16 in range(16)]
    if stage <= 7:
        P.emit()
        return nc, dbg_outs

    gfrow = P.sb("gfrow", [128, 1024], F32, off=rows_off + 4096)
    DMA("sync", "d_gfrow", gfrow[:], g_final.partition_broadcast(128), [], ["gfrow"] + FENCE)
    z4 = [P.sb(f"z4_{i}", [128, 4, 1024], BF16, off=bgT_off + i * 8192) for i in range(2)]
    evs = []
    for tile in range(16):
        i = xt_rr[0] % 2
        xt_rr[0] += 1
        xtile, xn = xt[i], f"xt{i}"
        zi = tile % 2
        DMA("sync", "d_" + xn, xtile[:], x1d[tile * 128:(tile + 1) * 128, :], [("x1d", tile)], [xn])
        for r in range(4):
            P.dma("gpsimd", f"d_z4_{zi}_{r}", (lambda zi=zi, r=r, tile=tile: (lambda e: e.indirect_dma_start(
                out=z4[zi][:, r, :], out_offset=None, in_=Zall, in_offset=bass.IndirectOffsetOnAxis(ap=ZIDX[:, r, tile:tile + 1], axis=0))))(),
                ZALLD + ["ZIDX"], [(f"z4_{zi}", r)] + ([("XS", c) for c in range(8)] + ["XS"] if tile < 2 else []))
        zd = [(f"z4_{zi}", r) for r in range(4)]
        TT("vector", tf[:], z4[zi][:, 0, :], z4[zi][:, 1, :], ALU.add, zd, ["tf"])
        TT("vector", tf[:], tf[:], z4[zi][:, 2, :], ALU.add, zd + ["tf"], ["tf"])
        TT("vector", tf[:], tf[:], z4[zi][:, 3, :], ALU.add, zd + ["tf"], ["tf"])
        TT("vector", tf[:], tf[:], rows["GT2"][:], ALU.mult, ["tf"] + rowdeps("GT2"), ["tf"])
        TT("vector", xtile[:], xtile[:], tf[:], ALU.add, [xn, "tf"], [xn])
        ss = small[:, 16:17]
        rstd = small[:, 17:18]
        ACT(tf[:], xtile[:], AF.Square, [xn], ["tf"])
        RED("vector", ss, tf[:], ALU.add, ["tf"], ["ss"])
        TS("vector", rstd, ss, 1.0 / 1024.0, 1e-6, ALU.mult, ALU.add, ["ss"], ["rstd"])
        ACT(rstd, rstd, AF.Ln, ["rstd"], ["rstd"])
        ACT(rstd, rstd, AF.Exp, ["rstd"], ["rstd"], scale=-0.5)
        ACT(tf[:], xtile[:], AF.Copy, [xn, "rstd"], ["tf"], scale=rstd)
        TT("vector", xtile[:], tf[:], gfrow[:], ALU.mult, ["tf", "gfrow"], [xn])
        evs.append(DMA("sync", "d_out_" + xn, out[tile * 128:(tile + 1) * 128, :], xtile[:], [xn], [("out", tile)]))
    P.finish("sync", evs)
    P.emit()
    return nc, dbg_outs


def make_in_maps(inp):
    x = np.ascontiguousarray(inp["x"], dtype=np.float32)
    maps = []
    for c in range(NCORES):
        b, q = c // 4, c % 4
        t0 = q * 2048
        xw = np.zeros((W, 1024), np.float32)
        lo, hi = t0 - 128, t0 + 2048 + 128
        slo, shi = max(lo, 0), min(hi, 8192)
        xw[slo - lo:shi - lo] = x[b, slo:shi]
        ccv = np.stack([inp["c"][b].reshape(8, 128).T, inp["c_ctx"].reshape(8, 128).T], axis=-1)
        meta = np.zeros((128, 80), np.float32)
        meta[:, 0] = 1.0 if q > 0 else 0.0
        meta[:, 1] = 1.0 if q < 3 else 0.0
        meta[:, 2] = float(q * 32 - 2)
        meta[:, 3] = float(q * 2048)
        meta[:, 4] = float(4 * q * 128)
        meta[:, 5] = float(q * 8192)
        for k in range(4):
            meta[:, 8 + (4 * q + k) * 4 + k] = 1.0
        maps.append({
            "x": xw, "ctx": np.ascontiguousarray(inp["ctx"][b]), "cc": np.ascontiguousarray(ccv.reshape(128, 16)),
            "meta": meta, "w_ada": inp["w_ada"][0], "b_ada": inp["b_ada"][0], "g_mix": inp["g_mix"][0],
            "g_ffn": inp["g_ffn"][0], "g_final": inp["g_final"], "w_in": inp["w_in"][0], "conv_w": inp["conv_w"][0],
            "sink": inp["sink"][0], "w_out": inp["w_out"][0], "w_router": inp["w_router"][0],
            "w_gate": np.ascontiguousarray(inp["w_gate"][0, 4 * q:4 * q + 4]),
            "w_up": np.ascontiguousarray(inp["w_up"][0, 4 * q:4 * q + 4]),
            "w_down": np.ascontiguousarray(inp["w_down"][0, 4 * q:4 * q + 4]),
        })
    return maps


def kernel(**inputs):
    inp = {k: np.asarray(v) for k, v in inputs.items()}
    nc, _ = build_nc()
    res = run_bass_kernel_spmd(nc, make_in_maps(inp), core_ids=list(range(NCORES)))
    outp = np.zeros((2, 8192, 1024), np.float32)
    for c in range(NCORES):
        b, q = c // 4, c % 4
        outp[b, q * 2048:(q + 1) * 2048] = res.results[c]["out"]
    return outp
```

```python
import os
import numpy as np
import concourse.bass as bass
import concourse.mybir as mybir
from concourse.bass_utils import run_bass_kernel_spmd

F32 = mybir.dt.float32
BF16 = mybir.dt.bfloat16
I32 = mybir.dt.int32
ALU = mybir.AluOpType
AF = mybir.ActivationFunctionType
AX = mybir.AxisListType

COMPUTE = ("tensor", "vector", "scalar", "gpsimd")
QUEUES = ("sync",)
NCORES = 8
GROUPS = [[0, 1, 2, 3], [4, 5, 6, 7]]
W = 2304
NT = 16
NIT = 7


class Prog:
    def __init__(self, nc):
        self.nc = nc
        self.streams = {e: [] for e in COMPUTE + QUEUES}
        self.cnt = {e: 0 for e in COMPUTE}
        self.dma_cnt = {}
        self.waited = {}
        self.res = {}
        self.sem_handles = {}
        self.final_events = []
        self.sb_off = 16512
        self.sb_top = 229344

    def sb(self, name, shape, dtype, off=None):
        esz = {F32: 4, BF16: 2, I32: 4}[dtype]
        n = 1
        for s in shape[1:]:
            n *= s
        nbytes = (n * esz + 63) // 64 * 64
        if off is None:
            off = self.sb_off
            self.sb_off += nbytes
        assert off >= 16512 and off + nbytes <= self.sb_top, (name, off, nbytes)
        return self.nc.alloc_sbuf_tensor_at(name, list(shape), dtype, offset=off)

    def _deps(self, reads, writes):
        need = []
        for r in reads:
            st = self.res.get(r)
            if st and st["w"] is not None:
                need.append(st["w"])
        for w in writes:
            st = self.res.get(w)
            if st:
                if st["w"] is not None:
                    need.append(st["w"])
                need.extend(st["r"])
        return need

    def _commit(self, ev, reads, writes):
        for r in reads:
            st = self.res.setdefault(r, {"w": None, "r": []})
            st["r"].append(ev)
        for w in writes:
            self.res[w] = {"w": ev, "r": []}

    def _waits(self, eng, need):
        best = {}
        for (k, v) in need:
            if k == "tensor" and eng == "tensor":
                continue
            if v > best.get(k, 0):
                best[k] = v
        out = []
        for k, v in best.items():
            if self.waited.get((eng, k), 0) >= v:
                continue
            self.waited[(eng, k)] = v
            out.append((k, v))
        return out

    def op(self, eng, fn, reads=(), writes=()):
        need = self._deps(reads, writes)
        waits = self._waits(eng, need)
        self.cnt[eng] += 1
        ev = (eng, self.cnt[eng])
        self.streams[eng].append((waits, fn, (eng, 1)))
        self._commit(ev, reads, writes)
        return ev

    def dma(self, q, sem, fn, reads=(), writes=(), inc=16):
        need = self._deps(reads, writes)
        waits = self._waits(q, need)
        self.dma_cnt[sem] = self.dma_cnt.get(sem, 0) + inc
        ev = (sem, self.dma_cnt[sem])
        self.streams[q].append((waits, fn, (sem, inc)))
        self._commit(ev, reads, writes)
        return ev

    def finish(self, eng, events):
        self.final_events.append((eng, events))

    def check_deadlock(self):
        sem = {}
        pos = {e: 0 for e in self.streams}
        progressed = True
        while progressed:
            progressed = False
            for e, st in self.streams.items():
                while pos[e] < len(st):
                    waits, fn, inc = st[pos[e]]
                    if all(sem.get(k, 0) >= v for (k, v) in waits):
                        sem[inc[0]] = sem.get(inc[0], 0) + inc[1]
                        pos[e] += 1
                        progressed = True
                    else:
                        break
        stuck = {e: (pos[e], len(st), st[pos[e]][0]) for e, st in self.streams.items() if pos[e] < len(st)}
        assert not stuck, ("DEADLOCK", stuck, {k: sem.get(k) for e in stuck for (k, v) in stuck[e][2]})

    def emit(self):
        self.check_deadlock()
        nc = self.nc
        names = set(COMPUTE)
        for e in self.streams:
            for (waits, fn, inc) in self.streams[e]:
                names.add(inc[0])
                for (k, v) in waits:
                    names.add(k)
        for n in sorted(names):
            self.sem_handles[n] = nc.alloc_semaphore("s_" + n)
        H = self.sem_handles
        fin = {}
        for eng, evs in self.final_events:
            fin.setdefault(eng, []).extend(evs)
        with nc.Block() as block:
            def make(ename):
                def body(e):
                    for (waits, fn, inc) in self.streams[ename]:
                        for (k, v) in waits:
                            e.wait_ge(H[k], v)
                        fn(e).then_inc(H[inc[0]], inc[1])
                    best = {}
                    for (k, v) in fin.get(ename, []):
                        best[k] = max(best.get(k, 0), v)
                    for k, v in best.items():
                        e.wait_ge(H[k], v)
                return body
            for ename in self.streams:
                if not self.streams[ename] and ename not in fin:
                    continue
                getattr(block, ename)(make(ename))


def build_nc(stage=99, dbg=()):
    nc = bass.Bass("TRN2", target_bir_lowering=False)
    P = Prog(nc)
    dbg_outs = {}

    def din(name, shape, dt=F32):
        return nc.dram_tensor(name, list(shape), dt, kind="ExternalInput").ap()

    x = din("x", [W, 1024])
    ctx = din("ctx", [256, 1024])
    cc = din("cc", [128, 16])
    meta = din("meta", [128, 80])
    w_ada = din("w_ada", [1024, 6144])
    b_ada = din("b_ada", [6144])
    g_mix = din("g_mix", [1024])
    g_ffn = din("g_ffn", [1024])
    g_final = din("g_final", [1024])
    w_in = din("w_in", [1024, 2304])
    conv_w = din("conv_w", [3, 512])
    sink = din("sink", [8])
    w_out = din("w_out", [1024, 1024])
    w_router = din("w_router", [1024, 16])
    w_gate = din("w_gate", [4, 1024, 1024])
    w_up = din("w_up", [4, 1024, 1024])
    w_down = din("w_down", [4, 1024, 1024])
    out = nc.dram_tensor("out", [2048, 1024], F32, kind="ExternalOutput").ap()

    x1d = nc.dram_tensor("x1d", [2048, 1024], F32).ap()
    h2loc = nc.dram_tensor("h2loc", [2048, 1024], BF16).ap()
    h2all = nc.dram_tensor("h2all", [8192, 1024], BF16).ap()
    affloc = nc.dram_tensor("affloc", [16, 2048], F32).ap()
    affall = nc.dram_tensor("affall", [64, 2048], F32).ap()
    tabd = nc.dram_tensor("tabd", [2048, 128], F32).ap()
    Zd = nc.dram_tensor("Zd", [8192, 1024], BF16).ap()
    Zall = nc.dram_tensor("Zall", [32768, 1024], BF16).ap()

    def dbg_out(name, shape, dt=F32):
        t = nc.dram_tensor("dbg_" + name, list(shape), dt, kind="ExternalOutput").ap()
        dbg_outs[name] = t
        return t

    def ACT(out_, in_, func, r, w, **kw):
        return P.op("scalar", lambda e: e.activation(out=out_, in_=in_, func=func, **kw), r, w)

    def TT(eng, out_, in0, in1, op, r, w):
        return P.op(eng, lambda e: e.tensor_tensor(out=out_, in0=in0, in1=in1, op=op), r, w)

    def TS(eng, out_, in0, s1, s2, op0, op1, r, w):
        if op1 is None:
            return P.op(eng, lambda e: e.tensor_scalar(out=out_, in0=in0, scalar1=s1, scalar2=None, op0=op0), r, w)
        return P.op(eng, lambda e: e.tensor_scalar(out=out_, in0=in0, scalar1=s1, scalar2=s2, op0=op0, op1=op1), r, w)

    def STT(eng, out_, in0, scalar, in1, op0, op1, r, w):
        return P.op(eng, lambda e: e.scalar_tensor_tensor(out=out_, in0=in0, scalar=scalar, in1=in1, op0=op0, op1=op1), r, w)

    def RED(eng, out_, in_, op, r, w):
        return P.op(eng, lambda e: e.tensor_reduce(out=out_, in_=in_, axis=AX.X, op=op), r, w)

    def CP(eng, out_, in_, r, w):
        return P.op(eng, lambda e: e.tensor_copy(out=out_, in_=in_), r, w)

    def MSET(eng, out_, val, w):
        return P.op(eng, lambda e: e.memset(out_, val), (), w)

    def MM(out_, lhsT, rhs, start, stop, r, w):
        return P.op("tensor", lambda e: e.matmul(out_, lhsT, rhs, start=start, stop=stop), r, w)

    def TR(out_, in_, ident, r, w):
        return P.op("tensor", lambda e: e.transpose(out_, in_, ident), r, w)

    def DMA(q, sem, out_, in_, r, w):
        return P.dma(q, sem, lambda e: e.dma_start(out=out_, in_=in_), r, w)

    PSF = [nc.alloc_psum_tensor(f"psf{i}", [128, 512], F32) for i in range(6)]
    PSB = [nc.alloc_psum_tensor(f"psb{i}", [128, 1024], BF16) for i in range(2)]
    psf_rr = {"v": 0, "a": 0}

    def psf(cons):
        i = psf_rr[cons] % 3 + (0 if cons == "v" else 3)
        psf_rr[cons] += 1
        return PSF[i], f"psf{i}"

    psb_rr = [0]

    def psb():
        i = psb_rr[0] % 2
        psb_rr[0] += 1
        return PSB[i], f"psb{i}"

    ident_f = P.sb("ident_f", [128, 128], F32)
    ident_b = P.sb("ident_b", [128, 128], BF16)
    iot = P.sb("iot", [128, 128], F32)
    ones_b = P.sb("ones_b", [128, 128], BF16)
    U_b = P.sb("U_b", [128, 128], BF16)
    UI_b = P.sb("UI_b", [128, 128], BF16)
    mask3 = P.sb("mask3", [128, 3, 384], BF16)
    metat = P.sb("metat", [128, 80], F32)
    esink = P.sb("esink", [128, 8], F32)
    rows = {}
    for nm in ("S1", "G1", "GT1", "S2", "G2", "GT2", "cS1", "cG1"):
        rows[nm] = P.sb("row_" + nm, [128, 1024], F32)
    REG0 = P.sb_off

    P.op("gpsimd", lambda e: e.iota(iot[:], pattern=[[1, 128]], base=0, channel_multiplier=-1,
                                    allow_small_or_imprecise_dtypes=True), (), ["iot"])
    TS("vector", ident_f[:], iot[:], 0.0, None, ALU.is_equal, None, ["iot"], ["ident_f"])
    CP("vector", ident_b[:], ident_f[:], ["ident_f"], ["ident_b"])
    TS("vector", U_b[:], iot[:], 0.0, None, ALU.is_ge, None, ["iot"], ["U_b"])
    MSET("vector", ones_b[:], 1.0, ["ones_b"])
    DMA("sync", "d_meta", metat[:], meta, [], ["metat"])
    DMA("sync", "d_sink", esink[:], sink.partition_broadcast(128), [], ["esink"])
    ACT(esink[:], esink[:], AF.Exp, ["esink"], ["esink"])
    for v in range(3):
        TS("vector", mask3[:, v, 0:128], iot[:], 0.0, None, ALU.is_le, None, ["iot"], [("mask3", v)])
        MSET("vector", mask3[:, v, 128:256], 1.0, [("mask3", v, 1)])
        TS("vector", mask3[:, v, 256:384], iot[:], 0.0, None, ALU.is_ge, None, ["iot"], [("mask3", v, 2)])
    TS("vector", mask3[:, 1, 0:128], mask3[:, 1, 0:128], metat[:, 0:1], None, ALU.mult, None,
       ["metat", ("mask3", 1)], [("mask3", 1)])
    TS("vector", mask3[:, 2, 256:384], mask3[:, 2, 256:384], metat[:, 1:2], None, ALU.mult, None,
       ["metat", ("mask3", 2, 2)], [("mask3", 2, 2)])

    if "const" in dbg:
        d1 = dbg_out("ident", [128, 128])
        d2 = dbg_out("mask3", [128, 3 * 384], BF16)
        d3 = dbg_out("esink", [128, 8])
        e1 = DMA("sync", "d_dbg", d1, ident_f[:], ["ident_f"], ["dbg1"])
        e2 = DMA("sync", "d_dbg", d2, mask3[:].rearrange("p a b -> p (a b)"),
                 [("mask3", v) for v in range(3)] + [("mask3", v, 1) for v in range(3)] + [("mask3", v, 2) for v in range(3)], ["dbg2"])
        e3 = DMA("sync", "d_dbg", d3, esink[:], ["esink"], ["dbg3"])
        P.finish("sync", [e1, e2, e3])
    if stage <= 0:
        P.emit()
        return nc, dbg_outs

    o = REG0
    WIN = P.sb("WIN", [128, 8, 2944], BF16, off=o)
    mixT = P.sb("mixT", [128, 8, 2048], BF16, off=o)
    o += 47104
    COS = P.sb("COS", [128, W], F32, off=o); o += W * 4
    SINS = P.sb("SINS", [128, W], F32, off=o); o += W * 4
    xt = [P.sb(f"xt{i}", [128, 1024], F32, off=o + i * 4096) for i in range(2)]; o += 8192
    tf = P.sb("tf", [128, 1024], F32, off=o); o += 4096
    hb = [P.sb(f"hb{i}", [128, 1024], BF16, off=o + i * 2048) for i in range(2)]; o += 4096
    hT = [P.sb(f"hT{i}", [128, 8, 512], BF16, off=o + i * 8192) for i in range(2)]
    wo = P.sb("wo", [128, 8, 1024], BF16, off=o)
    o += 16384
    qT_off = o
    qT = P.sb("qT", [128, 4, W], BF16, off=o); o += 4 * W * 2
    kT_off = o
    kT = P.sb("kT", [128, W], BF16, off=o); o += W * 2
    Vt = P.sb("Vt", [128, 18, 2, 65], BF16, off=o); o += 4736
    kcT = P.sb("kcT", [128, 256], BF16, off=o); o += 512
    Vc = P.sb("Vc", [128, 2, 2, 65], BF16, off=o); o += 576
    bgT_off = o
    bgT = P.sb("bgT", [128, 4, 2048], BF16, off=o)
    stg = P.sb("stg", [128, 8, 640], F32, off=o)
    o += 20480
    uT_off = o
    uT = P.sb("uT", [128, 4, W], BF16, off=o); o += 4 * W * 2
    rt1_off = o
    rt1 = P.sb("rt1", [128, 512], F32, off=o); o += 2048
    rt2 = P.sb("rt2", [128, 512], F32, off=o); o += 2048
    cgs = P.sb("cgs", [128, 512], F32, off=o); o += 2048
    small = P.sb("small", [128, 64], F32, off=o); o += 256
    cw = P.sb("cw", [128, 4, 3], F32, off=o); o += 64
    assert o <= P.sb_top, o
    A_END = o

    wa = [P.sb("wa0", [128, 8, 1024], BF16, off=qT_off), P.sb("wa1", [128, 8, 1024], BF16, off=uT_off)]
    o = kT_off
    lb = P.sb("lb", [128, 8, 2, 128], BF16, off=o); o += 4096
    brow = P.sb("brow", [128, 1024], F32, off=o); o += 4096
    cct = P.sb("cct", [128, 8, 2], F32, off=o); o += 64
    scl = P.sb("scl", [128, 8, 2], F32, off=o); o += 64
    assert o <= bgT_off
    gmrow = P.sb("gmrow", [128, 1024], F32, off=rt1_off)

    DMA("sync", "d_cc", cct[:], cc.rearrange("p (k v) -> p k v", v=2), [], ["cct"])
    ACT(scl[:], cct[:], AF.Silu, ["cct"], ["scl"])
    for v in range(2):
        CP("vector", lb[:, :, v, :], scl[:, :, v:v + 1].to_broadcast([128, 8, 128]), ["scl"], [("lb", v)])
    if stage <= 0.3:
        d1 = dbg_out("lb", [128, 8 * 2 * 128], BF16)
        e1 = DMA("sync", "d_dbg", d1, lb[:].rearrange("p a b c -> p (a b c)"), [("lb", 0), ("lb", 1)], ["dbg1"])
        P.finish("sync", [e1])
        P.emit()
        return nc, dbg_outs
    w_ada_v = w_ada.rearrange("(k p) n -> p k n", p=128)
    grp = [(0, [("S1", 0), ("cS1", 1)]), (1, [("G1", 0), ("cG1", 1)]), (2, [("GT1", 0)]),
           (3, [("S2", 0)]), (4, [("G2", 0)]), (5, [("GT2", 0)])]
    for gi, (g, uses) in enumerate(grp):
        wb = wa[gi % 2]
        wn = f"wa{gi % 2}"
        P.dma("gpsimd", "d_" + wn, (lambda wb=wb, g=g: (lambda e: e.dma_start(out=wb[:], in_=w_ada_v[:, :, g * 1024:(g + 1) * 1024])))(),
              [], [wn])
        DMA("sync", "d_brow", brow[:], b_ada[g * 1024:(g + 1) * 1024].partition_broadcast(128), [], ["brow"])
        if stage <= 0.5:
            d1 = dbg_out("wa", [128, 8 * 1024], BF16)
            d2 = dbg_out("brow", [128, 1024])
            e1 = DMA("sync", "d_dbg", d1, wb[:].rearrange("p a b -> p (a b)"), [wn], ["dbg1"])
            e2 = DMA("sync", "d_dbg", d2, brow[:], ["brow"], ["dbg2"])
            P.finish("sync", [e1, e2])
            P.emit()
            return nc, dbg_outs
        for (nm, v) in uses:
            for n in range(2):
                ps, psn = psf("v")
                for k in range(8):
                    MM(ps[:], lb[:, k, v, :], wb[:, k, n * 512:(n + 1) * 512], k == 0, k == 7,
                       [("lb", v), wn], [psn])
                TT("vector", rows[nm][:, n * 512:(n + 1) * 512], ps[:], brow[:, n * 512:(n + 1) * 512], ALU.add,
                   [psn, "brow"], [("row", nm, n)])
                if stage <= 0.7:
                    d1 = dbg_out("r0", [128, 512])
                    e1 = DMA("sync", "d_dbg", d1, rows[nm][:, 0:512], [("row", nm, n)], ["dbg1"])
                    P.finish("sync", [e1])
                    P.emit()
                    return nc, dbg_outs
    for (gsrc, names) in (((g_mix, ("G1", "cG1")), (g_ffn, ("G2",))) if stage > 0.8 else ()):
        DMA("sync", "d_gmrow", gmrow[:], gsrc.partition_broadcast(128), [], ["gmrow"])
        for nm in names:
            TS("vector", rows[nm][:], rows[nm][:], 1.0, None, ALU.add, None,
               [("row", nm, 0), ("row", nm, 1)], [("row", nm, 0), ("row", nm, 1)])
            TT("vector", rows[nm][:], rows[nm][:], gmrow[:], ALU.mult,
               [("row", nm, 0), ("row", nm, 1), "gmrow"], [("row", nm, 0), ("row", nm, 1)])

    def rowdeps(nm):
        return [("row", nm, 0), ("row", nm, 1)]

    if "rows" in dbg:
        d = dbg_out("rows", [8, 128, 1024])
        for i, nm in enumerate(("S1", "G1", "GT1", "S2", "G2", "GT2", "cS1", "cG1")):
            ev = DMA("sync", "d_dbg", d[i], rows[nm][:], rowdeps(nm), ["dbg"])
        P.finish("sync", [ev])
    if stage <= 1:
        P.emit()
        return nc, dbg_outs


    def sc(i):
        return small[:, i:i + 1]
    pid, dd, i32_, isC, ff, inv, invC, invR, sgn, tmpc = [sc(i) for i in range(10)]
    P.op("gpsimd", lambda e: e.iota(small[:, 0:1], pattern=[[0, 1]], base=0, channel_multiplier=1,
                                    allow_small_or_imprecise_dtypes=True), (), ["small"])
    TS("vector", tmpc, pid, 64.0, -64.0, ALU.is_ge, ALU.mult, ["small"], ["small"])
    TT("vector", dd, pid, tmpc, ALU.add, ["small"], ["small"])
    TS("vector", sgn, dd, 32.0, None, ALU.is_ge, None, ["small"], ["small"])
    TS("vector", tmpc, sgn, -32.0, None, ALU.mult, None, ["small"], ["small"])
    TT("vector", i32_, dd, tmpc, ALU.add, ["small"], ["small"])
    TS("vector", isC, i32_, 16.0, None, ALU.is_ge, None, ["small"], ["small"])
    TS("vector", tmpc, isC, -16.0, None, ALU.mult, None, ["small"], ["small"])
    TT("vector", ff, i32_, tmpc, ALU.add, ["small"], ["small"])
    ACT(inv, ff, AF.Exp, ["small"], ["small"], scale=-float(np.log(10000.0) / 16.0))
    TT("vector", invC, inv, isC, ALU.mult, ["small"], ["small"])
    TT("vector", invR, inv, invC, ALU.subtract, ["small"], ["small"])
    TS("vector", sgn, sgn, 2.0, -1.0, ALU.mult, ALU.add, ["small"], ["small"])
    rrA = P.sb("rrA", [128, W], F32, off=qT_off)
    rrI = P.sb("rrI", [128, W], I32, off=qT_off + W * 4)
    ang = P.sb("ang", [128, W], F32, off=uT_off)
    P.op("gpsimd", lambda e: e.iota(COS[:], pattern=[[1, 36], [0, 64]], base=0, channel_multiplier=0,
                                    allow_small_or_imprecise_dtypes=True), (), ["COS"])
    P.op("gpsimd", lambda e: e.iota(SINS[:], pattern=[[0, 36], [1, 64]], base=0, channel_multiplier=0,
                                    allow_small_or_imprecise_dtypes=True), (), ["SINS"])
    HW_ = W // 2
    TWO_PI = float(2 * np.pi)
    for hh in range(2):
        sl = slice(hh * HW_, (hh + 1) * HW_)
        TS("vector", COS[:, sl], COS[:, sl], metat[:, 2:3], None, ALU.add, None, ["COS", "metat"], ["COS"])
        TS("vector", COS[:, sl], COS[:, sl], invR, None, ALU.mult, None, ["COS", "small"], ["COS"])
        TS("vector", SINS[:, sl], SINS[:, sl], invC, None, ALU.mult, None, ["SINS", "small"], ["SINS"])
    TT("vector", ang[:], COS[:], SINS[:], ALU.add, ["COS", "SINS"], ["ang"])

    def range_reduce_sin(dst, dstn, offset):
        TS("vector", rrA[:], ang[:], 1.0 / TWO_PI, offset / TWO_PI + 8.5, ALU.mult, ALU.add, ["ang"], ["rrA"])
        CP("vector", rrI[:], rrA[:], ["rrA"], ["rrI"])
        CP("vector", rrA[:], rrI[:], ["rrI"], ["rrA"])
        TS("vector", rrA[:], rrA[:], -TWO_PI, 8 * TWO_PI + offset, ALU.mult, ALU.add, ["rrA"], ["rrA"])
        TT("vector", dst[:], ang[:], rrA[:], ALU.add, ["ang", "rrA"], [dstn])
        TS("vector", rrA[:], dst[:], float(np.pi), -TWO_PI, ALU.is_gt, ALU.mult, [dstn], ["rrA"])
        TT("vector", dst[:], dst[:], rrA[:], ALU.add, [dstn, "rrA"], [dstn])
        TS("vector", rrA[:], dst[:], -float(np.pi), TWO_PI, ALU.is_lt, ALU.mult, [dstn], ["rrA"])
        TT("vector", dst[:], dst[:], rrA[:], ALU.add, [dstn, "rrA"], [dstn])
        ACT(dst[:], dst[:], AF.Sin, [dstn], [dstn])

    range_reduce_sin(SINS, "SINS", 0.0)
    range_reduce_sin(COS, "COS", float(np.pi / 2))
    for hh in range(2):
        sl = slice(hh * HW_, (hh + 1) * HW_)
        TS("vector", SINS[:, sl], SINS[:, sl], sgn, None, ALU.mult, None, ["SINS", "small"], ["SINS"])

    w_in_v = w_in.rearrange("(k p) n -> p k n", p=128)
    DMA("sync", "d_stg", stg[:], w_in_v[:, :, 0:640], [], ["stg"])
    qd = WIN[:, :, 0:512].rearrange("p k (c h d) -> p k c h d", c=4, h=2, d=64)
    qs = stg[:, :, 0:512].rearrange("p k (h c d) -> p k c h d", h=2, c=4, d=64)
    for h in range(2):
        ACT(qd[:, :, :, h, :], qs[:, :, :, h, :], AF.Copy, ["stg"], [("WIN", "q", h)])
    qd2 = WIN[:, :, 512:1024].rearrange("p k (c h s d) -> p k c h s d", c=4, h=2, s=2, d=32)
    qs2 = stg[:, :, 0:512].rearrange("p k (h c s d) -> p k c h s d", h=2, c=4, s=2, d=32)
    for h in range(2):
        for s in range(2):
            ACT(qd2[:, :, :, h, s, :], qs2[:, :, :, h, 1 - s, :], AF.Copy, ["stg"], [("WIN", "qsw", h, s)])
    ACT(WIN[:, :, 1024:1152], stg[:, :, 512:640], AF.Copy, ["stg"], [("WIN", "k")])
    kd2 = WIN[:, :, 1152:1280].rearrange("p k (h s d) -> p k h s d", h=2, s=2, d=32)
    ks2 = stg[:, :, 512:640].rearrange("p k (h s d) -> p k h s d", h=2, s=2, d=32)
    for s in range(2):
        ACT(kd2[:, :, :, s, :], ks2[:, :, :, 1 - s, :], AF.Copy, ["stg"], [("WIN", "ksw", s)])
    WINQ = [("WIN", "q", 0), ("WIN", "q", 1)]
    WINQS = [("WIN", "qsw", h, s) for h in range(2) for s in range(2)]
    WINK = [("WIN", "k")]
    WINKS = [("WIN", "ksw", 0), ("WIN", "ksw", 1)]
    for (nm, d0, s0, n) in (("v", 1280, 640, 128), ("bg", 1408, 768, 512), ("cg", 1920, 1280, 512), ("hv", 2432, 1792, 512)):
        P.dma("gpsimd", "d_win_" + nm, (lambda d0=d0, s0=s0, n=n: (lambda e: e.dma_start(out=WIN[:, :, d0:d0 + n], in_=w_in_v[:, :, s0:s0 + n])))(),
              [], [("WIN", nm)])
    for kk in range(3):
        for c4 in range(4):
            P.dma("sync", "d_cw", (lambda kk=kk, c4=c4: (lambda e: e.dma_start(
                out=cw[:, c4, kk:kk + 1], in_=conv_w[kk, c4 * 128:(c4 + 1) * 128].rearrange("(p o) -> p o", o=1))))(),
                [], [("cw", kk, c4)])
    MSET("vector", Vt[:, :, :, 64:65], 1.0, [("Vt", "ones")])
    MSET("vector", Vc[:, :, :, 64:65], 1.0, [("Vc", "ones")])

    xt_rr = [0]

    def norm_mod(src_rows, Gn, Sn, hbuf, hname, extra_r=()):
        i = xt_rr[0] % 2
        xt_rr[0] += 1
        xtile, xn = xt[i], f"xt{i}"
        DMA("sync", "d_" + xn, xtile[:], src_rows, list(extra_r), [xn])
        norm_mod_sb(xtile, xn, Gn, Sn, hbuf, hname)
        return xtile, xn

    tf2 = P.sb("tf2", [128, 1024], F32, off=bgT_off + 16384)
    nm_rr = [0]

    def norm_mod_sb(xtile, xn, Gn, Sn, hbuf, hname):
        pi = nm_rr[0] % 2
        nm_rr[0] += 1
        tfx, tfn = (tf, "tf") if pi == 0 else (tf2, "tf2")
        ss = small[:, 16 + 2 * pi:17 + 2 * pi]
        rstd = small[:, 17 + 2 * pi:18 + 2 * pi]
        ssn, rsn = f"ss{pi}", f"rstd{pi}"
        ACT(tfx[:], xtile[:], AF.Square, [xn], [tfn])
        RED("vector", ss, tfx[:], ALU.add, [tfn], [ssn])
        TS("vector", rstd, ss, 1.0 / 1024.0, 1e-6, ALU.mult, ALU.add, [ssn], [rsn])
        ACT(rstd, rstd, AF.Ln, [rsn], [rsn])
        ACT(rstd, rstd, AF.Exp, [rsn], [rsn], scale=-0.5)
        ACT(tfx[:], xtile[:], AF.Copy, [xn, rsn], [tfn], scale=rstd)
        TT("vector", tfx[:], tfx[:], rows[Gn][:], ALU.mult, [tfn] + rowdeps(Gn), [tfn])
        TT("vector", hbuf[:], tfx[:], rows[Sn][:], ALU.add, [tfn] + rowdeps(Sn), [hname])

    def transpose_to(hbuf, hname, dst, dst_name):
        pb, pbn = psb()
        pbv = pb[:].rearrange("p (k t) -> p k t", k=8)
        for k in range(8):
            TR(pbv[:, k, :], hbuf[:, k * 128:(k + 1) * 128], ident_b[:], [hname, "ident_b"], [(pbn, k)])
        ACT(dst, pbv, AF.Copy, [(pbn, k) for k in range(8)], [dst_name])

    hcT = hT[0]
    for t in range(2):
        norm_mod(ctx[t * 128:(t + 1) * 128, :], "cG1", "cS1", hb[t % 2], f"hb{t % 2}")
        transpose_to(hb[t % 2], f"hb{t % 2}", hcT[:, :, t * 128:(t + 1) * 128], ("hT0", t))
    ps, psn = psf("a")
    for k in range(8):
        MM(ps[:, 0:256], WIN[:, k, 1024:1152], hcT[:, k, 0:256], k == 0, k == 7,
           WINK + [("hT0", 0), ("hT0", 1)], [psn])
    ACT(kcT[:], ps[:, 0:256], AF.Copy, [psn], ["kcT"])
    for t in range(2):
        ps, psn = psf("a")
        for k in range(8):
            MM(ps[:, 0:128], hcT[:, k, t * 128:(t + 1) * 128], WIN[:, k, 1280:1408], k == 0, k == 7,
               [("WIN", "v"), ("hT0", t)], [psn])
        ACT(Vc[:, t, :, 0:64], ps[:, 0:128].rearrange("p (h d) -> p h d", h=2), AF.Copy, [psn], [("Vc", t)])

    if "ctx" in dbg:
        d1 = dbg_out("kcT", [128, 256], BF16)
        d2 = dbg_out("Vc", [128, 2 * 2 * 65], BF16)
        e1 = DMA("sync", "d_dbg", d1, kcT[:], ["kcT"], ["dbg1"])
        e2 = DMA("sync", "d_dbg", d2, Vc[:].rearrange("p a b c -> p (a b c)"), [("Vc", 0), ("Vc", 1), ("Vc", "ones")], ["dbg2"])
        P.finish("sync", [e1, e2])
    if stage <= 2:
        P.emit()
        return nc, dbg_outs

    chunks = [(0, 128, False)] + [(128 + 512 * i, 512, True) for i in range(4)] + [(2176, 128, False)]

    def prep_norm(ci, tiles):
        w0, n, central = chunks[ci]
        for t in tiles:
            norm_mod(x[w0 + t * 128:w0 + (t + 1) * 128, :], "G1", "S1", hb[t % 2], f"hb{t % 2}")

    def prep_trans(ci, tiles):
        hTc, hTn = hT[ci % 2], f"hT{ci % 2}"
        for t in tiles:
            transpose_to(hb[t % 2], f"hb{t % 2}", hTc[:, :, t * 128:(t + 1) * 128], (hTn, t))

    def build_items(ci):
        w0, n, central = chunks[ci]
        hTc, hTn = hT[ci % 2], f"hT{ci % 2}"
        ntile = n // 128
        hdeps = [(hTn, t) for t in range(ntile)]
        items = []

        def proj(col0, wdeps, cons):
            ps, psn = psf(cons)
            for k in range(8):
                MM(ps[:, 0:n], WIN[:, k, col0:col0 + 128], hTc[:, k, 0:n], k == 0, k == 7, wdeps + hdeps, [psn])
            return ps, psn

        def rope_out(col0, colsw, wd, wsd, dst, dstn):
            def f():
                pa, pan = proj(col0, wd, "v")
                pb_, pbn_ = proj(colsw, wsd, "v")
                TT("vector", rt1[:, 0:n], pa[:, 0:n], COS[:, w0:w0 + n], ALU.mult, [pan, "COS"], ["rt1"])
                TT("vector", rt2[:, 0:n], pb_[:, 0:n], SINS[:, w0:w0 + n], ALU.mult, [pbn_, "SINS"], ["rt2"])
                TT("vector", dst, rt1[:, 0:n], rt2[:, 0:n], ALU.add, ["rt1", "rt2"], [dstn])
            return f

        def v_item(t):
            def f():
                ps, psn = psf("a")
                for k in range(8):
                    MM(ps[:, 0:128], hTc[:, k, t * 128:(t + 1) * 128], WIN[:, k, 1280:1408], k == 0, k == 7,
                       [("WIN", "v"), (hTn, t)], [psn])
                wt = w0 // 128 + t
                ACT(Vt[:, wt, :, 0:64], ps[:, 0:128].rearrange("p (h d) -> p h d", h=2), AF.Copy, [psn], [("Vt", wt)])
            return f

        def bg_item(c):
            def f():
                ps, psn = proj(1408 + c * 128, [("WIN", "bg")], "a")
                ACT(bgT[:, c, w0 - 128:w0 - 128 + n], ps[:, 0:n], AF.Copy, [psn], [("bgT", c, ci)])
            return f

        def u_item(c):
            def f():
                pc, pcn = proj(1920 + c * 128, [("WIN", "cg")], "a")
                ph, phn = proj(2432 + c * 128, [("WIN", "hv")], "v")
                ACT(cgs[:, 0:n], pc[:, 0:n], AF.Copy, [pcn], ["cgs"])
                TT("vector", uT[:, c, w0:w0 + n], ph[:, 0:n], cgs[:, 0:n], ALU.mult, [phn, "cgs"], [("uT", c, ci)])
            return f

        if central:
            for c in range(4):
                items.append(rope_out(c * 128, 512 + c * 128, WINQ, WINQS, qT[:, c, w0:w0 + n], ("qT", c, ci)))
        items.append(rope_out(1024, 1152, WINK, WINKS, kT[:, w0:w0 + n], ("kT", ci)))
        for t in range(ntile):
            items.append(v_item(t))
        for c in range(4):
            if central:
                items.append(bg_item(c))
            items.append(u_item(c))
        return items

    def tiles_of(ci):
        return list(range(chunks[ci][1] // 128))

    prep_norm(0, tiles_of(0))
    prep_trans(0, tiles_of(0))
    for ci in range(len(chunks)):
        items = build_items(ci)
        nxt = ci + 1 if ci + 1 < len(chunks) else None
        half = (len(items) + 1) // 2
        if nxt is not None:
            prep_norm(nxt, tiles_of(nxt)[0:2])
        for f in items[:half]:
            f()
        if nxt is not None:
            prep_trans(nxt, tiles_of(nxt)[0:2])
            prep_norm(nxt, tiles_of(nxt)[2:4])
        for f in items[half:]:
            f()
        if nxt is not None:
            prep_trans(nxt, tiles_of(nxt)[2:4])

    if "proj" in dbg:
        d1 = dbg_out("qT", [128, 4 * W], BF16)
        d2 = dbg_out("kT", [128, W], BF16)
        d3 = dbg_out("Vt", [128, 18 * 130], BF16)
        d4 = dbg_out("uT", [128, 4 * W], BF16)
        d5 = dbg_out("bgT", [128, 4 * 2048], BF16)
        allq = [("qT", c, ci) for c in range(4) for ci in range(1, 5)]
        allk = [("kT", ci) for ci in range(6)]
        allv = [("Vt", t) for t in range(18)] + [("Vt", "ones")]
        allu = [("uT", c, ci) for c in range(4) for ci in range(6)]
        allb = [("bgT", c, ci) for c in range(4) for ci in range(1, 5)]
        evs = [DMA("sync", "d_dbg", d1, qT[:].rearrange("p a b -> p (a b)"), allq, ["dbg1"]),
               DMA("sync", "d_dbg", d2, kT[:], allk, ["dbg2"]),
               DMA("sync", "d_dbg", d3, Vt[:].rearrange("p a b c -> p (a b c)"), allv, ["dbg3"]),
               DMA("sync", "d_dbg", d4, uT[:].rearrange("p a b -> p (a b)"), allu, ["dbg4"]),
               DMA("sync", "d_dbg", d5, bgT[:].rearrange("p a b -> p (a b)"), allb, ["dbg5"])]
        P.finish("sync", evs)
    if stage <= 3:
        P.emit()
        return nc, dbg_outs

    ALLWIN = WINQ + WINQS + WINK + WINKS + [("WIN", nm) for nm in ("v", "bg", "cg", "hv")]
    o2 = REG0 + 32768
    PL = [P.sb(f"PL{i}", [128, 384], BF16, off=o2 + i * 768) for i in range(2)]; o2 += 1536
    PC = [P.sb(f"PC{i}", [128, 256], BF16, off=o2 + i * 512) for i in range(2)]; o2 += 1024
    att_tm = P.sb("att_tm", [128, 512], BF16, off=o2); o2 += 1024
    rec = P.sb("rec", [128, 8], F32, off=o2); o2 += 64
    cvt = [P.sb(f"cvt{i}", [128, 512], F32, off=o2 + i * 2048) for i in range(2)]; o2 += 4096
    assert o2 <= REG0 + 47104

    def kchunk(wb):
        return 0 if wb == 0 else (5 if wb == 17 else 1 + (wb - 1) // 4)

    VONES = [("Vt", "ones")]
    TS("vector", uT[:, :, 127:128], uT[:, :, 127:128], metat[:, 0:1], None, ALU.mult, None,
       [("uT", c, 0) for c in range(4)] + ["metat"], [("uT", c, 0) for c in range(4)])
    TS("vector", uT[:, :, 2176:2177], uT[:, :, 2176:2177], metat[:, 1:2], None, ALU.mult, None,
       [("uT", c, 5) for c in range(4)] + ["metat"], [("uT", c, 5) for c in range(4)])

    def conv_unit(tcn, c):
        w0 = 128 + tcn * 512
        ud = [("uT", c, ci) for ci in (tcn, tcn + 1, tcn + 2)]
        cwd = [("cw", kk, c4) for kk in range(3) for c4 in range(4)]
        TS("vector", cvt[0][:], uT[:, c, w0 - 1:w0 + 511], cw[:, c, 0:1], None, ALU.mult, None, ud + cwd, ["cvt0"] + ALLWIN)
        TS("vector", cvt[1][:], uT[:, c, w0:w0 + 512], cw[:, c, 1:2], None, ALU.mult, None, ud + cwd, ["cvt1"] + ALLWIN)
        TT("vector", cvt[0][:], cvt[0][:], cvt[1][:], ALU.add, ["cvt0", "cvt1"], ["cvt0"])
        TS("vector", cvt[1][:], uT[:, c, w0 + 1:w0 + 513], cw[:, c, 2:3], None, ALU.mult, None, ud + cwd, ["cvt1"])
        TT("vector", cvt[0][:], cvt[0][:], cvt[1][:], ALU.add, ["cvt0", "cvt1"], ["cvt0"])
        TT("vector", mixT[:, 4 + c, tcn * 512:(tcn + 1) * 512], cvt[0][:], bgT[:, c, tcn * 512:(tcn + 1) * 512], ALU.mult,
           ["cvt0", ("bgT", c, tcn + 1)], [("mixT", "conv", c, tcn)] + ALLWIN)

    for i in range(1, 17):
        ci_q = 1 + (i - 1) // 4
        mv = 1 if i == 1 else (2 if i == 16 else 0)
        pvs = [psf("v"), psf("v")]
        for hn in range(8):
            half, c = hn // 4, hn % 4
            r0 = half * 64
            j = hn % 2
            sl, sln = psf("a")
            sc_, scn = psf("a")
            qsl = qT[r0:r0 + 64, c, i * 128:(i + 1) * 128]
            for kb in range(3):
                wb = i - 1 + kb
                MM(sl[:, kb * 128:(kb + 1) * 128], kT[r0:r0 + 64, wb * 128:(wb + 1) * 128], qsl, True, True,
                   [("qT", c, ci_q), ("kT", kchunk(wb))], [sln])
            for cb in range(2):
                MM(sc_[:, cb * 128:(cb + 1) * 128], kcT[r0:r0 + 64, cb * 128:(cb + 1) * 128], qsl, True, True,
                   [("qT", c, ci_q), "kcT"], [scn])
            ACT(PL[j][:], sl[:, 0:384], AF.Exp, [sln], [f"PL{j}"] + ALLWIN, scale=0.125)
            ACT(PC[j][:], sc_[:, 0:256], AF.Exp, [scn], [f"PC{j}"] + ALLWIN, scale=0.125)
            TT("vector", PL[j][:], PL[j][:], mask3[:, mv, :], ALU.mult,
               [f"PL{j}", ("mask3", mv), ("mask3", mv, 1), ("mask3", mv, 2)], [f"PL{j}"])
            pv, pvn = pvs[half]
            pvr = pv[:, c * 65:(c + 1) * 65]
            for kb in range(3):
                wb = i - 1 + kb
                MM(pvr, PL[j][:, kb * 128:(kb + 1) * 128], Vt[:, wb, half, :], kb == 0, False,
                   [f"PL{j}", ("Vt", wb)] + VONES, [pvn])
            for cb in range(2):
                MM(pvr, PC[j][:, cb * 128:(cb + 1) * 128], Vc[:, cb, half, :], False, cb == 1,
                   [f"PC{j}", ("Vc", cb), ("Vc", "ones")], [pvn])
        for b in range(2):
            pv, pvn = pvs[b]
            pvv = pv[:, 0:260].rearrange("p (h e) -> p h e", h=4)
            TT("vector", rec[:, b * 4:(b + 1) * 4].unsqueeze(2), pvv[:, :, 64:65], esink[:, b * 4:(b + 1) * 4].unsqueeze(2),
               ALU.add, [pvn, "esink"], [("rec", b)] + ALLWIN)
            P.op("vector", (lambda b=b: (lambda e: e.reciprocal(rec[:, b * 4:(b + 1) * 4], rec[:, b * 4:(b + 1) * 4])))(),
                 [("rec", b)], [("rec", b)])
            TT("vector", att_tm[:, b * 256:(b + 1) * 256].rearrange("p (h d) -> p h d", h=4), pvv[:, :, 0:64],
               rec[:, b * 4:(b + 1) * 4].unsqueeze(2).to_broadcast([128, 4, 64]), ALU.mult,
               [pvn, ("rec", b)], [("att_tm", b)] + ALLWIN)
        pb, pbn = psb()
        pbv = pb[:, 0:512].rearrange("p (k t) -> p k t", k=4)
        for cc in range(4):
            TR(pbv[:, cc, :], att_tm[:, cc * 128:(cc + 1) * 128], ident_b[:], [("att_tm", cc // 2), "ident_b"], [(pbn, cc)])
        ACT(mixT[:, 0:4, (i - 1) * 128:i * 128], pbv, AF.Copy, [(pbn, cc) for cc in range(4)],
            [("mixT", "att", i - 1)] + ALLWIN)
        conv_unit((i - 1) // 4, (i - 1) % 4)

    if stage <= 4:
        P.emit()
        return nc, dbg_outs

    HTALL = [(f"hT{a}", t) for a in range(2) for t in range(4)]
    P.dma("gpsimd", "d_wo", lambda e: e.dma_start(out=wo[:], in_=w_out.rearrange("(k p) n -> p k n", p=128)), [], ["wo"] + HTALL)
    QALL = [("qT", c, ci) for c in range(4) for ci in range(1, 5)]
    o3 = qT_off
    h2T = [P.sb(f"h2T{i}", [128, 8, 128], BF16, off=o3 + i * 2048) for i in range(2)]; o3 += 4096
    CONVDEAD = [("uT", c, ci) for c in range(4) for ci in range(6)] + [("bgT", c, ci) for c in range(4) for ci in range(1, 5)]
    rows_off = REG0 - 8 * 4096
    affTM = P.sb("affTM", [128, 16, 16], F32, off=rows_off)
    gm = P.sb("gm", [128, 16, 16], F32, off=rows_off + 1024)
    thr = P.sb("thr", [128, 16], F32, off=rows_off + 2048)
    affT = P.sb("affT", [16, 2048], F32, off=o3); o3 += 8192
    wr = P.sb("wr", [128, 8, 16], BF16, off=o3); o3 += 256
    sm = P.sb("sm", [128, 64], F32, off=o3); o3 += 256
    assert o3 <= qT_off + 4 * W * 2
    P.dma("gpsimd", "d_wr", lambda e: e.dma_start(out=wr[:], in_=w_router.rearrange("(k p) e -> p k e", p=128)), [], ["wr"] + QALL)
    def a3_A(tile):
        tcn = tile // 4
        mdeps = [("mixT", "att", tile)] + [("mixT", "conv", c, tcn) for c in range(4)]
        i = xt_rr[0] % 2
        xt_rr[0] += 1
        xtile, xn = xt[i], f"xt{i}"
        DMA("sync", "d_" + xn, xtile[:], x[128 + tile * 128:256 + tile * 128, :], [], [xn])
        for n in range(2):
            ps, psn = psf("v")
            for k in range(8):
                MM(ps[:], mixT[:, k, tile * 128:(tile + 1) * 128], wo[:, k, n * 512:(n + 1) * 512], k == 0, k == 7,
                   mdeps + ["wo"], [psn])
            TT("vector", tf[:, n * 512:(n + 1) * 512], ps[:], rows["GT1"][:, n * 512:(n + 1) * 512], ALU.mult,
               [psn] + rowdeps("GT1"), ["tf"])
        TT("vector", xtile[:], xtile[:], tf[:], ALU.add, [xn, "tf"], [xn])
        DMA("sync", "d_x1d_" + xn, x1d[tile * 128:(tile + 1) * 128, :], xtile[:], [xn], [("x1d", tile)])
        j = tile % 2
        norm_mod_sb(xtile, xn, "G2", "S2", hb[j], f"hb{j}")
        DMA("sync", f"d_h2loc{j}", h2loc[tile * 128:(tile + 1) * 128, :], hb[j][:], [f"hb{j}"], [("h2loc", tile)])

    def a3_B(tile):
        j = tile % 2
        h2v = h2T[j][:]
        pb, pbn = psb()
        pbv = pb[:].rearrange("p (k t) -> p k t", k=8)
        for k in range(8):
            TR(pbv[:, k, :], hb[j][:, k * 128:(k + 1) * 128], ident_b[:], [f"hb{j}", "ident_b"], [(pbn, k)])
        ACT(h2v, pbv, AF.Copy, [(pbn, k) for k in range(8)], [f"h2T{j}"] + QALL)
        ps, psn = psf("v")
        for k in range(8):
            MM(ps[:, 0:16], h2T[j][:, k, :], wr[:, k, :], k == 0, k == 7, [f"h2T{j}", "wr"], [psn])
        mx, nmx, ssum, ex = sm[:, 0:1], sm[:, 1:2], sm[:, 2:3], sm[:, 16:32]
        af = affTM[:, tile, :]
        RED("vector", mx, ps[:, 0:16], ALU.max, [psn], ["sm_mx"])
        TS("vector", nmx, mx, -1.0, None, ALU.mult, None, ["sm_mx"], ["sm_nmx"])
        ACT(ex, ps[:, 0:16], AF.Exp, [psn, "sm_nmx"], ["sm_ex"], bias=nmx)
        RED("vector", ssum, ex, ALU.add, ["sm_ex"], ["sm_sum"])
        P.op("vector", lambda e: e.reciprocal(sm[:, 2:3], sm[:, 2:3]), ["sm_sum"], ["sm_sum"])
        TS("vector", af, ex, ssum, None, ALU.mult, None, ["sm_ex", "sm_sum"], [("affTM", tile)])
        pt, ptn = psf("a")
        TR(pt[0:16, 0:128], af, ident_f[:], [("affTM", tile), "ident_f"], [ptn])
        ACT(affT[:, tile * 128:(tile + 1) * 128], pt[0:16, 0:128], AF.Copy, [ptn], [("affT", tile)] + QALL)

    a3_A(0)
    for tile in range(16):
        if tile + 1 < 16:
            a3_A(tile + 1)
        a3_B(tile)
    DMA("sync", "d_affloc", affloc, affT[:], [("affT", t) for t in range(16)], ["affloc"])

    if "a3" in dbg:
        d1 = dbg_out("x1", [2048, 1024])
        d2 = dbg_out("aff", [16, 2048])
        d3 = dbg_out("h2", [2048, 1024], BF16)
        e1 = DMA("sync", "d_dbg1", d1, x1d, [("x1d", t) for t in range(16)], ["dbg1"])
        e2 = DMA("sync", "d_dbg2", d2, affloc, ["affloc"], ["dbg2"])
        e3 = DMA("sync", "d_dbg3", d3, h2loc, [("h2loc", t) for t in range(16)], ["dbg3"])
        P.finish("sync", [e1, e2, e3])
    if stage <= 5:
        P.emit()
        return nc, dbg_outs

    FENCE = [k for k in P.res.keys() if not (isinstance(k, str) and (k.startswith("psf") or k in ("ident_b", "ident_f", "ones_b", "metat")))
             and not (isinstance(k, tuple) and k[0] in ("row", "h2T", "affTM", "x1d", "h2loc"))]
    NTB, NITB = 8, 8
    P.dma("gpsimd", "d_ag_aff", lambda e: e.collective_compute("AllGather", ALU.bypass, replica_groups=GROUPS,
                                                               ins=[affloc.opt()], outs=[affall.opt()]),
          ["affloc"], ["affall", "agchain"], inc=1)
    ob = REG0 + 159936 - 0
    ob = bgT_off + 32768
    AFt = P.sb("AFt", [128, 16, 64], F32, off=ob); ob += 4096
    FR = P.sb("FR", [128, 16, NTB], F32, off=ob); ob += 512
    Tt = P.sb("Tt", [128, 16, NTB], F32, off=ob); ob += 512
    tmpa = P.sb("tmpa", [128, 16, NTB], F32, off=ob); ob += 512
    get = P.sb("get", [128, 16, NTB], F32, off=ob); ob += 512
    cntb = P.sb("cntb", [128, 16 * NTB], BF16, off=ob); ob += 256
    lo = P.sb("lo", [128, 16], F32, off=ob); ob += 64
    hi = P.sb("hi", [128, 16], F32, off=ob); ob += 64
    wdt = P.sb("wdt", [128, 16], F32, off=ob); ob += 64
    red = P.sb("red", [128, 16], F32, off=ob); ob += 64
    idxt = P.sb("idxt", [128, 16], I32, off=ob); ob += 64
    assert ob <= rt1_off + 6144
    cmpb = P.sb("cmpb", [128, 16, NTB, 64], BF16, off=REG0 + 49152)
    for r in range(4):
        DMA("sync", "d_AFt", AFt[32 * r:32 * (r + 1), :, :],
            affall[r * 16:(r + 1) * 16, :].rearrange("e (p j) -> p e j", p=32, j=64), ["affall"], [("AFt", r)] + FENCE)
    AFD = [("AFt", r) for r in range(4)]
    P.op("gpsimd", lambda e: e.iota(FR[:], pattern=[[0, 16], [1, NTB]], base=1, channel_multiplier=0,
                                    allow_small_or_imprecise_dtypes=True), (), ["FR"] + FENCE)
    P.op("gpsimd", lambda e: e.iota(idxt[:], pattern=[[128, 16]], base=0, channel_multiplier=1), (), ["idxt"] + FENCE)
    TS("vector", FR[:], FR[:], 1.0 / (NTB + 1), None, ALU.mult, None, ["FR"], ["FR"])
    MSET("vector", lo[:], 0.0, ["lo"] + FENCE)
    MSET("vector", hi[:], 1.0, ["hi"])
    for it in range(NITB):
        TT("vector", wdt[:], hi[:], lo[:], ALU.subtract, ["hi", "lo"], ["wdt"])
        TT("vector", Tt[:], FR[:], wdt[:].unsqueeze(2).to_broadcast([128, 16, NTB]), ALU.mult, ["FR", "wdt"], ["Tt"])
        TT("vector", Tt[:], Tt[:], lo[:].unsqueeze(2).to_broadcast([128, 16, NTB]), ALU.add, ["Tt", "lo"], ["Tt"])
        TT("vector", cmpb[:], AFt[:].unsqueeze(2).to_broadcast([128, 16, NTB, 64]),
           Tt[:].unsqueeze(3).to_broadcast([128, 16, NTB, 64]), ALU.is_ge, AFD + ["Tt"], ["cmpb"] + FENCE)
        RED("vector", tmpa[:], cmpb[:], ALU.add, ["cmpb"], ["tmpa"])
        CP("vector", cntb[:], tmpa[:].rearrange("p e k -> p (e k)"), ["tmpa"], ["cntb"])
        ps, psn = psf("v")
        MM(ps[:, 0:16 * NTB], ones_b[:], cntb[:], True, True, ["cntb", "ones_b"], [psn])
        TS("vector", get[:].rearrange("p e k -> p (e k)"), ps[:, 0:16 * NTB], 1024.0, None, ALU.is_ge, None, [psn], ["get"])
        TT("vector", tmpa[:], Tt[:], get[:], ALU.mult, ["Tt", "get"], ["tmpa"])
        RED("vector", red[:], tmpa[:], ALU.max, ["tmpa"], ["red"])
        TT("vector", lo[:], lo[:], red[:], ALU.max, ["lo", "red"], ["lo"])
        TS("vector", tmpa[:], get[:], 2.0, None, ALU.mult, None, ["get"], ["tmpa"])
        TT("vector", tmpa[:], tmpa[:], Tt[:], ALU.add, ["tmpa", "Tt"], ["tmpa"])
        RED("vector", red[:], tmpa[:], ALU.min, ["tmpa"], ["red"])
        TT("vector", hi[:], hi[:], red[:], ALU.min, ["hi", "red"], ["hi"])
    CP("vector", thr[:], lo[:], ["lo"], ["thr"])
    AFFTM = [("affTM", t) for t in range(16)]
    TT("vector", gm[:], affTM[:], thr[:].unsqueeze(1).to_broadcast([128, 16, 16]), ALU.is_ge, AFFTM + ["thr"], ["gm"])
    TT("vector", gm[:], gm[:], affTM[:], ALU.mult, ["gm"] + AFFTM, ["gm"])

    if "thr" in dbg:
        d1 = dbg_out("thr", [128, 16])
        d2 = dbg_out("gm", [128, 256])
        e1 = DMA("sync", "d_dbg1", d1, thr[:], ["thr"], ["dbg1"])
        e2 = DMA("sync", "d_dbg2", d2, gm[:].rearrange("p a b -> p (a b)"), ["gm"], ["dbg2"])
        P.finish("sync", [e1, e2])
    if stage <= 6:
        P.emit()
        return nc, dbg_outs

    for j4 in range(4):
        P.dma("gpsimd", "d_ag_h2", (lambda j4=j4: (lambda e: e.collective_compute(
            "AllGather", ALU.bypass, replica_groups=GROUPS,
            ins=[h2loc[j4 * 512:(j4 + 1) * 512, :].opt()], outs=[h2all[j4 * 2048:(j4 + 1) * 2048, :].opt()])))(),
            [("h2loc", t) for t in range(j4 * 4, j4 * 4 + 4)] + ["agchain"], [("h2all", j4), "agchain"], inc=1)
    H2ALLD = [("h2all", j4) for j4 in range(4)]
    TAB = P.sb("TAB", [128, 16, 128], F32, off=qT_off)
    ob2 = kT_off
    ones64 = P.sb("ones64", [128, 64], F32, off=ob2); ob2 += 256
    n4 = P.sb("n4", [128, 4], F32, off=ob2); ob2 += 64
    t16 = P.sb("t16", [128, 16], F32, off=ob2); ob2 += 64
    rhsU = P.sb("rhsU", [128, 4, 128], BF16, off=ob2); ob2 += 1024
    rhsI = P.sb("rhsI", [128, 4, 128], BF16, off=ob2); ob2 += 1024
    offs_sb = P.sb("offs_sb", [128, 4, 128], F32, off=ob2); ob2 += 2048
    nrow_sb = P.sb("nrow_sb", [128, 4, 128], F32, off=ob2); ob2 += 2048
    sval = P.sb("sval", [128, 8], F32, off=ob2); ob2 += 64
    koffs = P.sb("koffs", [128, 4, 8], F32, off=ob2); ob2 += 128
    pS = P.sb("pS", [128, 4, 8], F32, off=ob2); ob2 += 128
    oex = P.sb("oex", [128, 4, 8], F32, off=ob2); ob2 += 128
    rS = P.sb("rS", [128, 4, 8], F32, off=ob2); ob2 += 128
    jS = P.sb("jS", [128, 4, 8], F32, off=ob2); ob2 += 128
    gS = P.sb("gS", [128, 4, 8], F32, off=ob2); ob2 += 128
    tSf = P.sb("tSf", [128, 4, 8], F32, off=ob2); ob2 += 128
    RIDX = P.sb("RIDX", [128, 4, 8], I32, off=ob2); ob2 += 128
    TIDX = P.sb("TIDX", [128, 4, 8], I32, off=ob2); ob2 += 128
    ZIDXf = P.sb("ZIDXf", [128, 4, 16], F32, off=ob2); ob2 += 256
    ZIDX = P.sb("ZIDX", [128, 4, 16], I32, off=ob2); ob2 += 256
    yz = [P.sb(f"yz{i}", [128, 1024], BF16, off=rows_off + 3 * 4096 + i * 2048) for i in range(2)]
    assert ob2 <= bgT_off, ob2
    cmpP = P.sb("cmpP", [128, 4, 8, 128], F32, off=REG0 + 32768)
    Gt = P.sb("Gt", [128, 4, 8, 128], F32, off=REG0 + 49152)

    MSET("vector", ones64[:], 1.0, ["ones64"] + FENCE)
    TT("vector", TAB[:, :, 64:128], AFt[:], thr[:].unsqueeze(2).to_broadcast([128, 16, 64]), ALU.is_ge, AFD + ["thr"], ["TABm"] + FENCE)
    for e16 in range(16):
        P.op("vector", (lambda e16=e16: (lambda e: e.tensor_tensor_scan(out=TAB[:, e16, 0:64], data0=ones64[:], data1=TAB[:, e16, 64:128],
                                                                          initial=0.0, op0=ALU.mult, op1=ALU.add)))(),
             ["TABm", "ones64"], [("TABc", e16)])
    TABC = [("TABc", e16) for e16 in range(16)]
    TT("vector", TAB[:, :, 64:128], TAB[:, :, 64:128], AFt[:], ALU.mult, ["TABm"] + TABC + AFD, ["TABm"])
    DMA("sync", "d_tabd", tabd.rearrange("(e p) c -> p e c", p=128), TAB[:], ["TABm"] + TABC, ["tabd"])
    selv = metat[:, 8:72].rearrange("p (e k) -> p e k", k=4)
    for k in range(4):
        TT("vector", t16[:], TAB[:, :, 63], selv[:, :, k], ALU.mult, TABC + ["metat"], ["t16"])
        RED("vector", n4[:, k:k + 1], t16[:], ALU.add, ["t16"], [("n4", k)])
    N4 = [("n4", k) for k in range(4)]
    TT("vector", rhsU[:], n4[:].unsqueeze(2).to_broadcast([128, 4, 128]), U_b[:].unsqueeze(1).to_broadcast([128, 4, 128]), ALU.mult,
       N4 + ["U_b"], ["rhsU"])
    TT("vector", rhsI[:], n4[:].unsqueeze(2).to_broadcast([128, 4, 128]), ident_b[:].unsqueeze(1).to_broadcast([128, 4, 128]), ALU.mult,
       N4 + ["ident_b"], ["rhsI"])
    ps, psn = psf("v")
    MM(ps[:], ones_b[:], rhsU[:].rearrange("p k q -> p (k q)"), True, True, ["rhsU", "ones_b"], [psn])
    CP("vector", offs_sb[:].rearrange("p k q -> p (k q)"), ps[:], [psn], ["offs_sb"])
    ps, psn = psf("v")
    MM(ps[:], ones_b[:], rhsI[:].rearrange("p k q -> p (k q)"), True, True, ["rhsI", "ones_b"], [psn])
    CP("vector", nrow_sb[:].rearrange("p k q -> p (k q)"), ps[:], [psn], ["nrow_sb"])
    P.op("gpsimd", lambda e: e.iota(sval[:], pattern=[[128, 8]], base=0, channel_multiplier=1,
                                    allow_small_or_imprecise_dtypes=True), (), ["sval"])
    P.op("gpsimd", lambda e: e.iota(koffs[:], pattern=[[128, 4], [0, 8]], base=0, channel_multiplier=0,
                                    allow_small_or_imprecise_dtypes=True), (), ["koffs"])
    P.op("gpsimd", lambda e: e.iota(ZIDXf[:], pattern=[[512, 4], [2048, 4], [128, 4]], base=0, channel_multiplier=1,
                                    allow_small_or_imprecise_dtypes=True), (), ["ZIDXf"])
    TS("vector", koffs[:], koffs[:], metat[:, 4:5], None, ALU.add, None, ["koffs", "metat"], ["koffs"])
    TS("vector", ZIDXf[:], ZIDXf[:], metat[:, 5:6], None, ALU.add, None, ["ZIDXf", "metat"], ["ZIDXf"])
    CP("vector", ZIDX[:], ZIDXf[:], ["ZIDXf"], ["ZIDX"])
    svb = sval[:].unsqueeze(1).to_broadcast([128, 4, 8])
    TT("vector", cmpP[:], offs_sb[:].unsqueeze(2).to_broadcast([128, 4, 8, 128]),
       svb.unsqueeze(3).to_broadcast([128, 4, 8, 128]), ALU.is_le, ["offs_sb", "sval"], ["cmpP"] + FENCE)
    RED("vector", pS[:], cmpP[:], ALU.add, ["cmpP"], ["pS"])
    TT("vector", cmpP[:], cmpP[:], nrow_sb[:].unsqueeze(2).to_broadcast([128, 4, 8, 128]), ALU.mult, ["cmpP", "nrow_sb"], ["cmpP"])
    RED("vector", oex[:], cmpP[:], ALU.add, ["cmpP"], ["oex"])
    TT("vector", rS[:], svb, oex[:], ALU.subtract, ["sval", "oex"], ["rS"])
    TT("vector", tSf[:], pS[:], koffs[:], ALU.add, ["pS", "koffs"], ["tSf"])
    CP("vector", RIDX[:], tSf[:], ["tSf"], ["RIDX"])
    for k in range(4):
        for c in range(8):
            P.dma("gpsimd", "d_G", (lambda k=k, c=c: (lambda e: e.indirect_dma_start(
                out=Gt[:, k, c, :], out_offset=None, in_=tabd, in_offset=bass.IndirectOffsetOnAxis(ap=RIDX[:, k, c:c + 1], axis=0))))(),
                ["tabd", "RIDX"], [("Gt", k, c), "cmpb"] if (k == 0 and c == 0) else [("Gt", k, c)])
    GALL = [("Gt", k, c) for k in range(4) for c in range(8)]
    cmpG = cmpP[:, :, :, 0:64]
    TT("vector", cmpG, Gt[:, :, :, 0:64], rS[:].unsqueeze(3).to_broadcast([128, 4, 8, 64]), ALU.is_le, GALL + ["rS"], ["cmpP"])
    RED("vector", jS[:], cmpG, ALU.add, ["cmpP"], ["jS"])
    TS("vector", oex[:], rS[:], 1.0, None, ALU.add, None, ["rS"], ["oex"])
    TT("vector", cmpG, Gt[:, :, :, 0:64], oex[:].unsqueeze(3).to_broadcast([128, 4, 8, 64]), ALU.is_equal, GALL + ["oex"], ["cmpP"])
    TT("vector", cmpG, cmpG, Gt[:, :, :, 64:128], ALU.mult, ["cmpP"] + GALL, ["cmpP"])
    RED("vector", gS[:], cmpG, ALU.add, ["cmpP"], ["gS"])
    TS("vector", tSf[:], pS[:], 64.0, None, ALU.mult, None, ["pS"], ["tSf"])
    TT("vector", tSf[:], tSf[:], jS[:], ALU.add, ["tSf", "jS"], ["tSf"])
    CP("vector", TIDX[:], tSf[:], ["tSf"], ["TIDX"])
    ra = P.sb("ra", [128, 4, 8], F32, off=ob2); rb = P.sb("rb", [128, 4, 8], F32, off=ob2 + 128)
    rj = P.sb("rj", [128, 4, 8], F32, off=ob2 + 256); GIDX = P.sb("GIDX", [128, 4, 8], I32, off=ob2 + 384)
    assert ob2 + 512 <= bgT_off
    TS("vector", ra[:], tSf[:], 2048.0, None, ALU.is_ge, None, ["tSf"], ["ra"])
    for thv in (4096.0, 6144.0):
        TS("vector", rb[:], tSf[:], thv, None, ALU.is_ge, None, ["tSf"], ["rb"])
        TT("vector", ra[:], ra[:], rb[:], ALU.add, ["ra", "rb"], ["ra"])
    TS("vector", rb[:], ra[:], -2048.0, None, ALU.mult, None, ["ra"], ["rb"])
    TT("vector", rb[:], rb[:], tSf[:], ALU.add, ["rb", "tSf"], ["rb"])
    TS("vector", rj[:], rb[:], 512.0, None, ALU.is_ge, None, ["rb"], ["rj"])
    for thv in (1024.0, 1536.0):
        TS("vector", oex[:], rb[:], thv, None, ALU.is_ge, None, ["rb"], ["oex"])
        TT("vector", rj[:], rj[:], oex[:], ALU.add, ["rj", "oex"], ["rj"])
    TT("vector", rj[:], rj[:], ra[:], ALU.subtract, ["rj", "ra"], ["rj"])
    TS("vector", rj[:], rj[:], 1536.0, None, ALU.mult, None, ["rj"], ["rj"])
    TT("vector", rj[:], rj[:], tSf[:], ALU.add, ["rj", "tSf"], ["rj"])
    CP("vector", GIDX[:], rj[:], ["rj"], ["GIDX"])

    if "idx" in dbg:
        d1 = dbg_out("tidx", [128, 32])
        d2 = dbg_out("gS", [128, 32])
        e1 = DMA("sync", "d_dbg1", d1, tSf[:].rearrange("p a b -> p (a b)"), ["tSf", "TIDX"], ["dbg1"])
        e2 = DMA("sync", "d_dbg2", d2, gS[:].rearrange("p a b -> p (a b)"), ["gS"], ["dbg2"])
        P.finish("sync", [e1, e2])
    if stage <= 6.5:
        P.emit()
        return nc, dbg_outs

    wslot = [P.sb(f"wslot{i}", [128, 8, 1024], BF16, off=REG0 + i * 16384) for i in range(4)]
    wdt_ = P.sb("wd_", [128, 8, 1024], BF16, off=REG0 + 81920)
    hid = [P.sb(f"hid{i}", [128, 8, 512], BF16, off=qT_off + i * 8192) for i in range(2)]
    XS = P.sb("XS", [128, 8, 1024], BF16, off=bgT_off)
    xsT = P.sb("xsT", [128, 8, 1024], BF16, off=bgT_off + 16384)
    sgs = P.sb("sgs", [128, 512], F32, off=rt1_off + 4096)
    MSET("vector", XS[:], 0.0, ["XS"] + FENCE)
    ZD0 = []
    for t in range(8):
        ZD0.append(("Zd0", t))
        DMA("sync", "d_z0", Zd[t * 1024:(t + 1) * 1024, :].rearrange("(p c) d -> p c d", c=8), XS[:], ["XS"], [("Zd0", t)])
    wgv = w_gate.rearrange("e (k p) n -> e p k n", p=128)
    wuv = w_up.rearrange("e (k p) n -> e p k n", p=128)
    wdv = w_down.rearrange("e (k p) n -> e p k n", p=128)
    yrr = [0]
    def issue_loads(k4):
        sg_, su_ = (k4 % 2) * 2, (k4 % 2) * 2 + 1
        wg_t, wu_t = wslot[sg_], wslot[su_]
        ex2 = (["cmpP"] if sg_ == 2 else [])
        ex3 = (["cmpb"] + GALL if su_ == 3 else [])
        P.dma("gpsimd", f"d_ws{sg_}", (lambda wg_t=wg_t, k4=k4: (lambda e: e.dma_start(out=wg_t[:], in_=wgv[k4])))(), [], [f"ws{sg_}"] + ex2 + (FENCE if k4 < 2 else []))
        P.dma("gpsimd", f"d_ws{su_}", (lambda wu_t=wu_t, k4=k4: (lambda e: e.dma_start(out=wu_t[:], in_=wuv[k4])))(), [], [f"ws{su_}"] + ex3 + (FENCE if k4 < 2 else []))
        for c in range(8):
            P.dma("gpsimd", f"d_XS{c}", (lambda k4=k4, c=c: (lambda e: e.indirect_dma_start(
                out=XS[:, c, :], out_offset=None, in_=h2all, in_offset=bass.IndirectOffsetOnAxis(ap=GIDX[:, k4, c:c + 1], axis=0))))(),
                H2ALLD + ["GIDX"], [("XS", c)] + (["XS"] if c == 0 else []))

    def issue_wd(k4):
        P.dma("gpsimd", "d_wd", (lambda k4=k4: (lambda e: e.dma_start(out=wdt_[:], in_=wdv[k4])))(), [], ["wd_"] + (FENCE if k4 < 1 else []))

    issue_loads(0)
    issue_wd(0)
    for k4 in range(4):
        sg_, su_ = (k4 % 2) * 2, (k4 % 2) * 2 + 1
        wg_t, wu_t = wslot[sg_], wslot[su_]
        for c in range(8):
            pb, pbn = psb()
            pbv = pb[:].rearrange("p (k t) -> p k t", k=8)
            for kc in range(8):
                TR(pbv[:, kc, :], XS[:, c, kc * 128:(kc + 1) * 128], ident_b[:], [("XS", c), "XS", "ident_b"], [(pbn, kc)])
            ACT(xsT[:, :, c * 128:(c + 1) * 128], pbv, AF.Copy, [(pbn, kc) for kc in range(8)], [("xsT", c)] + (FENCE if k4 == 0 else []))
        if k4 < 3:
            issue_loads(k4 + 1)
        for sch in range(2):
            hd = hid[(k4 * 2 + sch) % 2]
            hdn = f"hid{(k4 * 2 + sch) % 2}"
            xdeps = [("xsT", sch * 4 + t) for t in range(4)]
            for fo in range(8):
                pa, pan = psf("a")
                for kc in range(8):
                    MM(pa[:], wg_t[:, kc, fo * 128:(fo + 1) * 128], xsT[:, kc, sch * 512:(sch + 1) * 512], kc == 0, kc == 7,
                       [f"ws{sg_}"] + xdeps, [pan])
                pu, pun = psf("v")
                for kc in range(8):
                    MM(pu[:], wu_t[:, kc, fo * 128:(fo + 1) * 128], xsT[:, kc, sch * 512:(sch + 1) * 512], kc == 0, kc == 7,
                       [f"ws{su_}"] + xdeps, [pun])
                ACT(sgs[:], pa[:], AF.Silu, [pan], ["sgs"] + (FENCE if k4 == 0 and sch == 0 and fo == 0 else []))
                TT("vector", hd[:, fo, :], pu[:], sgs[:], ALU.mult, [pun, "sgs"], [(hdn, fo)] + (FENCE + ["TABm"] + TABC if k4 == 0 else []))
            for t in range(4):
                c = sch * 4 + t
                yi = yrr[0] % 2
                yrr[0] += 1
                for dn in range(2):
                    py, pyn = psf("v")
                    for kc in range(8):
                        MM(py[:], hd[:, kc, t * 128:(t + 1) * 128], wdt_[:, kc, dn * 512:(dn + 1) * 512], kc == 0, kc == 7,
                           [(hdn, kc), "wd_"], [pyn])
                    TS("vector", yz[yi][:, dn * 512:(dn + 1) * 512], py[:], gS[:, k4, c:c + 1], None, ALU.mult, None,
                       [pyn, "gS"], [f"yz{yi}"])
                P.dma("gpsimd", f"d_sz{yi}", (lambda yi=yi, k4=k4, c=c: (lambda e: e.indirect_dma_start(
                    out=Zd, out_offset=bass.IndirectOffsetOnAxis(ap=TIDX[:, k4, c:c + 1], axis=0), in_=yz[yi][:], in_offset=None,
                    compute_op=ALU.add, oob_is_err=True)))(), [f"yz{yi}", "TIDX", "Zd"] + ZD0, ["Zd"])
        if k4 < 3:
            issue_wd(k4 + 1)

    for j16 in range(16):
        P.dma("gpsimd", "d_ag_z", (lambda j16=j16: (lambda e: e.collective_compute(
            "AllGather", ALU.bypass, replica_groups=GROUPS,
            ins=[Zd[j16 * 512:(j16 + 1) * 512, :].opt()], outs=[Zall[j16 * 2048:(j16 + 1) * 2048, :].opt()])))(),
            ["Zd", "agchain"], [("Zall", j16), "agchain"], inc=1)
    ZALLD = [("Zall", j16) for j16 in range(16)]
    if stage <= 7:
        P.emit()
        return nc, dbg_outs

    gfrow = P.sb("gfrow", [128, 1024], F32, off=rows_off + 4096)
    DMA("sync", "d_gfrow", gfrow[:], g_final.partition_broadcast(128), [], ["gfrow"] + FENCE)
    z4 = [P.sb(f"z4_{i}", [128, 4, 1024], BF16, off=bgT_off + i * 8192) for i in range(2)]
    evs = []
    for tile in range(16):
        i = xt_rr[0] % 2
        xt_rr[0] += 1
        xtile, xn = xt[i], f"xt{i}"
        zi = tile % 2
        DMA("sync", "d_" + xn, xtile[:], x1d[tile * 128:(tile + 1) * 128, :], [("x1d", tile)], [xn])
        for r in range(4):
            P.dma("gpsimd", f"d_z4_{zi}_{r}", (lambda zi=zi, r=r, tile=tile: (lambda e: e.indirect_dma_start(
                out=z4[zi][:, r, :], out_offset=None, in_=Zall, in_offset=bass.IndirectOffsetOnAxis(ap=ZIDX[:, r, tile:tile + 1], axis=0))))(),
                ZALLD + ["ZIDX"], [(f"z4_{zi}", r)] + ([("XS", c) for c in range(8)] + ["XS"] if tile < 2 else []))
        zd = [(f"z4_{zi}", r) for r in range(4)]
        TT("vector", tf[:], z4[zi][:, 0, :], z4[zi][:, 1, :], ALU.add, zd, ["tf"])
        TT("vector", tf[:], tf[:], z4[zi][:, 2, :], ALU.add, zd + ["tf"], ["tf"])
        TT("vector", tf[:], tf[:], z4[zi][:, 3, :], ALU.add, zd + ["tf"], ["tf"])
        TT("vector", tf[:], tf[:], rows["GT2"][:], ALU.mult, ["tf"] + rowdeps("GT2"), ["tf"])
        TT("vector", xtile[:], xtile[:], tf[:], ALU.add, [xn, "tf"], [xn])
        ss = small[:, 16:17]
        rstd = small[:, 17:18]
        ACT(tf[:], xtile[:], AF.Square, [xn], ["tf"])
        RED("vector", ss, tf[:], ALU.add, ["tf"], ["ss"])
        TS("vector", rstd, ss, 1.0 / 1024.0, 1e-6, ALU.mult, ALU.add, ["ss"], ["rstd"])
        ACT(rstd, rstd, AF.Ln, ["rstd"], ["rstd"])
        ACT(rstd, rstd, AF.Exp, ["rstd"], ["rstd"], scale=-0.5)
        ACT(tf[:], xtile[:], AF.Copy, [xn, "rstd"], ["tf"], scale=rstd)
        TT("vector", xtile[:], tf[:], gfrow[:], ALU.mult, ["tf", "gfrow"], [xn])
        evs.append(DMA("sync", "d_out_" + xn, out[tile * 128:(tile + 1) * 128, :], xtile[:], [xn], [("out", tile)]))
    P.finish("sync", evs)
    P.emit()
    return nc, dbg_outs


def make_in_maps(inp):
    x = np.ascontiguousarray(inp["x"], dtype=np.float32)
    maps = []
    for c in range(NCORES):
        b, q = c // 4, c % 4
        t0 = q * 2048
        xw = np.zeros((W, 1024), np.float32)
        lo, hi = t0 - 128, t0 + 2048 + 128
        slo, shi = max(lo, 0), min(hi, 8192)
        xw[slo - lo:shi - lo] = x[b, slo:shi]
        ccv = np.stack([inp["c"][b].reshape(8, 128).T, inp["c_ctx"].reshape(8, 128).T], axis=-1)
        meta = np.zeros((128, 80), np.float32)
        meta[:, 0] = 1.0 if q > 0 else 0.0
        meta[:, 1] = 1.0 if q < 3 else 0.0
        meta[:, 2] = float(q * 32 - 2)
        meta[:, 3] = float(q * 2048)
        meta[:, 4] = float(4 * q * 128)
        meta[:, 5] = float(q * 8192)
        for k in range(4):
            meta[:, 8 + (4 * q + k) * 4 + k] = 1.0
        maps.append({
            "x": xw, "ctx": np.ascontiguousarray(inp["ctx"][b]), "cc": np.ascontiguousarray(ccv.reshape(128, 16)),
            "meta": meta, "w_ada": inp["w_ada"][0], "b_ada": inp["b_ada"][0], "g_mix": inp["g_mix"][0],
            "g_ffn": inp["g_ffn"][0], "g_final": inp["g_final"], "w_in": inp["w_in"][0], "conv_w": inp["conv_w"][0],
            "sink": inp["sink"][0], "w_out": inp["w_out"][0], "w_router": inp["w_router"][0],
            "w_gate": np.ascontiguousarray(inp["w_gate"][0, 4 * q:4 * q + 4]),
            "w_up": np.ascontiguousarray(inp["w_up"][0, 4 * q:4 * q + 4]),
            "w_down": np.ascontiguousarray(inp["w_down"][0, 4 * q:4 * q + 4]),
        })
    return maps


def kernel(**inputs):
    inp = {k: np.asarray(v) for k, v in inputs.items()}
    nc, _ = build_nc()
    res = run_bass_kernel_spmd(nc, make_in_maps(inp), core_ids=list(range(NCORES)))
    outp = np.zeros((2, 8192, 1024), np.float32)
    for c in range(NCORES):
        b, q = c // 4, c % 4
        outp[b, q * 2048:(q + 1) * 2048] = res.results[c]["out"]
    return outp
```

```python
import os
import numpy as np
import concourse.bass as bass
import concourse.mybir as mybir
from concourse.bass_utils import run_bass_kernel_spmd

F32 = mybir.dt.float32
BF16 = mybir.dt.bfloat16
I32 = mybir.dt.int32
ALU = mybir.AluOpType
AF = mybir.ActivationFunctionType
AX = mybir.AxisListType

COMPUTE = ("tensor", "vector", "scalar", "gpsimd")
QUEUES = ("sync",)
NCORES = 8
GROUPS = [[0, 1, 2, 3], [4, 5, 6, 7]]
W = 2304
NT = 16
NIT = 7


class Prog:
    def __init__(self, nc):
        self.nc = nc
        self.streams = {e: [] for e in COMPUTE + QUEUES}
        self.cnt = {e: 0 for e in COMPUTE}
        self.dma_cnt = {}
        self.waited = {}
        self.res = {}
        self.sem_handles = {}
        self.final_events = []
        self.sb_off = 16512
        self.sb_top = 229344

    def sb(self, name, shape, dtype, off=None):
        esz = {F32: 4, BF16: 2, I32: 4}[dtype]
        n = 1
        for s in shape[1:]:
            n *= s
        nbytes = (n * esz + 63) // 64 * 64
        if off is None:
            off = self.sb_off
            self.sb_off += nbytes
        assert off >= 16512 and off + nbytes <= self.sb_top, (name, off, nbytes)
        return self.nc.alloc_sbuf_tensor_at(name, list(shape), dtype, offset=off)

    def _deps(self, reads, writes):
        need = []
        for r in reads:
            st = self.res.get(r)
            if st and st["w"] is not None:
                need.append(st["w"])
        for w in writes:
            st = self.res.get(w)
            if st:
                if st["w"] is not None:
                    need.append(st["w"])
                need.extend(st["r"])
        return need

    def _commit(self, ev, reads, writes):
        for r in reads:
            st = self.res.setdefault(r, {"w": None, "r": []})
            st["r"].append(ev)
        for w in writes:
            self.res[w] = {"w": ev, "r": []}

    def _waits(self, eng, need):
        best = {}
        for (k, v) in need:
            if k == "tensor" and eng == "tensor":
                continue
            if v > best.get(k, 0):
                best[k] = v
        out = []
        for k, v in best.items():
            if self.waited.get((eng, k), 0) >= v:
                continue
            self.waited[(eng, k)] = v
            out.append((k, v))
        return out

    def op(self, eng, fn, reads=(), writes=()):
        need = self._deps(reads, writes)
        waits = self._waits(eng, need)
        self.cnt[eng] += 1
        ev = (eng, self.cnt[eng])
        self.streams[eng].append((waits, fn, (eng, 1)))
        self._commit(ev, reads, writes)
        return ev

    def dma(self, q, sem, fn, reads=(), writes=(), inc=16):
        need = self._deps(reads, writes)
        waits = self._waits(q, need)
        self.dma_cnt[sem] = self.dma_cnt.get(sem, 0) + inc
        ev = (sem, self.dma_cnt[sem])
        self.streams[q].append((waits, fn, (sem, inc)))
        self._commit(ev, reads, writes)
        return ev

    def finish(self, eng, events):
        self.final_events.append((eng, events))

    def check_deadlock(self):
        sem = {}
        pos = {e: 0 for e in self.streams}
        progressed = True
        while progressed:
            progressed = False
            for e, st in self.streams.items():
                while pos[e] < len(st):
                    waits, fn, inc = st[pos[e]]
                    if all(sem.get(k, 0) >= v for (k, v) in waits):
                        sem[inc[0]] = sem.get(inc[0], 0) + inc[1]
                        pos[e] += 1
                        progressed = True
                    else:
                        break
        stuck = {e: (pos[e], len(st), st[pos[e]][0]) for e, st in self.streams.items() if pos[e] < len(st)}
        assert not stuck, ("DEADLOCK", stuck, {k: sem.get(k) for e in stuck for (k, v) in stuck[e][2]})

    def emit(self):
        self.check_deadlock()
        nc = self.nc
        names = set(COMPUTE)
        for e in self.streams:
            for (waits, fn, inc) in self.streams[e]:
                names.add(inc[0])
                for (k, v) in waits:
                    names.add(k)
        for n in sorted(names):
            self.sem_handles[n] = nc.alloc_semaphore("s_" + n)
        H = self.sem_handles
        fin = {}
        for eng, evs in self.final_events:
            fin.setdefault(eng, []).extend(evs)
        with nc.Block() as block:
            def make(ename):
                def body(e):
                    for (waits, fn, inc) in self.streams[ename]:
                        for (k, v) in waits:
                            e.wait_ge(H[k], v)
                        fn(e).then_inc(H[inc[0]], inc[1])
                    best = {}
                    for (k, v) in fin.get(ename, []):
                        best[k] = max(best.get(k, 0), v)
                    for k, v in best.items():
                        e.wait_ge(H[k], v)
                return body
            for ename in self.streams:
                if not self.streams[ename] and ename not in fin:
                    continue
                getattr(block, ename)(make(ename))


def build_nc(stage=99, dbg=()):
    nc = bass.Bass("TRN2", target_bir_lowering=False)
    P = Prog(nc)
    dbg_outs = {}

    def din(name, shape, dt=F32):
        return nc.dram_tensor(name, list(shape), dt, kind="ExternalInput").ap()

    x = din("x", [W, 1024])
    ctx = din("ctx", [256, 1024])
    cc = din("cc", [128, 16])
    meta = din("meta", [128, 80])
    w_ada = din("w_ada", [1024, 6144])
    b_ada = din("b_ada", [6144])
    g_mix = din("g_mix", [1024])
    g_ffn = din("g_ffn", [1024])
    g_final = din("g_final", [1024])
    w_in = din("w_in", [1024, 2304])
    conv_w = din("conv_w", [3, 512])
    sink = din("sink", [8])
    w_out = din("w_out", [1024, 1024])
    w_router = din("w_router", [1024, 16])
    w_gate = din("w_gate", [4, 1024, 1024])
    w_up = din("w_up", [4, 1024, 1024])
    w_down = din("w_down", [4, 1024, 1024])
    out = nc.dram_tensor("out", [2048, 1024], F32, kind="ExternalOutput").ap()

    x1d = nc.dram_tensor("x1d", [2048, 1024], F32).ap()
    h2loc = nc.dram_tensor("h2loc", [2048, 1024], BF16).ap()
    h2all = nc.dram_tensor("h2all", [8192, 1024], BF16).ap()
    affloc = nc.dram_tensor("affloc", [16, 2048], F32).ap()
    affall = nc.dram_tensor("affall", [64, 2048], F32).ap()
    tabd = nc.dram_tensor("tabd", [2048, 128], F32).ap()
    Zd = nc.dram_tensor("Zd", [8192, 1024], BF16).ap()
    Zall = nc.dram_tensor("Zall", [32768, 1024], BF16).ap()

    def dbg_out(name, shape, dt=F32):
        t = nc.dram_tensor("dbg_" + name, list(shape), dt, kind="ExternalOutput").ap()
        dbg_outs[name] = t
        return t

    def ACT(out_, in_, func, r, w, **kw):
        return P.op("scalar", lambda e: e.activation(out=out_, in_=in_, func=func, **kw), r, w)

    def TT(eng, out_, in0, in1, op, r, w):
        return P.op(eng, lambda e: e.tensor_tensor(out=out_, in0=in0, in1=in1, op=op), r, w)

    def TS(eng, out_, in0, s1, s2, op0, op1, r, w):
        if op1 is None:
            return P.op(eng, lambda e: e.tensor_scalar(out=out_, in0=in0, scalar1=s1, scalar2=None, op0=op0), r, w)
        return P.op(eng, lambda e: e.tensor_scalar(out=out_, in0=in0, scalar1=s1, scalar2=s2, op0=op0, op1=op1), r, w)

    def STT(eng, out_, in0, scalar, in1, op0, op1, r, w):
        return P.op(eng, lambda e: e.scalar_tensor_tensor(out=out_, in0=in0, scalar=scalar, in1=in1, op0=op0, op1=op1), r, w)

    def RED(eng, out_, in_, op, r, w):
        return P.op(eng, lambda e: e.tensor_reduce(out=out_, in_=in_, axis=AX.X, op=op), r, w)

    def CP(eng, out_, in_, r, w):
        return P.op(eng, lambda e: e.tensor_copy(out=out_, in_=in_), r, w)

    def MSET(eng, out_, val, w):
        return P.op(eng, lambda e: e.memset(out_, val), (), w)

    def MM(out_, lhsT, rhs, start, stop, r, w):
        return P.op("tensor", lambda e: e.matmul(out_, lhsT, rhs, start=start, stop=stop), r, w)

    def TR(out_, in_, ident, r, w):
        return P.op("tensor", lambda e: e.transpose(out_, in_, ident), r, w)

    def DMA(q, sem, out_, in_, r, w):
        return P.dma(q, sem, lambda e: e.dma_start(out=out_, in_=in_), r, w)

    PSF = [nc.alloc_psum_tensor(f"psf{i}", [128, 512], F32) for i in range(6)]
    PSB = [nc.alloc_psum_tensor(f"psb{i}", [128, 1024], BF16) for i in range(2)]
    psf_rr = {"v": 0, "a": 0}

    def psf(cons):
        i = psf_rr[cons] % 3 + (0 if cons == "v" else 3)
        psf_rr[cons] += 1
        return PSF[i], f"psf{i}"

    psb_rr = [0]

    def psb():
        i = psb_rr[0] % 2
        psb_rr[0] += 1
        return PSB[i], f"psb{i}"

    ident_f = P.sb("ident_f", [128, 128], F32)
    ident_b = P.sb("ident_b", [128, 128], BF16)
    iot = P.sb("iot", [128, 128], F32)
    ones_b = P.sb("ones_b", [128, 128], BF16)
    U_b = P.sb("U_b", [128, 128], BF16)
    UI_b = P.sb("UI_b", [128, 128], BF16)
    mask3 = P.sb("mask3", [128, 3, 384], BF16)
    metat = P.sb("metat", [128, 80], F32)
    esink = P.sb("esink", [128, 8], F32)
    rows = {}
    for nm in ("S1", "G1", "GT1", "S2", "G2", "GT2", "cS1", "cG1"):
        rows[nm] = P.sb("row_" + nm, [128, 1024], F32)
    REG0 = P.sb_off

    P.op("gpsimd", lambda e: e.iota(iot[:], pattern=[[1, 128]], base=0, channel_multiplier=-1,
                                    allow_small_or_imprecise_dtypes=True), (), ["iot"])
    TS("vector", ident_f[:], iot[:], 0.0, None, ALU.is_equal, None, ["iot"], ["ident_f"])
    CP("vector", ident_b[:], ident_f[:], ["ident_f"], ["ident_b"])
    TS("vector", U_b[:], iot[:], 0.0, None, ALU.is_ge, None, ["iot"], ["U_b"])
    MSET("vector", ones_b[:], 1.0, ["ones_b"])
    DMA("sync", "d_meta", metat[:], meta, [], ["metat"])
    DMA("sync", "d_sink", esink[:], sink.partition_broadcast(128), [], ["esink"])
    ACT(esink[:], esink[:], AF.Exp, ["esink"], ["esink"])
    for v in range(3):
        TS("vector", mask3[:, v, 0:128], iot[:], 0.0, None, ALU.is_le, None, ["iot"], [("mask3", v)])
        MSET("vector", mask3[:, v, 128:256], 1.0, [("mask3", v, 1)])
        TS("vector", mask3[:, v, 256:384], iot[:], 0.0, None, ALU.is_ge, None, ["iot"], [("mask3", v, 2)])
    TS("vector", mask3[:, 1, 0:128], mask3[:, 1, 0:128], metat[:, 0:1], None, ALU.mult, None,
       ["metat", ("mask3", 1)], [("mask3", 1)])
    TS("vector", mask3[:, 2, 256:384], mask3[:, 2, 256:384], metat[:, 1:2], None, ALU.mult, None,
       ["metat", ("mask3", 2, 2)], [("mask3", 2, 2)])

    if "const" in dbg:
        d1 = dbg_out("ident", [128, 128])
        d2 = dbg_out("mask3", [128, 3 * 384], BF16)
        d3 = dbg_out("esink", [128, 8])
        e1 = DMA("sync", "d_dbg", d1, ident_f[:], ["ident_f"], ["dbg1"])
        e2 = DMA("sync", "d_dbg", d2, mask3[:].rearrange("p a b -> p (a b)"),
                 [("mask3", v) for v in range(3)] + [("mask3", v, 1) for v in range(3)] + [("mask3", v, 2) for v in range(3)], ["dbg2"])
        e3 = DMA("sync", "d_dbg", d3, esink[:], ["esink"], ["dbg3"])
        P.finish("sync", [e1, e2, e3])
    if stage <= 0:
        P.emit()
        return nc, dbg_outs

    o = REG0
    WIN = P.sb("WIN", [128, 8, 2944], BF16, off=o)
    mixT = P.sb("mixT", [128, 8, 2048], BF16, off=o)
    o += 47104
    COS = P.sb("COS", [128, W], F32, off=o); o += W * 4
    SINS = P.sb("SINS", [128, W], F32, off=o); o += W * 4
    xt = [P.sb(f"xt{i}", [128, 1024], F32, off=o + i * 4096) for i in range(2)]; o += 8192
    tf = P.sb("tf", [128, 1024], F32, off=o); o += 4096
    hb = [P.sb(f"hb{i}", [128, 1024], BF16, off=o + i * 2048) for i in range(2)]; o += 4096
    hT = [P.sb(f"hT{i}", [128, 8, 512], BF16, off=o + i * 8192) for i in range(2)]
    wo = P.sb("wo", [128, 8, 1024], BF16, off=o)
    o += 16384
    qT_off = o
    qT = P.sb("qT", [128, 4, W], BF16, off=o); o += 4 * W * 2
    kT_off = o
    kT = P.sb("kT", [128, W], BF16, off=o); o += W * 2
    Vt = P.sb("Vt", [128, 18, 2, 65], BF16, off=o); o += 4736
    kcT = P.sb("kcT", [128, 256], BF16, off=o); o += 512
    Vc = P.sb("Vc", [128, 2, 2, 65], BF16, off=o); o += 576
    bgT_off = o
    bgT = P.sb("bgT", [128, 4, 2048], BF16, off=o)
    stg = P.sb("stg", [128, 8, 640], F32, off=o)
    o += 20480
    uT_off = o
    uT = P.sb("uT", [128, 4, W], BF16, off=o); o += 4 * W * 2
    rt1_off = o
    rt1 = P.sb("rt1", [128, 512], F32, off=o); o += 2048
    rt2 = P.sb("rt2", [128, 512], F32, off=o); o += 2048
    cgs = P.sb("cgs", [128, 512], F32, off=o); o += 2048
    small = P.sb("small", [128, 64], F32, off=o); o += 256
    cw = P.sb("cw", [128, 4, 3], F32, off=o); o += 64
    assert o <= P.sb_top, o
    A_END = o

    wa = [P.sb("wa0", [128, 8, 1024], BF16, off=qT_off), P.sb("wa1", [128, 8, 1024], BF16, off=uT_off)]
    o = kT_off
    lb = P.sb("lb", [128, 8, 2, 128], BF16, off=o); o += 4096
    brow = P.sb("brow", [128, 1024], F32, off=o); o += 4096
    cct = P.sb("cct", [128, 8, 2], F32, off=o); o += 64
    scl = P.sb("scl", [128, 8, 2], F32, off=o); o += 64
    assert o <= bgT_off
    gmrow = P.sb("gmrow", [128, 1024], F32, off=rt1_off)

    DMA("sync", "d_cc", cct[:], cc.rearrange("p (k v) -> p k v", v=2), [], ["cct"])
    ACT(scl[:], cct[:], AF.Silu, ["cct"], ["scl"])
    for v in range(2):
        CP("vector", lb[:, :, v, :], scl[:, :, v:v + 1].to_broadcast([128, 8, 128]), ["scl"], [("lb", v)])
    if stage <= 0.3:
        d1 = dbg_out("lb", [128, 8 * 2 * 128], BF16)
        e1 = DMA("sync", "d_dbg", d1, lb[:].rearrange("p a b c -> p (a b c)"), [("lb", 0), ("lb", 1)], ["dbg1"])
        P.finish("sync", [e1])
        P.emit()
        return nc, dbg_outs
    w_ada_v = w_ada.rearrange("(k p) n -> p k n", p=128)
    grp = [(0, [("S1", 0), ("cS1", 1)]), (1, [("G1", 0), ("cG1", 1)]), (2, [("GT1", 0)]),
           (3, [("S2", 0)]), (4, [("G2", 0)]), (5, [("GT2", 0)])]
    for gi, (g, uses) in enumerate(grp):
        wb = wa[gi % 2]
        wn = f"wa{gi % 2}"
        P.dma("gpsimd", "d_" + wn, (lambda wb=wb, g=g: (lambda e: e.dma_start(out=wb[:], in_=w_ada_v[:, :, g * 1024:(g + 1) * 1024])))(),
              [], [wn])
        DMA("sync", "d_brow", brow[:], b_ada[g * 1024:(g + 1) * 1024].partition_broadcast(128), [], ["brow"])
        if stage <= 0.5:
            d1 = dbg_out("wa", [128, 8 * 1024], BF16)
            d2 = dbg_out("brow", [128, 1024])
            e1 = DMA("sync", "d_dbg", d1, wb[:].rearrange("p a b -> p (a b)"), [wn], ["dbg1"])
            e2 = DMA("sync", "d_dbg", d2, brow[:], ["brow"], ["dbg2"])
            P.finish("sync", [e1, e2])
            P.emit()
            return nc, dbg_outs
        for (nm, v) in uses:
            for n in range(2):
                ps, psn = psf("v")
                for k in range(8):
                    MM(ps[:], lb[:, k, v, :], wb[:, k, n * 512:(n + 1) * 512], k == 0, k == 7,
                       [("lb", v), wn], [psn])
                TT("vector", rows[nm][:, n * 512:(n + 1) * 512], ps[:], brow[:, n * 512:(n + 1) * 512], ALU.add,
                   [psn, "brow"], [("row", nm, n)])
                if stage <= 0.7:
                    d1 = dbg_out("r0", [128, 512])
                    e1 = DMA("sync", "d_dbg", d1, rows[nm][:, 0:512], [("row", nm, n)], ["dbg1"])
                    P.finish("sync", [e1])
                    P.emit()
                    return nc, dbg_outs
    for (gsrc, names) in (((g_mix, ("G1", "cG1")), (g_ffn, ("G2",))) if stage > 0.8 else ()):
        DMA("sync", "d_gmrow", gmrow[:], gsrc.partition_broadcast(128), [], ["gmrow"])
        for nm in names:
            TS("vector", rows[nm][:], rows[nm][:], 1.0, None, ALU.add, None,
               [("row", nm, 0), ("row", nm, 1)], [("row", nm, 0), ("row", nm, 1)])
            TT("vector", rows[nm][:], rows[nm][:], gmrow[:], ALU.mult,
               [("row", nm, 0), ("row", nm, 1), "gmrow"], [("row", nm, 0), ("row", nm, 1)])

    def rowdeps(nm):
        return [("row", nm, 0), ("row", nm, 1)]

    if "rows" in dbg:
        d = dbg_out("rows", [8, 128, 1024])
        for i, nm in enumerate(("S1", "G1", "GT1", "S2", "G2", "GT2", "cS1", "cG1")):
            ev = DMA("sync", "d_dbg", d[i], rows[nm][:], rowdeps(nm), ["dbg"])
        P.finish("sync", [ev])
    if stage <= 1:
        P.emit()
        return nc, dbg_outs


    def sc(i):
        return small[:, i:i + 1]
    pid, dd, i32_, isC, ff, inv, invC, invR, sgn, tmpc = [sc(i) for i in range(10)]
    P.op("gpsimd", lambda e: e.iota(small[:, 0:1], pattern=[[0, 1]], base=0, channel_multiplier=1,
                                    allow_small_or_imprecise_dtypes=True), (), ["small"])
    TS("vector", tmpc, pid, 64.0, -64.0, ALU.is_ge, ALU.mult, ["small"], ["small"])
    TT("vector", dd, pid, tmpc, ALU.add, ["small"], ["small"])
    TS("vector", sgn, dd, 32.0, None, ALU.is_ge, None, ["small"], ["small"])
    TS("vector", tmpc, sgn, -32.0, None, ALU.mult, None, ["small"], ["small"])
    TT("vector", i32_, dd, tmpc, ALU.add, ["small"], ["small"])
    TS("vector", isC, i32_, 16.0, None, ALU.is_ge, None, ["small"], ["small"])
    TS("vector", tmpc, isC, -16.0, None, ALU.mult, None, ["small"], ["small"])
    TT("vector", ff, i32_, tmpc, ALU.add, ["small"], ["small"])
    ACT(inv, ff, AF.Exp, ["small"], ["small"], scale=-float(np.log(10000.0) / 16.0))
    TT("vector", invC, inv, isC, ALU.mult, ["small"], ["small"])
    TT("vector", invR, inv, invC, ALU.subtract, ["small"], ["small"])
    TS("vector", sgn, sgn, 2.0, -1.0, ALU.mult, ALU.add, ["small"], ["small"])
    rrA = P.sb("rrA", [128, W], F32, off=qT_off)
    rrI = P.sb("rrI", [128, W], I32, off=qT_off + W * 4)
    ang = P.sb("ang", [128, W], F32, off=uT_off)
    P.op("gpsimd", lambda e: e.iota(COS[:], pattern=[[1, 36], [0, 64]], base=0, channel_multiplier=0,
                                    allow_small_or_imprecise_dtypes=True), (), ["COS"])
    P.op("gpsimd", lambda e: e.iota(SINS[:], pattern=[[0, 36], [1, 64]], base=0, channel_multiplier=0,
                                    allow_small_or_imprecise_dtypes=True), (), ["SINS"])
    HW_ = W // 2
    TWO_PI = float(2 * np.pi)
    for hh in range(2):
        sl = slice(hh * HW_, (hh + 1) * HW_)
        TS("vector", COS[:, sl], COS[:, sl], metat[:, 2:3], None, ALU.add, None, ["COS", "metat"], ["COS"])
        TS("vector", COS[:, sl], COS[:, sl], invR, None, ALU.mult, None, ["COS", "small"], ["COS"])
        TS("vector", SINS[:, sl], SINS[:, sl], invC, None, ALU.mult, None, ["SINS", "small"], ["SINS"])
    TT("vector", ang[:], COS[:], SINS[:], ALU.add, ["COS", "SINS"], ["ang"])

    def range_reduce_sin(dst, dstn, offset):
        TS("vector", rrA[:], ang[:], 1.0 / TWO_PI, offset / TWO_PI + 8.5, ALU.mult, ALU.add, ["ang"], ["rrA"])
        CP("vector", rrI[:], rrA[:], ["rrA"], ["rrI"])
        CP("vector", rrA[:], rrI[:], ["rrI"], ["rrA"])
        TS("vector", rrA[:], rrA[:], -TWO_PI, 8 * TWO_PI + offset, ALU.mult, ALU.add, ["rrA"], ["rrA"])
        TT("vector", dst[:], ang[:], rrA[:], ALU.add, ["ang", "rrA"], [dstn])
        TS("vector", rrA[:], dst[:], float(np.pi), -TWO_PI, ALU.is_gt, ALU.mult, [dstn], ["rrA"])
        TT("vector", dst[:], dst[:], rrA[:], ALU.add, [dstn, "rrA"], [dstn])
        TS("vector", rrA[:], dst[:], -float(np.pi), TWO_PI, ALU.is_lt, ALU.mult, [dstn], ["rrA"])
        TT("vector", dst[:], dst[:], rrA[:], ALU.add, [dstn, "rrA"], [dstn])
        ACT(dst[:], dst[:], AF.Sin, [dstn], [dstn])

    range_reduce_sin(SINS, "SINS", 0.0)
    range_reduce_sin(COS, "COS", float(np.pi / 2))
    for hh in range(2):
        sl = slice(hh * HW_, (hh + 1) * HW_)
        TS("vector", SINS[:, sl], SINS[:, sl], sgn, None, ALU.mult, None, ["SINS", "small"], ["SINS"])

    w_in_v = w_in.rearrange("(k p) n -> p k n", p=128)
    DMA("sync", "d_stg", stg[:], w_in_v[:, :, 0:640], [], ["stg"])
    qd = WIN[:, :, 0:512].rearrange("p k (c h d) -> p k c h d", c=4, h=2, d=64)
    qs = stg[:, :, 0:512].rearrange("p k (h c d) -> p k c h d", h=2, c=4, d=64)
    for h in range(2):
        ACT(qd[:, :, :, h, :], qs[:, :, :, h, :], AF.Copy, ["stg"], [("WIN", "q", h)])
    qd2 = WIN[:, :, 512:1024].rearrange("p k (c h s d) -> p k c h s d", c=4, h=2, s=2, d=32)
    qs2 = stg[:, :, 0:512].rearrange("p k (h c s d) -> p k c h s d", h=2, c=4, s=2, d=32)
    for h in range(2):
        for s in range(2):
            ACT(qd2[:, :, :, h, s, :], qs2[:, :, :, h, 1 - s, :], AF.Copy, ["stg"], [("WIN", "qsw", h, s)])
    ACT(WIN[:, :, 1024:1152], stg[:, :, 512:640], AF.Copy, ["stg"], [("WIN", "k")])
    kd2 = WIN[:, :, 1152:1280].rearrange("p k (h s d) -> p k h s d", h=2, s=2, d=32)
    ks2 = stg[:, :, 512:640].rearrange("p k (h s d) -> p k h s d", h=2, s=2, d=32)
    for s in range(2):
        ACT(kd2[:, :, :, s, :], ks2[:, :, :, 1 - s, :], AF.Copy, ["stg"], [("WIN", "ksw", s)])
    WINQ = [("WIN", "q", 0), ("WIN", "q", 1)]
    WINQS = [("WIN", "qsw", h, s) for h in range(2) for s in range(2)]
    WINK = [("WIN", "k")]
    WINKS = [("WIN", "ksw", 0), ("WIN", "ksw", 1)]
    for (nm, d0, s0, n) in (("v", 1280, 640, 128), ("bg", 1408, 768, 512), ("cg", 1920, 1280, 512), ("hv", 2432, 1792, 512)):
        P.dma("gpsimd", "d_win_" + nm, (lambda d0=d0, s0=s0, n=n: (lambda e: e.dma_start(out=WIN[:, :, d0:d0 + n], in_=w_in_v[:, :, s0:s0 + n])))(),
              [], [("WIN", nm)])
    for kk in range(3):
        for c4 in range(4):
            P.dma("sync", "d_cw", (lambda kk=kk, c4=c4: (lambda e: e.dma_start(
                out=cw[:, c4, kk:kk + 1], in_=conv_w[kk, c4 * 128:(c4 + 1) * 128].rearrange("(p o) -> p o", o=1))))(),
                [], [("cw", kk, c4)])
    MSET("vector", Vt[:, :, :, 64:65], 1.0, [("Vt", "ones")])
    MSET("vector", Vc[:, :, :, 64:65], 1.0, [("Vc", "ones")])

    xt_rr = [0]

    def norm_mod(src_rows, Gn, Sn, hbuf, hname, extra_r=()):
        i = xt_rr[0] % 2
        xt_rr[0] += 1
        xtile, xn = xt[i], f"xt{i}"
        DMA("sync", "d_" + xn, xtile[:], src_rows, list(extra_r), [xn])
        norm_mod_sb(xtile, xn, Gn, Sn, hbuf, hname)
        return xtile, xn

    tf2 = P.sb("tf2", [128, 1024], F32, off=bgT_off + 16384)
    nm_rr = [0]

    def norm_mod_sb(xtile, xn, Gn, Sn, hbuf, hname):
        pi = nm_rr[0] % 2
        nm_rr[0] += 1
        tfx, tfn = (tf, "tf") if pi == 0 else (tf2, "tf2")
        ss = small[:, 16 + 2 * pi:17 + 2 * pi]
        rstd = small[:, 17 + 2 * pi:18 + 2 * pi]
        ssn, rsn = f"ss{pi}", f"rstd{pi}"
        ACT(tfx[:], xtile[:], AF.Square, [xn], [tfn])
        RED("vector", ss, tfx[:], ALU.add, [tfn], [ssn])
        TS("vector", rstd, ss, 1.0 / 1024.0, 1e-6, ALU.mult, ALU.add, [ssn], [rsn])
        ACT(rstd, rstd, AF.Ln, [rsn], [rsn])
        ACT(rstd, rstd, AF.Exp, [rsn], [rsn], scale=-0.5)
        ACT(tfx[:], xtile[:], AF.Copy, [xn, rsn], [tfn], scale=rstd)
        TT("vector", tfx[:], tfx[:], rows[Gn][:], ALU.mult, [tfn] + rowdeps(Gn), [tfn])
        TT("vector", hbuf[:], tfx[:], rows[Sn][:], ALU.add, [tfn] + rowdeps(Sn), [hname])

    def transpose_to(hbuf, hname, dst, dst_name):
        pb, pbn = psb()
        pbv = pb[:].rearrange("p (k t) -> p k t", k=8)
        for k in range(8):
            TR(pbv[:, k, :], hbuf[:, k * 128:(k + 1) * 128], ident_b[:], [hname, "ident_b"], [(pbn, k)])
        ACT(dst, pbv, AF.Copy, [(pbn, k) for k in range(8)], [dst_name])

    hcT = hT[0]
    for t in range(2):
        norm_mod(ctx[t * 128:(t + 1) * 128, :], "cG1", "cS1", hb[t % 2], f"hb{t % 2}")
        transpose_to(hb[t % 2], f"hb{t % 2}", hcT[:, :, t * 128:(t + 1) * 128], ("hT0", t))
    ps, psn = psf("a")
    for k in range(8):
        MM(ps[:, 0:256], WIN[:, k, 1024:1152], hcT[:, k, 0:256], k == 0, k == 7,
           WINK + [("hT0", 0), ("hT0", 1)], [psn])
    ACT(kcT[:], ps[:, 0:256], AF.Copy, [psn], ["kcT"])
    for t in range(2):
        ps, psn = psf("a")
        for k in range(8):
            MM(ps[:, 0:128], hcT[:, k, t * 128:(t + 1) * 128], WIN[:, k, 1280:1408], k == 0, k == 7,
               [("WIN", "v"), ("hT0", t)], [psn])
        ACT(Vc[:, t, :, 0:64], ps[:, 0:128].rearrange("p (h d) -> p h d", h=2), AF.Copy, [psn], [("Vc", t)])

    if "ctx" in dbg:
        d1 = dbg_out("kcT", [128, 256], BF16)
        d2 = dbg_out("Vc", [128, 2 * 2 * 65], BF16)
        e1 = DMA("sync", "d_dbg", d1, kcT[:], ["kcT"], ["dbg1"])
        e2 = DMA("sync", "d_dbg", d2, Vc[:].rearrange("p a b c -> p (a b c)"), [("Vc", 0), ("Vc", 1), ("Vc", "ones")], ["dbg2"])
        P.finish("sync", [e1, e2])
    if stage <= 2:
        P.emit()
        return nc, dbg_outs

    chunks = [(0, 128, False)] + [(128 + 512 * i, 512, True) for i in range(4)] + [(2176, 128, False)]

    def prep_norm(ci, tiles):
        w0, n, central = chunks[ci]
        for t in tiles:
            norm_mod(x[w0 + t * 128:w0 + (t + 1) * 128, :], "G1", "S1", hb[t % 2], f"hb{t % 2}")

    def prep_trans(ci, tiles):
        hTc, hTn = hT[ci % 2], f"hT{ci % 2}"
        for t in tiles:
            transpose_to(hb[t % 2], f"hb{t % 2}", hTc[:, :, t * 128:(t + 1) * 128], (hTn, t))

    def build_items(ci):
        w0, n, central = chunks[ci]
        hTc, hTn = hT[ci % 2], f"hT{ci % 2}"
        ntile = n // 128
        hdeps = [(hTn, t) for t in range(ntile)]
        items = []

        def proj(col0, wdeps, cons):
            ps, psn = psf(cons)
            for k in range(8):
                MM(ps[:, 0:n], WIN[:, k, col0:col0 + 128], hTc[:, k, 0:n], k == 0, k == 7, wdeps + hdeps, [psn])
            return ps, psn

        def rope_out(col0, colsw, wd, wsd, dst, dstn):
            def f():
                pa, pan = proj(col0, wd, "v")
                pb_, pbn_ = proj(colsw, wsd, "v")
                TT("vector", rt1[:, 0:n], pa[:, 0:n], COS[:, w0:w0 + n], ALU.mult, [pan, "COS"], ["rt1"])
                TT("vector", rt2[:, 0:n], pb_[:, 0:n], SINS[:, w0:w0 + n], ALU.mult, [pbn_, "SINS"], ["rt2"])
                TT("vector", dst, rt1[:, 0:n], rt2[:, 0:n], ALU.add, ["rt1", "rt2"], [dstn])
            return f

        def v_item(t):
            def f():
                ps, psn = psf("a")
                for k in range(8):
                    MM(ps[:, 0:128], hTc[:, k, t * 128:(t + 1) * 128], WIN[:, k, 1280:1408], k == 0, k == 7,
                       [("WIN", "v"), (hTn, t)], [psn])
                wt = w0 // 128 + t
                ACT(Vt[:, wt, :, 0:64], ps[:, 0:128].rearrange("p (h d) -> p h d", h=2), AF.Copy, [psn], [("Vt", wt)])
            return f

        def bg_item(c):
            def f():
                ps, psn = proj(1408 + c * 128, [("WIN", "bg")], "a")
                ACT(bgT[:, c, w0 - 128:w0 - 128 + n], ps[:, 0:n], AF.Copy, [psn], [("bgT", c, ci)])
            return f

        def u_item(c):
            def f():
                pc, pcn = proj(1920 + c * 128, [("WIN", "cg")], "a")
                ph, phn = proj(2432 + c * 128, [("WIN", "hv")], "v")
                ACT(cgs[:, 0:n], pc[:, 0:n], AF.Copy, [pcn], ["cgs"])
                TT("vector", uT[:, c, w0:w0 + n], ph[:, 0:n], cgs[:, 0:n], ALU.mult, [phn, "cgs"], [("uT", c, ci)])
            return f

        if central:
            for c in range(4):
                items.append(rope_out(c * 128, 512 + c * 128, WINQ, WINQS, qT[:, c, w0:w0 + n], ("qT", c, ci)))
        items.append(rope_out(1024, 1152, WINK, WINKS, kT[:, w0:w0 + n], ("kT", ci)))
        for t in range(ntile):
            items.append(v_item(t))
        for c in range(4):
            if central:
                items.append(bg_item(c))
            items.append(u_item(c))
        return items

    def tiles_of(ci):
        return list(range(chunks[ci][1] // 128))

    prep_norm(0, tiles_of(0))
    prep_trans(0, tiles_of(0))
    for ci in range(len(chunks)):
        items = build_items(ci)
        nxt = ci + 1 if ci + 1 < len(chunks) else None
        half = (len(items) + 1) // 2
        if nxt is not None:
            prep_norm(nxt, tiles_of(nxt)[0:2])
        for f in items[:half]:
            f()
        if nxt is not None:
            prep_trans(nxt, tiles_of(nxt)[0:2])
            prep_norm(nxt, tiles_of(nxt)[2:4])
        for f in items[half:]:
            f()
        if nxt is not None:
            prep_trans(nxt, tiles_of(nxt)[2:4])

    if "proj" in dbg:
        d1 = dbg_out("qT", [128, 4 * W], BF16)
        d2 = dbg_out("kT", [128, W], BF16)
        d3 = dbg_out("Vt", [128, 18 * 130], BF16)
        d4 = dbg_out("uT", [128, 4 * W], BF16)
        d5 = dbg_out("bgT", [128, 4 * 2048], BF16)
        allq = [("qT", c, ci) for c in range(4) for ci in range(1, 5)]
        allk = [("kT", ci) for ci in range(6)]
        allv = [("Vt", t) for t in range(18)] + [("Vt", "ones")]
        allu = [("uT", c, ci) for c in range(4) for ci in range(6)]
        allb = [("bgT", c, ci) for c in range(4) for ci in range(1, 5)]
        evs = [DMA("sync", "d_dbg", d1, qT[:].rearrange("p a b -> p (a b)"), allq, ["dbg1"]),
               DMA("sync", "d_dbg", d2, kT[:], allk, ["dbg2"]),
               DMA("sync", "d_dbg", d3, Vt[:].rearrange("p a b c -> p (a b c)"), allv, ["dbg3"]),
               DMA("sync", "d_dbg", d4, uT[:].rearrange("p a b -> p (a b)"), allu, ["dbg4"]),
               DMA("sync", "d_dbg", d5, bgT[:].rearrange("p a b -> p (a b)"), allb, ["dbg5"])]
        P.finish("sync", evs)
    if stage <= 3:
        P.emit()
        return nc, dbg_outs

    ALLWIN = WINQ + WINQS + WINK + WINKS + [("WIN", nm) for nm in ("v", "bg", "cg", "hv")]
    o2 = REG0 + 32768
    PL = [P.sb(f"PL{i}", [128, 384], BF16, off=o2 + i * 768) for i in range(2)]; o2 += 1536
    PC = [P.sb(f"PC{i}", [128, 256], BF16, off=o2 + i * 512) for i in range(2)]; o2 += 1024
    att_tm = P.sb("att_tm", [128, 512], BF16, off=o2); o2 += 1024
    rec = P.sb("rec", [128, 8], F32, off=o2); o2 += 64
    cvt = [P.sb(f"cvt{i}", [128, 512], F32, off=o2 + i * 2048) for i in range(2)]; o2 += 4096
    assert o2 <= REG0 + 47104

    def kchunk(wb):
        return 0 if wb == 0 else (5 if wb == 17 else 1 + (wb - 1) // 4)

    VONES = [("Vt", "ones")]
    TS("vector", uT[:, :, 127:128], uT[:, :, 127:128], metat[:, 0:1], None, ALU.mult, None,
       [("uT", c, 0) for c in range(4)] + ["metat"], [("uT", c, 0) for c in range(4)])
    TS("vector", uT[:, :, 2176:2177], uT[:, :, 2176:2177], metat[:, 1:2], None, ALU.mult, None,
       [("uT", c, 5) for c in range(4)] + ["metat"], [("uT", c, 5) for c in range(4)])

    def conv_unit(tcn, c):
        w0 = 128 + tcn * 512
        ud = [("uT", c, ci) for ci in (tcn, tcn + 1, tcn + 2)]
        cwd = [("cw", kk, c4) for kk in range(3) for c4 in range(4)]
        TS("vector", cvt[0][:], uT[:, c, w0 - 1:w0 + 511], cw[:, c, 0:1], None, ALU.mult, None, ud + cwd, ["cvt0"] + ALLWIN)
        TS("vector", cvt[1][:], uT[:, c, w0:w0 + 512], cw[:, c, 1:2], None, ALU.mult, None, ud + cwd, ["cvt1"] + ALLWIN)
        TT("vector", cvt[0][:], cvt[0][:], cvt[1][:], ALU.add, ["cvt0", "cvt1"], ["cvt0"])
        TS("vector", cvt[1][:], uT[:, c, w0 + 1:w0 + 513], cw[:, c, 2:3], None, ALU.mult, None, ud + cwd, ["cvt1"])
        TT("vector", cvt[0][:], cvt[0][:], cvt[1][:], ALU.add, ["cvt0", "cvt1"], ["cvt0"])
        TT("vector", mixT[:, 4 + c, tcn * 512:(tcn + 1) * 512], cvt[0][:], bgT[:, c, tcn * 512:(tcn + 1) * 512], ALU.mult,
           ["cvt0", ("bgT", c, tcn + 1)], [("mixT", "conv", c, tcn)] + ALLWIN)

    sbanks = [3, 4, 5, 2]
    srr = [0]

    def sbank():
        bi = sbanks[srr[0] % 4]
        srr[0] += 1
        return PSF[bi], f"psf{bi}"

    for i in range(1, 17):
        ci_q = 1 + (i - 1) // 4
        mv = 1 if i == 1 else (2 if i == 16 else 0)
        pvs = [(PSF[0], "psf0"), (PSF[1], "psf1")]

        def S_(hn, i=i, ci_q=ci_q):
            half, c = hn // 4, hn % 4
            r0 = half * 64
            sl, sln = sbank()
            sc_, scn = sbank()
            qsl = qT[r0:r0 + 64, c, i * 128:(i + 1) * 128]
            for kb in range(3):
                wb = i - 1 + kb
                MM(sl[:, kb * 128:(kb + 1) * 128], kT[r0:r0 + 64, wb * 128:(wb + 1) * 128], qsl, True, True,
                   [("qT", c, ci_q), ("kT", kchunk(wb))], [sln])
            for cb in range(2):
                MM(sc_[:, cb * 128:(cb + 1) * 128], kcT[r0:r0 + 64, cb * 128:(cb + 1) * 128], qsl, True, True,
                   [("qT", c, ci_q), "kcT"], [scn])
            return sl, sln, sc_, scn

        def EPV_(hn, st, i=i, mv=mv, pvs=pvs):
            sl, sln, sc_, scn = st
            half, c = hn // 4, hn % 4
            j = hn % 2
            ACT(PL[j][:], sl[:, 0:384], AF.Exp, [sln], [f"PL{j}"] + ALLWIN, scale=0.125)
            ACT(PC[j][:], sc_[:, 0:256], AF.Exp, [scn], [f"PC{j}"] + ALLWIN, scale=0.125)
            TT("vector", PL[j][:], PL[j][:], mask3[:, mv, :], ALU.mult,
               [f"PL{j}", ("mask3", mv), ("mask3", mv, 1), ("mask3", mv, 2)], [f"PL{j}"])
            pv, pvn = pvs[half]
            pvr = pv[:, c * 65:(c + 1) * 65]
            for kb in range(3):
                wb = i - 1 + kb
                MM(pvr, PL[j][:, kb * 128:(kb + 1) * 128], Vt[:, wb, half, :], kb == 0, False,
                   [f"PL{j}", ("Vt", wb)] + VONES, [pvn])
            for cb in range(2):
                MM(pvr, PC[j][:, cb * 128:(cb + 1) * 128], Vc[:, cb, half, :], False, cb == 1,
                   [f"PC{j}", ("Vc", cb), ("Vc", "ones")], [pvn])

        st = S_(0)
        for hn in range(8):
            nxt = S_(hn + 1) if hn < 7 else None
            EPV_(hn, st)
            st = nxt
        for b in range(2):
            pv, pvn = pvs[b]
            pvv = pv[:, 0:260].rearrange("p (h e) -> p h e", h=4)
            TT("vector", rec[:, b * 4:(b + 1) * 4].unsqueeze(2), pvv[:, :, 64:65], esink[:, b * 4:(b + 1) * 4].unsqueeze(2),
               ALU.add, [pvn, "esink"], [("rec", b)] + ALLWIN)
            P.op("vector", (lambda b=b: (lambda e: e.reciprocal(rec[:, b * 4:(b + 1) * 4], rec[:, b * 4:(b + 1) * 4])))(),
                 [("rec", b)], [("rec", b)])
            TT("vector", att_tm[:, b * 256:(b + 1) * 256].rearrange("p (h d) -> p h d", h=4), pvv[:, :, 0:64],
               rec[:, b * 4:(b + 1) * 4].unsqueeze(2).to_broadcast([128, 4, 64]), ALU.mult,
               [pvn, ("rec", b)], [("att_tm", b)] + ALLWIN)
        pb, pbn = psb()
        pbv = pb[:, 0:512].rearrange("p (k t) -> p k t", k=4)
        for cc in range(4):
            TR(pbv[:, cc, :], att_tm[:, cc * 128:(cc + 1) * 128], ident_b[:], [("att_tm", cc // 2), "ident_b"], [(pbn, cc)])
        ACT(mixT[:, 0:4, (i - 1) * 128:i * 128], pbv, AF.Copy, [(pbn, cc) for cc in range(4)],
            [("mixT", "att", i - 1)] + ALLWIN)
        conv_unit((i - 1) // 4, (i - 1) % 4)

    if stage <= 4:
        P.emit()
        return nc, dbg_outs

    HTALL = [(f"hT{a}", t) for a in range(2) for t in range(4)]
    P.dma("gpsimd", "d_wo", lambda e: e.dma_start(out=wo[:], in_=w_out.rearrange("(k p) n -> p k n", p=128)), [], ["wo"] + HTALL)
    QALL = [("qT", c, ci) for c in range(4) for ci in range(1, 5)]
    o3 = qT_off
    h2T = [P.sb(f"h2T{i}", [128, 8, 128], BF16, off=o3 + i * 2048) for i in range(2)]; o3 += 4096
    CONVDEAD = [("uT", c, ci) for c in range(4) for ci in range(6)] + [("bgT", c, ci) for c in range(4) for ci in range(1, 5)]
    rows_off = REG0 - 8 * 4096
    affTM = P.sb("affTM", [128, 16, 16], F32, off=rows_off)
    gm = P.sb("gm", [128, 16, 16], F32, off=rows_off + 1024)
    thr = P.sb("thr", [128, 16], F32, off=rows_off + 2048)
    affT = P.sb("affT", [16, 2048], F32, off=o3); o3 += 8192
    wr = P.sb("wr", [128, 8, 16], BF16, off=o3); o3 += 256
    sm = P.sb("sm", [128, 64], F32, off=o3); o3 += 256
    assert o3 <= qT_off + 4 * W * 2
    P.dma("gpsimd", "d_wr", lambda e: e.dma_start(out=wr[:], in_=w_router.rearrange("(k p) e -> p k e", p=128)), [], ["wr"] + QALL)
    def a3_A(tile):
        tcn = tile // 4
        mdeps = [("mixT", "att", tile)] + [("mixT", "conv", c, tcn) for c in range(4)]
        i = xt_rr[0] % 2
        xt_rr[0] += 1
        xtile, xn = xt[i], f"xt{i}"
        DMA("sync", "d_" + xn, xtile[:], x[128 + tile * 128:256 + tile * 128, :], [], [xn])
        for n in range(2):
            ps, psn = psf("v")
            for k in range(8):
                MM(ps[:], mixT[:, k, tile * 128:(tile + 1) * 128], wo[:, k, n * 512:(n + 1) * 512], k == 0, k == 7,
                   mdeps + ["wo"], [psn])
            TT("vector", tf[:, n * 512:(n + 1) * 512], ps[:], rows["GT1"][:, n * 512:(n + 1) * 512], ALU.mult,
               [psn] + rowdeps("GT1"), ["tf"])
        TT("vector", xtile[:], xtile[:], tf[:], ALU.add, [xn, "tf"], [xn])
        DMA("sync", "d_x1d_" + xn, x1d[tile * 128:(tile + 1) * 128, :], xtile[:], [xn], [("x1d", tile)])
        j = tile % 2
        norm_mod_sb(xtile, xn, "G2", "S2", hb[j], f"hb{j}")
        DMA("sync", f"d_h2loc{j}", h2loc[tile * 128:(tile + 1) * 128, :], hb[j][:], [f"hb{j}"], [("h2loc", tile)])

    def a3_B(tile):
        j = tile % 2
        h2v = h2T[j][:]
        pb, pbn = psb()
        pbv = pb[:].rearrange("p (k t) -> p k t", k=8)
        for k in range(8):
            TR(pbv[:, k, :], hb[j][:, k * 128:(k + 1) * 128], ident_b[:], [f"hb{j}", "ident_b"], [(pbn, k)])
        ACT(h2v, pbv, AF.Copy, [(pbn, k) for k in range(8)], [f"h2T{j}"] + QALL)
        ps, psn = psf("v")
        for k in range(8):
            MM(ps[:, 0:16], h2T[j][:, k, :], wr[:, k, :], k == 0, k == 7, [f"h2T{j}", "wr"], [psn])
        mx, nmx, ssum, ex = sm[:, 0:1], sm[:, 1:2], sm[:, 2:3], sm[:, 16:32]
        af = affTM[:, tile, :]
        RED("vector", mx, ps[:, 0:16], ALU.max, [psn], ["sm_mx"])
        TS("vector", nmx, mx, -1.0, None, ALU.mult, None, ["sm_mx"], ["sm_nmx"])
        ACT(ex, ps[:, 0:16], AF.Exp, [psn, "sm_nmx"], ["sm_ex"], bias=nmx)
        RED("vector", ssum, ex, ALU.add, ["sm_ex"], ["sm_sum"])
        P.op("vector", lambda e: e.reciprocal(sm[:, 2:3], sm[:, 2:3]), ["sm_sum"], ["sm_sum"])
        TS("vector", af, ex, ssum, None, ALU.mult, None, ["sm_ex", "sm_sum"], [("affTM", tile)])
        pt, ptn = psf("a")
        TR(pt[0:16, 0:128], af, ident_f[:], [("affTM", tile), "ident_f"], [ptn])
        ACT(affT[:, tile * 128:(tile + 1) * 128], pt[0:16, 0:128], AF.Copy, [ptn], [("affT", tile)] + QALL)

    a3_A(0)
    for tile in range(16):
        if tile + 1 < 16:
            a3_A(tile + 1)
        a3_B(tile)
    DMA("sync", "d_affloc", affloc, affT[:], [("affT", t) for t in range(16)], ["affloc"])

    if "a3" in dbg:
        d1 = dbg_out("x1", [2048, 1024])
        d2 = dbg_out("aff", [16, 2048])
        d3 = dbg_out("h2", [2048, 1024], BF16)
        e1 = DMA("sync", "d_dbg1", d1, x1d, [("x1d", t) for t in range(16)], ["dbg1"])
        e2 = DMA("sync", "d_dbg2", d2, affloc, ["affloc"], ["dbg2"])
        e3 = DMA("sync", "d_dbg3", d3, h2loc, [("h2loc", t) for t in range(16)], ["dbg3"])
        P.finish("sync", [e1, e2, e3])
    if stage <= 5:
        P.emit()
        return nc, dbg_outs

    FENCE = [k for k in P.res.keys() if not (isinstance(k, str) and (k.startswith("psf") or k in ("ident_b", "ident_f", "ones_b", "metat")))
             and not (isinstance(k, tuple) and k[0] in ("row", "h2T", "affTM", "x1d", "h2loc"))]
    NTB, NITB = 8, 8
    P.dma("gpsimd", "d_ag_aff", lambda e: e.collective_compute("AllGather", ALU.bypass, replica_groups=GROUPS,
                                                               ins=[affloc.opt()], outs=[affall.opt()]),
          ["affloc"], ["affall", "agchain"], inc=1)
    ob = REG0 + 159936 - 0
    ob = bgT_off + 32768
    AFt = P.sb("AFt", [128, 16, 64], F32, off=ob); ob += 4096
    FR = P.sb("FR", [128, 16, NTB], F32, off=ob); ob += 512
    Tt = P.sb("Tt", [128, 16, NTB], F32, off=ob); ob += 512
    tmpa = P.sb("tmpa", [128, 16, NTB], F32, off=ob); ob += 512
    get = P.sb("get", [128, 16, NTB], F32, off=ob); ob += 512
    cntb = P.sb("cntb", [128, 16 * NTB], BF16, off=ob); ob += 256
    lo = P.sb("lo", [128, 16], F32, off=ob); ob += 64
    hi = P.sb("hi", [128, 16], F32, off=ob); ob += 64
    wdt = P.sb("wdt", [128, 16], F32, off=ob); ob += 64
    red = P.sb("red", [128, 16], F32, off=ob); ob += 64
    idxt = P.sb("idxt", [128, 16], I32, off=ob); ob += 64
    assert ob <= rt1_off + 6144
    cmpb = P.sb("cmpb", [128, 16, NTB, 64], BF16, off=REG0 + 49152)
    for r in range(4):
        DMA("sync", "d_AFt", AFt[32 * r:32 * (r + 1), :, :],
            affall[r * 16:(r + 1) * 16, :].rearrange("e (p j) -> p e j", p=32, j=64), ["affall"], [("AFt", r)] + FENCE)
    AFD = [("AFt", r) for r in range(4)]
    P.op("gpsimd", lambda e: e.iota(FR[:], pattern=[[0, 16], [1, NTB]], base=1, channel_multiplier=0,
                                    allow_small_or_imprecise_dtypes=True), (), ["FR"] + FENCE)
    P.op("gpsimd", lambda e: e.iota(idxt[:], pattern=[[128, 16]], base=0, channel_multiplier=1), (), ["idxt"] + FENCE)
    TS("vector", FR[:], FR[:], 1.0 / (NTB + 1), None, ALU.mult, None, ["FR"], ["FR"])
    MSET("vector", lo[:], 0.0, ["lo"] + FENCE)
    MSET("vector", hi[:], 1.0, ["hi"])
    for it in range(NITB):
        TT("vector", wdt[:], hi[:], lo[:], ALU.subtract, ["hi", "lo"], ["wdt"])
        TT("vector", Tt[:], FR[:], wdt[:].unsqueeze(2).to_broadcast([128, 16, NTB]), ALU.mult, ["FR", "wdt"], ["Tt"])
        TT("vector", Tt[:], Tt[:], lo[:].unsqueeze(2).to_broadcast([128, 16, NTB]), ALU.add, ["Tt", "lo"], ["Tt"])
        TT("vector", cmpb[:], AFt[:].unsqueeze(2).to_broadcast([128, 16, NTB, 64]),
           Tt[:].unsqueeze(3).to_broadcast([128, 16, NTB, 64]), ALU.is_ge, AFD + ["Tt"], ["cmpb"] + FENCE)
        RED("vector", tmpa[:], cmpb[:], ALU.add, ["cmpb"], ["tmpa"])
        CP("vector", cntb[:], tmpa[:].rearrange("p e k -> p (e k)"), ["tmpa"], ["cntb"])
        ps, psn = psf("v")
        MM(ps[:, 0:16 * NTB], ones_b[:], cntb[:], True, True, ["cntb", "ones_b"], [psn])
        TS("vector", get[:].rearrange("p e k -> p (e k)"), ps[:, 0:16 * NTB], 1024.0, None, ALU.is_ge, None, [psn], ["get"])
        TT("vector", tmpa[:], Tt[:], get[:], ALU.mult, ["Tt", "get"], ["tmpa"])
        RED("vector", red[:], tmpa[:], ALU.max, ["tmpa"], ["red"])
        TT("vector", lo[:], lo[:], red[:], ALU.max, ["lo", "red"], ["lo"])
        TS("vector", tmpa[:], get[:], 2.0, None, ALU.mult, None, ["get"], ["tmpa"])
        TT("vector", tmpa[:], tmpa[:], Tt[:], ALU.add, ["tmpa", "Tt"], ["tmpa"])
        RED("vector", red[:], tmpa[:], ALU.min, ["tmpa"], ["red"])
        TT("vector", hi[:], hi[:], red[:], ALU.min, ["hi", "red"], ["hi"])
    CP("vector", thr[:], lo[:], ["lo"], ["thr"])
    AFFTM = [("affTM", t) for t in range(16)]
    TT("vector", gm[:], affTM[:], thr[:].unsqueeze(1).to_broadcast([128, 16, 16]), ALU.is_ge, AFFTM + ["thr"], ["gm"])
    TT("vector", gm[:], gm[:], affTM[:], ALU.mult, ["gm"] + AFFTM, ["gm"])

    if "thr" in dbg:
        d1 = dbg_out("thr", [128, 16])
        d2 = dbg_out("gm", [128, 256])
        e1 = DMA("sync", "d_dbg1", d1, thr[:], ["thr"], ["dbg1"])
        e2 = DMA("sync", "d_dbg2", d2, gm[:].rearrange("p a b -> p (a b)"), ["gm"], ["dbg2"])
        P.finish("sync", [e1, e2])
    if stage <= 6:
        P.emit()
        return nc, dbg_outs

    for j4 in range(4):
        P.dma("gpsimd", "d_ag_h2", (lambda j4=j4: (lambda e: e.collective_compute(
            "AllGather", ALU.bypass, replica_groups=GROUPS,
            ins=[h2loc[j4 * 512:(j4 + 1) * 512, :].opt()], outs=[h2all[j4 * 2048:(j4 + 1) * 2048, :].opt()])))(),
            [("h2loc", t) for t in range(j4 * 4, j4 * 4 + 4)] + ["agchain"], [("h2all", j4), "agchain"], inc=1)
    H2ALLD = [("h2all", j4) for j4 in range(4)]
    TAB = P.sb("TAB", [128, 16, 128], F32, off=qT_off)
    ob2 = kT_off
    ones64 = P.sb("ones64", [128, 64], F32, off=ob2); ob2 += 256
    n4 = P.sb("n4", [128, 4], F32, off=ob2); ob2 += 64
    t16 = P.sb("t16", [128, 16], F32, off=ob2); ob2 += 64
    rhsU = P.sb("rhsU", [128, 4, 128], BF16, off=ob2); ob2 += 1024
    rhsI = P.sb("rhsI", [128, 4, 128], BF16, off=ob2); ob2 += 1024
    offs_sb = P.sb("offs_sb", [128, 4, 128], F32, off=ob2); ob2 += 2048
    nrow_sb = P.sb("nrow_sb", [128, 4, 128], F32, off=ob2); ob2 += 2048
    sval = P.sb("sval", [128, 8], F32, off=ob2); ob2 += 64
    koffs = P.sb("koffs", [128, 4, 8], F32, off=ob2); ob2 += 128
    pS = P.sb("pS", [128, 4, 8], F32, off=ob2); ob2 += 128
    oex = P.sb("oex", [128, 4, 8], F32, off=ob2); ob2 += 128
    rS = P.sb("rS", [128, 4, 8], F32, off=ob2); ob2 += 128
    jS = P.sb("jS", [128, 4, 8], F32, off=ob2); ob2 += 128
    gS = P.sb("gS", [128, 4, 8], F32, off=ob2); ob2 += 128
    tSf = P.sb("tSf", [128, 4, 8], F32, off=ob2); ob2 += 128
    RIDX = P.sb("RIDX", [128, 4, 8], I32, off=ob2); ob2 += 128
    TIDX = P.sb("TIDX", [128, 4, 8], I32, off=ob2); ob2 += 128
    ZIDXf = P.sb("ZIDXf", [128, 4, 16], F32, off=ob2); ob2 += 256
    ZIDX = P.sb("ZIDX", [128, 4, 16], I32, off=ob2); ob2 += 256
    yz = [P.sb(f"yz{i}", [128, 1024], BF16, off=rows_off + 3 * 4096 + i * 2048) for i in range(2)]
    assert ob2 <= bgT_off, ob2
    cmpP = P.sb("cmpP", [128, 4, 8, 128], F32, off=REG0 + 32768)
    Gt = P.sb("Gt", [128, 4, 8, 128], F32, off=REG0 + 49152)

    MSET("vector", ones64[:], 1.0, ["ones64"] + FENCE)
    TT("vector", TAB[:, :, 64:128], AFt[:], thr[:].unsqueeze(2).to_broadcast([128, 16, 64]), ALU.is_ge, AFD + ["thr"], ["TABm"] + FENCE)
    for e16 in range(16):
        P.op("vector", (lambda e16=e16: (lambda e: e.tensor_tensor_scan(out=TAB[:, e16, 0:64], data0=ones64[:], data1=TAB[:, e16, 64:128],
                                                                          initial=0.0, op0=ALU.mult, op1=ALU.add)))(),
             ["TABm", "ones64"], [("TABc", e16)])
    TABC = [("TABc", e16) for e16 in range(16)]
    TT("vector", TAB[:, :, 64:128], TAB[:, :, 64:128], AFt[:], ALU.mult, ["TABm"] + TABC + AFD, ["TABm"])
    DMA("sync", "d_tabd", tabd.rearrange("(e p) c -> p e c", p=128), TAB[:], ["TABm"] + TABC, ["tabd"])
    selv = metat[:, 8:72].rearrange("p (e k) -> p e k", k=4)
    for k in range(4):
        TT("vector", t16[:], TAB[:, :, 63], selv[:, :, k], ALU.mult, TABC + ["metat"], ["t16"])
        RED("vector", n4[:, k:k + 1], t16[:], ALU.add, ["t16"], [("n4", k)])
    N4 = [("n4", k) for k in range(4)]
    TT("vector", rhsU[:], n4[:].unsqueeze(2).to_broadcast([128, 4, 128]), U_b[:].unsqueeze(1).to_broadcast([128, 4, 128]), ALU.mult,
       N4 + ["U_b"], ["rhsU"])
    TT("vector", rhsI[:], n4[:].unsqueeze(2).to_broadcast([128, 4, 128]), ident_b[:].unsqueeze(1).to_broadcast([128, 4, 128]), ALU.mult,
       N4 + ["ident_b"], ["rhsI"])
    ps, psn = psf("v")
    MM(ps[:], ones_b[:], rhsU[:].rearrange("p k q -> p (k q)"), True, True, ["rhsU", "ones_b"], [psn])
    CP("vector", offs_sb[:].rearrange("p k q -> p (k q)"), ps[:], [psn], ["offs_sb"])
    ps, psn = psf("v")
    MM(ps[:], ones_b[:], rhsI[:].rearrange("p k q -> p (k q)"), True, True, ["rhsI", "ones_b"], [psn])
    CP("vector", nrow_sb[:].rearrange("p k q -> p (k q)"), ps[:], [psn], ["nrow_sb"])
    P.op("gpsimd", lambda e: e.iota(sval[:], pattern=[[128, 8]], base=0, channel_multiplier=1,
                                    allow_small_or_imprecise_dtypes=True), (), ["sval"])
    P.op("gpsimd", lambda e: e.iota(koffs[:], pattern=[[128, 4], [0, 8]], base=0, channel_multiplier=0,
                                    allow_small_or_imprecise_dtypes=True), (), ["koffs"])
    P.op("gpsimd", lambda e: e.iota(ZIDXf[:], pattern=[[512, 4], [2048, 4], [128, 4]], base=0, channel_multiplier=1,
                                    allow_small_or_imprecise_dtypes=True), (), ["ZIDXf"])
    TS("vector", koffs[:], koffs[:], metat[:, 4:5], None, ALU.add, None, ["koffs", "metat"], ["koffs"])
    TS("vector", ZIDXf[:], ZIDXf[:], metat[:, 5:6], None, ALU.add, None, ["ZIDXf", "metat"], ["ZIDXf"])
    CP("vector", ZIDX[:], ZIDXf[:], ["ZIDXf"], ["ZIDX"])
    svb = sval[:].unsqueeze(1).to_broadcast([128, 4, 8])
    TT("vector", cmpP[:], offs_sb[:].unsqueeze(2).to_broadcast([128, 4, 8, 128]),
       svb.unsqueeze(3).to_broadcast([128, 4, 8, 128]), ALU.is_le, ["offs_sb", "sval"], ["cmpP"] + FENCE)
    RED("vector", pS[:], cmpP[:], ALU.add, ["cmpP"], ["pS"])
    TT("vector", cmpP[:], cmpP[:], nrow_sb[:].unsqueeze(2).to_broadcast([128, 4, 8, 128]), ALU.mult, ["cmpP", "nrow_sb"], ["cmpP"])
    RED("vector", oex[:], cmpP[:], ALU.add, ["cmpP"], ["oex"])
    TT("vector", rS[:], svb, oex[:], ALU.subtract, ["sval", "oex"], ["rS"])
    TT("vector", tSf[:], pS[:], koffs[:], ALU.add, ["pS", "koffs"], ["tSf"])
    CP("vector", RIDX[:], tSf[:], ["tSf"], ["RIDX"])
    for k in range(4):
        for c in range(8):
            P.dma("gpsimd", "d_G", (lambda k=k, c=c: (lambda e: e.indirect_dma_start(
                out=Gt[:, k, c, :], out_offset=None, in_=tabd, in_offset=bass.IndirectOffsetOnAxis(ap=RIDX[:, k, c:c + 1], axis=0))))(),
                ["tabd", "RIDX"], [("Gt", k, c), "cmpb"] if (k == 0 and c == 0) else [("Gt", k, c)])
    GALL = [("Gt", k, c) for k in range(4) for c in range(8)]
    cmpG = cmpP[:, :, :, 0:64]
    TT("vector", cmpG, Gt[:, :, :, 0:64], rS[:].unsqueeze(3).to_broadcast([128, 4, 8, 64]), ALU.is_le, GALL + ["rS"], ["cmpP"])
    RED("vector", jS[:], cmpG, ALU.add, ["cmpP"], ["jS"])
    TS("vector", oex[:], rS[:], 1.0, None, ALU.add, None, ["rS"], ["oex"])
    TT("vector", cmpG, Gt[:, :, :, 0:64], oex[:].unsqueeze(3).to_broadcast([128, 4, 8, 64]), ALU.is_equal, GALL + ["oex"], ["cmpP"])
    TT("vector", cmpG, cmpG, Gt[:, :, :, 64:128], ALU.mult, ["cmpP"] + GALL, ["cmpP"])
    RED("vector", gS[:], cmpG, ALU.add, ["cmpP"], ["gS"])
    TS("vector", tSf[:], pS[:], 64.0, None, ALU.mult, None, ["pS"], ["tSf"])
    TT("vector", tSf[:], tSf[:], jS[:], ALU.add, ["tSf", "jS"], ["tSf"])
    CP("vector", TIDX[:], tSf[:], ["tSf"], ["TIDX"])
    ra = P.sb("ra", [128, 4, 8], F32, off=ob2); rb = P.sb("rb", [128, 4, 8], F32, off=ob2 + 128)
    rj = P.sb("rj", [128, 4, 8], F32, off=ob2 + 256); GIDX = P.sb("GIDX", [128, 4, 8], I32, off=ob2 + 384)
    assert ob2 + 512 <= bgT_off
    TS("vector", ra[:], tSf[:], 2048.0, None, ALU.is_ge, None, ["tSf"], ["ra"])
    for thv in (4096.0, 6144.0):
        TS("vector", rb[:], tSf[:], thv, None, ALU.is_ge, None, ["tSf"], ["rb"])
        TT("vector", ra[:], ra[:], rb[:], ALU.add, ["ra", "rb"], ["ra"])
    TS("vector", rb[:], ra[:], -2048.0, None, ALU.mult, None, ["ra"], ["rb"])
    TT("vector", rb[:], rb[:], tSf[:], ALU.add, ["rb", "tSf"], ["rb"])
    TS("vector", rj[:], rb[:], 512.0, None, ALU.is_ge, None, ["rb"], ["rj"])
    for thv in (1024.0, 1536.0):
        TS("vector", oex[:], rb[:], thv, None, ALU.is_ge, None, ["rb"], ["oex"])
        TT("vector", rj[:], rj[:], oex[:], ALU.add, ["rj", "oex"], ["rj"])
    TT("vector", rj[:], rj[:], ra[:], ALU.subtract, ["rj", "ra"], ["rj"])
    TS("vector", rj[:], rj[:], 1536.0, None, ALU.mult, None, ["rj"], ["rj"])
    TT("vector", rj[:], rj[:], tSf[:], ALU.add, ["rj", "tSf"], ["rj"])
    CP("vector", GIDX[:], rj[:], ["rj"], ["GIDX"])

    if "idx" in dbg:
        d1 = dbg_out("tidx", [128, 32])
        d2 = dbg_out("gS", [128, 32])
        e1 = DMA("sync", "d_dbg1", d1, tSf[:].rearrange("p a b -> p (a b)"), ["tSf", "TIDX"], ["dbg1"])
        e2 = DMA("sync", "d_dbg2", d2, gS[:].rearrange("p a b -> p (a b)"), ["gS"], ["dbg2"])
        P.finish("sync", [e1, e2])
    if stage <= 6.5:
        P.emit()
        return nc, dbg_outs

    wslot = [P.sb(f"wslot{i}", [128, 8, 1024], BF16, off=REG0 + i * 16384) for i in range(4)]
    wdt_ = P.sb("wd_", [128, 8, 1024], BF16, off=REG0 + 81920)
    hid = [P.sb(f"hid{i}", [128, 8, 512], BF16, off=qT_off + i * 8192) for i in range(2)]
    XS = P.sb("XS", [128, 8, 1024], BF16, off=bgT_off)
    xsT = P.sb("xsT", [128, 8, 1024], BF16, off=bgT_off + 16384)
    sgs = P.sb("sgs", [128, 512], F32, off=rt1_off + 4096)
    MSET("vector", XS[:], 0.0, ["XS"] + FENCE)
    ZD0 = []
    for t in range(8):
        ZD0.append(("Zd0", t))
        DMA("sync", "d_z0", Zd[t * 1024:(t + 1) * 1024, :].rearrange("(p c) d -> p c d", c=8), XS[:], ["XS"], [("Zd0", t)])
    wgv = w_gate.rearrange("e (k p) n -> e p k n", p=128)
    wuv = w_up.rearrange("e (k p) n -> e p k n", p=128)
    wdv = w_down.rearrange("e (k p) n -> e p k n", p=128)
    yrr = [0]
    def issue_loads(k4):
        sg_, su_ = (k4 % 2) * 2, (k4 % 2) * 2 + 1
        wg_t, wu_t = wslot[sg_], wslot[su_]
        ex2 = (["cmpP"] if sg_ == 2 else [])
        ex3 = (["cmpb"] + GALL if su_ == 3 else [])
        P.dma("gpsimd", f"d_ws{sg_}", (lambda wg_t=wg_t, k4=k4: (lambda e: e.dma_start(out=wg_t[:], in_=wgv[k4])))(), [], [f"ws{sg_}"] + ex2 + (FENCE if k4 < 2 else []))
        P.dma("gpsimd", f"d_ws{su_}", (lambda wu_t=wu_t, k4=k4: (lambda e: e.dma_start(out=wu_t[:], in_=wuv[k4])))(), [], [f"ws{su_}"] + ex3 + (FENCE if k4 < 2 else []))
        for c in range(8):
            P.dma("gpsimd", f"d_XS{c}", (lambda k4=k4, c=c: (lambda e: e.indirect_dma_start(
                out=XS[:, c, :], out_offset=None, in_=h2all, in_offset=bass.IndirectOffsetOnAxis(ap=GIDX[:, k4, c:c + 1], axis=0))))(),
                H2ALLD + ["GIDX"], [("XS", c)] + (["XS"] if c == 0 else []))

    def issue_wd(k4):
        P.dma("gpsimd", "d_wd", (lambda k4=k4: (lambda e: e.dma_start(out=wdt_[:], in_=wdv[k4])))(), [], ["wd_"] + (FENCE if k4 < 1 else []))

    issue_loads(0)
    issue_wd(0)
    for k4 in range(4):
        sg_, su_ = (k4 % 2) * 2, (k4 % 2) * 2 + 1
        wg_t, wu_t = wslot[sg_], wslot[su_]
        for c in range(8):
            pb, pbn = psb()
            pbv = pb[:].rearrange("p (k t) -> p k t", k=8)
            for kc in range(8):
                TR(pbv[:, kc, :], XS[:, c, kc * 128:(kc + 1) * 128], ident_b[:], [("XS", c), "XS", "ident_b"], [(pbn, kc)])
            ACT(xsT[:, :, c * 128:(c + 1) * 128], pbv, AF.Copy, [(pbn, kc) for kc in range(8)], [("xsT", c)] + (FENCE if k4 == 0 else []))
        if k4 < 3:
            issue_loads(k4 + 1)
        for sch in range(2):
            hd = hid[(k4 * 2 + sch) % 2]
            hdn = f"hid{(k4 * 2 + sch) % 2}"
            xdeps = [("xsT", sch * 4 + t) for t in range(4)]
            for fo in range(8):
                pa, pan = psf("a")
                for kc in range(8):
                    MM(pa[:], wg_t[:, kc, fo * 128:(fo + 1) * 128], xsT[:, kc, sch * 512:(sch + 1) * 512], kc == 0, kc == 7,
                       [f"ws{sg_}"] + xdeps, [pan])
                pu, pun = psf("v")
                for kc in range(8):
                    MM(pu[:], wu_t[:, kc, fo * 128:(fo + 1) * 128], xsT[:, kc, sch * 512:(sch + 1) * 512], kc == 0, kc == 7,
                       [f"ws{su_}"] + xdeps, [pun])
                ACT(sgs[:], pa[:], AF.Silu, [pan], ["sgs"] + (FENCE if k4 == 0 and sch == 0 and fo == 0 else []))
                TT("vector", hd[:, fo, :], pu[:], sgs[:], ALU.mult, [pun, "sgs"], [(hdn, fo)] + (FENCE + ["TABm"] + TABC if k4 == 0 else []))
            for t in range(4):
                c = sch * 4 + t
                yi = yrr[0] % 2
                yrr[0] += 1
                for dn in range(2):
                    py, pyn = psf("v")
                    for kc in range(8):
                        MM(py[:], hd[:, kc, t * 128:(t + 1) * 128], wdt_[:, kc, dn * 512:(dn + 1) * 512], kc == 0, kc == 7,
                           [(hdn, kc), "wd_"], [pyn])
                    TS("vector", yz[yi][:, dn * 512:(dn + 1) * 512], py[:], gS[:, k4, c:c + 1], None, ALU.mult, None,
                       [pyn, "gS"], [f"yz{yi}"])
                P.dma("gpsimd", f"d_sz{yi}", (lambda yi=yi, k4=k4, c=c: (lambda e: e.indirect_dma_start(
                    out=Zd, out_offset=bass.IndirectOffsetOnAxis(ap=TIDX[:, k4, c:c + 1], axis=0), in_=yz[yi][:], in_offset=None,
                    compute_op=ALU.add, oob_is_err=True)))(), [f"yz{yi}", "TIDX", "Zd"] + ZD0, ["Zd"])
        if k4 < 3:
            issue_wd(k4 + 1)

    for j16 in range(16):
        P.dma("gpsimd", "d_ag_z", (lambda j16=j16: (lambda e: e.collective_compute(
            "AllGather", ALU.bypass, replica_groups=GROUPS,
            ins=[Zd[j16 * 512:(j16 + 1) * 512, :].opt()], outs=[Zall[j16 * 2048:(j16 + 1) * 2048, :].opt()])))(),
            ["Zd", "agchain"], [("Zall", j16), "agchain"], inc=1)
    ZALLD = [("Zall", j16) for j16 in range(16)]
    if stage <= 7:
        P.emit()
        return nc, dbg_outs

    gfrow = P.sb("gfrow", [128, 1024], F32, off=rows_off + 4096)
    DMA("sync", "d_gfrow", gfrow[:], g_final.partition_broadcast(128), [], ["gfrow"] + FENCE)
    z4 = [P.sb(f"z4_{i}", [128, 4, 1024], BF16, off=bgT_off + i * 8192) for i in range(2)]
    evs = []
    for tile in range(16):
        i = xt_rr[0] % 2
        xt_rr[0] += 1
        xtile, xn = xt[i], f"xt{i}"
        zi = tile % 2
        DMA("sync", "d_" + xn, xtile[:], x1d[tile * 128:(tile + 1) * 128, :], [("x1d", tile)], [xn])
        for r in range(4):
            P.dma("gpsimd", f"d_z4_{zi}_{r}", (lambda zi=zi, r=r, tile=tile: (lambda e: e.indirect_dma_start(
                out=z4[zi][:, r, :], out_offset=None, in_=Zall, in_offset=bass.IndirectOffsetOnAxis(ap=ZIDX[:, r, tile:tile + 1], axis=0))))(),
                ZALLD + ["ZIDX"], [(f"z4_{zi}", r)] + ([("XS", c) for c in range(8)] + ["XS"] if tile < 2 else []))
        zd = [(f"z4_{zi}", r) for r in range(4)]
        TT("vector", tf[:], z4[zi][:, 0, :], z4[zi][:, 1, :], ALU.add, zd, ["tf"])
        TT("vector", tf[:], tf[:], z4[zi][:, 2, :], ALU.add, zd + ["tf"], ["tf"])
        TT("vector", tf[:], tf[:], z4[zi][:, 3, :], ALU.add, zd + ["tf"], ["tf"])
        TT("vector", tf[:], tf[:], rows["GT2"][:], ALU.mult, ["tf"] + rowdeps("GT2"), ["tf"])
        TT("vector", xtile[:], xtile[:], tf[:], ALU.add, [xn, "tf"], [xn])
        ss = small[:, 16:17]
        rstd = small[:, 17:18]
        ACT(tf[:], xtile[:], AF.Square, [xn], ["tf"])
        RED("vector", ss, tf[:], ALU.add, ["tf"], ["ss"])
        TS("vector", rstd, ss, 1.0 / 1024.0, 1e-6, ALU.mult, ALU.add, ["ss"], ["rstd"])
        ACT(rstd, rstd, AF.Ln, ["rstd"], ["rstd"])
        ACT(rstd, rstd, AF.Exp, ["rstd"], ["rstd"], scale=-0.5)
        ACT(tf[:], xtile[:], AF.Copy, [xn, "rstd"], ["tf"], scale=rstd)
        TT("vector", xtile[:], tf[:], gfrow[:], ALU.mult, ["tf", "gfrow"], [xn])
        evs.append(DMA("sync", "d_out_" + xn, out[tile * 128:(tile + 1) * 128, :], xtile[:], [xn], [("out", tile)]))
    P.finish("sync", evs)
    P.emit()
    return nc, dbg_outs


def make_in_maps(inp):
    x = np.ascontiguousarray(inp["x"], dtype=np.float32)
    maps = []
    for c in range(NCORES):
        b, q = c // 4, c % 4
        t0 = q * 2048
        xw = np.zeros((W, 1024), np.float32)
        lo, hi = t0 - 128, t0 + 2048 + 128
        slo, shi = max(lo, 0), min(hi, 8192)
        xw[slo - lo:shi - lo] = x[b, slo:shi]
        ccv = np.stack([inp["c"][b].reshape(8, 128).T, inp["c_ctx"].reshape(8, 128).T], axis=-1)
        meta = np.zeros((128, 80), np.float32)
        meta[:, 0] = 1.0 if q > 0 else 0.0
        meta[:, 1] = 1.0 if q < 3 else 0.0
        meta[:, 2] = float(q * 32 - 2)
        meta[:, 3] = float(q * 2048)
        meta[:, 4] = float(4 * q * 128)
        meta[:, 5] = float(q * 8192)
        for k in range(4):
            meta[:, 8 + (4 * q + k) * 4 + k] = 1.0
        maps.append({
            "x": xw, "ctx": np.ascontiguousarray(inp["ctx"][b]), "cc": np.ascontiguousarray(ccv.reshape(128, 16)),
            "meta": meta, "w_ada": inp["w_ada"][0], "b_ada": inp["b_ada"][0], "g_mix": inp["g_mix"][0],
            "g_ffn": inp["g_ffn"][0], "g_final": inp["g_final"], "w_in": inp["w_in"][0], "conv_w": inp["conv_w"][0],
            "sink": inp["sink"][0], "w_out": inp["w_out"][0], "w_router": inp["w_router"][0],
            "w_gate": np.ascontiguousarray(inp["w_gate"][0, 4 * q:4 * q + 4]),
            "w_up": np.ascontiguousarray(inp["w_up"][0, 4 * q:4 * q + 4]),
            "w_down": np.ascontiguousarray(inp["w_down"][0, 4 * q:4 * q + 4]),
        })
    return maps


def kernel(**inputs):
    inp = {k: np.asarray(v) for k, v in inputs.items()}
    nc, _ = build_nc()
    res = run_bass_kernel_spmd(nc, make_in_maps(inp), core_ids=list(range(NCORES)))
    outp = np.zeros((2, 8192, 1024), np.float32)
    for c in range(NCORES):
        b, q = c // 4, c % 4
        outp[b, q * 2048:(q + 1) * 2048] = res.results[c]["out"]
    return outp
```

```python
import os
import numpy as np
import concourse.bass as bass
import concourse.mybir as mybir
from concourse.bass_utils import run_bass_kernel_spmd

F32 = mybir.dt.float32
BF16 = mybir.dt.bfloat16
I32 = mybir.dt.int32
ALU = mybir.AluOpType
AF = mybir.ActivationFunctionType
AX = mybir.AxisListType

COMPUTE = ("tensor", "vector", "scalar", "gpsimd")
QUEUES = ("sync",)
NCORES = 8
GROUPS = [[0, 1, 2, 3], [4, 5, 6, 7]]
W = 2304
NT = 16
NIT = 7


class Prog:
    def __init__(self, nc):
        self.nc = nc
        self.streams = {e: [] for e in COMPUTE + QUEUES}
        self.cnt = {e: 0 for e in COMPUTE}
        self.dma_cnt = {}
        self.waited = {}
        self.res = {}
        self.sem_handles = {}
        self.final_events = []
        self.sb_off = 16512
        self.sb_top = 229344

    def sb(self, name, shape, dtype, off=None):
        esz = {F32: 4, BF16: 2, I32: 4}[dtype]
        n = 1
        for s in shape[1:]:
            n *= s
        nbytes = (n * esz + 63) // 64 * 64
        if off is None:
            off = self.sb_off
            self.sb_off += nbytes
        assert off >= 16512 and off + nbytes <= self.sb_top, (name, off, nbytes)
        return self.nc.alloc_sbuf_tensor_at(name, list(shape), dtype, offset=off)

    def _deps(self, reads, writes):
        need = []
        for r in reads:
            st = self.res.get(r)
            if st and st["w"] is not None:
                need.append(st["w"])
        for w in writes:
            st = self.res.get(w)
            if st:
                if st["w"] is not None:
                    need.append(st["w"])
                need.extend(st["r"])
        return need

    def _commit(self, ev, reads, writes):
        for r in reads:
            st = self.res.setdefault(r, {"w": None, "r": []})
            st["r"].append(ev)
        for w in writes:
            self.res[w] = {"w": ev, "r": []}

    def _waits(self, eng, need):
        best = {}
        for (k, v) in need:
            if k == "tensor" and eng == "tensor":
                continue
            if v > best.get(k, 0):
                best[k] = v
        out = []
        for k, v in best.items():
            if self.waited.get((eng, k), 0) >= v:
                continue
            self.waited[(eng, k)] = v
            out.append((k, v))
        return out

    def op(self, eng, fn, reads=(), writes=()):
        need = self._deps(reads, writes)
        waits = self._waits(eng, need)
        self.cnt[eng] += 1
        ev = (eng, self.cnt[eng])
        self.streams[eng].append((waits, fn, (eng, 1)))
        self._commit(ev, reads, writes)
        return ev

    def dma(self, q, sem, fn, reads=(), writes=(), inc=16):
        need = self._deps(reads, writes)
        waits = self._waits(q, need)
        self.dma_cnt[sem] = self.dma_cnt.get(sem, 0) + inc
        ev = (sem, self.dma_cnt[sem])
        self.streams[q].append((waits, fn, (sem, inc)))
        self._commit(ev, reads, writes)
        return ev

    def finish(self, eng, events):
        self.final_events.append((eng, events))

    def check_deadlock(self):
        sem = {}
        pos = {e: 0 for e in self.streams}
        progressed = True
        while progressed:
            progressed = False
            for e, st in self.streams.items():
                while pos[e] < len(st):
                    waits, fn, inc = st[pos[e]]
                    if all(sem.get(k, 0) >= v for (k, v) in waits):
                        sem[inc[0]] = sem.get(inc[0], 0) + inc[1]
                        pos[e] += 1
                        progressed = True
                    else:
                        break
        stuck = {e: (pos[e], len(st), st[pos[e]][0]) for e, st in self.streams.items() if pos[e] < len(st)}
        assert not stuck, ("DEADLOCK", stuck, {k: sem.get(k) for e in stuck for (k, v) in stuck[e][2]})

    def emit(self):
        self.check_deadlock()
        nc = self.nc
        names = set(COMPUTE)
        for e in self.streams:
            for (waits, fn, inc) in self.streams[e]:
                names.add(inc[0])
                for (k, v) in waits:
                    names.add(k)
        for n in sorted(names):
            self.sem_handles[n] = nc.alloc_semaphore("s_" + n)
        H = self.sem_handles
        fin = {}
        for eng, evs in self.final_events:
            fin.setdefault(eng, []).extend(evs)
        with nc.Block() as block:
            def make(ename):
                def body(e):
                    for (waits, fn, inc) in self.streams[ename]:
                        for (k, v) in waits:
                            e.wait_ge(H[k], v)
                        fn(e).then_inc(H[inc[0]], inc[1])
                    best = {}
                    for (k, v) in fin.get(ename, []):
                        best[k] = max(best.get(k, 0), v)
                    for k, v in best.items():
                        e.wait_ge(H[k], v)
                return body
            for ename in self.streams:
                if not self.streams[ename] and ename not in fin:
                    continue
                getattr(block, ename)(make(ename))


def build_nc(stage=99, dbg=()):
    nc = bass.Bass("TRN2", target_bir_lowering=False)
    P = Prog(nc)
    dbg_outs = {}

    def din(name, shape, dt=F32):
        return nc.dram_tensor(name, list(shape), dt, kind="ExternalInput").ap()

    x = din("x", [W, 1024])
    ctx = din("ctx", [256, 1024])
    cc = din("cc", [128, 16])
    meta = din("meta", [128, 80])
    w_ada = din("w_ada", [1024, 6144])
    b_ada = din("b_ada", [6144])
    g_mix = din("g_mix", [1024])
    g_ffn = din("g_ffn", [1024])
    g_final = din("g_final", [1024])
    w_in = din("w_in", [1024, 2304])
    conv_w = din("conv_w", [3, 512])
    sink = din("sink", [8])
    w_out = din("w_out", [1024, 1024])
    w_router = din("w_router", [1024, 16])
    w_gate = din("w_gate", [4, 1024, 1024])
    w_up = din("w_up", [4, 1024, 1024])
    w_down = din("w_down", [4, 1024, 1024])
    out = nc.dram_tensor("out", [2048, 1024], F32, kind="ExternalOutput").ap()

    x1d = nc.dram_tensor("x1d", [2048, 1024], F32).ap()
    h2loc = nc.dram_tensor("h2loc", [2048, 1024], BF16).ap()
    h2all = nc.dram_tensor("h2all", [8192, 1024], BF16).ap()
    affloc = nc.dram_tensor("affloc", [16, 2048], F32).ap()
    affall = nc.dram_tensor("affall", [64, 2048], F32).ap()
    tabd = nc.dram_tensor("tabd", [2048, 128], F32).ap()
    Zd = nc.dram_tensor("Zd", [8192, 1024], BF16).ap()
    Zall = nc.dram_tensor("Zall", [32768, 1024], BF16).ap()

    def dbg_out(name, shape, dt=F32):
        t = nc.dram_tensor("dbg_" + name, list(shape), dt, kind="ExternalOutput").ap()
        dbg_outs[name] = t
        return t

    def ACT(out_, in_, func, r, w, **kw):
        return P.op("scalar", lambda e: e.activation(out=out_, in_=in_, func=func, **kw), r, w)

    def TT(eng, out_, in0, in1, op, r, w):
        return P.op(eng, lambda e: e.tensor_tensor(out=out_, in0=in0, in1=in1, op=op), r, w)

    def TS(eng, out_, in0, s1, s2, op0, op1, r, w):
        if op1 is None:
            return P.op(eng, lambda e: e.tensor_scalar(out=out_, in0=in0, scalar1=s1, scalar2=None, op0=op0), r, w)
        return P.op(eng, lambda e: e.tensor_scalar(out=out_, in0=in0, scalar1=s1, scalar2=s2, op0=op0, op1=op1), r, w)

    def STT(eng, out_, in0, scalar, in1, op0, op1, r, w):
        return P.op(eng, lambda e: e.scalar_tensor_tensor(out=out_, in0=in0, scalar=scalar, in1=in1, op0=op0, op1=op1), r, w)

    def RED(eng, out_, in_, op, r, w):
        return P.op(eng, lambda e: e.tensor_reduce(out=out_, in_=in_, axis=AX.X, op=op), r, w)

    def CP(eng, out_, in_, r, w):
        return P.op(eng, lambda e: e.tensor_copy(out=out_, in_=in_), r, w)

    def MSET(eng, out_, val, w):
        return P.op(eng, lambda e: e.memset(out_, val), (), w)

    def MM(out_, lhsT, rhs, start, stop, r, w):
        return P.op("tensor", lambda e: e.matmul(out_, lhsT, rhs, start=start, stop=stop), r, w)

    def TR(out_, in_, ident, r, w):
        return P.op("tensor", lambda e: e.transpose(out_, in_, ident), r, w)

    def DMA(q, sem, out_, in_, r, w):
        return P.dma(q, sem, lambda e: e.dma_start(out=out_, in_=in_), r, w)

    PSF = [nc.alloc_psum_tensor(f"psf{i}", [128, 512], F32) for i in range(6)]
    PSB = [nc.alloc_psum_tensor(f"psb{i}", [128, 1024], BF16) for i in range(2)]
    psf_rr = {"v": 0, "a": 0}

    def psf(cons):
        i = psf_rr[cons] % 3 + (0 if cons == "v" else 3)
        psf_rr[cons] += 1
        return PSF[i], f"psf{i}"

    psb_rr = [0]

    def psb():
        i = psb_rr[0] % 2
        psb_rr[0] += 1
        return PSB[i], f"psb{i}"

    ident_f = P.sb("ident_f", [128, 128], F32)
    ident_b = P.sb("ident_b", [128, 128], BF16)
    iot = P.sb("iot", [128, 128], F32)
    ones_b = P.sb("ones_b", [128, 128], BF16)
    U_b = P.sb("U_b", [128, 128], BF16)
    UI_b = P.sb("UI_b", [128, 128], BF16)
    mask3 = P.sb("mask3", [128, 3, 384], BF16)
    metat = P.sb("metat", [128, 80], F32)
    esink = P.sb("esink", [128, 8], F32)
    rows = {}
    for nm in ("S1", "G1", "GT1", "S2", "G2", "GT2", "cS1", "cG1"):
        rows[nm] = P.sb("row_" + nm, [128, 1024], F32)
    REG0 = P.sb_off

    P.op("gpsimd", lambda e: e.iota(iot[:], pattern=[[1, 128]], base=0, channel_multiplier=-1,
                                    allow_small_or_imprecise_dtypes=True), (), ["iot"])
    TS("vector", ident_f[:], iot[:], 0.0, None, ALU.is_equal, None, ["iot"], ["ident_f"])
    CP("vector", ident_b[:], ident_f[:], ["ident_f"], ["ident_b"])
    TS("vector", U_b[:], iot[:], 0.0, None, ALU.is_ge, None, ["iot"], ["U_b"])
    MSET("vector", ones_b[:], 1.0, ["ones_b"])
    DMA("sync", "d_meta", metat[:], meta, [], ["metat"])
    DMA("sync", "d_sink", esink[:], sink.partition_broadcast(128), [], ["esink"])
    ACT(esink[:], esink[:], AF.Exp, ["esink"], ["esink"])
    for v in range(3):
        TS("vector", mask3[:, v, 0:128], iot[:], 0.0, None, ALU.is_le, None, ["iot"], [("mask3", v)])
        MSET("vector", mask3[:, v, 128:256], 1.0, [("mask3", v, 1)])
        TS("vector", mask3[:, v, 256:384], iot[:], 0.0, None, ALU.is_ge, None, ["iot"], [("mask3", v, 2)])
    TS("vector", mask3[:, 1, 0:128], mask3[:, 1, 0:128], metat[:, 0:1], None, ALU.mult, None,
       ["metat", ("mask3", 1)], [("mask3", 1)])
    TS("vector", mask3[:, 2, 256:384], mask3[:, 2, 256:384], metat[:, 1:2], None, ALU.mult, None,
       ["metat", ("mask3", 2, 2)], [("mask3", 2, 2)])

    if "const" in dbg:
        d1 = dbg_out("ident", [128, 128])
        d2 = dbg_out("mask3", [128, 3 * 384], BF16)
        d3 = dbg_out("esink", [128, 8])
        e1 = DMA("sync", "d_dbg", d1, ident_f[:], ["ident_f"], ["dbg1"])
        e2 = DMA("sync", "d_dbg", d2, mask3[:].rearrange("p a b -> p (a b)"),
                 [("mask3", v) for v in range(3)] + [("mask3", v, 1) for v in range(3)] + [("mask3", v, 2) for v in range(3)], ["dbg2"])
        e3 = DMA("sync", "d_dbg", d3, esink[:], ["esink"], ["dbg3"])
        P.finish("sync", [e1, e2, e3])
    if stage <= 0:
        P.emit()
        return nc, dbg_outs

    o = REG0
    WIN = P.sb("WIN", [128, 8, 2944], BF16, off=o)
    mixT = P.sb("mixT", [128, 8, 2048], BF16, off=o)
    o += 47104
    COS = P.sb("COS", [128, W], F32, off=o); o += W * 4
    SINS = P.sb("SINS", [128, W], F32, off=o); o += W * 4
    xt = [P.sb(f"xt{i}", [128, 1024], F32, off=o + i * 4096) for i in range(2)]; o += 8192
    tf = P.sb("tf", [128, 1024], F32, off=o); o += 4096
    hb = [P.sb(f"hb{i}", [128, 1024], BF16, off=o + i * 2048) for i in range(2)]; o += 4096
    hT = [P.sb(f"hT{i}", [128, 8, 512], BF16, off=o + i * 8192) for i in range(2)]
    wo = P.sb("wo", [128, 8, 1024], BF16, off=o)
    o += 16384
    qT_off = o
    qT = P.sb("qT", [128, 4, W], BF16, off=o); o += 4 * W * 2
    kT_off = o
    kT = P.sb("kT", [128, W], BF16, off=o); o += W * 2
    Vt = P.sb("Vt", [128, 18, 2, 65], BF16, off=o); o += 4736
    kcT = P.sb("kcT", [128, 256], BF16, off=o); o += 512
    Vc = P.sb("Vc", [128, 2, 2, 65], BF16, off=o); o += 576
    bgT_off = o
    bgT = P.sb("bgT", [128, 4, 2048], BF16, off=o)
    stg = P.sb("stg", [128, 8, 640], F32, off=o)
    o += 20480
    uT_off = o
    uT = P.sb("uT", [128, 4, W], BF16, off=o); o += 4 * W * 2
    rt1_off = o
    rt1 = P.sb("rt1", [128, 512], F32, off=o); o += 2048
    rt2 = P.sb("rt2", [128, 512], F32, off=o); o += 2048
    cgs = P.sb("cgs", [128, 512], F32, off=o); o += 2048
    small = P.sb("small", [128, 64], F32, off=o); o += 256
    cw = P.sb("cw", [128, 4, 3], F32, off=o); o += 64
    assert o <= P.sb_top, o
    A_END = o

    wa = [P.sb("wa0", [128, 8, 1024], BF16, off=qT_off), P.sb("wa1", [128, 8, 1024], BF16, off=uT_off)]
    o = kT_off
    lb = P.sb("lb", [128, 8, 2, 128], BF16, off=o); o += 4096
    brow = P.sb("brow", [128, 1024], F32, off=o); o += 4096
    cct = P.sb("cct", [128, 8, 2], F32, off=o); o += 64
    scl = P.sb("scl", [128, 8, 2], F32, off=o); o += 64
    assert o <= bgT_off
    gmrow = P.sb("gmrow", [128, 1024], F32, off=rt1_off)

    DMA("sync", "d_cc", cct[:], cc.rearrange("p (k v) -> p k v", v=2), [], ["cct"])
    ACT(scl[:], cct[:], AF.Silu, ["cct"], ["scl"])
    for v in range(2):
        CP("vector", lb[:, :, v, :], scl[:, :, v:v + 1].to_broadcast([128, 8, 128]), ["scl"], [("lb", v)])
    if stage <= 0.3:
        d1 = dbg_out("lb", [128, 8 * 2 * 128], BF16)
        e1 = DMA("sync", "d_dbg", d1, lb[:].rearrange("p a b c -> p (a b c)"), [("lb", 0), ("lb", 1)], ["dbg1"])
        P.finish("sync", [e1])
        P.emit()
        return nc, dbg_outs
    w_ada_v = w_ada.rearrange("(k p) n -> p k n", p=128)
    grp = [(0, [("S1", 0), ("cS1", 1)]), (1, [("G1", 0), ("cG1", 1)]), (2, [("GT1", 0)]),
           (3, [("S2", 0)]), (4, [("G2", 0)]), (5, [("GT2", 0)])]
    for gi, (g, uses) in enumerate(grp):
        wb = wa[gi % 2]
        wn = f"wa{gi % 2}"
        P.dma("gpsimd", "d_" + wn, (lambda wb=wb, g=g: (lambda e: e.dma_start(out=wb[:], in_=w_ada_v[:, :, g * 1024:(g + 1) * 1024])))(),
              [], [wn])
        DMA("sync", "d_brow", brow[:], b_ada[g * 1024:(g + 1) * 1024].partition_broadcast(128), [], ["brow"])
        if stage <= 0.5:
            d1 = dbg_out("wa", [128, 8 * 1024], BF16)
            d2 = dbg_out("brow", [128, 1024])
            e1 = DMA("sync", "d_dbg", d1, wb[:].rearrange("p a b -> p (a b)"), [wn], ["dbg1"])
            e2 = DMA("sync", "d_dbg", d2, brow[:], ["brow"], ["dbg2"])
            P.finish("sync", [e1, e2])
            P.emit()
            return nc, dbg_outs
        for (nm, v) in uses:
            for n in range(2):
                ps, psn = psf("v")
                for k in range(8):
                    MM(ps[:], lb[:, k, v, :], wb[:, k, n * 512:(n + 1) * 512], k == 0, k == 7,
                       [("lb", v), wn], [psn])
                TT("vector", rows[nm][:, n * 512:(n + 1) * 512], ps[:], brow[:, n * 512:(n + 1) * 512], ALU.add,
                   [psn, "brow"], [("row", nm, n)])
                if stage <= 0.7:
                    d1 = dbg_out("r0", [128, 512])
                    e1 = DMA("sync", "d_dbg", d1, rows[nm][:, 0:512], [("row", nm, n)], ["dbg1"])
                    P.finish("sync", [e1])
                    P.emit()
                    return nc, dbg_outs
    for (gsrc, names) in (((g_mix, ("G1", "cG1")), (g_ffn, ("G2",))) if stage > 0.8 else ()):
        DMA("sync", "d_gmrow", gmrow[:], gsrc.partition_broadcast(128), [], ["gmrow"])
        for nm in names:
            TS("vector", rows[nm][:], rows[nm][:], 1.0, None, ALU.add, None,
               [("row", nm, 0), ("row", nm, 1)], [("row", nm, 0), ("row", nm, 1)])
            TT("vector", rows[nm][:], rows[nm][:], gmrow[:], ALU.mult,
               [("row", nm, 0), ("row", nm, 1), "gmrow"], [("row", nm, 0), ("row", nm, 1)])

    def rowdeps(nm):
        return [("row", nm, 0), ("row", nm, 1)]

    if "rows" in dbg:
        d = dbg_out("rows", [8, 128, 1024])
        for i, nm in enumerate(("S1", "G1", "GT1", "S2", "G2", "GT2", "cS1", "cG1")):
            ev = DMA("sync", "d_dbg", d[i], rows[nm][:], rowdeps(nm), ["dbg"])
        P.finish("sync", [ev])
    if stage <= 1:
        P.emit()
        return nc, dbg_outs


    def sc(i):
        return small[:, i:i + 1]
    pid, dd, i32_, isC, ff, inv, invC, invR, sgn, tmpc = [sc(i) for i in range(10)]
    P.op("gpsimd", lambda e: e.iota(small[:, 0:1], pattern=[[0, 1]], base=0, channel_multiplier=1,
                                    allow_small_or_imprecise_dtypes=True), (), ["small"])
    TS("vector", tmpc, pid, 64.0, -64.0, ALU.is_ge, ALU.mult, ["small"], ["small"])
    TT("vector", dd, pid, tmpc, ALU.add, ["small"], ["small"])
    TS("vector", sgn, dd, 32.0, None, ALU.is_ge, None, ["small"], ["small"])
    TS("vector", tmpc, sgn, -32.0, None, ALU.mult, None, ["small"], ["small"])
    TT("vector", i32_, dd, tmpc, ALU.add, ["small"], ["small"])
    TS("vector", isC, i32_, 16.0, None, ALU.is_ge, None, ["small"], ["small"])
    TS("vector", tmpc, isC, -16.0, None, ALU.mult, None, ["small"], ["small"])
    TT("vector", ff, i32_, tmpc, ALU.add, ["small"], ["small"])
    ACT(inv, ff, AF.Exp, ["small"], ["small"], scale=-float(np.log(10000.0) / 16.0))
    TT("vector", invC, inv, isC, ALU.mult, ["small"], ["small"])
    TT("vector", invR, inv, invC, ALU.subtract, ["small"], ["small"])
    TS("vector", sgn, sgn, 2.0, -1.0, ALU.mult, ALU.add, ["small"], ["small"])
    rrA = P.sb("rrA", [128, W], F32, off=qT_off)
    rrI = P.sb("rrI", [128, W], I32, off=qT_off + W * 4)
    ang = P.sb("ang", [128, W], F32, off=uT_off)
    P.op("gpsimd", lambda e: e.iota(COS[:], pattern=[[1, 36], [0, 64]], base=0, channel_multiplier=0,
                                    allow_small_or_imprecise_dtypes=True), (), ["COS"])
    P.op("gpsimd", lambda e: e.iota(SINS[:], pattern=[[0, 36], [1, 64]], base=0, channel_multiplier=0,
                                    allow_small_or_imprecise_dtypes=True), (), ["SINS"])
    HW_ = W // 2
    TWO_PI = float(2 * np.pi)
    for hh in range(2):
        sl = slice(hh * HW_, (hh + 1) * HW_)
        TS("vector", COS[:, sl], COS[:, sl], metat[:, 2:3], None, ALU.add, None, ["COS", "metat"], ["COS"])
        TS("vector", COS[:, sl], COS[:, sl], invR, None, ALU.mult, None, ["COS", "small"], ["COS"])
        TS("vector", SINS[:, sl], SINS[:, sl], invC, None, ALU.mult, None, ["SINS", "small"], ["SINS"])
    TT("vector", ang[:], COS[:], SINS[:], ALU.add, ["COS", "SINS"], ["ang"])

    def range_reduce_sin(dst, dstn, offset):
        TS("vector", rrA[:], ang[:], 1.0 / TWO_PI, offset / TWO_PI + 8.5, ALU.mult, ALU.add, ["ang"], ["rrA"])
        CP("vector", rrI[:], rrA[:], ["rrA"], ["rrI"])
        CP("vector", rrA[:], rrI[:], ["rrI"], ["rrA"])
        TS("vector", rrA[:], rrA[:], -TWO_PI, 8 * TWO_PI + offset, ALU.mult, ALU.add, ["rrA"], ["rrA"])
        TT("vector", dst[:], ang[:], rrA[:], ALU.add, ["ang", "rrA"], [dstn])
        TS("vector", rrA[:], dst[:], float(np.pi), -TWO_PI, ALU.is_gt, ALU.mult, [dstn], ["rrA"])
        TT("vector", dst[:], dst[:], rrA[:], ALU.add, [dstn, "rrA"], [dstn])
        TS("vector", rrA[:], dst[:], -float(np.pi), TWO_PI, ALU.is_lt, ALU.mult, [dstn], ["rrA"])
        TT("vector", dst[:], dst[:], rrA[:], ALU.add, [dstn, "rrA"], [dstn])
        ACT(dst[:], dst[:], AF.Sin, [dstn], [dstn])

    range_reduce_sin(SINS, "SINS", 0.0)
    range_reduce_sin(COS, "COS", float(np.pi / 2))
    for hh in range(2):
        sl = slice(hh * HW_, (hh + 1) * HW_)
        TS("vector", SINS[:, sl], SINS[:, sl], sgn, None, ALU.mult, None, ["SINS", "small"], ["SINS"])

    w_in_v = w_in.rearrange("(k p) n -> p k n", p=128)
    DMA("sync", "d_stg", stg[:], w_in_v[:, :, 0:640], [], ["stg"])
    qd = WIN[:, :, 0:512].rearrange("p k (c h d) -> p k c h d", c=4, h=2, d=64)
    qs = stg[:, :, 0:512].rearrange("p k (h c d) -> p k c h d", h=2, c=4, d=64)
    for h in range(2):
        ACT(qd[:, :, :, h, :], qs[:, :, :, h, :], AF.Copy, ["stg"], [("WIN", "q", h)])
    qd2 = WIN[:, :, 512:1024].rearrange("p k (c h s d) -> p k c h s d", c=4, h=2, s=2, d=32)
    qs2 = stg[:, :, 0:512].rearrange("p k (h c s d) -> p k c h s d", h=2, c=4, s=2, d=32)
    for h in range(2):
        for s in range(2):
            ACT(qd2[:, :, :, h, s, :], qs2[:, :, :, h, 1 - s, :], AF.Copy, ["stg"], [("WIN", "qsw", h, s)])
    ACT(WIN[:, :, 1024:1152], stg[:, :, 512:640], AF.Copy, ["stg"], [("WIN", "k")])
    kd2 = WIN[:, :, 1152:1280].rearrange("p k (h s d) -> p k h s d", h=2, s=2, d=32)
    ks2 = stg[:, :, 512:640].rearrange("p k (h s d) -> p k h s d", h=2, s=2, d=32)
    for s in range(2):
        ACT(kd2[:, :, :, s, :], ks2[:, :, :, 1 - s, :], AF.Copy, ["stg"], [("WIN", "ksw", s)])
    WINQ = [("WIN", "q", 0), ("WIN", "q", 1)]
    WINQS = [("WIN", "qsw", h, s) for h in range(2) for s in range(2)]
    WINK = [("WIN", "k")]
    WINKS = [("WIN", "ksw", 0), ("WIN", "ksw", 1)]
    for (nm, d0, s0, n) in (("v", 1280, 640, 128), ("bg", 1408, 768, 512), ("cg", 1920, 1280, 512), ("hv", 2432, 1792, 512)):
        P.dma("gpsimd", "d_win_" + nm, (lambda d0=d0, s0=s0, n=n: (lambda e: e.dma_start(out=WIN[:, :, d0:d0 + n], in_=w_in_v[:, :, s0:s0 + n])))(),
              [], [("WIN", nm)])
    for kk in range(3):
        for c4 in range(4):
            P.dma("sync", "d_cw", (lambda kk=kk, c4=c4: (lambda e: e.dma_start(
                out=cw[:, c4, kk:kk + 1], in_=conv_w[kk, c4 * 128:(c4 + 1) * 128].rearrange("(p o) -> p o", o=1))))(),
                [], [("cw", kk, c4)])
    MSET("vector", Vt[:, :, :, 64:65], 1.0, [("Vt", "ones")])
    MSET("vector", Vc[:, :, :, 64:65], 1.0, [("Vc", "ones")])

    xt_rr = [0]

    def norm_mod(src_rows, Gn, Sn, hbuf, hname, extra_r=()):
        i = xt_rr[0] % 2
        xt_rr[0] += 1
        xtile, xn = xt[i], f"xt{i}"
        DMA("sync", "d_" + xn, xtile[:], src_rows, list(extra_r), [xn])
        norm_mod_sb(xtile, xn, Gn, Sn, hbuf, hname)
        return xtile, xn

    tf2 = P.sb("tf2", [128, 1024], F32, off=bgT_off + 16384)
    nm_rr = [0]

    def norm_mod_sb(xtile, xn, Gn, Sn, hbuf, hname):
        pi = nm_rr[0] % 2
        nm_rr[0] += 1
        tfx, tfn = (tf, "tf") if pi == 0 else (tf2, "tf2")
        ss = small[:, 16 + 2 * pi:17 + 2 * pi]
        rstd = small[:, 17 + 2 * pi:18 + 2 * pi]
        ssn, rsn = f"ss{pi}", f"rstd{pi}"
        ACT(tfx[:], xtile[:], AF.Square, [xn], [tfn])
        RED("vector", ss, tfx[:], ALU.add, [tfn], [ssn])
        TS("vector", rstd, ss, 1.0 / 1024.0, 1e-6, ALU.mult, ALU.add, [ssn], [rsn])
        ACT(rstd, rstd, AF.Ln, [rsn], [rsn])
        ACT(rstd, rstd, AF.Exp, [rsn], [rsn], scale=-0.5)
        ACT(tfx[:], xtile[:], AF.Copy, [xn, rsn], [tfn], scale=rstd)
        TT("vector", tfx[:], tfx[:], rows[Gn][:], ALU.mult, [tfn] + rowdeps(Gn), [tfn])
        TT("vector", hbuf[:], tfx[:], rows[Sn][:], ALU.add, [tfn] + rowdeps(Sn), [hname])

    def transpose_to(hbuf, hname, dst, dst_name):
        pb, pbn = psb()
        pbv = pb[:].rearrange("p (k t) -> p k t", k=8)
        for k in range(8):
            TR(pbv[:, k, :], hbuf[:, k * 128:(k + 1) * 128], ident_b[:], [hname, "ident_b"], [(pbn, k)])
        ACT(dst, pbv, AF.Copy, [(pbn, k) for k in range(8)], [dst_name])

    hcT = hT[0]
    for t in range(2):
        norm_mod(ctx[t * 128:(t + 1) * 128, :], "cG1", "cS1", hb[t % 2], f"hb{t % 2}")
        transpose_to(hb[t % 2], f"hb{t % 2}", hcT[:, :, t * 128:(t + 1) * 128], ("hT0", t))
    ps, psn = psf("a")
    for k in range(8):
        MM(ps[:, 0:256], WIN[:, k, 1024:1152], hcT[:, k, 0:256], k == 0, k == 7,
           WINK + [("hT0", 0), ("hT0", 1)], [psn])
    ACT(kcT[:], ps[:, 0:256], AF.Copy, [psn], ["kcT"])
    for t in range(2):
        ps, psn = psf("a")
        for k in range(8):
            MM(ps[:, 0:128], hcT[:, k, t * 128:(t + 1) * 128], WIN[:, k, 1280:1408], k == 0, k == 7,
               [("WIN", "v"), ("hT0", t)], [psn])
        ACT(Vc[:, t, :, 0:64], ps[:, 0:128].rearrange("p (h d) -> p h d", h=2), AF.Copy, [psn], [("Vc", t)])

    if "ctx" in dbg:
        d1 = dbg_out("kcT", [128, 256], BF16)
        d2 = dbg_out("Vc", [128, 2 * 2 * 65], BF16)
        e1 = DMA("sync", "d_dbg", d1, kcT[:], ["kcT"], ["dbg1"])
        e2 = DMA("sync", "d_dbg", d2, Vc[:].rearrange("p a b c -> p (a b c)"), [("Vc", 0), ("Vc", 1), ("Vc", "ones")], ["dbg2"])
        P.finish("sync", [e1, e2])
    if stage <= 2:
        P.emit()
        return nc, dbg_outs

    chunks = [(0, 128, False)] + [(128 + 512 * i, 512, True) for i in range(4)] + [(2176, 128, False)]

    def prep_norm(ci, tiles):
        w0, n, central = chunks[ci]
        for t in tiles:
            norm_mod(x[w0 + t * 128:w0 + (t + 1) * 128, :], "G1", "S1", hb[t % 2], f"hb{t % 2}")

    def prep_trans(ci, tiles):
        hTc, hTn = hT[ci % 2], f"hT{ci % 2}"
        for t in tiles:
            transpose_to(hb[t % 2], f"hb{t % 2}", hTc[:, :, t * 128:(t + 1) * 128], (hTn, t))

    def build_items(ci):
        w0, n, central = chunks[ci]
        hTc, hTn = hT[ci % 2], f"hT{ci % 2}"
        ntile = n // 128
        hdeps = [(hTn, t) for t in range(ntile)]
        items = []

        def proj(col0, wdeps, cons):
            ps, psn = psf(cons)
            for k in range(8):
                MM(ps[:, 0:n], WIN[:, k, col0:col0 + 128], hTc[:, k, 0:n], k == 0, k == 7, wdeps + hdeps, [psn])
            return ps, psn

        def rope_out(col0, colsw, wd, wsd, dst, dstn):
            def f():
                pa, pan = proj(col0, wd, "v")
                pb_, pbn_ = proj(colsw, wsd, "v")
                TT("vector", rt1[:, 0:n], pa[:, 0:n], COS[:, w0:w0 + n], ALU.mult, [pan, "COS"], ["rt1"])
                TT("vector", rt2[:, 0:n], pb_[:, 0:n], SINS[:, w0:w0 + n], ALU.mult, [pbn_, "SINS"], ["rt2"])
                TT("vector", dst, rt1[:, 0:n], rt2[:, 0:n], ALU.add, ["rt1", "rt2"], [dstn])
            return f

        def v_item(t):
            def f():
                ps, psn = psf("a")
                for k in range(8):
                    MM(ps[:, 0:128], hTc[:, k, t * 128:(t + 1) * 128], WIN[:, k, 1280:1408], k == 0, k == 7,
                       [("WIN", "v"), (hTn, t)], [psn])
                wt = w0 // 128 + t
                ACT(Vt[:, wt, :, 0:64], ps[:, 0:128].rearrange("p (h d) -> p h d", h=2), AF.Copy, [psn], [("Vt", wt)])
            return f

        def bg_item(c):
            def f():
                ps, psn = proj(1408 + c * 128, [("WIN", "bg")], "a")
                ACT(bgT[:, c, w0 - 128:w0 - 128 + n], ps[:, 0:n], AF.Copy, [psn], [("bgT", c, ci)])
            return f

        def u_item(c):
            def f():
                pc, pcn = proj(1920 + c * 128, [("WIN", "cg")], "a")
                ph, phn = proj(2432 + c * 128, [("WIN", "hv")], "v")
                ACT(cgs[:, 0:n], pc[:, 0:n], AF.Copy, [pcn], ["cgs"])
                TT("vector", uT[:, c, w0:w0 + n], ph[:, 0:n], cgs[:, 0:n], ALU.mult, [phn, "cgs"], [("uT", c, ci)])
            return f

        if central:
            for c in range(4):
                items.append(rope_out(c * 128, 512 + c * 128, WINQ, WINQS, qT[:, c, w0:w0 + n], ("qT", c, ci)))
        items.append(rope_out(1024, 1152, WINK, WINKS, kT[:, w0:w0 + n], ("kT", ci)))
        for t in range(ntile):
            items.append(v_item(t))
        for c in range(4):
            if central:
                items.append(bg_item(c))
            items.append(u_item(c))
        return items

    def tiles_of(ci):
        return list(range(chunks[ci][1] // 128))

    prep_norm(0, tiles_of(0))
    prep_trans(0, tiles_of(0))
    for ci in range(len(chunks)):
        items = build_items(ci)
        nxt = ci + 1 if ci + 1 < len(chunks) else None
        half = (len(items) + 1) // 2
        if nxt is not None:
            prep_norm(nxt, tiles_of(nxt)[0:2])
        for f in items[:half]:
            f()
        if nxt is not None:
            prep_trans(nxt, tiles_of(nxt)[0:2])
            prep_norm(nxt, tiles_of(nxt)[2:4])
        for f in items[half:]:
            f()
        if nxt is not None:
            prep_trans(nxt, tiles_of(nxt)[2:4])

    if "proj" in dbg:
        d1 = dbg_out("qT", [128, 4 * W], BF16)
        d2 = dbg_out("kT", [128, W], BF16)
        d3 = dbg_out("Vt", [128, 18 * 130], BF16)
        d4 = dbg_out("uT", [128, 4 * W], BF16)
        d5 = dbg_out("bgT", [128, 4 * 2048], BF16)
        allq = [("qT", c, ci) for c in range(4) for ci in range(1, 5)]
        allk = [("kT", ci) for ci in range(6)]
        allv = [("Vt", t) for t in range(18)] + [("Vt", "ones")]
        allu = [("uT", c, ci) for c in range(4) for ci in range(6)]
        allb = [("bgT", c, ci) for c in range(4) for ci in range(1, 5)]
        evs = [DMA("sync", "d_dbg", d1, qT[:].rearrange("p a b -> p (a b)"), allq, ["dbg1"]),
               DMA("sync", "d_dbg", d2, kT[:], allk, ["dbg2"]),
               DMA("sync", "d_dbg", d3, Vt[:].rearrange("p a b c -> p (a b c)"), allv, ["dbg3"]),
               DMA("sync", "d_dbg", d4, uT[:].rearrange("p a b -> p (a b)"), allu, ["dbg4"]),
               DMA("sync", "d_dbg", d5, bgT[:].rearrange("p a b -> p (a b)"), allb, ["dbg5"])]
        P.finish("sync", evs)
    if stage <= 3:
        P.emit()
        return nc, dbg_outs

    ALLWIN = WINQ + WINQS + WINK + WINKS + [("WIN", nm) for nm in ("v", "bg", "cg", "hv")]
    o2 = REG0 + 32768
    PL = [P.sb(f"PL{i}", [128, 384], BF16, off=o2 + i * 768) for i in range(2)]; o2 += 1536
    PC = [P.sb(f"PC{i}", [128, 256], BF16, off=o2 + i * 512) for i in range(2)]; o2 += 1024
    att_tm = P.sb("att_tm", [128, 512], BF16, off=o2); o2 += 1024
    rec = P.sb("rec", [128, 8], F32, off=o2); o2 += 64
    cvt = [P.sb(f"cvt{i}", [128, 512], F32, off=o2 + i * 2048) for i in range(2)]; o2 += 4096
    assert o2 <= REG0 + 47104

    def kchunk(wb):
        return 0 if wb == 0 else (5 if wb == 17 else 1 + (wb - 1) // 4)

    VONES = [("Vt", "ones")]
    TS("vector", uT[:, :, 127:128], uT[:, :, 127:128], metat[:, 0:1], None, ALU.mult, None,
       [("uT", c, 0) for c in range(4)] + ["metat"], [("uT", c, 0) for c in range(4)])
    TS("vector", uT[:, :, 2176:2177], uT[:, :, 2176:2177], metat[:, 1:2], None, ALU.mult, None,
       [("uT", c, 5) for c in range(4)] + ["metat"], [("uT", c, 5) for c in range(4)])

    def conv_unit(tcn, c):
        w0 = 128 + tcn * 512
        ud = [("uT", c, ci) for ci in (tcn, tcn + 1, tcn + 2)]
        cwd = [("cw", kk, c4) for kk in range(3) for c4 in range(4)]
        TS("vector", cvt[0][:], uT[:, c, w0 - 1:w0 + 511], cw[:, c, 0:1], None, ALU.mult, None, ud + cwd, ["cvt0"] + ALLWIN)
        TS("vector", cvt[1][:], uT[:, c, w0:w0 + 512], cw[:, c, 1:2], None, ALU.mult, None, ud + cwd, ["cvt1"] + ALLWIN)
        TT("vector", cvt[0][:], cvt[0][:], cvt[1][:], ALU.add, ["cvt0", "cvt1"], ["cvt0"])
        TS("vector", cvt[1][:], uT[:, c, w0 + 1:w0 + 513], cw[:, c, 2:3], None, ALU.mult, None, ud + cwd, ["cvt1"])
        TT("vector", cvt[0][:], cvt[0][:], cvt[1][:], ALU.add, ["cvt0", "cvt1"], ["cvt0"])
        TT("vector", mixT[:, 4 + c, tcn * 512:(tcn + 1) * 512], cvt[0][:], bgT[:, c, tcn * 512:(tcn + 1) * 512], ALU.mult,
           ["cvt0", ("bgT", c, tcn + 1)], [("mixT", "conv", c, tcn)] + ALLWIN)

    sbanks = [3, 4, 5, 2]
    srr = [0]

    def sbank():
        bi = sbanks[srr[0] % 4]
        srr[0] += 1
        return PSF[bi], f"psf{bi}"

    for i in range(1, 17):
        ci_q = 1 + (i - 1) // 4
        mv = 1 if i == 1 else (2 if i == 16 else 0)
        pvs = [(PSF[0], "psf0"), (PSF[1], "psf1")]

        def S_(hn, i=i, ci_q=ci_q):
            half, c = hn // 4, hn % 4
            r0 = half * 64
            sl, sln = sbank()
            sc_, scn = sbank()
            qsl = qT[r0:r0 + 64, c, i * 128:(i + 1) * 128]
            for kb in range(3):
                wb = i - 1 + kb
                MM(sl[:, kb * 128:(kb + 1) * 128], kT[r0:r0 + 64, wb * 128:(wb + 1) * 128], qsl, True, True,
                   [("qT", c, ci_q), ("kT", kchunk(wb))], [sln])
            for cb in range(2):
                MM(sc_[:, cb * 128:(cb + 1) * 128], kcT[r0:r0 + 64, cb * 128:(cb + 1) * 128], qsl, True, True,
                   [("qT", c, ci_q), "kcT"], [scn])
            return sl, sln, sc_, scn

        def EPV_(hn, st, i=i, mv=mv, pvs=pvs):
            sl, sln, sc_, scn = st
            half, c = hn // 4, hn % 4
            j = hn % 2
            ACT(PL[j][:], sl[:, 0:384], AF.Exp, [sln], [f"PL{j}"] + ALLWIN, scale=0.125)
            ACT(PC[j][:], sc_[:, 0:256], AF.Exp, [scn], [f"PC{j}"] + ALLWIN, scale=0.125)
            TT("vector", PL[j][:], PL[j][:], mask3[:, mv, :], ALU.mult,
               [f"PL{j}", ("mask3", mv), ("mask3", mv, 1), ("mask3", mv, 2)], [f"PL{j}"])
            pv, pvn = pvs[half]
            pvr = pv[:, c * 65:(c + 1) * 65]
            for kb in range(3):
                wb = i - 1 + kb
                MM(pvr, PL[j][:, kb * 128:(kb + 1) * 128], Vt[:, wb, half, :], kb == 0, False,
                   [f"PL{j}", ("Vt", wb)] + VONES, [pvn])
            for cb in range(2):
                MM(pvr, PC[j][:, cb * 128:(cb + 1) * 128], Vc[:, cb, half, :], False, cb == 1,
                   [f"PC{j}", ("Vc", cb), ("Vc", "ones")], [pvn])

        st = S_(0)
        for hn in range(8):
            nxt = S_(hn + 1) if hn < 7 else None
            EPV_(hn, st)
            st = nxt
        for b in range(2):
            pv, pvn = pvs[b]
            pvv = pv[:, 0:260].rearrange("p (h e) -> p h e", h=4)
            TT("vector", rec[:, b * 4:(b + 1) * 4].unsqueeze(2), pvv[:, :, 64:65], esink[:, b * 4:(b + 1) * 4].unsqueeze(2),
               ALU.add, [pvn, "esink"], [("rec", b)] + ALLWIN)
            P.op("vector", (lambda b=b: (lambda e: e.reciprocal(rec[:, b * 4:(b + 1) * 4], rec[:, b * 4:(b + 1) * 4])))(),
                 [("rec", b)], [("rec", b)])
            TT("vector", att_tm[:, b * 256:(b + 1) * 256].rearrange("p (h d) -> p h d", h=4), pvv[:, :, 0:64],
               rec[:, b * 4:(b + 1) * 4].unsqueeze(2).to_broadcast([128, 4, 64]), ALU.mult,
               [pvn, ("rec", b)], [("att_tm", b)] + ALLWIN)
        pb, pbn = psb()
        pbv = pb[:, 0:512].rearrange("p (k t) -> p k t", k=4)
        for cc in range(4):
            TR(pbv[:, cc, :], att_tm[:, cc * 128:(cc + 1) * 128], ident_b[:], [("att_tm", cc // 2), "ident_b"], [(pbn, cc)])
        ACT(mixT[:, 0:4, (i - 1) * 128:i * 128], pbv, AF.Copy, [(pbn, cc) for cc in range(4)],
            [("mixT", "att", i - 1)] + ALLWIN)
        conv_unit((i - 1) // 4, (i - 1) % 4)

    if stage <= 4:
        P.emit()
        return nc, dbg_outs

    HTALL = [(f"hT{a}", t) for a in range(2) for t in range(4)]
    P.dma("gpsimd", "d_wo", lambda e: e.dma_start(out=wo[:], in_=w_out.rearrange("(k p) n -> p k n", p=128)), [], ["wo"] + HTALL)
    QALL = [("qT", c, ci) for c in range(4) for ci in range(1, 5)]
    o3 = qT_off
    h2T = [P.sb(f"h2T{i}", [128, 8, 128], BF16, off=o3 + i * 2048) for i in range(2)]; o3 += 4096
    CONVDEAD = [("uT", c, ci) for c in range(4) for ci in range(6)] + [("bgT", c, ci) for c in range(4) for ci in range(1, 5)]
    rows_off = REG0 - 8 * 4096
    affTM = P.sb("affTM", [128, 16, 16], F32, off=rows_off)
    gm = P.sb("gm", [128, 16, 16], F32, off=rows_off + 1024)
    thr = P.sb("thr", [128, 16], F32, off=rows_off + 2048)
    affT = P.sb("affT", [16, 2048], F32, off=o3); o3 += 8192
    wr = P.sb("wr", [128, 8, 16], BF16, off=o3); o3 += 256
    sm = P.sb("sm", [128, 64], F32, off=o3); o3 += 256
    assert o3 <= qT_off + 4 * W * 2
    P.dma("gpsimd", "d_wr", lambda e: e.dma_start(out=wr[:], in_=w_router.rearrange("(k p) e -> p k e", p=128)), [], ["wr"] + QALL)
    def a3_A(tile):
        tcn = tile // 4
        mdeps = [("mixT", "att", tile)] + [("mixT", "conv", c, tcn) for c in range(4)]
        i = xt_rr[0] % 2
        xt_rr[0] += 1
        xtile, xn = xt[i], f"xt{i}"
        DMA("sync", "d_" + xn, xtile[:], x[128 + tile * 128:256 + tile * 128, :], [], [xn])
        for n in range(2):
            ps, psn = psf("v")
            for k in range(8):
                MM(ps[:], mixT[:, k, tile * 128:(tile + 1) * 128], wo[:, k, n * 512:(n + 1) * 512], k == 0, k == 7,
                   mdeps + ["wo"], [psn])
            TT("vector", tf[:, n * 512:(n + 1) * 512], ps[:], rows["GT1"][:, n * 512:(n + 1) * 512], ALU.mult,
               [psn] + rowdeps("GT1"), ["tf"])
        TT("vector", xtile[:], xtile[:], tf[:], ALU.add, [xn, "tf"], [xn])
        DMA("sync", "d_x1d_" + xn, x1d[tile * 128:(tile + 1) * 128, :], xtile[:], [xn], [("x1d", tile)])
        j = tile % 2
        norm_mod_sb(xtile, xn, "G2", "S2", hb[j], f"hb{j}")
        DMA("sync", f"d_h2loc{j}", h2loc[tile * 128:(tile + 1) * 128, :], hb[j][:], [f"hb{j}"], [("h2loc", tile)])

    def a3_B(tile):
        j = tile % 2
        h2v = h2T[j][:]
        pb, pbn = psb()
        pbv = pb[:].rearrange("p (k t) -> p k t", k=8)
        for k in range(8):
            TR(pbv[:, k, :], hb[j][:, k * 128:(k + 1) * 128], ident_b[:], [f"hb{j}", "ident_b"], [(pbn, k)])
        ACT(h2v, pbv, AF.Copy, [(pbn, k) for k in range(8)], [f"h2T{j}"] + QALL)
        ps, psn = psf("v")
        for k in range(8):
            MM(ps[:, 0:16], h2T[j][:, k, :], wr[:, k, :], k == 0, k == 7, [f"h2T{j}", "wr"], [psn])
        mx, nmx, ssum, ex = sm[:, 0:1], sm[:, 1:2], sm[:, 2:3], sm[:, 16:32]
        af = affTM[:, tile, :]
        RED("vector", mx, ps[:, 0:16], ALU.max, [psn], ["sm_mx"])
        TS("vector", nmx, mx, -1.0, None, ALU.mult, None, ["sm_mx"], ["sm_nmx"])
        ACT(ex, ps[:, 0:16], AF.Exp, [psn, "sm_nmx"], ["sm_ex"], bias=nmx)
        RED("vector", ssum, ex, ALU.add, ["sm_ex"], ["sm_sum"])
        P.op("vector", lambda e: e.reciprocal(sm[:, 2:3], sm[:, 2:3]), ["sm_sum"], ["sm_sum"])
        TS("vector", af, ex, ssum, None, ALU.mult, None, ["sm_ex", "sm_sum"], [("affTM", tile)])
        pt, ptn = psf("a")
        TR(pt[0:16, 0:128], af, ident_f[:], [("affTM", tile), "ident_f"], [ptn])
        ACT(affT[:, tile * 128:(tile + 1) * 128], pt[0:16, 0:128], AF.Copy, [ptn], [("affT", tile)] + QALL)

    a3_A(0)
    for tile in range(16):
        if tile + 1 < 16:
            a3_A(tile + 1)
        a3_B(tile)
    DMA("sync", "d_affloc", affloc, affT[:], [("affT", t) for t in range(16)], ["affloc"])

    if "a3" in dbg:
        d1 = dbg_out("x1", [2048, 1024])
        d2 = dbg_out("aff", [16, 2048])
        d3 = dbg_out("h2", [2048, 1024], BF16)
        e1 = DMA("sync", "d_dbg1", d1, x1d, [("x1d", t) for t in range(16)], ["dbg1"])
        e2 = DMA("sync", "d_dbg2", d2, affloc, ["affloc"], ["dbg2"])
        e3 = DMA("sync", "d_dbg3", d3, h2loc, [("h2loc", t) for t in range(16)], ["dbg3"])
        P.finish("sync", [e1, e2, e3])
    if stage <= 5:
        P.emit()
        return nc, dbg_outs

    FENCE = [k for k in P.res.keys() if not (isinstance(k, str) and (k.startswith("psf") or k in ("ident_b", "ident_f", "ones_b", "metat")))
             and not (isinstance(k, tuple) and k[0] in ("row", "h2T", "affTM", "x1d", "h2loc"))]
    NTB, NITB = 8, 8
    P.dma("gpsimd", "d_ag_aff", lambda e: e.collective_compute("AllGather", ALU.bypass, replica_groups=GROUPS,
                                                               ins=[affloc.opt()], outs=[affall.opt()]),
          ["affloc"], ["affall", "agchain"], inc=1)
    wslot = [P.sb(f"wslot{i}", [128, 8, 1024], BF16, off=REG0 + i * 16384) for i in range(4)]
    wdt_ = P.sb("wd_", [128, 8, 1024], BF16, off=REG0 + 81920)
    wgv = w_gate.rearrange("e (k p) n -> e p k n", p=128)
    wuv = w_up.rearrange("e (k p) n -> e p k n", p=128)
    wdv = w_down.rearrange("e (k p) n -> e p k n", p=128)
    P.dma("gpsimd", "d_ws0", lambda e: e.dma_start(out=wslot[0][:], in_=wgv[0]), [], ["ws0"] + FENCE)
    P.dma("gpsimd", "d_ws1", lambda e: e.dma_start(out=wslot[1][:], in_=wuv[0]), [], ["ws1"] + FENCE)
    P.dma("gpsimd", "d_wd", lambda e: e.dma_start(out=wdt_[:], in_=wdv[0]), [], ["wd_"] + FENCE)
    ob = bgT_off + 32768
    AFt = P.sb("AFt", [128, 16, 64], F32, off=ob); ob += 4096
    FR = P.sb("FR", [128, 16, NTB], F32, off=ob); ob += 512
    Tt = P.sb("Tt", [128, 16, NTB], F32, off=ob); ob += 512
    tmpa = P.sb("tmpa", [128, 16, NTB], F32, off=ob); ob += 512
    get = P.sb("get", [128, 16, NTB], F32, off=ob); ob += 512
    cntb = P.sb("cntb", [128, 16 * NTB], BF16, off=ob); ob += 256
    lo = P.sb("lo", [128, 16], F32, off=ob); ob += 64
    hi = P.sb("hi", [128, 16], F32, off=ob); ob += 64
    wdt = P.sb("wdt", [128, 16], F32, off=ob); ob += 64
    red = P.sb("red", [128, 16], F32, off=ob); ob += 64
    idxt = P.sb("idxt", [128, 16], I32, off=ob); ob += 64
    assert ob <= rt1_off + 6144
    cmpb = P.sb("cmpb", [128, 16, NTB, 64], BF16, off=REG0 + 49152)
    for r in range(4):
        DMA("sync", "d_AFt", AFt[32 * r:32 * (r + 1), :, :],
            affall[r * 16:(r + 1) * 16, :].rearrange("e (p j) -> p e j", p=32, j=64), ["affall"], [("AFt", r)] + FENCE)
    AFD = [("AFt", r) for r in range(4)]
    P.op("gpsimd", lambda e: e.iota(FR[:], pattern=[[0, 16], [1, NTB]], base=1, channel_multiplier=0,
                                    allow_small_or_imprecise_dtypes=True), (), ["FR"] + FENCE)
    P.op("gpsimd", lambda e: e.iota(idxt[:], pattern=[[128, 16]], base=0, channel_multiplier=1), (), ["idxt"] + FENCE)
    TS("vector", FR[:], FR[:], 1.0 / (NTB + 1), None, ALU.mult, None, ["FR"], ["FR"])
    MSET("vector", lo[:], 0.0, ["lo"] + FENCE)
    MSET("vector", hi[:], 1.0, ["hi"])
    for it in range(NITB):
        TT("vector", wdt[:], hi[:], lo[:], ALU.subtract, ["hi", "lo"], ["wdt"])
        TT("vector", Tt[:], FR[:], wdt[:].unsqueeze(2).to_broadcast([128, 16, NTB]), ALU.mult, ["FR", "wdt"], ["Tt"])
        TT("vector", Tt[:], Tt[:], lo[:].unsqueeze(2).to_broadcast([128, 16, NTB]), ALU.add, ["Tt", "lo"], ["Tt"])
        TT("vector", cmpb[:], AFt[:].unsqueeze(2).to_broadcast([128, 16, NTB, 64]),
           Tt[:].unsqueeze(3).to_broadcast([128, 16, NTB, 64]), ALU.is_ge, AFD + ["Tt"], ["cmpb"] + FENCE)
        RED("vector", tmpa[:], cmpb[:], ALU.add, ["cmpb"], ["tmpa"])
        CP("vector", cntb[:], tmpa[:].rearrange("p e k -> p (e k)"), ["tmpa"], ["cntb"])
        ps, psn = psf("v")
        MM(ps[:, 0:16 * NTB], ones_b[:], cntb[:], True, True, ["cntb", "ones_b"], [psn])
        TS("vector", get[:].rearrange("p e k -> p (e k)"), ps[:, 0:16 * NTB], 1024.0, None, ALU.is_ge, None, [psn], ["get"])
        TT("vector", tmpa[:], Tt[:], get[:], ALU.mult, ["Tt", "get"], ["tmpa"])
        RED("vector", red[:], tmpa[:], ALU.max, ["tmpa"], ["red"])
        TT("vector", lo[:], lo[:], red[:], ALU.max, ["lo", "red"], ["lo"])
        TS("vector", tmpa[:], get[:], 2.0, None, ALU.mult, None, ["get"], ["tmpa"])
        TT("vector", tmpa[:], tmpa[:], Tt[:], ALU.add, ["tmpa", "Tt"], ["tmpa"])
        RED("vector", red[:], tmpa[:], ALU.min, ["tmpa"], ["red"])
        TT("vector", hi[:], hi[:], red[:], ALU.min, ["hi", "red"], ["hi"])
    CP("vector", thr[:], lo[:], ["lo"], ["thr"])
    AFFTM = [("affTM", t) for t in range(16)]
    TT("vector", gm[:], affTM[:], thr[:].unsqueeze(1).to_broadcast([128, 16, 16]), ALU.is_ge, AFFTM + ["thr"], ["gm"])
    TT("vector", gm[:], gm[:], affTM[:], ALU.mult, ["gm"] + AFFTM, ["gm"])

    if "thr" in dbg:
        d1 = dbg_out("thr", [128, 16])
        d2 = dbg_out("gm", [128, 256])
        e1 = DMA("sync", "d_dbg1", d1, thr[:], ["thr"], ["dbg1"])
        e2 = DMA("sync", "d_dbg2", d2, gm[:].rearrange("p a b -> p (a b)"), ["gm"], ["dbg2"])
        P.finish("sync", [e1, e2])
    if stage <= 6:
        P.emit()
        return nc, dbg_outs

    for j4 in range(4):
        P.dma("gpsimd", "d_ag_h2", (lambda j4=j4: (lambda e: e.collective_compute(
            "AllGather", ALU.bypass, replica_groups=GROUPS,
            ins=[h2loc[j4 * 512:(j4 + 1) * 512, :].opt()], outs=[h2all[j4 * 2048:(j4 + 1) * 2048, :].opt()])))(),
            [("h2loc", t) for t in range(j4 * 4, j4 * 4 + 4)] + ["agchain"], [("h2all", j4), "agchain"], inc=1)
    H2ALLD = [("h2all", j4) for j4 in range(4)]
    TAB = P.sb("TAB", [128, 16, 128], F32, off=qT_off)
    ob2 = kT_off
    ones64 = P.sb("ones64", [128, 64], F32, off=ob2); ob2 += 256
    n4 = P.sb("n4", [128, 4], F32, off=ob2); ob2 += 64
    t16 = P.sb("t16", [128, 16], F32, off=ob2); ob2 += 64
    rhsU = P.sb("rhsU", [128, 4, 128], BF16, off=ob2); ob2 += 1024
    rhsI = P.sb("rhsI", [128, 4, 128], BF16, off=ob2); ob2 += 1024
    offs_sb = P.sb("offs_sb", [128, 4, 128], F32, off=ob2); ob2 += 2048
    nrow_sb = P.sb("nrow_sb", [128, 4, 128], F32, off=ob2); ob2 += 2048
    sval = P.sb("sval", [128, 8], F32, off=ob2); ob2 += 64
    koffs = P.sb("koffs", [128, 4, 8], F32, off=ob2); ob2 += 128
    pS = P.sb("pS", [128, 4, 8], F32, off=ob2); ob2 += 128
    oex = P.sb("oex", [128, 4, 8], F32, off=ob2); ob2 += 128
    rS = P.sb("rS", [128, 4, 8], F32, off=ob2); ob2 += 128
    jS = P.sb("jS", [128, 4, 8], F32, off=ob2); ob2 += 128
    gS = P.sb("gS", [128, 4, 8], F32, off=ob2); ob2 += 128
    tSf = P.sb("tSf", [128, 4, 8], F32, off=ob2); ob2 += 128
    RIDX = P.sb("RIDX", [128, 4, 8], I32, off=ob2); ob2 += 128
    TIDX = P.sb("TIDX", [128, 4, 8], I32, off=ob2); ob2 += 128
    ZIDXf = P.sb("ZIDXf", [128, 4, 16], F32, off=ob2); ob2 += 256
    ZIDX = P.sb("ZIDX", [128, 4, 16], I32, off=ob2); ob2 += 256
    yz = [P.sb(f"yz{i}", [128, 1024], BF16, off=rows_off + 3 * 4096 + i * 2048) for i in range(2)]
    assert ob2 <= bgT_off, ob2
    cmpP = P.sb("cmpP", [128, 4, 8, 128], F32, off=REG0 + 32768)
    Gt = P.sb("Gt", [128, 4, 8, 128], F32, off=REG0 + 49152)

    MSET("vector", ones64[:], 1.0, ["ones64"] + FENCE)
    TT("vector", TAB[:, :, 64:128], AFt[:], thr[:].unsqueeze(2).to_broadcast([128, 16, 64]), ALU.is_ge, AFD + ["thr"], ["TABm"] + FENCE)
    for e16 in range(16):
        P.op("vector", (lambda e16=e16: (lambda e: e.tensor_tensor_scan(out=TAB[:, e16, 0:64], data0=ones64[:], data1=TAB[:, e16, 64:128],
                                                                          initial=0.0, op0=ALU.mult, op1=ALU.add)))(),
             ["TABm", "ones64"], [("TABc", e16)])
    TABC = [("TABc", e16) for e16 in range(16)]
    TT("vector", TAB[:, :, 64:128], TAB[:, :, 64:128], AFt[:], ALU.mult, ["TABm"] + TABC + AFD, ["TABm"])
    DMA("sync", "d_tabd", tabd.rearrange("(e p) c -> p e c", p=128), TAB[:], ["TABm"] + TABC, ["tabd"])
    selv = metat[:, 8:72].rearrange("p (e k) -> p e k", k=4)
    for k in range(4):
        TT("vector", t16[:], TAB[:, :, 63], selv[:, :, k], ALU.mult, TABC + ["metat"], ["t16"])
        RED("vector", n4[:, k:k + 1], t16[:], ALU.add, ["t16"], [("n4", k)])
    N4 = [("n4", k) for k in range(4)]
    TT("vector", rhsU[:], n4[:].unsqueeze(2).to_broadcast([128, 4, 128]), U_b[:].unsqueeze(1).to_broadcast([128, 4, 128]), ALU.mult,
       N4 + ["U_b"], ["rhsU"])
    TT("vector", rhsI[:], n4[:].unsqueeze(2).to_broadcast([128, 4, 128]), ident_b[:].unsqueeze(1).to_broadcast([128, 4, 128]), ALU.mult,
       N4 + ["ident_b"], ["rhsI"])
    ps, psn = psf("v")
    MM(ps[:], ones_b[:], rhsU[:].rearrange("p k q -> p (k q)"), True, True, ["rhsU", "ones_b"], [psn])
    CP("vector", offs_sb[:].rearrange("p k q -> p (k q)"), ps[:], [psn], ["offs_sb"])
    ps, psn = psf("v")
    MM(ps[:], ones_b[:], rhsI[:].rearrange("p k q -> p (k q)"), True, True, ["rhsI", "ones_b"], [psn])
    CP("vector", nrow_sb[:].rearrange("p k q -> p (k q)"), ps[:], [psn], ["nrow_sb"])
    P.op("gpsimd", lambda e: e.iota(sval[:], pattern=[[128, 8]], base=0, channel_multiplier=1,
                                    allow_small_or_imprecise_dtypes=True), (), ["sval"])
    P.op("gpsimd", lambda e: e.iota(koffs[:], pattern=[[128, 4], [0, 8]], base=0, channel_multiplier=0,
                                    allow_small_or_imprecise_dtypes=True), (), ["koffs"])
    P.op("gpsimd", lambda e: e.iota(ZIDXf[:], pattern=[[512, 4], [2048, 16]], base=0, channel_multiplier=1,
                                    allow_small_or_imprecise_dtypes=True), (), ["ZIDXf"])
    TS("vector", koffs[:], koffs[:], metat[:, 4:5], None, ALU.add, None, ["koffs", "metat"], ["koffs"])
    TS("vector", ZIDXf[:], ZIDXf[:], metat[:, 6:7], None, ALU.add, None, ["ZIDXf", "metat"], ["ZIDXf"])
    CP("vector", ZIDX[:], ZIDXf[:], ["ZIDXf"], ["ZIDX"])
    svb = sval[:].unsqueeze(1).to_broadcast([128, 4, 8])
    TT("vector", cmpP[:], offs_sb[:].unsqueeze(2).to_broadcast([128, 4, 8, 128]),
       svb.unsqueeze(3).to_broadcast([128, 4, 8, 128]), ALU.is_le, ["offs_sb", "sval"], ["cmpP"] + FENCE)
    RED("vector", pS[:], cmpP[:], ALU.add, ["cmpP"], ["pS"])
    TT("vector", cmpP[:], cmpP[:], nrow_sb[:].unsqueeze(2).to_broadcast([128, 4, 8, 128]), ALU.mult, ["cmpP", "nrow_sb"], ["cmpP"])
    RED("vector", oex[:], cmpP[:], ALU.add, ["cmpP"], ["oex"])
    TT("vector", rS[:], svb, oex[:], ALU.subtract, ["sval", "oex"], ["rS"])
    TT("vector", tSf[:], pS[:], koffs[:], ALU.add, ["pS", "koffs"], ["tSf"])
    CP("vector", RIDX[:], tSf[:], ["tSf"], ["RIDX"])
    for k in range(4):
        for c in range(8):
            P.dma("gpsimd", "d_G", (lambda k=k, c=c: (lambda e: e.indirect_dma_start(
                out=Gt[:, k, c, :], out_offset=None, in_=tabd, in_offset=bass.IndirectOffsetOnAxis(ap=RIDX[:, k, c:c + 1], axis=0))))(),
                ["tabd", "RIDX"], [("Gt", k, c), "cmpb"] if (k == 0 and c == 0) else [("Gt", k, c)])
    GALL = [("Gt", k, c) for k in range(4) for c in range(8)]
    cmpG = cmpP[:, :, :, 0:64]
    TT("vector", cmpG, Gt[:, :, :, 0:64], rS[:].unsqueeze(3).to_broadcast([128, 4, 8, 64]), ALU.is_le, GALL + ["rS"], ["cmpP"])
    RED("vector", jS[:], cmpG, ALU.add, ["cmpP"], ["jS"])
    TS("vector", oex[:], rS[:], 1.0, None, ALU.add, None, ["rS"], ["oex"])
    TT("vector", cmpG, Gt[:, :, :, 0:64], oex[:].unsqueeze(3).to_broadcast([128, 4, 8, 64]), ALU.is_equal, GALL + ["oex"], ["cmpP"])
    TT("vector", cmpG, cmpG, Gt[:, :, :, 64:128], ALU.mult, ["cmpP"] + GALL, ["cmpP"])
    RED("vector", gS[:], cmpG, ALU.add, ["cmpP"], ["gS"])
    TS("vector", tSf[:], pS[:], 64.0, None, ALU.mult, None, ["pS"], ["tSf"])
    TT("vector", tSf[:], tSf[:], jS[:], ALU.add, ["tSf", "jS"], ["tSf"])
    CP("vector", TIDX[:], tSf[:], ["tSf"], ["TIDX"])
    ra = P.sb("ra", [128, 4, 8], F32, off=ob2); rb = P.sb("rb", [128, 4, 8], F32, off=ob2 + 128)
    rj = P.sb("rj", [128, 4, 8], F32, off=ob2 + 256); GIDX = P.sb("GIDX", [128, 4, 8], I32, off=ob2 + 384)
    assert ob2 + 512 <= bgT_off
    TS("vector", ra[:], tSf[:], 2048.0, None, ALU.is_ge, None, ["tSf"], ["ra"])
    for thv in (4096.0, 6144.0):
        TS("vector", rb[:], tSf[:], thv, None, ALU.is_ge, None, ["tSf"], ["rb"])
        TT("vector", ra[:], ra[:], rb[:], ALU.add, ["ra", "rb"], ["ra"])
    TS("vector", rb[:], ra[:], -2048.0, None, ALU.mult, None, ["ra"], ["rb"])
    TT("vector", rb[:], rb[:], tSf[:], ALU.add, ["rb", "tSf"], ["rb"])
    TS("vector", rj[:], rb[:], 512.0, None, ALU.is_ge, None, ["rb"], ["rj"])
    for thv in (1024.0, 1536.0):
        TS("vector", oex[:], rb[:], thv, None, ALU.is_ge, None, ["rb"], ["oex"])
        TT("vector", rj[:], rj[:], oex[:], ALU.add, ["rj", "oex"], ["rj"])
    TT("vector", rj[:], rj[:], ra[:], ALU.subtract, ["rj", "ra"], ["rj"])
    TS("vector", rj[:], rj[:], 1536.0, None, ALU.mult, None, ["rj"], ["rj"])
    TT("vector", rj[:], rj[:], tSf[:], ALU.add, ["rj", "tSf"], ["rj"])
    CP("vector", GIDX[:], rj[:], ["rj"], ["GIDX"])
    ZSIDX = P.sb("ZSIDX", [128, 4, 8], I32, off=ob2 + 512)
    assert ob2 + 640 <= bgT_off
    TS("vector", rj[:], rb[:], 128.0, None, ALU.is_ge, None, ["rb"], ["rj"])
    for kk in range(2, 16):
        TS("vector", oex[:], rb[:], 128.0 * kk, None, ALU.is_ge, None, ["rb"], ["oex"])
        TT("vector", rj[:], rj[:], oex[:], ALU.add, ["rj", "oex"], ["rj"])
    TS("vector", rj[:], rj[:], 384.0, None, ALU.mult, None, ["rj"], ["rj"])
    TS("vector", oex[:], ra[:], -1920.0, None, ALU.mult, None, ["ra"], ["oex"])
    TT("vector", rj[:], rj[:], oex[:], ALU.add, ["rj", "oex"], ["rj"])
    TT("vector", rj[:], rj[:], tSf[:], ALU.add, ["rj", "tSf"], ["rj"])
    CP("vector", ZSIDX[:], rj[:], ["rj"], ["ZSIDX"])

    if "idx" in dbg:
        d1 = dbg_out("tidx", [128, 32])
        d2 = dbg_out("gS", [128, 32])
        e1 = DMA("sync", "d_dbg1", d1, tSf[:].rearrange("p a b -> p (a b)"), ["tSf", "TIDX"], ["dbg1"])
        e2 = DMA("sync", "d_dbg2", d2, gS[:].rearrange("p a b -> p (a b)"), ["gS"], ["dbg2"])
        P.finish("sync", [e1, e2])
    if stage <= 6.5:
        P.emit()
        return nc, dbg_outs

    hid = [P.sb(f"hid{i}", [128, 8, 512], BF16, off=qT_off + i * 8192) for i in range(2)]
    XS = P.sb("XS", [128, 8, 1024], BF16, off=bgT_off)
    xsT = P.sb("xsT", [128, 8, 1024], BF16, off=bgT_off + 16384)
    sgs = P.sb("sgs", [128, 512], F32, off=rt1_off + 4096)
    MSET("vector", XS[:], 0.0, ["XS"] + FENCE)
    ZD0 = []
    for t in range(8):
        ZD0.append(("Zd0", t))
        DMA("sync", "d_z0", Zd[t * 1024:(t + 1) * 1024, :].rearrange("(p c) d -> p c d", c=8), XS[:], ["XS"], [("Zd0", t)])
    yrr = [0]
    def issue_loads(k4):
        sg_, su_ = (k4 % 2) * 2, (k4 % 2) * 2 + 1
        wg_t, wu_t = wslot[sg_], wslot[su_]
        ex2 = (["cmpP"] if sg_ == 2 else [])
        ex3 = (["cmpb"] + GALL if su_ == 3 else [])
        if k4 > 0:
          P.dma("gpsimd", f"d_ws{sg_}", (lambda wg_t=wg_t, k4=k4: (lambda e: e.dma_start(out=wg_t[:], in_=wgv[k4])))(), [], [f"ws{sg_}"] + ex2 + (FENCE if k4 < 2 else []))
          P.dma("gpsimd", f"d_ws{su_}", (lambda wu_t=wu_t, k4=k4: (lambda e: e.dma_start(out=wu_t[:], in_=wuv[k4])))(), [], [f"ws{su_}"] + ex3 + (FENCE if k4 < 2 else []))
        for c in range(8):
            P.dma("gpsimd", f"d_XS{c}", (lambda k4=k4, c=c: (lambda e: e.indirect_dma_start(
                out=XS[:, c, :], out_offset=None, in_=h2all, in_offset=bass.IndirectOffsetOnAxis(ap=GIDX[:, k4, c:c + 1], axis=0))))(),
                H2ALLD + ["GIDX"], [("XS", c)] + (["XS"] if c == 0 else []))

    def issue_wd(k4):
        P.dma("gpsimd", "d_wd", (lambda k4=k4: (lambda e: e.dma_start(out=wdt_[:], in_=wdv[k4])))(), [], ["wd_"] + (FENCE if k4 < 1 else []))

    issue_loads(0)
    for k4 in range(4):
        sg_, su_ = (k4 % 2) * 2, (k4 % 2) * 2 + 1
        wg_t, wu_t = wslot[sg_], wslot[su_]
        for c in range(8):
            pb, pbn = psb()
            pbv = pb[:].rearrange("p (k t) -> p k t", k=8)
            for kc in range(8):
                TR(pbv[:, kc, :], XS[:, c, kc * 128:(kc + 1) * 128], ident_b[:], [("XS", c), "XS", "ident_b"], [(pbn, kc)])
            ACT(xsT[:, :, c * 128:(c + 1) * 128], pbv, AF.Copy, [(pbn, kc) for kc in range(8)], [("xsT", c)] + (FENCE if k4 == 0 else []))
        if k4 < 3:
            issue_loads(k4 + 1)
        for sch in range(2):
            hd = hid[(k4 * 2 + sch) % 2]
            hdn = f"hid{(k4 * 2 + sch) % 2}"
            xdeps = [("xsT", sch * 4 + t) for t in range(4)]
            for fo in range(8):
                pa, pan = psf("a")
                for kc in range(8):
                    MM(pa[:], wg_t[:, kc, fo * 128:(fo + 1) * 128], xsT[:, kc, sch * 512:(sch + 1) * 512], kc == 0, kc == 7,
                       [f"ws{sg_}"] + xdeps, [pan])
                pu, pun = psf("v")
                for kc in range(8):
                    MM(pu[:], wu_t[:, kc, fo * 128:(fo + 1) * 128], xsT[:, kc, sch * 512:(sch + 1) * 512], kc == 0, kc == 7,
                       [f"ws{su_}"] + xdeps, [pun])
                ACT(sgs[:], pa[:], AF.Silu, [pan], ["sgs"] + (FENCE if k4 == 0 and sch == 0 and fo == 0 else []))
                TT("vector", hd[:, fo, :], pu[:], sgs[:], ALU.mult, [pun, "sgs"], [(hdn, fo)] + (FENCE + ["TABm"] + TABC if k4 == 0 else []))
            for t in range(4):
                c = sch * 4 + t
                yi = yrr[0] % 2
                yrr[0] += 1
                for dn in range(2):
                    py, pyn = psf("v")
                    for kc in range(8):
                        MM(py[:], hd[:, kc, t * 128:(t + 1) * 128], wdt_[:, kc, dn * 512:(dn + 1) * 512], kc == 0, kc == 7,
                           [(hdn, kc), "wd_"], [pyn])
                    TS("vector", yz[yi][:, dn * 512:(dn + 1) * 512], py[:], gS[:, k4, c:c + 1], None, ALU.mult, None,
                       [pyn, "gS"], [f"yz{yi}"])
                P.dma("gpsimd", f"d_sz{yi}", (lambda yi=yi, k4=k4, c=c: (lambda e: e.indirect_dma_start(
                    out=Zd, out_offset=bass.IndirectOffsetOnAxis(ap=ZSIDX[:, k4, c:c + 1], axis=0), in_=yz[yi][:], in_offset=None,
                    compute_op=ALU.add, oob_is_err=True)))(), [f"yz{yi}", "ZSIDX", "Zd"] + ZD0, ["Zd"])
        if k4 < 3:
            issue_wd(k4 + 1)

    if stage <= 7:
        P.emit()
        return nc, dbg_outs

    gfrow = P.sb("gfrow", [128, 1024], F32, off=rows_off + 4096)
    DMA("sync", "d_gfrow", gfrow[:], g_final.partition_broadcast(128), [], ["gfrow"] + FENCE)
    z4 = [P.sb(f"z4_{i}", [128, 4, 1024], BF16, off=bgT_off + i * 8192) for i in range(2)]
    evs = []

    def z_allgather(j16):
        P.dma("gpsimd", "d_ag_z", (lambda j16=j16: (lambda e: e.collective_compute(
            "AllGather", ALU.bypass, replica_groups=GROUPS,
            ins=[Zd[j16 * 512:(j16 + 1) * 512, :].opt()], outs=[Zall[j16 * 2048:(j16 + 1) * 2048, :].opt()])))(),
            ["Zd", "agchain"], [("Zall", j16), "agchain"], inc=1)

    def phaseC_tile(tile):
        i = xt_rr[0] % 2
        xt_rr[0] += 1
        xtile, xn = xt[i], f"xt{i}"
        zi = tile % 2
        DMA("sync", "d_" + xn, xtile[:], x1d[tile * 128:(tile + 1) * 128, :], [("x1d", tile)], [xn])
        for r in range(4):
            P.dma("gpsimd", f"d_z4_{zi}_{r}", (lambda zi=zi, r=r, tile=tile: (lambda e: e.indirect_dma_start(
                out=z4[zi][:, r, :], out_offset=None, in_=Zall, in_offset=bass.IndirectOffsetOnAxis(ap=ZIDX[:, r, tile:tile + 1], axis=0))))(),
                [("Zall", tile), "ZIDX"], [(f"z4_{zi}", r)] + ([("XS", c) for c in range(8)] + ["XS"] if tile < 2 else []))
        zd = [(f"z4_{zi}", r) for r in range(4)]
        TT("vector", tf[:], z4[zi][:, 0, :], z4[zi][:, 1, :], ALU.add, zd, ["tf"])
        TT("vector", tf[:], tf[:], z4[zi][:, 2, :], ALU.add, zd + ["tf"], ["tf"])
        TT("vector", tf[:], tf[:], z4[zi][:, 3, :], ALU.add, zd + ["tf"], ["tf"])
        TT("vector", tf[:], tf[:], rows["GT2"][:], ALU.mult, ["tf"] + rowdeps("GT2"), ["tf"])
        TT("vector", xtile[:], xtile[:], tf[:], ALU.add, [xn, "tf"], [xn])
        ss = small[:, 16:17]
        rstd = small[:, 17:18]
        ACT(tf[:], xtile[:], AF.Square, [xn], ["tf"])
        RED("vector", ss, tf[:], ALU.add, ["tf"], ["ss"])
        TS("vector", rstd, ss, 1.0 / 1024.0, 1e-6, ALU.mult, ALU.add, ["ss"], ["rstd"])
        ACT(rstd, rstd, AF.Ln, ["rstd"], ["rstd"])
        ACT(rstd, rstd, AF.Exp, ["rstd"], ["rstd"], scale=-0.5)
        ACT(tf[:], xtile[:], AF.Copy, [xn, "rstd"], ["tf"], scale=rstd)
        TT("vector", xtile[:], tf[:], gfrow[:], ALU.mult, ["tf", "gfrow"], [xn])
        evs.append(DMA("sync", "d_out_" + xn, out[tile * 128:(tile + 1) * 128, :], xtile[:], [xn], [("out", tile)]))

    for j16 in range(16):
        z_allgather(j16)
        if j16 >= 1:
            phaseC_tile(j16 - 1)
    phaseC_tile(15)
    P.finish("sync", evs)
    P.emit()
    return nc, dbg_outs


def make_in_maps(inp):
    x = np.ascontiguousarray(inp["x"], dtype=np.float32)
    maps = []
    for c in range(NCORES):
        b, q = c // 4, c % 4
        t0 = q * 2048
        xw = np.zeros((W, 1024), np.float32)
        lo, hi = t0 - 128, t0 + 2048 + 128
        slo, shi = max(lo, 0), min(hi, 8192)
        xw[slo - lo:shi - lo] = x[b, slo:shi]
        ccv = np.stack([inp["c"][b].reshape(8, 128).T, inp["c_ctx"].reshape(8, 128).T], axis=-1)
        meta = np.zeros((128, 80), np.float32)
        meta[:, 0] = 1.0 if q > 0 else 0.0
        meta[:, 1] = 1.0 if q < 3 else 0.0
        meta[:, 2] = float(q * 32 - 2)
        meta[:, 3] = float(q * 2048)
        meta[:, 4] = float(4 * q * 128)
        meta[:, 5] = float(q * 8192)
        meta[:, 6] = float(q * 128)
        for k in range(4):
            meta[:, 8 + (4 * q + k) * 4 + k] = 1.0
        maps.append({
            "x": xw, "ctx": np.ascontiguousarray(inp["ctx"][b]), "cc": np.ascontiguousarray(ccv.reshape(128, 16)),
            "meta": meta, "w_ada": inp["w_ada"][0], "b_ada": inp["b_ada"][0], "g_mix": inp["g_mix"][0],
            "g_ffn": inp["g_ffn"][0], "g_final": inp["g_final"], "w_in": inp["w_in"][0], "conv_w": inp["conv_w"][0],
            "sink": inp["sink"][0], "w_out": inp["w_out"][0], "w_router": inp["w_router"][0],
            "w_gate": np.ascontiguousarray(inp["w_gate"][0, 4 * q:4 * q + 4]),
            "w_up": np.ascontiguousarray(inp["w_up"][0, 4 * q:4 * q + 4]),
            "w_down": np.ascontiguousarray(inp["w_down"][0, 4 * q:4 * q + 4]),
        })
    return maps


def kernel(**inputs):
    inp = {k: np.asarray(v) for k, v in inputs.items()}
    nc, _ = build_nc()
    res = run_bass_kernel_spmd(nc, make_in_maps(inp), core_ids=list(range(NCORES)))
    outp = np.zeros((2, 8192, 1024), np.float32)
    for c in range(NCORES):
        b, q = c // 4, c % 4
        outp[b, q * 2048:(q + 1) * 2048] = res.results[c]["out"]
    return outp
```

```python
import os
import numpy as np
import concourse.bass as bass
import concourse.mybir as mybir
from concourse.bass_utils import run_bass_kernel_spmd

F32 = mybir.dt.float32
BF16 = mybir.dt.bfloat16
I32 = mybir.dt.int32
ALU = mybir.AluOpType
AF = mybir.ActivationFunctionType
AX = mybir.AxisListType

COMPUTE = ("tensor", "vector", "scalar", "gpsimd")
QUEUES = ("sync",)
NCORES = 8
GROUPS = [[0, 1, 2, 3], [4, 5, 6, 7]]
W = 2304
NT = 16
NIT = 7


class Prog:
    def __init__(self, nc):
        self.nc = nc
        self.streams = {e: [] for e in COMPUTE + QUEUES}
        self.cnt = {e: 0 for e in COMPUTE}
        self.dma_cnt = {}
        self.waited = {}
        self.res = {}
        self.sem_handles = {}
        self.final_events = []
        self.sb_off = 16512
        self.sb_top = 229344

    def sb(self, name, shape, dtype, off=None):
        esz = {F32: 4, BF16: 2, I32: 4}[dtype]
        n = 1
        for s in shape[1:]:
            n *= s
        nbytes = (n * esz + 63) // 64 * 64
        if off is None:
            off = self.sb_off
            self.sb_off += nbytes
        assert off >= 16512 and off + nbytes <= self.sb_top, (name, off, nbytes)
        return self.nc.alloc_sbuf_tensor_at(name, list(shape), dtype, offset=off)

    def _deps(self, reads, writes):
        need = []
        for r in reads:
            st = self.res.get(r)
            if st and st["w"] is not None:
                need.append(st["w"])
        for w in writes:
            st = self.res.get(w)
            if st:
                if st["w"] is not None:
                    need.append(st["w"])
                need.extend(st["r"])
        return need

    def _commit(self, ev, reads, writes):
        for r in reads:
            st = self.res.setdefault(r, {"w": None, "r": []})
            st["r"].append(ev)
        for w in writes:
            self.res[w] = {"w": ev, "r": []}

    def _waits(self, eng, need):
        best = {}
        for (k, v) in need:
            if k == "tensor" and eng == "tensor":
                continue
            if v > best.get(k, 0):
                best[k] = v
        out = []
        for k, v in best.items():
            if self.waited.get((eng, k), 0) >= v:
                continue
            self.waited[(eng, k)] = v
            out.append((k, v))
        return out

    def op(self, eng, fn, reads=(), writes=()):
        need = self._deps(reads, writes)
        waits = self._waits(eng, need)
        self.cnt[eng] += 1
        ev = (eng, self.cnt[eng])
        self.streams[eng].append((waits, fn, (eng, 1)))
        self._commit(ev, reads, writes)
        return ev

    def dma(self, q, sem, fn, reads=(), writes=(), inc=16):
        need = self._deps(reads, writes)
        waits = self._waits(q, need)
        self.dma_cnt[sem] = self.dma_cnt.get(sem, 0) + inc
        ev = (sem, self.dma_cnt[sem])
        self.streams[q].append((waits, fn, (sem, inc)))
        self._commit(ev, reads, writes)
        return ev

    def finish(self, eng, events):
        self.final_events.append((eng, events))

    def check_deadlock(self):
        sem = {}
        pos = {e: 0 for e in self.streams}
        progressed = True
        while progressed:
            progressed = False
            for e, st in self.streams.items():
                while pos[e] < len(st):
                    waits, fn, inc = st[pos[e]]
                    if all(sem.get(k, 0) >= v for (k, v) in waits):
                        sem[inc[0]] = sem.get(inc[0], 0) + inc[1]
                        pos[e] += 1
                        progressed = True
                    else:
                        break
        stuck = {e: (pos[e], len(st), st[pos[e]][0]) for e, st in self.streams.items() if pos[e] < len(st)}
        assert not stuck, ("DEADLOCK", stuck, {k: sem.get(k) for e in stuck for (k, v) in stuck[e][2]})

    def emit(self):
        self.check_deadlock()
        nc = self.nc
        names = set(COMPUTE)
        for e in self.streams:
            for (waits, fn, inc) in self.streams[e]:
                names.add(inc[0])
                for (k, v) in waits:
                    names.add(k)
        for n in sorted(names):
            self.sem_handles[n] = nc.alloc_semaphore("s_" + n)
        H = self.sem_handles
        fin = {}
        for eng, evs in self.final_events:
            fin.setdefault(eng, []).extend(evs)
        with nc.Block() as block:
            def make(ename):
                def body(e):
                    for (waits, fn, inc) in self.streams[ename]:
                        for (k, v) in waits:
                            e.wait_ge(H[k], v)
                        fn(e).then_inc(H[inc[0]], inc[1])
                    best = {}
                    for (k, v) in fin.get(ename, []):
                        best[k] = max(best.get(k, 0), v)
                    for k, v in best.items():
                        e.wait_ge(H[k], v)
                return body
            for ename in self.streams:
                if not self.streams[ename] and ename not in fin:
                    continue
                getattr(block, ename)(make(ename))


def build_nc(stage=99, dbg=()):
    nc = bass.Bass("TRN2", target_bir_lowering=False)
    P = Prog(nc)
    dbg_outs = {}

    def din(name, shape, dt=F32):
        return nc.dram_tensor(name, list(shape), dt, kind="ExternalInput").ap()

    x = din("x", [W, 1024])
    ctx = din("ctx", [256, 1024])
    cc = din("cc", [128, 16])
    meta = din("meta", [128, 80])
    w_ada = din("w_ada", [1024, 6144])
    b_ada = din("b_ada", [6144])
    g_mix = din("g_mix", [1024])
    g_ffn = din("g_ffn", [1024])
    g_final = din("g_final", [1024])
    w_in = din("w_in", [1024, 2304])
    conv_w = din("conv_w", [3, 512])
    sink = din("sink", [8])
    w_out = din("w_out", [1024, 1024])
    w_router = din("w_router", [1024, 16])
    w_gate = din("w_gate", [4, 1024, 1024])
    w_up = din("w_up", [4, 1024, 1024])
    w_down = din("w_down", [4, 1024, 1024])
    out = nc.dram_tensor("out", [2048, 1024], F32, kind="ExternalOutput").ap()

    x1d = nc.dram_tensor("x1d", [2048, 1024], F32).ap()
    h2loc = nc.dram_tensor("h2loc", [2048, 1024], BF16).ap()
    h2all = nc.dram_tensor("h2all", [8192, 1024], BF16).ap()
    affloc = nc.dram_tensor("affloc", [16, 2048], F32).ap()
    affall = nc.dram_tensor("affall", [64, 2048], F32).ap()
    tabd = nc.dram_tensor("tabd", [2048, 128], F32).ap()
    Zd = nc.dram_tensor("Zd", [8192, 1024], BF16).ap()
    Zall = nc.dram_tensor("Zall", [32768, 1024], BF16).ap()

    def dbg_out(name, shape, dt=F32):
        t = nc.dram_tensor("dbg_" + name, list(shape), dt, kind="ExternalOutput").ap()
        dbg_outs[name] = t
        return t

    def ACT(out_, in_, func, r, w, **kw):
        return P.op("scalar", lambda e: e.activation(out=out_, in_=in_, func=func, **kw), r, w)

    def TT(eng, out_, in0, in1, op, r, w):
        return P.op(eng, lambda e: e.tensor_tensor(out=out_, in0=in0, in1=in1, op=op), r, w)

    def TS(eng, out_, in0, s1, s2, op0, op1, r, w):
        if op1 is None:
            return P.op(eng, lambda e: e.tensor_scalar(out=out_, in0=in0, scalar1=s1, scalar2=None, op0=op0), r, w)
        return P.op(eng, lambda e: e.tensor_scalar(out=out_, in0=in0, scalar1=s1, scalar2=s2, op0=op0, op1=op1), r, w)

    def STT(eng, out_, in0, scalar, in1, op0, op1, r, w):
        return P.op(eng, lambda e: e.scalar_tensor_tensor(out=out_, in0=in0, scalar=scalar, in1=in1, op0=op0, op1=op1), r, w)

    def RED(eng, out_, in_, op, r, w):
        return P.op(eng, lambda e: e.tensor_reduce(out=out_, in_=in_, axis=AX.X, op=op), r, w)

    def CP(eng, out_, in_, r, w):
        return P.op(eng, lambda e: e.tensor_copy(out=out_, in_=in_), r, w)

    def MSET(eng, out_, val, w):
        return P.op(eng, lambda e: e.memset(out_, val), (), w)

    def MM(out_, lhsT, rhs, start, stop, r, w):
        return P.op("tensor", lambda e: e.matmul(out_, lhsT, rhs, start=start, stop=stop), r, w)

    def TR(out_, in_, ident, r, w):
        return P.op("tensor", lambda e: e.transpose(out_, in_, ident), r, w)

    def DMA(q, sem, out_, in_, r, w):
        return P.dma(q, sem, lambda e: e.dma_start(out=out_, in_=in_), r, w)

    PSF = [nc.alloc_psum_tensor(f"psf{i}", [128, 512], F32) for i in range(6)]
    PSB = [nc.alloc_psum_tensor(f"psb{i}", [128, 1024], BF16) for i in range(2)]
    psf_rr = {"v": 0, "a": 0}

    def psf(cons):
        i = psf_rr[cons] % 3 + (0 if cons == "v" else 3)
        psf_rr[cons] += 1
        return PSF[i], f"psf{i}"

    psb_rr = [0]

    def psb():
        i = psb_rr[0] % 2
        psb_rr[0] += 1
        return PSB[i], f"psb{i}"

    ident_f = P.sb("ident_f", [128, 128], F32)
    ident_b = P.sb("ident_b", [128, 128], BF16)
    iot = P.sb("iot", [128, 128], F32)
    ones_b = P.sb("ones_b", [128, 128], BF16)
    U_b = P.sb("U_b", [128, 128], BF16)
    UI_b = P.sb("UI_b", [128, 128], BF16)
    mask3 = P.sb("mask3", [128, 3, 384], BF16)
    metat = P.sb("metat", [128, 80], F32)
    esink = P.sb("esink", [128, 8], F32)
    rows = {}
    for nm in ("S1", "G1", "GT1", "S2", "G2", "GT2", "cS1", "cG1"):
        rows[nm] = P.sb("row_" + nm, [128, 1024], F32)
    REG0 = P.sb_off

    P.op("gpsimd", lambda e: e.iota(iot[:], pattern=[[1, 128]], base=0, channel_multiplier=-1,
                                    allow_small_or_imprecise_dtypes=True), (), ["iot"])
    TS("vector", ident_f[:], iot[:], 0.0, None, ALU.is_equal, None, ["iot"], ["ident_f"])
    CP("vector", ident_b[:], ident_f[:], ["ident_f"], ["ident_b"])
    TS("vector", U_b[:], iot[:], 0.0, None, ALU.is_ge, None, ["iot"], ["U_b"])
    MSET("vector", ones_b[:], 1.0, ["ones_b"])
    DMA("sync", "d_meta", metat[:], meta, [], ["metat"])
    DMA("sync", "d_sink", esink[:], sink.partition_broadcast(128), [], ["esink"])
    ACT(esink[:], esink[:], AF.Exp, ["esink"], ["esink"])
    for v in range(3):
        TS("vector", mask3[:, v, 0:128], iot[:], 0.0, None, ALU.is_le, None, ["iot"], [("mask3", v)])
        MSET("vector", mask3[:, v, 128:256], 1.0, [("mask3", v, 1)])
        TS("vector", mask3[:, v, 256:384], iot[:], 0.0, None, ALU.is_ge, None, ["iot"], [("mask3", v, 2)])
    TS("vector", mask3[:, 1, 0:128], mask3[:, 1, 0:128], metat[:, 0:1], None, ALU.mult, None,
       ["metat", ("mask3", 1)], [("mask3", 1)])
    TS("vector", mask3[:, 2, 256:384], mask3[:, 2, 256:384], metat[:, 1:2], None, ALU.mult, None,
       ["metat", ("mask3", 2, 2)], [("mask3", 2, 2)])

    if "const" in dbg:
        d1 = dbg_out("ident", [128, 128])
        d2 = dbg_out("mask3", [128, 3 * 384], BF16)
        d3 = dbg_out("esink", [128, 8])
        e1 = DMA("sync", "d_dbg", d1, ident_f[:], ["ident_f"], ["dbg1"])
        e2 = DMA("sync", "d_dbg", d2, mask3[:].rearrange("p a b -> p (a b)"),
                 [("mask3", v) for v in range(3)] + [("mask3", v, 1) for v in range(3)] + [("mask3", v, 2) for v in range(3)], ["dbg2"])
        e3 = DMA("sync", "d_dbg", d3, esink[:], ["esink"], ["dbg3"])
        P.finish("sync", [e1, e2, e3])
    if stage <= 0:
        P.emit()
        return nc, dbg_outs

    o = REG0
    WIN = P.sb("WIN", [128, 8, 2944], BF16, off=o)
    mixT = P.sb("mixT", [128, 8, 2048], BF16, off=o)
    o += 47104
    COS = P.sb("COS", [128, W], F32, off=o); o += W * 4
    SINS = P.sb("SINS", [128, W], F32, off=o); o += W * 4
    xt = [P.sb(f"xt{i}", [128, 1024], F32, off=o + i * 4096) for i in range(2)]; o += 8192
    tf = P.sb("tf", [128, 1024], F32, off=o); o += 4096
    hb = [P.sb(f"hb{i}", [128, 1024], BF16, off=o + i * 2048) for i in range(2)]; o += 4096
    hT = [P.sb(f"hT{i}", [128, 8, 512], BF16, off=o + i * 8192) for i in range(2)]
    wo = P.sb("wo", [128, 8, 1024], BF16, off=o)
    o += 16384
    qT_off = o
    qT = P.sb("qT", [128, 4, W], BF16, off=o); o += 4 * W * 2
    kT_off = o
    kT = P.sb("kT", [128, W], BF16, off=o); o += W * 2
    Vt = P.sb("Vt", [128, 18, 2, 65], BF16, off=o); o += 4736
    kcT = P.sb("kcT", [128, 256], BF16, off=o); o += 512
    Vc = P.sb("Vc", [128, 2, 2, 65], BF16, off=o); o += 576
    bgT_off = o
    bgT = P.sb("bgT", [128, 4, 2048], BF16, off=o)
    stg = P.sb("stg", [128, 8, 640], F32, off=o)
    o += 20480
    uT_off = o
    uT = P.sb("uT", [128, 4, W], BF16, off=o); o += 4 * W * 2
    rt1_off = o
    rt1 = P.sb("rt1", [128, 512], F32, off=o); o += 2048
    rt2 = P.sb("rt2", [128, 512], F32, off=o); o += 2048
    cgs = P.sb("cgs", [128, 512], F32, off=o); o += 2048
    small = P.sb("small", [128, 64], F32, off=o); o += 256
    cw = P.sb("cw", [128, 4, 3], F32, off=o); o += 64
    assert o <= P.sb_top, o
    A_END = o

    wa = [P.sb("wa0", [128, 8, 1024], BF16, off=qT_off), P.sb("wa1", [128, 8, 1024], BF16, off=uT_off)]
    o = kT_off
    lb = P.sb("lb", [128, 8, 2, 128], BF16, off=o); o += 4096
    brow = P.sb("brow", [128, 1024], F32, off=o); o += 4096
    cct = P.sb("cct", [128, 8, 2], F32, off=o); o += 64
    scl = P.sb("scl", [128, 8, 2], F32, off=o); o += 64
    assert o <= bgT_off
    gmrow = P.sb("gmrow", [128, 1024], F32, off=rt1_off)

    DMA("sync", "d_cc", cct[:], cc.rearrange("p (k v) -> p k v", v=2), [], ["cct"])
    ACT(scl[:], cct[:], AF.Silu, ["cct"], ["scl"])
    for v in range(2):
        CP("vector", lb[:, :, v, :], scl[:, :, v:v + 1].to_broadcast([128, 8, 128]), ["scl"], [("lb", v)])
    if stage <= 0.3:
        d1 = dbg_out("lb", [128, 8 * 2 * 128], BF16)
        e1 = DMA("sync", "d_dbg", d1, lb[:].rearrange("p a b c -> p (a b c)"), [("lb", 0), ("lb", 1)], ["dbg1"])
        P.finish("sync", [e1])
        P.emit()
        return nc, dbg_outs
    w_ada_v = w_ada.rearrange("(k p) n -> p k n", p=128)
    grp = [(0, [("S1", 0), ("cS1", 1)]), (1, [("G1", 0), ("cG1", 1)]), (2, [("GT1", 0)]),
           (3, [("S2", 0)]), (4, [("G2", 0)]), (5, [("GT2", 0)])]
    for gi, (g, uses) in enumerate(grp):
        wb = wa[gi % 2]
        wn = f"wa{gi % 2}"
        P.dma("gpsimd", "d_" + wn, (lambda wb=wb, g=g: (lambda e: e.dma_start(out=wb[:], in_=w_ada_v[:, :, g * 1024:(g + 1) * 1024])))(),
              [], [wn])
        DMA("sync", "d_brow", brow[:], b_ada[g * 1024:(g + 1) * 1024].partition_broadcast(128), [], ["brow"])
        if stage <= 0.5:
            d1 = dbg_out("wa", [128, 8 * 1024], BF16)
            d2 = dbg_out("brow", [128, 1024])
            e1 = DMA("sync", "d_dbg", d1, wb[:].rearrange("p a b -> p (a b)"), [wn], ["dbg1"])
            e2 = DMA("sync", "d_dbg", d2, brow[:], ["brow"], ["dbg2"])
            P.finish("sync", [e1, e2])
            P.emit()
            return nc, dbg_outs
        for (nm, v) in uses:
            for n in range(2):
                ps, psn = psf("v")
                for k in range(8):
                    MM(ps[:], lb[:, k, v, :], wb[:, k, n * 512:(n + 1) * 512], k == 0, k == 7,
                       [("lb", v), wn], [psn])
                TT("vector", rows[nm][:, n * 512:(n + 1) * 512], ps[:], brow[:, n * 512:(n + 1) * 512], ALU.add,
                   [psn, "brow"], [("row", nm, n)])
                if stage <= 0.7:
                    d1 = dbg_out("r0", [128, 512])
                    e1 = DMA("sync", "d_dbg", d1, rows[nm][:, 0:512], [("row", nm, n)], ["dbg1"])
                    P.finish("sync", [e1])
                    P.emit()
                    return nc, dbg_outs
    for (gsrc, names) in (((g_mix, ("G1", "cG1")), (g_ffn, ("G2",))) if stage > 0.8 else ()):
        DMA("sync", "d_gmrow", gmrow[:], gsrc.partition_broadcast(128), [], ["gmrow"])
        for nm in names:
            TS("vector", rows[nm][:], rows[nm][:], 1.0, None, ALU.add, None,
               [("row", nm, 0), ("row", nm, 1)], [("row", nm, 0), ("row", nm, 1)])
            TT("vector", rows[nm][:], rows[nm][:], gmrow[:], ALU.mult,
               [("row", nm, 0), ("row", nm, 1), "gmrow"], [("row", nm, 0), ("row", nm, 1)])

    def rowdeps(nm):
        return [("row", nm, 0), ("row", nm, 1)]

    if "rows" in dbg:
        d = dbg_out("rows", [8, 128, 1024])
        for i, nm in enumerate(("S1", "G1", "GT1", "S2", "G2", "GT2", "cS1", "cG1")):
            ev = DMA("sync", "d_dbg", d[i], rows[nm][:], rowdeps(nm), ["dbg"])
        P.finish("sync", [ev])
    if stage <= 1:
        P.emit()
        return nc, dbg_outs


    def sc(i):
        return small[:, i:i + 1]
    pid, dd, i32_, isC, ff, inv, invC, invR, sgn, tmpc = [sc(i) for i in range(10)]
    P.op("gpsimd", lambda e: e.iota(small[:, 0:1], pattern=[[0, 1]], base=0, channel_multiplier=1,
                                    allow_small_or_imprecise_dtypes=True), (), ["small"])
    TS("vector", tmpc, pid, 64.0, -64.0, ALU.is_ge, ALU.mult, ["small"], ["small"])
    TT("vector", dd, pid, tmpc, ALU.add, ["small"], ["small"])
    TS("vector", sgn, dd, 32.0, None, ALU.is_ge, None, ["small"], ["small"])
    TS("vector", tmpc, sgn, -32.0, None, ALU.mult, None, ["small"], ["small"])
    TT("vector", i32_, dd, tmpc, ALU.add, ["small"], ["small"])
    TS("vector", isC, i32_, 16.0, None, ALU.is_ge, None, ["small"], ["small"])
    TS("vector", tmpc, isC, -16.0, None, ALU.mult, None, ["small"], ["small"])
    TT("vector", ff, i32_, tmpc, ALU.add, ["small"], ["small"])
    ACT(inv, ff, AF.Exp, ["small"], ["small"], scale=-float(np.log(10000.0) / 16.0))
    TT("vector", invC, inv, isC, ALU.mult, ["small"], ["small"])
    TT("vector", invR, inv, invC, ALU.subtract, ["small"], ["small"])
    TS("vector", sgn, sgn, 2.0, -1.0, ALU.mult, ALU.add, ["small"], ["small"])
    rrA = P.sb("rrA", [128, W], F32, off=qT_off)
    rrI = P.sb("rrI", [128, W], I32, off=qT_off + W * 4)
    ang = P.sb("ang", [128, W], F32, off=uT_off)
    P.op("gpsimd", lambda e: e.iota(COS[:], pattern=[[1, 36], [0, 64]], base=0, channel_multiplier=0,
                                    allow_small_or_imprecise_dtypes=True), (), ["COS"])
    P.op("gpsimd", lambda e: e.iota(SINS[:], pattern=[[0, 36], [1, 64]], base=0, channel_multiplier=0,
                                    allow_small_or_imprecise_dtypes=True), (), ["SINS"])
    HW_ = W // 2
    TWO_PI = float(2 * np.pi)
    for hh in range(2):
        sl = slice(hh * HW_, (hh + 1) * HW_)
        TS("vector", COS[:, sl], COS[:, sl], metat[:, 2:3], None, ALU.add, None, ["COS", "metat"], ["COS"])
        TS("vector", COS[:, sl], COS[:, sl], invR, None, ALU.mult, None, ["COS", "small"], ["COS"])
        TS("vector", SINS[:, sl], SINS[:, sl], invC, None, ALU.mult, None, ["SINS", "small"], ["SINS"])
    TT("vector", ang[:], COS[:], SINS[:], ALU.add, ["COS", "SINS"], ["ang"])

    def range_reduce_sin(dst, dstn, offset):
        TS("vector", rrA[:], ang[:], 1.0 / TWO_PI, offset / TWO_PI + 8.5, ALU.mult, ALU.add, ["ang"], ["rrA"])
        CP("vector", rrI[:], rrA[:], ["rrA"], ["rrI"])
        CP("vector", rrA[:], rrI[:], ["rrI"], ["rrA"])
        TS("vector", rrA[:], rrA[:], -TWO_PI, 8 * TWO_PI + offset, ALU.mult, ALU.add, ["rrA"], ["rrA"])
        TT("vector", dst[:], ang[:], rrA[:], ALU.add, ["ang", "rrA"], [dstn])
        TS("vector", rrA[:], dst[:], float(np.pi), -TWO_PI, ALU.is_gt, ALU.mult, [dstn], ["rrA"])
        TT("vector", dst[:], dst[:], rrA[:], ALU.add, [dstn, "rrA"], [dstn])
        TS("vector", rrA[:], dst[:], -float(np.pi), TWO_PI, ALU.is_lt, ALU.mult, [dstn], ["rrA"])
        TT("vector", dst[:], dst[:], rrA[:], ALU.add, [dstn, "rrA"], [dstn])
        ACT(dst[:], dst[:], AF.Sin, [dstn], [dstn])

    range_reduce_sin(SINS, "SINS", 0.0)
    range_reduce_sin(COS, "COS", float(np.pi / 2))
    for hh in range(2):
        sl = slice(hh * HW_, (hh + 1) * HW_)
        TS("vector", SINS[:, sl], SINS[:, sl], sgn, None, ALU.mult, None, ["SINS", "small"], ["SINS"])

    w_in_v = w_in.rearrange("(k p) n -> p k n", p=128)
    DMA("sync", "d_stg", stg[:], w_in_v[:, :, 0:640], [], ["stg"])
    qd = WIN[:, :, 0:512].rearrange("p k (c h d) -> p k c h d", c=4, h=2, d=64)
    qs = stg[:, :, 0:512].rearrange("p k (h c d) -> p k c h d", h=2, c=4, d=64)
    for h in range(2):
        ACT(qd[:, :, :, h, :], qs[:, :, :, h, :], AF.Copy, ["stg"], [("WIN", "q", h)])
    qd2 = WIN[:, :, 512:1024].rearrange("p k (c h s d) -> p k c h s d", c=4, h=2, s=2, d=32)
    qs2 = stg[:, :, 0:512].rearrange("p k (h c s d) -> p k c h s d", h=2, c=4, s=2, d=32)
    for h in range(2):
        for s in range(2):
            ACT(qd2[:, :, :, h, s, :], qs2[:, :, :, h, 1 - s, :], AF.Copy, ["stg"], [("WIN", "qsw", h, s)])
    ACT(WIN[:, :, 1024:1152], stg[:, :, 512:640], AF.Copy, ["stg"], [("WIN", "k")])
    kd2 = WIN[:, :, 1152:1280].rearrange("p k (h s d) -> p k h s d", h=2, s=2, d=32)
    ks2 = stg[:, :, 512:640].rearrange("p k (h s d) -> p k h s d", h=2, s=2, d=32)
    for s in range(2):
        ACT(kd2[:, :, :, s, :], ks2[:, :, :, 1 - s, :], AF.Copy, ["stg"], [("WIN", "ksw", s)])
    WINQ = [("WIN", "q", 0), ("WIN", "q", 1)]
    WINQS = [("WIN", "qsw", h, s) for h in range(2) for s in range(2)]
    WINK = [("WIN", "k")]
    WINKS = [("WIN", "ksw", 0), ("WIN", "ksw", 1)]
    for (nm, d0, s0, n) in (("v", 1280, 640, 128), ("bg", 1408, 768, 512), ("cg", 1920, 1280, 512), ("hv", 2432, 1792, 512)):
        P.dma("gpsimd", "d_win_" + nm, (lambda d0=d0, s0=s0, n=n: (lambda e: e.dma_start(out=WIN[:, :, d0:d0 + n], in_=w_in_v[:, :, s0:s0 + n])))(),
              [], [("WIN", nm)])
    for kk in range(3):
        for c4 in range(4):
            P.dma("sync", "d_cw", (lambda kk=kk, c4=c4: (lambda e: e.dma_start(
                out=cw[:, c4, kk:kk + 1], in_=conv_w[kk, c4 * 128:(c4 + 1) * 128].rearrange("(p o) -> p o", o=1))))(),
                [], [("cw", kk, c4)])
    MSET("vector", Vt[:, :, :, 64:65], 1.0, [("Vt", "ones")])
    MSET("vector", Vc[:, :, :, 64:65], 1.0, [("Vc", "ones")])

    xt_rr = [0]

    def norm_mod(src_rows, Gn, Sn, hbuf, hname, extra_r=()):
        i = xt_rr[0] % 2
        xt_rr[0] += 1
        xtile, xn = xt[i], f"xt{i}"
        DMA("sync", "d_" + xn, xtile[:], src_rows, list(extra_r), [xn])
        norm_mod_sb(xtile, xn, Gn, Sn, hbuf, hname)
        return xtile, xn

    tf2 = P.sb("tf2", [128, 1024], F32, off=bgT_off + 16384)
    nm_rr = [0]

    def norm_mod_sb(xtile, xn, Gn, Sn, hbuf, hname):
        pi = nm_rr[0] % 2
        nm_rr[0] += 1
        tfx, tfn = (tf, "tf") if pi == 0 else (tf2, "tf2")
        ss = small[:, 16 + 2 * pi:17 + 2 * pi]
        rstd = small[:, 17 + 2 * pi:18 + 2 * pi]
        ssn, rsn = f"ss{pi}", f"rstd{pi}"
        ACT(tfx[:], xtile[:], AF.Square, [xn], [tfn])
        RED("vector", ss, tfx[:], ALU.add, [tfn], [ssn])
        TS("vector", rstd, ss, 1.0 / 1024.0, 1e-6, ALU.mult, ALU.add, [ssn], [rsn])
        ACT(rstd, rstd, AF.Ln, [rsn], [rsn])
        ACT(rstd, rstd, AF.Exp, [rsn], [rsn], scale=-0.5)
        ACT(tfx[:], xtile[:], AF.Copy, [xn, rsn], [tfn], scale=rstd)
        TT("vector", tfx[:], tfx[:], rows[Gn][:], ALU.mult, [tfn] + rowdeps(Gn), [tfn])
        TT("vector", hbuf[:], tfx[:], rows[Sn][:], ALU.add, [tfn] + rowdeps(Sn), [hname])

    def transpose_to(hbuf, hname, dst, dst_name):
        pb, pbn = psb()
        pbv = pb[:].rearrange("p (k t) -> p k t", k=8)
        for k in range(8):
            TR(pbv[:, k, :], hbuf[:, k * 128:(k + 1) * 128], ident_b[:], [hname, "ident_b"], [(pbn, k)])
        ACT(dst, pbv, AF.Copy, [(pbn, k) for k in range(8)], [dst_name])

    hcT = hT[0]
    for t in range(2):
        norm_mod(ctx[t * 128:(t + 1) * 128, :], "cG1", "cS1", hb[t % 2], f"hb{t % 2}")
        transpose_to(hb[t % 2], f"hb{t % 2}", hcT[:, :, t * 128:(t + 1) * 128], ("hT0", t))
    ps, psn = psf("a")
    for k in range(8):
        MM(ps[:, 0:256], WIN[:, k, 1024:1152], hcT[:, k, 0:256], k == 0, k == 7,
           WINK + [("hT0", 0), ("hT0", 1)], [psn])
    ACT(kcT[:], ps[:, 0:256], AF.Copy, [psn], ["kcT"])
    for t in range(2):
        ps, psn = psf("a")
        for k in range(8):
            MM(ps[:, 0:128], hcT[:, k, t * 128:(t + 1) * 128], WIN[:, k, 1280:1408], k == 0, k == 7,
               [("WIN", "v"), ("hT0", t)], [psn])
        ACT(Vc[:, t, :, 0:64], ps[:, 0:128].rearrange("p (h d) -> p h d", h=2), AF.Copy, [psn], [("Vc", t)])

    if "ctx" in dbg:
        d1 = dbg_out("kcT", [128, 256], BF16)
        d2 = dbg_out("Vc", [128, 2 * 2 * 65], BF16)
        e1 = DMA("sync", "d_dbg", d1, kcT[:], ["kcT"], ["dbg1"])
        e2 = DMA("sync", "d_dbg", d2, Vc[:].rearrange("p a b c -> p (a b c)"), [("Vc", 0), ("Vc", 1), ("Vc", "ones")], ["dbg2"])
        P.finish("sync", [e1, e2])
    if stage <= 2:
        P.emit()
        return nc, dbg_outs

    chunks = [(0, 128, False)] + [(128 + 512 * i, 512, True) for i in range(4)] + [(2176, 128, False)]

    def prep_norm(ci, tiles):
        w0, n, central = chunks[ci]
        for t in tiles:
            norm_mod(x[w0 + t * 128:w0 + (t + 1) * 128, :], "G1", "S1", hb[t % 2], f"hb{t % 2}")

    def prep_trans(ci, tiles):
        hTc, hTn = hT[ci % 2], f"hT{ci % 2}"
        for t in tiles:
            transpose_to(hb[t % 2], f"hb{t % 2}", hTc[:, :, t * 128:(t + 1) * 128], (hTn, t))

    def build_items(ci):
        w0, n, central = chunks[ci]
        hTc, hTn = hT[ci % 2], f"hT{ci % 2}"
        ntile = n // 128
        hdeps = [(hTn, t) for t in range(ntile)]
        items = []

        def proj(col0, wdeps, cons):
            ps, psn = psf(cons)
            for k in range(8):
                MM(ps[:, 0:n], WIN[:, k, col0:col0 + 128], hTc[:, k, 0:n], k == 0, k == 7, wdeps + hdeps, [psn])
            return ps, psn

        def rope_out(col0, colsw, wd, wsd, dst, dstn):
            def f():
                pa, pan = proj(col0, wd, "v")
                pb_, pbn_ = proj(colsw, wsd, "v")
                TT("vector", rt1[:, 0:n], pa[:, 0:n], COS[:, w0:w0 + n], ALU.mult, [pan, "COS"], ["rt1"])
                TT("vector", rt2[:, 0:n], pb_[:, 0:n], SINS[:, w0:w0 + n], ALU.mult, [pbn_, "SINS"], ["rt2"])
                TT("vector", dst, rt1[:, 0:n], rt2[:, 0:n], ALU.add, ["rt1", "rt2"], [dstn])
            return f

        def v_item(t):
            def f():
                ps, psn = psf("a")
                for k in range(8):
                    MM(ps[:, 0:128], hTc[:, k, t * 128:(t + 1) * 128], WIN[:, k, 1280:1408], k == 0, k == 7,
                       [("WIN", "v"), (hTn, t)], [psn])
                wt = w0 // 128 + t
                ACT(Vt[:, wt, :, 0:64], ps[:, 0:128].rearrange("p (h d) -> p h d", h=2), AF.Copy, [psn], [("Vt", wt)])
            return f

        def bg_item(c):
            def f():
                ps, psn = proj(1408 + c * 128, [("WIN", "bg")], "a")
                ACT(bgT[:, c, w0 - 128:w0 - 128 + n], ps[:, 0:n], AF.Copy, [psn], [("bgT", c, ci)])
            return f

        def u_item(c):
            def f():
                pc, pcn = proj(1920 + c * 128, [("WIN", "cg")], "a")
                ph, phn = proj(2432 + c * 128, [("WIN", "hv")], "v")
                ACT(cgs[:, 0:n], pc[:, 0:n], AF.Copy, [pcn], ["cgs"])
                TT("vector", uT[:, c, w0:w0 + n], ph[:, 0:n], cgs[:, 0:n], ALU.mult, [phn, "cgs"], [("uT", c, ci)])
            return f

        if central:
            for c in range(4):
                items.append(rope_out(c * 128, 512 + c * 128, WINQ, WINQS, qT[:, c, w0:w0 + n], ("qT", c, ci)))
        items.append(rope_out(1024, 1152, WINK, WINKS, kT[:, w0:w0 + n], ("kT", ci)))
        for t in range(ntile):
            items.append(v_item(t))
        for c in range(4):
            if central:
                items.append(bg_item(c))
            items.append(u_item(c))
        return items

    def tiles_of(ci):
        return list(range(chunks[ci][1] // 128))

    prep_norm(0, tiles_of(0))
    prep_trans(0, tiles_of(0))
    for ci in range(len(chunks)):
        items = build_items(ci)
        nxt = ci + 1 if ci + 1 < len(chunks) else None
        half = (len(items) + 1) // 2
        if nxt is not None:
            prep_norm(nxt, tiles_of(nxt)[0:2])
        for f in items[:half]:
            f()
        if nxt is not None:
            prep_trans(nxt, tiles_of(nxt)[0:2])
            prep_norm(nxt, tiles_of(nxt)[2:4])
        for f in items[half:]:
            f()
        if nxt is not None:
            prep_trans(nxt, tiles_of(nxt)[2:4])

    if "proj" in dbg:
        d1 = dbg_out("qT", [128, 4 * W], BF16)
        d2 = dbg_out("kT", [128, W], BF16)
        d3 = dbg_out("Vt", [128, 18 * 130], BF16)
        d4 = dbg_out("uT", [128, 4 * W], BF16)
        d5 = dbg_out("bgT", [128, 4 * 2048], BF16)
        allq = [("qT", c, ci) for c in range(4) for ci in range(1, 5)]
        allk = [("kT", ci) for ci in range(6)]
        allv = [("Vt", t) for t in range(18)] + [("Vt", "ones")]
        allu = [("uT", c, ci) for c in range(4) for ci in range(6)]
        allb = [("bgT", c, ci) for c in range(4) for ci in range(1, 5)]
        evs = [DMA("sync", "d_dbg", d1, qT[:].rearrange("p a b -> p (a b)"), allq, ["dbg1"]),
               DMA("sync", "d_dbg", d2, kT[:], allk, ["dbg2"]),
               DMA("sync", "d_dbg", d3, Vt[:].rearrange("p a b c -> p (a b c)"), allv, ["dbg3"]),
               DMA("sync", "d_dbg", d4, uT[:].rearrange("p a b -> p (a b)"), allu, ["dbg4"]),
               DMA("sync", "d_dbg", d5, bgT[:].rearrange("p a b -> p (a b)"), allb, ["dbg5"])]
        P.finish("sync", evs)
    if stage <= 3:
        P.emit()
        return nc, dbg_outs

    ALLWIN = WINQ + WINQS + WINK + WINKS + [("WIN", nm) for nm in ("v", "bg", "cg", "hv")]
    o2 = REG0 + 32768
    PL = [P.sb(f"PL{i}", [128, 384], BF16, off=o2 + i * 768) for i in range(2)]; o2 += 1536
    PC = [P.sb(f"PC{i}", [128, 256], BF16, off=o2 + i * 512) for i in range(2)]; o2 += 1024
    att_tm = P.sb("att_tm", [128, 512], BF16, off=o2); o2 += 1024
    rec = P.sb("rec", [128, 8], F32, off=o2); o2 += 64
    cvt = [P.sb(f"cvt{i}", [128, 512], F32, off=o2 + i * 2048) for i in range(2)]; o2 += 4096
    assert o2 <= REG0 + 47104

    def kchunk(wb):
        return 0 if wb == 0 else (5 if wb == 17 else 1 + (wb - 1) // 4)

    VONES = [("Vt", "ones")]
    TS("vector", uT[:, :, 127:128], uT[:, :, 127:128], metat[:, 0:1], None, ALU.mult, None,
       [("uT", c, 0) for c in range(4)] + ["metat"], [("uT", c, 0) for c in range(4)])
    TS("vector", uT[:, :, 2176:2177], uT[:, :, 2176:2177], metat[:, 1:2], None, ALU.mult, None,
       [("uT", c, 5) for c in range(4)] + ["metat"], [("uT", c, 5) for c in range(4)])

    def conv_unit(tcn, c):
        w0 = 128 + tcn * 512
        ud = [("uT", c, ci) for ci in (tcn, tcn + 1, tcn + 2)]
        cwd = [("cw", kk, c4) for kk in range(3) for c4 in range(4)]
        TS("vector", cvt[0][:], uT[:, c, w0 - 1:w0 + 511], cw[:, c, 0:1], None, ALU.mult, None, ud + cwd, ["cvt0"] + ALLWIN)
        TS("vector", cvt[1][:], uT[:, c, w0:w0 + 512], cw[:, c, 1:2], None, ALU.mult, None, ud + cwd, ["cvt1"] + ALLWIN)
        TT("vector", cvt[0][:], cvt[0][:], cvt[1][:], ALU.add, ["cvt0", "cvt1"], ["cvt0"])
        TS("vector", cvt[1][:], uT[:, c, w0 + 1:w0 + 513], cw[:, c, 2:3], None, ALU.mult, None, ud + cwd, ["cvt1"])
        TT("vector", cvt[0][:], cvt[0][:], cvt[1][:], ALU.add, ["cvt0", "cvt1"], ["cvt0"])
        TT("vector", mixT[:, 4 + c, tcn * 512:(tcn + 1) * 512], cvt[0][:], bgT[:, c, tcn * 512:(tcn + 1) * 512], ALU.mult,
           ["cvt0", ("bgT", c, tcn + 1)], [("mixT", "conv", c, tcn)] + ALLWIN)

    sbanks = [3, 4, 5, 2]
    srr = [0]

    def sbank():
        bi = sbanks[srr[0] % 4]
        srr[0] += 1
        return PSF[bi], f"psf{bi}"

    for i in range(1, 17):
        ci_q = 1 + (i - 1) // 4
        mv = 1 if i == 1 else (2 if i == 16 else 0)
        pvs = [(PSF[0], "psf0"), (PSF[1], "psf1")]

        def S_(hn, i=i, ci_q=ci_q):
            half, c = hn // 4, hn % 4
            r0 = half * 64
            sl, sln = sbank()
            sc_, scn = sbank()
            qsl = qT[r0:r0 + 64, c, i * 128:(i + 1) * 128]
            for kb in range(3):
                wb = i - 1 + kb
                MM(sl[:, kb * 128:(kb + 1) * 128], kT[r0:r0 + 64, wb * 128:(wb + 1) * 128], qsl, True, True,
                   [("qT", c, ci_q), ("kT", kchunk(wb))], [sln])
            for cb in range(2):
                MM(sc_[:, cb * 128:(cb + 1) * 128], kcT[r0:r0 + 64, cb * 128:(cb + 1) * 128], qsl, True, True,
                   [("qT", c, ci_q), "kcT"], [scn])
            return sl, sln, sc_, scn

        def EPV_(hn, st, i=i, mv=mv, pvs=pvs):
            sl, sln, sc_, scn = st
            half, c = hn // 4, hn % 4
            j = hn % 2
            ACT(PL[j][:], sl[:, 0:384], AF.Exp, [sln], [f"PL{j}"] + ALLWIN, scale=0.125)
            ACT(PC[j][:], sc_[:, 0:256], AF.Exp, [scn], [f"PC{j}"] + ALLWIN, scale=0.125)
            TT("vector", PL[j][:], PL[j][:], mask3[:, mv, :], ALU.mult,
               [f"PL{j}", ("mask3", mv), ("mask3", mv, 1), ("mask3", mv, 2)], [f"PL{j}"])
            pv, pvn = pvs[half]
            pvr = pv[:, c * 65:(c + 1) * 65]
            for kb in range(3):
                wb = i - 1 + kb
                MM(pvr, PL[j][:, kb * 128:(kb + 1) * 128], Vt[:, wb, half, :], kb == 0, False,
                   [f"PL{j}", ("Vt", wb)] + VONES, [pvn])
            for cb in range(2):
                MM(pvr, PC[j][:, cb * 128:(cb + 1) * 128], Vc[:, cb, half, :], False, cb == 1,
                   [f"PC{j}", ("Vc", cb), ("Vc", "ones")], [pvn])

        st = S_(0)
        for hn in range(8):
            nxt = S_(hn + 1) if hn < 7 else None
            EPV_(hn, st)
            st = nxt
        for b in range(2):
            pv, pvn = pvs[b]
            pvv = pv[:, 0:260].rearrange("p (h e) -> p h e", h=4)
            TT("vector", rec[:, b * 4:(b + 1) * 4].unsqueeze(2), pvv[:, :, 64:65], esink[:, b * 4:(b + 1) * 4].unsqueeze(2),
               ALU.add, [pvn, "esink"], [("rec", b)] + ALLWIN)
            P.op("vector", (lambda b=b: (lambda e: e.reciprocal(rec[:, b * 4:(b + 1) * 4], rec[:, b * 4:(b + 1) * 4])))(),
                 [("rec", b)], [("rec", b)])
            TT("vector", att_tm[:, b * 256:(b + 1) * 256].rearrange("p (h d) -> p h d", h=4), pvv[:, :, 0:64],
               rec[:, b * 4:(b + 1) * 4].unsqueeze(2).to_broadcast([128, 4, 64]), ALU.mult,
               [pvn, ("rec", b)], [("att_tm", b)] + ALLWIN)
        pb, pbn = psb()
        pbv = pb[:, 0:512].rearrange("p (k t) -> p k t", k=4)
        for cc in range(4):
            TR(pbv[:, cc, :], att_tm[:, cc * 128:(cc + 1) * 128], ident_b[:], [("att_tm", cc // 2), "ident_b"], [(pbn, cc)])
        ACT(mixT[:, 0:4, (i - 1) * 128:i * 128], pbv, AF.Copy, [(pbn, cc) for cc in range(4)],
            [("mixT", "att", i - 1)] + ALLWIN)
        conv_unit((i - 1) // 4, (i - 1) % 4)

    if stage <= 4:
        P.emit()
        return nc, dbg_outs

    HTALL = [(f"hT{a}", t) for a in range(2) for t in range(4)]
    P.dma("gpsimd", "d_wo", lambda e: e.dma_start(out=wo[:], in_=w_out.rearrange("(k p) n -> p k n", p=128)), [], ["wo"] + HTALL)
    QALL = [("qT", c, ci) for c in range(4) for ci in range(1, 5)]
    o3 = qT_off
    h2T = [P.sb(f"h2T{i}", [128, 8, 128], BF16, off=o3 + i * 2048) for i in range(2)]; o3 += 4096
    CONVDEAD = [("uT", c, ci) for c in range(4) for ci in range(6)] + [("bgT", c, ci) for c in range(4) for ci in range(1, 5)]
    rows_off = REG0 - 8 * 4096
    affTM = P.sb("affTM", [128, 16, 16], F32, off=rows_off)
    gm = P.sb("gm", [128, 16, 16], F32, off=rows_off + 1024)
    thr = P.sb("thr", [128, 16], F32, off=rows_off + 2048)
    affT = P.sb("affT", [16, 2048], F32, off=o3); o3 += 8192
    wr = P.sb("wr", [128, 8, 16], BF16, off=o3); o3 += 256
    sm = P.sb("sm", [128, 64], F32, off=o3); o3 += 256
    assert o3 <= qT_off + 4 * W * 2
    P.dma("gpsimd", "d_wr", lambda e: e.dma_start(out=wr[:], in_=w_router.rearrange("(k p) e -> p k e", p=128)), [], ["wr"] + QALL)
    def a3_A(tile):
        tcn = tile // 4
        mdeps = [("mixT", "att", tile)] + [("mixT", "conv", c, tcn) for c in range(4)]
        i = xt_rr[0] % 2
        xt_rr[0] += 1
        xtile, xn = xt[i], f"xt{i}"
        DMA("sync", "d_" + xn, xtile[:], x[128 + tile * 128:256 + tile * 128, :], [], [xn])
        for n in range(2):
            ps, psn = psf("v")
            for k in range(8):
                MM(ps[:], mixT[:, k, tile * 128:(tile + 1) * 128], wo[:, k, n * 512:(n + 1) * 512], k == 0, k == 7,
                   mdeps + ["wo"], [psn])
            TT("vector", tf[:, n * 512:(n + 1) * 512], ps[:], rows["GT1"][:, n * 512:(n + 1) * 512], ALU.mult,
               [psn] + rowdeps("GT1"), ["tf"])
        TT("vector", xtile[:], xtile[:], tf[:], ALU.add, [xn, "tf"], [xn])
        DMA("sync", "d_x1d_" + xn, x1d[tile * 128:(tile + 1) * 128, :], xtile[:], [xn], [("x1d", tile)])
        j = tile % 2
        norm_mod_sb(xtile, xn, "G2", "S2", hb[j], f"hb{j}")
        DMA("sync", f"d_h2loc{j}", h2loc[tile * 128:(tile + 1) * 128, :], hb[j][:], [f"hb{j}"], [("h2loc", tile)])

    def a3_B(tile):
        j = tile % 2
        h2v = h2T[j][:]
        pb, pbn = psb()
        pbv = pb[:].rearrange("p (k t) -> p k t", k=8)
        for k in range(8):
            TR(pbv[:, k, :], hb[j][:, k * 128:(k + 1) * 128], ident_b[:], [f"hb{j}", "ident_b"], [(pbn, k)])
        ACT(h2v, pbv, AF.Copy, [(pbn, k) for k in range(8)], [f"h2T{j}"] + QALL)
        ps, psn = psf("v")
        for k in range(8):
            MM(ps[:, 0:16], h2T[j][:, k, :], wr[:, k, :], k == 0, k == 7, [f"h2T{j}", "wr"], [psn])
        mx, nmx, ssum, ex = sm[:, 0:1], sm[:, 1:2], sm[:, 2:3], sm[:, 16:32]
        af = affTM[:, tile, :]
        RED("vector", mx, ps[:, 0:16], ALU.max, [psn], ["sm_mx"])
        TS("vector", nmx, mx, -1.0, None, ALU.mult, None, ["sm_mx"], ["sm_nmx"])
        ACT(ex, ps[:, 0:16], AF.Exp, [psn, "sm_nmx"], ["sm_ex"], bias=nmx)
        RED("vector", ssum, ex, ALU.add, ["sm_ex"], ["sm_sum"])
        P.op("vector", lambda e: e.reciprocal(sm[:, 2:3], sm[:, 2:3]), ["sm_sum"], ["sm_sum"])
        TS("vector", af, ex, ssum, None, ALU.mult, None, ["sm_ex", "sm_sum"], [("affTM", tile)])
        pt, ptn = psf("a")
        TR(pt[0:16, 0:128], af, ident_f[:], [("affTM", tile), "ident_f"], [ptn])
        ACT(affT[:, tile * 128:(tile + 1) * 128], pt[0:16, 0:128], AF.Copy, [ptn], [("affT", tile)] + QALL)

    a3_A(0)
    for tile in range(16):
        if tile + 1 < 16:
            a3_A(tile + 1)
        a3_B(tile)
    DMA("sync", "d_affloc", affloc, affT[:], [("affT", t) for t in range(16)], ["affloc"])

    if "a3" in dbg:
        d1 = dbg_out("x1", [2048, 1024])
        d2 = dbg_out("aff", [16, 2048])
        d3 = dbg_out("h2", [2048, 1024], BF16)
        e1 = DMA("sync", "d_dbg1", d1, x1d, [("x1d", t) for t in range(16)], ["dbg1"])
        e2 = DMA("sync", "d_dbg2", d2, affloc, ["affloc"], ["dbg2"])
        e3 = DMA("sync", "d_dbg3", d3, h2loc, [("h2loc", t) for t in range(16)], ["dbg3"])
        P.finish("sync", [e1, e2, e3])
    if stage <= 5:
        P.emit()
        return nc, dbg_outs

    FENCE = [k for k in P.res.keys() if not (isinstance(k, str) and (k.startswith("psf") or k in ("ident_b", "ident_f", "ones_b", "metat")))
             and not (isinstance(k, tuple) and k[0] in ("row", "h2T", "affTM", "x1d", "h2loc"))]
    P.dma("gpsimd", "d_ag_aff", lambda e: e.collective_compute("AllGather", ALU.bypass, replica_groups=GROUPS,
                                                               ins=[affloc.opt()], outs=[affall.opt()]),
          ["affloc"], ["affall", "agchain"], inc=1)
    wslot = [P.sb(f"wslot{i}", [128, 8, 1024], BF16, off=REG0 + i * 16384) for i in range(4)]
    wdt_ = P.sb("wd_", [128, 8, 1024], BF16, off=REG0 + 81920)
    wgv = w_gate.rearrange("e (k p) n -> e p k n", p=128)
    wuv = w_up.rearrange("e (k p) n -> e p k n", p=128)
    wdv = w_down.rearrange("e (k p) n -> e p k n", p=128)
    P.dma("gpsimd", "d_ws0", lambda e: e.dma_start(out=wslot[0][:], in_=wgv[0]), [], ["ws0"] + FENCE)
    P.dma("gpsimd", "d_ws1", lambda e: e.dma_start(out=wslot[1][:], in_=wuv[0]), [], ["ws1"] + FENCE)
    P.dma("gpsimd", "d_wd", lambda e: e.dma_start(out=wdt_[:], in_=wdv[0]), [], ["wd_"] + FENCE)
    NTB, NITB, NE = 16, 7, 4
    ob = bgT_off + 32768
    AFt = P.sb("AFt", [128, NE, 64], F32, off=ob); ob += 1024
    FR = P.sb("FR", [128, NE, NTB], F32, off=ob); ob += 256
    Tt = P.sb("Tt", [128, NE, NTB], F32, off=ob); ob += 256
    tmpa = P.sb("tmpa", [128, NE, NTB], F32, off=ob); ob += 256
    get = P.sb("get", [128, NE, NTB], F32, off=ob); ob += 256
    cntb = P.sb("cntb", [128, NE * NTB], BF16, off=ob); ob += 128
    lo = P.sb("lo", [128, NE], F32, off=ob); ob += 64
    hi = P.sb("hi", [128, NE], F32, off=ob); ob += 64
    wdt = P.sb("wdt", [128, NE], F32, off=ob); ob += 64
    red = P.sb("red", [128, NE], F32, off=ob); ob += 64
    aix = P.sb("aix", [128, 8], F32, off=ob); ob += 64
    AIDX = P.sb("AIDX", [128, NE], I32, off=ob); ob += 64
    assert ob <= rt1_off + 6144
    cmpb = P.sb("cmpb", [128, NE, NTB, 64], BF16, off=REG0 + 49152)
    P.op("gpsimd", lambda e: e.iota(aix[:, 0:1], pattern=[[0, 1]], base=0, channel_multiplier=1,
                                    allow_small_or_imprecise_dtypes=True), (), ["aix"] + FENCE)
    TS("vector", aix[:, 1:2], aix[:, 0:1], 32.0, None, ALU.is_ge, None, ["aix"], ["aix"])
    for thv in (64.0, 96.0):
        TS("vector", aix[:, 2:3], aix[:, 0:1], thv, None, ALU.is_ge, None, ["aix"], ["aix"])
        TT("vector", aix[:, 1:2], aix[:, 1:2], aix[:, 2:3], ALU.add, ["aix"], ["aix"])
    TS("vector", aix[:, 1:2], aix[:, 1:2], 480.0, None, ALU.mult, None, ["aix"], ["aix"])
    TT("vector", aix[:, 1:2], aix[:, 1:2], aix[:, 0:1], ALU.add, ["aix"], ["aix"])
    TS("vector", aix[:, 1:2], aix[:, 1:2], metat[:, 7:8], None, ALU.add, None, ["aix", "metat"], ["aix"])
    for k in range(NE):
        TS("vector", aix[:, 4 + k:5 + k], aix[:, 1:2], 32.0 * k, None, ALU.add, None, ["aix"], ["aix"])
    CP("vector", AIDX[:], aix[:, 4:8], ["aix"], ["AIDX"])
    affrows = affall.rearrange("a (p j) -> (a p) j", j=64)
    for k in range(NE):
        P.dma("gpsimd", "d_AFt", (lambda k=k: (lambda e: e.indirect_dma_start(
            out=AFt[:, k, :], out_offset=None, in_=affrows, in_offset=bass.IndirectOffsetOnAxis(ap=AIDX[:, k:k + 1], axis=0))))(),
            ["affall", "AIDX"], [("AFt", k)] + FENCE)
    AFD = [("AFt", k) for k in range(NE)]
    P.op("gpsimd", lambda e: e.iota(FR[:], pattern=[[0, NE], [1, NTB]], base=1, channel_multiplier=0,
                                    allow_small_or_imprecise_dtypes=True), (), ["FR"] + FENCE)
    TS("vector", FR[:], FR[:], 1.0 / (NTB + 1), None, ALU.mult, None, ["FR"], ["FR"])
    MSET("vector", lo[:], 0.0, ["lo"] + FENCE)
    MSET("vector", hi[:], 1.0, ["hi"])
    for it in range(NITB):
        TT("vector", wdt[:], hi[:], lo[:], ALU.subtract, ["hi", "lo"], ["wdt"])
        TT("vector", Tt[:], FR[:], wdt[:].unsqueeze(2).to_broadcast([128, NE, NTB]), ALU.mult, ["FR", "wdt"], ["Tt"])
        TT("vector", Tt[:], Tt[:], lo[:].unsqueeze(2).to_broadcast([128, NE, NTB]), ALU.add, ["Tt", "lo"], ["Tt"])
        TT("vector", cmpb[:], AFt[:].unsqueeze(2).to_broadcast([128, NE, NTB, 64]),
           Tt[:].unsqueeze(3).to_broadcast([128, NE, NTB, 64]), ALU.is_ge, AFD + ["Tt"], ["cmpb"] + FENCE)
        RED("vector", tmpa[:], cmpb[:], ALU.add, ["cmpb"], ["tmpa"])
        CP("vector", cntb[:], tmpa[:].rearrange("p e k -> p (e k)"), ["tmpa"], ["cntb"])
        ps, psn = psf("v")
        MM(ps[:, 0:NE * NTB], ones_b[:], cntb[:], True, True, ["cntb", "ones_b"], [psn])
        TS("vector", get[:].rearrange("p e k -> p (e k)"), ps[:, 0:NE * NTB], 1024.0, None, ALU.is_ge, None, [psn], ["get"])
        TT("vector", tmpa[:], Tt[:], get[:], ALU.mult, ["Tt", "get"], ["tmpa"])
        RED("vector", red[:], tmpa[:], ALU.max, ["tmpa"], ["red"])
        TT("vector", lo[:], lo[:], red[:], ALU.max, ["lo", "red"], ["lo"])
        TS("vector", tmpa[:], get[:], 2.0, None, ALU.mult, None, ["get"], ["tmpa"])
        TT("vector", tmpa[:], tmpa[:], Tt[:], ALU.add, ["tmpa", "Tt"], ["tmpa"])
        RED("vector", red[:], tmpa[:], ALU.min, ["tmpa"], ["red"])
        TT("vector", hi[:], hi[:], red[:], ALU.min, ["hi", "red"], ["hi"])
    thr = lo

    if "thr" in dbg:
        d1 = dbg_out("thr", [128, 4])
        e1 = DMA("sync", "d_dbg1", d1, lo[:], ["lo"], ["dbg1"])
        P.finish("sync", [e1])
    if stage <= 6:
        P.emit()
        return nc, dbg_outs

    for j4 in range(4):
        P.dma("gpsimd", "d_ag_h2", (lambda j4=j4: (lambda e: e.collective_compute(
            "AllGather", ALU.bypass, replica_groups=GROUPS,
            ins=[h2loc[j4 * 512:(j4 + 1) * 512, :].opt()], outs=[h2all[j4 * 2048:(j4 + 1) * 2048, :].opt()])))(),
            [("h2loc", t) for t in range(j4 * 4, j4 * 4 + 4)] + ["agchain"], [("h2all", j4), "agchain"], inc=1)
    H2ALLD = [("h2all", j4) for j4 in range(4)]
    TAB = P.sb("TAB", [128, 4, 128], F32, off=qT_off)
    ob2 = kT_off
    ones64 = P.sb("ones64", [128, 64], F32, off=ob2); ob2 += 256
    n4 = P.sb("n4", [128, 4], F32, off=ob2); ob2 += 64
    t16 = P.sb("t16", [128, 16], F32, off=ob2); ob2 += 64
    rhsU = P.sb("rhsU", [128, 4, 128], BF16, off=ob2); ob2 += 1024
    rhsI = P.sb("rhsI", [128, 4, 128], BF16, off=ob2); ob2 += 1024
    offs_sb = P.sb("offs_sb", [128, 4, 128], F32, off=ob2); ob2 += 2048
    nrow_sb = P.sb("nrow_sb", [128, 4, 128], F32, off=ob2); ob2 += 2048
    sval = P.sb("sval", [128, 8], F32, off=ob2); ob2 += 64
    koffs = P.sb("koffs", [128, 4, 8], F32, off=ob2); ob2 += 128
    pS = P.sb("pS", [128, 4, 8], F32, off=ob2); ob2 += 128
    oex = P.sb("oex", [128, 4, 8], F32, off=ob2); ob2 += 128
    rS = P.sb("rS", [128, 4, 8], F32, off=ob2); ob2 += 128
    jS = P.sb("jS", [128, 4, 8], F32, off=ob2); ob2 += 128
    gS = P.sb("gS", [128, 4, 8], F32, off=ob2); ob2 += 128
    tSf = P.sb("tSf", [128, 4, 8], F32, off=ob2); ob2 += 128
    RIDX = P.sb("RIDX", [128, 4, 8], I32, off=ob2); ob2 += 128
    TIDX = P.sb("TIDX", [128, 4, 8], I32, off=ob2); ob2 += 128
    ZIDXf = P.sb("ZIDXf", [128, 4, 16], F32, off=ob2); ob2 += 256
    ZIDX = P.sb("ZIDX", [128, 4, 16], I32, off=ob2); ob2 += 256
    yz = [P.sb(f"yz{i}", [128, 1024], BF16, off=rows_off + 3 * 4096 + i * 2048) for i in range(2)]
    assert ob2 <= bgT_off, ob2
    cmpP = P.sb("cmpP", [128, 4, 8, 128], F32, off=REG0 + 32768)
    Gt = P.sb("Gt", [128, 4, 8, 128], F32, off=REG0 + 49152)

    MSET("vector", ones64[:], 1.0, ["ones64"] + FENCE)
    TT("vector", TAB[:, :, 64:128], AFt[:], lo[:].unsqueeze(2).to_broadcast([128, 4, 64]), ALU.is_ge, AFD + ["lo"], ["TABm"] + FENCE)
    for e16 in range(4):
        P.op("vector", (lambda e16=e16: (lambda e: e.tensor_tensor_scan(out=TAB[:, e16, 0:64], data0=ones64[:], data1=TAB[:, e16, 64:128],
                                                                          initial=0.0, op0=ALU.mult, op1=ALU.add)))(),
             ["TABm", "ones64"], [("TABc", e16)])
    TABC = [("TABc", e16) for e16 in range(4)]
    TT("vector", TAB[:, :, 64:128], TAB[:, :, 64:128], AFt[:], ALU.mult, ["TABm"] + TABC + AFD, ["TABm"])
    DMA("sync", "d_tabd", tabd[0:512, :].rearrange("(e p) c -> p e c", p=128), TAB[:], ["TABm"] + TABC, ["tabd"])
    for k in range(4):
        CP("vector", n4[:, k:k + 1], TAB[:, k, 63:64], TABC, [("n4", k)])
    N4 = [("n4", k) for k in range(4)]
    TT("vector", rhsU[:], n4[:].unsqueeze(2).to_broadcast([128, 4, 128]), U_b[:].unsqueeze(1).to_broadcast([128, 4, 128]), ALU.mult,
       N4 + ["U_b"], ["rhsU"])
    TT("vector", rhsI[:], n4[:].unsqueeze(2).to_broadcast([128, 4, 128]), ident_b[:].unsqueeze(1).to_broadcast([128, 4, 128]), ALU.mult,
       N4 + ["ident_b"], ["rhsI"])
    ps, psn = psf("v")
    MM(ps[:], ones_b[:], rhsU[:].rearrange("p k q -> p (k q)"), True, True, ["rhsU", "ones_b"], [psn])
    CP("vector", offs_sb[:].rearrange("p k q -> p (k q)"), ps[:], [psn], ["offs_sb"])
    ps, psn = psf("v")
    MM(ps[:], ones_b[:], rhsI[:].rearrange("p k q -> p (k q)"), True, True, ["rhsI", "ones_b"], [psn])
    CP("vector", nrow_sb[:].rearrange("p k q -> p (k q)"), ps[:], [psn], ["nrow_sb"])
    P.op("gpsimd", lambda e: e.iota(sval[:], pattern=[[128, 8]], base=0, channel_multiplier=1,
                                    allow_small_or_imprecise_dtypes=True), (), ["sval"])
    P.op("gpsimd", lambda e: e.iota(koffs[:], pattern=[[128, 4], [0, 8]], base=0, channel_multiplier=0,
                                    allow_small_or_imprecise_dtypes=True), (), ["koffs"])
    P.op("gpsimd", lambda e: e.iota(ZIDXf[:], pattern=[[512, 4], [2048, 16]], base=0, channel_multiplier=1,
                                    allow_small_or_imprecise_dtypes=True), (), ["ZIDXf"])
    TS("vector", ZIDXf[:], ZIDXf[:], metat[:, 6:7], None, ALU.add, None, ["ZIDXf", "metat"], ["ZIDXf"])
    CP("vector", ZIDX[:], ZIDXf[:], ["ZIDXf"], ["ZIDX"])
    svb = sval[:].unsqueeze(1).to_broadcast([128, 4, 8])
    TT("vector", cmpP[:], offs_sb[:].unsqueeze(2).to_broadcast([128, 4, 8, 128]),
       svb.unsqueeze(3).to_broadcast([128, 4, 8, 128]), ALU.is_le, ["offs_sb", "sval"], ["cmpP"] + FENCE)
    RED("vector", pS[:], cmpP[:], ALU.add, ["cmpP"], ["pS"])
    TT("vector", cmpP[:], cmpP[:], nrow_sb[:].unsqueeze(2).to_broadcast([128, 4, 8, 128]), ALU.mult, ["cmpP", "nrow_sb"], ["cmpP"])
    RED("vector", oex[:], cmpP[:], ALU.add, ["cmpP"], ["oex"])
    TT("vector", rS[:], svb, oex[:], ALU.subtract, ["sval", "oex"], ["rS"])
    TT("vector", tSf[:], pS[:], koffs[:], ALU.add, ["pS", "koffs"], ["tSf"])
    CP("vector", RIDX[:], tSf[:], ["tSf"], ["RIDX"])
    for k in range(4):
        for c in range(8):
            P.dma("gpsimd", "d_G", (lambda k=k, c=c: (lambda e: e.indirect_dma_start(
                out=Gt[:, k, c, :], out_offset=None, in_=tabd[0:512, :], in_offset=bass.IndirectOffsetOnAxis(ap=RIDX[:, k, c:c + 1], axis=0))))(),
                ["tabd", "RIDX"], [("Gt", k, c), "cmpb"] if (k == 0 and c == 0) else [("Gt", k, c)])
    GALL = [("Gt", k, c) for k in range(4) for c in range(8)]
    cmpG = cmpP[:, :, :, 0:64]
    TT("vector", cmpG, Gt[:, :, :, 0:64], rS[:].unsqueeze(3).to_broadcast([128, 4, 8, 64]), ALU.is_le, GALL + ["rS"], ["cmpP"])
    RED("vector", jS[:], cmpG, ALU.add, ["cmpP"], ["jS"])
    TS("vector", oex[:], rS[:], 1.0, None, ALU.add, None, ["rS"], ["oex"])
    TT("vector", cmpG, Gt[:, :, :, 0:64], oex[:].unsqueeze(3).to_broadcast([128, 4, 8, 64]), ALU.is_equal, GALL + ["oex"], ["cmpP"])
    TT("vector", cmpG, cmpG, Gt[:, :, :, 64:128], ALU.mult, ["cmpP"] + GALL, ["cmpP"])
    RED("vector", gS[:], cmpG, ALU.add, ["cmpP"], ["gS"])
    TS("vector", tSf[:], pS[:], 64.0, None, ALU.mult, None, ["pS"], ["tSf"])
    TT("vector", tSf[:], tSf[:], jS[:], ALU.add, ["tSf", "jS"], ["tSf"])
    CP("vector", TIDX[:], tSf[:], ["tSf"], ["TIDX"])
    ra = P.sb("ra", [128, 4, 8], F32, off=ob2); rb = P.sb("rb", [128, 4, 8], F32, off=ob2 + 128)
    rj = P.sb("rj", [128, 4, 8], F32, off=ob2 + 256); GIDX = P.sb("GIDX", [128, 4, 8], I32, off=ob2 + 384)
    assert ob2 + 512 <= bgT_off
    TS("vector", ra[:], tSf[:], 2048.0, None, ALU.is_ge, None, ["tSf"], ["ra"])
    for thv in (4096.0, 6144.0):
        TS("vector", rb[:], tSf[:], thv, None, ALU.is_ge, None, ["tSf"], ["rb"])
        TT("vector", ra[:], ra[:], rb[:], ALU.add, ["ra", "rb"], ["ra"])
    TS("vector", rb[:], ra[:], -2048.0, None, ALU.mult, None, ["ra"], ["rb"])
    TT("vector", rb[:], rb[:], tSf[:], ALU.add, ["rb", "tSf"], ["rb"])
    TS("vector", rj[:], rb[:], 512.0, None, ALU.is_ge, None, ["rb"], ["rj"])
    for thv in (1024.0, 1536.0):
        TS("vector", oex[:], rb[:], thv, None, ALU.is_ge, None, ["rb"], ["oex"])
        TT("vector", rj[:], rj[:], oex[:], ALU.add, ["rj", "oex"], ["rj"])
    TT("vector", rj[:], rj[:], ra[:], ALU.subtract, ["rj", "ra"], ["rj"])
    TS("vector", rj[:], rj[:], 1536.0, None, ALU.mult, None, ["rj"], ["rj"])
    TT("vector", rj[:], rj[:], tSf[:], ALU.add, ["rj", "tSf"], ["rj"])
    CP("vector", GIDX[:], rj[:], ["rj"], ["GIDX"])
    ZSIDX = P.sb("ZSIDX", [128, 4, 8], I32, off=ob2 + 512)
    assert ob2 + 640 <= bgT_off
    TS("vector", rj[:], rb[:], 128.0, None, ALU.is_ge, None, ["rb"], ["rj"])
    for kk in range(2, 16):
        TS("vector", oex[:], rb[:], 128.0 * kk, None, ALU.is_ge, None, ["rb"], ["oex"])
        TT("vector", rj[:], rj[:], oex[:], ALU.add, ["rj", "oex"], ["rj"])
    TS("vector", rj[:], rj[:], 384.0, None, ALU.mult, None, ["rj"], ["rj"])
    TS("vector", oex[:], ra[:], -1920.0, None, ALU.mult, None, ["ra"], ["oex"])
    TT("vector", rj[:], rj[:], oex[:], ALU.add, ["rj", "oex"], ["rj"])
    TT("vector", rj[:], rj[:], tSf[:], ALU.add, ["rj", "tSf"], ["rj"])
    CP("vector", ZSIDX[:], rj[:], ["rj"], ["ZSIDX"])

    if "idx" in dbg:
        d1 = dbg_out("tidx", [128, 32])
        d2 = dbg_out("gS", [128, 32])
        e1 = DMA("sync", "d_dbg1", d1, tSf[:].rearrange("p a b -> p (a b)"), ["tSf", "TIDX"], ["dbg1"])
        e2 = DMA("sync", "d_dbg2", d2, gS[:].rearrange("p a b -> p (a b)"), ["gS"], ["dbg2"])
        P.finish("sync", [e1, e2])
    if stage <= 6.5:
        P.emit()
        return nc, dbg_outs

    hid = [P.sb(f"hid{i}", [128, 8, 512], BF16, off=qT_off + i * 8192) for i in range(2)]
    XS = P.sb("XS", [128, 8, 1024], BF16, off=bgT_off)
    xsT = P.sb("xsT", [128, 8, 1024], BF16, off=bgT_off + 16384)
    sgs = P.sb("sgs", [128, 512], F32, off=rt1_off + 4096)
    MSET("vector", XS[:], 0.0, ["XS"] + FENCE)
    ZD0 = []
    for t in range(8):
        ZD0.append(("Zd0", t))
        DMA("sync", "d_z0", Zd[t * 1024:(t + 1) * 1024, :].rearrange("(p c) d -> p c d", c=8), XS[:], ["XS"], [("Zd0", t)])
    yrr = [0]
    def issue_loads(k4):
        sg_, su_ = (k4 % 2) * 2, (k4 % 2) * 2 + 1
        wg_t, wu_t = wslot[sg_], wslot[su_]
        ex2 = (["cmpP"] if sg_ == 2 else [])
        ex3 = (["cmpb"] + GALL if su_ == 3 else [])
        if k4 > 0:
          P.dma("gpsimd", f"d_ws{sg_}", (lambda wg_t=wg_t, k4=k4: (lambda e: e.dma_start(out=wg_t[:], in_=wgv[k4])))(), [], [f"ws{sg_}"] + ex2 + (FENCE if k4 < 2 else []))
          P.dma("gpsimd", f"d_ws{su_}", (lambda wu_t=wu_t, k4=k4: (lambda e: e.dma_start(out=wu_t[:], in_=wuv[k4])))(), [], [f"ws{su_}"] + ex3 + (FENCE if k4 < 2 else []))
        for c in range(8):
            P.dma("gpsimd", f"d_XS{c}", (lambda k4=k4, c=c: (lambda e: e.indirect_dma_start(
                out=XS[:, c, :], out_offset=None, in_=h2all, in_offset=bass.IndirectOffsetOnAxis(ap=GIDX[:, k4, c:c + 1], axis=0))))(),
                H2ALLD + ["GIDX"], [("XS", c)] + (["XS"] if c == 0 else []))

    def issue_wd(k4):
        P.dma("gpsimd", "d_wd", (lambda k4=k4: (lambda e: e.dma_start(out=wdt_[:], in_=wdv[k4])))(), [], ["wd_"] + (FENCE if k4 < 1 else []))

    issue_loads(0)
    for k4 in range(4):
        sg_, su_ = (k4 % 2) * 2, (k4 % 2) * 2 + 1
        wg_t, wu_t = wslot[sg_], wslot[su_]
        for c in range(8):
            pb, pbn = psb()
            pbv = pb[:].rearrange("p (k t) -> p k t", k=8)
            for kc in range(8):
                TR(pbv[:, kc, :], XS[:, c, kc * 128:(kc + 1) * 128], ident_b[:], [("XS", c), "XS", "ident_b"], [(pbn, kc)])
            ACT(xsT[:, :, c * 128:(c + 1) * 128], pbv, AF.Copy, [(pbn, kc) for kc in range(8)], [("xsT", c)] + (FENCE if k4 == 0 else []))
        if k4 < 3:
            issue_loads(k4 + 1)
        for sch in range(2):
            hd = hid[(k4 * 2 + sch) % 2]
            hdn = f"hid{(k4 * 2 + sch) % 2}"
            xdeps = [("xsT", sch * 4 + t) for t in range(4)]
            for fo in range(8):
                pa, pan = psf("a")
                for kc in range(8):
                    MM(pa[:], wg_t[:, kc, fo * 128:(fo + 1) * 128], xsT[:, kc, sch * 512:(sch + 1) * 512], kc == 0, kc == 7,
                       [f"ws{sg_}"] + xdeps, [pan])
                pu, pun = psf("v")
                for kc in range(8):
                    MM(pu[:], wu_t[:, kc, fo * 128:(fo + 1) * 128], xsT[:, kc, sch * 512:(sch + 1) * 512], kc == 0, kc == 7,
                       [f"ws{su_}"] + xdeps, [pun])
                ACT(sgs[:], pa[:], AF.Silu, [pan], ["sgs"] + (FENCE if k4 == 0 and sch == 0 and fo == 0 else []))
                TT("vector", hd[:, fo, :], pu[:], sgs[:], ALU.mult, [pun, "sgs"], [(hdn, fo)] + (FENCE + ["TABm"] + TABC if k4 == 0 else []))
            for t in range(4):
                c = sch * 4 + t
                yi = yrr[0] % 2
                yrr[0] += 1
                for dn in range(2):
                    py, pyn = psf("v")
                    for kc in range(8):
                        MM(py[:], hd[:, kc, t * 128:(t + 1) * 128], wdt_[:, kc, dn * 512:(dn + 1) * 512], kc == 0, kc == 7,
                           [(hdn, kc), "wd_"], [pyn])
                    TS("vector", yz[yi][:, dn * 512:(dn + 1) * 512], py[:], gS[:, k4, c:c + 1], None, ALU.mult, None,
                       [pyn, "gS"], [f"yz{yi}"])
                P.dma("gpsimd", f"d_sz{yi}", (lambda yi=yi, k4=k4, c=c: (lambda e: e.indirect_dma_start(
                    out=Zd, out_offset=bass.IndirectOffsetOnAxis(ap=ZSIDX[:, k4, c:c + 1], axis=0), in_=yz[yi][:], in_offset=None,
                    compute_op=ALU.add, oob_is_err=True)))(), [f"yz{yi}", "ZSIDX", "Zd"] + ZD0, ["Zd"])
        if k4 < 3:
            issue_wd(k4 + 1)

    if stage <= 7:
        P.emit()
        return nc, dbg_outs

    gfrow = P.sb("gfrow", [128, 1024], F32, off=rows_off + 4096)
    DMA("sync", "d_gfrow", gfrow[:], g_final.partition_broadcast(128), [], ["gfrow"] + FENCE)
    z4 = [P.sb(f"z4_{i}", [128, 4, 1024], BF16, off=bgT_off + i * 8192) for i in range(2)]
    evs = []

    def z_allgather(j16):
        P.dma("gpsimd", "d_ag_z", (lambda j16=j16: (lambda e: e.collective_compute(
            "AllGather", ALU.bypass, replica_groups=GROUPS,
            ins=[Zd[j16 * 512:(j16 + 1) * 512, :].opt()], outs=[Zall[j16 * 2048:(j16 + 1) * 2048, :].opt()])))(),
            ["Zd", "agchain"], [("Zall", j16), "agchain"], inc=1)

    def phaseC_tile(tile):
        i = xt_rr[0] % 2
        xt_rr[0] += 1
        xtile, xn = xt[i], f"xt{i}"
        zi = tile % 2
        DMA("sync", "d_" + xn, xtile[:], x1d[tile * 128:(tile + 1) * 128, :], [("x1d", tile)], [xn])
        for r in range(4):
            P.dma("gpsimd", f"d_z4_{zi}_{r}", (lambda zi=zi, r=r, tile=tile: (lambda e: e.indirect_dma_start(
                out=z4[zi][:, r, :], out_offset=None, in_=Zall, in_offset=bass.IndirectOffsetOnAxis(ap=ZIDX[:, r, tile:tile + 1], axis=0))))(),
                [("Zall", tile), "ZIDX"], [(f"z4_{zi}", r)] + ([("XS", c) for c in range(8)] + ["XS"] if tile < 2 else []))
        zd = [(f"z4_{zi}", r) for r in range(4)]
        TT("vector", tf[:], z4[zi][:, 0, :], z4[zi][:, 1, :], ALU.add, zd, ["tf"])
        TT("vector", tf[:], tf[:], z4[zi][:, 2, :], ALU.add, zd + ["tf"], ["tf"])
        TT("vector", tf[:], tf[:], z4[zi][:, 3, :], ALU.add, zd + ["tf"], ["tf"])
        TT("vector", tf[:], tf[:], rows["GT2"][:], ALU.mult, ["tf"] + rowdeps("GT2"), ["tf"])
        TT("vector", xtile[:], xtile[:], tf[:], ALU.add, [xn, "tf"], [xn])
        ss = small[:, 16:17]
        rstd = small[:, 17:18]
        ACT(tf[:], xtile[:], AF.Square, [xn], ["tf"])
        RED("vector", ss, tf[:], ALU.add, ["tf"], ["ss"])
        TS("vector", rstd, ss, 1.0 / 1024.0, 1e-6, ALU.mult, ALU.add, ["ss"], ["rstd"])
        ACT(rstd, rstd, AF.Ln, ["rstd"], ["rstd"])
        ACT(rstd, rstd, AF.Exp, ["rstd"], ["rstd"], scale=-0.5)
        ACT(tf[:], xtile[:], AF.Copy, [xn, "rstd"], ["tf"], scale=rstd)
        TT("vector", xtile[:], tf[:], gfrow[:], ALU.mult, ["tf", "gfrow"], [xn])
        evs.append(DMA("sync", "d_out_" + xn, out[tile * 128:(tile + 1) * 128, :], xtile[:], [xn], [("out", tile)]))

    for j16 in range(16):
        z_allgather(j16)
        if j16 >= 1:
            phaseC_tile(j16 - 1)
    phaseC_tile(15)
    P.finish("sync", evs)
    P.emit()
    return nc, dbg_outs


def make_in_maps(inp):
    x = np.ascontiguousarray(inp["x"], dtype=np.float32)
    maps = []
    for c in range(NCORES):
        b, q = c // 4, c % 4
        t0 = q * 2048
        xw = np.zeros((W, 1024), np.float32)
        lo, hi = t0 - 128, t0 + 2048 + 128
        slo, shi = max(lo, 0), min(hi, 8192)
        xw[slo - lo:shi - lo] = x[b, slo:shi]
        ccv = np.stack([inp["c"][b].reshape(8, 128).T, inp["c_ctx"].reshape(8, 128).T], axis=-1)
        meta = np.zeros((128, 80), np.float32)
        meta[:, 0] = 1.0 if q > 0 else 0.0
        meta[:, 1] = 1.0 if q < 3 else 0.0
        meta[:, 2] = float(q * 32 - 2)
        meta[:, 3] = float(q * 2048)
        meta[:, 4] = float(4 * q * 128)
        meta[:, 5] = float(q * 8192)
        meta[:, 6] = float(q * 128)
        meta[:, 7] = float(4 * q * 32)
        for k in range(4):
            meta[:, 8 + (4 * q + k) * 4 + k] = 1.0
        maps.append({
            "x": xw, "ctx": np.ascontiguousarray(inp["ctx"][b]), "cc": np.ascontiguousarray(ccv.reshape(128, 16)),
            "meta": meta, "w_ada": inp["w_ada"][0], "b_ada": inp["b_ada"][0], "g_mix": inp["g_mix"][0],
            "g_ffn": inp["g_ffn"][0], "g_final": inp["g_final"], "w_in": inp["w_in"][0], "conv_w": inp["conv_w"][0],
            "sink": inp["sink"][0], "w_out": inp["w_out"][0], "w_router": inp["w_router"][0],
            "w_gate": np.ascontiguousarray(inp["w_gate"][0, 4 * q:4 * q + 4]),
            "w_up": np.ascontiguousarray(inp["w_up"][0, 4 * q:4 * q + 4]),
            "w_down": np.ascontiguousarray(inp["w_down"][0, 4 * q:4 * q + 4]),
        })
    return maps


def kernel(**inputs):
    inp = {k: np.asarray(v) for k, v in inputs.items()}
    nc, _ = build_nc()
    res = run_bass_kernel_spmd(nc, make_in_maps(inp), core_ids=list(range(NCORES)))
    outp = np.zeros((2, 8192, 1024), np.float32)
    for c in range(NCORES):
        b, q = c // 4, c % 4
        outp[b, q * 2048:(q + 1) * 2048] = res.results[c]["out"]
    return outp
```

```python
import os
import numpy as np
import concourse.bass as bass
import concourse.mybir as mybir
from concourse.bass_utils import run_bass_kernel_spmd

F32 = mybir.dt.float32
BF16 = mybir.dt.bfloat16
I32 = mybir.dt.int32
ALU = mybir.AluOpType
AF = mybir.ActivationFunctionType
AX = mybir.AxisListType

COMPUTE = ("tensor", "vector", "scalar", "gpsimd")
QUEUES = ("sync",)
NCORES = 8
GROUPS = [[0, 1, 2, 3], [4, 5, 6, 7]]
W = 2304
NT = 16
NIT = 7


class Prog:
    def __init__(self, nc):
        self.nc = nc
        self.streams = {e: [] for e in COMPUTE + QUEUES}
        self.cnt = {e: 0 for e in COMPUTE}
        self.dma_cnt = {}
        self.waited = {}
        self.res = {}
        self.sem_handles = {}
        self.final_events = []
        self.sb_off = 16512
        self.sb_top = 229344

    def sb(self, name, shape, dtype, off=None):
        esz = {F32: 4, BF16: 2, I32: 4}[dtype]
        n = 1
        for s in shape[1:]:
            n *= s
        nbytes = (n * esz + 63) // 64 * 64
        if off is None:
            off = self.sb_off
            self.sb_off += nbytes
        assert off >= 16512 and off + nbytes <= self.sb_top, (name, off, nbytes)
        return self.nc.alloc_sbuf_tensor_at(name, list(shape), dtype, offset=off)

    def _deps(self, reads, writes):
        need = []
        for r in reads:
            st = self.res.get(r)
            if st and st["w"] is not None:
                need.append(st["w"])
        for w in writes:
            st = self.res.get(w)
            if st:
                if st["w"] is not None:
                    need.append(st["w"])
                need.extend(st["r"])
        return need

    def _commit(self, ev, reads, writes):
        for r in reads:
            st = self.res.setdefault(r, {"w": None, "r": []})
            st["r"].append(ev)
        for w in writes:
            self.res[w] = {"w": ev, "r": []}

    def _waits(self, eng, need):
        best = {}
        for (k, v) in need:
            if k == "tensor" and eng == "tensor":
                continue
            if v > best.get(k, 0):
                best[k] = v
        out = []
        for k, v in best.items():
            if self.waited.get((eng, k), 0) >= v:
                continue
            self.waited[(eng, k)] = v
            out.append((k, v))
        return out

    def op(self, eng, fn, reads=(), writes=()):
        need = self._deps(reads, writes)
        waits = self._waits(eng, need)
        self.cnt[eng] += 1
        ev = (eng, self.cnt[eng])
        self.streams[eng].append((waits, fn, (eng, 1)))
        self._commit(ev, reads, writes)
        return ev

    def dma(self, q, sem, fn, reads=(), writes=(), inc=16):
        need = self._deps(reads, writes)
        waits = self._waits(q, need)
        self.dma_cnt[sem] = self.dma_cnt.get(sem, 0) + inc
        ev = (sem, self.dma_cnt[sem])
        self.streams[q].append((waits, fn, (sem, inc)))
        self._commit(ev, reads, writes)
        return ev

    def finish(self, eng, events):
        self.final_events.append((eng, events))

    def check_deadlock(self):
        sem = {}
        pos = {e: 0 for e in self.streams}
        progressed = True
        while progressed:
            progressed = False
            for e, st in self.streams.items():
                while pos[e] < len(st):
                    waits, fn, inc = st[pos[e]]
                    if all(sem.get(k, 0) >= v for (k, v) in waits):
                        sem[inc[0]] = sem.get(inc[0], 0) + inc[1]
                        pos[e] += 1
                        progressed = True
                    else:
                        break
        stuck = {e: (pos[e], len(st), st[pos[e]][0]) for e, st in self.streams.items() if pos[e] < len(st)}
        assert not stuck, ("DEADLOCK", stuck, {k: sem.get(k) for e in stuck for (k, v) in stuck[e][2]})

    def emit(self):
        self.check_deadlock()
        nc = self.nc
        names = set(COMPUTE)
        for e in self.streams:
            for (waits, fn, inc) in self.streams[e]:
                names.add(inc[0])
                for (k, v) in waits:
                    names.add(k)
        for n in sorted(names):
            self.sem_handles[n] = nc.alloc_semaphore("s_" + n)
        H = self.sem_handles
        fin = {}
        for eng, evs in self.final_events:
            fin.setdefault(eng, []).extend(evs)
        with nc.Block() as block:
            def make(ename):
                def body(e):
                    for (waits, fn, inc) in self.streams[ename]:
                        for (k, v) in waits:
                            e.wait_ge(H[k], v)
                        fn(e).then_inc(H[inc[0]], inc[1])
                    best = {}
                    for (k, v) in fin.get(ename, []):
                        best[k] = max(best.get(k, 0), v)
                    for k, v in best.items():
                        e.wait_ge(H[k], v)
                return body
            for ename in self.streams:
                if not self.streams[ename] and ename not in fin:
                    continue
                getattr(block, ename)(make(ename))


def build_nc(stage=99, dbg=()):
    nc = bass.Bass("TRN2", target_bir_lowering=False)
    P = Prog(nc)
    dbg_outs = {}

    def din(name, shape, dt=F32):
        return nc.dram_tensor(name, list(shape), dt, kind="ExternalInput").ap()

    x = din("x", [W, 1024])
    ctx = din("ctx", [256, 1024])
    cc = din("cc", [128, 16])
    meta = din("meta", [128, 80])
    w_ada = din("w_ada", [1024, 6144])
    b_ada = din("b_ada", [6144])
    g_mix = din("g_mix", [1024])
    g_ffn = din("g_ffn", [1024])
    g_final = din("g_final", [1024])
    w_in = din("w_in", [1024, 2304])
    conv_w = din("conv_w", [3, 512])
    sink = din("sink", [8])
    w_out = din("w_out", [1024, 1024])
    w_router = din("w_router", [1024, 16])
    w_gate = din("w_gate", [4, 1024, 1024])
    w_up = din("w_up", [4, 1024, 1024])
    w_down = din("w_down", [4, 1024, 1024])
    out = nc.dram_tensor("out", [2048, 1024], F32, kind="ExternalOutput").ap()

    x1d = nc.dram_tensor("x1d", [2048, 1024], F32).ap()
    h2loc = nc.dram_tensor("h2loc", [2048, 1024], BF16).ap()
    h2all = nc.dram_tensor("h2all", [8192, 1024], BF16).ap()
    affloc = nc.dram_tensor("affloc", [16, 2048], F32).ap()
    affall = nc.dram_tensor("affall", [64, 2048], F32).ap()
    tabd = nc.dram_tensor("tabd", [2048, 128], F32).ap()
    Zd = nc.dram_tensor("Zd", [8192, 1024], BF16).ap()
    Zall = [nc.dram_tensor(f"Zall{j}", [2048, 1024], BF16).ap() for j in range(16)]

    def dbg_out(name, shape, dt=F32):
        t = nc.dram_tensor("dbg_" + name, list(shape), dt, kind="ExternalOutput").ap()
        dbg_outs[name] = t
        return t

    def ACT(out_, in_, func, r, w, **kw):
        return P.op("scalar", lambda e: e.activation(out=out_, in_=in_, func=func, **kw), r, w)

    def TT(eng, out_, in0, in1, op, r, w):
        return P.op(eng, lambda e: e.tensor_tensor(out=out_, in0=in0, in1=in1, op=op), r, w)

    def TS(eng, out_, in0, s1, s2, op0, op1, r, w):
        if op1 is None:
            return P.op(eng, lambda e: e.tensor_scalar(out=out_, in0=in0, scalar1=s1, scalar2=None, op0=op0), r, w)
        return P.op(eng, lambda e: e.tensor_scalar(out=out_, in0=in0, scalar1=s1, scalar2=s2, op0=op0, op1=op1), r, w)

    def STT(eng, out_, in0, scalar, in1, op0, op1, r, w):
        return P.op(eng, lambda e: e.scalar_tensor_tensor(out=out_, in0=in0, scalar=scalar, in1=in1, op0=op0, op1=op1), r, w)

    def RED(eng, out_, in_, op, r, w):
        return P.op(eng, lambda e: e.tensor_reduce(out=out_, in_=in_, axis=AX.X, op=op), r, w)

    def CP(eng, out_, in_, r, w):
        return P.op(eng, lambda e: e.tensor_copy(out=out_, in_=in_), r, w)

    def MSET(eng, out_, val, w):
        return P.op(eng, lambda e: e.memset(out_, val), (), w)

    def MM(out_, lhsT, rhs, start, stop, r, w):
        return P.op("tensor", lambda e: e.matmul(out_, lhsT, rhs, start=start, stop=stop), r, w)

    def TR(out_, in_, ident, r, w):
        return P.op("tensor", lambda e: e.transpose(out_, in_, ident), r, w)

    def DMA(q, sem, out_, in_, r, w):
        return P.dma(q, sem, lambda e: e.dma_start(out=out_, in_=in_), r, w)

    PSF = [nc.alloc_psum_tensor(f"psf{i}", [128, 512], F32) for i in range(6)]
    PSB = [nc.alloc_psum_tensor(f"psb{i}", [128, 1024], BF16) for i in range(2)]
    psf_rr = {"v": 0, "a": 0}

    def psf(cons):
        i = psf_rr[cons] % 3 + (0 if cons == "v" else 3)
        psf_rr[cons] += 1
        return PSF[i], f"psf{i}"

    psb_rr = [0]

    def psb():
        i = psb_rr[0] % 2
        psb_rr[0] += 1
        return PSB[i], f"psb{i}"

    ident_f = P.sb("ident_f", [128, 128], F32)
    ident_b = P.sb("ident_b", [128, 128], BF16)
    iot = P.sb("iot", [128, 128], F32)
    ones_b = P.sb("ones_b", [128, 128], BF16)
    U_b = P.sb("U_b", [128, 128], BF16)
    UI_b = P.sb("UI_b", [128, 128], BF16)
    mask3 = P.sb("mask3", [128, 3, 384], BF16)
    metat = P.sb("metat", [128, 80], F32)
    esink = P.sb("esink", [128, 8], F32)
    rows = {}
    for nm in ("S1", "G1", "GT1", "S2", "G2", "GT2", "cS1", "cG1"):
        rows[nm] = P.sb("row_" + nm, [128, 1024], F32)
    REG0 = P.sb_off

    P.op("gpsimd", lambda e: e.iota(iot[:], pattern=[[1, 128]], base=0, channel_multiplier=-1,
                                    allow_small_or_imprecise_dtypes=True), (), ["iot"])
    TS("vector", ident_f[:], iot[:], 0.0, None, ALU.is_equal, None, ["iot"], ["ident_f"])
    CP("vector", ident_b[:], ident_f[:], ["ident_f"], ["ident_b"])
    TS("vector", U_b[:], iot[:], 0.0, None, ALU.is_ge, None, ["iot"], ["U_b"])
    MSET("vector", ones_b[:], 1.0, ["ones_b"])
    DMA("sync", "d_meta", metat[:], meta, [], ["metat"])
    DMA("sync", "d_sink", esink[:], sink.partition_broadcast(128), [], ["esink"])
    ACT(esink[:], esink[:], AF.Exp, ["esink"], ["esink"])
    for v in range(3):
        TS("vector", mask3[:, v, 0:128], iot[:], 0.0, None, ALU.is_le, None, ["iot"], [("mask3", v)])
        MSET("vector", mask3[:, v, 128:256], 1.0, [("mask3", v, 1)])
        TS("vector", mask3[:, v, 256:384], iot[:], 0.0, None, ALU.is_ge, None, ["iot"], [("mask3", v, 2)])
    TS("vector", mask3[:, 1, 0:128], mask3[:, 1, 0:128], metat[:, 0:1], None, ALU.mult, None,
       ["metat", ("mask3", 1)], [("mask3", 1)])
    TS("vector", mask3[:, 2, 256:384], mask3[:, 2, 256:384], metat[:, 1:2], None, ALU.mult, None,
       ["metat", ("mask3", 2, 2)], [("mask3", 2, 2)])

    if "const" in dbg:
        d1 = dbg_out("ident", [128, 128])
        d2 = dbg_out("mask3", [128, 3 * 384], BF16)
        d3 = dbg_out("esink", [128, 8])
        e1 = DMA("sync", "d_dbg", d1, ident_f[:], ["ident_f"], ["dbg1"])
        e2 = DMA("sync", "d_dbg", d2, mask3[:].rearrange("p a b -> p (a b)"),
                 [("mask3", v) for v in range(3)] + [("mask3", v, 1) for v in range(3)] + [("mask3", v, 2) for v in range(3)], ["dbg2"])
        e3 = DMA("sync", "d_dbg", d3, esink[:], ["esink"], ["dbg3"])
        P.finish("sync", [e1, e2, e3])
    if stage <= 0:
        P.emit()
        return nc, dbg_outs

    o = REG0
    WIN = P.sb("WIN", [128, 8, 2944], BF16, off=o)
    mixT = P.sb("mixT", [128, 8, 2048], BF16, off=o)
    o += 47104
    COS = P.sb("COS", [128, W], F32, off=o); o += W * 4
    SINS = P.sb("SINS", [128, W], F32, off=o); o += W * 4
    xt = [P.sb(f"xt{i}", [128, 1024], F32, off=o + i * 4096) for i in range(2)]; o += 8192
    tf = P.sb("tf", [128, 1024], F32, off=o); o += 4096
    hb = [P.sb(f"hb{i}", [128, 1024], BF16, off=o + i * 2048) for i in range(2)]; o += 4096
    hT = [P.sb(f"hT{i}", [128, 8, 512], BF16, off=o + i * 8192) for i in range(2)]
    wo = P.sb("wo", [128, 8, 1024], BF16, off=o)
    o += 16384
    qT_off = o
    qT = P.sb("qT", [128, 4, W], BF16, off=o); o += 4 * W * 2
    kT_off = o
    kT = P.sb("kT", [128, W], BF16, off=o); o += W * 2
    Vt = P.sb("Vt", [128, 18, 2, 65], BF16, off=o); o += 4736
    kcT = P.sb("kcT", [128, 256], BF16, off=o); o += 512
    Vc = P.sb("Vc", [128, 2, 2, 65], BF16, off=o); o += 576
    bgT_off = o
    bgT = P.sb("bgT", [128, 4, 2048], BF16, off=o)
    stg = P.sb("stg", [128, 8, 640], F32, off=o)
    o += 20480
    uT_off = o
    uT = P.sb("uT", [128, 4, W], BF16, off=o); o += 4 * W * 2
    rt1_off = o
    rt1 = P.sb("rt1", [128, 512], F32, off=o); o += 2048
    rt2 = P.sb("rt2", [128, 512], F32, off=o); o += 2048
    cgs = P.sb("cgs", [128, 512], F32, off=o); o += 2048
    small = P.sb("small", [128, 64], F32, off=o); o += 256
    cw = P.sb("cw", [128, 4, 3], F32, off=o); o += 64
    assert o <= P.sb_top, o
    A_END = o

    wa = [P.sb("wa0", [128, 8, 1024], BF16, off=qT_off), P.sb("wa1", [128, 8, 1024], BF16, off=uT_off)]
    o = kT_off
    lb = P.sb("lb", [128, 8, 2, 128], BF16, off=o); o += 4096
    brow = P.sb("brow", [128, 1024], F32, off=o); o += 4096
    cct = P.sb("cct", [128, 8, 2], F32, off=o); o += 64
    scl = P.sb("scl", [128, 8, 2], F32, off=o); o += 64
    assert o <= bgT_off
    gmrow = P.sb("gmrow", [128, 1024], F32, off=rt1_off)

    DMA("sync", "d_cc", cct[:], cc.rearrange("p (k v) -> p k v", v=2), [], ["cct"])
    ACT(scl[:], cct[:], AF.Silu, ["cct"], ["scl"])
    for v in range(2):
        CP("vector", lb[:, :, v, :], scl[:, :, v:v + 1].to_broadcast([128, 8, 128]), ["scl"], [("lb", v)])
    if stage <= 0.3:
        d1 = dbg_out("lb", [128, 8 * 2 * 128], BF16)
        e1 = DMA("sync", "d_dbg", d1, lb[:].rearrange("p a b c -> p (a b c)"), [("lb", 0), ("lb", 1)], ["dbg1"])
        P.finish("sync", [e1])
        P.emit()
        return nc, dbg_outs
    w_ada_v = w_ada.rearrange("(k p) n -> p k n", p=128)
    grp = [(0, [("S1", 0), ("cS1", 1)]), (1, [("G1", 0), ("cG1", 1)]), (2, [("GT1", 0)]),
           (3, [("S2", 0)]), (4, [("G2", 0)]), (5, [("GT2", 0)])]
    for gi, (g, uses) in enumerate(grp):
        wb = wa[gi % 2]
        wn = f"wa{gi % 2}"
        P.dma("gpsimd", "d_" + wn, (lambda wb=wb, g=g: (lambda e: e.dma_start(out=wb[:], in_=w_ada_v[:, :, g * 1024:(g + 1) * 1024])))(),
              [], [wn])
        DMA("sync", "d_brow", brow[:], b_ada[g * 1024:(g + 1) * 1024].partition_broadcast(128), [], ["brow"])
        if stage <= 0.5:
            d1 = dbg_out("wa", [128, 8 * 1024], BF16)
            d2 = dbg_out("brow", [128, 1024])
            e1 = DMA("sync", "d_dbg", d1, wb[:].rearrange("p a b -> p (a b)"), [wn], ["dbg1"])
            e2 = DMA("sync", "d_dbg", d2, brow[:], ["brow"], ["dbg2"])
            P.finish("sync", [e1, e2])
            P.emit()
            return nc, dbg_outs
        for (nm, v) in uses:
            for n in range(2):
                ps, psn = psf("v")
                for k in range(8):
                    MM(ps[:], lb[:, k, v, :], wb[:, k, n * 512:(n + 1) * 512], k == 0, k == 7,
                       [("lb", v), wn], [psn])
                TT("vector", rows[nm][:, n * 512:(n + 1) * 512], ps[:], brow[:, n * 512:(n + 1) * 512], ALU.add,
                   [psn, "brow"], [("row", nm, n)])
                if stage <= 0.7:
                    d1 = dbg_out("r0", [128, 512])
                    e1 = DMA("sync", "d_dbg", d1, rows[nm][:, 0:512], [("row", nm, n)], ["dbg1"])
                    P.finish("sync", [e1])
                    P.emit()
                    return nc, dbg_outs
    for (gsrc, names) in (((g_mix, ("G1", "cG1")), (g_ffn, ("G2",))) if stage > 0.8 else ()):
        DMA("sync", "d_gmrow", gmrow[:], gsrc.partition_broadcast(128), [], ["gmrow"])
        for nm in names:
            TS("vector", rows[nm][:], rows[nm][:], 1.0, None, ALU.add, None,
               [("row", nm, 0), ("row", nm, 1)], [("row", nm, 0), ("row", nm, 1)])
            TT("vector", rows[nm][:], rows[nm][:], gmrow[:], ALU.mult,
               [("row", nm, 0), ("row", nm, 1), "gmrow"], [("row", nm, 0), ("row", nm, 1)])

    def rowdeps(nm):
        return [("row", nm, 0), ("row", nm, 1)]

    if "rows" in dbg:
        d = dbg_out("rows", [8, 128, 1024])
        for i, nm in enumerate(("S1", "G1", "GT1", "S2", "G2", "GT2", "cS1", "cG1")):
            ev = DMA("sync", "d_dbg", d[i], rows[nm][:], rowdeps(nm), ["dbg"])
        P.finish("sync", [ev])
    if stage <= 1:
        P.emit()
        return nc, dbg_outs


    def sc(i):
        return small[:, i:i + 1]
    pid, dd, i32_, isC, ff, inv, invC, invR, sgn, tmpc = [sc(i) for i in range(10)]
    P.op("gpsimd", lambda e: e.iota(small[:, 0:1], pattern=[[0, 1]], base=0, channel_multiplier=1,
                                    allow_small_or_imprecise_dtypes=True), (), ["small"])
    TS("vector", tmpc, pid, 64.0, -64.0, ALU.is_ge, ALU.mult, ["small"], ["small"])
    TT("vector", dd, pid, tmpc, ALU.add, ["small"], ["small"])
    TS("vector", sgn, dd, 32.0, None, ALU.is_ge, None, ["small"], ["small"])
    TS("vector", tmpc, sgn, -32.0, None, ALU.mult, None, ["small"], ["small"])
    TT("vector", i32_, dd, tmpc, ALU.add, ["small"], ["small"])
    TS("vector", isC, i32_, 16.0, None, ALU.is_ge, None, ["small"], ["small"])
    TS("vector", tmpc, isC, -16.0, None, ALU.mult, None, ["small"], ["small"])
    TT("vector", ff, i32_, tmpc, ALU.add, ["small"], ["small"])
    ACT(inv, ff, AF.Exp, ["small"], ["small"], scale=-float(np.log(10000.0) / 16.0))
    TT("vector", invC, inv, isC, ALU.mult, ["small"], ["small"])
    TT("vector", invR, inv, invC, ALU.subtract, ["small"], ["small"])
    TS("vector", sgn, sgn, 2.0, -1.0, ALU.mult, ALU.add, ["small"], ["small"])
    rrA = P.sb("rrA", [128, W], F32, off=qT_off)
    rrI = P.sb("rrI", [128, W], I32, off=qT_off + W * 4)
    ang = P.sb("ang", [128, W], F32, off=uT_off)
    P.op("gpsimd", lambda e: e.iota(COS[:], pattern=[[1, 36], [0, 64]], base=0, channel_multiplier=0,
                                    allow_small_or_imprecise_dtypes=True), (), ["COS"])
    P.op("gpsimd", lambda e: e.iota(SINS[:], pattern=[[0, 36], [1, 64]], base=0, channel_multiplier=0,
                                    allow_small_or_imprecise_dtypes=True), (), ["SINS"])
    HW_ = W // 2
    TWO_PI = float(2 * np.pi)
    for hh in range(2):
        sl = slice(hh * HW_, (hh + 1) * HW_)
        TS("vector", COS[:, sl], COS[:, sl], metat[:, 2:3], None, ALU.add, None, ["COS", "metat"], ["COS"])
        TS("vector", COS[:, sl], COS[:, sl], invR, None, ALU.mult, None, ["COS", "small"], ["COS"])
        TS("vector", SINS[:, sl], SINS[:, sl], invC, None, ALU.mult, None, ["SINS", "small"], ["SINS"])
    TT("vector", ang[:], COS[:], SINS[:], ALU.add, ["COS", "SINS"], ["ang"])

    def range_reduce_sin(dst, dstn, offset):
        TS("vector", rrA[:], ang[:], 1.0 / TWO_PI, offset / TWO_PI + 8.5, ALU.mult, ALU.add, ["ang"], ["rrA"])
        CP("vector", rrI[:], rrA[:], ["rrA"], ["rrI"])
        CP("vector", rrA[:], rrI[:], ["rrI"], ["rrA"])
        TS("vector", rrA[:], rrA[:], -TWO_PI, 8 * TWO_PI + offset, ALU.mult, ALU.add, ["rrA"], ["rrA"])
        TT("vector", dst[:], ang[:], rrA[:], ALU.add, ["ang", "rrA"], [dstn])
        TS("vector", rrA[:], dst[:], float(np.pi), -TWO_PI, ALU.is_gt, ALU.mult, [dstn], ["rrA"])
        TT("vector", dst[:], dst[:], rrA[:], ALU.add, [dstn, "rrA"], [dstn])
        TS("vector", rrA[:], dst[:], -float(np.pi), TWO_PI, ALU.is_lt, ALU.mult, [dstn], ["rrA"])
        TT("vector", dst[:], dst[:], rrA[:], ALU.add, [dstn, "rrA"], [dstn])
        ACT(dst[:], dst[:], AF.Sin, [dstn], [dstn])

    range_reduce_sin(SINS, "SINS", 0.0)
    range_reduce_sin(COS, "COS", float(np.pi / 2))
    for hh in range(2):
        sl = slice(hh * HW_, (hh + 1) * HW_)
        TS("vector", SINS[:, sl], SINS[:, sl], sgn, None, ALU.mult, None, ["SINS", "small"], ["SINS"])

    w_in_v = w_in.rearrange("(k p) n -> p k n", p=128)
    DMA("sync", "d_stg", stg[:], w_in_v[:, :, 0:640], [], ["stg"])
    qd = WIN[:, :, 0:512].rearrange("p k (c h d) -> p k c h d", c=4, h=2, d=64)
    qs = stg[:, :, 0:512].rearrange("p k (h c d) -> p k c h d", h=2, c=4, d=64)
    for h in range(2):
        ACT(qd[:, :, :, h, :], qs[:, :, :, h, :], AF.Copy, ["stg"], [("WIN", "q", h)])
    qd2 = WIN[:, :, 512:1024].rearrange("p k (c h s d) -> p k c h s d", c=4, h=2, s=2, d=32)
    qs2 = stg[:, :, 0:512].rearrange("p k (h c s d) -> p k c h s d", h=2, c=4, s=2, d=32)
    for h in range(2):
        for s in range(2):
            ACT(qd2[:, :, :, h, s, :], qs2[:, :, :, h, 1 - s, :], AF.Copy, ["stg"], [("WIN", "qsw", h, s)])
    ACT(WIN[:, :, 1024:1152], stg[:, :, 512:640], AF.Copy, ["stg"], [("WIN", "k")])
    kd2 = WIN[:, :, 1152:1280].rearrange("p k (h s d) -> p k h s d", h=2, s=2, d=32)
    ks2 = stg[:, :, 512:640].rearrange("p k (h s d) -> p k h s d", h=2, s=2, d=32)
    for s in range(2):
        ACT(kd2[:, :, :, s, :], ks2[:, :, :, 1 - s, :], AF.Copy, ["stg"], [("WIN", "ksw", s)])
    WINQ = [("WIN", "q", 0), ("WIN", "q", 1)]
    WINQS = [("WIN", "qsw", h, s) for h in range(2) for s in range(2)]
    WINK = [("WIN", "k")]
    WINKS = [("WIN", "ksw", 0), ("WIN", "ksw", 1)]
    for (nm, d0, s0, n) in (("v", 1280, 640, 128), ("bg", 1408, 768, 512), ("cg", 1920, 1280, 512), ("hv", 2432, 1792, 512)):
        P.dma("gpsimd", "d_win_" + nm, (lambda d0=d0, s0=s0, n=n: (lambda e: e.dma_start(out=WIN[:, :, d0:d0 + n], in_=w_in_v[:, :, s0:s0 + n])))(),
              [], [("WIN", nm)])
    for kk in range(3):
        for c4 in range(4):
            P.dma("sync", "d_cw", (lambda kk=kk, c4=c4: (lambda e: e.dma_start(
                out=cw[:, c4, kk:kk + 1], in_=conv_w[kk, c4 * 128:(c4 + 1) * 128].rearrange("(p o) -> p o", o=1))))(),
                [], [("cw", kk, c4)])
    MSET("vector", Vt[:, :, :, 64:65], 1.0, [("Vt", "ones")])
    MSET("vector", Vc[:, :, :, 64:65], 1.0, [("Vc", "ones")])

    xt_rr = [0]

    def norm_mod(src_rows, Gn, Sn, hbuf, hname, extra_r=()):
        i = xt_rr[0] % 2
        xt_rr[0] += 1
        xtile, xn = xt[i], f"xt{i}"
        DMA("sync", "d_" + xn, xtile[:], src_rows, list(extra_r), [xn])
        norm_mod_sb(xtile, xn, Gn, Sn, hbuf, hname)
        return xtile, xn

    tf2 = P.sb("tf2", [128, 1024], F32, off=bgT_off + 16384)
    nm_rr = [0]

    def norm_mod_sb(xtile, xn, Gn, Sn, hbuf, hname):
        pi = nm_rr[0] % 2
        nm_rr[0] += 1
        tfx, tfn = (tf, "tf") if pi == 0 else (tf2, "tf2")
        ss = small[:, 16 + 2 * pi:17 + 2 * pi]
        rstd = small[:, 17 + 2 * pi:18 + 2 * pi]
        ssn, rsn = f"ss{pi}", f"rstd{pi}"
        ACT(tfx[:], xtile[:], AF.Square, [xn], [tfn])
        RED("vector", ss, tfx[:], ALU.add, [tfn], [ssn])
        TS("vector", rstd, ss, 1.0 / 1024.0, 1e-6, ALU.mult, ALU.add, [ssn], [rsn])
        ACT(rstd, rstd, AF.Ln, [rsn], [rsn])
        ACT(rstd, rstd, AF.Exp, [rsn], [rsn], scale=-0.5)
        ACT(tfx[:], xtile[:], AF.Copy, [xn, rsn], [tfn], scale=rstd)
        TT("vector", tfx[:], tfx[:], rows[Gn][:], ALU.mult, [tfn] + rowdeps(Gn), [tfn])
        TT("vector", hbuf[:], tfx[:], rows[Sn][:], ALU.add, [tfn] + rowdeps(Sn), [hname])

    def transpose_to(hbuf, hname, dst, dst_name):
        pb, pbn = psb()
        pbv = pb[:].rearrange("p (k t) -> p k t", k=8)
        for k in range(8):
            TR(pbv[:, k, :], hbuf[:, k * 128:(k + 1) * 128], ident_b[:], [hname, "ident_b"], [(pbn, k)])
        ACT(dst, pbv, AF.Copy, [(pbn, k) for k in range(8)], [dst_name])

    hcT = hT[0]
    for t in range(2):
        norm_mod(ctx[t * 128:(t + 1) * 128, :], "cG1", "cS1", hb[t % 2], f"hb{t % 2}")
        transpose_to(hb[t % 2], f"hb{t % 2}", hcT[:, :, t * 128:(t + 1) * 128], ("hT0", t))
    ps, psn = psf("a")
    for k in range(8):
        MM(ps[:, 0:256], WIN[:, k, 1024:1152], hcT[:, k, 0:256], k == 0, k == 7,
           WINK + [("hT0", 0), ("hT0", 1)], [psn])
    ACT(kcT[:], ps[:, 0:256], AF.Copy, [psn], ["kcT"])
    for t in range(2):
        ps, psn = psf("a")
        for k in range(8):
            MM(ps[:, 0:128], hcT[:, k, t * 128:(t + 1) * 128], WIN[:, k, 1280:1408], k == 0, k == 7,
               [("WIN", "v"), ("hT0", t)], [psn])
        ACT(Vc[:, t, :, 0:64], ps[:, 0:128].rearrange("p (h d) -> p h d", h=2), AF.Copy, [psn], [("Vc", t)])

    if "ctx" in dbg:
        d1 = dbg_out("kcT", [128, 256], BF16)
        d2 = dbg_out("Vc", [128, 2 * 2 * 65], BF16)
        e1 = DMA("sync", "d_dbg", d1, kcT[:], ["kcT"], ["dbg1"])
        e2 = DMA("sync", "d_dbg", d2, Vc[:].rearrange("p a b c -> p (a b c)"), [("Vc", 0), ("Vc", 1), ("Vc", "ones")], ["dbg2"])
        P.finish("sync", [e1, e2])
    if stage <= 2:
        P.emit()
        return nc, dbg_outs

    chunks = [(0, 128, False)] + [(128 + 512 * i, 512, True) for i in range(4)] + [(2176, 128, False)]

    def prep_norm(ci, tiles):
        w0, n, central = chunks[ci]
        for t in tiles:
            norm_mod(x[w0 + t * 128:w0 + (t + 1) * 128, :], "G1", "S1", hb[t % 2], f"hb{t % 2}")

    def prep_trans(ci, tiles):
        hTc, hTn = hT[ci % 2], f"hT{ci % 2}"
        for t in tiles:
            transpose_to(hb[t % 2], f"hb{t % 2}", hTc[:, :, t * 128:(t + 1) * 128], (hTn, t))

    def build_items(ci):
        w0, n, central = chunks[ci]
        hTc, hTn = hT[ci % 2], f"hT{ci % 2}"
        ntile = n // 128
        hdeps = [(hTn, t) for t in range(ntile)]
        items = []

        def proj(col0, wdeps, cons):
            ps, psn = psf(cons)
            for k in range(8):
                MM(ps[:, 0:n], WIN[:, k, col0:col0 + 128], hTc[:, k, 0:n], k == 0, k == 7, wdeps + hdeps, [psn])
            return ps, psn

        def rope_out(col0, colsw, wd, wsd, dst, dstn):
            def f():
                pa, pan = proj(col0, wd, "v")
                pb_, pbn_ = proj(colsw, wsd, "v")
                TT("vector", rt1[:, 0:n], pa[:, 0:n], COS[:, w0:w0 + n], ALU.mult, [pan, "COS"], ["rt1"])
                TT("vector", rt2[:, 0:n], pb_[:, 0:n], SINS[:, w0:w0 + n], ALU.mult, [pbn_, "SINS"], ["rt2"])
                TT("vector", dst, rt1[:, 0:n], rt2[:, 0:n], ALU.add, ["rt1", "rt2"], [dstn])
            return f

        def v_item(t):
            def f():
                ps, psn = psf("a")
                for k in range(8):
                    MM(ps[:, 0:128], hTc[:, k, t * 128:(t + 1) * 128], WIN[:, k, 1280:1408], k == 0, k == 7,
                       [("WIN", "v"), (hTn, t)], [psn])
                wt = w0 // 128 + t
                ACT(Vt[:, wt, :, 0:64], ps[:, 0:128].rearrange("p (h d) -> p h d", h=2), AF.Copy, [psn], [("Vt", wt)])
            return f

        def bg_item(c):
            def f():
                ps, psn = proj(1408 + c * 128, [("WIN", "bg")], "a")
                ACT(bgT[:, c, w0 - 128:w0 - 128 + n], ps[:, 0:n], AF.Copy, [psn], [("bgT", c, ci)])
            return f

        def u_item(c):
            def f():
                pc, pcn = proj(1920 + c * 128, [("WIN", "cg")], "a")
                ph, phn = proj(2432 + c * 128, [("WIN", "hv")], "v")
                ACT(cgs[:, 0:n], pc[:, 0:n], AF.Copy, [pcn], ["cgs"])
                TT("vector", uT[:, c, w0:w0 + n], ph[:, 0:n], cgs[:, 0:n], ALU.mult, [phn, "cgs"], [("uT", c, ci)])
            return f

        if central:
            for c in range(4):
                items.append(rope_out(c * 128, 512 + c * 128, WINQ, WINQS, qT[:, c, w0:w0 + n], ("qT", c, ci)))
        items.append(rope_out(1024, 1152, WINK, WINKS, kT[:, w0:w0 + n], ("kT", ci)))
        for t in range(ntile):
            items.append(v_item(t))
        for c in range(4):
            if central:
                items.append(bg_item(c))
            items.append(u_item(c))
        return items

    def tiles_of(ci):
        return list(range(chunks[ci][1] // 128))

    prep_norm(0, tiles_of(0))
    prep_trans(0, tiles_of(0))
    for ci in range(len(chunks)):
        items = build_items(ci)
        nxt = ci + 1 if ci + 1 < len(chunks) else None
        half = (len(items) + 1) // 2
        if nxt is not None:
            prep_norm(nxt, tiles_of(nxt)[0:2])
        for f in items[:half]:
            f()
        if nxt is not None:
            prep_trans(nxt, tiles_of(nxt)[0:2])
            prep_norm(nxt, tiles_of(nxt)[2:4])
        for f in items[half:]:
            f()
        if nxt is not None:
            prep_trans(nxt, tiles_of(nxt)[2:4])

    if "proj" in dbg:
        d1 = dbg_out("qT", [128, 4 * W], BF16)
        d2 = dbg_out("kT", [128, W], BF16)
        d3 = dbg_out("Vt", [128, 18 * 130], BF16)
        d4 = dbg_out("uT", [128, 4 * W], BF16)
        d5 = dbg_out("bgT", [128, 4 * 2048], BF16)
        allq = [("qT", c, ci) for c in range(4) for ci in range(1, 5)]
        allk = [("kT", ci) for ci in range(6)]
        allv = [("Vt", t) for t in range(18)] + [("Vt", "ones")]
        allu = [("uT", c, ci) for c in range(4) for ci in range(6)]
        allb = [("bgT", c, ci) for c in range(4) for ci in range(1, 5)]
        evs = [DMA("sync", "d_dbg", d1, qT[:].rearrange("p a b -> p (a b)"), allq, ["dbg1"]),
               DMA("sync", "d_dbg", d2, kT[:], allk, ["dbg2"]),
               DMA("sync", "d_dbg", d3, Vt[:].rearrange("p a b c -> p (a b c)"), allv, ["dbg3"]),
               DMA("sync", "d_dbg", d4, uT[:].rearrange("p a b -> p (a b)"), allu, ["dbg4"]),
               DMA("sync", "d_dbg", d5, bgT[:].rearrange("p a b -> p (a b)"), allb, ["dbg5"])]
        P.finish("sync", evs)
    if stage <= 3:
        P.emit()
        return nc, dbg_outs

    ALLWIN = WINQ + WINQS + WINK + WINKS + [("WIN", nm) for nm in ("v", "bg", "cg", "hv")]
    o2 = REG0 + 32768
    PL = [P.sb(f"PL{i}", [128, 384], BF16, off=o2 + i * 768) for i in range(2)]; o2 += 1536
    PC = [P.sb(f"PC{i}", [128, 256], BF16, off=o2 + i * 512) for i in range(2)]; o2 += 1024
    att_tm = P.sb("att_tm", [128, 512], BF16, off=o2); o2 += 1024
    rec = P.sb("rec", [128, 8], F32, off=o2); o2 += 64
    cvt = [P.sb(f"cvt{i}", [128, 512], F32, off=o2 + i * 2048) for i in range(2)]; o2 += 4096
    assert o2 <= REG0 + 47104

    def kchunk(wb):
        return 0 if wb == 0 else (5 if wb == 17 else 1 + (wb - 1) // 4)

    VONES = [("Vt", "ones")]
    TS("vector", uT[:, :, 127:128], uT[:, :, 127:128], metat[:, 0:1], None, ALU.mult, None,
       [("uT", c, 0) for c in range(4)] + ["metat"], [("uT", c, 0) for c in range(4)])
    TS("vector", uT[:, :, 2176:2177], uT[:, :, 2176:2177], metat[:, 1:2], None, ALU.mult, None,
       [("uT", c, 5) for c in range(4)] + ["metat"], [("uT", c, 5) for c in range(4)])

    def conv_unit(tcn, c):
        w0 = 128 + tcn * 512
        ud = [("uT", c, ci) for ci in (tcn, tcn + 1, tcn + 2)]
        cwd = [("cw", kk, c4) for kk in range(3) for c4 in range(4)]
        TS("vector", cvt[0][:], uT[:, c, w0 - 1:w0 + 511], cw[:, c, 0:1], None, ALU.mult, None, ud + cwd, ["cvt0"] + ALLWIN)
        TS("vector", cvt[1][:], uT[:, c, w0:w0 + 512], cw[:, c, 1:2], None, ALU.mult, None, ud + cwd, ["cvt1"] + ALLWIN)
        TT("vector", cvt[0][:], cvt[0][:], cvt[1][:], ALU.add, ["cvt0", "cvt1"], ["cvt0"])
        TS("vector", cvt[1][:], uT[:, c, w0 + 1:w0 + 513], cw[:, c, 2:3], None, ALU.mult, None, ud + cwd, ["cvt1"])
        TT("vector", cvt[0][:], cvt[0][:], cvt[1][:], ALU.add, ["cvt0", "cvt1"], ["cvt0"])
        TT("vector", mixT[:, 4 + c, tcn * 512:(tcn + 1) * 512], cvt[0][:], bgT[:, c, tcn * 512:(tcn + 1) * 512], ALU.mult,
           ["cvt0", ("bgT", c, tcn + 1)], [("mixT", "conv", c, tcn)] + ALLWIN)

    sbanks = [3, 4, 5, 2]
    srr = [0]

    def sbank():
        bi = sbanks[srr[0] % 4]
        srr[0] += 1
        return PSF[bi], f"psf{bi}"

    for i in range(1, 17):
        ci_q = 1 + (i - 1) // 4
        mv = 1 if i == 1 else (2 if i == 16 else 0)
        pvs = [(PSF[0], "psf0"), (PSF[1], "psf1")]

        def S_(hn, i=i, ci_q=ci_q):
            half, c = hn // 4, hn % 4
            r0 = half * 64
            sl, sln = sbank()
            sc_, scn = sbank()
            qsl = qT[r0:r0 + 64, c, i * 128:(i + 1) * 128]
            for kb in range(3):
                wb = i - 1 + kb
                MM(sl[:, kb * 128:(kb + 1) * 128], kT[r0:r0 + 64, wb * 128:(wb + 1) * 128], qsl, True, True,
                   [("qT", c, ci_q), ("kT", kchunk(wb))], [sln])
            for cb in range(2):
                MM(sc_[:, cb * 128:(cb + 1) * 128], kcT[r0:r0 + 64, cb * 128:(cb + 1) * 128], qsl, True, True,
                   [("qT", c, ci_q), "kcT"], [scn])
            return sl, sln, sc_, scn

        def EPV_(hn, st, i=i, mv=mv, pvs=pvs):
            sl, sln, sc_, scn = st
            half, c = hn // 4, hn % 4
            j = hn % 2
            ACT(PL[j][:], sl[:, 0:384], AF.Exp, [sln], [f"PL{j}"] + ALLWIN, scale=0.125)
            ACT(PC[j][:], sc_[:, 0:256], AF.Exp, [scn], [f"PC{j}"] + ALLWIN, scale=0.125)
            TT("vector", PL[j][:], PL[j][:], mask3[:, mv, :], ALU.mult,
               [f"PL{j}", ("mask3", mv), ("mask3", mv, 1), ("mask3", mv, 2)], [f"PL{j}"])
            pv, pvn = pvs[half]
            pvr = pv[:, c * 65:(c + 1) * 65]
            for kb in range(3):
                wb = i - 1 + kb
                MM(pvr, PL[j][:, kb * 128:(kb + 1) * 128], Vt[:, wb, half, :], kb == 0, False,
                   [f"PL{j}", ("Vt", wb)] + VONES, [pvn])
            for cb in range(2):
                MM(pvr, PC[j][:, cb * 128:(cb + 1) * 128], Vc[:, cb, half, :], False, cb == 1,
                   [f"PC{j}", ("Vc", cb), ("Vc", "ones")], [pvn])

        st = S_(0)
        for hn in range(8):
            nxt = S_(hn + 1) if hn < 7 else None
            EPV_(hn, st)
            st = nxt
        for b in range(2):
            pv, pvn = pvs[b]
            pvv = pv[:, 0:260].rearrange("p (h e) -> p h e", h=4)
            TT("vector", rec[:, b * 4:(b + 1) * 4].unsqueeze(2), pvv[:, :, 64:65], esink[:, b * 4:(b + 1) * 4].unsqueeze(2),
               ALU.add, [pvn, "esink"], [("rec", b)] + ALLWIN)
            P.op("vector", (lambda b=b: (lambda e: e.reciprocal(rec[:, b * 4:(b + 1) * 4], rec[:, b * 4:(b + 1) * 4])))(),
                 [("rec", b)], [("rec", b)])
            TT("vector", att_tm[:, b * 256:(b + 1) * 256].rearrange("p (h d) -> p h d", h=4), pvv[:, :, 0:64],
               rec[:, b * 4:(b + 1) * 4].unsqueeze(2).to_broadcast([128, 4, 64]), ALU.mult,
               [pvn, ("rec", b)], [("att_tm", b)] + ALLWIN)
        pb, pbn = psb()
        pbv = pb[:, 0:512].rearrange("p (k t) -> p k t", k=4)
        for cc in range(4):
            TR(pbv[:, cc, :], att_tm[:, cc * 128:(cc + 1) * 128], ident_b[:], [("att_tm", cc // 2), "ident_b"], [(pbn, cc)])
        ACT(mixT[:, 0:4, (i - 1) * 128:i * 128], pbv, AF.Copy, [(pbn, cc) for cc in range(4)],
            [("mixT", "att", i - 1)] + ALLWIN)
        conv_unit((i - 1) // 4, (i - 1) % 4)

    if stage <= 4:
        P.emit()
        return nc, dbg_outs

    HTALL = [(f"hT{a}", t) for a in range(2) for t in range(4)]
    P.dma("gpsimd", "d_wo", lambda e: e.dma_start(out=wo[:], in_=w_out.rearrange("(k p) n -> p k n", p=128)), [], ["wo"] + HTALL)
    QALL = [("qT", c, ci) for c in range(4) for ci in range(1, 5)]
    o3 = qT_off
    h2T = [P.sb(f"h2T{i}", [128, 8, 128], BF16, off=o3 + i * 2048) for i in range(2)]; o3 += 4096
    CONVDEAD = [("uT", c, ci) for c in range(4) for ci in range(6)] + [("bgT", c, ci) for c in range(4) for ci in range(1, 5)]
    rows_off = REG0 - 8 * 4096
    affTM = P.sb("affTM", [128, 16, 16], F32, off=rows_off)
    gm = P.sb("gm", [128, 16, 16], F32, off=rows_off + 1024)
    thr = P.sb("thr", [128, 16], F32, off=rows_off + 2048)
    affT = P.sb("affT", [16, 2048], F32, off=o3); o3 += 8192
    wr = P.sb("wr", [128, 8, 16], BF16, off=o3); o3 += 256
    sm = P.sb("sm", [128, 64], F32, off=o3); o3 += 256
    assert o3 <= qT_off + 4 * W * 2
    P.dma("gpsimd", "d_wr", lambda e: e.dma_start(out=wr[:], in_=w_router.rearrange("(k p) e -> p k e", p=128)), [], ["wr"] + QALL)
    def a3_A(tile):
        tcn = tile // 4
        mdeps = [("mixT", "att", tile)] + [("mixT", "conv", c, tcn) for c in range(4)]
        i = xt_rr[0] % 2
        xt_rr[0] += 1
        xtile, xn = xt[i], f"xt{i}"
        DMA("sync", "d_" + xn, xtile[:], x[128 + tile * 128:256 + tile * 128, :], [], [xn])
        for n in range(2):
            ps, psn = psf("v")
            for k in range(8):
                MM(ps[:], mixT[:, k, tile * 128:(tile + 1) * 128], wo[:, k, n * 512:(n + 1) * 512], k == 0, k == 7,
                   mdeps + ["wo"], [psn])
            TT("vector", tf[:, n * 512:(n + 1) * 512], ps[:], rows["GT1"][:, n * 512:(n + 1) * 512], ALU.mult,
               [psn] + rowdeps("GT1"), ["tf"])
        TT("vector", xtile[:], xtile[:], tf[:], ALU.add, [xn, "tf"], [xn])
        DMA("sync", "d_x1d_" + xn, x1d[tile * 128:(tile + 1) * 128, :], xtile[:], [xn], [("x1d", tile)])
        j = tile % 2
        norm_mod_sb(xtile, xn, "G2", "S2", hb[j], f"hb{j}")
        DMA("sync", f"d_h2loc{j}", h2loc[tile * 128:(tile + 1) * 128, :], hb[j][:], [f"hb{j}"], [("h2loc", tile)])

    def a3_B(tile):
        j = tile % 2
        h2v = h2T[j][:]
        pb, pbn = psb()
        pbv = pb[:].rearrange("p (k t) -> p k t", k=8)
        for k in range(8):
            TR(pbv[:, k, :], hb[j][:, k * 128:(k + 1) * 128], ident_b[:], [f"hb{j}", "ident_b"], [(pbn, k)])
        ACT(h2v, pbv, AF.Copy, [(pbn, k) for k in range(8)], [f"h2T{j}"] + QALL)
        ps, psn = psf("v")
        for k in range(8):
            MM(ps[:, 0:16], h2T[j][:, k, :], wr[:, k, :], k == 0, k == 7, [f"h2T{j}", "wr"], [psn])
        mx, nmx, ssum, ex = sm[:, 0:1], sm[:, 1:2], sm[:, 2:3], sm[:, 16:32]
        af = affTM[:, tile, :]
        RED("vector", mx, ps[:, 0:16], ALU.max, [psn], ["sm_mx"])
        TS("vector", nmx, mx, -1.0, None, ALU.mult, None, ["sm_mx"], ["sm_nmx"])
        ACT(ex, ps[:, 0:16], AF.Exp, [psn, "sm_nmx"], ["sm_ex"], bias=nmx)
        RED("vector", ssum, ex, ALU.add, ["sm_ex"], ["sm_sum"])
        P.op("vector", lambda e: e.reciprocal(sm[:, 2:3], sm[:, 2:3]), ["sm_sum"], ["sm_sum"])
        TS("vector", af, ex, ssum, None, ALU.mult, None, ["sm_ex", "sm_sum"], [("affTM", tile)])
        pt, ptn = psf("a")
        TR(pt[0:16, 0:128], af, ident_f[:], [("affTM", tile), "ident_f"], [ptn])
        ACT(affT[:, tile * 128:(tile + 1) * 128], pt[0:16, 0:128], AF.Copy, [ptn], [("affT", tile)] + QALL)

    a3_A(0)
    for tile in range(16):
        if tile + 1 < 16:
            a3_A(tile + 1)
        a3_B(tile)
    DMA("sync", "d_affloc", affloc, affT[:], [("affT", t) for t in range(16)], ["affloc"])

    if "a3" in dbg:
        d1 = dbg_out("x1", [2048, 1024])
        d2 = dbg_out("aff", [16, 2048])
        d3 = dbg_out("h2", [2048, 1024], BF16)
        e1 = DMA("sync", "d_dbg1", d1, x1d, [("x1d", t) for t in range(16)], ["dbg1"])
        e2 = DMA("sync", "d_dbg2", d2, affloc, ["affloc"], ["dbg2"])
        e3 = DMA("sync", "d_dbg3", d3, h2loc, [("h2loc", t) for t in range(16)], ["dbg3"])
        P.finish("sync", [e1, e2, e3])
    if stage <= 5:
        P.emit()
        return nc, dbg_outs

    FENCE = [k for k in P.res.keys() if not (isinstance(k, str) and (k.startswith("psf") or k in ("ident_b", "ident_f", "ones_b", "metat")))
             and not (isinstance(k, tuple) and k[0] in ("row", "h2T", "affTM", "x1d", "h2loc"))]
    P.dma("gpsimd", "d_ag_aff", lambda e: e.collective_compute("AllGather", ALU.bypass, replica_groups=GROUPS,
                                                               ins=[affloc.opt()], outs=[affall.opt()]),
          ["affloc"], ["affall", "agchain"], inc=1)
    wslot = [P.sb(f"wslot{i}", [128, 8, 1024], BF16, off=REG0 + i * 16384) for i in range(4)]
    wdt_ = P.sb("wd_", [128, 8, 1024], BF16, off=REG0 + 81920)
    wgv = w_gate.rearrange("e (k p) n -> e p k n", p=128)
    wuv = w_up.rearrange("e (k p) n -> e p k n", p=128)
    wdv = w_down.rearrange("e (k p) n -> e p k n", p=128)
    P.dma("gpsimd", "d_ws0", lambda e: e.dma_start(out=wslot[0][:], in_=wgv[0]), [], ["ws0"] + FENCE)
    P.dma("gpsimd", "d_ws1", lambda e: e.dma_start(out=wslot[1][:], in_=wuv[0]), [], ["ws1"] + FENCE)
    P.dma("gpsimd", "d_wd", lambda e: e.dma_start(out=wdt_[:], in_=wdv[0]), [], ["wd_"] + FENCE)
    NTB, NITB, NE = 16, 7, 4
    ob = bgT_off + 32768
    AFt = P.sb("AFt", [128, NE, 64], F32, off=ob); ob += 1024
    FR = P.sb("FR", [128, NE, NTB], F32, off=ob); ob += 256
    Tt = P.sb("Tt", [128, NE, NTB], F32, off=ob); ob += 256
    tmpa = P.sb("tmpa", [128, NE, NTB], F32, off=ob); ob += 256
    get = P.sb("get", [128, NE, NTB], F32, off=ob); ob += 256
    cntb = P.sb("cntb", [128, NE * NTB], BF16, off=ob); ob += 128
    lo = P.sb("lo", [128, NE], F32, off=ob); ob += 64
    hi = P.sb("hi", [128, NE], F32, off=ob); ob += 64
    wdt = P.sb("wdt", [128, NE], F32, off=ob); ob += 64
    red = P.sb("red", [128, NE], F32, off=ob); ob += 64
    aix = P.sb("aix", [128, 8], F32, off=ob); ob += 64
    AIDX = P.sb("AIDX", [128, NE], I32, off=ob); ob += 64
    assert ob <= rt1_off + 6144
    cmpb = P.sb("cmpb", [128, NE, NTB, 64], BF16, off=REG0 + 49152)
    P.op("gpsimd", lambda e: e.iota(aix[:, 0:1], pattern=[[0, 1]], base=0, channel_multiplier=1,
                                    allow_small_or_imprecise_dtypes=True), (), ["aix"] + FENCE)
    TS("vector", aix[:, 1:2], aix[:, 0:1], 32.0, None, ALU.is_ge, None, ["aix"], ["aix"])
    for thv in (64.0, 96.0):
        TS("vector", aix[:, 2:3], aix[:, 0:1], thv, None, ALU.is_ge, None, ["aix"], ["aix"])
        TT("vector", aix[:, 1:2], aix[:, 1:2], aix[:, 2:3], ALU.add, ["aix"], ["aix"])
    TS("vector", aix[:, 1:2], aix[:, 1:2], 480.0, None, ALU.mult, None, ["aix"], ["aix"])
    TT("vector", aix[:, 1:2], aix[:, 1:2], aix[:, 0:1], ALU.add, ["aix"], ["aix"])
    TS("vector", aix[:, 1:2], aix[:, 1:2], metat[:, 7:8], None, ALU.add, None, ["aix", "metat"], ["aix"])
    for k in range(NE):
        TS("vector", aix[:, 4 + k:5 + k], aix[:, 1:2], 32.0 * k, None, ALU.add, None, ["aix"], ["aix"])
    CP("vector", AIDX[:], aix[:, 4:8], ["aix"], ["AIDX"])
    affrows = affall.rearrange("a (p j) -> (a p) j", j=64)
    for k in range(NE):
        P.dma("gpsimd", "d_AFt", (lambda k=k: (lambda e: e.indirect_dma_start(
            out=AFt[:, k, :], out_offset=None, in_=affrows, in_offset=bass.IndirectOffsetOnAxis(ap=AIDX[:, k:k + 1], axis=0))))(),
            ["affall", "AIDX"], [("AFt", k)] + FENCE)
    AFD = [("AFt", k) for k in range(NE)]
    P.op("gpsimd", lambda e: e.iota(FR[:], pattern=[[0, NE], [1, NTB]], base=1, channel_multiplier=0,
                                    allow_small_or_imprecise_dtypes=True), (), ["FR"] + FENCE)
    TS("vector", FR[:], FR[:], 1.0 / (NTB + 1), None, ALU.mult, None, ["FR"], ["FR"])
    MSET("vector", lo[:], 0.0, ["lo"] + FENCE)
    MSET("vector", hi[:], 1.0, ["hi"])
    for it in range(NITB):
        TT("vector", wdt[:], hi[:], lo[:], ALU.subtract, ["hi", "lo"], ["wdt"])
        TT("vector", Tt[:], FR[:], wdt[:].unsqueeze(2).to_broadcast([128, NE, NTB]), ALU.mult, ["FR", "wdt"], ["Tt"])
        TT("vector", Tt[:], Tt[:], lo[:].unsqueeze(2).to_broadcast([128, NE, NTB]), ALU.add, ["Tt", "lo"], ["Tt"])
        TT("vector", cmpb[:], AFt[:].unsqueeze(2).to_broadcast([128, NE, NTB, 64]),
           Tt[:].unsqueeze(3).to_broadcast([128, NE, NTB, 64]), ALU.is_ge, AFD + ["Tt"], ["cmpb"] + FENCE)
        RED("vector", tmpa[:], cmpb[:], ALU.add, ["cmpb"], ["tmpa"])
        CP("vector", cntb[:], tmpa[:].rearrange("p e k -> p (e k)"), ["tmpa"], ["cntb"])
        ps, psn = psf("v")
        MM(ps[:, 0:NE * NTB], ones_b[:], cntb[:], True, True, ["cntb", "ones_b"], [psn])
        TS("vector", get[:].rearrange("p e k -> p (e k)"), ps[:, 0:NE * NTB], 1024.0, None, ALU.is_ge, None, [psn], ["get"])
        TT("vector", tmpa[:], Tt[:], get[:], ALU.mult, ["Tt", "get"], ["tmpa"])
        RED("vector", red[:], tmpa[:], ALU.max, ["tmpa"], ["red"])
        TT("vector", lo[:], lo[:], red[:], ALU.max, ["lo", "red"], ["lo"])
        TS("vector", tmpa[:], get[:], 2.0, None, ALU.mult, None, ["get"], ["tmpa"])
        TT("vector", tmpa[:], tmpa[:], Tt[:], ALU.add, ["tmpa", "Tt"], ["tmpa"])
        RED("vector", red[:], tmpa[:], ALU.min, ["tmpa"], ["red"])
        TT("vector", hi[:], hi[:], red[:], ALU.min, ["hi", "red"], ["hi"])
    thr = lo

    if "thr" in dbg:
        d1 = dbg_out("thr", [128, 4])
        e1 = DMA("sync", "d_dbg1", d1, lo[:], ["lo"], ["dbg1"])
        P.finish("sync", [e1])
    if stage <= 6:
        P.emit()
        return nc, dbg_outs

    for j4 in range(4):
        P.dma("gpsimd", "d_ag_h2", (lambda j4=j4: (lambda e: e.collective_compute(
            "AllGather", ALU.bypass, replica_groups=GROUPS,
            ins=[h2loc[j4 * 512:(j4 + 1) * 512, :].opt()], outs=[h2all[j4 * 2048:(j4 + 1) * 2048, :].opt()])))(),
            [("h2loc", t) for t in range(j4 * 4, j4 * 4 + 4)] + ["agchain"], [("h2all", j4), "agchain"], inc=1)
    H2ALLD = [("h2all", j4) for j4 in range(4)]
    TAB = P.sb("TAB", [128, 4, 128], F32, off=qT_off)
    ob2 = kT_off
    ones64 = P.sb("ones64", [128, 64], F32, off=ob2); ob2 += 256
    n4 = P.sb("n4", [128, 4], F32, off=ob2); ob2 += 64
    t16 = P.sb("t16", [128, 16], F32, off=ob2); ob2 += 64
    rhsU = P.sb("rhsU", [128, 4, 128], BF16, off=ob2); ob2 += 1024
    rhsI = P.sb("rhsI", [128, 4, 128], BF16, off=ob2); ob2 += 1024
    offs_sb = P.sb("offs_sb", [128, 4, 128], F32, off=ob2); ob2 += 2048
    nrow_sb = P.sb("nrow_sb", [128, 4, 128], F32, off=ob2); ob2 += 2048
    sval = P.sb("sval", [128, 8], F32, off=ob2); ob2 += 64
    koffs = P.sb("koffs", [128, 4, 8], F32, off=ob2); ob2 += 128
    pS = P.sb("pS", [128, 4, 8], F32, off=ob2); ob2 += 128
    oex = P.sb("oex", [128, 4, 8], F32, off=ob2); ob2 += 128
    rS = P.sb("rS", [128, 4, 8], F32, off=ob2); ob2 += 128
    jS = P.sb("jS", [128, 4, 8], F32, off=ob2); ob2 += 128
    gS = P.sb("gS", [128, 4, 8], F32, off=ob2); ob2 += 128
    tSf = P.sb("tSf", [128, 4, 8], F32, off=ob2); ob2 += 128
    RIDX = P.sb("RIDX", [128, 4, 8], I32, off=ob2); ob2 += 128
    TIDX = P.sb("TIDX", [128, 4, 8], I32, off=ob2); ob2 += 128
    ZIDXf = P.sb("ZIDXf", [128, 4, 16], F32, off=ob2); ob2 += 256
    ZIDX = P.sb("ZIDX", [128, 4, 16], I32, off=ob2); ob2 += 256
    yz = [P.sb(f"yz{i}", [128, 1024], BF16, off=rows_off + 3 * 4096 + i * 2048) for i in range(2)]
    assert ob2 <= bgT_off, ob2
    cmpP = P.sb("cmpP", [128, 4, 8, 128], F32, off=REG0 + 32768)
    Gt = P.sb("Gt", [128, 4, 8, 128], F32, off=REG0 + 49152)

    MSET("vector", ones64[:], 1.0, ["ones64"] + FENCE)
    TT("vector", TAB[:, :, 64:128], AFt[:], lo[:].unsqueeze(2).to_broadcast([128, 4, 64]), ALU.is_ge, AFD + ["lo"], ["TABm"] + FENCE)
    for e16 in range(4):
        P.op("vector", (lambda e16=e16: (lambda e: e.tensor_tensor_scan(out=TAB[:, e16, 0:64], data0=ones64[:], data1=TAB[:, e16, 64:128],
                                                                          initial=0.0, op0=ALU.mult, op1=ALU.add)))(),
             ["TABm", "ones64"], [("TABc", e16)])
    TABC = [("TABc", e16) for e16 in range(4)]
    TT("vector", TAB[:, :, 64:128], TAB[:, :, 64:128], AFt[:], ALU.mult, ["TABm"] + TABC + AFD, ["TABm"])
    DMA("sync", "d_tabd", tabd[0:512, :].rearrange("(e p) c -> p e c", p=128), TAB[:], ["TABm"] + TABC, ["tabd"])
    for k in range(4):
        CP("vector", n4[:, k:k + 1], TAB[:, k, 63:64], TABC, [("n4", k)])
    N4 = [("n4", k) for k in range(4)]
    TT("vector", rhsU[:], n4[:].unsqueeze(2).to_broadcast([128, 4, 128]), U_b[:].unsqueeze(1).to_broadcast([128, 4, 128]), ALU.mult,
       N4 + ["U_b"], ["rhsU"])
    TT("vector", rhsI[:], n4[:].unsqueeze(2).to_broadcast([128, 4, 128]), ident_b[:].unsqueeze(1).to_broadcast([128, 4, 128]), ALU.mult,
       N4 + ["ident_b"], ["rhsI"])
    ps, psn = psf("v")
    MM(ps[:], ones_b[:], rhsU[:].rearrange("p k q -> p (k q)"), True, True, ["rhsU", "ones_b"], [psn])
    CP("vector", offs_sb[:].rearrange("p k q -> p (k q)"), ps[:], [psn], ["offs_sb"])
    ps, psn = psf("v")
    MM(ps[:], ones_b[:], rhsI[:].rearrange("p k q -> p (k q)"), True, True, ["rhsI", "ones_b"], [psn])
    CP("vector", nrow_sb[:].rearrange("p k q -> p (k q)"), ps[:], [psn], ["nrow_sb"])
    P.op("gpsimd", lambda e: e.iota(sval[:], pattern=[[128, 8]], base=0, channel_multiplier=1,
                                    allow_small_or_imprecise_dtypes=True), (), ["sval"])
    P.op("gpsimd", lambda e: e.iota(koffs[:], pattern=[[128, 4], [0, 8]], base=0, channel_multiplier=0,
                                    allow_small_or_imprecise_dtypes=True), (), ["koffs"])
    P.op("gpsimd", lambda e: e.iota(ZIDXf[:], pattern=[[512, 4], [2048, 16]], base=0, channel_multiplier=1,
                                    allow_small_or_imprecise_dtypes=True), (), ["ZIDXf"])
    TS("vector", ZIDXf[:], ZIDXf[:], metat[:, 6:7], None, ALU.add, None, ["ZIDXf", "metat"], ["ZIDXf"])
    CP("vector", ZIDX[:], ZIDXf[:], ["ZIDXf"], ["ZIDX"])
    svb = sval[:].unsqueeze(1).to_broadcast([128, 4, 8])
    TT("vector", cmpP[:], offs_sb[:].unsqueeze(2).to_broadcast([128, 4, 8, 128]),
       svb.unsqueeze(3).to_broadcast([128, 4, 8, 128]), ALU.is_le, ["offs_sb", "sval"], ["cmpP"] + FENCE)
    RED("vector", pS[:], cmpP[:], ALU.add, ["cmpP"], ["pS"])
    TT("vector", cmpP[:], cmpP[:], nrow_sb[:].unsqueeze(2).to_broadcast([128, 4, 8, 128]), ALU.mult, ["cmpP", "nrow_sb"], ["cmpP"])
    RED("vector", oex[:], cmpP[:], ALU.add, ["cmpP"], ["oex"])
    TT("vector", rS[:], svb, oex[:], ALU.subtract, ["sval", "oex"], ["rS"])
    TT("vector", tSf[:], pS[:], koffs[:], ALU.add, ["pS", "koffs"], ["tSf"])
    CP("vector", RIDX[:], tSf[:], ["tSf"], ["RIDX"])
    for k in range(4):
        for c in range(8):
            P.dma("gpsimd", "d_G", (lambda k=k, c=c: (lambda e: e.indirect_dma_start(
                out=Gt[:, k, c, :], out_offset=None, in_=tabd[0:512, :], in_offset=bass.IndirectOffsetOnAxis(ap=RIDX[:, k, c:c + 1], axis=0))))(),
                ["tabd", "RIDX"], [("Gt", k, c), "cmpb"] if (k == 0 and c == 0) else [("Gt", k, c)])
    GALL = [("Gt", k, c) for k in range(4) for c in range(8)]
    cmpG = cmpP[:, :, :, 0:64]
    TT("vector", cmpG, Gt[:, :, :, 0:64], rS[:].unsqueeze(3).to_broadcast([128, 4, 8, 64]), ALU.is_le, GALL + ["rS"], ["cmpP"])
    RED("vector", jS[:], cmpG, ALU.add, ["cmpP"], ["jS"])
    TS("vector", oex[:], rS[:], 1.0, None, ALU.add, None, ["rS"], ["oex"])
    TT("vector", cmpG, Gt[:, :, :, 0:64], oex[:].unsqueeze(3).to_broadcast([128, 4, 8, 64]), ALU.is_equal, GALL + ["oex"], ["cmpP"])
    TT("vector", cmpG, cmpG, Gt[:, :, :, 64:128], ALU.mult, ["cmpP"] + GALL, ["cmpP"])
    RED("vector", gS[:], cmpG, ALU.add, ["cmpP"], ["gS"])
    TS("vector", tSf[:], pS[:], 64.0, None, ALU.mult, None, ["pS"], ["tSf"])
    TT("vector", tSf[:], tSf[:], jS[:], ALU.add, ["tSf", "jS"], ["tSf"])
    CP("vector", TIDX[:], tSf[:], ["tSf"], ["TIDX"])
    ra = P.sb("ra", [128, 4, 8], F32, off=ob2); rb = P.sb("rb", [128, 4, 8], F32, off=ob2 + 128)
    rj = P.sb("rj", [128, 4, 8], F32, off=ob2 + 256); GIDX = P.sb("GIDX", [128, 4, 8], I32, off=ob2 + 384)
    assert ob2 + 512 <= bgT_off
    TS("vector", ra[:], tSf[:], 2048.0, None, ALU.is_ge, None, ["tSf"], ["ra"])
    for thv in (4096.0, 6144.0):
        TS("vector", rb[:], tSf[:], thv, None, ALU.is_ge, None, ["tSf"], ["rb"])
        TT("vector", ra[:], ra[:], rb[:], ALU.add, ["ra", "rb"], ["ra"])
    TS("vector", rb[:], ra[:], -2048.0, None, ALU.mult, None, ["ra"], ["rb"])
    TT("vector", rb[:], rb[:], tSf[:], ALU.add, ["rb", "tSf"], ["rb"])
    TS("vector", rj[:], rb[:], 512.0, None, ALU.is_ge, None, ["rb"], ["rj"])
    for thv in (1024.0, 1536.0):
        TS("vector", oex[:], rb[:], thv, None, ALU.is_ge, None, ["rb"], ["oex"])
        TT("vector", rj[:], rj[:], oex[:], ALU.add, ["rj", "oex"], ["rj"])
    TT("vector", rj[:], rj[:], ra[:], ALU.subtract, ["rj", "ra"], ["rj"])
    TS("vector", rj[:], rj[:], 1536.0, None, ALU.mult, None, ["rj"], ["rj"])
    TT("vector", rj[:], rj[:], tSf[:], ALU.add, ["rj", "tSf"], ["rj"])
    CP("vector", GIDX[:], rj[:], ["rj"], ["GIDX"])
    ZSIDX = P.sb("ZSIDX", [128, 4, 8], I32, off=ob2 + 512)
    assert ob2 + 640 <= bgT_off
    TS("vector", rj[:], rb[:], 128.0, None, ALU.is_ge, None, ["rb"], ["rj"])
    for kk in range(2, 16):
        TS("vector", oex[:], rb[:], 128.0 * kk, None, ALU.is_ge, None, ["rb"], ["oex"])
        TT("vector", rj[:], rj[:], oex[:], ALU.add, ["rj", "oex"], ["rj"])
    TS("vector", rj[:], rj[:], 384.0, None, ALU.mult, None, ["rj"], ["rj"])
    TS("vector", oex[:], ra[:], -1920.0, None, ALU.mult, None, ["ra"], ["oex"])
    TT("vector", rj[:], rj[:], oex[:], ALU.add, ["rj", "oex"], ["rj"])
    TT("vector", rj[:], rj[:], tSf[:], ALU.add, ["rj", "tSf"], ["rj"])
    CP("vector", ZSIDX[:], rj[:], ["rj"], ["ZSIDX"])

    if "idx" in dbg:
        d1 = dbg_out("tidx", [128, 32])
        d2 = dbg_out("gS", [128, 32])
        e1 = DMA("sync", "d_dbg1", d1, tSf[:].rearrange("p a b -> p (a b)"), ["tSf", "TIDX"], ["dbg1"])
        e2 = DMA("sync", "d_dbg2", d2, gS[:].rearrange("p a b -> p (a b)"), ["gS"], ["dbg2"])
        P.finish("sync", [e1, e2])
    if stage <= 6.5:
        P.emit()
        return nc, dbg_outs

    hid = [P.sb(f"hid{i}", [128, 8, 512], BF16, off=qT_off + i * 8192) for i in range(2)]
    XS = P.sb("XS", [128, 8, 1024], BF16, off=bgT_off)
    xsT = P.sb("xsT", [128, 8, 1024], BF16, off=bgT_off + 16384)
    sgs = P.sb("sgs", [128, 512], F32, off=rt1_off + 4096)
    MSET("vector", XS[:], 0.0, ["XS"] + FENCE)
    ZD0 = []
    for t in range(8):
        ZD0.append(("Zd0", t))
        DMA("sync", "d_z0", Zd[t * 1024:(t + 1) * 1024, :].rearrange("(p c) d -> p c d", c=8), XS[:], ["XS"], [("Zd0", t)])
    yrr = [0]
    def issue_loads(k4):
        sg_, su_ = (k4 % 2) * 2, (k4 % 2) * 2 + 1
        wg_t, wu_t = wslot[sg_], wslot[su_]
        ex2 = (["cmpP"] if sg_ == 2 else [])
        ex3 = (["cmpb"] + GALL if su_ == 3 else [])
        if k4 > 0:
          P.dma("gpsimd", f"d_ws{sg_}", (lambda wg_t=wg_t, k4=k4: (lambda e: e.dma_start(out=wg_t[:], in_=wgv[k4])))(), [], [f"ws{sg_}"] + ex2 + (FENCE if k4 < 2 else []))
          P.dma("gpsimd", f"d_ws{su_}", (lambda wu_t=wu_t, k4=k4: (lambda e: e.dma_start(out=wu_t[:], in_=wuv[k4])))(), [], [f"ws{su_}"] + ex3 + (FENCE if k4 < 2 else []))
        for c in range(8):
            P.dma("gpsimd", f"d_XS{c}", (lambda k4=k4, c=c: (lambda e: e.indirect_dma_start(
                out=XS[:, c, :], out_offset=None, in_=h2all, in_offset=bass.IndirectOffsetOnAxis(ap=GIDX[:, k4, c:c + 1], axis=0))))(),
                H2ALLD + ["GIDX"], [("XS", c)] + (["XS"] if c == 0 else []))

    def issue_wd(k4):
        P.dma("gpsimd", "d_wd", (lambda k4=k4: (lambda e: e.dma_start(out=wdt_[:], in_=wdv[k4])))(), [], ["wd_"] + (FENCE if k4 < 1 else []))

    issue_loads(0)
    for k4 in range(4):
        sg_, su_ = (k4 % 2) * 2, (k4 % 2) * 2 + 1
        wg_t, wu_t = wslot[sg_], wslot[su_]
        for c in range(8):
            pb, pbn = psb()
            pbv = pb[:].rearrange("p (k t) -> p k t", k=8)
            for kc in range(8):
                TR(pbv[:, kc, :], XS[:, c, kc * 128:(kc + 1) * 128], ident_b[:], [("XS", c), "XS", "ident_b"], [(pbn, kc)])
            ACT(xsT[:, :, c * 128:(c + 1) * 128], pbv, AF.Copy, [(pbn, kc) for kc in range(8)], [("xsT", c)] + (FENCE if k4 == 0 else []))
        if k4 < 3:
            issue_loads(k4 + 1)
        for sch in range(2):
            hd = hid[(k4 * 2 + sch) % 2]
            hdn = f"hid{(k4 * 2 + sch) % 2}"
            xdeps = [("xsT", sch * 4 + t) for t in range(4)]
            for fo in range(8):
                pa, pan = psf("a")
                for kc in range(8):
                    MM(pa[:], wg_t[:, kc, fo * 128:(fo + 1) * 128], xsT[:, kc, sch * 512:(sch + 1) * 512], kc == 0, kc == 7,
                       [f"ws{sg_}"] + xdeps, [pan])
                pu, pun = psf("v")
                for kc in range(8):
                    MM(pu[:], wu_t[:, kc, fo * 128:(fo + 1) * 128], xsT[:, kc, sch * 512:(sch + 1) * 512], kc == 0, kc == 7,
                       [f"ws{su_}"] + xdeps, [pun])
                ACT(sgs[:], pa[:], AF.Silu, [pan], ["sgs"] + (FENCE if k4 == 0 and sch == 0 and fo == 0 else []))
                TT("vector", hd[:, fo, :], pu[:], sgs[:], ALU.mult, [pun, "sgs"], [(hdn, fo)] + (FENCE + ["TABm"] + TABC if k4 == 0 else []))
            for t in range(4):
                c = sch * 4 + t
                yi = yrr[0] % 2
                yrr[0] += 1
                for dn in range(2):
                    py, pyn = psf("v")
                    for kc in range(8):
                        MM(py[:], hd[:, kc, t * 128:(t + 1) * 128], wdt_[:, kc, dn * 512:(dn + 1) * 512], kc == 0, kc == 7,
                           [(hdn, kc), "wd_"], [pyn])
                    TS("vector", yz[yi][:, dn * 512:(dn + 1) * 512], py[:], gS[:, k4, c:c + 1], None, ALU.mult, None,
                       [pyn, "gS"], [f"yz{yi}"])
                P.dma("gpsimd", f"d_sz{yi}", (lambda yi=yi, k4=k4, c=c: (lambda e: e.indirect_dma_start(
                    out=Zd, out_offset=bass.IndirectOffsetOnAxis(ap=ZSIDX[:, k4, c:c + 1], axis=0), in_=yz[yi][:], in_offset=None,
                    compute_op=ALU.add, oob_is_err=True)))(), [f"yz{yi}", "ZSIDX", "Zd"] + ZD0, ["Zd"])
        if k4 < 3:
            issue_wd(k4 + 1)

    if stage <= 7:
        P.emit()
        return nc, dbg_outs

    gfrow = P.sb("gfrow", [128, 1024], F32, off=rows_off + 4096)
    DMA("sync", "d_gfrow", gfrow[:], g_final.partition_broadcast(128), [], ["gfrow"] + FENCE)
    z4 = [P.sb(f"z4_{i}", [128, 4, 1024], BF16, off=bgT_off + i * 8192) for i in range(2)]
    evs = []

    def z_allgather(j16):
        P.dma("gpsimd", "d_ag_z", (lambda j16=j16: (lambda e: e.collective_compute(
            "AllGather", ALU.bypass, replica_groups=GROUPS,
            ins=[Zd[j16 * 512:(j16 + 1) * 512, :].opt()], outs=[Zall[j16].opt()])))(),
            ["Zd", "agchain"], [("Zall", j16), "agchain"], inc=1)

    def phaseC_tile(tile):
        i = xt_rr[0] % 2
        xt_rr[0] += 1
        xtile, xn = xt[i], f"xt{i}"
        zi = tile % 2
        DMA("sync", "d_" + xn, xtile[:], x1d[tile * 128:(tile + 1) * 128, :], [("x1d", tile)], [xn])
        for r in range(4):
            P.dma("gpsimd", f"d_z4_{zi}_{r}", (lambda zi=zi, r=r, tile=tile: (lambda e: e.indirect_dma_start(
                out=z4[zi][:, r, :], out_offset=None, in_=Zall[tile], in_offset=bass.IndirectOffsetOnAxis(ap=ZIDX[:, r, 0:1], axis=0))))(),
                [("Zall", tile), "ZIDX"], [(f"z4_{zi}", r)] + ([("XS", c) for c in range(8)] + ["XS"] if tile < 2 else []))
        zd = [(f"z4_{zi}", r) for r in range(4)]
        TT("vector", tf[:], z4[zi][:, 0, :], z4[zi][:, 1, :], ALU.add, zd, ["tf"])
        TT("vector", tf[:], tf[:], z4[zi][:, 2, :], ALU.add, zd + ["tf"], ["tf"])
        TT("vector", tf[:], tf[:], z4[zi][:, 3, :], ALU.add, zd + ["tf"], ["tf"])
        TT("vector", tf[:], tf[:], rows["GT2"][:], ALU.mult, ["tf"] + rowdeps("GT2"), ["tf"])
        TT("vector", xtile[:], xtile[:], tf[:], ALU.add, [xn, "tf"], [xn])
        ss = small[:, 16:17]
        rstd = small[:, 17:18]
        ACT(tf[:], xtile[:], AF.Square, [xn], ["tf"])
        RED("vector", ss, tf[:], ALU.add, ["tf"], ["ss"])
        TS("vector", rstd, ss, 1.0 / 1024.0, 1e-6, ALU.mult, ALU.add, ["ss"], ["rstd"])
        ACT(rstd, rstd, AF.Ln, ["rstd"], ["rstd"])
        ACT(rstd, rstd, AF.Exp, ["rstd"], ["rstd"], scale=-0.5)
        ACT(tf[:], xtile[:], AF.Copy, [xn, "rstd"], ["tf"], scale=rstd)
        TT("vector", xtile[:], tf[:], gfrow[:], ALU.mult, ["tf", "gfrow"], [xn])
        evs.append(DMA("sync", "d_out_" + xn, out[tile * 128:(tile + 1) * 128, :], xtile[:], [xn], [("out", tile)]))

    for j16 in range(16):
        z_allgather(j16)
        if j16 >= 1:
            phaseC_tile(j16 - 1)
    phaseC_tile(15)
    P.finish("sync", evs)
    P.emit()
    return nc, dbg_outs


def make_in_maps(inp):
    x = np.ascontiguousarray(inp["x"], dtype=np.float32)
    maps = []
    for c in range(NCORES):
        b, q = c // 4, c % 4
        t0 = q * 2048
        xw = np.zeros((W, 1024), np.float32)
        lo, hi = t0 - 128, t0 + 2048 + 128
        slo, shi = max(lo, 0), min(hi, 8192)
        xw[slo - lo:shi - lo] = x[b, slo:shi]
        ccv = np.stack([inp["c"][b].reshape(8, 128).T, inp["c_ctx"].reshape(8, 128).T], axis=-1)
        meta = np.zeros((128, 80), np.float32)
        meta[:, 0] = 1.0 if q > 0 else 0.0
        meta[:, 1] = 1.0 if q < 3 else 0.0
        meta[:, 2] = float(q * 32 - 2)
        meta[:, 3] = float(q * 2048)
        meta[:, 4] = float(4 * q * 128)
        meta[:, 5] = float(q * 8192)
        meta[:, 6] = float(q * 128)
        meta[:, 7] = float(4 * q * 32)
        for k in range(4):
            meta[:, 8 + (4 * q + k) * 4 + k] = 1.0
        maps.append({
            "x": xw, "ctx": np.ascontiguousarray(inp["ctx"][b]), "cc": np.ascontiguousarray(ccv.reshape(128, 16)),
            "meta": meta, "w_ada": inp["w_ada"][0], "b_ada": inp["b_ada"][0], "g_mix": inp["g_mix"][0],
            "g_ffn": inp["g_ffn"][0], "g_final": inp["g_final"], "w_in": inp["w_in"][0], "conv_w": inp["conv_w"][0],
            "sink": inp["sink"][0], "w_out": inp["w_out"][0], "w_router": inp["w_router"][0],
            "w_gate": np.ascontiguousarray(inp["w_gate"][0, 4 * q:4 * q + 4]),
            "w_up": np.ascontiguousarray(inp["w_up"][0, 4 * q:4 * q + 4]),
            "w_down": np.ascontiguousarray(inp["w_down"][0, 4 * q:4 * q + 4]),
        })
    return maps


def kernel(**inputs):
    inp = {k: np.asarray(v) for k, v in inputs.items()}
    nc, _ = build_nc()
    res = run_bass_kernel_spmd(nc, make_in_maps(inp), core_ids=list(range(NCORES)))
    outp = np.zeros((2, 8192, 1024), np.float32)
    for c in range(NCORES):
        b, q = c // 4, c % 4
        outp[b, q * 2048:(q + 1) * 2048] = res.results[c]["out"]
    return outp
```

```python
import os
import numpy as np
import concourse.bass as bass
import concourse.mybir as mybir
from concourse.bass_utils import run_bass_kernel_spmd

F32 = mybir.dt.float32
BF16 = mybir.dt.bfloat16
I32 = mybir.dt.int32
ALU = mybir.AluOpType
AF = mybir.ActivationFunctionType
AX = mybir.AxisListType

COMPUTE = ("tensor", "vector", "scalar", "gpsimd")
QUEUES = ("sync",)
NCORES = 8
GROUPS = [[0, 1, 2, 3], [4, 5, 6, 7]]
W = 2304
NT = 16
NIT = 7


class Prog:
    def __init__(self, nc):
        self.nc = nc
        self.streams = {e: [] for e in COMPUTE + QUEUES}
        self.cnt = {e: 0 for e in COMPUTE}
        self.dma_cnt = {}
        self.waited = {}
        self.res = {}
        self.sem_handles = {}
        self.final_events = []
        self.sb_off = 16512
        self.sb_top = 229344

    def sb(self, name, shape, dtype, off=None):
        esz = {F32: 4, BF16: 2, I32: 4}[dtype]
        n = 1
        for s in shape[1:]:
            n *= s
        nbytes = (n * esz + 63) // 64 * 64
        if off is None:
            off = self.sb_off
            self.sb_off += nbytes
        assert off >= 16512 and off + nbytes <= self.sb_top, (name, off, nbytes)
        return self.nc.alloc_sbuf_tensor_at(name, list(shape), dtype, offset=off)

    def _deps(self, reads, writes):
        need = []
        for r in reads:
            st = self.res.get(r)
            if st and st["w"] is not None:
                need.append(st["w"])
        for w in writes:
            st = self.res.get(w)
            if st:
                if st["w"] is not None:
                    need.append(st["w"])
                need.extend(st["r"])
        return need

    def _commit(self, ev, reads, writes):
        for r in reads:
            st = self.res.setdefault(r, {"w": None, "r": []})
            st["r"].append(ev)
        for w in writes:
            self.res[w] = {"w": ev, "r": []}

    def _waits(self, eng, need):
        best = {}
        for (k, v) in need:
            if k == "tensor" and eng == "tensor":
                continue
            if v > best.get(k, 0):
                best[k] = v
        out = []
        for k, v in best.items():
            if self.waited.get((eng, k), 0) >= v:
                continue
            self.waited[(eng, k)] = v
            out.append((k, v))
        return out

    def op(self, eng, fn, reads=(), writes=()):
        need = self._deps(reads, writes)
        waits = self._waits(eng, need)
        self.cnt[eng] += 1
        ev = (eng, self.cnt[eng])
        self.streams[eng].append((waits, fn, (eng, 1)))
        self._commit(ev, reads, writes)
        return ev

    def dma(self, q, sem, fn, reads=(), writes=(), inc=16):
        need = self._deps(reads, writes)
        waits = self._waits(q, need)
        self.dma_cnt[sem] = self.dma_cnt.get(sem, 0) + inc
        ev = (sem, self.dma_cnt[sem])
        self.streams[q].append((waits, fn, (sem, inc)))
        self._commit(ev, reads, writes)
        return ev

    def finish(self, eng, events):
        self.final_events.append((eng, events))

    def check_deadlock(self):
        sem = {}
        pos = {e: 0 for e in self.streams}
        progressed = True
        while progressed:
            progressed = False
            for e, st in self.streams.items():
                while pos[e] < len(st):
                    waits, fn, inc = st[pos[e]]
                    if all(sem.get(k, 0) >= v for (k, v) in waits):
                        sem[inc[0]] = sem.get(inc[0], 0) + inc[1]
                        pos[e] += 1
                        progressed = True
                    else:
                        break
        stuck = {e: (pos[e], len(st), st[pos[e]][0]) for e, st in self.streams.items() if pos[e] < len(st)}
        assert not stuck, ("DEADLOCK", stuck, {k: sem.get(k) for e in stuck for (k, v) in stuck[e][2]})

    def emit(self):
        self.check_deadlock()
        nc = self.nc
        names = set(COMPUTE)
        for e in self.streams:
            for (waits, fn, inc) in self.streams[e]:
                names.add(inc[0])
                for (k, v) in waits:
                    names.add(k)
        for n in sorted(names):
            self.sem_handles[n] = nc.alloc_semaphore("s_" + n)
        H = self.sem_handles
        fin = {}
        for eng, evs in self.final_events:
            fin.setdefault(eng, []).extend(evs)
        with nc.Block() as block:
            def make(ename):
                def body(e):
                    for (waits, fn, inc) in self.streams[ename]:
                        for (k, v) in waits:
                            e.wait_ge(H[k], v)
                        fn(e).then_inc(H[inc[0]], inc[1])
                    best = {}
                    for (k, v) in fin.get(ename, []):
                        best[k] = max(best.get(k, 0), v)
                    for k, v in best.items():
                        e.wait_ge(H[k], v)
                return body
            for ename in self.streams:
                if not self.streams[ename] and ename not in fin:
                    continue
                getattr(block, ename)(make(ename))


def build_nc(stage=99, dbg=()):
    nc = bass.Bass("TRN2", target_bir_lowering=False)
    P = Prog(nc)
    dbg_outs = {}

    def din(name, shape, dt=F32):
        return nc.dram_tensor(name, list(shape), dt, kind="ExternalInput").ap()

    x = din("x", [W, 1024])
    ctx = din("ctx", [256, 1024])
    cc = din("cc", [128, 16])
    meta = din("meta", [128, 80])
    w_ada = din("w_ada", [1024, 6144])
    b_ada = din("b_ada", [6144])
    g_mix = din("g_mix", [1024])
    g_ffn = din("g_ffn", [1024])
    g_final = din("g_final", [1024])
    w_in = din("w_in", [1024, 2304])
    conv_w = din("conv_w", [3, 512])
    sink = din("sink", [8])
    w_out = din("w_out", [1024, 1024])
    w_router = din("w_router", [1024, 16])
    w_gate = din("w_gate", [4, 1024, 1024])
    w_up = din("w_up", [4, 1024, 1024])
    w_down = din("w_down", [4, 1024, 1024])
    out = nc.dram_tensor("out", [2048, 1024], F32, kind="ExternalOutput").ap()

    x1d = nc.dram_tensor("x1d", [2048, 1024], F32).ap()
    h2loc = nc.dram_tensor("h2loc", [2048, 1024], BF16).ap()
    h2all = nc.dram_tensor("h2all", [8192, 1024], BF16).ap()
    affloc = nc.dram_tensor("affloc", [16, 2048], F32).ap()
    affall = nc.dram_tensor("affall", [64, 2048], F32).ap()
    tabd = nc.dram_tensor("tabd", [2048, 128], F32).ap()
    Zd = nc.dram_tensor("Zd", [8192, 1024], BF16).ap()
    Zall = nc.dram_tensor("Zall", [32768, 1024], BF16).ap()

    def dbg_out(name, shape, dt=F32):
        t = nc.dram_tensor("dbg_" + name, list(shape), dt, kind="ExternalOutput").ap()
        dbg_outs[name] = t
        return t

    def ACT(out_, in_, func, r, w, **kw):
        return P.op("scalar", lambda e: e.activation(out=out_, in_=in_, func=func, **kw), r, w)

    def TT(eng, out_, in0, in1, op, r, w):
        return P.op(eng, lambda e: e.tensor_tensor(out=out_, in0=in0, in1=in1, op=op), r, w)

    def TS(eng, out_, in0, s1, s2, op0, op1, r, w):
        if op1 is None:
            return P.op(eng, lambda e: e.tensor_scalar(out=out_, in0=in0, scalar1=s1, scalar2=None, op0=op0), r, w)
        return P.op(eng, lambda e: e.tensor_scalar(out=out_, in0=in0, scalar1=s1, scalar2=s2, op0=op0, op1=op1), r, w)

    def STT(eng, out_, in0, scalar, in1, op0, op1, r, w):
        return P.op(eng, lambda e: e.scalar_tensor_tensor(out=out_, in0=in0, scalar=scalar, in1=in1, op0=op0, op1=op1), r, w)

    def RED(eng, out_, in_, op, r, w):
        return P.op(eng, lambda e: e.tensor_reduce(out=out_, in_=in_, axis=AX.X, op=op), r, w)

    def CP(eng, out_, in_, r, w):
        return P.op(eng, lambda e: e.tensor_copy(out=out_, in_=in_), r, w)

    def MSET(eng, out_, val, w):
        return P.op(eng, lambda e: e.memset(out_, val), (), w)

    def MM(out_, lhsT, rhs, start, stop, r, w):
        return P.op("tensor", lambda e: e.matmul(out_, lhsT, rhs, start=start, stop=stop), r, w)

    def TR(out_, in_, ident, r, w):
        return P.op("tensor", lambda e: e.transpose(out_, in_, ident), r, w)

    def DMA(q, sem, out_, in_, r, w):
        return P.dma(q, sem, lambda e: e.dma_start(out=out_, in_=in_), r, w)

    PSF = [nc.alloc_psum_tensor(f"psf{i}", [128, 512], F32) for i in range(6)]
    PSB = [nc.alloc_psum_tensor(f"psb{i}", [128, 1024], BF16) for i in range(2)]
    psf_rr = {"v": 0, "a": 0}

    def psf(cons):
        i = psf_rr[cons] % 3 + (0 if cons == "v" else 3)
        psf_rr[cons] += 1
        return PSF[i], f"psf{i}"

    psb_rr = [0]

    def psb():
        i = psb_rr[0] % 2
        psb_rr[0] += 1
        return PSB[i], f"psb{i}"

    ident_f = P.sb("ident_f", [128, 128], F32)
    ident_b = P.sb("ident_b", [128, 128], BF16)
    iot = P.sb("iot", [128, 128], F32)
    ones_b = P.sb("ones_b", [128, 128], BF16)
    U_b = P.sb("U_b", [128, 128], BF16)
    UI_b = P.sb("UI_b", [128, 128], BF16)
    mask3 = P.sb("mask3", [128, 3, 384], BF16)
    metat = P.sb("metat", [128, 80], F32)
    esink = P.sb("esink", [128, 8], F32)
    rows = {}
    for nm in ("S1", "G1", "GT1", "S2", "G2", "GT2", "cS1", "cG1"):
        rows[nm] = P.sb("row_" + nm, [128, 1024], F32)
    REG0 = P.sb_off

    P.op("gpsimd", lambda e: e.iota(iot[:], pattern=[[1, 128]], base=0, channel_multiplier=-1,
                                    allow_small_or_imprecise_dtypes=True), (), ["iot"])
    TS("vector", ident_f[:], iot[:], 0.0, None, ALU.is_equal, None, ["iot"], ["ident_f"])
    CP("vector", ident_b[:], ident_f[:], ["ident_f"], ["ident_b"])
    TS("vector", U_b[:], iot[:], 0.0, None, ALU.is_ge, None, ["iot"], ["U_b"])
    MSET("vector", ones_b[:], 1.0, ["ones_b"])
    DMA("sync", "d_meta", metat[:], meta, [], ["metat"])
    DMA("sync", "d_sink", esink[:], sink.partition_broadcast(128), [], ["esink"])
    ACT(esink[:], esink[:], AF.Exp, ["esink"], ["esink"])
    for v in range(3):
        TS("vector", mask3[:, v, 0:128], iot[:], 0.0, None, ALU.is_le, None, ["iot"], [("mask3", v)])
        MSET("vector", mask3[:, v, 128:256], 1.0, [("mask3", v, 1)])
        TS("vector", mask3[:, v, 256:384], iot[:], 0.0, None, ALU.is_ge, None, ["iot"], [("mask3", v, 2)])
    TS("vector", mask3[:, 1, 0:128], mask3[:, 1, 0:128], metat[:, 0:1], None, ALU.mult, None,
       ["metat", ("mask3", 1)], [("mask3", 1)])
    TS("vector", mask3[:, 2, 256:384], mask3[:, 2, 256:384], metat[:, 1:2], None, ALU.mult, None,
       ["metat", ("mask3", 2, 2)], [("mask3", 2, 2)])

    if "const" in dbg:
        d1 = dbg_out("ident", [128, 128])
        d2 = dbg_out("mask3", [128, 3 * 384], BF16)
        d3 = dbg_out("esink", [128, 8])
        e1 = DMA("sync", "d_dbg", d1, ident_f[:], ["ident_f"], ["dbg1"])
        e2 = DMA("sync", "d_dbg", d2, mask3[:].rearrange("p a b -> p (a b)"),
                 [("mask3", v) for v in range(3)] + [("mask3", v, 1) for v in range(3)] + [("mask3", v, 2) for v in range(3)], ["dbg2"])
        e3 = DMA("sync", "d_dbg", d3, esink[:], ["esink"], ["dbg3"])
        P.finish("sync", [e1, e2, e3])
    if stage <= 0:
        P.emit()
        return nc, dbg_outs

    o = REG0
    WIN = P.sb("WIN", [128, 8, 2944], BF16, off=o)
    mixT = P.sb("mixT", [128, 8, 2048], BF16, off=o)
    o += 47104
    COS = P.sb("COS", [128, W], F32, off=o); o += W * 4
    SINS = P.sb("SINS", [128, W], F32, off=o); o += W * 4
    xt = [P.sb(f"xt{i}", [128, 1024], F32, off=o + i * 4096) for i in range(2)]; o += 8192
    tf = P.sb("tf", [128, 1024], F32, off=o); o += 4096
    hb = [P.sb(f"hb{i}", [128, 1024], BF16, off=o + i * 2048) for i in range(2)]; o += 4096
    hT = [P.sb(f"hT{i}", [128, 8, 512], BF16, off=o + i * 8192) for i in range(2)]
    wo = P.sb("wo", [128, 8, 1024], BF16, off=o)
    o += 16384
    qT_off = o
    qT = P.sb("qT", [128, 4, W], BF16, off=o); o += 4 * W * 2
    kT_off = o
    kT = P.sb("kT", [128, W], BF16, off=o); o += W * 2
    Vt = P.sb("Vt", [128, 18, 2, 65], BF16, off=o); o += 4736
    kcT = P.sb("kcT", [128, 256], BF16, off=o); o += 512
    Vc = P.sb("Vc", [128, 2, 2, 65], BF16, off=o); o += 576
    bgT_off = o
    bgT = P.sb("bgT", [128, 4, 2048], BF16, off=o)
    stg = P.sb("stg", [128, 8, 640], F32, off=o)
    o += 20480
    uT_off = o
    uT = P.sb("uT", [128, 4, W], BF16, off=o); o += 4 * W * 2
    rt1_off = o
    rt1 = P.sb("rt1", [128, 512], F32, off=o); o += 2048
    rt2 = P.sb("rt2", [128, 512], F32, off=o); o += 2048
    cgs = P.sb("cgs", [128, 512], F32, off=o); o += 2048
    small = P.sb("small", [128, 64], F32, off=o); o += 256
    cw = P.sb("cw", [128, 4, 3], F32, off=o); o += 64
    assert o <= P.sb_top, o
    A_END = o

    wa = [P.sb("wa0", [128, 8, 1024], BF16, off=qT_off), P.sb("wa1", [128, 8, 1024], BF16, off=uT_off)]
    o = kT_off
    lb = P.sb("lb", [128, 8, 2, 128], BF16, off=o); o += 4096
    brow = P.sb("brow", [128, 1024], F32, off=o); o += 4096
    cct = P.sb("cct", [128, 8, 2], F32, off=o); o += 64
    scl = P.sb("scl", [128, 8, 2], F32, off=o); o += 64
    assert o <= bgT_off
    gmrow = P.sb("gmrow", [128, 1024], F32, off=rt1_off)

    DMA("sync", "d_cc", cct[:], cc.rearrange("p (k v) -> p k v", v=2), [], ["cct"])
    ACT(scl[:], cct[:], AF.Silu, ["cct"], ["scl"])
    for v in range(2):
        CP("vector", lb[:, :, v, :], scl[:, :, v:v + 1].to_broadcast([128, 8, 128]), ["scl"], [("lb", v)])
    if stage <= 0.3:
        d1 = dbg_out("lb", [128, 8 * 2 * 128], BF16)
        e1 = DMA("sync", "d_dbg", d1, lb[:].rearrange("p a b c -> p (a b c)"), [("lb", 0), ("lb", 1)], ["dbg1"])
        P.finish("sync", [e1])
        P.emit()
        return nc, dbg_outs
    w_ada_v = w_ada.rearrange("(k p) n -> p k n", p=128)
    grp = [(0, [("S1", 0), ("cS1", 1)]), (1, [("G1", 0), ("cG1", 1)]), (2, [("GT1", 0)]),
           (3, [("S2", 0)]), (4, [("G2", 0)]), (5, [("GT2", 0)])]
    for gi, (g, uses) in enumerate(grp):
        wb = wa[gi % 2]
        wn = f"wa{gi % 2}"
        P.dma("gpsimd", "d_" + wn, (lambda wb=wb, g=g: (lambda e: e.dma_start(out=wb[:], in_=w_ada_v[:, :, g * 1024:(g + 1) * 1024])))(),
              [], [wn])
        DMA("sync", "d_brow", brow[:], b_ada[g * 1024:(g + 1) * 1024].partition_broadcast(128), [], ["brow"])
        if stage <= 0.5:
            d1 = dbg_out("wa", [128, 8 * 1024], BF16)
            d2 = dbg_out("brow", [128, 1024])
            e1 = DMA("sync", "d_dbg", d1, wb[:].rearrange("p a b -> p (a b)"), [wn], ["dbg1"])
            e2 = DMA("sync", "d_dbg", d2, brow[:], ["brow"], ["dbg2"])
            P.finish("sync", [e1, e2])
            P.emit()
            return nc, dbg_outs
        for (nm, v) in uses:
            for n in range(2):
                ps, psn = psf("v")
                for k in range(8):
                    MM(ps[:], lb[:, k, v, :], wb[:, k, n * 512:(n + 1) * 512], k == 0, k == 7,
                       [("lb", v), wn], [psn])
                TT("vector", rows[nm][:, n * 512:(n + 1) * 512], ps[:], brow[:, n * 512:(n + 1) * 512], ALU.add,
                   [psn, "brow"], [("row", nm, n)])
                if stage <= 0.7:
                    d1 = dbg_out("r0", [128, 512])
                    e1 = DMA("sync", "d_dbg", d1, rows[nm][:, 0:512], [("row", nm, n)], ["dbg1"])
                    P.finish("sync", [e1])
                    P.emit()
                    return nc, dbg_outs
    for (gsrc, names) in (((g_mix, ("G1", "cG1")), (g_ffn, ("G2",))) if stage > 0.8 else ()):
        DMA("sync", "d_gmrow", gmrow[:], gsrc.partition_broadcast(128), [], ["gmrow"])
        for nm in names:
            TS("vector", rows[nm][:], rows[nm][:], 1.0, None, ALU.add, None,
               [("row", nm, 0), ("row", nm, 1)], [("row", nm, 0), ("row", nm, 1)])
            TT("vector", rows[nm][:], rows[nm][:], gmrow[:], ALU.mult,
               [("row", nm, 0), ("row", nm, 1), "gmrow"], [("row", nm, 0), ("row", nm, 1)])

    def rowdeps(nm):
        return [("row", nm, 0), ("row", nm, 1)]

    if "rows" in dbg:
        d = dbg_out("rows", [8, 128, 1024])
        for i, nm in enumerate(("S1", "G1", "GT1", "S2", "G2", "GT2", "cS1", "cG1")):
            ev = DMA("sync", "d_dbg", d[i], rows[nm][:], rowdeps(nm), ["dbg"])
        P.finish("sync", [ev])
    if stage <= 1:
        P.emit()
        return nc, dbg_outs


    def sc(i):
        return small[:, i:i + 1]
    pid, dd, i32_, isC, ff, inv, invC, invR, sgn, tmpc = [sc(i) for i in range(10)]
    P.op("gpsimd", lambda e: e.iota(small[:, 0:1], pattern=[[0, 1]], base=0, channel_multiplier=1,
                                    allow_small_or_imprecise_dtypes=True), (), ["small"])
    TS("vector", tmpc, pid, 64.0, -64.0, ALU.is_ge, ALU.mult, ["small"], ["small"])
    TT("vector", dd, pid, tmpc, ALU.add, ["small"], ["small"])
    TS("vector", sgn, dd, 32.0, None, ALU.is_ge, None, ["small"], ["small"])
    TS("vector", tmpc, sgn, -32.0, None, ALU.mult, None, ["small"], ["small"])
    TT("vector", i32_, dd, tmpc, ALU.add, ["small"], ["small"])
    TS("vector", isC, i32_, 16.0, None, ALU.is_ge, None, ["small"], ["small"])
    TS("vector", tmpc, isC, -16.0, None, ALU.mult, None, ["small"], ["small"])
    TT("vector", ff, i32_, tmpc, ALU.add, ["small"], ["small"])
    ACT(inv, ff, AF.Exp, ["small"], ["small"], scale=-float(np.log(10000.0) / 16.0))
    TT("vector", invC, inv, isC, ALU.mult, ["small"], ["small"])
    TT("vector", invR, inv, invC, ALU.subtract, ["small"], ["small"])
    TS("vector", sgn, sgn, 2.0, -1.0, ALU.mult, ALU.add, ["small"], ["small"])
    rrA = P.sb("rrA", [128, W], F32, off=qT_off)
    rrI = P.sb("rrI", [128, W], I32, off=qT_off + W * 4)
    ang = P.sb("ang", [128, W], F32, off=uT_off)
    P.op("gpsimd", lambda e: e.iota(COS[:], pattern=[[1, 36], [0, 64]], base=0, channel_multiplier=0,
                                    allow_small_or_imprecise_dtypes=True), (), ["COS"])
    P.op("gpsimd", lambda e: e.iota(SINS[:], pattern=[[0, 36], [1, 64]], base=0, channel_multiplier=0,
                                    allow_small_or_imprecise_dtypes=True), (), ["SINS"])
    HW_ = W // 2
    TWO_PI = float(2 * np.pi)
    for hh in range(2):
        sl = slice(hh * HW_, (hh + 1) * HW_)
        TS("vector", COS[:, sl], COS[:, sl], metat[:, 2:3], None, ALU.add, None, ["COS", "metat"], ["COS"])
        TS("vector", COS[:, sl], COS[:, sl], invR, None, ALU.mult, None, ["COS", "small"], ["COS"])
        TS("vector", SINS[:, sl], SINS[:, sl], invC, None, ALU.mult, None, ["SINS", "small"], ["SINS"])
    TT("vector", ang[:], COS[:], SINS[:], ALU.add, ["COS", "SINS"], ["ang"])

    def range_reduce_sin(dst, dstn, offset):
        TS("vector", rrA[:], ang[:], 1.0 / TWO_PI, offset / TWO_PI + 8.5, ALU.mult, ALU.add, ["ang"], ["rrA"])
        CP("vector", rrI[:], rrA[:], ["rrA"], ["rrI"])
        CP("vector", rrA[:], rrI[:], ["rrI"], ["rrA"])
        TS("vector", rrA[:], rrA[:], -TWO_PI, 8 * TWO_PI + offset, ALU.mult, ALU.add, ["rrA"], ["rrA"])
        TT("vector", dst[:], ang[:], rrA[:], ALU.add, ["ang", "rrA"], [dstn])
        TS("vector", rrA[:], dst[:], float(np.pi), -TWO_PI, ALU.is_gt, ALU.mult, [dstn], ["rrA"])
        TT("vector", dst[:], dst[:], rrA[:], ALU.add, [dstn, "rrA"], [dstn])
        TS("vector", rrA[:], dst[:], -float(np.pi), TWO_PI, ALU.is_lt, ALU.mult, [dstn], ["rrA"])
        TT("vector", dst[:], dst[:], rrA[:], ALU.add, [dstn, "rrA"], [dstn])
        ACT(dst[:], dst[:], AF.Sin, [dstn], [dstn])

    range_reduce_sin(SINS, "SINS", 0.0)
    range_reduce_sin(COS, "COS", float(np.pi / 2))
    for hh in range(2):
        sl = slice(hh * HW_, (hh + 1) * HW_)
        TS("vector", SINS[:, sl], SINS[:, sl], sgn, None, ALU.mult, None, ["SINS", "small"], ["SINS"])

    w_in_v = w_in.rearrange("(k p) n -> p k n", p=128)
    DMA("sync", "d_stg", stg[:], w_in_v[:, :, 0:640], [], ["stg"])
    qd = WIN[:, :, 0:512].rearrange("p k (c h d) -> p k c h d", c=4, h=2, d=64)
    qs = stg[:, :, 0:512].rearrange("p k (h c d) -> p k c h d", h=2, c=4, d=64)
    for h in range(2):
        ACT(qd[:, :, :, h, :], qs[:, :, :, h, :], AF.Copy, ["stg"], [("WIN", "q", h)])
    qd2 = WIN[:, :, 512:1024].rearrange("p k (c h s d) -> p k c h s d", c=4, h=2, s=2, d=32)
    qs2 = stg[:, :, 0:512].rearrange("p k (h c s d) -> p k c h s d", h=2, c=4, s=2, d=32)
    for h in range(2):
        for s in range(2):
            ACT(qd2[:, :, :, h, s, :], qs2[:, :, :, h, 1 - s, :], AF.Copy, ["stg"], [("WIN", "qsw", h, s)])
    ACT(WIN[:, :, 1024:1152], stg[:, :, 512:640], AF.Copy, ["stg"], [("WIN", "k")])
    kd2 = WIN[:, :, 1152:1280].rearrange("p k (h s d) -> p k h s d", h=2, s=2, d=32)
    ks2 = stg[:, :, 512:640].rearrange("p k (h s d) -> p k h s d", h=2, s=2, d=32)
    for s in range(2):
        ACT(kd2[:, :, :, s, :], ks2[:, :, :, 1 - s, :], AF.Copy, ["stg"], [("WIN", "ksw", s)])
    WINQ = [("WIN", "q", 0), ("WIN", "q", 1)]
    WINQS = [("WIN", "qsw", h, s) for h in range(2) for s in range(2)]
    WINK = [("WIN", "k")]
    WINKS = [("WIN", "ksw", 0), ("WIN", "ksw", 1)]
    for (nm, d0, s0, n) in (("v", 1280, 640, 128), ("bg", 1408, 768, 512), ("cg", 1920, 1280, 512), ("hv", 2432, 1792, 512)):
        P.dma("gpsimd", "d_win_" + nm, (lambda d0=d0, s0=s0, n=n: (lambda e: e.dma_start(out=WIN[:, :, d0:d0 + n], in_=w_in_v[:, :, s0:s0 + n])))(),
              [], [("WIN", nm)])
    for kk in range(3):
        for c4 in range(4):
            P.dma("sync", "d_cw", (lambda kk=kk, c4=c4: (lambda e: e.dma_start(
                out=cw[:, c4, kk:kk + 1], in_=conv_w[kk, c4 * 128:(c4 + 1) * 128].rearrange("(p o) -> p o", o=1))))(),
                [], [("cw", kk, c4)])
    MSET("vector", Vt[:, :, :, 64:65], 1.0, [("Vt", "ones")])
    MSET("vector", Vc[:, :, :, 64:65], 1.0, [("Vc", "ones")])

    xt_rr = [0]

    def norm_mod(src_rows, Gn, Sn, hbuf, hname, extra_r=()):
        i = xt_rr[0] % 2
        xt_rr[0] += 1
        xtile, xn = xt[i], f"xt{i}"
        DMA("sync", "d_" + xn, xtile[:], src_rows, list(extra_r), [xn])
        norm_mod_sb(xtile, xn, Gn, Sn, hbuf, hname)
        return xtile, xn

    tf2 = P.sb("tf2", [128, 1024], F32, off=bgT_off + 16384)
    nm_rr = [0]

    def norm_mod_sb(xtile, xn, Gn, Sn, hbuf, hname):
        pi = nm_rr[0] % 2
        nm_rr[0] += 1
        tfx, tfn = (tf, "tf") if pi == 0 else (tf2, "tf2")
        ss = small[:, 16 + 2 * pi:17 + 2 * pi]
        rstd = small[:, 17 + 2 * pi:18 + 2 * pi]
        ssn, rsn = f"ss{pi}", f"rstd{pi}"
        ACT(tfx[:], xtile[:], AF.Square, [xn], [tfn])
        RED("vector", ss, tfx[:], ALU.add, [tfn], [ssn])
        TS("vector", rstd, ss, 1.0 / 1024.0, 1e-6, ALU.mult, ALU.add, [ssn], [rsn])
        ACT(rstd, rstd, AF.Ln, [rsn], [rsn])
        ACT(rstd, rstd, AF.Exp, [rsn], [rsn], scale=-0.5)
        ACT(tfx[:], xtile[:], AF.Copy, [xn, rsn], [tfn], scale=rstd)
        TT("vector", tfx[:], tfx[:], rows[Gn][:], ALU.mult, [tfn] + rowdeps(Gn), [tfn])
        TT("vector", hbuf[:], tfx[:], rows[Sn][:], ALU.add, [tfn] + rowdeps(Sn), [hname])

    def transpose_to(hbuf, hname, dst, dst_name):
        pb, pbn = psb()
        pbv = pb[:].rearrange("p (k t) -> p k t", k=8)
        for k in range(8):
            TR(pbv[:, k, :], hbuf[:, k * 128:(k + 1) * 128], ident_b[:], [hname, "ident_b"], [(pbn, k)])
        ACT(dst, pbv, AF.Copy, [(pbn, k) for k in range(8)], [dst_name])

    hcT = hT[0]
    for t in range(2):
        norm_mod(ctx[t * 128:(t + 1) * 128, :], "cG1", "cS1", hb[t % 2], f"hb{t % 2}")
        transpose_to(hb[t % 2], f"hb{t % 2}", hcT[:, :, t * 128:(t + 1) * 128], ("hT0", t))
    ps, psn = psf("a")
    for k in range(8):
        MM(ps[:, 0:256], WIN[:, k, 1024:1152], hcT[:, k, 0:256], k == 0, k == 7,
           WINK + [("hT0", 0), ("hT0", 1)], [psn])
    ACT(kcT[:], ps[:, 0:256], AF.Copy, [psn], ["kcT"])
    for t in range(2):
        ps, psn = psf("a")
        for k in range(8):
            MM(ps[:, 0:128], hcT[:, k, t * 128:(t + 1) * 128], WIN[:, k, 1280:1408], k == 0, k == 7,
               [("WIN", "v"), ("hT0", t)], [psn])
        ACT(Vc[:, t, :, 0:64], ps[:, 0:128].rearrange("p (h d) -> p h d", h=2), AF.Copy, [psn], [("Vc", t)])

    if "ctx" in dbg:
        d1 = dbg_out("kcT", [128, 256], BF16)
        d2 = dbg_out("Vc", [128, 2 * 2 * 65], BF16)
        e1 = DMA("sync", "d_dbg", d1, kcT[:], ["kcT"], ["dbg1"])
        e2 = DMA("sync", "d_dbg", d2, Vc[:].rearrange("p a b c -> p (a b c)"), [("Vc", 0), ("Vc", 1), ("Vc", "ones")], ["dbg2"])
        P.finish("sync", [e1, e2])
    if stage <= 2:
        P.emit()
        return nc, dbg_outs

    chunks = [(0, 128, False)] + [(128 + 512 * i, 512, True) for i in range(4)] + [(2176, 128, False)]

    def prep_norm(ci, tiles):
        w0, n, central = chunks[ci]
        for t in tiles:
            norm_mod(x[w0 + t * 128:w0 + (t + 1) * 128, :], "G1", "S1", hb[t % 2], f"hb{t % 2}")

    def prep_trans(ci, tiles):
        hTc, hTn = hT[ci % 2], f"hT{ci % 2}"
        for t in tiles:
            transpose_to(hb[t % 2], f"hb{t % 2}", hTc[:, :, t * 128:(t + 1) * 128], (hTn, t))

    def build_items(ci):
        w0, n, central = chunks[ci]
        hTc, hTn = hT[ci % 2], f"hT{ci % 2}"
        ntile = n // 128
        hdeps = [(hTn, t) for t in range(ntile)]
        items = []

        def proj(col0, wdeps, cons):
            ps, psn = psf(cons)
            for k in range(8):
                MM(ps[:, 0:n], WIN[:, k, col0:col0 + 128], hTc[:, k, 0:n], k == 0, k == 7, wdeps + hdeps, [psn])
            return ps, psn

        def rope_out(col0, colsw, wd, wsd, dst, dstn):
            def f():
                pa, pan = proj(col0, wd, "v")
                pb_, pbn_ = proj(colsw, wsd, "v")
                TT("vector", rt1[:, 0:n], pa[:, 0:n], COS[:, w0:w0 + n], ALU.mult, [pan, "COS"], ["rt1"])
                TT("vector", rt2[:, 0:n], pb_[:, 0:n], SINS[:, w0:w0 + n], ALU.mult, [pbn_, "SINS"], ["rt2"])
                TT("vector", dst, rt1[:, 0:n], rt2[:, 0:n], ALU.add, ["rt1", "rt2"], [dstn])
            return f

        def v_item(t):
            def f():
                ps, psn = psf("a")
                for k in range(8):
                    MM(ps[:, 0:128], hTc[:, k, t * 128:(t + 1) * 128], WIN[:, k, 1280:1408], k == 0, k == 7,
                       [("WIN", "v"), (hTn, t)], [psn])
                wt = w0 // 128 + t
                ACT(Vt[:, wt, :, 0:64], ps[:, 0:128].rearrange("p (h d) -> p h d", h=2), AF.Copy, [psn], [("Vt", wt)])
            return f

        def bg_item(c):
            def f():
                ps, psn = proj(1408 + c * 128, [("WIN", "bg")], "a")
                ACT(bgT[:, c, w0 - 128:w0 - 128 + n], ps[:, 0:n], AF.Copy, [psn], [("bgT", c, ci)])
            return f

        def u_item(c):
            def f():
                pc, pcn = proj(1920 + c * 128, [("WIN", "cg")], "a")
                ph, phn = proj(2432 + c * 128, [("WIN", "hv")], "v")
                ACT(cgs[:, 0:n], pc[:, 0:n], AF.Copy, [pcn], ["cgs"])
                TT("vector", uT[:, c, w0:w0 + n], ph[:, 0:n], cgs[:, 0:n], ALU.mult, [phn, "cgs"], [("uT", c, ci)])
            return f

        if central:
            for c in range(4):
                items.append(rope_out(c * 128, 512 + c * 128, WINQ, WINQS, qT[:, c, w0:w0 + n], ("qT", c, ci)))
        items.append(rope_out(1024, 1152, WINK, WINKS, kT[:, w0:w0 + n], ("kT", ci)))
        for t in range(ntile):
            items.append(v_item(t))
        for c in range(4):
            if central:
                items.append(bg_item(c))
            items.append(u_item(c))
        return items

    def tiles_of(ci):
        return list(range(chunks[ci][1] // 128))

    prep_norm(0, tiles_of(0))
    prep_trans(0, tiles_of(0))
    for ci in range(len(chunks)):
        items = build_items(ci)
        nxt = ci + 1 if ci + 1 < len(chunks) else None
        half = (len(items) + 1) // 2
        if nxt is not None:
            prep_norm(nxt, tiles_of(nxt)[0:2])
        for f in items[:half]:
            f()
        if nxt is not None:
            prep_trans(nxt, tiles_of(nxt)[0:2])
            prep_norm(nxt, tiles_of(nxt)[2:4])
        for f in items[half:]:
            f()
        if nxt is not None:
            prep_trans(nxt, tiles_of(nxt)[2:4])

    if "proj" in dbg:
        d1 = dbg_out("qT", [128, 4 * W], BF16)
        d2 = dbg_out("kT", [128, W], BF16)
        d3 = dbg_out("Vt", [128, 18 * 130], BF16)
        d4 = dbg_out("uT", [128, 4 * W], BF16)
        d5 = dbg_out("bgT", [128, 4 * 2048], BF16)
        allq = [("qT", c, ci) for c in range(4) for ci in range(1, 5)]
        allk = [("kT", ci) for ci in range(6)]
        allv = [("Vt", t) for t in range(18)] + [("Vt", "ones")]
        allu = [("uT", c, ci) for c in range(4) for ci in range(6)]
        allb = [("bgT", c, ci) for c in range(4) for ci in range(1, 5)]
        evs = [DMA("sync", "d_dbg", d1, qT[:].rearrange("p a b -> p (a b)"), allq, ["dbg1"]),
               DMA("sync", "d_dbg", d2, kT[:], allk, ["dbg2"]),
               DMA("sync", "d_dbg", d3, Vt[:].rearrange("p a b c -> p (a b c)"), allv, ["dbg3"]),
               DMA("sync", "d_dbg", d4, uT[:].rearrange("p a b -> p (a b)"), allu, ["dbg4"]),
               DMA("sync", "d_dbg", d5, bgT[:].rearrange("p a b -> p (a b)"), allb, ["dbg5"])]
        P.finish("sync", evs)
    if stage <= 3:
        P.emit()
        return nc, dbg_outs

    ALLWIN = WINQ + WINQS + WINK + WINKS + [("WIN", nm) for nm in ("v", "bg", "cg", "hv")]
    o2 = REG0 + 32768
    PL = [P.sb(f"PL{i}", [128, 384], BF16, off=o2 + i * 768) for i in range(2)]; o2 += 1536
    PC = [P.sb(f"PC{i}", [128, 256], BF16, off=o2 + i * 512) for i in range(2)]; o2 += 1024
    att_tm = P.sb("att_tm", [128, 512], BF16, off=o2); o2 += 1024
    rec = P.sb("rec", [128, 8], F32, off=o2); o2 += 64
    cvt = [P.sb(f"cvt{i}", [128, 512], F32, off=o2 + i * 2048) for i in range(2)]; o2 += 4096
    assert o2 <= REG0 + 47104

    def kchunk(wb):
        return 0 if wb == 0 else (5 if wb == 17 else 1 + (wb - 1) // 4)

    VONES = [("Vt", "ones")]
    TS("vector", uT[:, :, 127:128], uT[:, :, 127:128], metat[:, 0:1], None, ALU.mult, None,
       [("uT", c, 0) for c in range(4)] + ["metat"], [("uT", c, 0) for c in range(4)])
    TS("vector", uT[:, :, 2176:2177], uT[:, :, 2176:2177], metat[:, 1:2], None, ALU.mult, None,
       [("uT", c, 5) for c in range(4)] + ["metat"], [("uT", c, 5) for c in range(4)])

    def conv_unit(tcn, c):
        w0 = 128 + tcn * 512
        ud = [("uT", c, ci) for ci in (tcn, tcn + 1, tcn + 2)]
        cwd = [("cw", kk, c4) for kk in range(3) for c4 in range(4)]
        TS("vector", cvt[0][:], uT[:, c, w0 - 1:w0 + 511], cw[:, c, 0:1], None, ALU.mult, None, ud + cwd, ["cvt0"] + ALLWIN)
        TS("vector", cvt[1][:], uT[:, c, w0:w0 + 512], cw[:, c, 1:2], None, ALU.mult, None, ud + cwd, ["cvt1"] + ALLWIN)
        TT("vector", cvt[0][:], cvt[0][:], cvt[1][:], ALU.add, ["cvt0", "cvt1"], ["cvt0"])
        TS("vector", cvt[1][:], uT[:, c, w0 + 1:w0 + 513], cw[:, c, 2:3], None, ALU.mult, None, ud + cwd, ["cvt1"])
        TT("vector", cvt[0][:], cvt[0][:], cvt[1][:], ALU.add, ["cvt0", "cvt1"], ["cvt0"])
        TT("vector", mixT[:, 4 + c, tcn * 512:(tcn + 1) * 512], cvt[0][:], bgT[:, c, tcn * 512:(tcn + 1) * 512], ALU.mult,
           ["cvt0", ("bgT", c, tcn + 1)], [("mixT", "conv", c, tcn)] + ALLWIN)

    sbanks = [3, 4, 5, 2]
    srr = [0]

    def sbank():
        bi = sbanks[srr[0] % 4]
        srr[0] += 1
        return PSF[bi], f"psf{bi}"

    for i in range(1, 17):
        ci_q = 1 + (i - 1) // 4
        mv = 1 if i == 1 else (2 if i == 16 else 0)
        pvs = [(PSF[0], "psf0"), (PSF[1], "psf1")]

        def S_(hn, i=i, ci_q=ci_q):
            half, c = hn // 4, hn % 4
            r0 = half * 64
            sl, sln = sbank()
            sc_, scn = sbank()
            qsl = qT[r0:r0 + 64, c, i * 128:(i + 1) * 128]
            for kb in range(3):
                wb = i - 1 + kb
                MM(sl[:, kb * 128:(kb + 1) * 128], kT[r0:r0 + 64, wb * 128:(wb + 1) * 128], qsl, True, True,
                   [("qT", c, ci_q), ("kT", kchunk(wb))], [sln])
            for cb in range(2):
                MM(sc_[:, cb * 128:(cb + 1) * 128], kcT[r0:r0 + 64, cb * 128:(cb + 1) * 128], qsl, True, True,
                   [("qT", c, ci_q), "kcT"], [scn])
            return sl, sln, sc_, scn

        def EPV_(hn, st, i=i, mv=mv, pvs=pvs):
            sl, sln, sc_, scn = st
            half, c = hn // 4, hn % 4
            j = hn % 2
            ACT(PL[j][:], sl[:, 0:384], AF.Exp, [sln], [f"PL{j}"] + ALLWIN, scale=0.125)
            ACT(PC[j][:], sc_[:, 0:256], AF.Exp, [scn], [f"PC{j}"] + ALLWIN, scale=0.125)
            TT("vector", PL[j][:], PL[j][:], mask3[:, mv, :], ALU.mult,
               [f"PL{j}", ("mask3", mv), ("mask3", mv, 1), ("mask3", mv, 2)], [f"PL{j}"])
            pv, pvn = pvs[half]
            pvr = pv[:, c * 65:(c + 1) * 65]
            for kb in range(3):
                wb = i - 1 + kb
                MM(pvr, PL[j][:, kb * 128:(kb + 1) * 128], Vt[:, wb, half, :], kb == 0, False,
                   [f"PL{j}", ("Vt", wb)] + VONES, [pvn])
            for cb in range(2):
                MM(pvr, PC[j][:, cb * 128:(cb + 1) * 128], Vc[:, cb, half, :], False, cb == 1,
                   [f"PC{j}", ("Vc", cb), ("Vc", "ones")], [pvn])

        st = S_(0)
        for hn in range(8):
            nxt = S_(hn + 1) if hn < 7 else None
            EPV_(hn, st)
            st = nxt
        for b in range(2):
            pv, pvn = pvs[b]
            pvv = pv[:, 0:260].rearrange("p (h e) -> p h e", h=4)
            TT("vector", rec[:, b * 4:(b + 1) * 4].unsqueeze(2), pvv[:, :, 64:65], esink[:, b * 4:(b + 1) * 4].unsqueeze(2),
               ALU.add, [pvn, "esink"], [("rec", b)] + ALLWIN)
            P.op("vector", (lambda b=b: (lambda e: e.reciprocal(rec[:, b * 4:(b + 1) * 4], rec[:, b * 4:(b + 1) * 4])))(),
                 [("rec", b)], [("rec", b)])
            TT("vector", att_tm[:, b * 256:(b + 1) * 256].rearrange("p (h d) -> p h d", h=4), pvv[:, :, 0:64],
               rec[:, b * 4:(b + 1) * 4].unsqueeze(2).to_broadcast([128, 4, 64]), ALU.mult,
               [pvn, ("rec", b)], [("att_tm", b)] + ALLWIN)
        pb, pbn = psb()
        pbv = pb[:, 0:512].rearrange("p (k t) -> p k t", k=4)
        for cc in range(4):
            TR(pbv[:, cc, :], att_tm[:, cc * 128:(cc + 1) * 128], ident_b[:], [("att_tm", cc // 2), "ident_b"], [(pbn, cc)])
        ACT(mixT[:, 0:4, (i - 1) * 128:i * 128], pbv, AF.Copy, [(pbn, cc) for cc in range(4)],
            [("mixT", "att", i - 1)] + ALLWIN)
        conv_unit((i - 1) // 4, (i - 1) % 4)

    if stage <= 4:
        P.emit()
        return nc, dbg_outs

    HTALL = [(f"hT{a}", t) for a in range(2) for t in range(4)]
    P.dma("gpsimd", "d_wo", lambda e: e.dma_start(out=wo[:], in_=w_out.rearrange("(k p) n -> p k n", p=128)), [], ["wo"] + HTALL)
    QALL = [("qT", c, ci) for c in range(4) for ci in range(1, 5)]
    o3 = qT_off
    h2T = [P.sb(f"h2T{i}", [128, 8, 128], BF16, off=o3 + i * 2048) for i in range(2)]; o3 += 4096
    CONVDEAD = [("uT", c, ci) for c in range(4) for ci in range(6)] + [("bgT", c, ci) for c in range(4) for ci in range(1, 5)]
    rows_off = REG0 - 8 * 4096
    affTM = P.sb("affTM", [128, 16, 16], F32, off=rows_off)
    gm = P.sb("gm", [128, 16, 16], F32, off=rows_off + 1024)
    thr = P.sb("thr", [128, 16], F32, off=rows_off + 2048)
    affT = P.sb("affT", [16, 2048], F32, off=o3); o3 += 8192
    wr = P.sb("wr", [128, 8, 16], BF16, off=o3); o3 += 256
    sm = P.sb("sm", [128, 64], F32, off=o3); o3 += 256
    assert o3 <= qT_off + 4 * W * 2
    P.dma("gpsimd", "d_wr", lambda e: e.dma_start(out=wr[:], in_=w_router.rearrange("(k p) e -> p k e", p=128)), [], ["wr"] + QALL)
    def a3_A(tile):
        tcn = tile // 4
        mdeps = [("mixT", "att", tile)] + [("mixT", "conv", c, tcn) for c in range(4)]
        i = xt_rr[0] % 2
        xt_rr[0] += 1
        xtile, xn = xt[i], f"xt{i}"
        DMA("sync", "d_" + xn, xtile[:], x[128 + tile * 128:256 + tile * 128, :], [], [xn])
        for n in range(2):
            ps, psn = psf("v")
            for k in range(8):
                MM(ps[:], mixT[:, k, tile * 128:(tile + 1) * 128], wo[:, k, n * 512:(n + 1) * 512], k == 0, k == 7,
                   mdeps + ["wo"], [psn])
            TT("vector", tf[:, n * 512:(n + 1) * 512], ps[:], rows["GT1"][:, n * 512:(n + 1) * 512], ALU.mult,
               [psn] + rowdeps("GT1"), ["tf"])
        TT("vector", xtile[:], xtile[:], tf[:], ALU.add, [xn, "tf"], [xn])
        DMA("sync", "d_x1d_" + xn, x1d[tile * 128:(tile + 1) * 128, :], xtile[:], [xn], [("x1d", tile)])
        j = tile % 2
        norm_mod_sb(xtile, xn, "G2", "S2", hb[j], f"hb{j}")
        DMA("sync", f"d_h2loc{j}", h2loc[tile * 128:(tile + 1) * 128, :], hb[j][:], [f"hb{j}"], [("h2loc", tile)])

    def a3_B(tile):
        j = tile % 2
        h2v = h2T[j][:]
        pb, pbn = psb()
        pbv = pb[:].rearrange("p (k t) -> p k t", k=8)
        for k in range(8):
            TR(pbv[:, k, :], hb[j][:, k * 128:(k + 1) * 128], ident_b[:], [f"hb{j}", "ident_b"], [(pbn, k)])
        ACT(h2v, pbv, AF.Copy, [(pbn, k) for k in range(8)], [f"h2T{j}"] + QALL)
        ps, psn = psf("v")
        for k in range(8):
            MM(ps[:, 0:16], h2T[j][:, k, :], wr[:, k, :], k == 0, k == 7, [f"h2T{j}", "wr"], [psn])
        mx, nmx, ssum, ex = sm[:, 0:1], sm[:, 1:2], sm[:, 2:3], sm[:, 16:32]
        af = affTM[:, tile, :]
        RED("vector", mx, ps[:, 0:16], ALU.max, [psn], ["sm_mx"])
        TS("vector", nmx, mx, -1.0, None, ALU.mult, None, ["sm_mx"], ["sm_nmx"])
        ACT(ex, ps[:, 0:16], AF.Exp, [psn, "sm_nmx"], ["sm_ex"], bias=nmx)
        RED("vector", ssum, ex, ALU.add, ["sm_ex"], ["sm_sum"])
        P.op("vector", lambda e: e.reciprocal(sm[:, 2:3], sm[:, 2:3]), ["sm_sum"], ["sm_sum"])
        TS("vector", af, ex, ssum, None, ALU.mult, None, ["sm_ex", "sm_sum"], [("affTM", tile)])
        pt, ptn = psf("a")
        TR(pt[0:16, 0:128], af, ident_f[:], [("affTM", tile), "ident_f"], [ptn])
        ACT(affT[:, tile * 128:(tile + 1) * 128], pt[0:16, 0:128], AF.Copy, [ptn], [("affT", tile)] + QALL)

    a3_A(0)
    for tile in range(16):
        if tile + 1 < 16:
            a3_A(tile + 1)
        a3_B(tile)
    DMA("sync", "d_affloc", affloc, affT[:], [("affT", t) for t in range(16)], ["affloc"])

    if "a3" in dbg:
        d1 = dbg_out("x1", [2048, 1024])
        d2 = dbg_out("aff", [16, 2048])
        d3 = dbg_out("h2", [2048, 1024], BF16)
        e1 = DMA("sync", "d_dbg1", d1, x1d, [("x1d", t) for t in range(16)], ["dbg1"])
        e2 = DMA("sync", "d_dbg2", d2, affloc, ["affloc"], ["dbg2"])
        e3 = DMA("sync", "d_dbg3", d3, h2loc, [("h2loc", t) for t in range(16)], ["dbg3"])
        P.finish("sync", [e1, e2, e3])
    if stage <= 5:
        P.emit()
        return nc, dbg_outs

    FENCE = [k for k in P.res.keys() if not (isinstance(k, str) and (k.startswith("psf") or k in ("ident_b", "ident_f", "ones_b", "metat")))
             and not (isinstance(k, tuple) and k[0] in ("row", "h2T", "affTM", "x1d", "h2loc"))]
    P.dma("gpsimd", "d_ag_aff", lambda e: e.collective_compute("AllGather", ALU.bypass, replica_groups=GROUPS,
                                                               ins=[affloc.opt()], outs=[affall.opt()]),
          ["affloc"], ["affall", "agchain"], inc=1)
    wslot = [P.sb(f"wslot{i}", [128, 8, 1024], BF16, off=REG0 + i * 16384) for i in range(4)]
    wdt_ = P.sb("wd_", [128, 8, 1024], BF16, off=REG0 + 81920)
    wgv = w_gate.rearrange("e (k p) n -> e p k n", p=128)
    wuv = w_up.rearrange("e (k p) n -> e p k n", p=128)
    wdv = w_down.rearrange("e (k p) n -> e p k n", p=128)
    NTB, NITB, NE = 16, 7, 4
    ob = bgT_off + 32768
    AFt = P.sb("AFt", [128, NE, 64], F32, off=ob); ob += 1024
    FR = P.sb("FR", [128, NE, NTB], F32, off=ob); ob += 256
    Tt = P.sb("Tt", [128, NE, NTB], F32, off=ob); ob += 256
    tmpa = P.sb("tmpa", [128, NE, NTB], F32, off=ob); ob += 256
    get = P.sb("get", [128, NE, NTB], F32, off=ob); ob += 256
    cntb = P.sb("cntb", [128, NE * NTB], BF16, off=ob); ob += 128
    lo = P.sb("lo", [128, NE], F32, off=ob); ob += 64
    hi = P.sb("hi", [128, NE], F32, off=ob); ob += 64
    wdt = P.sb("wdt", [128, NE], F32, off=ob); ob += 64
    red = P.sb("red", [128, NE], F32, off=ob); ob += 64
    aix = P.sb("aix", [128, 8], F32, off=ob); ob += 64
    AIDX = P.sb("AIDX", [128, NE], I32, off=ob); ob += 64
    assert ob <= rt1_off + 6144
    cmpb = P.sb("cmpb", [128, NE, NTB, 64], BF16, off=REG0 + 49152)
    P.op("gpsimd", lambda e: e.iota(aix[:, 0:1], pattern=[[0, 1]], base=0, channel_multiplier=1,
                                    allow_small_or_imprecise_dtypes=True), (), ["aix"] + FENCE)
    TS("vector", aix[:, 1:2], aix[:, 0:1], 32.0, None, ALU.is_ge, None, ["aix"], ["aix"])
    for thv in (64.0, 96.0):
        TS("vector", aix[:, 2:3], aix[:, 0:1], thv, None, ALU.is_ge, None, ["aix"], ["aix"])
        TT("vector", aix[:, 1:2], aix[:, 1:2], aix[:, 2:3], ALU.add, ["aix"], ["aix"])
    TS("vector", aix[:, 1:2], aix[:, 1:2], 480.0, None, ALU.mult, None, ["aix"], ["aix"])
    TT("vector", aix[:, 1:2], aix[:, 1:2], aix[:, 0:1], ALU.add, ["aix"], ["aix"])
    TS("vector", aix[:, 1:2], aix[:, 1:2], metat[:, 7:8], None, ALU.add, None, ["aix", "metat"], ["aix"])
    for k in range(NE):
        TS("vector", aix[:, 4 + k:5 + k], aix[:, 1:2], 32.0 * k, None, ALU.add, None, ["aix"], ["aix"])
    CP("vector", AIDX[:], aix[:, 4:8], ["aix"], ["AIDX"])
    affrows = affall.rearrange("a (p j) -> (a p) j", j=64)
    for k in range(NE):
        P.dma("gpsimd", "d_AFt", (lambda k=k: (lambda e: e.indirect_dma_start(
            out=AFt[:, k, :], out_offset=None, in_=affrows, in_offset=bass.IndirectOffsetOnAxis(ap=AIDX[:, k:k + 1], axis=0))))(),
            ["affall", "AIDX"], [("AFt", k)] + FENCE)
    AFD = [("AFt", k) for k in range(NE)]
    P.dma("gpsimd", "d_ws0", lambda e: e.dma_start(out=wslot[0][:], in_=wgv[0]), [], ["ws0"] + FENCE)
    P.dma("gpsimd", "d_ws1", lambda e: e.dma_start(out=wslot[1][:], in_=wuv[0]), [], ["ws1"] + FENCE)
    P.dma("gpsimd", "d_wd", lambda e: e.dma_start(out=wdt_[:], in_=wdv[0]), [], ["wd_"] + FENCE)
    P.op("gpsimd", lambda e: e.iota(FR[:], pattern=[[0, NE], [1, NTB]], base=1, channel_multiplier=0,
                                    allow_small_or_imprecise_dtypes=True), (), ["FR"] + FENCE)
    TS("vector", FR[:], FR[:], 1.0 / (NTB + 1), None, ALU.mult, None, ["FR"], ["FR"])
    MSET("vector", lo[:], 0.0, ["lo"] + FENCE)
    MSET("vector", hi[:], 1.0, ["hi"])
    for it in range(NITB):
        TT("vector", wdt[:], hi[:], lo[:], ALU.subtract, ["hi", "lo"], ["wdt"])
        TT("vector", Tt[:], FR[:], wdt[:].unsqueeze(2).to_broadcast([128, NE, NTB]), ALU.mult, ["FR", "wdt"], ["Tt"])
        TT("vector", Tt[:], Tt[:], lo[:].unsqueeze(2).to_broadcast([128, NE, NTB]), ALU.add, ["Tt", "lo"], ["Tt"])
        TT("vector", cmpb[:], AFt[:].unsqueeze(2).to_broadcast([128, NE, NTB, 64]),
           Tt[:].unsqueeze(3).to_broadcast([128, NE, NTB, 64]), ALU.is_ge, AFD + ["Tt"], ["cmpb"] + FENCE)
        RED("vector", tmpa[:], cmpb[:], ALU.add, ["cmpb"], ["tmpa"])
        CP("vector", cntb[:], tmpa[:].rearrange("p e k -> p (e k)"), ["tmpa"], ["cntb"])
        ps, psn = psf("v")
        MM(ps[:, 0:NE * NTB], ones_b[:], cntb[:], True, True, ["cntb", "ones_b"], [psn])
        TS("vector", get[:].rearrange("p e k -> p (e k)"), ps[:, 0:NE * NTB], 1024.0, None, ALU.is_ge, None, [psn], ["get"])
        TT("vector", tmpa[:], Tt[:], get[:], ALU.mult, ["Tt", "get"], ["tmpa"])
        RED("vector", red[:], tmpa[:], ALU.max, ["tmpa"], ["red"])
        TT("vector", lo[:], lo[:], red[:], ALU.max, ["lo", "red"], ["lo"])
        TS("vector", tmpa[:], get[:], 2.0, None, ALU.mult, None, ["get"], ["tmpa"])
        TT("vector", tmpa[:], tmpa[:], Tt[:], ALU.add, ["tmpa", "Tt"], ["tmpa"])
        RED("vector", red[:], tmpa[:], ALU.min, ["tmpa"], ["red"])
        TT("vector", hi[:], hi[:], red[:], ALU.min, ["hi", "red"], ["hi"])
    thr = lo

    if "thr" in dbg:
        d1 = dbg_out("thr", [128, 4])
        e1 = DMA("sync", "d_dbg1", d1, lo[:], ["lo"], ["dbg1"])
        P.finish("sync", [e1])
    if stage <= 6:
        P.emit()
        return nc, dbg_outs

    TAB = P.sb("TAB", [128, 4, 128], F32, off=qT_off)
    ob2 = kT_off
    ones64 = P.sb("ones64", [128, 64], F32, off=ob2); ob2 += 256
    n4 = P.sb("n4", [128, 4], F32, off=ob2); ob2 += 64
    t16 = P.sb("t16", [128, 16], F32, off=ob2); ob2 += 64
    rhsU = P.sb("rhsU", [128, 4, 128], BF16, off=ob2); ob2 += 1024
    rhsI = P.sb("rhsI", [128, 4, 128], BF16, off=ob2); ob2 += 1024
    offs_sb = P.sb("offs_sb", [128, 4, 128], F32, off=ob2); ob2 += 2048
    nrow_sb = P.sb("nrow_sb", [128, 4, 128], F32, off=ob2); ob2 += 2048
    sval = P.sb("sval", [128, 8], F32, off=ob2); ob2 += 64
    koffs = P.sb("koffs", [128, 4, 8], F32, off=ob2); ob2 += 128
    pS = P.sb("pS", [128, 4, 8], F32, off=ob2); ob2 += 128
    oex = P.sb("oex", [128, 4, 8], F32, off=ob2); ob2 += 128
    rS = P.sb("rS", [128, 4, 8], F32, off=ob2); ob2 += 128
    jS = P.sb("jS", [128, 4, 8], F32, off=ob2); ob2 += 128
    gS = P.sb("gS", [128, 4, 8], F32, off=ob2); ob2 += 128
    tSf = P.sb("tSf", [128, 4, 8], F32, off=ob2); ob2 += 128
    RIDX = P.sb("RIDX", [128, 4, 8], I32, off=ob2); ob2 += 128
    TIDX = P.sb("TIDX", [128, 4, 8], I32, off=ob2); ob2 += 128
    ZIDXf = P.sb("ZIDXf", [128, 4, 16], F32, off=ob2); ob2 += 256
    ZIDX = P.sb("ZIDX", [128, 4, 16], I32, off=ob2); ob2 += 256
    yz = [P.sb(f"yz{i}", [128, 1024], BF16, off=rows_off + 3 * 4096 + i * 2048) for i in range(2)]
    assert ob2 <= bgT_off, ob2
    cmpP = P.sb("cmpP", [128, 4, 8, 128], F32, off=REG0 + 32768)
    Gt = P.sb("Gt", [128, 4, 8, 128], F32, off=REG0 + 49152)

    MSET("vector", ones64[:], 1.0, ["ones64"] + FENCE)
    TT("vector", TAB[:, :, 64:128], AFt[:], lo[:].unsqueeze(2).to_broadcast([128, 4, 64]), ALU.is_ge, AFD + ["lo"], ["TABm"] + FENCE)
    for e16 in range(4):
        P.op("vector", (lambda e16=e16: (lambda e: e.tensor_tensor_scan(out=TAB[:, e16, 0:64], data0=ones64[:], data1=TAB[:, e16, 64:128],
                                                                          initial=0.0, op0=ALU.mult, op1=ALU.add)))(),
             ["TABm", "ones64"], [("TABc", e16)])
    TABC = [("TABc", e16) for e16 in range(4)]
    TT("vector", TAB[:, :, 64:128], TAB[:, :, 64:128], AFt[:], ALU.mult, ["TABm"] + TABC + AFD, ["TABm"])
    DMA("sync", "d_tabd", tabd[0:512, :].rearrange("(e p) c -> p e c", p=128), TAB[:], ["TABm"] + TABC, ["tabd"])
    for k in range(4):
        CP("vector", n4[:, k:k + 1], TAB[:, k, 63:64], TABC, [("n4", k)])
    N4 = [("n4", k) for k in range(4)]
    TT("vector", rhsU[:], n4[:].unsqueeze(2).to_broadcast([128, 4, 128]), U_b[:].unsqueeze(1).to_broadcast([128, 4, 128]), ALU.mult,
       N4 + ["U_b"], ["rhsU"])
    TT("vector", rhsI[:], n4[:].unsqueeze(2).to_broadcast([128, 4, 128]), ident_b[:].unsqueeze(1).to_broadcast([128, 4, 128]), ALU.mult,
       N4 + ["ident_b"], ["rhsI"])
    ps, psn = psf("v")
    MM(ps[:], ones_b[:], rhsU[:].rearrange("p k q -> p (k q)"), True, True, ["rhsU", "ones_b"], [psn])
    CP("vector", offs_sb[:].rearrange("p k q -> p (k q)"), ps[:], [psn], ["offs_sb"])
    ps, psn = psf("v")
    MM(ps[:], ones_b[:], rhsI[:].rearrange("p k q -> p (k q)"), True, True, ["rhsI", "ones_b"], [psn])
    CP("vector", nrow_sb[:].rearrange("p k q -> p (k q)"), ps[:], [psn], ["nrow_sb"])
    P.op("gpsimd", lambda e: e.iota(sval[:], pattern=[[128, 8]], base=0, channel_multiplier=1,
                                    allow_small_or_imprecise_dtypes=True), (), ["sval"])
    P.op("gpsimd", lambda e: e.iota(koffs[:], pattern=[[128, 4], [0, 8]], base=0, channel_multiplier=0,
                                    allow_small_or_imprecise_dtypes=True), (), ["koffs"])
    P.op("gpsimd", lambda e: e.iota(ZIDXf[:], pattern=[[512, 4], [2048, 16]], base=0, channel_multiplier=1,
                                    allow_small_or_imprecise_dtypes=True), (), ["ZIDXf"])
    for j4 in range(4):
        P.dma("gpsimd", "d_ag_h2", (lambda j4=j4: (lambda e: e.collective_compute(
            "AllGather", ALU.bypass, replica_groups=GROUPS,
            ins=[h2loc[j4 * 512:(j4 + 1) * 512, :].opt()], outs=[h2all[j4 * 2048:(j4 + 1) * 2048, :].opt()])))(),
            [("h2loc", t) for t in range(j4 * 4, j4 * 4 + 4)] + ["agchain"], [("h2all", j4), "agchain"], inc=1)
    H2ALLD = [("h2all", j4) for j4 in range(4)]
    TS("vector", ZIDXf[:], ZIDXf[:], metat[:, 6:7], None, ALU.add, None, ["ZIDXf", "metat"], ["ZIDXf"])
    CP("vector", ZIDX[:], ZIDXf[:], ["ZIDXf"], ["ZIDX"])
    svb = sval[:].unsqueeze(1).to_broadcast([128, 4, 8])
    TT("vector", cmpP[:], offs_sb[:].unsqueeze(2).to_broadcast([128, 4, 8, 128]),
       svb.unsqueeze(3).to_broadcast([128, 4, 8, 128]), ALU.is_le, ["offs_sb", "sval"], ["cmpP"] + FENCE)
    RED("vector", pS[:], cmpP[:], ALU.add, ["cmpP"], ["pS"])
    TT("vector", cmpP[:], cmpP[:], nrow_sb[:].unsqueeze(2).to_broadcast([128, 4, 8, 128]), ALU.mult, ["cmpP", "nrow_sb"], ["cmpP"])
    RED("vector", oex[:], cmpP[:], ALU.add, ["cmpP"], ["oex"])
    TT("vector", rS[:], svb, oex[:], ALU.subtract, ["sval", "oex"], ["rS"])
    TT("vector", tSf[:], pS[:], koffs[:], ALU.add, ["pS", "koffs"], ["tSf"])
    CP("vector", RIDX[:], tSf[:], ["tSf"], ["RIDX"])
    for k in range(4):
        for c in range(8):
            P.dma("gpsimd", "d_G", (lambda k=k, c=c: (lambda e: e.indirect_dma_start(
                out=Gt[:, k, c, :], out_offset=None, in_=tabd[0:512, :], in_offset=bass.IndirectOffsetOnAxis(ap=RIDX[:, k, c:c + 1], axis=0))))(),
                ["tabd", "RIDX"], [("Gt", k, c), "cmpb"] if (k == 0 and c == 0) else [("Gt", k, c)])
    GALL = [("Gt", k, c) for k in range(4) for c in range(8)]
    cmpG = cmpP[:, :, :, 0:64]
    TT("vector", cmpG, Gt[:, :, :, 0:64], rS[:].unsqueeze(3).to_broadcast([128, 4, 8, 64]), ALU.is_le, GALL + ["rS"], ["cmpP"])
    RED("vector", jS[:], cmpG, ALU.add, ["cmpP"], ["jS"])
    TS("vector", oex[:], rS[:], 1.0, None, ALU.add, None, ["rS"], ["oex"])
    TT("vector", cmpG, Gt[:, :, :, 0:64], oex[:].unsqueeze(3).to_broadcast([128, 4, 8, 64]), ALU.is_equal, GALL + ["oex"], ["cmpP"])
    TT("vector", cmpG, cmpG, Gt[:, :, :, 64:128], ALU.mult, ["cmpP"] + GALL, ["cmpP"])
    RED("vector", gS[:], cmpG, ALU.add, ["cmpP"], ["gS"])
    TS("vector", tSf[:], pS[:], 64.0, None, ALU.mult, None, ["pS"], ["tSf"])
    TT("vector", tSf[:], tSf[:], jS[:], ALU.add, ["tSf", "jS"], ["tSf"])
    CP("vector", TIDX[:], tSf[:], ["tSf"], ["TIDX"])
    ra = P.sb("ra", [128, 4, 8], F32, off=ob2); rb = P.sb("rb", [128, 4, 8], F32, off=ob2 + 128)
    rj = P.sb("rj", [128, 4, 8], F32, off=ob2 + 256); GIDX = P.sb("GIDX", [128, 4, 8], I32, off=ob2 + 384)
    assert ob2 + 512 <= bgT_off
    TS("vector", ra[:], tSf[:], 2048.0, None, ALU.is_ge, None, ["tSf"], ["ra"])
    for thv in (4096.0, 6144.0):
        TS("vector", rb[:], tSf[:], thv, None, ALU.is_ge, None, ["tSf"], ["rb"])
        TT("vector", ra[:], ra[:], rb[:], ALU.add, ["ra", "rb"], ["ra"])
    TS("vector", rb[:], ra[:], -2048.0, None, ALU.mult, None, ["ra"], ["rb"])
    TT("vector", rb[:], rb[:], tSf[:], ALU.add, ["rb", "tSf"], ["rb"])
    TS("vector", rj[:], rb[:], 512.0, None, ALU.is_ge, None, ["rb"], ["rj"])
    for thv in (1024.0, 1536.0):
        TS("vector", oex[:], rb[:], thv, None, ALU.is_ge, None, ["rb"], ["oex"])
        TT("vector", rj[:], rj[:], oex[:], ALU.add, ["rj", "oex"], ["rj"])
    TT("vector", rj[:], rj[:], ra[:], ALU.subtract, ["rj", "ra"], ["rj"])
    TS("vector", rj[:], rj[:], 1536.0, None, ALU.mult, None, ["rj"], ["rj"])
    TT("vector", rj[:], rj[:], tSf[:], ALU.add, ["rj", "tSf"], ["rj"])
    CP("vector", GIDX[:], rj[:], ["rj"], ["GIDX"])
    ZSIDX = P.sb("ZSIDX", [128, 4, 8], I32, off=ob2 + 512)
    assert ob2 + 640 <= bgT_off
    TS("vector", rj[:], rb[:], 128.0, None, ALU.is_ge, None, ["rb"], ["rj"])
    for kk in range(2, 16):
        TS("vector", oex[:], rb[:], 128.0 * kk, None, ALU.is_ge, None, ["rb"], ["oex"])
        TT("vector", rj[:], rj[:], oex[:], ALU.add, ["rj", "oex"], ["rj"])
    TS("vector", rj[:], rj[:], 384.0, None, ALU.mult, None, ["rj"], ["rj"])
    TS("vector", oex[:], ra[:], -1920.0, None, ALU.mult, None, ["ra"], ["oex"])
    TT("vector", rj[:], rj[:], oex[:], ALU.add, ["rj", "oex"], ["rj"])
    TT("vector", rj[:], rj[:], tSf[:], ALU.add, ["rj", "tSf"], ["rj"])
    CP("vector", ZSIDX[:], rj[:], ["rj"], ["ZSIDX"])

    if "idx" in dbg:
        d1 = dbg_out("tidx", [128, 32])
        d2 = dbg_out("gS", [128, 32])
        e1 = DMA("sync", "d_dbg1", d1, tSf[:].rearrange("p a b -> p (a b)"), ["tSf", "TIDX"], ["dbg1"])
        e2 = DMA("sync", "d_dbg2", d2, gS[:].rearrange("p a b -> p (a b)"), ["gS"], ["dbg2"])
        P.finish("sync", [e1, e2])
    if stage <= 6.5:
        P.emit()
        return nc, dbg_outs

    hid = [P.sb(f"hid{i}", [128, 8, 512], BF16, off=qT_off + i * 8192) for i in range(2)]
    XS = P.sb("XS", [128, 8, 1024], BF16, off=bgT_off)
    xsT = P.sb("xsT", [128, 8, 1024], BF16, off=bgT_off + 16384)
    sgs = P.sb("sgs", [128, 512], F32, off=rt1_off + 4096)
    MSET("vector", XS[:], 0.0, ["XS"] + FENCE)
    ZD0 = []
    for t in range(8):
        ZD0.append(("Zd0", t))
        DMA("sync", "d_z0", Zd[t * 1024:(t + 1) * 1024, :].rearrange("(p c) d -> p c d", c=8), XS[:], ["XS"], [("Zd0", t)])
    yrr = [0]
    def issue_loads(k4):
        sg_, su_ = (k4 % 2) * 2, (k4 % 2) * 2 + 1
        wg_t, wu_t = wslot[sg_], wslot[su_]
        ex2 = (["cmpP"] if sg_ == 2 else [])
        ex3 = (["cmpb"] + GALL if su_ == 3 else [])
        if k4 > 0:
          P.dma("gpsimd", f"d_ws{sg_}", (lambda wg_t=wg_t, k4=k4: (lambda e: e.dma_start(out=wg_t[:], in_=wgv[k4])))(), [], [f"ws{sg_}"] + ex2 + (FENCE if k4 < 2 else []))
          P.dma("gpsimd", f"d_ws{su_}", (lambda wu_t=wu_t, k4=k4: (lambda e: e.dma_start(out=wu_t[:], in_=wuv[k4])))(), [], [f"ws{su_}"] + ex3 + (FENCE if k4 < 2 else []))
        for c in range(8):
            P.dma("gpsimd", f"d_XS{c}", (lambda k4=k4, c=c: (lambda e: e.indirect_dma_start(
                out=XS[:, c, :], out_offset=None, in_=h2all, in_offset=bass.IndirectOffsetOnAxis(ap=GIDX[:, k4, c:c + 1], axis=0))))(),
                H2ALLD + ["GIDX"], [("XS", c)] + (["XS"] if c == 0 else []))

    def issue_wd(k4):
        P.dma("gpsimd", "d_wd", (lambda k4=k4: (lambda e: e.dma_start(out=wdt_[:], in_=wdv[k4])))(), [], ["wd_"] + (FENCE if k4 < 1 else []))

    issue_loads(0)
    for k4 in range(4):
        sg_, su_ = (k4 % 2) * 2, (k4 % 2) * 2 + 1
        wg_t, wu_t = wslot[sg_], wslot[su_]
        for c in range(8):
            pb, pbn = psb()
            pbv = pb[:].rearrange("p (k t) -> p k t", k=8)
            for kc in range(8):
                TR(pbv[:, kc, :], XS[:, c, kc * 128:(kc + 1) * 128], ident_b[:], [("XS", c), "XS", "ident_b"], [(pbn, kc)])
            ACT(xsT[:, :, c * 128:(c + 1) * 128], pbv, AF.Copy, [(pbn, kc) for kc in range(8)], [("xsT", c)] + (FENCE if k4 == 0 else []))
        if k4 < 3:
            issue_loads(k4 + 1)
        for sch in range(2):
            hd = hid[(k4 * 2 + sch) % 2]
            hdn = f"hid{(k4 * 2 + sch) % 2}"
            xdeps = [("xsT", sch * 4 + t) for t in range(4)]
            for fo in range(8):
                pa, pan = psf("a")
                for kc in range(8):
                    MM(pa[:], wg_t[:, kc, fo * 128:(fo + 1) * 128], xsT[:, kc, sch * 512:(sch + 1) * 512], kc == 0, kc == 7,
                       [f"ws{sg_}"] + xdeps, [pan])
                pu, pun = psf("v")
                for kc in range(8):
                    MM(pu[:], wu_t[:, kc, fo * 128:(fo + 1) * 128], xsT[:, kc, sch * 512:(sch + 1) * 512], kc == 0, kc == 7,
                       [f"ws{su_}"] + xdeps, [pun])
                ACT(sgs[:], pa[:], AF.Silu, [pan], ["sgs"] + (FENCE if k4 == 0 and sch == 0 and fo == 0 else []))
                TT("vector", hd[:, fo, :], pu[:], sgs[:], ALU.mult, [pun, "sgs"], [(hdn, fo)] + (FENCE + ["TABm"] + TABC if k4 == 0 else []))
            for t in range(4):
                c = sch * 4 + t
                yi = yrr[0] % 2
                yrr[0] += 1
                for dn in range(2):
                    py, pyn = psf("v")
                    for kc in range(8):
                        MM(py[:], hd[:, kc, t * 128:(t + 1) * 128], wdt_[:, kc, dn * 512:(dn + 1) * 512], kc == 0, kc == 7,
                           [(hdn, kc), "wd_"], [pyn])
                    TS("vector", yz[yi][:, dn * 512:(dn + 1) * 512], py[:], gS[:, k4, c:c + 1], None, ALU.mult, None,
                       [pyn, "gS"], [f"yz{yi}"])
                P.dma("gpsimd", f"d_sz{yi}", (lambda yi=yi, k4=k4, c=c: (lambda e: e.indirect_dma_start(
                    out=Zd, out_offset=bass.IndirectOffsetOnAxis(ap=ZSIDX[:, k4, c:c + 1], axis=0), in_=yz[yi][:], in_offset=None,
                    compute_op=ALU.add, oob_is_err=True)))(), [f"yz{yi}", "ZSIDX", "Zd"] + ZD0, ["Zd"])
        if k4 < 3:
            issue_wd(k4 + 1)

    if stage <= 7:
        P.emit()
        return nc, dbg_outs

    gfrow = P.sb("gfrow", [128, 1024], F32, off=rows_off + 4096)
    DMA("sync", "d_gfrow", gfrow[:], g_final.partition_broadcast(128), [], ["gfrow"] + FENCE)
    z4 = [P.sb(f"z4_{i}", [128, 4, 1024], BF16, off=bgT_off + i * 8192) for i in range(2)]
    evs = []

    def z_allgather(j16):
        P.dma("gpsimd", "d_ag_z", (lambda j16=j16: (lambda e: e.collective_compute(
            "AllGather", ALU.bypass, replica_groups=GROUPS,
            ins=[Zd[j16 * 512:(j16 + 1) * 512, :].opt()], outs=[Zall[j16 * 2048:(j16 + 1) * 2048, :].opt()])))(),
            ["Zd", "agchain"], [("Zall", j16), "agchain"], inc=1)

    def phaseC_tile(tile):
        i = xt_rr[0] % 2
        xt_rr[0] += 1
        xtile, xn = xt[i], f"xt{i}"
        zi = tile % 2
        DMA("sync", "d_" + xn, xtile[:], x1d[tile * 128:(tile + 1) * 128, :], [("x1d", tile)], [xn])
        for r in range(4):
            P.dma("gpsimd", f"d_z4_{zi}_{r}", (lambda zi=zi, r=r, tile=tile: (lambda e: e.indirect_dma_start(
                out=z4[zi][:, r, :], out_offset=None, in_=Zall, in_offset=bass.IndirectOffsetOnAxis(ap=ZIDX[:, r, tile:tile + 1], axis=0))))(),
                [("Zall", tile), "ZIDX"], [(f"z4_{zi}", r)] + ([("XS", c) for c in range(8)] + ["XS"] if tile < 2 else []))
        zd = [(f"z4_{zi}", r) for r in range(4)]
        TT("vector", tf[:], z4[zi][:, 0, :], z4[zi][:, 1, :], ALU.add, zd, ["tf"])
        TT("vector", tf[:], tf[:], z4[zi][:, 2, :], ALU.add, zd + ["tf"], ["tf"])
        TT("vector", tf[:], tf[:], z4[zi][:, 3, :], ALU.add, zd + ["tf"], ["tf"])
        TT("vector", tf[:], tf[:], rows["GT2"][:], ALU.mult, ["tf"] + rowdeps("GT2"), ["tf"])
        TT("vector", xtile[:], xtile[:], tf[:], ALU.add, [xn, "tf"], [xn])
        ss = small[:, 16:17]
        rstd = small[:, 17:18]
        ACT(tf[:], xtile[:], AF.Square, [xn], ["tf"])
        RED("vector", ss, tf[:], ALU.add, ["tf"], ["ss"])
        TS("vector", rstd, ss, 1.0 / 1024.0, 1e-6, ALU.mult, ALU.add, ["ss"], ["rstd"])
        ACT(rstd, rstd, AF.Ln, ["rstd"], ["rstd"])
        ACT(rstd, rstd, AF.Exp, ["rstd"], ["rstd"], scale=-0.5)
        ACT(tf[:], xtile[:], AF.Copy, [xn, "rstd"], ["tf"], scale=rstd)
        TT("vector", xtile[:], tf[:], gfrow[:], ALU.mult, ["tf", "gfrow"], [xn])
        evs.append(DMA("sync", "d_out_" + xn, out[tile * 128:(tile + 1) * 128, :], xtile[:], [xn], [("out", tile)]))

    for j16 in range(16):
        z_allgather(j16)
        if j16 >= 1:
            phaseC_tile(j16 - 1)
    phaseC_tile(15)
    P.finish("sync", evs)
    P.emit()
    return nc, dbg_outs


def make_in_maps(inp):
    x = np.ascontiguousarray(inp["x"], dtype=np.float32)
    maps = []
    for c in range(NCORES):
        b, q = c // 4, c % 4
        t0 = q * 2048
        xw = np.zeros((W, 1024), np.float32)
        lo, hi = t0 - 128, t0 + 2048 + 128
        slo, shi = max(lo, 0), min(hi, 8192)
        xw[slo - lo:shi - lo] = x[b, slo:shi]
        ccv = np.stack([inp["c"][b].reshape(8, 128).T, inp["c_ctx"].reshape(8, 128).T], axis=-1)
        meta = np.zeros((128, 80), np.float32)
        meta[:, 0] = 1.0 if q > 0 else 0.0
        meta[:, 1] = 1.0 if q < 3 else 0.0
        meta[:, 2] = float(q * 32 - 2)
        meta[:, 3] = float(q * 2048)
        meta[:, 4] = float(4 * q * 128)
        meta[:, 5] = float(q * 8192)
        meta[:, 6] = float(q * 128)
        meta[:, 7] = float(4 * q * 32)
        for k in range(4):
            meta[:, 8 + (4 * q + k) * 4 + k] = 1.0
        maps.append({
            "x": xw, "ctx": np.ascontiguousarray(inp["ctx"][b]), "cc": np.ascontiguousarray(ccv.reshape(128, 16)),
            "meta": meta, "w_ada": inp["w_ada"][0], "b_ada": inp["b_ada"][0], "g_mix": inp["g_mix"][0],
            "g_ffn": inp["g_ffn"][0], "g_final": inp["g_final"], "w_in": inp["w_in"][0], "conv_w": inp["conv_w"][0],
            "sink": inp["sink"][0], "w_out": inp["w_out"][0], "w_router": inp["w_router"][0],
            "w_gate": np.ascontiguousarray(inp["w_gate"][0, 4 * q:4 * q + 4]),
            "w_up": np.ascontiguousarray(inp["w_up"][0, 4 * q:4 * q + 4]),
            "w_down": np.ascontiguousarray(inp["w_down"][0, 4 * q:4 * q + 4]),
        })
    return maps


def kernel(**inputs):
    inp = {k: np.asarray(v) for k, v in inputs.items()}
    nc, _ = build_nc()
    res = run_bass_kernel_spmd(nc, make_in_maps(inp), core_ids=list(range(NCORES)))
    outp = np.zeros((2, 8192, 1024), np.float32)
    for c in range(NCORES):
        b, q = c // 4, c % 4
        outp[b, q * 2048:(q + 1) * 2048] = res.results[c]["out"]
    return outp
```
